# Optimizing a Trainium2 kernel written in Bass

```python
import math
import jax
import jax.numpy as jnp
from jax import lax
import numpy as np

D_MODEL = 1024
BATCH = 4
SEQ = 8192
DEPTH = 1

N_META = 16
CHUNK = 64
EPS = 1e-6
HG_HEADS = 4
HG_DIM = 128
HG_WIDTH = HG_HEADS * HG_DIM
GD_HEADS = 4
GD_DK = 128
GD_DV = 128
CONV_W = 4
N_GROUPS = 4
EXPERTS_PER_GROUP = 8
N_EXPERTS = N_GROUPS * EXPERTS_PER_GROUP
TOP_K_FINE = 2
D_FF_EXPERT = 512
MOE_BLOCK = 128
PROJ_SIZES = (HG_WIDTH, HG_WIDTH, HG_WIDTH, HG_WIDTH,
              GD_HEADS * GD_DK, GD_HEADS * GD_DK, GD_HEADS * GD_DV, GD_HEADS * GD_DV,
              GD_HEADS, GD_HEADS, D_MODEL, D_MODEL)
D_PROJ = sum(PROJ_SIZES)

kernel_name = 'hybrid_hgrn2_gdn_hmoe_block'


def rmsnorm(x, g):
    xf = x.astype(jnp.float32)
    y = xf * lax.rsqrt(jnp.mean(xf * xf, axis=-1, keepdims=True) + EPS) * g.astype(jnp.float32)
    return y.astype(x.dtype)


def head_rmsnorm(o, g):
    return o * lax.rsqrt(jnp.mean(o * o, axis=-1, keepdims=True) + EPS) * g.astype(jnp.float32)


def l2norm(t):
    return t * lax.rsqrt(jnp.sum(t * t, axis=-1, keepdims=True) + EPS)


def causal_depthwise_conv(x, w):
    return lax.conv_general_dilated(
        x, w[:, None, :].astype(x.dtype), window_strides=(1,), padding=[(CONV_W - 1, 0)],
        dimension_numbers=('NWC', 'WIO', 'NWC'), feature_group_count=x.shape[-1])


def run_chunked(step, state0, seqs):
    bsz, length = seqs[0].shape[:2]
    n_chunks = (length - N_META) // CHUNK
    state, out_meta = step(state0, tuple(s[:, :N_META] for s in seqs))
    real = tuple(jnp.moveaxis(s[:, N_META:].reshape(bsz, n_chunks, CHUNK, *s.shape[2:]), 1, 0) for s in seqs)
    _, out_real = lax.scan(step, state, real)
    out_real = jnp.moveaxis(out_real, 0, 1).reshape(bsz, n_chunks * CHUNK, *out_real.shape[3:])
    return jnp.concatenate([out_meta, out_real], axis=1)


def hgrn2_chunk(S, inp):
    q, k, v, logf = inp
    C = q.shape[1]
    b = jnp.cumsum(logf, axis=1)
    incl = jnp.tril(jnp.ones((C, C), dtype=bool))
    decay = jnp.exp(jnp.where(incl[None, :, :, None, None], b[:, :, None] - b[:, None, :], -jnp.inf))
    attn = jnp.einsum('bthk,bshk,btshk->bhts', q, k, decay)
    o = jnp.einsum('bthk,bhkv->bthv', q * jnp.exp(b), S) + jnp.einsum('bhts,bshv->bthv', attn, v)
    b_end = b[:, -1:]
    S_new = jnp.exp(b_end[:, 0])[..., None] * S + jnp.einsum('bshk,bshv->bhkv', k * jnp.exp(b_end - b), v)
    return S_new, o


def gdn_chunk(S, inp):
    q, k, v, beta, g = inp
    C = q.shape[1]
    q, k, v = (t.transpose(0, 2, 1, 3) for t in (q, k, v))
    beta = beta.transpose(0, 2, 1)
    G = jnp.cumsum(g, axis=1).transpose(0, 2, 1)
    incl = jnp.tril(jnp.ones((C, C), dtype=bool))
    strict = jnp.tril(jnp.ones((C, C), dtype=bool), -1)
    ratio = jnp.exp(jnp.where(incl, G[..., :, None] - G[..., None, :], -jnp.inf))
    L = jnp.where(strict, beta[..., :, None] * ratio * jnp.einsum('bhtk,bhsk->bhts', k, k), 0.0)
    rhs = jnp.concatenate([beta[..., None] * v, (beta * jnp.exp(G))[..., None] * k], axis=-1)
    sol = lax.linalg.triangular_solve(jnp.eye(C, dtype=L.dtype) + L, rhs,
                                      left_side=True, lower=True, unit_diagonal=True)
    u0, w = sol[..., :GD_DV], sol[..., GD_DV:]
    u = u0 - jnp.einsum('bhtk,bhkv->bhtv', w, S)
    attn = jnp.einsum('bhtk,bhsk->bhts', q, k) * ratio
    o = jnp.exp(G)[..., None] * jnp.einsum('bhtk,bhkv->bhtv', q, S) + jnp.einsum('bhts,bhsv->bhtv', attn, u)
    G_end = G[..., -1:]
    S_new = jnp.exp(G_end)[..., None] * S + jnp.einsum('bhsk,bhsv->bhkv', k * jnp.exp(G_end - G)[..., None], u)
    return S_new, o.transpose(0, 2, 1, 3)


def token_mixer(h, lb, w_in, conv_w, A_log, dt_bias, hg_norm_g, gd_norm_g, hg_up, gd_up, w_out):
    f32 = jnp.float32
    bsz, length, _ = h.shape
    idx = []
    acc = 0
    for s in PROJ_SIZES[:-1]:
        acc += s
        idx.append(acc)
    proj = h @ w_in.astype(h.dtype)
    hq, hf, hi, hg, gq, gk, gv, gz, gbeta, ga, pre_ga, pre_gb = jnp.split(proj, idx, axis=-1)

    def heads(t, n_h, d):
        return t.reshape(bsz, length, n_h, d)
    fgate = lb + (1.0 - lb) * jax.nn.sigmoid(hf.astype(f32))
    q_a = heads(jax.nn.silu(hq.astype(f32)), HG_HEADS, HG_DIM)
    k_a = heads(1.0 - fgate, HG_HEADS, HG_DIM)
    logf = heads(jnp.log(fgate), HG_HEADS, HG_DIM)
    v_a = heads(hi.astype(f32), HG_HEADS, HG_DIM)
    S0a = jnp.zeros((bsz, HG_HEADS, HG_DIM, HG_DIM), f32)
    o_a = run_chunked(hgrn2_chunk, S0a, (q_a, k_a, logf and v_a, logf) if False else (q_a, k_a, v_a, logf))
    o_a = head_rmsnorm(o_a, hg_norm_g) * jax.nn.silu(heads(hg.astype(f32), HG_HEADS, HG_DIM))
    o_a = o_a.reshape(bsz, length, HG_WIDTH).astype(h.dtype)

    qkv = jax.nn.silu(causal_depthwise_conv(jnp.concatenate([gq, gk, gv], axis=-1), conv_w)).astype(f32)
    cq, ck, cv = jnp.split(qkv, [GD_HEADS * GD_DK, 2 * GD_HEADS * GD_DK], axis=-1)
    q_b = l2norm(heads(cq, GD_HEADS, GD_DK)) * (GD_DK ** -0.5)
    k_b = l2norm(heads(ck, GD_HEADS, GD_DK))
    v_b = heads(cv, GD_HEADS, GD_DV)
    beta = jax.nn.sigmoid(gbeta.astype(f32))
    g = -jnp.exp(A_log.astype(f32)) * jax.nn.softplus(ga.astype(f32) + dt_bias.astype(f32))
    S0b = jnp.zeros((bsz, GD_HEADS, GD_DK, GD_DV), f32)
    o_b = run_chunked(gdn_chunk, S0b, (q_b, k_b, v_b, beta, g))
    o_b = head_rmsnorm(o_b, gd_norm_g) * jax.nn.silu(heads(gz.astype(f32), GD_HEADS, GD_DV))
    o_b = o_b.reshape(bsz, length, GD_HEADS * GD_DV).astype(h.dtype)

    merged = (jax.nn.sigmoid(pre_ga) * (o_a @ hg_up.astype(h.dtype))
              + jax.nn.sigmoid(pre_gb) * (o_b @ gd_up.astype(h.dtype)))
    return merged @ w_out.astype(h.dtype)


def hierarchical_moe(h, rg_w, rg_b, re_w, re_b, w_gate, w_up, w_down):
    bsz, length, d = h.shape
    T = bsz * length
    xf = h.reshape(T, d)
    g_logits = (xf @ rg_w.astype(h.dtype)).astype(jnp.float32) + rg_b.astype(jnp.float32)
    g_prob = jax.nn.softmax(g_logits, axis=-1)
    grp = jnp.argmax(g_logits, axis=-1)
    p_grp = jnp.take_along_axis(g_prob, grp[:, None], axis=-1)
    e_logits = ((xf @ re_w.astype(h.dtype)).astype(jnp.float32) + re_b.astype(jnp.float32))
    e_logits = e_logits.reshape(T, N_GROUPS, EXPERTS_PER_GROUP)
    sel = jnp.take_along_axis(e_logits, grp[:, None, None], axis=1)[:, 0]
    top_p, top_i = lax.top_k(jax.nn.softmax(sel, axis=-1), TOP_K_FINE)
    weights = p_grp * top_p / jnp.sum(top_p, axis=-1, keepdims=True)
    expert_ids = grp[:, None] * EXPERTS_PER_GROUP + top_i

    A = T * TOP_K_FINE
    e_flat = expert_ids.reshape(A).astype(jnp.int32)
    tok_flat = jnp.repeat(jnp.arange(T, dtype=jnp.int32), TOP_K_FINE)
    w_flat = weights.reshape(A)
    order = jnp.argsort(e_flat)
    e_s, tok_s, w_s = e_flat[order], tok_flat[order], w_flat[order]
    counts = jnp.bincount(e_flat, length=N_EXPERTS)
    start = jnp.cumsum(counts) - counts
    padded = ((counts + MOE_BLOCK - 1) // MOE_BLOCK) * MOE_BLOCK
    pend = jnp.cumsum(padded)
    pstart = pend - padded
    dest = pstart[e_s] + (jnp.arange(A, dtype=jnp.int32) - start[e_s])
    n_blocks = -(-A // MOE_BLOCK) + N_EXPERTS
    P = n_blocks * MOE_BLOCK
    row_tok = jnp.full((P,), T, dtype=jnp.int32).at[dest].set(tok_s)
    x_ext = jnp.concatenate([xf, jnp.zeros((1, d), xf.dtype)], axis=0)
    x_buf = x_ext[row_tok].reshape(n_blocks, MOE_BLOCK, d)
    blk_expert = jnp.clip(jnp.searchsorted(pend, jnp.arange(n_blocks) * MOE_BLOCK, side='right'), 0, N_EXPERTS - 1)

    def expert_block(args):
        xb, e = args
        a = xb @ w_gate[e].astype(xb.dtype)
        u = xb @ w_up[e].astype(xb.dtype)
        return (jax.nn.silu(a) * u) @ w_down[e].astype(xb.dtype)

    y_buf = lax.map(expert_block, (x_buf, blk_expert)).reshape(P, d)
    y_s = (y_buf[dest].astype(jnp.float32) * w_s[:, None]).astype(h.dtype)
    out = jnp.zeros((T, d), h.dtype).at[tok_s].add(y_s)
    return out.reshape(bsz, length, d)


def setup_inputs(seed: int = 0) -> dict:
    key = jax.random.key(seed)
    ks = jax.random.split(key, 24)
    f32 = jnp.float32
    nrm = lambda k, shape, scale: jax.random.normal(k, shape, f32) * scale
    dt = jnp.exp(jax.random.uniform(ks[7], (DEPTH, GD_HEADS), f32, math.log(1e-3), math.log(1e-1)))
    return {
        'x': nrm(ks[0], (BATCH, SEQ, D_MODEL), 1.0),
        'meta_tokens': nrm(ks[1], (N_META, D_MODEL), 1.0),
        'hg_lb_logits': nrm(ks[2], (DEPTH + 1, HG_WIDTH), 0.5),
        'norm_mix_g': 1.0 + nrm(ks[3], (DEPTH, D_MODEL), 0.01),
        'w_in': nrm(ks[4], (DEPTH, D_MODEL, D_PROJ), D_MODEL ** -0.5),
        'gd_conv_w': nrm(ks[5], (DEPTH, CONV_W, 2 * GD_HEADS * GD_DK + GD_HEADS * GD_DV), CONV_W ** -0.5),
        'gd_A_log': jnp.log(jax.random.uniform(ks[6], (DEPTH, GD_HEADS), f32, 1.0, 16.0)),
        'gd_dt_bias': dt + jnp.log(-jnp.expm1(-dt)),
        'hg_norm_g': 1.0 + nrm(ks[8], (DEPTH, HG_DIM), 0.01),
        'gd_norm_g': 1.0 + nrm(ks[9], (DEPTH, GD_DV), 0.01),
        'hg_up': nrm(ks[10], (DEPTH, HG_WIDTH, D_MODEL), HG_WIDTH ** -0.5),
        'gd_up': nrm(ks[11], (DEPTH, GD_HEADS * GD_DV, D_MODEL), (GD_HEADS * GD_DV) ** -0.5),
        'w_out': nrm(ks[12], (DEPTH, D_MODEL, D_MODEL), D_MODEL ** -0.5),
        'norm_ffn_g': 1.0 + nrm(ks[13], (DEPTH, D_MODEL), 0.01),
        'router_group_w': nrm(ks[14], (DEPTH, D_MODEL, N_GROUPS), D_MODEL ** -0.5),
        'router_group_b': nrm(ks[15], (DEPTH, N_GROUPS), 0.01),
        'router_expert_w': nrm(ks[16], (DEPTH, D_MODEL, N_EXPERTS), D_MODEL ** -0.5),
        'router_expert_b': nrm(ks[17], (DEPTH, N_EXPERTS), 0.01),
        'w_gate': nrm(ks[18], (DEPTH, N_EXPERTS, D_MODEL, D_FF_EXPERT), D_MODEL ** -0.5),
        'w_up': nrm(ks[19], (DEPTH, N_EXPERTS, D_MODEL, D_FF_EXPERT), D_MODEL ** -0.5),
        'w_down': nrm(ks[20], (DEPTH, N_EXPERTS, D_FF_EXPERT, D_MODEL), D_FF_EXPERT ** -0.5),
        'final_norm_g': 1.0 + nrm(ks[21], (D_MODEL,), 0.01),
    }


def reference(x, meta_tokens, hg_lb_logits, norm_mix_g, w_in, gd_conv_w, gd_A_log, gd_dt_bias,
              hg_norm_g, gd_norm_g, hg_up, gd_up, w_out, norm_ffn_g, router_group_w, router_group_b,
              router_expert_w, router_expert_b, w_gate, w_up, w_down, final_norm_g):
    bsz = x.shape[0]
    meta = jnp.broadcast_to(meta_tokens[None].astype(x.dtype), (bsz, N_META, x.shape[-1]))
    h = jnp.concatenate([meta, x], axis=1)
    lb_table = jnp.cumsum(jax.nn.softmax(hg_lb_logits.astype(jnp.float32), axis=0), axis=0)
    for l in range(DEPTH):
        h = h + token_mixer(rmsnorm(h, norm_mix_g[l]), lb_table[l], w_in[l], gd_conv_w[l], gd_A_log[l],
                            gd_dt_bias[l], hg_norm_g[l], gd_norm_g[l], hg_up[l], gd_up[l], w_out[l])
        h = h + hierarchical_moe(rmsnorm(h, norm_ffn_g[l]), router_group_w[l], router_group_b[l],
                                 router_expert_w[l], router_expert_b[l], w_gate[l], w_up[l], w_down[l])
    h = rmsnorm(h, final_norm_g)
    return h[:, N_META:]
```

```python
import numpy as np
import concourse.bass as bass
import concourse.mybir as mybir
from concourse.bass_utils import run_bass_kernel_spmd

F32 = mybir.dt.float32
BF16 = mybir.dt.bfloat16
I32 = mybir.dt.int32
U32 = mybir.dt.uint32
AF = mybir.ActivationFunctionType
ALU = mybir.AluOpType
AX = mybir.AxisListType


class Buf:
    __slots__ = ("name", "lw", "rd", "psum")

    def __init__(self, name="", psum=False):
        self.name = name
        self.lw = None
        self.rd = {}
        self.psum = psum


class Prog:
    ENGS = ("pe", "act", "dve", "pool", "sp")

    def __init__(self, kdma=6):
        self.ops = {e: [] for e in self.ENGS}
        self.waited = {e: {} for e in self.ENGS}
        self.ndma = {e: 0 for e in self.ENGS}
        self.K = kdma
        self.out_toks = []
        self.pending = {e: [] for e in self.ENGS}

    def barrier(self):
        toks = []
        for e in self.ENGS:
            for i in range(len(self.ops[e]) - 1, -1, -1):
                op = self.ops[e][i]
                if (not op["dma"]) and op["fn"] is not None:
                    toks.append((e, i))
                    break
            n = self.ndma[e]
            for slot in range(min(self.K, n)):
                last = ((n - 1 - slot) // self.K) * self.K + slot
                toks.append((("dma", e, slot), 16 * (last // self.K + 1)))
        for e in self.ENGS:
            self.pending[e] = list(toks)

    def _emit(self, eng, fn, reads, writes, dma=False, extra=()):
        deps = {}

        def need(tok):
            if tok is None:
                return
            k, v = tok
            if eng == "pe" and k == "pe":
                return
            if deps.get(k, -1) < v:
                deps[k] = v
        def need_x(tok):
            if tok is not None and tok[0] != eng:
                need(tok)
        for b in reads:
            if b.psum:
                need_x(b.lw)
            else:
                need(b.lw)
        for b in writes:
            if b.psum:
                need_x(b.lw)
            else:
                need(b.lw)
                for k, v in b.rd.items():
                    need((k, v))
        for t in extra:
            need(t)
        if self.pending[eng]:
            for t in self.pending[eng]:
                need(t)
            self.pending[eng] = []
        if dma:
            i = self.ndma[eng]
            self.ndma[eng] += 1
            slot = i % self.K
            val = 16 * (i // self.K + 1)
            key = ("dma", eng, slot)
            if val > 16:
                need((key, val - 16))
            tok = (key, val)
        else:
            tok = (eng, len(self.ops[eng]))
        waits = []
        w = self.waited[eng]
        for k, v in deps.items():
            if w.get(k, -1) < v:
                w[k] = v
                waits.append((k, v))
        self.ops[eng].append(dict(waits=waits, fn=fn, tok=tok, dma=dma))
        for b in reads:
            if b.psum:
                b.lw = tok
                continue
            k, v = tok
            if b.rd.get(k, -1) < v:
                b.rd[k] = v
        for b in writes:
            b.lw = tok
            b.rd = {}
        return tok

    def pe(self, fn, reads=(), writes=()):
        return self._emit("pe", fn, reads, writes)

    def act(self, fn, reads=(), writes=()):
        return self._emit("act", fn, reads, writes)

    def dve(self, fn, reads=(), writes=()):
        return self._emit("dve", fn, reads, writes)

    def pool(self, fn, reads=(), writes=()):
        return self._emit("pool", fn, reads, writes)

    def dma(self, fn, reads=(), writes=(), q="sp", out=False):
        t = self._emit(q, fn, reads, writes, dma=True)
        if out:
            self.out_toks.append(t)
        return t

    def finalize(self, nc):
        self._emit("sp", None, (), (), extra=self.out_toks)
        targets = {e: set() for e in self.ENGS}
        for e in self.ENGS:
            for op in self.ops[e]:
                for k, v in op["waits"]:
                    if isinstance(k, str):
                        targets[k].add(v)
        rank = {e: {} for e in self.ENGS}
        for e in self.ENGS:
            r = 0
            for i, op in enumerate(self.ops[e]):
                if (not op["dma"]) and i in targets[e]:
                    assert op["fn"] is not None
                    r += 1
                    rank[e][i] = r
        import contextlib
        with contextlib.ExitStack() as st:
            csem = {e: st.enter_context(nc.semaphore("c_" + e)) for e in self.ENGS}
            dsem = {}
            for e in self.ENGS:
                if self.ndma[e] > 0:
                    for s in range(min(self.K, self.ndma[e])):
                        dsem[("dma", e, s)] = st.enter_context(nc.semaphore("d_%s_%d" % (e, s)))
            block = st.enter_context(nc.Block())

            def run(e):
                def body(engine):
                    for i, op in enumerate(self.ops[e]):
                        for k, v in op["waits"]:
                            if isinstance(k, str):
                                engine.wait_ge(csem[k], rank[k][v])
                            else:
                                engine.wait_ge(dsem[k], v)
                        if op["fn"] is None:
                            continue
                        ins = op["fn"](engine)
                        if op["dma"]:
                            ins.then_inc(dsem[op["tok"][0]], 16)
                        elif i in rank[e]:
                            ins.then_inc(csem[e], 1)
                return body
            block.tensor(run("pe"))
            block.scalar(run("act"))
            block.vector(run("dve"))
            block.gpsimd(run("pool"))
            block.sync(run("sp"))


D = 1024
KC = 8
SBT = 256
NT = SBT // 128
NCH = SBT // 64
EPS = 1e-6
C_HQ, C_HF, C_HI, C_HG = 0, 512, 1024, 1536
C_GQ, C_GK, C_GV, C_GZ = 2048, 2560, 3072, 3584
C_GB, C_GA, C_PA, C_PB = 4096, 4100, 4104, 5128
DPROJ = 6152


class Arena:
    def __init__(self, ap, words):
        self.ap = ap
        self.words = words
        self.off = 0
        self.peak = 0

    def mark(self):
        return self.off

    def reset(self, m):
        self.off = m

    def alloc(self, free_shape, dt):
        n = 1
        for s in free_shape:
            n *= s
        esz = 4 if dt in (F32, I32, U32) else 2
        words = (n * esz + 3) // 4
        words = (words + 7) // 8 * 8
        assert self.off + words <= self.words, ("arena overflow", self.off, words, self.words)
        v = self.ap[:, self.off:self.off + words]
        self.off += words
        self.peak = max(self.peak, self.off)
        if esz == 2:
            v = v.bitcast(dt)
        elif dt != F32:
            v = v.bitcast(dt)
        v = v[:, 0:n]
        if len(free_shape) > 1:
            names = ["a%d" % i for i in range(len(free_shape))]
            pat = "p (%s) -> p %s" % (" ".join(names), " ".join(names))
            v = v.rearrange(pat, **{nm: s for nm, s in zip(names, free_shape)})
        return v


class KB:
    def __init__(self, npre, nfull, debug=()):
        import contextlib
        self.npre, self.nfull = npre, nfull
        self.nsb = npre + nfull
        self.ntok = self.nsb * SBT
        self.nfull_tok = nfull * SBT
        self.debug = set(debug)
        self.nc = bass.Bass("TRN2", target_bir_lowering=False)
        self.P = Prog()
        self.d = {}
        self.st = contextlib.ExitStack()

    def din(self, name, shape, dt=F32):
        self.d[name] = self.nc.dram_tensor(name, list(shape), dt, kind="ExternalInput").ap()
        return self.d[name]

    def dout(self, name, shape, dt=F32):
        self.d[name] = self.nc.dram_tensor(name, list(shape), dt, kind="ExternalOutput").ap()
        return self.d[name]

    def setup(self):
        nc, P, st = self.nc, self.P, self.st
        self.din("xs", [self.ntok, D])
        self.din("w_in", [D, DPROJ])
        self.din("norm_mix_g", [D])
        self.din("hg_lb", [128, 2, 4])
        self.din("hg_norm_g", [128])
        AW = 50000
        arena_t = st.enter_context(nc.sbuf_tensor("arena", [128, AW], F32))
        self.A = Arena(arena_t[:], AW)
        self.banks = [st.enter_context(nc.psum_tensor("pb%d" % i, [128, 512], F32)) for i in range(8)]
        self.bbank = [Buf("pb%d" % i, psum=True) for i in range(8)]
        A = self.A
        self.identf = A.alloc((128,), F32)
        self.ident = A.alloc((128,), BF16)
        self.ones_bf = A.alloc((128,), BF16)
        self.ones_f = A.alloc((512,), F32)
        self.mask2 = A.alloc((128,), F32)
        self.bconst = Buf("const")
        bc = self.bconst
        P.pool(lambda e: e.memset(self.identf, 0.0), writes=[bc])
        P.pool(lambda e: e.affine_select(out=self.identf, in_=self.identf, pattern=[[-1, 128]],
                                         compare_op=ALU.not_equal, fill=1.0, base=0, channel_multiplier=1),
               reads=[bc], writes=[bc])
        P.pool(lambda e: e.tensor_copy(out=self.ident, in_=self.identf), reads=[bc], writes=[bc])
        P.pool(lambda e: e.memset(self.ones_bf, 1.0), writes=[bc])
        P.pool(lambda e: e.memset(self.ones_f, 1.0), writes=[bc])
        P.pool(lambda e: e.memset(self.mask2, 1.0), writes=[bc])
        P.pool(lambda e: e.affine_select(out=self.mask2, in_=self.mask2, pattern=[[1, 128]],
                                         compare_op=ALU.is_ge, fill=0.0, base=0, channel_multiplier=-1),
               reads=[bc], writes=[bc])
        P.pool(lambda e: e.memset(self.mask2[0:64, 64:128], 0.0), reads=[bc], writes=[bc])
        self.xt = [A.alloc((D,), F32) for _ in range(2)]
        self.bxt = [Buf("xt%d" % i) for i in range(2)]
        self.junk = A.alloc((D,), BF16)
        self.bjunk = Buf("junk")
        self.ss = A.alloc((NT,), F32)
        self.rstd = A.alloc((NT,), F32)
        self.bss = Buf("ss")
        self.brstd = Buf("rstd")
        self.xsb = [A.alloc((D,), BF16) for _ in range(2)]
        self.bxsb = [Buf("xsb%d" % i) for i in range(2)]
        self.gbc = A.alloc((D,), F32)
        self.bgbc = Buf("gbc")
        self.nxt = 0
        self.d["x_buf"] = nc.dram_tensor("x_buf", [NSLOT, D], BF16, kind="Internal").ap()
        zt = A.alloc((D,), BF16)
        self.bxzero = Buf("xzero")
        P.pool(lambda e: e.memset(zt, 0.0), writes=[bc])
        xbv = self.d["x_buf"].rearrange("(b p) n -> p b n", p=128)
        nblk = NSLOT // 128
        step = 12
        self.bxz = []
        for b0 in range(0, nblk, step):
            bz = Buf("xz")
            self.bxz.append(bz)
            P.dma(lambda e, b0=b0: e.dma_start(out=xbv[:, b0:b0 + step, :],
                                               in_=zt.unsqueeze(1).to_broadcast([128, step, D])),
                  reads=[bc], writes=[bz])

    def stage_a(self, *a, **k):
        for _ in self.stage_a_gen(*a, **k):
            pass

    def stage_a_gen(self, sb, xnT, bxnT, ptr, bptr, gname="norm_mix_g", src="xs", tok_base=0, keep=None):
        nc, P = self.nc, self.P
        xs_d = self.d[src]
        tiles = []
        for t in range(NT):
            i = self.nxt % 2
            self.nxt += 1
            tok0 = tok_base + sb * SBT + t * 128
            xt, bxt = self.xt[i], self.bxt[i]
            P.dma(lambda e, xt=xt, tok0=tok0: e.dma_start(out=xt, in_=xs_d[tok0:tok0 + 128, :]), writes=[bxt])
            P.act(lambda e, xt=xt, t=t: e.activation(out=self.junk, in_=xt, func=AF.Square,
                                                     accum_out=self.ss[:, t:t + 1]),
                  reads=[bxt], writes=[self.bss])
            tiles.append((xt, bxt, i))
            if t % 2 == 1:
                t0 = t - 1
                P.act(lambda e, t0=t0: e.activation(out=self.rstd[:, t0:t0 + 2], in_=self.ss[:, t0:t0 + 2],
                                                    func=AF.Ln, scale=1.0 / D, bias=EPS),
                      reads=[self.bss], writes=[self.brstd])
                P.act(lambda e, t0=t0: e.activation(out=self.rstd[:, t0:t0 + 2], in_=self.rstd[:, t0:t0 + 2],
                                                    func=AF.Exp, scale=-0.5),
                      reads=[self.brstd], writes=[self.brstd])
                for tt in (t0, t):
                    xt2, bxt2, i2 = tiles[tt]
                    xsb, bxsb = self.xsb[i2], self.bxsb[i2]
                    P.dve(lambda e, xt2=xt2, xsb=xsb, tt=tt: e.scalar_tensor_tensor(
                        out=xsb, in0=xt2, scalar=self.rstd[:, tt:tt + 1], in1=self.gbc,
                        op0=ALU.mult, op1=ALU.mult),
                        reads=[bxt2, self.brstd, self.bgbc], writes=[bxsb])
                    for k in range(KC):
                        P.pe(lambda e, k=k, xsb=xsb: e.transpose(out=ptr[:, k, :], in_=xsb[:, k * 128:(k + 1) * 128],
                                                                 identity=self.ident),
                             reads=[bxsb, self.bconst], writes=[bptr])
                    P.dve(lambda e, tt=tt: e.tensor_copy(out=xnT[:, :, tt * 128:(tt + 1) * 128], in_=ptr),
                          reads=[bptr], writes=[bxnT])
                    yield

    def load_gain(self, gname):
        P = self.P
        g = self.d[gname]
        P.dma(lambda e: e.dma_start(out=self.gbc, in_=g.partition_broadcast(128)), writes=[self.bgbc])

    def pass1(self):
        nc, P, A = self.nc, self.P, self.A
        d = self.d
        H = 4
        self.W2 = A.alloc((KC, 2056), BF16)
        self.bW2 = [Buf("W2_%d" % k) for k in range(KC)]
        m0 = A.mark()
        self.OA = A.alloc((H, SBT), BF16)
        self.bOA = [Buf("OA%d" % h) for h in range(H)]
        W1 = A.alloc((KC, 2048), BF16)
        bW1 = [Buf("W1_%d" % k) for k in range(KC)]
        wv = d["w_in"].rearrange("(k p) n -> p k n", p=128)
        for k in range(KC):
            P.dma(lambda e, k=k: e.dma_start(out=W1[:, k, :], in_=wv[:, k, 0:2048]), writes=[bW1[k]], q="pool")
        for k in range(KC):
            P.dma(lambda e, k=k: e.dma_start(out=self.W2[:, k, :], in_=wv[:, k, 2048:2048 + 2056]), writes=[self.bW2[k]], q="pool")
        self.load_gain("norm_mix_g")
        lraw = A.alloc((2, H), F32)
        lb = A.alloc((H,), F32)
        oml = A.alloc((H,), F32)
        hgn = A.alloc((1,), F32)
        blb = Buf("lb")
        P.dma(lambda e: e.dma_start(out=lraw, in_=d["hg_lb"]), writes=[blb])
        P.dma(lambda e: e.dma_start(out=hgn, in_=d["hg_norm_g"].rearrange("(p o) -> p o", o=1)), writes=[blb])
        P.dve(lambda e: e.tensor_tensor(out=lb, in0=lraw[:, 0, :], in1=lraw[:, 1, :], op=ALU.subtract),
              reads=[blb], writes=[blb])
        P.act(lambda e: e.activation(out=oml, in_=lb, func=AF.Sigmoid, scale=-1.0), reads=[blb], writes=[blb])
        P.act(lambda e: e.activation(out=lb, in_=lb, func=AF.Sigmoid), reads=[blb], writes=[blb])

        xnT = A.alloc((KC, SBT), BF16)
        bxnT = Buf("xnT")
        Fb = A.alloc((H, SBT), F32)
        CS = A.alloc((H, SBT), F32)
        Kb = A.alloc((H, SBT), BF16)
        EB = A.alloc((H, SBT), BF16)
        ENB = A.alloc((H, SBT), BF16)
        EBEs = [A.alloc((H, NCH), F32) for _ in range(2)]
        QTs = [A.alloc((H, SBT), BF16) for _ in range(2)]
        KTs = [A.alloc((H, SBT), BF16) for _ in range(2)]
        KH = A.alloc((H, SBT), BF16)
        KHTs = [A.alloc((NT, 512), BF16) for _ in range(2)]
        Vs = [A.alloc((NT, 512), BF16) for _ in range(2)]
        Gs = [A.alloc((H, SBT), BF16) for _ in range(2)]
        O32 = A.alloc((H, SBT), F32)
        OSQ = A.alloc((H, SBT), BF16)
        LNV = A.alloc((SBT,), F32)
        ATS = A.alloc((H, 128), BF16)
        S32 = A.alloc((H, 128), F32)
        SBF = [A.alloc((H, 128), BF16) for _ in range(2)]
        bF = [Buf() for _ in range(H)]
        bCS = [Buf() for _ in range(H)]
        bK = Buf()
        bEB = [Buf() for _ in range(H)]
        bENB = [Buf() for _ in range(H)]
        bEBEs = [Buf(), Buf()]
        bQTs = [[Buf() for _ in range(H)] for _ in range(2)]
        bKTs = [Buf(), Buf()]
        bKH = Buf()
        bKHTs = [[Buf() for _ in range(NT)] for _ in range(2)]
        bVs = [[Buf() for _ in range(NT)] for _ in range(2)]
        bGs = [[Buf() for _ in range(H)] for _ in range(2)]
        bO32 = Buf()
        bOSQ = [Buf() for _ in range(H)]
        bLNV = Buf()
        bATS = Buf()
        bS32 = [Buf() for _ in range(H)]
        bSBF = [[Buf() for _ in range(H)] for _ in range(2)]
        sbf_i = [0] * H

        bk = self.banks
        bb = self.bbank
        ptr = bk[0][:].bitcast(BF16).rearrange("p (k t) -> p k t", k=KC)
        pkt = bk[1][:].bitcast(BF16)[:, 0:512]
        pj = [bk[2][:], bk[3][:]]
        bpj = [bb[2], bb[3]]
        pat = bk[4][:].rearrange("p (h t) -> p h t", h=H)
        po = bk[5][:].rearrange("p (h t) -> p h t", h=H)
        pS = [bk[6 + (h % 2)][:, 0:128] for h in range(H)]
        bpS = [bb[6 + (h % 2)] for h in range(H)]
        pji = [0]

        def nextpj():
            i = pji[0] % 2
            pji[0] += 1
            return pj[i], bpj[i]

        for h in range(H):
            P.pool(lambda e, h=h: e.memset(S32[:, h, :], 0.0), writes=[bS32[h]])
            P.pool(lambda e, h=h: e.memset(SBF[0][:, h, :], 0.0), writes=[bSBF[0][h]])

        def proj_fm(col0, h):
            p, bp = nextpj()
            for k in range(KC):
                P.pe(lambda e, k=k, p=p: e.matmul(p[:, 0:SBT], lhsT=W1[:, k, col0 + h * 128:col0 + (h + 1) * 128],
                                                  rhs=xnT[:, k, :], start=(k == 0), stop=(k == KC - 1)),
                     reads=[bW1[k], bxnT], writes=[bp])
            return p, bp

        def X1(sb):
            sl = sb % 2
            QT, KT, KHT, V, G, EBE = QTs[sl], KTs[sl], KHTs[sl], Vs[sl], Gs[sl], EBEs[sl]
            bQT, bKT, bKHT, bV, bG, bEBE = bQTs[sl], bKTs[sl], bKHTs[sl], bVs[sl], bGs[sl], bEBEs[sl]
            full = sb >= self.npre
            yield from self.stage_a_gen(sb, xnT, bxnT, ptr, bb[0])
            for h in range(H):
                p, bp = proj_fm(C_HF, h)
                P.act(lambda e, h=h, p=p: e.activation(out=Fb[:, h, :], in_=p[:, 0:SBT], func=AF.Sigmoid),
                      reads=[bp], writes=[bF[h]])
                yield
            if full:
                for h in range(H):
                    p, bp = proj_fm(C_HQ, h)
                    P.act(lambda e, h=h, p=p: e.activation(out=QT[:, h, :], in_=p[:, 0:SBT], func=AF.Silu),
                          reads=[bp], writes=[bQT[h]])
                    yield
                for h in range(H):
                    p, bp = proj_fm(C_HG, h)
                    P.act(lambda e, h=h, p=p: e.activation(out=G[:, h, :], in_=p[:, 0:SBT], func=AF.Silu),
                          reads=[bp], writes=[bG[h]])
                    yield
            for h in range(H):
                P.dve(lambda e, h=h: e.tensor_scalar(out=Fb[:, h, :], in0=Fb[:, h, :], scalar1=oml[:, h:h + 1],
                                                     scalar2=lb[:, h:h + 1], op0=ALU.mult, op1=ALU.add),
                      reads=[bF[h], blb], writes=[bF[h]])
            P.dve(lambda e: e.tensor_scalar(out=Kb, in0=Fb, scalar1=-1.0, scalar2=1.0, op0=ALU.mult, op1=ALU.add),
                  reads=bF, writes=[bK])
            for h in range(H):
                P.act(lambda e, h=h: e.activation(out=Fb[:, h, :], in_=Fb[:, h, :], func=AF.Ln),
                      reads=[bF[h], bK], writes=[bF[h]])
            for h in range(H):
                P.dve(lambda e, h=h: e.tensor_tensor_scan(out=CS[:, h, :], data0=self.ones_f[:, 0:SBT],
                                                          data1=Fb[:, h, :], initial=0.0,
                                                          op0=ALU.mult, op1=ALU.add),
                      reads=[bF[h], self.bconst], writes=[bCS[h]])
                yield
            Fb4 = Fb.rearrange("p h (c t) -> p h c t", c=NCH)
            CS4 = CS.rearrange("p h (c t) -> p h c t", c=NCH)
            P.dve(lambda e: e.tensor_tensor(out=Fb4[:, :, 1:NCH, :], in0=CS4[:, :, 1:NCH, :],
                                            in1=CS4[:, :, 0:NCH - 1, 63:64].to_broadcast([128, H, NCH - 1, 64]),
                                            op=ALU.subtract),
                  reads=bCS + bF, writes=bF)
            P.dve(lambda e: e.tensor_copy(out=Fb4[:, :, 0, :], in_=CS4[:, :, 0, :]), reads=bCS + bF, writes=bF)
            for h in range(H):
                P.act(lambda e, h=h: e.activation(out=ENB[:, h, :], in_=Fb[:, h, :], func=AF.Exp, scale=-1.0),
                      reads=[bF[h]], writes=[bENB[h]])
                yield
            P.act(lambda e: e.activation(out=EBE, in_=Fb4[:, :, :, 63], func=AF.Exp), reads=bF, writes=[bEBE])
            if full:
                for h in range(H):
                    P.act(lambda e, h=h: e.activation(out=EB[:, h, :], in_=Fb[:, h, :], func=AF.Exp),
                          reads=[bF[h]], writes=[bEB[h]])
            P.dve(lambda e: e.tensor_tensor(out=KT, in0=Kb, in1=ENB, op=ALU.mult), reads=[bK] + bENB, writes=[bKT])
            KT4 = KT.rearrange("p h (c t) -> p h c t", c=NCH)
            KH4 = KH.rearrange("p h (c t) -> p h c t", c=NCH)
            P.dve(lambda e: e.tensor_tensor(out=KH4, in0=KT4,
                                            in1=EBE.unsqueeze(3).to_broadcast([128, H, NCH, 64]), op=ALU.mult),
                  reads=[bKT, bEBE], writes=[bKH])
            if full:
                P.dve(lambda e: e.tensor_tensor(out=QT, in0=QT, in1=EB, op=ALU.mult), reads=bQT + bEB, writes=bQT)
            for t in range(NT):
                p, bp = nextpj()
                for k in range(KC):
                    P.pe(lambda e, k=k, p=p, t=t: e.matmul(p, lhsT=xnT[:, k, t * 128:(t + 1) * 128],
                                                           rhs=W1[:, k, C_HI:C_HI + 512],
                                                           start=(k == 0), stop=(k == KC - 1)),
                         reads=[bW1[k], bxnT], writes=[bp])
                P.act(lambda e, p=p, t=t: e.activation(out=V[:, t, :], in_=p, func=AF.Copy), reads=[bp], writes=[bV[t]])
                for h in range(H):
                    P.pe(lambda e, h=h, t=t: e.transpose(out=pkt[:, h * 128:(h + 1) * 128],
                                                         in_=KH[:, h, t * 128:(t + 1) * 128], identity=self.ident),
                         reads=[bKH, self.bconst], writes=[bb[1]])
                P.dve(lambda e, t=t: e.tensor_copy(out=KHT[:, t, :], in_=pkt), reads=[bb[1]], writes=[bKHT[t]])
                yield
        def Y1(sb):
            full = sb >= self.npre
            sl = sb % 2
            QT, KT, KHT, V, G, EBE = QTs[sl], KTs[sl], KHTs[sl], Vs[sl], Gs[sl], EBEs[sl]
            bQT, bKT, bKHT, bV, bG, bEBE = bQTs[sl], bKTs[sl], bKHTs[sl], bVs[sl], bGs[sl], bEBEs[sl]
            for j in range(NT):
                c0 = j * 128
                if full:
                    for h in range(H):
                        P.pe(lambda e, h=h, c0=c0: e.matmul(pat[:, h, :], lhsT=KT[:, h, c0:c0 + 128],
                                                            rhs=QT[:, h, c0:c0 + 128], start=True, stop=True),
                             reads=[bKT, bQT[h]], writes=[bb[4]])
                    P.dve(lambda e: e.tensor_tensor(out=ATS, in0=pat,
                                                    in1=self.mask2.unsqueeze(1).to_broadcast([128, H, 128]),
                                                    op=ALU.mult),
                          reads=[bb[4], self.bconst], writes=[bATS])
                    yield
                for half in range(2):
                    ch = 2 * j + half
                    r0 = half * 64
                    for h in range(H):
                        cur = sbf_i[h]
                        if full:
                            P.pe(lambda e, h=h, cur=cur, c0=c0, r0=r0: e.matmul(
                                po[:, h, r0:r0 + 64], lhsT=SBF[cur][:, h, :], rhs=QT[:, h, c0 + r0:c0 + r0 + 64],
                                start=(h == 0 and r0 == 0), stop=False, skip_group_check=True),
                                reads=[bSBF[cur][h], bQT[h]], writes=[bb[5]])
                        P.pe(lambda e, h=h, j=j, r0=r0: e.matmul(
                            pS[h], lhsT=KHT[r0:r0 + 64, j, h * 128:(h + 1) * 128],
                            rhs=V[r0:r0 + 64, j, h * 128:(h + 1) * 128], start=True, stop=True),
                            reads=[bKHT[j], bV[j]], writes=[bpS[h]])
                        P.dve(lambda e, h=h, ch=ch: e.scalar_tensor_tensor(
                            out=S32[:, h, :], in0=S32[:, h, :], scalar=EBE[:, h, ch:ch + 1], in1=pS[h],
                            op0=ALU.mult, op1=ALU.add),
                            reads=[bS32[h], bEBE, bpS[h]], writes=[bS32[h]])
                        nxt = 1 - cur
                        P.act(lambda e, h=h, nxt=nxt: e.activation(out=SBF[nxt][:, h, :], in_=S32[:, h, :], func=AF.Copy),
                              reads=[bS32[h]], writes=[bSBF[nxt][h]])
                        sbf_i[h] = nxt
                        yield
                if full:
                    for h in range(H):
                        P.pe(lambda e, h=h, j=j: e.matmul(po[:, h, :], lhsT=V[:, j, h * 128:(h + 1) * 128],
                                                          rhs=ATS[:, h, :], start=False, stop=True,
                                                          skip_group_check=True),
                             reads=[bV[j], bATS], writes=[bb[5]])
                    P.act(lambda e, c0=c0: e.activation(out=O32[:, :, c0:c0 + 128], in_=po, func=AF.Copy),
                          reads=[bb[5]], writes=[bO32])
                    yield
            if full:
                tok0 = (sb - self.npre) * SBT
                for h in range(H):
                    P.act(lambda e, h=h: e.activation(out=OSQ[:, h, :], in_=O32[:, h, :], func=AF.Square),
                          reads=[bO32], writes=[bOSQ[h]])
                for h in range(H):
                    pss, bpss = bk[4][:], bb[4]
                    P.pe(lambda e, h=h, pss=pss: e.matmul(pss[:, 0:SBT], lhsT=self.ones_bf, rhs=OSQ[:, h, :], start=True, stop=True),
                         reads=[bOSQ[h], self.bconst], writes=[bpss])
                    P.act(lambda e, pss=pss: e.activation(out=LNV, in_=pss[:, 0:SBT], func=AF.Ln, scale=1.0 / 128, bias=EPS),
                          reads=[bpss], writes=[bLNV])
                    P.act(lambda e: e.activation(out=LNV, in_=LNV, func=AF.Exp, scale=-0.5),
                          reads=[bLNV], writes=[bLNV])
                    P.dve(lambda e, h=h: e.tensor_tensor(out=O32[:, h, :], in0=O32[:, h, :], in1=LNV, op=ALU.mult),
                          reads=[bO32, bLNV], writes=[bO32])
                    P.dve(lambda e, h=h: e.scalar_tensor_tensor(
                        out=self.OA[:, h, :], in0=O32[:, h, :], scalar=hgn[:, 0:1], in1=G[:, h, :],
                        op0=ALU.mult, op1=ALU.mult),
                        reads=[bO32, blb, bG[h]], writes=[self.bOA[h]])
                    yield
                self.spill("oa_s", self.OA, self.bOA, sb)
                if "oa" in self.debug:
                    if "dbg_oa" not in self.d:
                        self.dout("dbg_oa", [128, H, self.nfull_tok], BF16)
                    P.dma(lambda e, tok0=tok0: e.dma_start(out=self.d["dbg_oa"][:, :, tok0:tok0 + SBT], in_=self.OA),
                          reads=self.bOA, out=True)
        import os
        WTS1 = [int(v) for v in os.environ.get("IL_W1", "1,1").split(",")]

        def run_il(gens):
            gens = list(gens)
            while gens:
                for g_, w_ in list(gens):
                    for _ in range(w_):
                        try:
                            next(g_)
                        except StopIteration:
                            gens.remove((g_, w_))
                            break

        n = self.nsb
        for r in range(-1, n):
            gs = []
            if 0 <= r < n:
                gs.append((Y1(r), WTS1[0]))
            if 0 <= r + 1 < n:
                gs.append((X1(r + 1), WTS1[1]))
            run_il(gs)
        A.reset(m0)
        P.barrier()

    def finish(self):
        self.P.finalize(self.nc)
        self.st.close()
        return self.nc


def _pass2(self):
    nc, P, A = self.nc, self.P, self.A
    d = self.d
    H = 4
    self.din("conv_wT", [128, 4, 12])
    self.din("gd_A_log", [4])
    self.din("gd_dt_bias", [4])
    self.din("gd_norm_g", [128])
    m0 = A.mark()
    self.OB = A.alloc((H, SBT), BF16)
    self.bOB = [Buf("OB%d" % h) for h in range(H)]
    NW = 2056
    if hasattr(self, "W2"):
        W2, bW2 = self.W2, self.bW2
    else:
        W2 = A.alloc((KC, NW), BF16)
        bW2 = [Buf("W2_%d" % k) for k in range(KC)]
        wv = d["w_in"].rearrange("(k p) n -> p k n", p=128)
        for k in range(KC):
            P.dma(lambda e, k=k: e.dma_start(out=W2[:, k, :], in_=wv[:, k, 2048:2048 + NW]), writes=[bW2[k]], q="pool")
    self.load_gain("norm_mix_g")
    cw = A.alloc((4, 12), F32)
    negA = A.alloc((H,), F32)
    dtb = A.alloc((H,), F32)
    gdn = A.alloc((1,), F32)
    bpar = Buf("par2")
    P.dma(lambda e: e.dma_start(out=cw, in_=d["conv_wT"]), writes=[bpar])
    P.dma(lambda e: e.dma_start(out=negA, in_=d["gd_A_log"].partition_broadcast(128)), writes=[bpar])
    P.dma(lambda e: e.dma_start(out=dtb, in_=d["gd_dt_bias"].partition_broadcast(128)), writes=[bpar])
    P.dma(lambda e: e.dma_start(out=gdn, in_=d["gd_norm_g"].rearrange("(p o) -> p o", o=1)), writes=[bpar])
    P.act(lambda e: e.activation(out=negA, in_=negA, func=AF.Exp), reads=[bpar], writes=[bpar])
    P.dve(lambda e: e.tensor_scalar(out=negA, in0=negA, scalar1=-1.0, scalar2=None, op0=ALU.mult),
          reads=[bpar], writes=[bpar])
    maskL = A.alloc((128,), F32)
    ch01 = A.alloc((2, 128), F32)
    bc = self.bconst
    P.pool(lambda e: e.memset(maskL, 1.0), writes=[bc])
    P.pool(lambda e: e.affine_select(out=maskL, in_=maskL, pattern=[[-1, 128]], compare_op=ALU.is_gt,
                                     fill=0.0, base=0, channel_multiplier=1), reads=[bc], writes=[bc])
    P.pool(lambda e: e.memset(maskL[64:128, 0:64], 0.0), reads=[bc], writes=[bc])
    bones = A.alloc((128,), F32)
    P.pool(lambda e: e.memset(bones, 0.0), writes=[bc])
    P.pool(lambda e: e.memset(bones[0:64, 0:64], 1.0), reads=[bc], writes=[bc])
    P.pool(lambda e: e.memset(bones[64:128, 64:128], 1.0), reads=[bc], writes=[bc])
    P.pool(lambda e: e.memset(ch01, 0.0), writes=[bc])
    P.pool(lambda e: e.memset(ch01[0:64, 0, :], 1.0), reads=[bc], writes=[bc])
    P.pool(lambda e: e.memset(ch01[64:128, 1, :], 1.0), reads=[bc], writes=[bc])

    xnT = A.alloc((KC, SBT), BF16)
    bxnT = Buf("xnT")
    XC = A.alloc((12, SBT + 3), BF16)
    DG = A.alloc((48, 128), BF16)
    bDG = Buf("DG")
    for _j in range(4):
        for _cb in range(12):
            P.dve(lambda e, _j=_j, _cb=_cb: e.tensor_scalar(out=DG[:, _j * 12 + _cb, :], in0=self.identf,
                                                           scalar1=cw[:, _j, _cb:_cb + 1], scalar2=None, op0=ALU.mult),
                  reads=[bpar, self.bconst], writes=[bDG])
    bXC = [Buf() for _ in range(12)]
    CV = [A.alloc((SBT,), F32) for _ in range(2)]
    bCV = [Buf(), Buf()]
    QK32 = A.alloc((8, SBT), F32)
    bQK32 = [Buf() for _ in range(8)]
    SQ = A.alloc((SBT,), BF16)
    bSQ = Buf()
    RS = A.alloc((SBT,), F32)
    bRS = Buf()
    NS = 3
    QTs = [A.alloc((H, SBT), BF16) for _ in range(NS)]
    KTs = [A.alloc((H, SBT), BF16) for _ in range(NS)]
    VTs = [A.alloc((H, SBT), BF16) for _ in range(NS)]
    GZs = [A.alloc((H, SBT), BF16) for _ in range(NS)]
    bQTs = [[Buf() for _ in range(H)] for _ in range(NS)]
    bKTs = [[Buf() for _ in range(H)] for _ in range(NS)]
    bVTs = [[Buf() for _ in range(H)] for _ in range(NS)]
    bGZs = [[Buf() for _ in range(H)] for _ in range(NS)]
    BGraws = [A.alloc((NT, 8), F32) for _ in range(NS)]
    LNBs = [A.alloc((NT, H), F32) for _ in range(NS)]
    BETAs = [A.alloc((NT, H), F32) for _ in range(NS)]
    GGs = [A.alloc((NT, H), F32) for _ in range(NS)]
    bBGs = [Buf() for _ in range(NS)]
    PBUF = []
    for _j in range(NT):
        pb = dict(
            GB=A.alloc((H, 128), F32), bGB=Buf(),
            E1=A.alloc((H, 128), F32), bE1=Buf(),
            E2=A.alloc((H, 128), F32), bE2=Buf(),
            EGR=A.alloc((H, 128), BF16), bEGR=Buf(),
            Lm=[A.alloc((H, 128), BF16) for _ in range(2)], bLm=[Buf(), Buf()],
            Um=[A.alloc((H, 128), BF16) for _ in range(2)], bUm=[Buf(), Buf()],
            Xm=[A.alloc((H, 128), BF16) for _ in range(2)], bXm=[Buf(), Buf()],
            VTK=A.alloc((H, 128), BF16), bVTK=Buf(),
        )
        PBUF.append(pb)
    PCAR = []
    for _s in range(2):
        row = []
        for _j in range(NT):
            row.append(dict(
                SC=A.alloc((8, H), F32), bSC=Buf(),
                QTG=A.alloc((H, 128), BF16), bQTG=Buf(),
                TT=A.alloc((H, 128), BF16), bTT=Buf(),
                ATT=A.alloc((H, 128), BF16), bATT=Buf(),
                KHT=A.alloc((H, 128), BF16), bKHT=Buf(),
                KTP=A.alloc((H, 128), BF16), bKTP=Buf(),
                BV=A.alloc((H, 128), F32), bBV=Buf(),
            ))
        PCAR.append(row)
    R = A.alloc((H, 128), BF16)
    USB = A.alloc((H, 128), BF16)
    bR = Buf()
    bUSB = Buf()
    S32 = A.alloc((H, 128), F32)
    SBF = [A.alloc((H, 128), BF16) for _ in range(2)]
    bS32 = [Buf() for _ in range(H)]
    bSBF = [[Buf() for _ in range(H)] for _ in range(2)]
    sbf_i = [0]
    O32 = A.alloc((H, SBT), F32)
    bO32 = Buf()
    OSQ = A.alloc((H, SBT), BF16)
    bOSQ = [Buf() for _ in range(H)]
    LNV = A.alloc((SBT,), F32)
    bLNV = Buf()
    identb4 = A.alloc((H, 128), BF16)
    P.pool(lambda e: e.tensor_copy(out=identb4, in_=self.ident.unsqueeze(1).to_broadcast([128, H, 128])),
           reads=[bc], writes=[bc])

    bk, bb = self.banks, self.bbank
    ptr = bk[0][:].bitcast(BF16).rearrange("p (k t) -> p k t", k=KC)
    ptr4 = bk[0][:].bitcast(BF16)[:, 0:512].rearrange("p (h t) -> p h t", h=H)
    pj = [bk[1][:], bk[2][:], bk[0][:]]
    bpj = [bb[1], bb[2], bb[0]]
    pA = bk[3][:].rearrange("p (h t) -> p h t", h=H)
    pB = bk[4][:].rearrange("p (h t) -> p h t", h=H)
    pBb = bk[4][:].bitcast(BF16)[:, 0:512].rearrange("p (h t) -> p h t", h=H)
    pC = bk[5][:].rearrange("p (h t) -> p h t", h=H)
    pR = bk[6][:].rearrange("p (h t) -> p h t", h=H)
    po = bk[7][:].rearrange("p (h t) -> p h t", h=H)
    pji = [0]

    def nextpj():
        i = pji[0] % 3
        pji[0] += 1
        return pj[i], bpj[i]

    for h in range(H):
        P.pool(lambda e, h=h: e.memset(S32[:, h, :], 0.0), writes=[bS32[h]])
        P.pool(lambda e, h=h: e.memset(SBF[0][:, h, :], 0.0), writes=[bSBF[0][h]])
    for cb in range(12):
        P.pool(lambda e, cb=cb: e.memset(XC[:, cb, :], 0.0), writes=[bXC[cb]])

    def proj_fm(col0):
        p, bp = nextpj()
        for k in range(KC):
            P.pe(lambda e, k=k, p=p: e.matmul(p[:, 0:SBT], lhsT=W2[:, k, col0:col0 + 128],
                                              rhs=xnT[:, k, :], start=(k == 0), stop=(k == KC - 1)),
                 reads=[bW2[k], bxnT], writes=[bp])
        return p, bp

    evi = [0]

    def evac(out, in_, reads, writes):
        i = evi[0]
        evi[0] += 1
        if i % 2 == 0:
            P.act(lambda e: e.activation(out=out, in_=in_, func=AF.Copy), reads=reads, writes=writes)
        else:
            P.dve(lambda e: e.tensor_copy(out=out, in_=in_), reads=reads, writes=writes)

    def X(sb):
        sl = sb % 3
        QT, KT, VT, GZ = QTs[sl], KTs[sl], VTs[sl], GZs[sl]
        bQT, bKT, bVT, bGZ = bQTs[sl], bKTs[sl], bVTs[sl], bGZs[sl]
        BGraw, LNB, BETA, GG, bBG = BGraws[sl], LNBs[sl], BETAs[sl], GGs[sl], bBGs[sl]
        full = sb >= self.npre
        yield from self.stage_a_gen(sb, xnT, bxnT, ptr, bb[0])
        cbs = list(range(12)) if full else list(range(4, 12))
        cbs_proj = list(range(12)) if sb >= self.npre - 1 else list(range(4, 12))
        for cb in cbs_proj:
            if sb > 0:
                P.pool(lambda e, cb=cb: e.tensor_copy(out=XC[:, cb, 0:3], in_=XC[:, cb, SBT:SBT + 3]),
                       reads=[bXC[cb]], writes=[bXC[cb]])
            p, bp = proj_fm(cb * 128)
            evac(XC[:, cb, 3:SBT + 3], p[:, 0:SBT], [bp], [bXC[cb]])
            yield
        pbg, bpbg = nextpj()
        for t in range(NT):
            for k in range(KC):
                P.pe(lambda e, k=k, t=t, pbg=pbg: e.matmul(pbg[:, t * 8:(t + 1) * 8], lhsT=xnT[:, k, t * 128:(t + 1) * 128],
                                                  rhs=W2[:, k, 2048:2056], start=(k == 0), stop=(k == KC - 1)),
                     reads=[bW2[k], bxnT], writes=[bpbg])
        P.act(lambda e, pbg=pbg: e.activation(out=BGraw, in_=pbg[:, 0:NT * 8].rearrange("p (t c) -> p t c", t=NT), func=AF.Copy),
              reads=[bpbg], writes=[bBG])
        P.act(lambda e: e.activation(out=LNB, in_=BGraw[:, :, 0:4], func=AF.Exp, scale=-1.0), reads=[bBG], writes=[bBG])
        P.act(lambda e: e.activation(out=LNB, in_=LNB, func=AF.Ln, bias=1.0), reads=[bBG], writes=[bBG])
        P.dve(lambda e: e.tensor_scalar(out=LNB, in0=LNB, scalar1=-1.0, scalar2=None, op0=ALU.mult),
              reads=[bBG], writes=[bBG])
        P.act(lambda e: e.activation(out=BETA, in_=LNB, func=AF.Exp), reads=[bBG], writes=[bBG])
        P.dve(lambda e: e.tensor_tensor(out=GG, in0=BGraw[:, :, 4:8], in1=dtb.unsqueeze(1).to_broadcast([128, NT, H]),
                                        op=ALU.add), reads=[bBG, bpar], writes=[bBG])
        P.act(lambda e: e.activation(out=GG, in_=GG, func=AF.Exp), reads=[bBG], writes=[bBG])
        P.act(lambda e: e.activation(out=GG, in_=GG, func=AF.Ln, bias=1.0), reads=[bBG], writes=[bBG])
        P.dve(lambda e: e.tensor_tensor(out=GG, in0=GG, in1=negA.unsqueeze(1).to_broadcast([128, NT, H]),
                                        op=ALU.mult), reads=[bBG, bpar], writes=[bBG])
        yield
        if full:
            for h in range(H):
                p, bp = proj_fm(1536 + h * 128)
                P.act(lambda e, h=h, p=p: e.activation(out=GZ[:, h, :], in_=p[:, 0:SBT], func=AF.Silu),
                      reads=[bp], writes=[bGZ[h]])
                yield
        for n, cb in enumerate(cbs):
            cv, bcv = nextpj()
            for j in range(4):
                P.pe(lambda e, cb=cb, cv=cv, j=j: e.matmul(cv[:, 0:SBT], lhsT=DG[:, j * 12 + cb, :], rhs=XC[:, cb, j:SBT + j],
                                                           start=(j == 0), stop=(j == 3)),
                     reads=[bXC[cb], bDG], writes=[bcv])
            if cb < 8:
                P.act(lambda e, cb=cb, cv=cv: e.activation(out=QK32[:, cb, :], in_=cv[:, 0:SBT], func=AF.Silu),
                      reads=[bcv], writes=[bQK32[cb]])
            else:
                P.act(lambda e, cb=cb, cv=cv: e.activation(out=VT[:, cb - 8, :], in_=cv[:, 0:SBT], func=AF.Silu),
                      reads=[bcv], writes=[bVT[cb - 8]])
            yield
        for cb in cbs:
            if cb >= 8:
                continue
            P.act(lambda e, cb=cb: e.activation(out=SQ, in_=QK32[:, cb, :], func=AF.Square),
                  reads=[bQK32[cb]], writes=[bSQ])
            p, bp = nextpj()
            P.pe(lambda e, p=p: e.matmul(p[:, 0:SBT], lhsT=self.ones_bf, rhs=SQ, start=True, stop=True),
                 reads=[bSQ, bc], writes=[bp])
            P.act(lambda e, p=p: e.activation(out=RS, in_=p[:, 0:SBT], func=AF.Ln, bias=EPS), reads=[bp], writes=[bRS])
            qbias = -0.5 * float(np.log(128.0)) if cb < 4 else 0.0
            P.act(lambda e, qbias=qbias: e.activation(out=RS, in_=RS, func=AF.Exp, scale=-0.5, bias=qbias),
                  reads=[bRS], writes=[bRS])
            dst, bdst = (QT[:, cb, :], bQT[cb]) if cb < 4 else (KT[:, cb - 4, :], bKT[cb - 4])
            P.dve(lambda e, cb=cb, dst=dst: e.tensor_tensor(out=dst, in0=QK32[:, cb, :], in1=RS, op=ALU.mult),
                  reads=[bQK32[cb], bRS], writes=[bdst])
            yield
    def Yp(sb):
        sl = sb % 3
        full = sb >= self.npre
        QT, KT, VT, GZ = QTs[sl], KTs[sl], VTs[sl], GZs[sl]
        bQT, bKT, bVT, bGZ = bQTs[sl], bKTs[sl], bVTs[sl], bGZs[sl]
        BGraw, LNB, BETA, GG, bBG = BGraws[sl], LNBs[sl], BETAs[sl], GGs[sl], bBGs[sl]
        LL = dict(L0)
        LL['PB'] = [dict(PBUF[j], **PCAR[sb % 2][j]) for j in range(NT)]
        LL.update(QT=QT, KT=KT, VT=VT, GZ=GZ, bQT=bQT, bKT=bKT, bVT=bVT, bGZ=bGZ, LNB=LNB, BETA=BETA, GG=GG, bBG=bBG)
        yield from self._gdn_prep(LL, full)
    def Yr(sb):
        sl = sb % 3
        full = sb >= self.npre
        QT, KT, VT, GZ = QTs[sl], KTs[sl], VTs[sl], GZs[sl]
        bQT, bKT, bVT, bGZ = bQTs[sl], bKTs[sl], bVTs[sl], bGZs[sl]
        BGraw, LNB, BETA, GG, bBG = BGraws[sl], LNBs[sl], BETAs[sl], GGs[sl], bBGs[sl]
        LL = dict(L0)
        LL['PB'] = [dict(PBUF[j], **PCAR[sb % 2][j]) for j in range(NT)]
        LL.update(QT=QT, KT=KT, VT=VT, GZ=GZ, bQT=bQT, bKT=bKT, bVT=bVT, bGZ=bGZ, LNB=LNB, BETA=BETA, GG=GG, bBG=bBG)
        yield from self._gdn_rec(LL, full)
        if full:
            tok0 = (sb - self.npre) * SBT
            for h in range(H):
                P.act(lambda e, h=h: e.activation(out=OSQ[:, h, :], in_=O32[:, h, :], func=AF.Square),
                      reads=[bO32], writes=[bOSQ[h]])
            for h in range(H):
                pss, bpss = bk[5][:], bb[5]
                P.pe(lambda e, h=h, pss=pss: e.matmul(pss[:, 0:SBT], lhsT=self.ones_bf, rhs=OSQ[:, h, :], start=True, stop=True),
                     reads=[bOSQ[h], bc], writes=[bpss])
                P.act(lambda e, pss=pss: e.activation(out=LNV, in_=pss[:, 0:SBT], func=AF.Ln, scale=1.0 / 128, bias=EPS),
                      reads=[bpss], writes=[bLNV])
                P.act(lambda e: e.activation(out=LNV, in_=LNV, func=AF.Exp, scale=-0.5), reads=[bLNV], writes=[bLNV])
                P.dve(lambda e, h=h: e.tensor_tensor(out=O32[:, h, :], in0=O32[:, h, :], in1=LNV, op=ALU.mult),
                      reads=[bO32, bLNV], writes=[bO32])
                P.dve(lambda e, h=h: e.scalar_tensor_tensor(
                    out=self.OB[:, h, :], in0=O32[:, h, :], scalar=gdn[:, 0:1], in1=GZ[:, h, :],
                    op0=ALU.mult, op1=ALU.mult), reads=[bO32, bpar, bGZ[h]], writes=[self.bOB[h]])
                yield
            self.spill("ob_s", self.OB, self.bOB, sb)
            if "ob" in self.debug:
                if "dbg_ob" not in self.d:
                    self.dout("dbg_ob", [128, H, self.nfull_tok], BF16)
                P.dma(lambda e, tok0=tok0: e.dma_start(out=self.d["dbg_ob"][:, :, tok0:tok0 + SBT], in_=self.OB),
                      reads=self.bOB, out=True)
    L0 = dict(locals())

    import os
    WTS = [int(v) for v in os.environ.get("IL_W", "1,2,2").split(",")]

    def run_il(gens):
        gens = list(gens)
        while gens:
            for g_, w_ in list(gens):
                for _ in range(w_):
                    try:
                        next(g_)
                    except StopIteration:
                        gens.remove((g_, w_))
                        break

    n = self.nsb
    for r in range(-2, n):
        gs = []
        if 0 <= r < n:
            gs.append((Yr(r), WTS[0]))
        if 0 <= r + 1 < n:
            gs.append((Yp(r + 1), WTS[1]))
        if 0 <= r + 2 < n:
            gs.append((X(r + 2), WTS[2]))
        run_il(gs)
    A.reset(m0)
    P.barrier()


KB.pass2 = _pass2


def _gdn_prep(self, L, full):
    P = self.P
    H = 4
    g = lambda n: L[n]
    bk, bb = self.banks, self.bbank
    bc = self.bconst
    mask2, ident = self.mask2, self.ident
    maskL, bones, ch01, identb4 = g("maskL"), g("bones"), g("ch01"), g("identb4")
    PBUF, GG, LNB, BETA, bBG = g("PB"), g("GG"), g("LNB"), g("BETA"), g("bBG")
    QT, KT, VT, bQT, bKT, bVT = g("QT"), g("KT"), g("VT"), g("bQT"), g("bKT"), g("bVT")
    R, USB, bR, bUSB = g("R"), g("USB"), g("bR"), g("bUSB")
    S32, SBF, bS32, bSBF, sbf_i = g("S32"), g("SBF"), g("bS32"), g("bSBF"), g("sbf_i")
    O32, bO32 = g("O32"), g("bO32")
    evac, nextpj = g("evac"), g("nextpj")
    NP = NT
    pP = [bk[3 + j][:].rearrange("p (h t) -> p h t", h=H) for j in range(NP)]
    pPb = [bk[3 + j][:].bitcast(BF16)[:, 0:512].rearrange("p (h t) -> p h t", h=H) for j in range(NP)]
    bpP = [bb[3 + j] for j in range(NP)]
    ptr4 = bk[5][:].bitcast(BF16)[:, 0:512].rearrange("p (h t) -> p h t", h=H)
    pKS = bk[5][:].rearrange("p (h t) -> p h t", h=H)
    pU = bk[6][:].rearrange("p (h t) -> p h t", h=H)
    po = bk[7][:].rearrange("p (h t) -> p h t", h=H)

    def bc4(ap):
        return ap.unsqueeze(2).to_broadcast([128, H, 128])

    for j in range(NP):
        pb = PBUF[j]
        SC, bSC = pb["SC"], pb["bSC"]
        ps = bk[3 + j][:, 0:16]
        gj = GG[:, j, :]
        for n, lhs in enumerate((mask2, bones, ch01[:, 0, :], ch01[:, 1, :])):
            P.pe(lambda e, n=n, lhs=lhs, ps=ps, gj=gj: e.matmul(ps[:, 4 * n:4 * n + 4], lhsT=lhs, rhs=gj,
                                                                 start=True, stop=True),
                 reads=[bBG, bc], writes=[bpP[j]])
        P.act(lambda e, SC=SC, ps=ps: e.activation(out=SC[:, 0, :], in_=ps[:, 0:4], func=AF.Copy),
              reads=[bpP[j]], writes=[bSC])
        P.dve(lambda e, SC=SC, ps=ps: e.tensor_tensor(out=SC[:, 5, :], in0=ps[:, 4:8], in1=SC[:, 0, :], op=ALU.subtract),
              reads=[bpP[j], bSC], writes=[bSC])
        P.act(lambda e, SC=SC, ps=ps: e.activation(out=SC[:, 6:8, :], in_=ps[:, 8:16].rearrange("p (a h) -> p a h", a=2),
                                                  func=AF.Exp), reads=[bpP[j]], writes=[bSC])
        P.dve(lambda e, SC=SC: e.tensor_scalar(out=SC[:, 1, :], in0=SC[:, 0, :], scalar1=-1.0, scalar2=None, op0=ALU.mult),
              reads=[bSC], writes=[bSC])
        P.dve(lambda e, SC=SC, j=j: e.tensor_tensor(out=SC[:, 2, :], in0=SC[:, 0, :], in1=LNB[:, j, :], op=ALU.add),
              reads=[bSC, bBG], writes=[bSC])
        P.act(lambda e, SC=SC: e.activation(out=SC[:, 3, :], in_=SC[:, 0, :], func=AF.Exp), reads=[bSC], writes=[bSC])
        P.act(lambda e, SC=SC: e.activation(out=SC[:, 5, :], in_=SC[:, 5, :], func=AF.Exp), reads=[bSC], writes=[bSC])
        P.dve(lambda e, SC=SC, j=j: e.scalar_tensor_tensor(out=SC[:, 4, :], in0=SC[:, 3, :], scalar=-1.0,
                                                          in1=BETA[:, j, :], op0=ALU.mult, op1=ALU.mult),
              reads=[bSC, bBG], writes=[bSC])
        yield
    for j in range(NP):
        pb = PBUF[j]
        P.pool(lambda e, pb=pb, j=j: e.tensor_copy(out=pb["GB"], in_=bc4(GG[:, j, :])), reads=[bBG], writes=[pb["bGB"]])
        yield
    for j in range(NP):
        pb = PBUF[j]
        for h in range(H):
            P.pe(lambda e, pb=pb, j=j, h=h: e.matmul(pP[j][:, h, :], lhsT=pb["GB"][:, h, :], rhs=mask2,
                                                     start=True, stop=True),
                 reads=[pb["bGB"], bc], writes=[bpP[j]])
        yield
    for j in range(NP):
        pb = PBUF[j]
        SC = pb["SC"]
        P.dve(lambda e, pb=pb, j=j, SC=SC: e.tensor_tensor(out=pb["E1"], in0=pP[j], in1=bc4(SC[:, 0, :]), op=ALU.max),
              reads=[bpP[j], pb["bSC"]], writes=[pb["bE1"]])
        if full:
            P.dve(lambda e, pb=pb, j=j, SC=SC: e.tensor_tensor(out=pb["E2"], in0=pP[j], in1=bc4(SC[:, 0, :]), op=ALU.min),
                  reads=[bpP[j], pb["bSC"]], writes=[pb["bE2"]])
            P.act(lambda e, pb=pb, j=j: e.activation(out=pb["EGR"], in_=pP[j], func=AF.Exp),
                  reads=[bpP[j]], writes=[pb["bEGR"]])
        yield
    for j in range(NP):
        pb = PBUF[j]
        SC = pb["SC"]
        for h in range(H):
            P.act(lambda e, pb=pb, h=h, SC=SC: e.activation(out=pb["E1"][:, h, :], in_=pb["E1"][:, h, :], func=AF.Exp,
                                                            scale=-1.0, bias=SC[:, 2, h:h + 1]),
                  reads=[pb["bE1"], pb["bSC"]], writes=[pb["bE1"]])
        if full:
            for h in range(H):
                P.act(lambda e, pb=pb, h=h, SC=SC: e.activation(out=pb["E2"][:, h, :], in_=pb["E2"][:, h, :], func=AF.Exp,
                                                                bias=SC[:, 1, h:h + 1]),
                      reads=[pb["bE2"], pb["bSC"]], writes=[pb["bE2"]])
        yield
    for j in range(NP):
        pb = PBUF[j]
        P.pool(lambda e, pb=pb: e.tensor_tensor(out=pb["E1"], in0=pb["E1"],
                                                in1=maskL.unsqueeze(1).to_broadcast([128, H, 128]), op=ALU.mult),
               reads=[pb["bE1"], bc], writes=[pb["bE1"]])
        if full:
            P.pool(lambda e, pb=pb: e.tensor_tensor(out=pb["E2"], in0=pb["E2"],
                                                    in1=mask2.unsqueeze(1).to_broadcast([128, H, 128]), op=ALU.mult),
                   reads=[pb["bE2"], bc], writes=[pb["bE2"]])
            c0 = j * 128
            P.dve(lambda e, pb=pb, c0=c0: e.tensor_tensor(out=pb["QTG"], in0=QT[:, :, c0:c0 + 128], in1=pb["EGR"], op=ALU.mult),
                  reads=bQT + [pb["bEGR"]], writes=[pb["bQTG"]])
        yield
    for j in range(NP):
        c0 = j * 128
        for h in range(H):
            P.pe(lambda e, j=j, h=h, c0=c0: e.matmul(pP[j][:, h, :], lhsT=KT[:, h, c0:c0 + 128], rhs=KT[:, h, c0:c0 + 128],
                                                     start=True, stop=True), reads=[bKT[h]], writes=[bpP[j]])
        yield
    for j in range(NP):
        pb = PBUF[j]
        P.dve(lambda e, pb=pb, j=j: e.tensor_tensor(out=pb["Lm"][0], in0=pP[j], in1=pb["E1"], op=ALU.mult),
              reads=[bpP[j], pb["bE1"]], writes=[pb["bLm"][0]])
        yield
    if full:
        for j in range(NP):
            c0 = j * 128
            for h in range(H):
                P.pe(lambda e, j=j, h=h, c0=c0: e.matmul(pP[j][:, h, :], lhsT=KT[:, h, c0:c0 + 128],
                                                         rhs=QT[:, h, c0:c0 + 128], start=True, stop=True),
                     reads=[bKT[h], bQT[h]], writes=[bpP[j]])
            yield
        for j in range(NP):
            pb = PBUF[j]
            P.dve(lambda e, pb=pb, j=j: e.tensor_tensor(out=pb["ATT"], in0=pP[j], in1=pb["E2"], op=ALU.mult),
                  reads=[bpP[j], pb["bE2"]], writes=[pb["bATT"]])
            yield
    for j in range(NP):
        pb = PBUF[j]
        for h in range(H):
            P.pe(lambda e, pb=pb, j=j, h=h: e.transpose(out=pPb[j][:, h, :], in_=pb["Lm"][0][:, h, :], identity=ident),
                 reads=[pb["bLm"][0], bc], writes=[bpP[j]])
        yield
    for j in range(NP):
        pb = PBUF[j]
        evac(pb["Um"][0], pPb[j], [bpP[j]], [pb["bUm"][0]])
        yield
    for j in range(NP):
        pb = PBUF[j]
        P.pool(lambda e, pb=pb: e.tensor_tensor(out=pb["Xm"][0], in0=identb4, in1=pb["Um"][0], op=ALU.subtract),
               reads=[pb["bUm"][0], bc], writes=[pb["bXm"][0]])
        yield
    cur, cx = 0, 0
    for lvl in range(5):
        for j in range(NP):
            pb = PBUF[j]
            for h in range(H):
                P.pe(lambda e, pb=pb, j=j, h=h, cur=cur: e.matmul(pP[j][:, h, :], lhsT=pb["Um"][cur][:, h, :],
                                                                  rhs=pb["Lm"][cur][:, h, :], start=True, stop=True),
                     reads=[pb["bUm"][cur], pb["bLm"][cur]], writes=[bpP[j]])
            yield
        for j in range(NP):
            pb = PBUF[j]
            evac(pb["Lm"][1 - cur], pP[j], [bpP[j]], [pb["bLm"][1 - cur]])
            yield
        if lvl < 4:
            for j in range(NP):
                pb = PBUF[j]
                for h in range(H):
                    P.pe(lambda e, pb=pb, j=j, h=h, cur=cur: e.matmul(pP[j][:, h, :], lhsT=pb["Lm"][cur][:, h, :],
                                                                      rhs=pb["Um"][cur][:, h, :], start=True, stop=True),
                         reads=[pb["bUm"][cur], pb["bLm"][cur]], writes=[bpP[j]])
                yield
            for j in range(NP):
                pb = PBUF[j]
                evac(pb["Um"][1 - cur], pP[j], [bpP[j]], [pb["bUm"][1 - cur]])
                yield
        for j in range(NP):
            pb = PBUF[j]
            for h in range(H):
                P.pe(lambda e, pb=pb, j=j, h=h, cur=cur, cx=cx: e.matmul(pP[j][:, h, :], lhsT=pb["Lm"][1 - cur][:, h, :],
                                                                         rhs=pb["Xm"][cx][:, h, :], start=True, stop=False),
                     reads=[pb["bLm"][1 - cur], pb["bXm"][cx]], writes=[bpP[j]])
                P.pe(lambda e, pb=pb, j=j, h=h, cx=cx: e.matmul(pP[j][:, h, :], lhsT=ident, rhs=pb["Xm"][cx][:, h, :],
                                                                start=False, stop=True),
                     reads=[pb["bXm"][cx], bc], writes=[bpP[j]])
            yield
        for j in range(NP):
            pb = PBUF[j]
            if lvl == 4:
                evac(pb["TT"], pP[j], [bpP[j]], [pb["bTT"]])
            else:
                evac(pb["Xm"][1 - cx], pP[j], [bpP[j]], [pb["bXm"][1 - cx]])
            yield
        cur, cx = 1 - cur, 1 - cx
    for j in range(NP):
        pb = PBUF[j]
        c0 = j * 128
        SC = pb["SC"]
        for h in range(H):
            P.pe(lambda e, h=h, c0=c0, j=j: e.transpose(out=pPb[j][:, h, :], in_=KT[:, h, c0:c0 + 128], identity=ident),
                 reads=[bKT[h], bc], writes=[bpP[j]])
        P.dve(lambda e, pb=pb, SC=SC, j=j: e.tensor_tensor(out=pb["KHT"], in0=pPb[j], in1=bc4(SC[:, 5, :]), op=ALU.mult),
              reads=[bpP[j], pb["bSC"]], writes=[pb["bKHT"]])
        for h in range(H):
            P.pe(lambda e, h=h, c0=c0, j=j: e.transpose(out=pPb[j][:, h, :], in_=VT[:, h, c0:c0 + 128], identity=ident),
                 reads=[bVT[h], bc], writes=[bpP[j]])
        P.act(lambda e, pb=pb, j=j: e.activation(out=pb["VTK"], in_=pPb[j], func=AF.Copy), reads=[bpP[j]], writes=[pb["bVTK"]])
        P.pool(lambda e, pb=pb, c0=c0: e.tensor_copy(out=pb["KTP"], in_=KT[:, :, c0:c0 + 128]), reads=bKT, writes=[pb["bKTP"]])
        P.pool(lambda e, pb=pb, j=j: e.tensor_tensor(out=pb["BV"], in0=pb["VTK"], in1=bc4(BETA[:, j, :]), op=ALU.mult),
               reads=[pb["bVTK"], bBG], writes=[pb["bBV"]])
        yield


def _gdn_rec(self, L, full):
    P = self.P
    H = 4
    g = lambda n: L[n]
    bk, bb = self.banks, self.bbank
    bc = self.bconst
    mask2, ident = self.mask2, self.ident
    maskL, bones, ch01, identb4 = g("maskL"), g("bones"), g("ch01"), g("identb4")
    PBUF, GG, LNB, BETA, bBG = g("PB"), g("GG"), g("LNB"), g("BETA"), g("bBG")
    QT, KT, VT, bQT, bKT, bVT = g("QT"), g("KT"), g("VT"), g("bQT"), g("bKT"), g("bVT")
    R, USB, bR, bUSB = g("R"), g("USB"), g("bR"), g("bUSB")
    S32, SBF, bS32, bSBF, sbf_i = g("S32"), g("SBF"), g("bS32"), g("bSBF"), g("sbf_i")
    O32, bO32 = g("O32"), g("bO32")
    evac, nextpj = g("evac"), g("nextpj")
    NP = NT
    pP = [bk[3 + j][:].rearrange("p (h t) -> p h t", h=H) for j in range(NP)]
    pPb = [bk[3 + j][:].bitcast(BF16)[:, 0:512].rearrange("p (h t) -> p h t", h=H) for j in range(NP)]
    bpP = [bb[3 + j] for j in range(NP)]
    ptr4 = bk[5][:].bitcast(BF16)[:, 0:512].rearrange("p (h t) -> p h t", h=H)
    pKS = bk[5][:].rearrange("p (h t) -> p h t", h=H)
    pU = bk[6][:].rearrange("p (h t) -> p h t", h=H)
    po = bk[7][:].rearrange("p (h t) -> p h t", h=H)

    def bc4(ap):
        return ap.unsqueeze(2).to_broadcast([128, H, 128])

    for j in range(NP):
        pb = PBUF[j]
        c0 = j * 128
        SC = pb["SC"]
        TT, bTT = pb["TT"], pb["bTT"]
        for half in range(2):
            r0 = half * 64
            cs = sbf_i[0]
            for h in range(H):
                P.pe(lambda e, h=h, pb=pb, cs=cs: e.matmul(pKS[:, h, :], lhsT=pb["KTP"][:, h, :], rhs=SBF[cs][:, h, :],
                                                           start=True, stop=True),
                     reads=[pb["bKTP"], bSBF[cs][h]], writes=[bb[5]])
            if full:
                for h in range(H):
                    P.pe(lambda e, pb=pb, h=h, cs=cs, r0=r0, half=half: e.matmul(
                        po[:, h, r0:r0 + 64], lhsT=SBF[cs][:, h, :], rhs=pb["QTG"][:, h, r0:r0 + 64],
                        start=(h == 0 and half == 0), stop=False, skip_group_check=True),
                        reads=[bSBF[cs][h], pb["bQTG"]], writes=[bb[7]])
            for h in range(H):
                P.dve(lambda e, pb=pb, h=h, r0=r0, SC=SC: e.scalar_tensor_tensor(
                    out=R[r0:r0 + 64, h, :], in0=pKS[r0:r0 + 64, h, :], scalar=SC[r0:r0 + 64, 4, h:h + 1],
                    in1=pb["BV"][r0:r0 + 64, h, :], op0=ALU.mult, op1=ALU.add),
                    reads=[bb[5], pb["bSC"], pb["bBV"]], writes=[bR])
            yield
            for h in range(H):
                P.pe(lambda e, h=h, r0=r0, TT=TT: e.matmul(pU[:, h, :], lhsT=TT[r0:r0 + 64, h, :], rhs=R[r0:r0 + 64, h, :],
                                                           start=True, stop=True),
                     reads=[bTT, bR], writes=[bb[6]])
            yield
            P.act(lambda e, r0=r0: e.activation(out=USB[r0:r0 + 64], in_=pU[r0:r0 + 64], func=AF.Copy),
                  reads=[bb[6]], writes=[bUSB])
            yield
            pS, bpS = bk[6][:], bb[6]
            pS4 = pS.rearrange("p (h t) -> p h t", h=H)
            for h in range(H):
                P.pe(lambda e, pb=pb, h=h, r0=r0, pS4=pS4: e.matmul(pS4[:, h, :], lhsT=pb["KHT"][r0:r0 + 64, h, :],
                                                                    rhs=USB[r0:r0 + 64, h, :], start=True, stop=True),
                     reads=[pb["bKHT"], bUSB], writes=[bpS])
            yield
            for h in range(H):
                P.dve(lambda e, h=h, half=half, SC=SC, pS4=pS4: e.scalar_tensor_tensor(
                    out=S32[:, h, :], in0=S32[:, h, :], scalar=SC[:, 6 + half, h:h + 1], in1=pS4[:, h, :],
                    op0=ALU.mult, op1=ALU.add), reads=[bS32[h], pb["bSC"], bpS], writes=[bS32[h]])
            yield
            nx = 1 - cs
            P.act(lambda e, nx=nx: e.activation(out=SBF[nx], in_=S32, func=AF.Copy), reads=bS32, writes=bSBF[nx])
            sbf_i[0] = nx
            yield
        if full:
            for h in range(H):
                P.pe(lambda e, pb=pb, h=h: e.matmul(po[:, h, :], lhsT=USB[:, h, :], rhs=pb["ATT"][:, h, :],
                                                    start=False, stop=True, skip_group_check=True),
                     reads=[bUSB, pb["bATT"]], writes=[bb[7]])
            P.act(lambda e, c0=c0: e.activation(out=O32[:, :, c0:c0 + 128], in_=po, func=AF.Copy),
                  reads=[bb[7]], writes=[bO32])


KB._gdn_prep = _gdn_prep
KB._gdn_rec = _gdn_rec


CAP = 384
NEXP = 32
NSLOT = NEXP * CAP
BIG = 1.0e4


def _spill(self, name, sb_ap, bufs, sb):
    P = self.P
    if name not in self.d:
        self.d[name] = self.nc.dram_tensor(name, [128, 4, self.nfull_tok], BF16, kind="Internal").ap()
        self.bspill = getattr(self, "bspill", {})
        self.bspill[name] = {}
    dst = self.d[name]
    tok0 = (sb - self.npre) * SBT
    b = Buf(name)
    self.bspill[name][sb - self.npre] = b
    P.dma(lambda e: e.dma_start(out=dst[:, :, tok0:tok0 + SBT], in_=sb_ap), reads=bufs, writes=[b])


KB.spill = _spill


def _pass3(self):
    nc, P, A = self.nc, self.P, self.A
    d = self.d
    H = 4
    for nm, shp in (("hg_up", [512, D]), ("gd_up", [512, D]), ("w_out", [D, D]), ("norm_ffn_g", [D]),
                    ("router_w", [D, 36]), ("router_b", [36])):
        self.din(nm, shp)
    ntile = self.nfull_tok // 128
    d["h2_s"] = nc.dram_tensor("h2_s", [self.nfull_tok, D], F32, kind="Internal").ap()
    self.bh2s = [Buf("h2_s%d" % i) for i in range(ntile)]
    self.bxbuf = []
    self.W12 = A.alloc((ntile, 2), F32)
    self.DST = A.alloc((ntile, 2), U32)
    self.bW12 = Buf("W12")
    self.bDST = Buf("DST")
    m0 = A.mark()
    W3 = A.alloc((KC, 2048), BF16)
    bW3 = [Buf() for _ in range(KC)]
    wv = d["w_in"].rearrange("(k p) n -> p k n", p=128)
    for k in range(KC):
        P.dma(lambda e, k=k: e.dma_start(out=W3[:, k, :], in_=wv[:, k, C_PA:C_PA + 2048]), writes=[bW3[k]], q="pool")
    HGUP = A.alloc((H, D), BF16)
    GDUP = A.alloc((H, D), BF16)
    WOUT = A.alloc((KC, D), BF16)
    bUP = Buf()
    bWO = [Buf() for _ in range(KC)]
    P.dma(lambda e: e.dma_start(out=HGUP, in_=d["hg_up"].rearrange("(h p) n -> p h n", p=128)), writes=[bUP], q="pool")
    P.dma(lambda e: e.dma_start(out=GDUP, in_=d["gd_up"].rearrange("(h p) n -> p h n", p=128)), writes=[bUP], q="pool")
    wo = d["w_out"].rearrange("(k p) n -> p k n", p=128)
    for k in range(KC):
        P.dma(lambda e, k=k: e.dma_start(out=WOUT[:, k, :], in_=wo[:, k, :]), writes=[bWO[k]], q="pool")
    WR = A.alloc((KC, 36), F32)
    RB = A.alloc((36,), F32)
    G2 = A.alloc((D,), F32)
    ECAP = A.alloc((NEXP,), F32)
    bpar = Buf("par3")
    P.dma(lambda e: e.dma_start(out=WR, in_=d["router_w"].rearrange("(k p) n -> p k n", p=128)), writes=[bpar])
    P.dma(lambda e: e.dma_start(out=RB, in_=d["router_b"].partition_broadcast(128)), writes=[bpar])
    P.dma(lambda e: e.dma_start(out=G2, in_=d["norm_ffn_g"].partition_broadcast(128)), writes=[bpar])
    self.load_gain("norm_mix_g")
    ecapi = A.alloc((NEXP,), I32)
    P.pool(lambda e: e.iota(ecapi, pattern=[[CAP, NEXP]], base=0, channel_multiplier=0), writes=[bpar])
    P.pool(lambda e: e.tensor_copy(out=ECAP, in_=ecapi), reads=[bpar], writes=[bpar])
    triS = A.alloc((128,), BF16)
    trif = A.alloc((128,), F32)
    bc = self.bconst
    P.pool(lambda e: e.memset(trif, 1.0), writes=[bc])
    P.pool(lambda e: e.affine_select(out=trif, in_=trif, pattern=[[1, 128]], compare_op=ALU.is_gt, fill=0.0,
                                     base=0, channel_multiplier=-1), reads=[bc], writes=[bc])
    P.pool(lambda e: e.tensor_copy(out=triS, in_=trif), reads=[bc], writes=[bc])
    BASE = A.alloc((NEXP,), F32)
    bBASE = Buf()
    P.pool(lambda e: e.tensor_copy(out=BASE, in_=ecapi), reads=[bpar], writes=[bBASE])

    xnT = A.alloc((KC, SBT), BF16)
    bxnT = Buf()
    SGs = [A.alloc((16, SBT), BF16) for _ in range(2)]
    bSGs = [[Buf() for _ in range(16)] for _ in range(2)]
    OAss = [A.alloc((H, SBT), BF16) for _ in range(2)]
    OBss = [A.alloc((H, SBT), BF16) for _ in range(2)]
    bOAss, bOBss = [Buf(), Buf()], [Buf(), Buf()]
    T1 = [A.alloc((SBT,), F32) for _ in range(2)]
    T2 = [A.alloc((SBT,), F32) for _ in range(2)]
    bT1 = [Buf(), Buf()]
    bT2 = [Buf(), Buf()]
    MG = A.alloc((KC, SBT), BF16)
    bMG = [Buf() for _ in range(KC)]
    XR = A.alloc((D,), F32)
    bXR = Buf()
    H2 = A.alloc((D,), F32)
    bH2 = Buf()
    JK = A.alloc((D,), BF16)
    SS2 = A.alloc((1,), F32)
    bSS2 = Buf()
    XF = A.alloc((D,), F32)
    XB = A.alloc((D,), BF16)
    bXF, bXB = Buf(), Buf()
    XFT = A.alloc((KC, 128), F32)
    bXFT = Buf()
    LG = A.alloc((36,), F32)
    ME = A.alloc((NEXP,), F32)
    SM = A.alloc((16,), F32)
    M8 = A.alloc((8,), F32)
    SEL1 = A.alloc((NEXP,), F32)
    SEL2 = A.alloc((NEXP,), F32)
    SELB = A.alloc((NEXP,), BF16)
    RK = A.alloc((NEXP,), F32)
    JK2 = A.alloc((NEXP,), F32)
    DF = A.alloc((2,), F32)
    brt = Buf("route")

    bk, bb = self.banks, self.bbank
    ptr = bk[0][:].bitcast(BF16).rearrange("p (k t) -> p k t", k=KC)
    pj = [bk[1][:], bk[2][:]]
    bpj = [bb[1], bb[2]]
    pup = [bk[3][:], bk[4][:]]
    pji = [0]

    def nextpj():
        i = pji[0] % 2
        pji[0] += 1
        return pj[i], bpj[i]

    pyi = [0]

    def nextpy():
        i = 5 + pyi[0] % 3
        pyi[0] += 1
        return bk[i][:], bb[i]

    oa_d, ob_d = d["oa_s"], d["ob_s"]
    def X3(sbi):
        sl = sbi % 2
        SG, bSG, OAs, OBs, bOAs, bOBs = SGs[sl], bSGs[sl], OAss[sl], OBss[sl], bOAss[sl], bOBss[sl]
        sb = self.npre + sbi
        tok0 = sbi * SBT
        yield from self.stage_a_gen(sb, xnT, bxnT, ptr, bb[0])
        P.dma(lambda e, tok0=tok0: e.dma_start(out=OAs, in_=oa_d[:, :, tok0:tok0 + SBT]),
              reads=[self.bspill["oa_s"][sbi]], writes=[bOAs])
        P.dma(lambda e, tok0=tok0: e.dma_start(out=OBs, in_=ob_d[:, :, tok0:tok0 + SBT]),
              reads=[self.bspill["ob_s"][sbi]], writes=[bOBs])
        for cb in range(16):
            p, bp = nextpj()
            for k in range(KC):
                P.pe(lambda e, k=k, p=p, cb=cb: e.matmul(p[:, 0:SBT], lhsT=W3[:, k, cb * 128:(cb + 1) * 128],
                                                         rhs=xnT[:, k, :], start=(k == 0), stop=(k == KC - 1)),
                     reads=[bW3[k], bxnT], writes=[bp])
            P.act(lambda e, cb=cb, p=p: e.activation(out=SG[:, cb, :], in_=p[:, 0:SBT], func=AF.Sigmoid),
                  reads=[bp], writes=[bSG[cb]])
            yield
    def Y3(sbi):
        sl = sbi % 2
        SG, bSG, OAs, OBs, bOAs, bOBs = SGs[sl], bSGs[sl], OAss[sl], OBss[sl], bOAss[sl], bOBss[sl]
        sb = self.npre + sbi
        tok0 = sbi * SBT
        for cb in range(KC):
            i = cb % 2
            for h in range(H):
                P.pe(lambda e, h=h, cb=cb: e.matmul(pup[0][:, 0:SBT], lhsT=HGUP[:, h, cb * 128:(cb + 1) * 128],
                                                    rhs=OAs[:, h, :], start=(h == 0), stop=(h == H - 1)),
                     reads=[bUP, bOAs], writes=[bb[3]])
            for h in range(H):
                P.pe(lambda e, h=h, cb=cb: e.matmul(pup[1][:, 0:SBT], lhsT=GDUP[:, h, cb * 128:(cb + 1) * 128],
                                                    rhs=OBs[:, h, :], start=(h == 0), stop=(h == H - 1)),
                     reads=[bUP, bOBs], writes=[bb[4]])
            P.dve(lambda e, cb=cb, i=i: e.tensor_tensor(out=T1[i], in0=pup[0][:, 0:SBT], in1=SG[:, cb, :], op=ALU.mult),
                  reads=[bb[3], bSG[cb]], writes=[bT1[i]])
            P.dve(lambda e, cb=cb, i=i: e.tensor_tensor(out=T2[i], in0=pup[1][:, 0:SBT], in1=SG[:, 8 + cb, :], op=ALU.mult),
                  reads=[bb[4], bSG[8 + cb]], writes=[bT2[i]])
            P.pool(lambda e, cb=cb, i=i: e.tensor_tensor(out=MG[:, cb, :], in0=T1[i], in1=T2[i], op=ALU.add),
                   reads=[bT1[i], bT2[i]], writes=[bMG[cb]])
            yield
        for t in range(NT):
            gt = sbi * NT + t
            gtok = self.npre * SBT + gt * 128
            P.dma(lambda e, gtok=gtok: e.dma_start(out=XR, in_=d["xs"][gtok:gtok + 128, :]), writes=[bXR])
            for half in range(2):
                p, bp = nextpy()
                for k in range(KC):
                    P.pe(lambda e, k=k, p=p, t=t, half=half: e.matmul(
                        p, lhsT=MG[:, k, t * 128:(t + 1) * 128], rhs=WOUT[:, k, half * 512:(half + 1) * 512],
                        start=(k == 0), stop=(k == KC - 1)), reads=[bMG[k], bWO[k]], writes=[bp])
                P.dve(lambda e, p=p, half=half: e.tensor_tensor(out=H2[:, half * 512:(half + 1) * 512], in0=p,
                                                               in1=XR[:, half * 512:(half + 1) * 512], op=ALU.add),
                      reads=[bp, bXR], writes=[bH2])
                yield
            P.dma(lambda e, gt=gt: e.dma_start(out=d["h2_s"][gt * 128:(gt + 1) * 128, :], in_=H2),
                  reads=[bH2], writes=[self.bh2s[gt]])
            LL = dict(L0)
            LL['nextpj'] = nextpy
            yield from self._route_tile(LL, gt)
    L0 = dict(locals())

    import os
    WTS3 = [int(v) for v in os.environ.get("IL_W3", "1,1").split(",")]

    def run_il(gens):
        gens = list(gens)
        while gens:
            for g_, w_ in list(gens):
                for _ in range(w_):
                    try:
                        next(g_)
                    except StopIteration:
                        gens.remove((g_, w_))
                        break

    n = self.nfull
    for r in range(-1, n):
        gs = []
        if 0 <= r < n:
            gs.append((Y3(r), WTS3[0]))
        if 0 <= r + 1 < n:
            gs.append((X3(r + 1), WTS3[1]))
        run_il(gs)
    A.reset(m0)
    P.barrier()


KB.pass3 = _pass3


def _route_tile(self, L, gt):
    P = self.P
    g = lambda n: L[n]
    bk, bb = self.banks, self.bbank
    bc = self.bconst
    H2, bH2, JK, SS2, bSS2 = g("H2"), g("bH2"), g("JK"), g("SS2"), g("bSS2")
    XF, XB, bXF, bXB, XFT, bXFT = g("XF"), g("XB"), g("bXF"), g("bXB"), g("XFT"), g("bXFT")
    G2, WR, RB, ECAP, bpar = g("G2"), g("WR"), g("RB"), g("ECAP"), g("bpar")
    LG, ME, SM, M8, SEL1, SEL2, SELB, RK, JK2, DF, brt = (g("LG"), g("ME"), g("SM"), g("M8"), g("SEL1"), g("SEL2"),
                                                           g("SELB"), g("RK"), g("JK2"), g("DF"), g("brt"))
    BASE, bBASE, triS = g("BASE"), g("bBASE"), g("triS")
    nextpj = g("nextpj")
    W12, DST = self.W12, self.DST
    P.act(lambda e: e.activation(out=JK, in_=H2, func=AF.Square, accum_out=SS2), reads=[bH2], writes=[bSS2])
    P.act(lambda e: e.activation(out=SM[:, 0:1], in_=SS2, func=AF.Ln, scale=1.0 / D, bias=EPS), reads=[bSS2], writes=[brt])
    P.act(lambda e: e.activation(out=SM[:, 0:1], in_=SM[:, 0:1], func=AF.Exp, scale=-0.5), reads=[brt], writes=[brt])
    P.dve(lambda e: e.scalar_tensor_tensor(out=XF, in0=H2, scalar=SM[:, 0:1], in1=G2, op0=ALU.mult, op1=ALU.mult),
          reads=[bH2, brt, bpar], writes=[bXF])
    P.pool(lambda e: e.tensor_copy(out=XB, in_=XF), reads=[bXF], writes=[bXB])
    yield
    for half in range(2):
        for kk in range(4):
            k = half * 4 + kk
            P.pe(lambda e, k=k, kk=kk, half=half: e.transpose(out=bk[3 + half][:, kk * 128:(kk + 1) * 128],
                                                              in_=XF[:, k * 128:(k + 1) * 128], identity=self.identf),
                 reads=[bXF, bc], writes=[bb[3 + half]])
    P.act(lambda e: e.activation(out=XFT[:, 0:4, :], in_=bk[3][:].rearrange("p (k t) -> p k t", k=4), func=AF.Copy),
          reads=[bb[3]], writes=[bXFT])
    P.dve(lambda e: e.tensor_copy(out=XFT[:, 4:8, :], in_=bk[4][:].rearrange("p (k t) -> p k t", k=4)),
          reads=[bb[4]], writes=[bXFT])
    yield
    p, bp = nextpj()
    for k in range(KC):
        P.pe(lambda e, k=k, p=p: e.matmul(p[:, 0:36], lhsT=XFT[:, k, :], rhs=WR[:, k, :], start=(k == 0), stop=(k == KC - 1)),
             reads=[bXFT, bpar], writes=[bp])
    P.dve(lambda e, p=p: e.tensor_tensor(out=LG, in0=p[:, 0:36], in1=RB, op=ALU.add), reads=[bp, bpar], writes=[brt])
    yield
    P.dve(lambda e: e.tensor_reduce(out=SM[:, 1:2], in_=LG[:, 0:4], axis=AX.X, op=ALU.max), reads=[brt], writes=[brt])
    P.dve(lambda e: e.tensor_scalar(out=SM[:, 2:3], in0=SM[:, 1:2], scalar1=-1.0, scalar2=None, op0=ALU.mult),
          reads=[brt], writes=[brt])
    P.act(lambda e: e.activation(out=JK2[:, 0:4], in_=LG[:, 0:4], func=AF.Exp, bias=SM[:, 2:3], accum_out=SM[:, 3:4]),
          reads=[brt], writes=[brt])
    P.dve(lambda e: e.reciprocal(out=SM[:, 4:5], in_=SM[:, 3:4]), reads=[brt], writes=[brt])
    yield
    P.dve(lambda e: e.tensor_scalar(out=SM[:, 12:16], in0=LG[:, 0:4], scalar1=SM[:, 1:2], scalar2=-1.0,
                                    op0=ALU.is_equal, op1=ALU.add), reads=[brt], writes=[brt])
    P.dve(lambda e: e.scalar_tensor_tensor(out=ME.rearrange("p (g j) -> p g j", g=4),
                                           in0=SM[:, 12:16].unsqueeze(2).to_broadcast([128, 4, 8]), scalar=BIG,
                                           in1=LG[:, 4:36].rearrange("p (g j) -> p g j", g=4),
                                           op0=ALU.mult, op1=ALU.add), reads=[brt], writes=[brt])
    P.dve(lambda e: e.max(out=M8, in_=ME), reads=[brt], writes=[brt])
    yield
    P.dve(lambda e: e.tensor_tensor(out=SM[:, 5:6], in0=M8[:, 1:2], in1=M8[:, 0:1], op=ALU.subtract), reads=[brt], writes=[brt])
    P.act(lambda e: e.activation(out=SM[:, 6:7], in_=SM[:, 5:6], func=AF.Exp), reads=[brt], writes=[brt])
    P.dve(lambda e: e.tensor_scalar(out=SM[:, 7:8], in0=SM[:, 6:7], scalar1=1.0, scalar2=None, op0=ALU.add),
          reads=[brt], writes=[brt])
    P.dve(lambda e: e.reciprocal(out=SM[:, 8:9], in_=SM[:, 7:8]), reads=[brt], writes=[brt])
    P.dve(lambda e, gt=gt: e.tensor_tensor(out=W12[:, gt, 0:1], in0=SM[:, 8:9], in1=SM[:, 4:5], op=ALU.mult),
          reads=[brt], writes=[self.bW12])
    P.dve(lambda e, gt=gt: e.tensor_tensor(out=W12[:, gt, 1:2], in0=SM[:, 4:5], in1=W12[:, gt, 0:1], op=ALU.subtract),
          reads=[brt, self.bW12], writes=[self.bW12])
    P.dve(lambda e: e.tensor_scalar(out=SEL1, in0=ME, scalar1=M8[:, 0:1], scalar2=None, op0=ALU.is_equal),
          reads=[brt], writes=[brt])
    P.dve(lambda e: e.tensor_scalar(out=SEL2, in0=ME, scalar1=M8[:, 1:2], scalar2=None, op0=ALU.is_equal),
          reads=[brt], writes=[brt])
    P.dve(lambda e: e.tensor_tensor(out=SELB, in0=SEL1, in1=SEL2, op=ALU.add), reads=[brt], writes=[brt])
    yield
    p2, bp2 = nextpj()
    P.pe(lambda e, p2=p2: e.matmul(p2[:, 0:32], lhsT=triS, rhs=SELB, start=True, stop=True), reads=[brt, bc], writes=[bp2])
    P.pe(lambda e, p2=p2: e.matmul(p2[:, 32:64], lhsT=self.ones_bf, rhs=SELB, start=True, stop=True),
         reads=[brt, bc], writes=[bp2])
    P.dve(lambda e, p2=p2: e.tensor_tensor(out=RK, in0=p2[:, 0:32], in1=BASE, op=ALU.add), reads=[bp2, bBASE], writes=[brt])
    P.dve(lambda e, p2=p2: e.tensor_tensor(out=BASE, in0=p2[:, 32:64], in1=BASE, op=ALU.add), reads=[bp2, bBASE], writes=[bBASE])
    P.dve(lambda e: e.scalar_tensor_tensor(out=JK2, in0=SEL1, scalar=1.0, in1=RK, op0=ALU.mult, op1=ALU.mult,
                                           accum_out=DF[:, 0:1]), reads=[brt], writes=[brt])
    P.dve(lambda e: e.scalar_tensor_tensor(out=JK2, in0=SEL2, scalar=1.0, in1=RK, op0=ALU.mult, op1=ALU.mult,
                                           accum_out=DF[:, 1:2]), reads=[brt], writes=[brt])
    P.dve(lambda e, gt=gt: e.tensor_copy(out=DST[:, gt, :], in_=DF), reads=[brt], writes=[self.bDST])
    yield
    xb = self.d["x_buf"]
    for k in range(2):
        bx = Buf("xbuf")
        self.bxbuf.append(bx)
        P.dma(lambda e, gt=gt, k=k: e.indirect_dma_start(
            out=xb, out_offset=bass.IndirectOffsetOnAxis(ap=DST[:, gt, k:k + 1], axis=0), in_=XB, in_offset=None),
            reads=[bXB, self.bDST] + self.bxz, writes=[bx], q="pool")


KB._route_tile = _route_tile


def _pass4(self):
    nc, P, A = self.nc, self.P, self.A
    d = self.d
    self.din("w_gate", [NEXP, D, 512])
    self.din("w_up", [NEXP, D, 512])
    self.din("w_down", [NEXP, 512, D])
    d["y_buf"] = nc.dram_tensor("y_buf", [NSLOT, D], F32, kind="Internal").ap()
    self.bybuf = []
    m0 = A.mark()
    NB = CAP // 128
    WG = [A.alloc((KC, 512), BF16) for _ in range(2)]
    WU = [A.alloc((KC, 512), BF16) for _ in range(2)]
    WD = [A.alloc((4, D), BF16) for _ in range(2)]
    bWG = [Buf(), Buf()]
    bWU = [Buf(), Buf()]
    bWD = [Buf(), Buf()]
    XE = [A.alloc((NB, D), BF16) for _ in range(2)]
    bXE = [Buf(), Buf()]
    XET = [A.alloc((KC, CAP), BF16) for _ in range(2)]
    bXET = [Buf(), Buf()]
    SGT = [A.alloc((CAP,), F32) for _ in range(2)]
    bSGT = [Buf(), Buf()]
    HT = A.alloc((4, CAP), BF16)
    bHT = [Buf() for _ in range(4)]
    YS = [A.alloc((D,), F32) for _ in range(2)]
    bYS = [Buf(), Buf()]
    bk, bb = self.banks, self.bbank
    ptr = bk[0][:].bitcast(BF16).rearrange("p (k t) -> p k t", k=KC)
    ident = self.ident
    bc = self.bconst
    xb, yb = d["x_buf"], d["y_buf"]
    cnt = [0]

    def bank(lo, n):
        i = lo + cnt[0] % n
        cnt[0] += 1
        return bk[i][:], bb[i]

    def load_w(e):
        i = e % 2
        P.dma(lambda eng, e=e, i=i: eng.dma_start(out=WG[i], in_=d["w_gate"][e].rearrange("(k p) n -> p k n", p=128)),
              writes=[bWG[i]], q="pool")
        P.dma(lambda eng, e=e, i=i: eng.dma_start(out=WU[i], in_=d["w_up"][e].rearrange("(k p) n -> p k n", p=128)),
              writes=[bWU[i]], q="pool")
        P.dma(lambda eng, e=e, i=i: eng.dma_start(out=WD[i], in_=d["w_down"][e].rearrange("(k p) n -> p k n", p=128)),
              writes=[bWD[i]], q="pool")

    def load_x(e):
        i = e % 2
        P.dma(lambda eng, e=e, i=i: eng.dma_start(out=XE[i], in_=xb[e * CAP:(e + 1) * CAP, :].rearrange("(b p) n -> p b n", p=128)),
              reads=self.bxbuf, writes=[bXE[i]])

    def transposes(e):
        i = e % 2
        for b in range(NB):
            pt, bpt = (ptr, bb[0]) if b % 2 == 0 else (ptr7, bb[7])
            for k in range(KC):
                P.pe(lambda eng, i=i, b=b, k=k, pt=pt: eng.transpose(out=pt[:, k, :], in_=XE[i][:, b, k * 128:(k + 1) * 128], identity=ident),
                     reads=[bXE[i], bc], writes=[bpt])
            if b % 2 == 0:
                P.act(lambda eng, b=b, i=i, pt=pt: eng.activation(out=XET[i][:, :, b * 128:(b + 1) * 128], in_=pt, func=AF.Copy),
                      reads=[bpt], writes=[bXET[i]])
            else:
                P.dve(lambda eng, b=b, i=i, pt=pt: eng.tensor_copy(out=XET[i][:, :, b * 128:(b + 1) * 128], in_=pt),
                      reads=[bpt], writes=[bXET[i]])

    ptr7 = bk[7][:].bitcast(BF16).rearrange("p (k t) -> p k t", k=KC)
    load_w(0)
    load_x(0)
    transposes(0)
    yi = 0
    for e in range(NEXP):
        i = e % 2
        if e + 1 < NEXP:
            load_w(e + 1)
            load_x(e + 1)
        for fc in range(4):
            pg, bpg = bk[1 + fc % 2][:], bb[1 + fc % 2]
            pu, bpu = bk[3 + fc % 2][:], bb[3 + fc % 2]
            for k in range(KC):
                P.pe(lambda eng, i=i, fc=fc, k=k, pg=pg: eng.matmul(pg[:, 0:CAP], lhsT=WG[i][:, k, fc * 128:(fc + 1) * 128],
                                                                    rhs=XET[i][:, k, :], start=(k == 0), stop=(k == KC - 1)),
                     reads=[bWG[i], bXET[i]], writes=[bpg])
            for k in range(KC):
                P.pe(lambda eng, i=i, fc=fc, k=k, pu=pu: eng.matmul(pu[:, 0:CAP], lhsT=WU[i][:, k, fc * 128:(fc + 1) * 128],
                                                                    rhs=XET[i][:, k, :], start=(k == 0), stop=(k == KC - 1)),
                     reads=[bWU[i], bXET[i]], writes=[bpu])
            j = fc % 2
            P.act(lambda eng, pg=pg, j=j: eng.activation(out=SGT[j], in_=pg[:, 0:CAP], func=AF.Silu), reads=[bpg], writes=[bSGT[j]])
            P.dve(lambda eng, pu=pu, j=j, fc=fc: eng.tensor_tensor(out=HT[:, fc, :], in0=pu[:, 0:CAP], in1=SGT[j], op=ALU.mult),
                  reads=[bpu, bSGT[j]], writes=[bHT[fc]])
        if e + 1 < NEXP:
            transposes(e + 1)
        for b in range(NB):
            ys, bys = YS[yi % 2], bYS[yi % 2]
            yi += 1
            for half in range(2):
                pd, bpd = bk[5 + half][:], bb[5 + half]
                for fc in range(4):
                    P.pe(lambda eng, i=i, b=b, fc=fc, half=half, pd=pd: eng.matmul(
                        pd, lhsT=HT[:, fc, b * 128:(b + 1) * 128], rhs=WD[i][:, fc, half * 512:(half + 1) * 512],
                        start=(fc == 0), stop=(fc == 3)), reads=[bHT[fc], bWD[i]], writes=[bpd])
                if half == 0:
                    P.act(lambda eng, pd=pd, ys=ys: eng.activation(out=ys[:, 0:512], in_=pd, func=AF.Copy), reads=[bpd], writes=[bys])
                else:
                    P.dve(lambda eng, pd=pd, ys=ys: eng.tensor_copy(out=ys[:, 512:1024], in_=pd), reads=[bpd], writes=[bys])
            r0 = e * CAP + b * 128
            by = Buf("ybuf")
            self.bybuf.append(by)
            P.dma(lambda eng, r0=r0, ys=ys: eng.dma_start(out=yb[r0:r0 + 128, :], in_=ys), reads=[bys], writes=[by])
    A.reset(m0)
    P.barrier()


def _pass5(self):
    nc, P, A = self.nc, self.P, self.A
    d = self.d
    self.din("final_norm_g", [D])
    out = self.dout("out", [self.nfull_tok, D], F32)
    m0 = A.mark()
    ntile = self.nfull_tok // 128
    FG = A.alloc((D,), F32)
    bFG = Buf()
    P.dma(lambda e: e.dma_start(out=FG, in_=d["final_norm_g"].partition_broadcast(128)), writes=[bFG])
    NB5 = 4
    Y1 = [A.alloc((D,), F32) for _ in range(NB5)]
    Y2 = [A.alloc((D,), F32) for _ in range(NB5)]
    HH = [A.alloc((D,), F32) for _ in range(NB5)]
    OT = [A.alloc((D,), F32) for _ in range(NB5)]
    bY1, bY2, bHH, bOT = ([Buf() for _ in range(NB5)], [Buf() for _ in range(NB5)], [Buf() for _ in range(NB5)],
                          [Buf() for _ in range(NB5)])
    JK = A.alloc((D,), BF16)
    SS = A.alloc((ntile,), F32)
    bSS = Buf()
    yb = d["y_buf"]
    for gt in range(ntile):
        i = gt % NB5
        P.dma(lambda e, gt=gt, i=i: e.indirect_dma_start(
            out=Y1[i], out_offset=None, in_=yb, in_offset=bass.IndirectOffsetOnAxis(ap=self.DST[:, gt, 0:1], axis=0)),
            reads=self.bybuf + [self.bDST], writes=[bY1[i]], q="pool")
        P.dma(lambda e, gt=gt, i=i: e.indirect_dma_start(
            out=Y2[i], out_offset=None, in_=yb, in_offset=bass.IndirectOffsetOnAxis(ap=self.DST[:, gt, 1:2], axis=0)),
            reads=self.bybuf + [self.bDST], writes=[bY2[i]], q="pool")
        P.dma(lambda e, gt=gt, i=i: e.dma_start(out=HH[i], in_=d["h2_s"][gt * 128:(gt + 1) * 128, :]),
              reads=[self.bh2s[gt]], writes=[bHH[i]])
        P.dve(lambda e, gt=gt, i=i: e.scalar_tensor_tensor(out=HH[i], in0=Y1[i], scalar=self.W12[:, gt, 0:1], in1=HH[i],
                                                          op0=ALU.mult, op1=ALU.add),
              reads=[bY1[i], bHH[i], self.bW12], writes=[bHH[i]])
        P.dve(lambda e, gt=gt, i=i: e.scalar_tensor_tensor(out=HH[i], in0=Y2[i], scalar=self.W12[:, gt, 1:2], in1=HH[i],
                                                          op0=ALU.mult, op1=ALU.add),
              reads=[bY2[i], bHH[i], self.bW12], writes=[bHH[i]])
        P.act(lambda e, gt=gt, i=i: e.activation(out=JK, in_=HH[i], func=AF.Square, accum_out=SS[:, gt:gt + 1]),
              reads=[bHH[i]], writes=[bSS])
        P.act(lambda e, gt=gt: e.activation(out=SS[:, gt:gt + 1], in_=SS[:, gt:gt + 1], func=AF.Ln, scale=1.0 / D, bias=EPS),
              reads=[bSS], writes=[bSS])
        P.act(lambda e, gt=gt: e.activation(out=SS[:, gt:gt + 1], in_=SS[:, gt:gt + 1], func=AF.Exp, scale=-0.5),
              reads=[bSS], writes=[bSS])
        P.dve(lambda e, gt=gt, i=i: e.scalar_tensor_tensor(out=OT[i], in0=HH[i], scalar=SS[:, gt:gt + 1], in1=FG,
                                                          op0=ALU.mult, op1=ALU.mult),
              reads=[bHH[i], bSS, bFG], writes=[bOT[i]])
        P.dma(lambda e, gt=gt, i=i: e.dma_start(out=out[gt * 128:(gt + 1) * 128, :], in_=OT[i]), reads=[bOT[i]], out=True)
    A.reset(m0)


KB.pass4 = _pass4
KB.pass5 = _pass5


NPRE_SB = 17
NFULL_SB = 16
_NC_CACHE = {}


def _build_full():
    if "nc" not in _NC_CACHE:
        kb = KB(NPRE_SB, NFULL_SB)
        kb.setup()
        kb.pass1()
        kb.pass2()
        kb.pass3()
        kb.pass4()
        kb.pass5()
        _NC_CACHE["nc"] = kb.finish()
    return _NC_CACHE["nc"]


def kernel(x, meta_tokens, hg_lb_logits, norm_mix_g, w_in, gd_conv_w, gd_A_log, gd_dt_bias, hg_norm_g, gd_norm_g,
           hg_up, gd_up, w_out, norm_ffn_g, router_group_w, router_group_b, router_expert_w, router_expert_b,
           w_gate, w_up, w_down, final_norm_g):
    f32 = np.float32
    c = lambda a: np.ascontiguousarray(np.asarray(a, dtype=f32))
    x = c(x)
    meta = c(meta_tokens)
    B, S, _ = x.shape
    half = S // 2
    npre_tok = NPRE_SB * SBT
    ntok = (NPRE_SB + NFULL_SB) * SBT
    nmeta = meta.shape[0]
    shared = {
        "w_in": c(w_in[0]),
        "norm_mix_g": c(norm_mix_g[0]),
        "hg_lb": c(np.asarray(hg_lb_logits, f32).reshape(2, 4, 128).transpose(2, 0, 1)),
        "hg_norm_g": c(hg_norm_g[0]),
        "conv_wT": c(np.asarray(gd_conv_w[0], f32).reshape(4, 12, 128).transpose(2, 0, 1)),
        "gd_A_log": c(gd_A_log[0]),
        "gd_dt_bias": c(gd_dt_bias[0]),
        "gd_norm_g": c(gd_norm_g[0]),
        "hg_up": c(hg_up[0]),
        "gd_up": c(gd_up[0]),
        "w_out": c(w_out[0]),
        "norm_ffn_g": c(norm_ffn_g[0]),
        "router_w": c(np.concatenate([np.asarray(router_group_w[0], f32), np.asarray(router_expert_w[0], f32)], axis=1)),
        "router_b": c(np.concatenate([np.asarray(router_group_b[0], f32), np.asarray(router_expert_b[0], f32)])),
        "w_gate": c(w_gate[0]),
        "w_up": c(w_up[0]),
        "w_down": c(w_down[0]),
        "final_norm_g": c(final_norm_g),
    }
    in_maps = []
    for core in range(2 * B):
        b, hf = core // 2, core % 2
        xs = np.zeros((ntok, D), f32)
        if hf == 0:
            xs[npre_tok - nmeta:npre_tok] = meta
            xs[npre_tok:] = x[b, 0:half]
        else:
            xs[npre_tok - half - nmeta:npre_tok - half] = meta
            xs[npre_tok - half:] = x[b]
        m = dict(shared)
        m["xs"] = xs
        in_maps.append(m)
    nc = _build_full()
    res = run_bass_kernel_spmd(nc, in_maps, core_ids=list(range(2 * B)))
    out = np.empty((B, S, D), f32)
    for core in range(2 * B):
        b, hf = core // 2, core % 2
        out[b, hf * half:(hf + 1) * half] = np.asarray(res.results[core]["out"], dtype=f32)
    return out
```

```python
import numpy as np
import concourse.bass as bass
import concourse.mybir as mybir
from concourse.bass_utils import run_bass_kernel_spmd

F32 = mybir.dt.float32
BF16 = mybir.dt.bfloat16
I32 = mybir.dt.int32
U32 = mybir.dt.uint32
AF = mybir.ActivationFunctionType
ALU = mybir.AluOpType
AX = mybir.AxisListType


class Buf:
    __slots__ = ("name", "lw", "rd", "psum")

    def __init__(self, name="", psum=False):
        self.name = name
        self.lw = None
        self.rd = {}
        self.psum = psum


class Prog:
    ENGS = ("pe", "act", "dve", "pool", "sp")

    def __init__(self, kdma=6):
        self.ops = {e: [] for e in self.ENGS}
        self.waited = {e: {} for e in self.ENGS}
        self.ndma = {e: 0 for e in self.ENGS}
        self.K = kdma
        self.out_toks = []
        self.pending = {e: [] for e in self.ENGS}

    def barrier(self):
        toks = []
        for e in self.ENGS:
            for i in range(len(self.ops[e]) - 1, -1, -1):
                op = self.ops[e][i]
                if (not op["dma"]) and op["fn"] is not None:
                    toks.append((e, i))
                    break
            n = self.ndma[e]
            for slot in range(min(self.K, n)):
                last = ((n - 1 - slot) // self.K) * self.K + slot
                toks.append((("dma", e, slot), 16 * (last // self.K + 1)))
        for e in self.ENGS:
            self.pending[e] = list(toks)

    def _emit(self, eng, fn, reads, writes, dma=False, extra=()):
        deps = {}

        def need(tok):
            if tok is None:
                return
            k, v = tok
            if eng == "pe" and k == "pe":
                return
            if deps.get(k, -1) < v:
                deps[k] = v
        def need_x(tok):
            if tok is not None and tok[0] != eng:
                need(tok)
        for b in reads:
            if b.psum:
                need_x(b.lw)
            else:
                need(b.lw)
        for b in writes:
            if b.psum:
                need_x(b.lw)
            else:
                need(b.lw)
                for k, v in b.rd.items():
                    need((k, v))
        for t in extra:
            need(t)
        if self.pending[eng]:
            for t in self.pending[eng]:
                need(t)
            self.pending[eng] = []
        if dma:
            i = self.ndma[eng]
            self.ndma[eng] += 1
            slot = i % self.K
            val = 16 * (i // self.K + 1)
            key = ("dma", eng, slot)
            if val > 16:
                need((key, val - 16))
            tok = (key, val)
        else:
            tok = (eng, len(self.ops[eng]))
        waits = []
        w = self.waited[eng]
        for k, v in deps.items():
            if w.get(k, -1) < v:
                w[k] = v
                waits.append((k, v))
        self.ops[eng].append(dict(waits=waits, fn=fn, tok=tok, dma=dma))
        for b in reads:
            if b.psum:
                b.lw = tok
                continue
            k, v = tok
            if b.rd.get(k, -1) < v:
                b.rd[k] = v
        for b in writes:
            b.lw = tok
            b.rd = {}
        return tok

    def pe(self, fn, reads=(), writes=()):
        return self._emit("pe", fn, reads, writes)

    def act(self, fn, reads=(), writes=()):
        return self._emit("act", fn, reads, writes)

    def dve(self, fn, reads=(), writes=()):
        return self._emit("dve", fn, reads, writes)

    def pool(self, fn, reads=(), writes=()):
        return self._emit("pool", fn, reads, writes)

    def dma(self, fn, reads=(), writes=(), q="sp", out=False):
        t = self._emit(q, fn, reads, writes, dma=True)
        if out:
            self.out_toks.append(t)
        return t

    def finalize(self, nc):
        self._emit("sp", None, (), (), extra=self.out_toks)
        targets = {e: set() for e in self.ENGS}
        for e in self.ENGS:
            for op in self.ops[e]:
                for k, v in op["waits"]:
                    if isinstance(k, str):
                        targets[k].add(v)
        rank = {e: {} for e in self.ENGS}
        for e in self.ENGS:
            r = 0
            for i, op in enumerate(self.ops[e]):
                if (not op["dma"]) and i in targets[e]:
                    assert op["fn"] is not None
                    r += 1
                    rank[e][i] = r
        import contextlib
        with contextlib.ExitStack() as st:
            csem = {e: st.enter_context(nc.semaphore("c_" + e)) for e in self.ENGS}
            dsem = {}
            for e in self.ENGS:
                if self.ndma[e] > 0:
                    for s in range(min(self.K, self.ndma[e])):
                        dsem[("dma", e, s)] = st.enter_context(nc.semaphore("d_%s_%d" % (e, s)))
            block = st.enter_context(nc.Block())

            def run(e):
                def body(engine):
                    for i, op in enumerate(self.ops[e]):
                        for k, v in op["waits"]:
                            if isinstance(k, str):
                                engine.wait_ge(csem[k], rank[k][v])
                            else:
                                engine.wait_ge(dsem[k], v)
                        if op["fn"] is None:
                            continue
                        ins = op["fn"](engine)
                        if op["dma"]:
                            ins.then_inc(dsem[op["tok"][0]], 16)
                        elif i in rank[e]:
                            ins.then_inc(csem[e], 1)
                return body
            block.tensor(run("pe"))
            block.scalar(run("act"))
            block.vector(run("dve"))
            block.gpsimd(run("pool"))
            block.sync(run("sp"))


D = 1024
KC = 8
SBT = 256
NT = SBT // 128
NCH = SBT // 64
EPS = 1e-6
C_HQ, C_HF, C_HI, C_HG = 0, 512, 1024, 1536
C_GQ, C_GK, C_GV, C_GZ = 2048, 2560, 3072, 3584
C_GB, C_GA, C_PA, C_PB = 4096, 4100, 4104, 5128
DPROJ = 6152


class Arena:
    def __init__(self, ap, words):
        self.ap = ap
        self.words = words
        self.off = 0
        self.peak = 0

    def mark(self):
        return self.off

    def reset(self, m):
        self.off = m

    def alloc(self, free_shape, dt):
        n = 1
        for s in free_shape:
            n *= s
        esz = 4 if dt in (F32, I32, U32) else 2
        words = (n * esz + 3) // 4
        words = (words + 7) // 8 * 8
        assert self.off + words <= self.words, ("arena overflow", self.off, words, self.words)
        v = self.ap[:, self.off:self.off + words]
        self.off += words
        self.peak = max(self.peak, self.off)
        if esz == 2:
            v = v.bitcast(dt)
        elif dt != F32:
            v = v.bitcast(dt)
        v = v[:, 0:n]
        if len(free_shape) > 1:
            names = ["a%d" % i for i in range(len(free_shape))]
            pat = "p (%s) -> p %s" % (" ".join(names), " ".join(names))
            v = v.rearrange(pat, **{nm: s for nm, s in zip(names, free_shape)})
        return v


class KB:
    def __init__(self, npre, nfull, debug=()):
        import contextlib
        self.npre, self.nfull = npre, nfull
        self.nsb = npre + nfull
        self.ntok = self.nsb * SBT
        self.nfull_tok = nfull * SBT
        self.debug = set(debug)
        self.nc = bass.Bass("TRN2", target_bir_lowering=False)
        self.P = Prog()
        self.d = {}
        self.st = contextlib.ExitStack()

    def din(self, name, shape, dt=F32):
        self.d[name] = self.nc.dram_tensor(name, list(shape), dt, kind="ExternalInput").ap()
        return self.d[name]

    def dout(self, name, shape, dt=F32):
        self.d[name] = self.nc.dram_tensor(name, list(shape), dt, kind="ExternalOutput").ap()
        return self.d[name]

    def setup(self):
        nc, P, st = self.nc, self.P, self.st
        self.din("xs", [self.ntok, D])
        self.din("w_in", [D, DPROJ])
        self.din("norm_mix_g", [D])
        self.din("hg_lb", [128, 2, 4])
        self.din("hg_norm_g", [128])
        AW = 50000
        arena_t = st.enter_context(nc.sbuf_tensor("arena", [128, AW], F32))
        self.A = Arena(arena_t[:], AW)
        self.banks = [st.enter_context(nc.psum_tensor("pb%d" % i, [128, 512], F32)) for i in range(8)]
        self.bbank = [Buf("pb%d" % i, psum=True) for i in range(8)]
        A = self.A
        self.identf = A.alloc((128,), F32)
        self.ident = A.alloc((128,), BF16)
        self.ones_bf = A.alloc((128,), BF16)
        self.ones_f = A.alloc((512,), F32)
        self.mask2 = A.alloc((128,), F32)
        self.bconst = Buf("const")
        bc = self.bconst
        P.pool(lambda e: e.memset(self.identf, 0.0), writes=[bc])
        P.pool(lambda e: e.affine_select(out=self.identf, in_=self.identf, pattern=[[-1, 128]],
                                         compare_op=ALU.not_equal, fill=1.0, base=0, channel_multiplier=1),
               reads=[bc], writes=[bc])
        P.pool(lambda e: e.tensor_copy(out=self.ident, in_=self.identf), reads=[bc], writes=[bc])
        P.pool(lambda e: e.memset(self.ones_bf, 1.0), writes=[bc])
        P.pool(lambda e: e.memset(self.ones_f, 1.0), writes=[bc])
        P.pool(lambda e: e.memset(self.mask2, 1.0), writes=[bc])
        P.pool(lambda e: e.affine_select(out=self.mask2, in_=self.mask2, pattern=[[1, 128]],
                                         compare_op=ALU.is_ge, fill=0.0, base=0, channel_multiplier=-1),
               reads=[bc], writes=[bc])
        P.pool(lambda e: e.memset(self.mask2[0:64, 64:128], 0.0), reads=[bc], writes=[bc])
        self.xt = [A.alloc((D,), F32) for _ in range(2)]
        self.bxt = [Buf("xt%d" % i) for i in range(2)]
        self.junk = A.alloc((D,), BF16)
        self.bjunk = Buf("junk")
        self.ss = A.alloc((NT,), F32)
        self.rstd = A.alloc((NT,), F32)
        self.bss = Buf("ss")
        self.brstd = Buf("rstd")
        self.xsb = [A.alloc((D,), BF16) for _ in range(2)]
        self.bxsb = [Buf("xsb%d" % i) for i in range(2)]
        self.gbc = A.alloc((D,), F32)
        self.bgbc = Buf("gbc")
        self.nxt = 0
        self.d["x_buf"] = nc.dram_tensor("x_buf", [NSLOT, D], BF16, kind="Internal").ap()
        zt = A.alloc((D,), BF16)
        self.bxzero = Buf("xzero")
        P.pool(lambda e: e.memset(zt, 0.0), writes=[bc])
        xbv = self.d["x_buf"].rearrange("(b p) n -> p b n", p=128)
        nblk = NSLOT // 128
        step = 12
        self.bxz = []
        for b0 in range(0, nblk, step):
            bz = Buf("xz")
            self.bxz.append(bz)
            P.dma(lambda e, b0=b0: e.dma_start(out=xbv[:, b0:b0 + step, :],
                                               in_=zt.unsqueeze(1).to_broadcast([128, step, D])),
                  reads=[bc], writes=[bz])

    def stage_a(self, *a, **k):
        for _ in self.stage_a_gen(*a, **k):
            pass

    def stage_a_gen(self, sb, xnT, bxnT, ptr, bptr, gname="norm_mix_g", src="xs", tok_base=0, keep=None):
        nc, P = self.nc, self.P
        xs_d = self.d[src]
        tiles = []
        for t in range(NT):
            i = self.nxt % 2
            self.nxt += 1
            tok0 = tok_base + sb * SBT + t * 128
            xt, bxt = self.xt[i], self.bxt[i]
            P.dma(lambda e, xt=xt, tok0=tok0: e.dma_start(out=xt, in_=xs_d[tok0:tok0 + 128, :]), writes=[bxt])
            P.act(lambda e, xt=xt, t=t: e.activation(out=self.junk, in_=xt, func=AF.Square,
                                                     accum_out=self.ss[:, t:t + 1]),
                  reads=[bxt], writes=[self.bss])
            tiles.append((xt, bxt, i))
            if t % 2 == 1:
                t0 = t - 1
                P.act(lambda e, t0=t0: e.activation(out=self.rstd[:, t0:t0 + 2], in_=self.ss[:, t0:t0 + 2],
                                                    func=AF.Ln, scale=1.0 / D, bias=EPS),
                      reads=[self.bss], writes=[self.brstd])
                P.act(lambda e, t0=t0: e.activation(out=self.rstd[:, t0:t0 + 2], in_=self.rstd[:, t0:t0 + 2],
                                                    func=AF.Exp, scale=-0.5),
                      reads=[self.brstd], writes=[self.brstd])
                for tt in (t0, t):
                    xt2, bxt2, i2 = tiles[tt]
                    xsb, bxsb = self.xsb[i2], self.bxsb[i2]
                    P.dve(lambda e, xt2=xt2, xsb=xsb, tt=tt: e.scalar_tensor_tensor(
                        out=xsb, in0=xt2, scalar=self.rstd[:, tt:tt + 1], in1=self.gbc,
                        op0=ALU.mult, op1=ALU.mult),
                        reads=[bxt2, self.brstd, self.bgbc], writes=[bxsb])
                    for k in range(KC):
                        P.pe(lambda e, k=k, xsb=xsb: e.transpose(out=ptr[:, k, :], in_=xsb[:, k * 128:(k + 1) * 128],
                                                                 identity=self.ident),
                             reads=[bxsb, self.bconst], writes=[bptr])
                    P.dve(lambda e, tt=tt: e.tensor_copy(out=xnT[:, :, tt * 128:(tt + 1) * 128], in_=ptr),
                          reads=[bptr], writes=[bxnT])
                    yield


    def xnt_fetch_gen(self, sb, last_sb, xnTs, bxnTs):
        P = self.P
        src = self.d["xnT_s"]
        if sb not in self._xfetched:
            self._xfetched.add(sb)
            P.dma(lambda e, sb=sb: e.dma_start(out=xnTs[sb % 2], in_=src[:, :, sb * SBT:(sb + 1) * SBT]),
                  reads=[self.bxns[sb]], writes=[bxnTs[sb % 2]])
        nx = sb + 1
        if nx <= last_sb and nx not in self._xfetched:
            self._xfetched.add(nx)
            P.dma(lambda e, nx=nx: e.dma_start(out=xnTs[nx % 2], in_=src[:, :, nx * SBT:(nx + 1) * SBT]),
                  reads=[self.bxns[nx]], writes=[bxnTs[nx % 2]])
        yield

    def load_gain(self, gname):
        P = self.P
        g = self.d[gname]
        P.dma(lambda e: e.dma_start(out=self.gbc, in_=g.partition_broadcast(128)), writes=[self.bgbc])

    def pass1(self):
        nc, P, A = self.nc, self.P, self.A
        d = self.d
        H = 4
        self.W2 = A.alloc((KC, 2056), BF16)
        self.bW2 = [Buf("W2_%d" % k) for k in range(KC)]
        m0 = A.mark()
        self.OA = A.alloc((H, SBT), BF16)
        self.bOA = [Buf("OA%d" % h) for h in range(H)]
        W1 = A.alloc((KC, 2048), BF16)
        bW1 = [Buf("W1_%d" % k) for k in range(KC)]
        wv = d["w_in"].rearrange("(k p) n -> p k n", p=128)
        for k in range(KC):
            P.dma(lambda e, k=k: e.dma_start(out=W1[:, k, :], in_=wv[:, k, 0:2048]), writes=[bW1[k]], q="pool")
        for k in range(KC):
            P.dma(lambda e, k=k: e.dma_start(out=self.W2[:, k, :], in_=wv[:, k, 2048:2048 + 2056]), writes=[self.bW2[k]], q="pool")
        self.load_gain("norm_mix_g")
        lraw = A.alloc((2, H), F32)
        lb = A.alloc((H,), F32)
        oml = A.alloc((H,), F32)
        hgn = A.alloc((1,), F32)
        blb = Buf("lb")
        P.dma(lambda e: e.dma_start(out=lraw, in_=d["hg_lb"]), writes=[blb])
        P.dma(lambda e: e.dma_start(out=hgn, in_=d["hg_norm_g"].rearrange("(p o) -> p o", o=1)), writes=[blb])
        P.dve(lambda e: e.tensor_tensor(out=lb, in0=lraw[:, 0, :], in1=lraw[:, 1, :], op=ALU.subtract),
              reads=[blb], writes=[blb])
        P.act(lambda e: e.activation(out=oml, in_=lb, func=AF.Sigmoid, scale=-1.0), reads=[blb], writes=[blb])
        P.act(lambda e: e.activation(out=lb, in_=lb, func=AF.Sigmoid), reads=[blb], writes=[blb])

        xnT = A.alloc((KC, SBT), BF16)
        bxnT = Buf("xnT")
        Fb = A.alloc((H, SBT), F32)
        CS = A.alloc((H, SBT), F32)
        Kb = A.alloc((H, SBT), BF16)
        EB = A.alloc((H, SBT), BF16)
        ENB = A.alloc((H, SBT), BF16)
        EBEs = [A.alloc((H, NCH), F32) for _ in range(2)]
        QTs = [A.alloc((H, SBT), BF16) for _ in range(2)]
        KTs = [A.alloc((H, SBT), BF16) for _ in range(2)]
        KH = A.alloc((H, SBT), BF16)
        KHTs = [A.alloc((NT, 512), BF16) for _ in range(2)]
        Vs = [A.alloc((NT, 512), BF16) for _ in range(2)]
        Gs = [A.alloc((H, SBT), BF16) for _ in range(2)]
        O32 = A.alloc((H, SBT), F32)
        OSQ = A.alloc((H, SBT), BF16)
        LNV = A.alloc((SBT,), F32)
        ATS = A.alloc((H, 128), BF16)
        S32 = A.alloc((H, 128), F32)
        SBF = [A.alloc((H, 128), BF16) for _ in range(2)]
        bF = [Buf() for _ in range(H)]
        bCS = [Buf() for _ in range(H)]
        bK = Buf()
        bEB = [Buf() for _ in range(H)]
        bENB = [Buf() for _ in range(H)]
        bEBEs = [Buf(), Buf()]
        bQTs = [[Buf() for _ in range(H)] for _ in range(2)]
        bKTs = [Buf(), Buf()]
        bKH = Buf()
        bKHTs = [[Buf() for _ in range(NT)] for _ in range(2)]
        bVs = [[Buf() for _ in range(NT)] for _ in range(2)]
        bGs = [[Buf() for _ in range(H)] for _ in range(2)]
        bO32 = Buf()
        bOSQ = [Buf() for _ in range(H)]
        bLNV = Buf()
        bATS = Buf()
        bS32 = [Buf() for _ in range(H)]
        bSBF = [[Buf() for _ in range(H)] for _ in range(2)]
        sbf_i = [0] * H

        bk = self.banks
        bb = self.bbank
        ptr = bk[0][:].bitcast(BF16).rearrange("p (k t) -> p k t", k=KC)
        pkt = bk[1][:].bitcast(BF16)[:, 0:512]
        pj = [bk[2][:], bk[3][:]]
        bpj = [bb[2], bb[3]]
        pat = bk[4][:].rearrange("p (h t) -> p h t", h=H)
        po = bk[5][:].rearrange("p (h t) -> p h t", h=H)
        pS = [bk[6 + (h % 2)][:, 0:128] for h in range(H)]
        bpS = [bb[6 + (h % 2)] for h in range(H)]
        pji = [0]

        def nextpj():
            i = pji[0] % 2
            pji[0] += 1
            return pj[i], bpj[i]

        for h in range(H):
            P.pool(lambda e, h=h: e.memset(S32[:, h, :], 0.0), writes=[bS32[h]])
            P.pool(lambda e, h=h: e.memset(SBF[0][:, h, :], 0.0), writes=[bSBF[0][h]])

        def proj_fm(col0, h):
            p, bp = nextpj()
            for k in range(KC):
                P.pe(lambda e, k=k, p=p: e.matmul(p[:, 0:SBT], lhsT=W1[:, k, col0 + h * 128:col0 + (h + 1) * 128],
                                                  rhs=xnT[:, k, :], start=(k == 0), stop=(k == KC - 1)),
                     reads=[bW1[k], bxnT], writes=[bp])
            return p, bp

        def X1(sb):
            sl = sb % 2
            QT, KT, KHT, V, G, EBE = QTs[sl], KTs[sl], KHTs[sl], Vs[sl], Gs[sl], EBEs[sl]
            bQT, bKT, bKHT, bV, bG, bEBE = bQTs[sl], bKTs[sl], bKHTs[sl], bVs[sl], bGs[sl], bEBEs[sl]
            full = sb >= self.npre
            yield from self.stage_a_gen(sb, xnT, bxnT, ptr, bb[0])
            if "xnT_s" not in self.d:
                self.d["xnT_s"] = self.nc.dram_tensor("xnT_s", [128, KC, self.ntok], BF16, kind="Internal").ap()
                self.bxns = {}
            bx_ = Buf("xns")
            self.bxns[sb] = bx_
            P.dma(lambda e, sb=sb: e.dma_start(out=self.d["xnT_s"][:, :, sb * SBT:(sb + 1) * SBT], in_=xnT),
                  reads=[bxnT], writes=[bx_])
            for h in range(H):
                p, bp = proj_fm(C_HF, h)
                P.act(lambda e, h=h, p=p: e.activation(out=Fb[:, h, :], in_=p[:, 0:SBT], func=AF.Sigmoid),
                      reads=[bp], writes=[bF[h]])
                yield
            if full:
                for h in range(H):
                    p, bp = proj_fm(C_HQ, h)
                    P.act(lambda e, h=h, p=p: e.activation(out=QT[:, h, :], in_=p[:, 0:SBT], func=AF.Silu),
                          reads=[bp], writes=[bQT[h]])
                    yield
                for h in range(H):
                    p, bp = proj_fm(C_HG, h)
                    P.act(lambda e, h=h, p=p: e.activation(out=G[:, h, :], in_=p[:, 0:SBT], func=AF.Silu),
                          reads=[bp], writes=[bG[h]])
                    yield
            for h in range(H):
                P.dve(lambda e, h=h: e.tensor_scalar(out=Fb[:, h, :], in0=Fb[:, h, :], scalar1=oml[:, h:h + 1],
                                                     scalar2=lb[:, h:h + 1], op0=ALU.mult, op1=ALU.add),
                      reads=[bF[h], blb], writes=[bF[h]])
            P.dve(lambda e: e.tensor_scalar(out=Kb, in0=Fb, scalar1=-1.0, scalar2=1.0, op0=ALU.mult, op1=ALU.add),
                  reads=bF, writes=[bK])
            for h in range(H):
                P.act(lambda e, h=h: e.activation(out=Fb[:, h, :], in_=Fb[:, h, :], func=AF.Ln),
                      reads=[bF[h], bK], writes=[bF[h]])
            for h in range(H):
                P.dve(lambda e, h=h: e.tensor_tensor_scan(out=CS[:, h, :], data0=self.ones_f[:, 0:SBT],
                                                          data1=Fb[:, h, :], initial=0.0,
                                                          op0=ALU.mult, op1=ALU.add),
                      reads=[bF[h], self.bconst], writes=[bCS[h]])
                yield
            Fb4 = Fb.rearrange("p h (c t) -> p h c t", c=NCH)
            CS4 = CS.rearrange("p h (c t) -> p h c t", c=NCH)
            P.dve(lambda e: e.tensor_tensor(out=Fb4[:, :, 1:NCH, :], in0=CS4[:, :, 1:NCH, :],
                                            in1=CS4[:, :, 0:NCH - 1, 63:64].to_broadcast([128, H, NCH - 1, 64]),
                                            op=ALU.subtract),
                  reads=bCS + bF, writes=bF)
            P.dve(lambda e: e.tensor_copy(out=Fb4[:, :, 0, :], in_=CS4[:, :, 0, :]), reads=bCS + bF, writes=bF)
            for h in range(H):
                P.act(lambda e, h=h: e.activation(out=ENB[:, h, :], in_=Fb[:, h, :], func=AF.Exp, scale=-1.0),
                      reads=[bF[h]], writes=[bENB[h]])
                yield
            P.act(lambda e: e.activation(out=EBE, in_=Fb4[:, :, :, 63], func=AF.Exp), reads=bF, writes=[bEBE])
            if full:
                for h in range(H):
                    P.act(lambda e, h=h: e.activation(out=EB[:, h, :], in_=Fb[:, h, :], func=AF.Exp),
                          reads=[bF[h]], writes=[bEB[h]])
            P.dve(lambda e: e.tensor_tensor(out=KT, in0=Kb, in1=ENB, op=ALU.mult), reads=[bK] + bENB, writes=[bKT])
            KT4 = KT.rearrange("p h (c t) -> p h c t", c=NCH)
            KH4 = KH.rearrange("p h (c t) -> p h c t", c=NCH)
            P.dve(lambda e: e.tensor_tensor(out=KH4, in0=KT4,
                                            in1=EBE.unsqueeze(3).to_broadcast([128, H, NCH, 64]), op=ALU.mult),
                  reads=[bKT, bEBE], writes=[bKH])
            if full:
                P.dve(lambda e: e.tensor_tensor(out=QT, in0=QT, in1=EB, op=ALU.mult), reads=bQT + bEB, writes=bQT)
            for t in range(NT):
                p, bp = nextpj()
                for k in range(KC):
                    P.pe(lambda e, k=k, p=p, t=t: e.matmul(p, lhsT=xnT[:, k, t * 128:(t + 1) * 128],
                                                           rhs=W1[:, k, C_HI:C_HI + 512],
                                                           start=(k == 0), stop=(k == KC - 1)),
                         reads=[bW1[k], bxnT], writes=[bp])
                P.act(lambda e, p=p, t=t: e.activation(out=V[:, t, :], in_=p, func=AF.Copy), reads=[bp], writes=[bV[t]])
                for h in range(H):
                    P.pe(lambda e, h=h, t=t: e.transpose(out=pkt[:, h * 128:(h + 1) * 128],
                                                         in_=KH[:, h, t * 128:(t + 1) * 128], identity=self.ident),
                         reads=[bKH, self.bconst], writes=[bb[1]])
                P.dve(lambda e, t=t: e.tensor_copy(out=KHT[:, t, :], in_=pkt), reads=[bb[1]], writes=[bKHT[t]])
                yield
        def Y1(sb):
            full = sb >= self.npre
            sl = sb % 2
            QT, KT, KHT, V, G, EBE = QTs[sl], KTs[sl], KHTs[sl], Vs[sl], Gs[sl], EBEs[sl]
            bQT, bKT, bKHT, bV, bG, bEBE = bQTs[sl], bKTs[sl], bKHTs[sl], bVs[sl], bGs[sl], bEBEs[sl]
            for j in range(NT):
                c0 = j * 128
                if full:
                    for h in range(H):
                        P.pe(lambda e, h=h, c0=c0: e.matmul(pat[:, h, :], lhsT=KT[:, h, c0:c0 + 128],
                                                            rhs=QT[:, h, c0:c0 + 128], start=True, stop=True),
                             reads=[bKT, bQT[h]], writes=[bb[4]])
                    P.dve(lambda e: e.tensor_tensor(out=ATS, in0=pat,
                                                    in1=self.mask2.unsqueeze(1).to_broadcast([128, H, 128]),
                                                    op=ALU.mult),
                          reads=[bb[4], self.bconst], writes=[bATS])
                    yield
                for half in range(2):
                    ch = 2 * j + half
                    r0 = half * 64
                    for h in range(H):
                        cur = sbf_i[h]
                        if full:
                            P.pe(lambda e, h=h, cur=cur, c0=c0, r0=r0: e.matmul(
                                po[:, h, r0:r0 + 64], lhsT=SBF[cur][:, h, :], rhs=QT[:, h, c0 + r0:c0 + r0 + 64],
                                start=(h == 0 and r0 == 0), stop=False, skip_group_check=True),
                                reads=[bSBF[cur][h], bQT[h]], writes=[bb[5]])
                        P.pe(lambda e, h=h, j=j, r0=r0: e.matmul(
                            pS[h], lhsT=KHT[r0:r0 + 64, j, h * 128:(h + 1) * 128],
                            rhs=V[r0:r0 + 64, j, h * 128:(h + 1) * 128], start=True, stop=True),
                            reads=[bKHT[j], bV[j]], writes=[bpS[h]])
                        P.dve(lambda e, h=h, ch=ch: e.scalar_tensor_tensor(
                            out=S32[:, h, :], in0=S32[:, h, :], scalar=EBE[:, h, ch:ch + 1], in1=pS[h],
                            op0=ALU.mult, op1=ALU.add),
                            reads=[bS32[h], bEBE, bpS[h]], writes=[bS32[h]])
                        nxt = 1 - cur
                        P.act(lambda e, h=h, nxt=nxt: e.activation(out=SBF[nxt][:, h, :], in_=S32[:, h, :], func=AF.Copy),
                              reads=[bS32[h]], writes=[bSBF[nxt][h]])
                        sbf_i[h] = nxt
                        yield
                if full:
                    for h in range(H):
                        P.pe(lambda e, h=h, j=j: e.matmul(po[:, h, :], lhsT=V[:, j, h * 128:(h + 1) * 128],
                                                          rhs=ATS[:, h, :], start=False, stop=True,
                                                          skip_group_check=True),
                             reads=[bV[j], bATS], writes=[bb[5]])
                    P.act(lambda e, c0=c0: e.activation(out=O32[:, :, c0:c0 + 128], in_=po, func=AF.Copy),
                          reads=[bb[5]], writes=[bO32])
                    yield
            if full:
                tok0 = (sb - self.npre) * SBT
                for h in range(H):
                    P.act(lambda e, h=h: e.activation(out=OSQ[:, h, :], in_=O32[:, h, :], func=AF.Square),
                          reads=[bO32], writes=[bOSQ[h]])
                for h in range(H):
                    pss, bpss = bk[4][:], bb[4]
                    P.pe(lambda e, h=h, pss=pss: e.matmul(pss[:, 0:SBT], lhsT=self.ones_bf, rhs=OSQ[:, h, :], start=True, stop=True),
                         reads=[bOSQ[h], self.bconst], writes=[bpss])
                    P.act(lambda e, pss=pss: e.activation(out=LNV, in_=pss[:, 0:SBT], func=AF.Ln, scale=1.0 / 128, bias=EPS),
                          reads=[bpss], writes=[bLNV])
                    P.act(lambda e: e.activation(out=LNV, in_=LNV, func=AF.Exp, scale=-0.5),
                          reads=[bLNV], writes=[bLNV])
                    P.dve(lambda e, h=h: e.tensor_tensor(out=O32[:, h, :], in0=O32[:, h, :], in1=LNV, op=ALU.mult),
                          reads=[bO32, bLNV], writes=[bO32])
                    P.dve(lambda e, h=h: e.scalar_tensor_tensor(
                        out=self.OA[:, h, :], in0=O32[:, h, :], scalar=hgn[:, 0:1], in1=G[:, h, :],
                        op0=ALU.mult, op1=ALU.mult),
                        reads=[bO32, blb, bG[h]], writes=[self.bOA[h]])
                    yield
                self.spill("oa_s", self.OA, self.bOA, sb)
                if "oa" in self.debug:
                    if "dbg_oa" not in self.d:
                        self.dout("dbg_oa", [128, H, self.nfull_tok], BF16)
                    P.dma(lambda e, tok0=tok0: e.dma_start(out=self.d["dbg_oa"][:, :, tok0:tok0 + SBT], in_=self.OA),
                          reads=self.bOA, out=True)
        import os
        WTS1 = [int(v) for v in os.environ.get("IL_W1", "1,1").split(",")]

        def run_il(gens):
            gens = list(gens)
            while gens:
                for g_, w_ in list(gens):
                    for _ in range(w_):
                        try:
                            next(g_)
                        except StopIteration:
                            gens.remove((g_, w_))
                            break

        n = self.nsb
        for r in range(-1, n):
            gs = []
            if 0 <= r < n:
                gs.append((Y1(r), WTS1[0]))
            if 0 <= r + 1 < n:
                gs.append((X1(r + 1), WTS1[1]))
            run_il(gs)
        A.reset(m0)
        P.barrier()

    def finish(self):
        self.P.finalize(self.nc)
        self.st.close()
        return self.nc


def _pass2(self):
    nc, P, A = self.nc, self.P, self.A
    d = self.d
    H = 4
    self.din("conv_wT", [128, 4, 12])
    self.din("gd_A_log", [4])
    self.din("gd_dt_bias", [4])
    self.din("gd_norm_g", [128])
    m0 = A.mark()
    self.OB = A.alloc((H, SBT), BF16)
    self.bOB = [Buf("OB%d" % h) for h in range(H)]
    NW = 2056
    if hasattr(self, "W2"):
        W2, bW2 = self.W2, self.bW2
    else:
        W2 = A.alloc((KC, NW), BF16)
        bW2 = [Buf("W2_%d" % k) for k in range(KC)]
        wv = d["w_in"].rearrange("(k p) n -> p k n", p=128)
        for k in range(KC):
            P.dma(lambda e, k=k: e.dma_start(out=W2[:, k, :], in_=wv[:, k, 2048:2048 + NW]), writes=[bW2[k]], q="pool")
    self.load_gain("norm_mix_g")
    cw = A.alloc((4, 12), F32)
    negA = A.alloc((H,), F32)
    dtb = A.alloc((H,), F32)
    gdn = A.alloc((1,), F32)
    bpar = Buf("par2")
    P.dma(lambda e: e.dma_start(out=cw, in_=d["conv_wT"]), writes=[bpar])
    P.dma(lambda e: e.dma_start(out=negA, in_=d["gd_A_log"].partition_broadcast(128)), writes=[bpar])
    P.dma(lambda e: e.dma_start(out=dtb, in_=d["gd_dt_bias"].partition_broadcast(128)), writes=[bpar])
    P.dma(lambda e: e.dma_start(out=gdn, in_=d["gd_norm_g"].rearrange("(p o) -> p o", o=1)), writes=[bpar])
    P.act(lambda e: e.activation(out=negA, in_=negA, func=AF.Exp), reads=[bpar], writes=[bpar])
    P.dve(lambda e: e.tensor_scalar(out=negA, in0=negA, scalar1=-1.0, scalar2=None, op0=ALU.mult),
          reads=[bpar], writes=[bpar])
    maskL = A.alloc((128,), F32)
    ch01 = A.alloc((2, 128), F32)
    bc = self.bconst
    P.pool(lambda e: e.memset(maskL, 1.0), writes=[bc])
    P.pool(lambda e: e.affine_select(out=maskL, in_=maskL, pattern=[[-1, 128]], compare_op=ALU.is_gt,
                                     fill=0.0, base=0, channel_multiplier=1), reads=[bc], writes=[bc])
    P.pool(lambda e: e.memset(maskL[64:128, 0:64], 0.0), reads=[bc], writes=[bc])
    bones = A.alloc((128,), F32)
    P.pool(lambda e: e.memset(bones, 0.0), writes=[bc])
    P.pool(lambda e: e.memset(bones[0:64, 0:64], 1.0), reads=[bc], writes=[bc])
    P.pool(lambda e: e.memset(bones[64:128, 64:128], 1.0), reads=[bc], writes=[bc])
    P.pool(lambda e: e.memset(ch01, 0.0), writes=[bc])
    P.pool(lambda e: e.memset(ch01[0:64, 0, :], 1.0), reads=[bc], writes=[bc])
    P.pool(lambda e: e.memset(ch01[64:128, 1, :], 1.0), reads=[bc], writes=[bc])

    xnTs = [A.alloc((KC, SBT), BF16) for _ in range(2)]
    bxnTs = [Buf("xnT0"), Buf("xnT1")]
    self._xfetched = set()
    use_fetch = hasattr(self, "bxns")
    XC = A.alloc((12, SBT + 3), BF16)
    DG = A.alloc((48, 128), BF16)
    bDG = Buf("DG")
    for _j in range(4):
        for _cb in range(12):
            P.dve(lambda e, _j=_j, _cb=_cb: e.tensor_scalar(out=DG[:, _j * 12 + _cb, :], in0=self.identf,
                                                           scalar1=cw[:, _j, _cb:_cb + 1], scalar2=None, op0=ALU.mult),
                  reads=[bpar, self.bconst], writes=[bDG])
    bXC = [Buf() for _ in range(12)]
    CV = [A.alloc((SBT,), F32) for _ in range(2)]
    bCV = [Buf(), Buf()]
    QK32 = A.alloc((8, SBT), F32)
    bQK32 = [Buf() for _ in range(8)]
    SQ = A.alloc((SBT,), BF16)
    bSQ = Buf()
    RS = A.alloc((SBT,), F32)
    bRS = Buf()
    NS = 3
    QTs = [A.alloc((H, SBT), BF16) for _ in range(NS)]
    KTs = [A.alloc((H, SBT), BF16) for _ in range(NS)]
    VTs = [A.alloc((H, SBT), BF16) for _ in range(NS)]
    GZs = [A.alloc((H, SBT), BF16) for _ in range(NS)]
    bQTs = [[Buf() for _ in range(H)] for _ in range(NS)]
    bKTs = [[Buf() for _ in range(H)] for _ in range(NS)]
    bVTs = [[Buf() for _ in range(H)] for _ in range(NS)]
    bGZs = [[Buf() for _ in range(H)] for _ in range(NS)]
    BGraws = [A.alloc((NT, 8), F32) for _ in range(NS)]
    LNBs = [A.alloc((NT, H), F32) for _ in range(NS)]
    BETAs = [A.alloc((NT, H), F32) for _ in range(NS)]
    GGs = [A.alloc((NT, H), F32) for _ in range(NS)]
    bBGs = [Buf() for _ in range(NS)]
    PBUF = []
    for _j in range(NT):
        pb = dict(
            GB=A.alloc((H, 128), F32), bGB=Buf(),
            E1=A.alloc((H, 128), F32), bE1=Buf(),
            E2=A.alloc((H, 128), F32), bE2=Buf(),
            EGR=A.alloc((H, 128), BF16), bEGR=Buf(),
            Lm=[A.alloc((H, 128), BF16) for _ in range(2)], bLm=[Buf(), Buf()],
            Um=[A.alloc((H, 128), BF16) for _ in range(2)], bUm=[Buf(), Buf()],
            Xm=[A.alloc((H, 128), BF16) for _ in range(2)], bXm=[Buf(), Buf()],
            VTK=A.alloc((H, 128), BF16), bVTK=Buf(),
        )
        PBUF.append(pb)
    PCAR = []
    for _s in range(2):
        row = []
        for _j in range(NT):
            row.append(dict(
                SC=A.alloc((8, H), F32), bSC=Buf(),
                QTG=A.alloc((H, 128), BF16), bQTG=Buf(),
                TT=A.alloc((H, 128), BF16), bTT=Buf(),
                ATT=A.alloc((H, 128), BF16), bATT=Buf(),
                KHT=A.alloc((H, 128), BF16), bKHT=Buf(),
                KTP=A.alloc((H, 128), BF16), bKTP=Buf(),
                BV=A.alloc((H, 128), F32), bBV=Buf(),
            ))
        PCAR.append(row)
    R = A.alloc((H, 128), BF16)
    USB = A.alloc((H, 128), BF16)
    bR = Buf()
    bUSB = Buf()
    S32 = A.alloc((H, 128), F32)
    SBF = [A.alloc((H, 128), BF16) for _ in range(2)]
    bS32 = [Buf() for _ in range(H)]
    bSBF = [[Buf() for _ in range(H)] for _ in range(2)]
    sbf_i = [0]
    O32 = A.alloc((H, SBT), F32)
    bO32 = Buf()
    OSQ = A.alloc((H, SBT), BF16)
    bOSQ = [Buf() for _ in range(H)]
    LNV = A.alloc((SBT,), F32)
    bLNV = Buf()
    identb4 = A.alloc((H, 128), BF16)
    P.pool(lambda e: e.tensor_copy(out=identb4, in_=self.ident.unsqueeze(1).to_broadcast([128, H, 128])),
           reads=[bc], writes=[bc])

    bk, bb = self.banks, self.bbank
    ptr = bk[0][:].bitcast(BF16).rearrange("p (k t) -> p k t", k=KC)
    ptr4 = bk[0][:].bitcast(BF16)[:, 0:512].rearrange("p (h t) -> p h t", h=H)
    pj = [bk[1][:], bk[2][:], bk[0][:]]
    bpj = [bb[1], bb[2], bb[0]]
    pA = bk[3][:].rearrange("p (h t) -> p h t", h=H)
    pB = bk[4][:].rearrange("p (h t) -> p h t", h=H)
    pBb = bk[4][:].bitcast(BF16)[:, 0:512].rearrange("p (h t) -> p h t", h=H)
    pC = bk[5][:].rearrange("p (h t) -> p h t", h=H)
    pR = bk[6][:].rearrange("p (h t) -> p h t", h=H)
    po = bk[7][:].rearrange("p (h t) -> p h t", h=H)
    pji = [0]

    def nextpj():
        i = pji[0] % 3
        pji[0] += 1
        return pj[i], bpj[i]

    for h in range(H):
        P.pool(lambda e, h=h: e.memset(S32[:, h, :], 0.0), writes=[bS32[h]])
        P.pool(lambda e, h=h: e.memset(SBF[0][:, h, :], 0.0), writes=[bSBF[0][h]])
    for cb in range(12):
        P.pool(lambda e, cb=cb: e.memset(XC[:, cb, :], 0.0), writes=[bXC[cb]])

    def proj_fm(col0, xnT, bxnT):
        p, bp = nextpj()
        for k in range(KC):
            P.pe(lambda e, k=k, p=p: e.matmul(p[:, 0:SBT], lhsT=W2[:, k, col0:col0 + 128],
                                              rhs=xnT[:, k, :], start=(k == 0), stop=(k == KC - 1)),
                 reads=[bW2[k], bxnT], writes=[bp])
        return p, bp

    evi = [0]

    def evac(out, in_, reads, writes):
        i = evi[0]
        evi[0] += 1
        if i % 2 == 0:
            P.act(lambda e: e.activation(out=out, in_=in_, func=AF.Copy), reads=reads, writes=writes)
        else:
            P.dve(lambda e: e.tensor_copy(out=out, in_=in_), reads=reads, writes=writes)

    def X(sb):
        sl = sb % 3
        QT, KT, VT, GZ = QTs[sl], KTs[sl], VTs[sl], GZs[sl]
        bQT, bKT, bVT, bGZ = bQTs[sl], bKTs[sl], bVTs[sl], bGZs[sl]
        BGraw, LNB, BETA, GG, bBG = BGraws[sl], LNBs[sl], BETAs[sl], GGs[sl], bBGs[sl]
        full = sb >= self.npre
        xnT, bxnT = xnTs[sb % 2], bxnTs[sb % 2]
        if use_fetch:
            yield from self.xnt_fetch_gen(sb, self.nsb - 1, xnTs, bxnTs)
        else:
            yield from self.stage_a_gen(sb, xnT, bxnT, ptr, bb[0])
        cbs = list(range(12)) if full else list(range(4, 12))
        cbs_proj = list(range(12)) if sb >= self.npre - 1 else list(range(4, 12))
        for cb in cbs_proj:
            if sb > 0:
                P.pool(lambda e, cb=cb: e.tensor_copy(out=XC[:, cb, 0:3], in_=XC[:, cb, SBT:SBT + 3]),
                       reads=[bXC[cb]], writes=[bXC[cb]])
            p, bp = proj_fm(cb * 128, xnT, bxnT)
            evac(XC[:, cb, 3:SBT + 3], p[:, 0:SBT], [bp], [bXC[cb]])
            yield
        pbg, bpbg = nextpj()
        for t in range(NT):
            for k in range(KC):
                P.pe(lambda e, k=k, t=t, pbg=pbg: e.matmul(pbg[:, t * 8:(t + 1) * 8], lhsT=xnT[:, k, t * 128:(t + 1) * 128],
                                                  rhs=W2[:, k, 2048:2056], start=(k == 0), stop=(k == KC - 1)),
                     reads=[bW2[k], bxnT], writes=[bpbg])
        P.act(lambda e, pbg=pbg: e.activation(out=BGraw, in_=pbg[:, 0:NT * 8].rearrange("p (t c) -> p t c", t=NT), func=AF.Copy),
              reads=[bpbg], writes=[bBG])
        P.act(lambda e: e.activation(out=LNB, in_=BGraw[:, :, 0:4], func=AF.Exp, scale=-1.0), reads=[bBG], writes=[bBG])
        P.act(lambda e: e.activation(out=LNB, in_=LNB, func=AF.Ln, bias=1.0), reads=[bBG], writes=[bBG])
        P.dve(lambda e: e.tensor_scalar(out=LNB, in0=LNB, scalar1=-1.0, scalar2=None, op0=ALU.mult),
              reads=[bBG], writes=[bBG])
        P.act(lambda e: e.activation(out=BETA, in_=LNB, func=AF.Exp), reads=[bBG], writes=[bBG])
        P.dve(lambda e: e.tensor_tensor(out=GG, in0=BGraw[:, :, 4:8], in1=dtb.unsqueeze(1).to_broadcast([128, NT, H]),
                                        op=ALU.add), reads=[bBG, bpar], writes=[bBG])
        P.act(lambda e: e.activation(out=GG, in_=GG, func=AF.Exp), reads=[bBG], writes=[bBG])
        P.act(lambda e: e.activation(out=GG, in_=GG, func=AF.Ln, bias=1.0), reads=[bBG], writes=[bBG])
        P.dve(lambda e: e.tensor_tensor(out=GG, in0=GG, in1=negA.unsqueeze(1).to_broadcast([128, NT, H]),
                                        op=ALU.mult), reads=[bBG, bpar], writes=[bBG])
        yield
        if full:
            for h in range(H):
                p, bp = proj_fm(1536 + h * 128, xnT, bxnT)
                P.act(lambda e, h=h, p=p: e.activation(out=GZ[:, h, :], in_=p[:, 0:SBT], func=AF.Silu),
                      reads=[bp], writes=[bGZ[h]])
                yield
        for n, cb in enumerate(cbs):
            cv, bcv = nextpj()
            for j in range(4):
                P.pe(lambda e, cb=cb, cv=cv, j=j: e.matmul(cv[:, 0:SBT], lhsT=DG[:, j * 12 + cb, :], rhs=XC[:, cb, j:SBT + j],
                                                           start=(j == 0), stop=(j == 3)),
                     reads=[bXC[cb], bDG], writes=[bcv])
            if cb < 8:
                P.act(lambda e, cb=cb, cv=cv: e.activation(out=QK32[:, cb, :], in_=cv[:, 0:SBT], func=AF.Silu),
                      reads=[bcv], writes=[bQK32[cb]])
            else:
                P.act(lambda e, cb=cb, cv=cv: e.activation(out=VT[:, cb - 8, :], in_=cv[:, 0:SBT], func=AF.Silu),
                      reads=[bcv], writes=[bVT[cb - 8]])
            yield
        for cb in cbs:
            if cb >= 8:
                continue
            P.pool(lambda e, cb=cb: e.tensor_tensor(out=SQ, in0=QK32[:, cb, :], in1=QK32[:, cb, :], op=ALU.mult),
                   reads=[bQK32[cb]], writes=[bSQ])
            p, bp = nextpj()
            P.pe(lambda e, p=p: e.matmul(p[:, 0:SBT], lhsT=self.ones_bf, rhs=SQ, start=True, stop=True),
                 reads=[bSQ, bc], writes=[bp])
            P.act(lambda e, p=p: e.activation(out=RS, in_=p[:, 0:SBT], func=AF.Ln, bias=EPS), reads=[bp], writes=[bRS])
            qbias = -0.5 * float(np.log(128.0)) if cb < 4 else 0.0
            P.act(lambda e, qbias=qbias: e.activation(out=RS, in_=RS, func=AF.Exp, scale=-0.5, bias=qbias),
                  reads=[bRS], writes=[bRS])
            dst, bdst = (QT[:, cb, :], bQT[cb]) if cb < 4 else (KT[:, cb - 4, :], bKT[cb - 4])
            P.pool(lambda e, cb=cb, dst=dst: e.tensor_tensor(out=dst, in0=QK32[:, cb, :], in1=RS, op=ALU.mult),
                   reads=[bQK32[cb], bRS], writes=[bdst])
            yield
    def Yp(sb):
        sl = sb % 3
        full = sb >= self.npre
        QT, KT, VT, GZ = QTs[sl], KTs[sl], VTs[sl], GZs[sl]
        bQT, bKT, bVT, bGZ = bQTs[sl], bKTs[sl], bVTs[sl], bGZs[sl]
        BGraw, LNB, BETA, GG, bBG = BGraws[sl], LNBs[sl], BETAs[sl], GGs[sl], bBGs[sl]
        LL = dict(L0)
        LL['PB'] = [dict(PBUF[j], **PCAR[sb % 2][j]) for j in range(NT)]
        LL.update(QT=QT, KT=KT, VT=VT, GZ=GZ, bQT=bQT, bKT=bKT, bVT=bVT, bGZ=bGZ, LNB=LNB, BETA=BETA, GG=GG, bBG=bBG)
        yield from self._gdn_prep(LL, full)
    def Yr(sb):
        sl = sb % 3
        full = sb >= self.npre
        QT, KT, VT, GZ = QTs[sl], KTs[sl], VTs[sl], GZs[sl]
        bQT, bKT, bVT, bGZ = bQTs[sl], bKTs[sl], bVTs[sl], bGZs[sl]
        BGraw, LNB, BETA, GG, bBG = BGraws[sl], LNBs[sl], BETAs[sl], GGs[sl], bBGs[sl]
        LL = dict(L0)
        LL['PB'] = [dict(PBUF[j], **PCAR[sb % 2][j]) for j in range(NT)]
        LL.update(QT=QT, KT=KT, VT=VT, GZ=GZ, bQT=bQT, bKT=bKT, bVT=bVT, bGZ=bGZ, LNB=LNB, BETA=BETA, GG=GG, bBG=bBG)
        yield from self._gdn_rec(LL, full)
        if full:
            tok0 = (sb - self.npre) * SBT
            for h in range(H):
                P.act(lambda e, h=h: e.activation(out=OSQ[:, h, :], in_=O32[:, h, :], func=AF.Square),
                      reads=[bO32], writes=[bOSQ[h]])
            for h in range(H):
                pss, bpss = bk[5][:], bb[5]
                P.pe(lambda e, h=h, pss=pss: e.matmul(pss[:, 0:SBT], lhsT=self.ones_bf, rhs=OSQ[:, h, :], start=True, stop=True),
                     reads=[bOSQ[h], bc], writes=[bpss])
                P.act(lambda e, pss=pss: e.activation(out=LNV, in_=pss[:, 0:SBT], func=AF.Ln, scale=1.0 / 128, bias=EPS),
                      reads=[bpss], writes=[bLNV])
                P.act(lambda e: e.activation(out=LNV, in_=LNV, func=AF.Exp, scale=-0.5), reads=[bLNV], writes=[bLNV])
                P.dve(lambda e, h=h: e.tensor_tensor(out=O32[:, h, :], in0=O32[:, h, :], in1=LNV, op=ALU.mult),
                      reads=[bO32, bLNV], writes=[bO32])
                P.dve(lambda e, h=h: e.scalar_tensor_tensor(
                    out=self.OB[:, h, :], in0=O32[:, h, :], scalar=gdn[:, 0:1], in1=GZ[:, h, :],
                    op0=ALU.mult, op1=ALU.mult), reads=[bO32, bpar, bGZ[h]], writes=[self.bOB[h]])
                yield
            self.spill("ob_s", self.OB, self.bOB, sb)
            if "ob" in self.debug:
                if "dbg_ob" not in self.d:
                    self.dout("dbg_ob", [128, H, self.nfull_tok], BF16)
                P.dma(lambda e, tok0=tok0: e.dma_start(out=self.d["dbg_ob"][:, :, tok0:tok0 + SBT], in_=self.OB),
                      reads=self.bOB, out=True)
    L0 = dict(locals())

    import os
    WTS = [int(v) for v in os.environ.get("IL_W", "1,2,2").split(",")]

    def run_il(gens):
        gens = list(gens)
        while gens:
            for g_, w_ in list(gens):
                for _ in range(w_):
                    try:
                        next(g_)
                    except StopIteration:
                        gens.remove((g_, w_))
                        break

    n = self.nsb
    for r in range(-2, n):
        gs = []
        if 0 <= r < n:
            gs.append((Yr(r), WTS[0]))
        if 0 <= r + 1 < n:
            gs.append((Yp(r + 1), WTS[1]))
        if 0 <= r + 2 < n:
            gs.append((X(r + 2), WTS[2]))
        run_il(gs)
    A.reset(m0)
    P.barrier()


KB.pass2 = _pass2


def _gdn_prep(self, L, full):
    P = self.P
    H = 4
    g = lambda n: L[n]
    bk, bb = self.banks, self.bbank
    bc = self.bconst
    mask2, ident = self.mask2, self.ident
    maskL, bones, ch01, identb4 = g("maskL"), g("bones"), g("ch01"), g("identb4")
    PBUF, GG, LNB, BETA, bBG = g("PB"), g("GG"), g("LNB"), g("BETA"), g("bBG")
    QT, KT, VT, bQT, bKT, bVT = g("QT"), g("KT"), g("VT"), g("bQT"), g("bKT"), g("bVT")
    R, USB, bR, bUSB = g("R"), g("USB"), g("bR"), g("bUSB")
    S32, SBF, bS32, bSBF, sbf_i = g("S32"), g("SBF"), g("bS32"), g("bSBF"), g("sbf_i")
    O32, bO32 = g("O32"), g("bO32")
    evac, nextpj = g("evac"), g("nextpj")
    NP = NT
    pP = [bk[3 + j][:].rearrange("p (h t) -> p h t", h=H) for j in range(NP)]
    pPb = [bk[3 + j][:].bitcast(BF16)[:, 0:512].rearrange("p (h t) -> p h t", h=H) for j in range(NP)]
    bpP = [bb[3 + j] for j in range(NP)]
    ptr4 = bk[5][:].bitcast(BF16)[:, 0:512].rearrange("p (h t) -> p h t", h=H)
    pKS = bk[5][:].rearrange("p (h t) -> p h t", h=H)
    pU = bk[6][:].rearrange("p (h t) -> p h t", h=H)
    po = bk[7][:].rearrange("p (h t) -> p h t", h=H)

    def bc4(ap):
        return ap.unsqueeze(2).to_broadcast([128, H, 128])

    for j in range(NP):
        pb = PBUF[j]
        SC, bSC = pb["SC"], pb["bSC"]
        ps = bk[3 + j][:, 0:16]
        gj = GG[:, j, :]
        for n, lhs in enumerate((mask2, bones, ch01[:, 0, :], ch01[:, 1, :])):
            P.pe(lambda e, n=n, lhs=lhs, ps=ps, gj=gj: e.matmul(ps[:, 4 * n:4 * n + 4], lhsT=lhs, rhs=gj,
                                                                 start=True, stop=True),
                 reads=[bBG, bc], writes=[bpP[j]])
        P.act(lambda e, SC=SC, ps=ps: e.activation(out=SC[:, 0, :], in_=ps[:, 0:4], func=AF.Copy),
              reads=[bpP[j]], writes=[bSC])
        P.dve(lambda e, SC=SC, ps=ps: e.tensor_tensor(out=SC[:, 5, :], in0=ps[:, 4:8], in1=SC[:, 0, :], op=ALU.subtract),
              reads=[bpP[j], bSC], writes=[bSC])
        P.act(lambda e, SC=SC, ps=ps: e.activation(out=SC[:, 6:8, :], in_=ps[:, 8:16].rearrange("p (a h) -> p a h", a=2),
                                                  func=AF.Exp), reads=[bpP[j]], writes=[bSC])
        P.dve(lambda e, SC=SC: e.tensor_scalar(out=SC[:, 1, :], in0=SC[:, 0, :], scalar1=-1.0, scalar2=None, op0=ALU.mult),
              reads=[bSC], writes=[bSC])
        P.dve(lambda e, SC=SC, j=j: e.tensor_tensor(out=SC[:, 2, :], in0=SC[:, 0, :], in1=LNB[:, j, :], op=ALU.add),
              reads=[bSC, bBG], writes=[bSC])
        P.act(lambda e, SC=SC: e.activation(out=SC[:, 3, :], in_=SC[:, 0, :], func=AF.Exp), reads=[bSC], writes=[bSC])
        P.act(lambda e, SC=SC: e.activation(out=SC[:, 5, :], in_=SC[:, 5, :], func=AF.Exp), reads=[bSC], writes=[bSC])
        P.dve(lambda e, SC=SC, j=j: e.scalar_tensor_tensor(out=SC[:, 4, :], in0=SC[:, 3, :], scalar=-1.0,
                                                          in1=BETA[:, j, :], op0=ALU.mult, op1=ALU.mult),
              reads=[bSC, bBG], writes=[bSC])
        yield
    for j in range(NP):
        pb = PBUF[j]
        P.pool(lambda e, pb=pb, j=j: e.tensor_copy(out=pb["GB"], in_=bc4(GG[:, j, :])), reads=[bBG], writes=[pb["bGB"]])
        yield
    for j in range(NP):
        pb = PBUF[j]
        for h in range(H):
            P.pe(lambda e, pb=pb, j=j, h=h: e.matmul(pP[j][:, h, :], lhsT=pb["GB"][:, h, :], rhs=mask2,
                                                     start=True, stop=True),
                 reads=[pb["bGB"], bc], writes=[bpP[j]])
        yield
    for j in range(NP):
        pb = PBUF[j]
        SC = pb["SC"]
        P.dve(lambda e, pb=pb, j=j, SC=SC: e.tensor_tensor(out=pb["E1"], in0=pP[j], in1=bc4(SC[:, 0, :]), op=ALU.max),
              reads=[bpP[j], pb["bSC"]], writes=[pb["bE1"]])
        if full:
            P.dve(lambda e, pb=pb, j=j, SC=SC: e.tensor_tensor(out=pb["E2"], in0=pP[j], in1=bc4(SC[:, 0, :]), op=ALU.min),
                  reads=[bpP[j], pb["bSC"]], writes=[pb["bE2"]])
            P.act(lambda e, pb=pb, j=j: e.activation(out=pb["EGR"], in_=pP[j], func=AF.Exp),
                  reads=[bpP[j]], writes=[pb["bEGR"]])
        yield
    for j in range(NP):
        pb = PBUF[j]
        SC = pb["SC"]
        for h in range(H):
            P.act(lambda e, pb=pb, h=h, SC=SC: e.activation(out=pb["E1"][:, h, :], in_=pb["E1"][:, h, :], func=AF.Exp,
                                                            scale=-1.0, bias=SC[:, 2, h:h + 1]),
                  reads=[pb["bE1"], pb["bSC"]], writes=[pb["bE1"]])
        if full:
            for h in range(H):
                P.act(lambda e, pb=pb, h=h, SC=SC: e.activation(out=pb["E2"][:, h, :], in_=pb["E2"][:, h, :], func=AF.Exp,
                                                                bias=SC[:, 1, h:h + 1]),
                      reads=[pb["bE2"], pb["bSC"]], writes=[pb["bE2"]])
        yield
    for j in range(NP):
        pb = PBUF[j]
        P.pool(lambda e, pb=pb: e.tensor_tensor(out=pb["E1"], in0=pb["E1"],
                                                in1=maskL.unsqueeze(1).to_broadcast([128, H, 128]), op=ALU.mult),
               reads=[pb["bE1"], bc], writes=[pb["bE1"]])
        if full:
            P.pool(lambda e, pb=pb: e.tensor_tensor(out=pb["E2"], in0=pb["E2"],
                                                    in1=mask2.unsqueeze(1).to_broadcast([128, H, 128]), op=ALU.mult),
                   reads=[pb["bE2"], bc], writes=[pb["bE2"]])
            c0 = j * 128
            P.dve(lambda e, pb=pb, c0=c0: e.tensor_tensor(out=pb["QTG"], in0=QT[:, :, c0:c0 + 128], in1=pb["EGR"], op=ALU.mult),
                  reads=bQT + [pb["bEGR"]], writes=[pb["bQTG"]])
        yield
    for j in range(NP):
        c0 = j * 128
        for h in range(H):
            P.pe(lambda e, j=j, h=h, c0=c0: e.matmul(pP[j][:, h, :], lhsT=KT[:, h, c0:c0 + 128], rhs=KT[:, h, c0:c0 + 128],
                                                     start=True, stop=True), reads=[bKT[h]], writes=[bpP[j]])
        yield
    for j in range(NP):
        pb = PBUF[j]
        P.dve(lambda e, pb=pb, j=j: e.tensor_tensor(out=pb["Lm"][0], in0=pP[j], in1=pb["E1"], op=ALU.mult),
              reads=[bpP[j], pb["bE1"]], writes=[pb["bLm"][0]])
        yield
    if full:
        for j in range(NP):
            c0 = j * 128
            for h in range(H):
                P.pe(lambda e, j=j, h=h, c0=c0: e.matmul(pP[j][:, h, :], lhsT=KT[:, h, c0:c0 + 128],
                                                         rhs=QT[:, h, c0:c0 + 128], start=True, stop=True),
                     reads=[bKT[h], bQT[h]], writes=[bpP[j]])
            yield
        for j in range(NP):
            pb = PBUF[j]
            P.dve(lambda e, pb=pb, j=j: e.tensor_tensor(out=pb["ATT"], in0=pP[j], in1=pb["E2"], op=ALU.mult),
                  reads=[bpP[j], pb["bE2"]], writes=[pb["bATT"]])
            yield
    for j in range(NP):
        pb = PBUF[j]
        for h in range(H):
            P.pe(lambda e, pb=pb, j=j, h=h: e.transpose(out=pPb[j][:, h, :], in_=pb["Lm"][0][:, h, :], identity=ident),
                 reads=[pb["bLm"][0], bc], writes=[bpP[j]])
        yield
    for j in range(NP):
        pb = PBUF[j]
        evac(pb["Um"][0], pPb[j], [bpP[j]], [pb["bUm"][0]])
        yield
    for j in range(NP):
        pb = PBUF[j]
        P.pool(lambda e, pb=pb: e.tensor_tensor(out=pb["Xm"][0], in0=identb4, in1=pb["Um"][0], op=ALU.subtract),
               reads=[pb["bUm"][0], bc], writes=[pb["bXm"][0]])
        yield
    cur, cx = 0, 0
    for lvl in range(5):
        for j in range(NP):
            pb = PBUF[j]
            for h in range(H):
                P.pe(lambda e, pb=pb, j=j, h=h, cur=cur: e.matmul(pP[j][:, h, :], lhsT=pb["Um"][cur][:, h, :],
                                                                  rhs=pb["Lm"][cur][:, h, :], start=True, stop=True),
                     reads=[pb["bUm"][cur], pb["bLm"][cur]], writes=[bpP[j]])
            yield
        for j in range(NP):
            pb = PBUF[j]
            evac(pb["Lm"][1 - cur], pP[j], [bpP[j]], [pb["bLm"][1 - cur]])
            yield
        if lvl < 4:
            for j in range(NP):
                pb = PBUF[j]
                for h in range(H):
                    P.pe(lambda e, pb=pb, j=j, h=h, cur=cur: e.matmul(pP[j][:, h, :], lhsT=pb["Lm"][cur][:, h, :],
                                                                      rhs=pb["Um"][cur][:, h, :], start=True, stop=True),
                         reads=[pb["bUm"][cur], pb["bLm"][cur]], writes=[bpP[j]])
                yield
            for j in range(NP):
                pb = PBUF[j]
                evac(pb["Um"][1 - cur], pP[j], [bpP[j]], [pb["bUm"][1 - cur]])
                yield
        for j in range(NP):
            pb = PBUF[j]
            for h in range(H):
                P.pe(lambda e, pb=pb, j=j, h=h, cur=cur, cx=cx: e.matmul(pP[j][:, h, :], lhsT=pb["Lm"][1 - cur][:, h, :],
                                                                         rhs=pb["Xm"][cx][:, h, :], start=True, stop=False),
                     reads=[pb["bLm"][1 - cur], pb["bXm"][cx]], writes=[bpP[j]])
                P.pe(lambda e, pb=pb, j=j, h=h, cx=cx: e.matmul(pP[j][:, h, :], lhsT=ident, rhs=pb["Xm"][cx][:, h, :],
                                                                start=False, stop=True),
                     reads=[pb["bXm"][cx], bc], writes=[bpP[j]])
            yield
        for j in range(NP):
            pb = PBUF[j]
            if lvl == 4:
                evac(pb["TT"], pP[j], [bpP[j]], [pb["bTT"]])
            else:
                evac(pb["Xm"][1 - cx], pP[j], [bpP[j]], [pb["bXm"][1 - cx]])
            yield
        cur, cx = 1 - cur, 1 - cx
    for j in range(NP):
        pb = PBUF[j]
        c0 = j * 128
        SC = pb["SC"]
        for h in range(H):
            P.pe(lambda e, h=h, c0=c0, j=j: e.transpose(out=pPb[j][:, h, :], in_=KT[:, h, c0:c0 + 128], identity=ident),
                 reads=[bKT[h], bc], writes=[bpP[j]])
        P.dve(lambda e, pb=pb, SC=SC, j=j: e.tensor_tensor(out=pb["KHT"], in0=pPb[j], in1=bc4(SC[:, 5, :]), op=ALU.mult),
              reads=[bpP[j], pb["bSC"]], writes=[pb["bKHT"]])
        for h in range(H):
            P.pe(lambda e, h=h, c0=c0, j=j: e.transpose(out=pPb[j][:, h, :], in_=VT[:, h, c0:c0 + 128], identity=ident),
                 reads=[bVT[h], bc], writes=[bpP[j]])
        P.act(lambda e, pb=pb, j=j: e.activation(out=pb["VTK"], in_=pPb[j], func=AF.Copy), reads=[bpP[j]], writes=[pb["bVTK"]])
        P.pool(lambda e, pb=pb, c0=c0: e.tensor_copy(out=pb["KTP"], in_=KT[:, :, c0:c0 + 128]), reads=bKT, writes=[pb["bKTP"]])
        P.pool(lambda e, pb=pb, j=j: e.tensor_tensor(out=pb["BV"], in0=pb["VTK"], in1=bc4(BETA[:, j, :]), op=ALU.mult),
               reads=[pb["bVTK"], bBG], writes=[pb["bBV"]])
        yield


def _gdn_rec(self, L, full):
    P = self.P
    H = 4
    g = lambda n: L[n]
    bk, bb = self.banks, self.bbank
    bc = self.bconst
    mask2, ident = self.mask2, self.ident
    maskL, bones, ch01, identb4 = g("maskL"), g("bones"), g("ch01"), g("identb4")
    PBUF, GG, LNB, BETA, bBG = g("PB"), g("GG"), g("LNB"), g("BETA"), g("bBG")
    QT, KT, VT, bQT, bKT, bVT = g("QT"), g("KT"), g("VT"), g("bQT"), g("bKT"), g("bVT")
    R, USB, bR, bUSB = g("R"), g("USB"), g("bR"), g("bUSB")
    S32, SBF, bS32, bSBF, sbf_i = g("S32"), g("SBF"), g("bS32"), g("bSBF"), g("sbf_i")
    O32, bO32 = g("O32"), g("bO32")
    evac, nextpj = g("evac"), g("nextpj")
    NP = NT
    pP = [bk[3 + j][:].rearrange("p (h t) -> p h t", h=H) for j in range(NP)]
    pPb = [bk[3 + j][:].bitcast(BF16)[:, 0:512].rearrange("p (h t) -> p h t", h=H) for j in range(NP)]
    bpP = [bb[3 + j] for j in range(NP)]
    ptr4 = bk[5][:].bitcast(BF16)[:, 0:512].rearrange("p (h t) -> p h t", h=H)
    pKS = bk[5][:].rearrange("p (h t) -> p h t", h=H)
    pU = bk[6][:].rearrange("p (h t) -> p h t", h=H)
    po = bk[7][:].rearrange("p (h t) -> p h t", h=H)

    def bc4(ap):
        return ap.unsqueeze(2).to_broadcast([128, H, 128])

    for j in range(NP):
        pb = PBUF[j]
        c0 = j * 128
        SC = pb["SC"]
        TT, bTT = pb["TT"], pb["bTT"]
        for half in range(2):
            r0 = half * 64
            cs = sbf_i[0]
            for h in range(H):
                P.pe(lambda e, h=h, pb=pb, cs=cs: e.matmul(pKS[:, h, :], lhsT=pb["KTP"][:, h, :], rhs=SBF[cs][:, h, :],
                                                           start=True, stop=True),
                     reads=[pb["bKTP"], bSBF[cs][h]], writes=[bb[5]])
            if full:
                for h in range(H):
                    P.pe(lambda e, pb=pb, h=h, cs=cs, r0=r0, half=half: e.matmul(
                        po[:, h, r0:r0 + 64], lhsT=SBF[cs][:, h, :], rhs=pb["QTG"][:, h, r0:r0 + 64],
                        start=(h == 0 and half == 0), stop=False, skip_group_check=True),
                        reads=[bSBF[cs][h], pb["bQTG"]], writes=[bb[7]])
            for h in range(H):
                P.dve(lambda e, pb=pb, h=h, r0=r0, SC=SC: e.scalar_tensor_tensor(
                    out=R[r0:r0 + 64, h, :], in0=pKS[r0:r0 + 64, h, :], scalar=SC[r0:r0 + 64, 4, h:h + 1],
                    in1=pb["BV"][r0:r0 + 64, h, :], op0=ALU.mult, op1=ALU.add),
                    reads=[bb[5], pb["bSC"], pb["bBV"]], writes=[bR])
            yield
            for h in range(H):
                P.pe(lambda e, h=h, r0=r0, TT=TT: e.matmul(pU[:, h, :], lhsT=TT[r0:r0 + 64, h, :], rhs=R[r0:r0 + 64, h, :],
                                                           start=True, stop=True),
                     reads=[bTT, bR], writes=[bb[6]])
            yield
            P.act(lambda e, r0=r0: e.activation(out=USB[r0:r0 + 64], in_=pU[r0:r0 + 64], func=AF.Copy),
                  reads=[bb[6]], writes=[bUSB])
            yield
            pS, bpS = bk[6][:], bb[6]
            pS4 = pS.rearrange("p (h t) -> p h t", h=H)
            for h in range(H):
                P.pe(lambda e, pb=pb, h=h, r0=r0, pS4=pS4: e.matmul(pS4[:, h, :], lhsT=pb["KHT"][r0:r0 + 64, h, :],
                                                                    rhs=USB[r0:r0 + 64, h, :], start=True, stop=True),
                     reads=[pb["bKHT"], bUSB], writes=[bpS])
            yield
            for h in range(H):
                P.dve(lambda e, h=h, half=half, SC=SC, pS4=pS4: e.scalar_tensor_tensor(
                    out=S32[:, h, :], in0=S32[:, h, :], scalar=SC[:, 6 + half, h:h + 1], in1=pS4[:, h, :],
                    op0=ALU.mult, op1=ALU.add), reads=[bS32[h], pb["bSC"], bpS], writes=[bS32[h]])
            yield
            nx = 1 - cs
            P.act(lambda e, nx=nx: e.activation(out=SBF[nx], in_=S32, func=AF.Copy), reads=bS32, writes=bSBF[nx])
            sbf_i[0] = nx
            yield
        if full:
            for h in range(H):
                P.pe(lambda e, pb=pb, h=h: e.matmul(po[:, h, :], lhsT=USB[:, h, :], rhs=pb["ATT"][:, h, :],
                                                    start=False, stop=True, skip_group_check=True),
                     reads=[bUSB, pb["bATT"]], writes=[bb[7]])
            P.act(lambda e, c0=c0: e.activation(out=O32[:, :, c0:c0 + 128], in_=po, func=AF.Copy),
                  reads=[bb[7]], writes=[bO32])


KB._gdn_prep = _gdn_prep
KB._gdn_rec = _gdn_rec


CAP = 384
NEXP = 32
NSLOT = NEXP * CAP
BIG = 1.0e4


def _spill(self, name, sb_ap, bufs, sb):
    P = self.P
    if name not in self.d:
        self.d[name] = self.nc.dram_tensor(name, [128, 4, self.nfull_tok], BF16, kind="Internal").ap()
        self.bspill = getattr(self, "bspill", {})
        self.bspill[name] = {}
    dst = self.d[name]
    tok0 = (sb - self.npre) * SBT
    b = Buf(name)
    self.bspill[name][sb - self.npre] = b
    P.dma(lambda e: e.dma_start(out=dst[:, :, tok0:tok0 + SBT], in_=sb_ap), reads=bufs, writes=[b])


KB.spill = _spill


def _pass3(self):
    nc, P, A = self.nc, self.P, self.A
    d = self.d
    H = 4
    for nm, shp in (("hg_up", [512, D]), ("gd_up", [512, D]), ("w_out", [D, D]), ("norm_ffn_g", [D]),
                    ("router_w", [D, 36]), ("router_b", [36])):
        self.din(nm, shp)
    ntile = self.nfull_tok // 128
    d["h2_s"] = nc.dram_tensor("h2_s", [self.nfull_tok, D], F32, kind="Internal").ap()
    self.bh2s = [Buf("h2_s%d" % i) for i in range(ntile)]
    self.bxbuf = []
    self.W12 = A.alloc((ntile, 2), F32)
    self.DST = A.alloc((ntile, 2), U32)
    self.bW12 = Buf("W12")
    self.bDST = Buf("DST")
    m0 = A.mark()
    W3 = A.alloc((KC, 2048), BF16)
    bW3 = [Buf() for _ in range(KC)]
    wv = d["w_in"].rearrange("(k p) n -> p k n", p=128)
    for k in range(KC):
        P.dma(lambda e, k=k: e.dma_start(out=W3[:, k, :], in_=wv[:, k, C_PA:C_PA + 2048]), writes=[bW3[k]], q="pool")
    HGUP = A.alloc((H, D), BF16)
    GDUP = A.alloc((H, D), BF16)
    WOUT = A.alloc((KC, D), BF16)
    bUP = Buf()
    bWO = [Buf() for _ in range(KC)]
    P.dma(lambda e: e.dma_start(out=HGUP, in_=d["hg_up"].rearrange("(h p) n -> p h n", p=128)), writes=[bUP], q="pool")
    P.dma(lambda e: e.dma_start(out=GDUP, in_=d["gd_up"].rearrange("(h p) n -> p h n", p=128)), writes=[bUP], q="pool")
    wo = d["w_out"].rearrange("(k p) n -> p k n", p=128)
    for k in range(KC):
        P.dma(lambda e, k=k: e.dma_start(out=WOUT[:, k, :], in_=wo[:, k, :]), writes=[bWO[k]], q="pool")
    WR = A.alloc((KC, 36), F32)
    RB = A.alloc((36,), F32)
    G2 = A.alloc((D,), F32)
    ECAP = A.alloc((NEXP,), F32)
    bpar = Buf("par3")
    P.dma(lambda e: e.dma_start(out=WR, in_=d["router_w"].rearrange("(k p) n -> p k n", p=128)), writes=[bpar])
    P.dma(lambda e: e.dma_start(out=RB, in_=d["router_b"].partition_broadcast(128)), writes=[bpar])
    P.dma(lambda e: e.dma_start(out=G2, in_=d["norm_ffn_g"].partition_broadcast(128)), writes=[bpar])
    self.load_gain("norm_mix_g")
    ecapi = A.alloc((NEXP,), I32)
    P.pool(lambda e: e.iota(ecapi, pattern=[[CAP, NEXP]], base=0, channel_multiplier=0), writes=[bpar])
    P.pool(lambda e: e.tensor_copy(out=ECAP, in_=ecapi), reads=[bpar], writes=[bpar])
    triS = A.alloc((128,), BF16)
    trif = A.alloc((128,), F32)
    bc = self.bconst
    P.pool(lambda e: e.memset(trif, 1.0), writes=[bc])
    P.pool(lambda e: e.affine_select(out=trif, in_=trif, pattern=[[1, 128]], compare_op=ALU.is_gt, fill=0.0,
                                     base=0, channel_multiplier=-1), reads=[bc], writes=[bc])
    P.pool(lambda e: e.tensor_copy(out=triS, in_=trif), reads=[bc], writes=[bc])
    BASE = A.alloc((NEXP,), F32)
    bBASE = Buf()
    P.pool(lambda e: e.tensor_copy(out=BASE, in_=ecapi), reads=[bpar], writes=[bBASE])

    xnTs = [A.alloc((KC, SBT), BF16) for _ in range(2)]
    bxnTs = [Buf(), Buf()]
    self._xfetched = set()
    use_fetch = hasattr(self, "bxns")
    SGs = [A.alloc((16, SBT), BF16) for _ in range(2)]
    bSGs = [[Buf() for _ in range(16)] for _ in range(2)]
    OAss = [A.alloc((H, SBT), BF16) for _ in range(2)]
    OBss = [A.alloc((H, SBT), BF16) for _ in range(2)]
    bOAss, bOBss = [Buf(), Buf()], [Buf(), Buf()]
    T1 = [A.alloc((SBT,), F32) for _ in range(2)]
    T2 = [A.alloc((SBT,), F32) for _ in range(2)]
    bT1 = [Buf(), Buf()]
    bT2 = [Buf(), Buf()]
    MG = A.alloc((KC, SBT), BF16)
    bMG = [Buf() for _ in range(KC)]
    XR = A.alloc((D,), F32)
    bXR = Buf()
    H2 = A.alloc((D,), F32)
    bH2 = Buf()
    JK = A.alloc((D,), BF16)
    SS2 = A.alloc((1,), F32)
    bSS2 = Buf()
    XF = A.alloc((D,), F32)
    XB = A.alloc((D,), BF16)
    bXF, bXB = Buf(), Buf()
    XFT = A.alloc((KC, 128), F32)
    bXFT = Buf()
    LG = A.alloc((36,), F32)
    ME = A.alloc((NEXP,), F32)
    SM = A.alloc((16,), F32)
    M8 = A.alloc((8,), F32)
    SEL1 = A.alloc((NEXP,), F32)
    SEL2 = A.alloc((NEXP,), F32)
    SELB = A.alloc((NEXP,), BF16)
    RK = A.alloc((NEXP,), F32)
    JK2 = A.alloc((NEXP,), F32)
    DF = A.alloc((2,), F32)
    brt = Buf("route")

    bk, bb = self.banks, self.bbank
    ptr = bk[0][:].bitcast(BF16).rearrange("p (k t) -> p k t", k=KC)
    pj = [bk[1][:], bk[2][:]]
    bpj = [bb[1], bb[2]]
    pup = [bk[3][:], bk[4][:]]
    pji = [0]

    def nextpj():
        i = pji[0] % 2
        pji[0] += 1
        return pj[i], bpj[i]

    pyi = [0]

    def nextpy():
        i = 5 + pyi[0] % 3
        pyi[0] += 1
        return bk[i][:], bb[i]

    oa_d, ob_d = d["oa_s"], d["ob_s"]
    def X3(sbi):
        sl = sbi % 2
        SG, bSG, OAs, OBs, bOAs, bOBs = SGs[sl], bSGs[sl], OAss[sl], OBss[sl], bOAss[sl], bOBss[sl]
        sb = self.npre + sbi
        tok0 = sbi * SBT
        xnT, bxnT = xnTs[sb % 2], bxnTs[sb % 2]
        if use_fetch:
            yield from self.xnt_fetch_gen(sb, self.nsb - 1, xnTs, bxnTs)
        else:
            yield from self.stage_a_gen(sb, xnT, bxnT, ptr, bb[0])
        P.dma(lambda e, tok0=tok0: e.dma_start(out=OAs, in_=oa_d[:, :, tok0:tok0 + SBT]),
              reads=[self.bspill["oa_s"][sbi]], writes=[bOAs])
        P.dma(lambda e, tok0=tok0: e.dma_start(out=OBs, in_=ob_d[:, :, tok0:tok0 + SBT]),
              reads=[self.bspill["ob_s"][sbi]], writes=[bOBs])
        for cb in range(16):
            p, bp = nextpj()
            for k in range(KC):
                P.pe(lambda e, k=k, p=p, cb=cb: e.matmul(p[:, 0:SBT], lhsT=W3[:, k, cb * 128:(cb + 1) * 128],
                                                         rhs=xnT[:, k, :], start=(k == 0), stop=(k == KC - 1)),
                     reads=[bW3[k], bxnT], writes=[bp])
            P.act(lambda e, cb=cb, p=p: e.activation(out=SG[:, cb, :], in_=p[:, 0:SBT], func=AF.Sigmoid),
                  reads=[bp], writes=[bSG[cb]])
            yield
    def Y3(sbi):
        sl = sbi % 2
        SG, bSG, OAs, OBs, bOAs, bOBs = SGs[sl], bSGs[sl], OAss[sl], OBss[sl], bOAss[sl], bOBss[sl]
        sb = self.npre + sbi
        tok0 = sbi * SBT
        for cb in range(KC):
            i = cb % 2
            for h in range(H):
                P.pe(lambda e, h=h, cb=cb: e.matmul(pup[0][:, 0:SBT], lhsT=HGUP[:, h, cb * 128:(cb + 1) * 128],
                                                    rhs=OAs[:, h, :], start=(h == 0), stop=(h == H - 1)),
                     reads=[bUP, bOAs], writes=[bb[3]])
            for h in range(H):
                P.pe(lambda e, h=h, cb=cb: e.matmul(pup[1][:, 0:SBT], lhsT=GDUP[:, h, cb * 128:(cb + 1) * 128],
                                                    rhs=OBs[:, h, :], start=(h == 0), stop=(h == H - 1)),
                     reads=[bUP, bOBs], writes=[bb[4]])
            P.dve(lambda e, cb=cb, i=i: e.tensor_tensor(out=T1[i], in0=pup[0][:, 0:SBT], in1=SG[:, cb, :], op=ALU.mult),
                  reads=[bb[3], bSG[cb]], writes=[bT1[i]])
            P.dve(lambda e, cb=cb, i=i: e.tensor_tensor(out=T2[i], in0=pup[1][:, 0:SBT], in1=SG[:, 8 + cb, :], op=ALU.mult),
                  reads=[bb[4], bSG[8 + cb]], writes=[bT2[i]])
            P.pool(lambda e, cb=cb, i=i: e.tensor_tensor(out=MG[:, cb, :], in0=T1[i], in1=T2[i], op=ALU.add),
                   reads=[bT1[i], bT2[i]], writes=[bMG[cb]])
            yield
        for t in range(NT):
            gt = sbi * NT + t
            gtok = self.npre * SBT + gt * 128
            P.dma(lambda e, gtok=gtok: e.dma_start(out=XR, in_=d["xs"][gtok:gtok + 128, :]), writes=[bXR])
            for half in range(2):
                p, bp = nextpy()
                for k in range(KC):
                    P.pe(lambda e, k=k, p=p, t=t, half=half: e.matmul(
                        p, lhsT=MG[:, k, t * 128:(t + 1) * 128], rhs=WOUT[:, k, half * 512:(half + 1) * 512],
                        start=(k == 0), stop=(k == KC - 1)), reads=[bMG[k], bWO[k]], writes=[bp])
                P.dve(lambda e, p=p, half=half: e.tensor_tensor(out=H2[:, half * 512:(half + 1) * 512], in0=p,
                                                               in1=XR[:, half * 512:(half + 1) * 512], op=ALU.add),
                      reads=[bp, bXR], writes=[bH2])
                yield
            P.dma(lambda e, gt=gt: e.dma_start(out=d["h2_s"][gt * 128:(gt + 1) * 128, :], in_=H2),
                  reads=[bH2], writes=[self.bh2s[gt]])
            LL = dict(L0)
            LL['nextpj'] = nextpy
            yield from self._route_tile(LL, gt)
    L0 = dict(locals())

    import os
    WTS3 = [int(v) for v in os.environ.get("IL_W3", "1,1").split(",")]

    def run_il(gens):
        gens = list(gens)
        while gens:
            for g_, w_ in list(gens):
                for _ in range(w_):
                    try:
                        next(g_)
                    except StopIteration:
                        gens.remove((g_, w_))
                        break

    n = self.nfull
    for r in range(-1, n):
        gs = []
        if 0 <= r < n:
            gs.append((Y3(r), WTS3[0]))
        if 0 <= r + 1 < n:
            gs.append((X3(r + 1), WTS3[1]))
        run_il(gs)
    A.reset(m0)
    P.barrier()


KB.pass3 = _pass3


def _route_tile(self, L, gt):
    P = self.P
    g = lambda n: L[n]
    bk, bb = self.banks, self.bbank
    bc = self.bconst
    H2, bH2, JK, SS2, bSS2 = g("H2"), g("bH2"), g("JK"), g("SS2"), g("bSS2")
    XF, XB, bXF, bXB, XFT, bXFT = g("XF"), g("XB"), g("bXF"), g("bXB"), g("XFT"), g("bXFT")
    G2, WR, RB, ECAP, bpar = g("G2"), g("WR"), g("RB"), g("ECAP"), g("bpar")
    LG, ME, SM, M8, SEL1, SEL2, SELB, RK, JK2, DF, brt = (g("LG"), g("ME"), g("SM"), g("M8"), g("SEL1"), g("SEL2"),
                                                           g("SELB"), g("RK"), g("JK2"), g("DF"), g("brt"))
    BASE, bBASE, triS = g("BASE"), g("bBASE"), g("triS")
    nextpj = g("nextpj")
    W12, DST = self.W12, self.DST
    P.act(lambda e: e.activation(out=JK, in_=H2, func=AF.Square, accum_out=SS2), reads=[bH2], writes=[bSS2])
    P.act(lambda e: e.activation(out=SM[:, 0:1], in_=SS2, func=AF.Ln, scale=1.0 / D, bias=EPS), reads=[bSS2], writes=[brt])
    P.act(lambda e: e.activation(out=SM[:, 0:1], in_=SM[:, 0:1], func=AF.Exp, scale=-0.5), reads=[brt], writes=[brt])
    P.dve(lambda e: e.scalar_tensor_tensor(out=XF, in0=H2, scalar=SM[:, 0:1], in1=G2, op0=ALU.mult, op1=ALU.mult),
          reads=[bH2, brt, bpar], writes=[bXF])
    P.pool(lambda e: e.tensor_copy(out=XB, in_=XF), reads=[bXF], writes=[bXB])
    yield
    for half in range(2):
        for kk in range(4):
            k = half * 4 + kk
            P.pe(lambda e, k=k, kk=kk, half=half: e.transpose(out=bk[3 + half][:, kk * 128:(kk + 1) * 128],
                                                              in_=XF[:, k * 128:(k + 1) * 128], identity=self.identf),
                 reads=[bXF, bc], writes=[bb[3 + half]])
    P.act(lambda e: e.activation(out=XFT[:, 0:4, :], in_=bk[3][:].rearrange("p (k t) -> p k t", k=4), func=AF.Copy),
          reads=[bb[3]], writes=[bXFT])
    P.dve(lambda e: e.tensor_copy(out=XFT[:, 4:8, :], in_=bk[4][:].rearrange("p (k t) -> p k t", k=4)),
          reads=[bb[4]], writes=[bXFT])
    yield
    p, bp = nextpj()
    for k in range(KC):
        P.pe(lambda e, k=k, p=p: e.matmul(p[:, 0:36], lhsT=XFT[:, k, :], rhs=WR[:, k, :], start=(k == 0), stop=(k == KC - 1)),
             reads=[bXFT, bpar], writes=[bp])
    P.dve(lambda e, p=p: e.tensor_tensor(out=LG, in0=p[:, 0:36], in1=RB, op=ALU.add), reads=[bp, bpar], writes=[brt])
    yield
    P.dve(lambda e: e.tensor_reduce(out=SM[:, 1:2], in_=LG[:, 0:4], axis=AX.X, op=ALU.max), reads=[brt], writes=[brt])
    P.dve(lambda e: e.tensor_scalar(out=SM[:, 2:3], in0=SM[:, 1:2], scalar1=-1.0, scalar2=None, op0=ALU.mult),
          reads=[brt], writes=[brt])
    P.act(lambda e: e.activation(out=JK2[:, 0:4], in_=LG[:, 0:4], func=AF.Exp, bias=SM[:, 2:3], accum_out=SM[:, 3:4]),
          reads=[brt], writes=[brt])
    P.dve(lambda e: e.reciprocal(out=SM[:, 4:5], in_=SM[:, 3:4]), reads=[brt], writes=[brt])
    yield
    P.dve(lambda e: e.tensor_scalar(out=SM[:, 12:16], in0=LG[:, 0:4], scalar1=SM[:, 1:2], scalar2=-1.0,
                                    op0=ALU.is_equal, op1=ALU.add), reads=[brt], writes=[brt])
    P.dve(lambda e: e.scalar_tensor_tensor(out=ME.rearrange("p (g j) -> p g j", g=4),
                                           in0=SM[:, 12:16].unsqueeze(2).to_broadcast([128, 4, 8]), scalar=BIG,
                                           in1=LG[:, 4:36].rearrange("p (g j) -> p g j", g=4),
                                           op0=ALU.mult, op1=ALU.add), reads=[brt], writes=[brt])
    P.dve(lambda e: e.max(out=M8, in_=ME), reads=[brt], writes=[brt])
    yield
    P.dve(lambda e: e.tensor_tensor(out=SM[:, 5:6], in0=M8[:, 1:2], in1=M8[:, 0:1], op=ALU.subtract), reads=[brt], writes=[brt])
    P.act(lambda e: e.activation(out=SM[:, 6:7], in_=SM[:, 5:6], func=AF.Exp), reads=[brt], writes=[brt])
    P.dve(lambda e: e.tensor_scalar(out=SM[:, 7:8], in0=SM[:, 6:7], scalar1=1.0, scalar2=None, op0=ALU.add),
          reads=[brt], writes=[brt])
    P.dve(lambda e: e.reciprocal(out=SM[:, 8:9], in_=SM[:, 7:8]), reads=[brt], writes=[brt])
    P.dve(lambda e, gt=gt: e.tensor_tensor(out=W12[:, gt, 0:1], in0=SM[:, 8:9], in1=SM[:, 4:5], op=ALU.mult),
          reads=[brt], writes=[self.bW12])
    P.dve(lambda e, gt=gt: e.tensor_tensor(out=W12[:, gt, 1:2], in0=SM[:, 4:5], in1=W12[:, gt, 0:1], op=ALU.subtract),
          reads=[brt, self.bW12], writes=[self.bW12])
    P.dve(lambda e: e.tensor_scalar(out=SEL1, in0=ME, scalar1=M8[:, 0:1], scalar2=None, op0=ALU.is_equal),
          reads=[brt], writes=[brt])
    P.dve(lambda e: e.tensor_scalar(out=SEL2, in0=ME, scalar1=M8[:, 1:2], scalar2=None, op0=ALU.is_equal),
          reads=[brt], writes=[brt])
    P.dve(lambda e: e.tensor_tensor(out=SELB, in0=SEL1, in1=SEL2, op=ALU.add), reads=[brt], writes=[brt])
    yield
    p2, bp2 = nextpj()
    P.pe(lambda e, p2=p2: e.matmul(p2[:, 0:32], lhsT=triS, rhs=SELB, start=True, stop=True), reads=[brt, bc], writes=[bp2])
    P.pe(lambda e, p2=p2: e.matmul(p2[:, 32:64], lhsT=self.ones_bf, rhs=SELB, start=True, stop=True),
         reads=[brt, bc], writes=[bp2])
    P.dve(lambda e, p2=p2: e.tensor_tensor(out=RK, in0=p2[:, 0:32], in1=BASE, op=ALU.add), reads=[bp2, bBASE], writes=[brt])
    P.dve(lambda e, p2=p2: e.tensor_tensor(out=BASE, in0=p2[:, 32:64], in1=BASE, op=ALU.add), reads=[bp2, bBASE], writes=[bBASE])
    P.dve(lambda e: e.scalar_tensor_tensor(out=JK2, in0=SEL1, scalar=1.0, in1=RK, op0=ALU.mult, op1=ALU.mult,
                                           accum_out=DF[:, 0:1]), reads=[brt], writes=[brt])
    P.dve(lambda e: e.scalar_tensor_tensor(out=JK2, in0=SEL2, scalar=1.0, in1=RK, op0=ALU.mult, op1=ALU.mult,
                                           accum_out=DF[:, 1:2]), reads=[brt], writes=[brt])
    P.dve(lambda e, gt=gt: e.tensor_copy(out=DST[:, gt, :], in_=DF), reads=[brt], writes=[self.bDST])
    yield
    xb = self.d["x_buf"]
    for k in range(2):
        bx = Buf("xbuf")
        self.bxbuf.append(bx)
        P.dma(lambda e, gt=gt, k=k: e.indirect_dma_start(
            out=xb, out_offset=bass.IndirectOffsetOnAxis(ap=DST[:, gt, k:k + 1], axis=0), in_=XB, in_offset=None),
            reads=[bXB, self.bDST] + self.bxz, writes=[bx], q="pool")


KB._route_tile = _route_tile


def _pass4(self):
    nc, P, A = self.nc, self.P, self.A
    d = self.d
    self.din("w_gate", [NEXP, D, 512])
    self.din("w_up", [NEXP, D, 512])
    self.din("w_down", [NEXP, 512, D])
    d["y_buf"] = nc.dram_tensor("y_buf", [NSLOT, D], F32, kind="Internal").ap()
    self.bybuf = []
    m0 = A.mark()
    NB = CAP // 128
    WG = [A.alloc((KC, 512), BF16) for _ in range(2)]
    WU = [A.alloc((KC, 512), BF16) for _ in range(2)]
    WD = [A.alloc((4, D), BF16) for _ in range(2)]
    bWG = [Buf(), Buf()]
    bWU = [Buf(), Buf()]
    bWD = [Buf(), Buf()]
    XE = [A.alloc((NB, D), BF16) for _ in range(2)]
    bXE = [Buf(), Buf()]
    XET = [A.alloc((KC, CAP), BF16) for _ in range(2)]
    bXET = [Buf(), Buf()]
    SGT = [A.alloc((CAP,), F32) for _ in range(2)]
    bSGT = [Buf(), Buf()]
    HT = A.alloc((4, CAP), BF16)
    bHT = [Buf() for _ in range(4)]
    YS = [A.alloc((D,), F32) for _ in range(2)]
    bYS = [Buf(), Buf()]
    bk, bb = self.banks, self.bbank
    ptr = bk[0][:].bitcast(BF16).rearrange("p (k t) -> p k t", k=KC)
    ident = self.ident
    bc = self.bconst
    xb, yb = d["x_buf"], d["y_buf"]
    cnt = [0]

    def bank(lo, n):
        i = lo + cnt[0] % n
        cnt[0] += 1
        return bk[i][:], bb[i]

    def load_w(e):
        i = e % 2
        P.dma(lambda eng, e=e, i=i: eng.dma_start(out=WG[i], in_=d["w_gate"][e].rearrange("(k p) n -> p k n", p=128)),
              writes=[bWG[i]], q="pool")
        P.dma(lambda eng, e=e, i=i: eng.dma_start(out=WU[i], in_=d["w_up"][e].rearrange("(k p) n -> p k n", p=128)),
              writes=[bWU[i]], q="pool")
        P.dma(lambda eng, e=e, i=i: eng.dma_start(out=WD[i], in_=d["w_down"][e].rearrange("(k p) n -> p k n", p=128)),
              writes=[bWD[i]], q="pool")

    def load_x(e):
        i = e % 2
        P.dma(lambda eng, e=e, i=i: eng.dma_start(out=XE[i], in_=xb[e * CAP:(e + 1) * CAP, :].rearrange("(b p) n -> p b n", p=128)),
              reads=self.bxbuf, writes=[bXE[i]])

    def transposes(e):
        i = e % 2
        for b in range(NB):
            pt, bpt = (ptr, bb[0]) if b % 2 == 0 else (ptr7, bb[7])
            for k in range(KC):
                P.pe(lambda eng, i=i, b=b, k=k, pt=pt: eng.transpose(out=pt[:, k, :], in_=XE[i][:, b, k * 128:(k + 1) * 128], identity=ident),
                     reads=[bXE[i], bc], writes=[bpt])
            if b % 2 == 0:
                P.act(lambda eng, b=b, i=i, pt=pt: eng.activation(out=XET[i][:, :, b * 128:(b + 1) * 128], in_=pt, func=AF.Copy),
                      reads=[bpt], writes=[bXET[i]])
            else:
                P.dve(lambda eng, b=b, i=i, pt=pt: eng.tensor_copy(out=XET[i][:, :, b * 128:(b + 1) * 128], in_=pt),
                      reads=[bpt], writes=[bXET[i]])

    ptr7 = bk[7][:].bitcast(BF16).rearrange("p (k t) -> p k t", k=KC)
    load_w(0)
    load_x(0)
    transposes(0)
    yi = 0
    for e in range(NEXP):
        i = e % 2
        if e + 1 < NEXP:
            load_w(e + 1)
            load_x(e + 1)
        for fc in range(4):
            pg, bpg = bk[1 + fc % 2][:], bb[1 + fc % 2]
            pu, bpu = bk[3 + fc % 2][:], bb[3 + fc % 2]
            for k in range(KC):
                P.pe(lambda eng, i=i, fc=fc, k=k, pg=pg: eng.matmul(pg[:, 0:CAP], lhsT=WG[i][:, k, fc * 128:(fc + 1) * 128],
                                                                    rhs=XET[i][:, k, :], start=(k == 0), stop=(k == KC - 1)),
                     reads=[bWG[i], bXET[i]], writes=[bpg])
            for k in range(KC):
                P.pe(lambda eng, i=i, fc=fc, k=k, pu=pu: eng.matmul(pu[:, 0:CAP], lhsT=WU[i][:, k, fc * 128:(fc + 1) * 128],
                                                                    rhs=XET[i][:, k, :], start=(k == 0), stop=(k == KC - 1)),
                     reads=[bWU[i], bXET[i]], writes=[bpu])
            j = fc % 2
            P.act(lambda eng, pg=pg, j=j: eng.activation(out=SGT[j], in_=pg[:, 0:CAP], func=AF.Silu), reads=[bpg], writes=[bSGT[j]])
            P.dve(lambda eng, pu=pu, j=j, fc=fc: eng.tensor_tensor(out=HT[:, fc, :], in0=pu[:, 0:CAP], in1=SGT[j], op=ALU.mult),
                  reads=[bpu, bSGT[j]], writes=[bHT[fc]])
        if e + 1 < NEXP:
            transposes(e + 1)
        for b in range(NB):
            ys, bys = YS[yi % 2], bYS[yi % 2]
            yi += 1
            for half in range(2):
                pd, bpd = bk[5 + half][:], bb[5 + half]
                for fc in range(4):
                    P.pe(lambda eng, i=i, b=b, fc=fc, half=half, pd=pd: eng.matmul(
                        pd, lhsT=HT[:, fc, b * 128:(b + 1) * 128], rhs=WD[i][:, fc, half * 512:(half + 1) * 512],
                        start=(fc == 0), stop=(fc == 3)), reads=[bHT[fc], bWD[i]], writes=[bpd])
                if half == 0:
                    P.act(lambda eng, pd=pd, ys=ys: eng.activation(out=ys[:, 0:512], in_=pd, func=AF.Copy), reads=[bpd], writes=[bys])
                else:
                    P.dve(lambda eng, pd=pd, ys=ys: eng.tensor_copy(out=ys[:, 512:1024], in_=pd), reads=[bpd], writes=[bys])
            r0 = e * CAP + b * 128
            by = Buf("ybuf")
            self.bybuf.append(by)
            P.dma(lambda eng, r0=r0, ys=ys: eng.dma_start(out=yb[r0:r0 + 128, :], in_=ys), reads=[bys], writes=[by])
    A.reset(m0)
    P.barrier()


def _pass5(self):
    nc, P, A = self.nc, self.P, self.A
    d = self.d
    self.din("final_norm_g", [D])
    out = self.dout("out", [self.nfull_tok, D], F32)
    m0 = A.mark()
    ntile = self.nfull_tok // 128
    FG = A.alloc((D,), F32)
    bFG = Buf()
    P.dma(lambda e: e.dma_start(out=FG, in_=d["final_norm_g"].partition_broadcast(128)), writes=[bFG])
    NB5 = 4
    Y1 = [A.alloc((D,), F32) for _ in range(NB5)]
    Y2 = [A.alloc((D,), F32) for _ in range(NB5)]
    HH = [A.alloc((D,), F32) for _ in range(NB5)]
    OT = [A.alloc((D,), F32) for _ in range(NB5)]
    bY1, bY2, bHH, bOT = ([Buf() for _ in range(NB5)], [Buf() for _ in range(NB5)], [Buf() for _ in range(NB5)],
                          [Buf() for _ in range(NB5)])
    JK = A.alloc((D,), BF16)
    SS = A.alloc((ntile,), F32)
    bSS = Buf()
    yb = d["y_buf"]
    for gt in range(ntile):
        i = gt % NB5
        P.dma(lambda e, gt=gt, i=i: e.indirect_dma_start(
            out=Y1[i], out_offset=None, in_=yb, in_offset=bass.IndirectOffsetOnAxis(ap=self.DST[:, gt, 0:1], axis=0)),
            reads=self.bybuf + [self.bDST], writes=[bY1[i]], q="pool")
        P.dma(lambda e, gt=gt, i=i: e.indirect_dma_start(
            out=Y2[i], out_offset=None, in_=yb, in_offset=bass.IndirectOffsetOnAxis(ap=self.DST[:, gt, 1:2], axis=0)),
            reads=self.bybuf + [self.bDST], writes=[bY2[i]], q="pool")
        P.dma(lambda e, gt=gt, i=i: e.dma_start(out=HH[i], in_=d["h2_s"][gt * 128:(gt + 1) * 128, :]),
              reads=[self.bh2s[gt]], writes=[bHH[i]])
        P.dve(lambda e, gt=gt, i=i: e.scalar_tensor_tensor(out=HH[i], in0=Y1[i], scalar=self.W12[:, gt, 0:1], in1=HH[i],
                                                          op0=ALU.mult, op1=ALU.add),
              reads=[bY1[i], bHH[i], self.bW12], writes=[bHH[i]])
        P.dve(lambda e, gt=gt, i=i: e.scalar_tensor_tensor(out=HH[i], in0=Y2[i], scalar=self.W12[:, gt, 1:2], in1=HH[i],
                                                          op0=ALU.mult, op1=ALU.add),
              reads=[bY2[i], bHH[i], self.bW12], writes=[bHH[i]])
        P.act(lambda e, gt=gt, i=i: e.activation(out=JK, in_=HH[i], func=AF.Square, accum_out=SS[:, gt:gt + 1]),
              reads=[bHH[i]], writes=[bSS])
        P.act(lambda e, gt=gt: e.activation(out=SS[:, gt:gt + 1], in_=SS[:, gt:gt + 1], func=AF.Ln, scale=1.0 / D, bias=EPS),
              reads=[bSS], writes=[bSS])
        P.act(lambda e, gt=gt: e.activation(out=SS[:, gt:gt + 1], in_=SS[:, gt:gt + 1], func=AF.Exp, scale=-0.5),
              reads=[bSS], writes=[bSS])
        P.dve(lambda e, gt=gt, i=i: e.scalar_tensor_tensor(out=OT[i], in0=HH[i], scalar=SS[:, gt:gt + 1], in1=FG,
                                                          op0=ALU.mult, op1=ALU.mult),
              reads=[bHH[i], bSS, bFG], writes=[bOT[i]])
        P.dma(lambda e, gt=gt, i=i: e.dma_start(out=out[gt * 128:(gt + 1) * 128, :], in_=OT[i]), reads=[bOT[i]], out=True)
    A.reset(m0)


KB.pass4 = _pass4
KB.pass5 = _pass5


NPRE_SB = 17
NFULL_SB = 16
_NC_CACHE = {}


def _build_full():
    if "nc" not in _NC_CACHE:
        kb = KB(NPRE_SB, NFULL_SB)
        kb.setup()
        kb.pass1()
        kb.pass2()
        kb.pass3()
        kb.pass4()
        kb.pass5()
        _NC_CACHE["nc"] = kb.finish()
    return _NC_CACHE["nc"]


def kernel(x, meta_tokens, hg_lb_logits, norm_mix_g, w_in, gd_conv_w, gd_A_log, gd_dt_bias, hg_norm_g, gd_norm_g,
           hg_up, gd_up, w_out, norm_ffn_g, router_group_w, router_group_b, router_expert_w, router_expert_b,
           w_gate, w_up, w_down, final_norm_g):
    f32 = np.float32
    c = lambda a: np.ascontiguousarray(np.asarray(a, dtype=f32))
    x = c(x)
    meta = c(meta_tokens)
    B, S, _ = x.shape
    half = S // 2
    npre_tok = NPRE_SB * SBT
    ntok = (NPRE_SB + NFULL_SB) * SBT
    nmeta = meta.shape[0]
    shared = {
        "w_in": c(w_in[0]),
        "norm_mix_g": c(norm_mix_g[0]),
        "hg_lb": c(np.asarray(hg_lb_logits, f32).reshape(2, 4, 128).transpose(2, 0, 1)),
        "hg_norm_g": c(hg_norm_g[0]),
        "conv_wT": c(np.asarray(gd_conv_w[0], f32).reshape(4, 12, 128).transpose(2, 0, 1)),
        "gd_A_log": c(gd_A_log[0]),
        "gd_dt_bias": c(gd_dt_bias[0]),
        "gd_norm_g": c(gd_norm_g[0]),
        "hg_up": c(hg_up[0]),
        "gd_up": c(gd_up[0]),
        "w_out": c(w_out[0]),
        "norm_ffn_g": c(norm_ffn_g[0]),
        "router_w": c(np.concatenate([np.asarray(router_group_w[0], f32), np.asarray(router_expert_w[0], f32)], axis=1)),
        "router_b": c(np.concatenate([np.asarray(router_group_b[0], f32), np.asarray(router_expert_b[0], f32)])),
        "w_gate": c(w_gate[0]),
        "w_up": c(w_up[0]),
        "w_down": c(w_down[0]),
        "final_norm_g": c(final_norm_g),
    }
    in_maps = []
    for core in range(2 * B):
        b, hf = core // 2, core % 2
        xs = np.zeros((ntok, D), f32)
        if hf == 0:
            xs[npre_tok - nmeta:npre_tok] = meta
            xs[npre_tok:] = x[b, 0:half]
        else:
            xs[npre_tok - half - nmeta:npre_tok - half] = meta
            xs[npre_tok - half:] = x[b]
        m = dict(shared)
        m["xs"] = xs
        in_maps.append(m)
    nc = _build_full()
    res = run_bass_kernel_spmd(nc, in_maps, core_ids=list(range(2 * B)))
    out = np.empty((B, S, D), f32)
    for core in range(2 * B):
        b, hf = core // 2, core % 2
        out[b, hf * half:(hf + 1) * half] = np.asarray(res.results[core]["out"], dtype=f32)
    return out
```

```python
import numpy as np
import concourse.bass as bass
import concourse.mybir as mybir
from concourse.bass_utils import run_bass_kernel_spmd

F32 = mybir.dt.float32
BF16 = mybir.dt.bfloat16
I32 = mybir.dt.int32
U32 = mybir.dt.uint32
AF = mybir.ActivationFunctionType
ALU = mybir.AluOpType
AX = mybir.AxisListType


class Buf:
    __slots__ = ("name", "lw", "rd", "psum")

    def __init__(self, name="", psum=False):
        self.name = name
        self.lw = None
        self.rd = {}
        self.psum = psum


class Prog:
    ENGS = ("pe", "act", "dve", "pool", "sp")

    def __init__(self, kdma=6):
        self.ops = {e: [] for e in self.ENGS}
        self.waited = {e: {} for e in self.ENGS}
        self.ndma = {e: 0 for e in self.ENGS}
        self.K = kdma
        self.out_toks = []
        self.pending = {e: [] for e in self.ENGS}

    def barrier(self):
        toks = []
        for e in self.ENGS:
            for i in range(len(self.ops[e]) - 1, -1, -1):
                op = self.ops[e][i]
                if (not op["dma"]) and op["fn"] is not None:
                    toks.append((e, i))
                    break
            n = self.ndma[e]
            for slot in range(min(self.K, n)):
                last = ((n - 1 - slot) // self.K) * self.K + slot
                toks.append((("dma", e, slot), 16 * (last // self.K + 1)))
        for e in self.ENGS:
            self.pending[e] = list(toks)

    def _emit(self, eng, fn, reads, writes, dma=False, extra=()):
        deps = {}

        def need(tok):
            if tok is None:
                return
            k, v = tok
            if eng == "pe" and k == "pe":
                return
            if deps.get(k, -1) < v:
                deps[k] = v
        def need_x(tok):
            if tok is not None and tok[0] != eng:
                need(tok)
        for b in reads:
            if b.psum:
                need_x(b.lw)
            else:
                need(b.lw)
        for b in writes:
            if b.psum:
                need_x(b.lw)
            else:
                need(b.lw)
                for k, v in b.rd.items():
                    need((k, v))
        for t in extra:
            need(t)
        if self.pending[eng]:
            for t in self.pending[eng]:
                need(t)
            self.pending[eng] = []
        if dma:
            i = self.ndma[eng]
            self.ndma[eng] += 1
            slot = i % self.K
            val = 16 * (i // self.K + 1)
            key = ("dma", eng, slot)
            if val > 16:
                need((key, val - 16))
            tok = (key, val)
        else:
            tok = (eng, len(self.ops[eng]))
        waits = []
        w = self.waited[eng]
        for k, v in deps.items():
            if w.get(k, -1) < v:
                w[k] = v
                waits.append((k, v))
        self.ops[eng].append(dict(waits=waits, fn=fn, tok=tok, dma=dma))
        for b in reads:
            if b.psum:
                b.lw = tok
                continue
            k, v = tok
            if b.rd.get(k, -1) < v:
                b.rd[k] = v
        for b in writes:
            b.lw = tok
            b.rd = {}
        return tok

    pe_dummy = None
    _pe_cnt = 0

    def pe(self, fn, reads=(), writes=()):
        t = self._emit("pe", fn, reads, writes)
        if self.pe_dummy is not None:
            dfn, every, buf = self.pe_dummy
            self._pe_cnt += 1
            if self._pe_cnt % every == 0:
                self._emit("pe", dfn, (), (buf,))
        return t

    def act(self, fn, reads=(), writes=()):
        return self._emit("act", fn, reads, writes)

    def dve(self, fn, reads=(), writes=()):
        return self._emit("dve", fn, reads, writes)

    def pool(self, fn, reads=(), writes=()):
        return self._emit("pool", fn, reads, writes)

    def dma(self, fn, reads=(), writes=(), q="sp", out=False):
        t = self._emit(q, fn, reads, writes, dma=True)
        if out:
            self.out_toks.append(t)
        return t

    def finalize(self, nc):
        self._emit("sp", None, (), (), extra=self.out_toks)
        targets = {e: set() for e in self.ENGS}
        for e in self.ENGS:
            for op in self.ops[e]:
                for k, v in op["waits"]:
                    if isinstance(k, str):
                        targets[k].add(v)
        rank = {e: {} for e in self.ENGS}
        for e in self.ENGS:
            r = 0
            for i, op in enumerate(self.ops[e]):
                if (not op["dma"]) and i in targets[e]:
                    assert op["fn"] is not None
                    r += 1
                    rank[e][i] = r
        import contextlib
        with contextlib.ExitStack() as st:
            csem = {e: st.enter_context(nc.semaphore("c_" + e)) for e in self.ENGS}
            dsem = {}
            for e in self.ENGS:
                if self.ndma[e] > 0:
                    for s in range(min(self.K, self.ndma[e])):
                        dsem[("dma", e, s)] = st.enter_context(nc.semaphore("d_%s_%d" % (e, s)))
            block = st.enter_context(nc.Block())

            def run(e):
                def body(engine):
                    for i, op in enumerate(self.ops[e]):
                        for k, v in op["waits"]:
                            if isinstance(k, str):
                                engine.wait_ge(csem[k], rank[k][v])
                            else:
                                engine.wait_ge(dsem[k], v)
                        if op["fn"] is None:
                            continue
                        ins = op["fn"](engine)
                        if op["dma"]:
                            ins.then_inc(dsem[op["tok"][0]], 16)
                        elif i in rank[e]:
                            ins.then_inc(csem[e], 1)
                return body
            block.tensor(run("pe"))
            block.scalar(run("act"))
            block.vector(run("dve"))
            block.gpsimd(run("pool"))
            block.sync(run("sp"))


D = 1024
KC = 8
SBT = 256
NT = SBT // 128
NCH = SBT // 64
EPS = 1e-6
C_HQ, C_HF, C_HI, C_HG = 0, 512, 1024, 1536
C_GQ, C_GK, C_GV, C_GZ = 2048, 2560, 3072, 3584
C_GB, C_GA, C_PA, C_PB = 4096, 4100, 4104, 5128
DPROJ = 6152


class Arena:
    def __init__(self, ap, words):
        self.ap = ap
        self.words = words
        self.off = 0
        self.peak = 0

    def mark(self):
        return self.off

    def reset(self, m):
        self.off = m

    def alloc(self, free_shape, dt):
        n = 1
        for s in free_shape:
            n *= s
        esz = 4 if dt in (F32, I32, U32) else 2
        words = (n * esz + 3) // 4
        words = (words + 7) // 8 * 8
        assert self.off + words <= self.words, ("arena overflow", self.off, words, self.words)
        v = self.ap[:, self.off:self.off + words]
        self.off += words
        self.peak = max(self.peak, self.off)
        if esz == 2:
            v = v.bitcast(dt)
        elif dt != F32:
            v = v.bitcast(dt)
        v = v[:, 0:n]
        if len(free_shape) > 1:
            names = ["a%d" % i for i in range(len(free_shape))]
            pat = "p (%s) -> p %s" % (" ".join(names), " ".join(names))
            v = v.rearrange(pat, **{nm: s for nm, s in zip(names, free_shape)})
        return v


class KB:
    def __init__(self, npre, nfull, debug=()):
        import contextlib
        self.npre, self.nfull = npre, nfull
        self.nsb = npre + nfull
        self.ntok = self.nsb * SBT
        self.nfull_tok = nfull * SBT
        self.debug = set(debug)
        self.nc = bass.Bass("TRN2", target_bir_lowering=False)
        self.P = Prog()
        self.d = {}
        self.st = contextlib.ExitStack()

    def din(self, name, shape, dt=F32):
        self.d[name] = self.nc.dram_tensor(name, list(shape), dt, kind="ExternalInput").ap()
        return self.d[name]

    def dout(self, name, shape, dt=F32):
        self.d[name] = self.nc.dram_tensor(name, list(shape), dt, kind="ExternalOutput").ap()
        return self.d[name]

    def setup(self):
        nc, P, st = self.nc, self.P, self.st
        self.din("xs", [self.ntok, D])
        self.din("w_in", [D, DPROJ])
        self.din("norm_mix_g", [D])
        self.din("hg_lb", [128, 2, 4])
        self.din("hg_norm_g", [128])
        AW = 50000
        arena_t = st.enter_context(nc.sbuf_tensor("arena", [128, AW], F32))
        self.A = Arena(arena_t[:], AW)
        self.banks = [st.enter_context(nc.psum_tensor("pb%d" % i, [128, 512], F32)) for i in range(8)]
        self.bbank = [Buf("pb%d" % i, psum=True) for i in range(8)]
        A = self.A
        self.identf = A.alloc((128,), F32)
        self.ident = A.alloc((128,), BF16)
        self.ones_bf = A.alloc((128,), BF16)
        self.ones_f = A.alloc((512,), F32)
        self.mask2 = A.alloc((128,), F32)
        self.bconst = Buf("const")
        bc = self.bconst
        P.pool(lambda e: e.memset(self.identf, 0.0), writes=[bc])
        P.pool(lambda e: e.affine_select(out=self.identf, in_=self.identf, pattern=[[-1, 128]],
                                         compare_op=ALU.not_equal, fill=1.0, base=0, channel_multiplier=1),
               reads=[bc], writes=[bc])
        P.pool(lambda e: e.tensor_copy(out=self.ident, in_=self.identf), reads=[bc], writes=[bc])
        P.pool(lambda e: e.memset(self.ones_bf, 1.0), writes=[bc])
        P.pool(lambda e: e.memset(self.ones_f, 1.0), writes=[bc])
        P.pool(lambda e: e.memset(self.mask2, 1.0), writes=[bc])
        P.pool(lambda e: e.affine_select(out=self.mask2, in_=self.mask2, pattern=[[1, 128]],
                                         compare_op=ALU.is_ge, fill=0.0, base=0, channel_multiplier=-1),
               reads=[bc], writes=[bc])
        P.pool(lambda e: e.memset(self.mask2[0:64, 64:128], 0.0), reads=[bc], writes=[bc])
        self.xt = [A.alloc((D,), F32) for _ in range(2)]
        self.bxt = [Buf("xt%d" % i) for i in range(2)]
        self.junk = A.alloc((D,), BF16)
        self.bjunk = Buf("junk")
        self.ss = A.alloc((NT,), F32)
        self.rstd = A.alloc((NT,), F32)
        self.bss = Buf("ss")
        self.brstd = Buf("rstd")
        self.xsb = [A.alloc((D,), BF16) for _ in range(2)]
        self.bxsb = [Buf("xsb%d" % i) for i in range(2)]
        self.gbc = A.alloc((D,), F32)
        self.bgbc = Buf("gbc")
        self.nxt = 0
        self.d["x_buf"] = nc.dram_tensor("x_buf", [NSLOT, D], BF16, kind="Internal").ap()
        zt = A.alloc((D,), BF16)
        self.bxzero = Buf("xzero")
        P.pool(lambda e: e.memset(zt, 0.0), writes=[bc])
        xbv = self.d["x_buf"].rearrange("(b p) n -> p b n", p=128)
        nblk = NSLOT // 128
        step = 12
        self.bxz = []
        for b0 in range(0, nblk, step):
            bz = Buf("xz")
            self.bxz.append(bz)
            P.dma(lambda e, b0=b0: e.dma_start(out=xbv[:, b0:b0 + step, :],
                                               in_=zt.unsqueeze(1).to_broadcast([128, step, D])),
                  reads=[bc], writes=[bz])

    def stage_a(self, *a, **k):
        for _ in self.stage_a_gen(*a, **k):
            pass

    def stage_a_gen(self, sb, xnT, bxnT, ptr, bptr, gname="norm_mix_g", src="xs", tok_base=0, keep=None):
        nc, P = self.nc, self.P
        xs_d = self.d[src]
        tiles = []
        for t in range(NT):
            i = self.nxt % 2
            self.nxt += 1
            tok0 = tok_base + sb * SBT + t * 128
            xt, bxt = self.xt[i], self.bxt[i]
            P.dma(lambda e, xt=xt, tok0=tok0: e.dma_start(out=xt, in_=xs_d[tok0:tok0 + 128, :]), writes=[bxt])
            P.act(lambda e, xt=xt, t=t: e.activation(out=self.junk, in_=xt, func=AF.Square,
                                                     accum_out=self.ss[:, t:t + 1]),
                  reads=[bxt], writes=[self.bss])
            tiles.append((xt, bxt, i))
            if t % 2 == 1:
                t0 = t - 1
                P.act(lambda e, t0=t0: e.activation(out=self.rstd[:, t0:t0 + 2], in_=self.ss[:, t0:t0 + 2],
                                                    func=AF.Ln, scale=1.0 / D, bias=EPS),
                      reads=[self.bss], writes=[self.brstd])
                P.act(lambda e, t0=t0: e.activation(out=self.rstd[:, t0:t0 + 2], in_=self.rstd[:, t0:t0 + 2],
                                                    func=AF.Exp, scale=-0.5),
                      reads=[self.brstd], writes=[self.brstd])
                for tt in (t0, t):
                    xt2, bxt2, i2 = tiles[tt]
                    xsb, bxsb = self.xsb[i2], self.bxsb[i2]
                    P.dve(lambda e, xt2=xt2, xsb=xsb, tt=tt: e.scalar_tensor_tensor(
                        out=xsb, in0=xt2, scalar=self.rstd[:, tt:tt + 1], in1=self.gbc,
                        op0=ALU.mult, op1=ALU.mult),
                        reads=[bxt2, self.brstd, self.bgbc], writes=[bxsb])
                    for k in range(KC):
                        P.pe(lambda e, k=k, xsb=xsb: e.transpose(out=ptr[:, k, :], in_=xsb[:, k * 128:(k + 1) * 128],
                                                                 identity=self.ident),
                             reads=[bxsb, self.bconst], writes=[bptr])
                    P.dve(lambda e, tt=tt: e.tensor_copy(out=xnT[:, :, tt * 128:(tt + 1) * 128], in_=ptr),
                          reads=[bptr], writes=[bxnT])
                    yield


    def xnt_fetch_gen(self, sb, last_sb, xnTs, bxnTs):
        P = self.P
        src = self.d["xnT_s"]
        if sb not in self._xfetched:
            self._xfetched.add(sb)
            P.dma(lambda e, sb=sb: e.dma_start(out=xnTs[sb % 2], in_=src[:, :, sb * SBT:(sb + 1) * SBT]),
                  reads=[self.bxns[sb]], writes=[bxnTs[sb % 2]])
        nx = sb + 1
        if nx <= last_sb and nx not in self._xfetched:
            self._xfetched.add(nx)
            P.dma(lambda e, nx=nx: e.dma_start(out=xnTs[nx % 2], in_=src[:, :, nx * SBT:(nx + 1) * SBT]),
                  reads=[self.bxns[nx]], writes=[bxnTs[nx % 2]])
        yield

    def load_gain(self, gname):
        P = self.P
        g = self.d[gname]
        P.dma(lambda e: e.dma_start(out=self.gbc, in_=g.partition_broadcast(128)), writes=[self.bgbc])

    def pass1(self):
        nc, P, A = self.nc, self.P, self.A
        d = self.d
        H = 4
        self.W2 = A.alloc((KC, 2056), BF16)
        self.bW2 = [Buf("W2_%d" % k) for k in range(KC)]
        m0 = A.mark()
        self.OA = A.alloc((H, SBT), BF16)
        self.bOA = [Buf("OA%d" % h) for h in range(H)]
        W1 = A.alloc((KC, 2048), BF16)
        bW1 = [Buf("W1_%d" % k) for k in range(KC)]
        wv = d["w_in"].rearrange("(k p) n -> p k n", p=128)
        for k in range(KC):
            P.dma(lambda e, k=k: e.dma_start(out=W1[:, k, :], in_=wv[:, k, 0:2048]), writes=[bW1[k]], q="pool")
        for k in range(KC):
            P.dma(lambda e, k=k: e.dma_start(out=self.W2[:, k, :], in_=wv[:, k, 2048:2048 + 2056]), writes=[self.bW2[k]], q="pool")
        self.load_gain("norm_mix_g")
        lraw = A.alloc((2, H), F32)
        lb = A.alloc((H,), F32)
        oml = A.alloc((H,), F32)
        hgn = A.alloc((1,), F32)
        blb = Buf("lb")
        P.dma(lambda e: e.dma_start(out=lraw, in_=d["hg_lb"]), writes=[blb])
        P.dma(lambda e: e.dma_start(out=hgn, in_=d["hg_norm_g"].rearrange("(p o) -> p o", o=1)), writes=[blb])
        P.dve(lambda e: e.tensor_tensor(out=lb, in0=lraw[:, 0, :], in1=lraw[:, 1, :], op=ALU.subtract),
              reads=[blb], writes=[blb])
        P.act(lambda e: e.activation(out=oml, in_=lb, func=AF.Sigmoid, scale=-1.0), reads=[blb], writes=[blb])
        P.act(lambda e: e.activation(out=lb, in_=lb, func=AF.Sigmoid), reads=[blb], writes=[blb])

        xnT = A.alloc((KC, SBT), BF16)
        bxnT = Buf("xnT")
        Fb = A.alloc((H, SBT), F32)
        CS = A.alloc((H, SBT), F32)
        Kb = A.alloc((H, SBT), BF16)
        EB = A.alloc((H, SBT), BF16)
        ENB = A.alloc((H, SBT), BF16)
        EBEs = [A.alloc((H, NCH), F32) for _ in range(2)]
        QTs = [A.alloc((H, SBT), BF16) for _ in range(2)]
        KTs = [A.alloc((H, SBT), BF16) for _ in range(2)]
        KH = A.alloc((H, SBT), BF16)
        KHTs = [A.alloc((NT, 512), BF16) for _ in range(2)]
        Vs = [A.alloc((NT, 512), BF16) for _ in range(2)]
        Gs = [A.alloc((H, SBT), BF16) for _ in range(2)]
        O32 = A.alloc((H, SBT), F32)
        OSQ = A.alloc((H, SBT), BF16)
        LNV = A.alloc((SBT,), F32)
        ATS = A.alloc((H, 128), BF16)
        S32 = A.alloc((H, 128), F32)
        SBF = [A.alloc((H, 128), BF16) for _ in range(2)]
        bF = [Buf() for _ in range(H)]
        bCS = [Buf() for _ in range(H)]
        bK = Buf()
        bEB = [Buf() for _ in range(H)]
        bENB = [Buf() for _ in range(H)]
        bEBEs = [Buf(), Buf()]
        bQTs = [[Buf() for _ in range(H)] for _ in range(2)]
        bKTs = [Buf(), Buf()]
        bKH = Buf()
        bKHTs = [[Buf() for _ in range(NT)] for _ in range(2)]
        bVs = [[Buf() for _ in range(NT)] for _ in range(2)]
        bGs = [[Buf() for _ in range(H)] for _ in range(2)]
        bO32 = Buf()
        bOSQ = [Buf() for _ in range(H)]
        bLNV = Buf()
        bATS = Buf()
        bS32 = [Buf() for _ in range(H)]
        bSBF = [[Buf() for _ in range(H)] for _ in range(2)]
        sbf_i = [0] * H

        bk = self.banks
        bb = self.bbank
        ptr = bk[0][:].bitcast(BF16).rearrange("p (k t) -> p k t", k=KC)
        pkt = bk[1][:].bitcast(BF16)[:, 0:512]
        pj = [bk[2][:], bk[3][:]]
        bpj = [bb[2], bb[3]]
        pat = bk[4][:].rearrange("p (h t) -> p h t", h=H)
        po = bk[5][:].rearrange("p (h t) -> p h t", h=H)
        pS = [bk[6 + (h % 2)][:, 0:128] for h in range(H)]
        bpS = [bb[6 + (h % 2)] for h in range(H)]
        pji = [0]

        def nextpj():
            i = pji[0] % 2
            pji[0] += 1
            return pj[i], bpj[i]

        for h in range(H):
            P.pool(lambda e, h=h: e.memset(S32[:, h, :], 0.0), writes=[bS32[h]])
            P.pool(lambda e, h=h: e.memset(SBF[0][:, h, :], 0.0), writes=[bSBF[0][h]])

        def proj_fm(col0, h):
            p, bp = nextpj()
            for k in range(KC):
                P.pe(lambda e, k=k, p=p: e.matmul(p[:, 0:SBT], lhsT=W1[:, k, col0 + h * 128:col0 + (h + 1) * 128],
                                                  rhs=xnT[:, k, :], start=(k == 0), stop=(k == KC - 1)),
                     reads=[bW1[k], bxnT], writes=[bp])
            return p, bp

        def X1(sb):
            sl = sb % 2
            QT, KT, KHT, V, G, EBE = QTs[sl], KTs[sl], KHTs[sl], Vs[sl], Gs[sl], EBEs[sl]
            bQT, bKT, bKHT, bV, bG, bEBE = bQTs[sl], bKTs[sl], bKHTs[sl], bVs[sl], bGs[sl], bEBEs[sl]
            full = sb >= self.npre
            yield from self.stage_a_gen(sb, xnT, bxnT, ptr, bb[0])
            if "xnT_s" not in self.d:
                self.d["xnT_s"] = self.nc.dram_tensor("xnT_s", [128, KC, self.ntok], BF16, kind="Internal").ap()
                self.bxns = {}
            bx_ = Buf("xns")
            self.bxns[sb] = bx_
            P.dma(lambda e, sb=sb: e.dma_start(out=self.d["xnT_s"][:, :, sb * SBT:(sb + 1) * SBT], in_=xnT),
                  reads=[bxnT], writes=[bx_])
            for h in range(H):
                p, bp = proj_fm(C_HF, h)
                P.act(lambda e, h=h, p=p: e.activation(out=Fb[:, h, :], in_=p[:, 0:SBT], func=AF.Sigmoid),
                      reads=[bp], writes=[bF[h]])
                yield
            if full:
                for h in range(H):
                    p, bp = proj_fm(C_HQ, h)
                    P.act(lambda e, h=h, p=p: e.activation(out=QT[:, h, :], in_=p[:, 0:SBT], func=AF.Silu),
                          reads=[bp], writes=[bQT[h]])
                    yield
                for h in range(H):
                    p, bp = proj_fm(C_HG, h)
                    P.act(lambda e, h=h, p=p: e.activation(out=G[:, h, :], in_=p[:, 0:SBT], func=AF.Silu),
                          reads=[bp], writes=[bG[h]])
                    yield
            for h in range(H):
                P.dve(lambda e, h=h: e.tensor_scalar(out=Fb[:, h, :], in0=Fb[:, h, :], scalar1=oml[:, h:h + 1],
                                                     scalar2=lb[:, h:h + 1], op0=ALU.mult, op1=ALU.add),
                      reads=[bF[h], blb], writes=[bF[h]])
            P.dve(lambda e: e.tensor_scalar(out=Kb, in0=Fb, scalar1=-1.0, scalar2=1.0, op0=ALU.mult, op1=ALU.add),
                  reads=bF, writes=[bK])
            for h in range(H):
                P.act(lambda e, h=h: e.activation(out=Fb[:, h, :], in_=Fb[:, h, :], func=AF.Ln),
                      reads=[bF[h], bK], writes=[bF[h]])
            for h in range(H):
                P.dve(lambda e, h=h: e.tensor_tensor_scan(out=CS[:, h, :], data0=self.ones_f[:, 0:SBT],
                                                          data1=Fb[:, h, :], initial=0.0,
                                                          op0=ALU.mult, op1=ALU.add),
                      reads=[bF[h], self.bconst], writes=[bCS[h]])
                yield
            if not full:
                for h in range(H):
                    P.act(lambda e, h=h: e.activation(out=ENB[:, h, :], in_=CS[:, h, :], func=AF.Exp, scale=-1.0,
                                                      bias=CS[:, h, SBT - 1:SBT]),
                          reads=[bCS[h]], writes=[bENB[h]])
                    yield
                P.act(lambda e: e.activation(out=EBE[:, :, 0], in_=CS[:, :, SBT - 1], func=AF.Exp), reads=bCS, writes=[bEBE])
                P.dve(lambda e: e.tensor_tensor(out=KH, in0=Kb, in1=ENB, op=ALU.mult), reads=[bK] + bENB, writes=[bKH])
            else:
                Fb4 = Fb.rearrange("p h (c t) -> p h c t", c=NCH)
                CS4 = CS.rearrange("p h (c t) -> p h c t", c=NCH)
                P.dve(lambda e: e.tensor_tensor(out=Fb4[:, :, 1:NCH, :], in0=CS4[:, :, 1:NCH, :],
                                                in1=CS4[:, :, 0:NCH - 1, 63:64].to_broadcast([128, H, NCH - 1, 64]),
                                                op=ALU.subtract),
                      reads=bCS + bF, writes=bF)
                P.dve(lambda e: e.tensor_copy(out=Fb4[:, :, 0, :], in_=CS4[:, :, 0, :]), reads=bCS + bF, writes=bF)
                for h in range(H):
                    P.act(lambda e, h=h: e.activation(out=ENB[:, h, :], in_=Fb[:, h, :], func=AF.Exp, scale=-1.0),
                          reads=[bF[h]], writes=[bENB[h]])
                    yield
                P.act(lambda e: e.activation(out=EBE, in_=Fb4[:, :, :, 63], func=AF.Exp), reads=bF, writes=[bEBE])
                if full:
                    for h in range(H):
                        P.act(lambda e, h=h: e.activation(out=EB[:, h, :], in_=Fb[:, h, :], func=AF.Exp),
                              reads=[bF[h]], writes=[bEB[h]])
                P.dve(lambda e: e.tensor_tensor(out=KT, in0=Kb, in1=ENB, op=ALU.mult), reads=[bK] + bENB, writes=[bKT])
                KT4 = KT.rearrange("p h (c t) -> p h c t", c=NCH)
                KH4 = KH.rearrange("p h (c t) -> p h c t", c=NCH)
                P.dve(lambda e: e.tensor_tensor(out=KH4, in0=KT4,
                                                in1=EBE.unsqueeze(3).to_broadcast([128, H, NCH, 64]), op=ALU.mult),
                      reads=[bKT, bEBE], writes=[bKH])
                if full:
                    P.dve(lambda e: e.tensor_tensor(out=QT, in0=QT, in1=EB, op=ALU.mult), reads=bQT + bEB, writes=bQT)
            for t in range(NT):
                p, bp = nextpj()
                for k in range(KC):
                    P.pe(lambda e, k=k, p=p, t=t: e.matmul(p, lhsT=xnT[:, k, t * 128:(t + 1) * 128],
                                                           rhs=W1[:, k, C_HI:C_HI + 512],
                                                           start=(k == 0), stop=(k == KC - 1)),
                         reads=[bW1[k], bxnT], writes=[bp])
                P.act(lambda e, p=p, t=t: e.activation(out=V[:, t, :], in_=p, func=AF.Copy), reads=[bp], writes=[bV[t]])
                for h in range(H):
                    P.pe(lambda e, h=h, t=t: e.transpose(out=pkt[:, h * 128:(h + 1) * 128],
                                                         in_=KH[:, h, t * 128:(t + 1) * 128], identity=self.ident),
                         reads=[bKH, self.bconst], writes=[bb[1]])
                P.dve(lambda e, t=t: e.tensor_copy(out=KHT[:, t, :], in_=pkt), reads=[bb[1]], writes=[bKHT[t]])
                yield
        def Y1(sb):
            full = sb >= self.npre
            sl = sb % 2
            QT, KT, KHT, V, G, EBE = QTs[sl], KTs[sl], KHTs[sl], Vs[sl], Gs[sl], EBEs[sl]
            bQT, bKT, bKHT, bV, bG, bEBE = bQTs[sl], bKTs[sl], bKHTs[sl], bVs[sl], bGs[sl], bEBEs[sl]
            if not full:
                for h in range(H):
                    for t in range(NT):
                        P.pe(lambda e, h=h, t=t: e.matmul(pS[h], lhsT=KHT[:, t, h * 128:(h + 1) * 128],
                                                          rhs=V[:, t, h * 128:(h + 1) * 128],
                                                          start=(t == 0), stop=(t == NT - 1)),
                             reads=[bKHT[t], bV[t]], writes=[bpS[h]])
                    P.dve(lambda e, h=h: e.scalar_tensor_tensor(
                        out=S32[:, h, :], in0=S32[:, h, :], scalar=EBE[:, h, 0:1], in1=pS[h],
                        op0=ALU.mult, op1=ALU.add),
                        reads=[bS32[h], bEBE, bpS[h]], writes=[bS32[h]])
                    cur = sbf_i[h]
                    nxt = 1 - cur
                    P.act(lambda e, h=h, nxt=nxt: e.activation(out=SBF[nxt][:, h, :], in_=S32[:, h, :], func=AF.Copy),
                          reads=[bS32[h]], writes=[bSBF[nxt][h]])
                    sbf_i[h] = nxt
                    yield
                return
            for j in range(NT):
                c0 = j * 128
                if full:
                    for h in range(H):
                        P.pe(lambda e, h=h, c0=c0: e.matmul(pat[:, h, :], lhsT=KT[:, h, c0:c0 + 128],
                                                            rhs=QT[:, h, c0:c0 + 128], start=True, stop=True),
                             reads=[bKT, bQT[h]], writes=[bb[4]])
                    P.dve(lambda e: e.tensor_tensor(out=ATS, in0=pat,
                                                    in1=self.mask2.unsqueeze(1).to_broadcast([128, H, 128]),
                                                    op=ALU.mult),
                          reads=[bb[4], self.bconst], writes=[bATS])
                    yield
                for half in range(2):
                    ch = 2 * j + half
                    r0 = half * 64
                    for h in range(H):
                        cur = sbf_i[h]
                        if full:
                            P.pe(lambda e, h=h, cur=cur, c0=c0, r0=r0: e.matmul(
                                po[:, h, r0:r0 + 64], lhsT=SBF[cur][:, h, :], rhs=QT[:, h, c0 + r0:c0 + r0 + 64],
                                start=(h == 0 and r0 == 0), stop=False, skip_group_check=True),
                                reads=[bSBF[cur][h], bQT[h]], writes=[bb[5]])
                        P.pe(lambda e, h=h, j=j, r0=r0: e.matmul(
                            pS[h], lhsT=KHT[r0:r0 + 64, j, h * 128:(h + 1) * 128],
                            rhs=V[r0:r0 + 64, j, h * 128:(h + 1) * 128], start=True, stop=True),
                            reads=[bKHT[j], bV[j]], writes=[bpS[h]])
                        P.dve(lambda e, h=h, ch=ch: e.scalar_tensor_tensor(
                            out=S32[:, h, :], in0=S32[:, h, :], scalar=EBE[:, h, ch:ch + 1], in1=pS[h],
                            op0=ALU.mult, op1=ALU.add),
                            reads=[bS32[h], bEBE, bpS[h]], writes=[bS32[h]])
                        nxt = 1 - cur
                        P.act(lambda e, h=h, nxt=nxt: e.activation(out=SBF[nxt][:, h, :], in_=S32[:, h, :], func=AF.Copy),
                              reads=[bS32[h]], writes=[bSBF[nxt][h]])
                        sbf_i[h] = nxt
                        yield
                if full:
                    for h in range(H):
                        P.pe(lambda e, h=h, j=j: e.matmul(po[:, h, :], lhsT=V[:, j, h * 128:(h + 1) * 128],
                                                          rhs=ATS[:, h, :], start=False, stop=True,
                                                          skip_group_check=True),
                             reads=[bV[j], bATS], writes=[bb[5]])
                    P.act(lambda e, c0=c0: e.activation(out=O32[:, :, c0:c0 + 128], in_=po, func=AF.Copy),
                          reads=[bb[5]], writes=[bO32])
                    yield
            if full:
                tok0 = (sb - self.npre) * SBT
                for h in range(H):
                    P.act(lambda e, h=h: e.activation(out=OSQ[:, h, :], in_=O32[:, h, :], func=AF.Square),
                          reads=[bO32], writes=[bOSQ[h]])
                for h in range(H):
                    pss, bpss = bk[4][:], bb[4]
                    P.pe(lambda e, h=h, pss=pss: e.matmul(pss[:, 0:SBT], lhsT=self.ones_bf, rhs=OSQ[:, h, :], start=True, stop=True),
                         reads=[bOSQ[h], self.bconst], writes=[bpss])
                    P.act(lambda e, pss=pss: e.activation(out=LNV, in_=pss[:, 0:SBT], func=AF.Ln, scale=1.0 / 128, bias=EPS),
                          reads=[bpss], writes=[bLNV])
                    P.act(lambda e: e.activation(out=LNV, in_=LNV, func=AF.Exp, scale=-0.5),
                          reads=[bLNV], writes=[bLNV])
                    P.dve(lambda e, h=h: e.tensor_tensor(out=O32[:, h, :], in0=O32[:, h, :], in1=LNV, op=ALU.mult),
                          reads=[bO32, bLNV], writes=[bO32])
                    P.dve(lambda e, h=h: e.scalar_tensor_tensor(
                        out=self.OA[:, h, :], in0=O32[:, h, :], scalar=hgn[:, 0:1], in1=G[:, h, :],
                        op0=ALU.mult, op1=ALU.mult),
                        reads=[bO32, blb, bG[h]], writes=[self.bOA[h]])
                    yield
                self.spill("oa_s", self.OA, self.bOA, sb)
                if "oa" in self.debug:
                    if "dbg_oa" not in self.d:
                        self.dout("dbg_oa", [128, H, self.nfull_tok], BF16)
                    P.dma(lambda e, tok0=tok0: e.dma_start(out=self.d["dbg_oa"][:, :, tok0:tok0 + SBT], in_=self.OA),
                          reads=self.bOA, out=True)
        import os
        WTS1 = [int(v) for v in os.environ.get("IL_W1", "1,1").split(",")]

        def run_il(gens):
            gens = list(gens)
            while gens:
                for g_, w_ in list(gens):
                    for _ in range(w_):
                        try:
                            next(g_)
                        except StopIteration:
                            gens.remove((g_, w_))
                            break

        n = self.nsb
        for r in range(-1, n):
            gs = []
            if 0 <= r < n:
                gs.append((Y1(r), WTS1[0]))
            if 0 <= r + 1 < n:
                gs.append((X1(r + 1), WTS1[1]))
            run_il(gs)
        A.reset(m0)
        P.barrier()

    def finish(self):
        self.P.finalize(self.nc)
        self.st.close()
        return self.nc


def _pass2(self):
    nc, P, A = self.nc, self.P, self.A
    d = self.d
    H = 4
    self.din("conv_wT", [128, 4, 12])
    self.din("gd_A_log", [4])
    self.din("gd_dt_bias", [4])
    self.din("gd_norm_g", [128])
    m0 = A.mark()
    self.OB = A.alloc((H, SBT), BF16)
    self.bOB = [Buf("OB%d" % h) for h in range(H)]
    NW = 2056
    if hasattr(self, "W2"):
        W2, bW2 = self.W2, self.bW2
    else:
        W2 = A.alloc((KC, NW), BF16)
        bW2 = [Buf("W2_%d" % k) for k in range(KC)]
        wv = d["w_in"].rearrange("(k p) n -> p k n", p=128)
        for k in range(KC):
            P.dma(lambda e, k=k: e.dma_start(out=W2[:, k, :], in_=wv[:, k, 2048:2048 + NW]), writes=[bW2[k]], q="pool")
    self.load_gain("norm_mix_g")
    cw = A.alloc((4, 12), F32)
    negA = A.alloc((H,), F32)
    dtb = A.alloc((H,), F32)
    gdn = A.alloc((1,), F32)
    bpar = Buf("par2")
    P.dma(lambda e: e.dma_start(out=cw, in_=d["conv_wT"]), writes=[bpar])
    P.dma(lambda e: e.dma_start(out=negA, in_=d["gd_A_log"].partition_broadcast(128)), writes=[bpar])
    P.dma(lambda e: e.dma_start(out=dtb, in_=d["gd_dt_bias"].partition_broadcast(128)), writes=[bpar])
    P.dma(lambda e: e.dma_start(out=gdn, in_=d["gd_norm_g"].rearrange("(p o) -> p o", o=1)), writes=[bpar])
    P.act(lambda e: e.activation(out=negA, in_=negA, func=AF.Exp), reads=[bpar], writes=[bpar])
    P.dve(lambda e: e.tensor_scalar(out=negA, in0=negA, scalar1=-1.0, scalar2=None, op0=ALU.mult),
          reads=[bpar], writes=[bpar])
    maskL = A.alloc((128,), F32)
    ch01 = A.alloc((2, 128), F32)
    bc = self.bconst
    P.pool(lambda e: e.memset(maskL, 1.0), writes=[bc])
    P.pool(lambda e: e.affine_select(out=maskL, in_=maskL, pattern=[[-1, 128]], compare_op=ALU.is_gt,
                                     fill=0.0, base=0, channel_multiplier=1), reads=[bc], writes=[bc])
    P.pool(lambda e: e.memset(maskL[64:128, 0:64], 0.0), reads=[bc], writes=[bc])
    bones = A.alloc((128,), F32)
    P.pool(lambda e: e.memset(bones, 0.0), writes=[bc])
    P.pool(lambda e: e.memset(bones[0:64, 0:64], 1.0), reads=[bc], writes=[bc])
    P.pool(lambda e: e.memset(bones[64:128, 64:128], 1.0), reads=[bc], writes=[bc])
    P.pool(lambda e: e.memset(ch01, 0.0), writes=[bc])
    P.pool(lambda e: e.memset(ch01[0:64, 0, :], 1.0), reads=[bc], writes=[bc])
    P.pool(lambda e: e.memset(ch01[64:128, 1, :], 1.0), reads=[bc], writes=[bc])

    xnTs = [A.alloc((KC, SBT), BF16) for _ in range(2)]
    bxnTs = [Buf("xnT0"), Buf("xnT1")]
    self._xfetched = set()
    use_fetch = hasattr(self, "bxns")
    XC = A.alloc((12, SBT + 3), BF16)
    DG = A.alloc((48, 128), BF16)
    bDG = Buf("DG")
    for _j in range(4):
        for _cb in range(12):
            P.dve(lambda e, _j=_j, _cb=_cb: e.tensor_scalar(out=DG[:, _j * 12 + _cb, :], in0=self.identf,
                                                           scalar1=cw[:, _j, _cb:_cb + 1], scalar2=None, op0=ALU.mult),
                  reads=[bpar, self.bconst], writes=[bDG])
    bXC = [Buf() for _ in range(12)]
    CV = [A.alloc((SBT,), F32) for _ in range(2)]
    bCV = [Buf(), Buf()]
    QK32 = A.alloc((8, SBT), F32)
    bQK32 = [Buf() for _ in range(8)]
    SQ = A.alloc((SBT,), BF16)
    bSQ = Buf()
    RS = A.alloc((SBT,), F32)
    bRS = Buf()
    NS = 3
    QTs = [A.alloc((H, SBT), BF16) for _ in range(NS)]
    KTs = [A.alloc((H, SBT), BF16) for _ in range(NS)]
    VTs = [A.alloc((H, SBT), BF16) for _ in range(NS)]
    GZs = [A.alloc((H, SBT), BF16) for _ in range(NS)]
    bQTs = [[Buf() for _ in range(H)] for _ in range(NS)]
    bKTs = [[Buf() for _ in range(H)] for _ in range(NS)]
    bVTs = [[Buf() for _ in range(H)] for _ in range(NS)]
    bGZs = [[Buf() for _ in range(H)] for _ in range(NS)]
    BGraws = [A.alloc((NT, 8), F32) for _ in range(NS)]
    LNBs = [A.alloc((NT, H), F32) for _ in range(NS)]
    BETAs = [A.alloc((NT, H), F32) for _ in range(NS)]
    GGs = [A.alloc((NT, H), F32) for _ in range(NS)]
    bBGs = [Buf() for _ in range(NS)]
    PBUF = []
    for _j in range(NT):
        pb = dict(
            GB=A.alloc((H, 128), F32), bGB=Buf(),
            E1=A.alloc((H, 128), F32), bE1=Buf(),
            E2=A.alloc((H, 128), F32), bE2=Buf(),
            EGR=A.alloc((H, 128), BF16), bEGR=Buf(),
            Lm=[A.alloc((H, 128), BF16) for _ in range(2)], bLm=[Buf(), Buf()],
            Um=[A.alloc((H, 128), BF16) for _ in range(2)], bUm=[Buf(), Buf()],
            Xm=[A.alloc((H, 128), BF16) for _ in range(2)], bXm=[Buf(), Buf()],
            VTK=A.alloc((H, 128), BF16), bVTK=Buf(),
        )
        PBUF.append(pb)
    PCAR = []
    for _s in range(2):
        row = []
        for _j in range(NT):
            row.append(dict(
                SC=A.alloc((8, H), F32), bSC=Buf(),
                QTG=A.alloc((H, 128), BF16), bQTG=Buf(),
                TT=A.alloc((H, 128), BF16), bTT=Buf(),
                ATT=A.alloc((H, 128), BF16), bATT=Buf(),
                KHT=A.alloc((H, 128), BF16), bKHT=Buf(),
                KTP=A.alloc((H, 128), BF16), bKTP=Buf(),
                BV=A.alloc((H, 128), F32), bBV=Buf(),
            ))
        PCAR.append(row)
    R = A.alloc((H, 128), BF16)
    USB = A.alloc((H, 128), BF16)
    bR = Buf()
    bUSB = Buf()
    S32 = A.alloc((H, 128), F32)
    SBF = [A.alloc((H, 128), BF16) for _ in range(2)]
    bS32 = [Buf() for _ in range(H)]
    bSBF = [[Buf() for _ in range(H)] for _ in range(2)]
    sbf_i = [0]
    O32 = A.alloc((H, SBT), F32)
    bO32 = Buf()
    OSQ = A.alloc((H, SBT), BF16)
    bOSQ = [Buf() for _ in range(H)]
    LNV = A.alloc((SBT,), F32)
    bLNV = Buf()
    identb4 = A.alloc((H, 128), BF16)
    P.pool(lambda e: e.tensor_copy(out=identb4, in_=self.ident.unsqueeze(1).to_broadcast([128, H, 128])),
           reads=[bc], writes=[bc])

    bk, bb = self.banks, self.bbank
    ptr = bk[0][:].bitcast(BF16).rearrange("p (k t) -> p k t", k=KC)
    ptr4 = bk[0][:].bitcast(BF16)[:, 0:512].rearrange("p (h t) -> p h t", h=H)
    import os as _os
    DUM = int(_os.environ.get("PE_DUMMY", "0"))
    DUMN = int(_os.environ.get("PE_DUMMY_N", "128"))
    if DUM > 0:
        pj = [bk[1][:], bk[2][:]]
        bpj = [bb[1], bb[2]]
        dones = A.alloc((512,), BF16)
        P.pool(lambda e: e.memset(dones, 1.0), writes=[self.bconst])
        dbuf = Buf("dummy", psum=True)
        P.pe_dummy = (lambda e: e.matmul(bk[0][:, 0:DUMN], lhsT=self.ones_bf, rhs=dones[:, 0:DUMN], start=True, stop=True),
                      DUM, dbuf)
    else:
        pj = [bk[1][:], bk[2][:], bk[0][:]]
        bpj = [bb[1], bb[2], bb[0]]
    pA = bk[3][:].rearrange("p (h t) -> p h t", h=H)
    pB = bk[4][:].rearrange("p (h t) -> p h t", h=H)
    pBb = bk[4][:].bitcast(BF16)[:, 0:512].rearrange("p (h t) -> p h t", h=H)
    pC = bk[5][:].rearrange("p (h t) -> p h t", h=H)
    pR = bk[6][:].rearrange("p (h t) -> p h t", h=H)
    po = bk[7][:].rearrange("p (h t) -> p h t", h=H)
    pji = [0]

    def nextpj():
        i = pji[0] % len(pj)
        pji[0] += 1
        return pj[i], bpj[i]

    for h in range(H):
        P.pool(lambda e, h=h: e.memset(S32[:, h, :], 0.0), writes=[bS32[h]])
        P.pool(lambda e, h=h: e.memset(SBF[0][:, h, :], 0.0), writes=[bSBF[0][h]])
    for cb in range(12):
        P.pool(lambda e, cb=cb: e.memset(XC[:, cb, :], 0.0), writes=[bXC[cb]])

    def proj_fm(col0, xnT, bxnT):
        p, bp = nextpj()
        for k in range(KC):
            P.pe(lambda e, k=k, p=p: e.matmul(p[:, 0:SBT], lhsT=W2[:, k, col0:col0 + 128],
                                              rhs=xnT[:, k, :], start=(k == 0), stop=(k == KC - 1)),
                 reads=[bW2[k], bxnT], writes=[bp])
        return p, bp

    evi = [0]

    def evac(out, in_, reads, writes):
        i = evi[0]
        evi[0] += 1
        if i % 2 == 0:
            P.act(lambda e: e.activation(out=out, in_=in_, func=AF.Copy), reads=reads, writes=writes)
        else:
            P.dve(lambda e: e.tensor_copy(out=out, in_=in_), reads=reads, writes=writes)

    def X(sb):
        sl = sb % 3
        QT, KT, VT, GZ = QTs[sl], KTs[sl], VTs[sl], GZs[sl]
        bQT, bKT, bVT, bGZ = bQTs[sl], bKTs[sl], bVTs[sl], bGZs[sl]
        BGraw, LNB, BETA, GG, bBG = BGraws[sl], LNBs[sl], BETAs[sl], GGs[sl], bBGs[sl]
        full = sb >= self.npre
        xnT, bxnT = xnTs[sb % 2], bxnTs[sb % 2]
        if use_fetch:
            yield from self.xnt_fetch_gen(sb, self.nsb - 1, xnTs, bxnTs)
        else:
            yield from self.stage_a_gen(sb, xnT, bxnT, ptr, bb[0])
        cbs = list(range(12)) if full else list(range(4, 12))
        cbs_proj = list(range(12)) if sb >= self.npre - 1 else list(range(4, 12))
        for cb in cbs_proj:
            if sb > 0:
                P.pool(lambda e, cb=cb: e.tensor_copy(out=XC[:, cb, 0:3], in_=XC[:, cb, SBT:SBT + 3]),
                       reads=[bXC[cb]], writes=[bXC[cb]])
            p, bp = proj_fm(cb * 128, xnT, bxnT)
            evac(XC[:, cb, 3:SBT + 3], p[:, 0:SBT], [bp], [bXC[cb]])
            yield
        pbg, bpbg = nextpj()
        for t in range(NT):
            for k in range(KC):
                P.pe(lambda e, k=k, t=t, pbg=pbg: e.matmul(pbg[:, t * 8:(t + 1) * 8], lhsT=xnT[:, k, t * 128:(t + 1) * 128],
                                                  rhs=W2[:, k, 2048:2056], start=(k == 0), stop=(k == KC - 1)),
                     reads=[bW2[k], bxnT], writes=[bpbg])
        P.act(lambda e, pbg=pbg: e.activation(out=BGraw, in_=pbg[:, 0:NT * 8].rearrange("p (t c) -> p t c", t=NT), func=AF.Copy),
              reads=[bpbg], writes=[bBG])
        P.act(lambda e: e.activation(out=LNB, in_=BGraw[:, :, 0:4], func=AF.Exp, scale=-1.0), reads=[bBG], writes=[bBG])
        P.act(lambda e: e.activation(out=LNB, in_=LNB, func=AF.Ln, bias=1.0), reads=[bBG], writes=[bBG])
        P.dve(lambda e: e.tensor_scalar(out=LNB, in0=LNB, scalar1=-1.0, scalar2=None, op0=ALU.mult),
              reads=[bBG], writes=[bBG])
        P.act(lambda e: e.activation(out=BETA, in_=LNB, func=AF.Exp), reads=[bBG], writes=[bBG])
        P.dve(lambda e: e.tensor_tensor(out=GG, in0=BGraw[:, :, 4:8], in1=dtb.unsqueeze(1).to_broadcast([128, NT, H]),
                                        op=ALU.add), reads=[bBG, bpar], writes=[bBG])
        P.act(lambda e: e.activation(out=GG, in_=GG, func=AF.Exp), reads=[bBG], writes=[bBG])
        P.act(lambda e: e.activation(out=GG, in_=GG, func=AF.Ln, bias=1.0), reads=[bBG], writes=[bBG])
        P.dve(lambda e: e.tensor_tensor(out=GG, in0=GG, in1=negA.unsqueeze(1).to_broadcast([128, NT, H]),
                                        op=ALU.mult), reads=[bBG, bpar], writes=[bBG])
        yield
        if full:
            for h in range(H):
                p, bp = proj_fm(1536 + h * 128, xnT, bxnT)
                P.act(lambda e, h=h, p=p: e.activation(out=GZ[:, h, :], in_=p[:, 0:SBT], func=AF.Silu),
                      reads=[bp], writes=[bGZ[h]])
                yield
        for n, cb in enumerate(cbs):
            cv, bcv = nextpj()
            for j in range(4):
                P.pe(lambda e, cb=cb, cv=cv, j=j: e.matmul(cv[:, 0:SBT], lhsT=DG[:, j * 12 + cb, :], rhs=XC[:, cb, j:SBT + j],
                                                           start=(j == 0), stop=(j == 3)),
                     reads=[bXC[cb], bDG], writes=[bcv])
            if cb < 8:
                P.act(lambda e, cb=cb, cv=cv: e.activation(out=QK32[:, cb, :], in_=cv[:, 0:SBT], func=AF.Silu),
                      reads=[bcv], writes=[bQK32[cb]])
            else:
                P.act(lambda e, cb=cb, cv=cv: e.activation(out=VT[:, cb - 8, :], in_=cv[:, 0:SBT], func=AF.Silu),
                      reads=[bcv], writes=[bVT[cb - 8]])
            yield
        for cb in cbs:
            if cb >= 8:
                continue
            P.pool(lambda e, cb=cb: e.tensor_tensor(out=SQ, in0=QK32[:, cb, :], in1=QK32[:, cb, :], op=ALU.mult),
                   reads=[bQK32[cb]], writes=[bSQ])
            p, bp = nextpj()
            P.pe(lambda e, p=p: e.matmul(p[:, 0:SBT], lhsT=self.ones_bf, rhs=SQ, start=True, stop=True),
                 reads=[bSQ, bc], writes=[bp])
            P.act(lambda e, p=p: e.activation(out=RS, in_=p[:, 0:SBT], func=AF.Ln, bias=EPS), reads=[bp], writes=[bRS])
            qbias = -0.5 * float(np.log(128.0)) if cb < 4 else 0.0
            P.act(lambda e, qbias=qbias: e.activation(out=RS, in_=RS, func=AF.Exp, scale=-0.5, bias=qbias),
                  reads=[bRS], writes=[bRS])
            dst, bdst = (QT[:, cb, :], bQT[cb]) if cb < 4 else (KT[:, cb - 4, :], bKT[cb - 4])
            P.pool(lambda e, cb=cb, dst=dst: e.tensor_tensor(out=dst, in0=QK32[:, cb, :], in1=RS, op=ALU.mult),
                   reads=[bQK32[cb], bRS], writes=[bdst])
            yield
    def Yp(sb):
        sl = sb % 3
        full = sb >= self.npre
        QT, KT, VT, GZ = QTs[sl], KTs[sl], VTs[sl], GZs[sl]
        bQT, bKT, bVT, bGZ = bQTs[sl], bKTs[sl], bVTs[sl], bGZs[sl]
        BGraw, LNB, BETA, GG, bBG = BGraws[sl], LNBs[sl], BETAs[sl], GGs[sl], bBGs[sl]
        LL = dict(L0)
        LL['PB'] = [dict(PBUF[j], **PCAR[sb % 2][j]) for j in range(NT)]
        LL.update(QT=QT, KT=KT, VT=VT, GZ=GZ, bQT=bQT, bKT=bKT, bVT=bVT, bGZ=bGZ, LNB=LNB, BETA=BETA, GG=GG, bBG=bBG)
        yield from self._gdn_prep(LL, full)
    def Yr(sb):
        sl = sb % 3
        full = sb >= self.npre
        QT, KT, VT, GZ = QTs[sl], KTs[sl], VTs[sl], GZs[sl]
        bQT, bKT, bVT, bGZ = bQTs[sl], bKTs[sl], bVTs[sl], bGZs[sl]
        BGraw, LNB, BETA, GG, bBG = BGraws[sl], LNBs[sl], BETAs[sl], GGs[sl], bBGs[sl]
        LL = dict(L0)
        LL['PB'] = [dict(PBUF[j], **PCAR[sb % 2][j]) for j in range(NT)]
        LL.update(QT=QT, KT=KT, VT=VT, GZ=GZ, bQT=bQT, bKT=bKT, bVT=bVT, bGZ=bGZ, LNB=LNB, BETA=BETA, GG=GG, bBG=bBG)
        yield from self._gdn_rec(LL, full)
        if full:
            tok0 = (sb - self.npre) * SBT
            for h in range(H):
                P.act(lambda e, h=h: e.activation(out=OSQ[:, h, :], in_=O32[:, h, :], func=AF.Square),
                      reads=[bO32], writes=[bOSQ[h]])
            for h in range(H):
                pss, bpss = bk[5][:], bb[5]
                P.pe(lambda e, h=h, pss=pss: e.matmul(pss[:, 0:SBT], lhsT=self.ones_bf, rhs=OSQ[:, h, :], start=True, stop=True),
                     reads=[bOSQ[h], bc], writes=[bpss])
                P.act(lambda e, pss=pss: e.activation(out=LNV, in_=pss[:, 0:SBT], func=AF.Ln, scale=1.0 / 128, bias=EPS),
                      reads=[bpss], writes=[bLNV])
                P.act(lambda e: e.activation(out=LNV, in_=LNV, func=AF.Exp, scale=-0.5), reads=[bLNV], writes=[bLNV])
                P.dve(lambda e, h=h: e.tensor_tensor(out=O32[:, h, :], in0=O32[:, h, :], in1=LNV, op=ALU.mult),
                      reads=[bO32, bLNV], writes=[bO32])
                P.dve(lambda e, h=h: e.scalar_tensor_tensor(
                    out=self.OB[:, h, :], in0=O32[:, h, :], scalar=gdn[:, 0:1], in1=GZ[:, h, :],
                    op0=ALU.mult, op1=ALU.mult), reads=[bO32, bpar, bGZ[h]], writes=[self.bOB[h]])
                yield
            self.spill("ob_s", self.OB, self.bOB, sb)
            if "ob" in self.debug:
                if "dbg_ob" not in self.d:
                    self.dout("dbg_ob", [128, H, self.nfull_tok], BF16)
                P.dma(lambda e, tok0=tok0: e.dma_start(out=self.d["dbg_ob"][:, :, tok0:tok0 + SBT], in_=self.OB),
                      reads=self.bOB, out=True)
    L0 = dict(locals())

    import os
    WTS = [int(v) for v in os.environ.get("IL_W", "1,2,2").split(",")]

    def run_il(gens):
        gens = list(gens)
        while gens:
            for g_, w_ in list(gens):
                for _ in range(w_):
                    try:
                        next(g_)
                    except StopIteration:
                        gens.remove((g_, w_))
                        break

    n = self.nsb
    for r in range(-2, n):
        gs = []
        if 0 <= r < n:
            gs.append((Yr(r), WTS[0]))
        if 0 <= r + 1 < n:
            gs.append((Yp(r + 1), WTS[1]))
        if 0 <= r + 2 < n:
            gs.append((X(r + 2), WTS[2]))
        run_il(gs)
    P.pe_dummy = None
    A.reset(m0)
    P.barrier()


KB.pass2 = _pass2


def _gdn_prep(self, L, full):
    P = self.P
    H = 4
    g = lambda n: L[n]
    bk, bb = self.banks, self.bbank
    bc = self.bconst
    mask2, ident = self.mask2, self.ident
    maskL, bones, ch01, identb4 = g("maskL"), g("bones"), g("ch01"), g("identb4")
    PBUF, GG, LNB, BETA, bBG = g("PB"), g("GG"), g("LNB"), g("BETA"), g("bBG")
    QT, KT, VT, bQT, bKT, bVT = g("QT"), g("KT"), g("VT"), g("bQT"), g("bKT"), g("bVT")
    R, USB, bR, bUSB = g("R"), g("USB"), g("bR"), g("bUSB")
    S32, SBF, bS32, bSBF, sbf_i = g("S32"), g("SBF"), g("bS32"), g("bSBF"), g("sbf_i")
    O32, bO32 = g("O32"), g("bO32")
    evac, nextpj = g("evac"), g("nextpj")
    NP = NT
    pP = [bk[3 + j][:].rearrange("p (h t) -> p h t", h=H) for j in range(NP)]
    pPb = [bk[3 + j][:].bitcast(BF16)[:, 0:512].rearrange("p (h t) -> p h t", h=H) for j in range(NP)]
    bpP = [bb[3 + j] for j in range(NP)]
    ptr4 = bk[5][:].bitcast(BF16)[:, 0:512].rearrange("p (h t) -> p h t", h=H)
    pKS = bk[5][:].rearrange("p (h t) -> p h t", h=H)
    pU = bk[6][:].rearrange("p (h t) -> p h t", h=H)
    po = bk[7][:].rearrange("p (h t) -> p h t", h=H)

    def bc4(ap):
        return ap.unsqueeze(2).to_broadcast([128, H, 128])

    for j in range(NP):
        pb = PBUF[j]
        SC, bSC = pb["SC"], pb["bSC"]
        ps = bk[3 + j][:, 0:16]
        gj = GG[:, j, :]
        for n, lhs in enumerate((mask2, bones, ch01[:, 0, :], ch01[:, 1, :])):
            P.pe(lambda e, n=n, lhs=lhs, ps=ps, gj=gj: e.matmul(ps[:, 4 * n:4 * n + 4], lhsT=lhs, rhs=gj,
                                                                 start=True, stop=True),
                 reads=[bBG, bc], writes=[bpP[j]])
        P.act(lambda e, SC=SC, ps=ps: e.activation(out=SC[:, 0, :], in_=ps[:, 0:4], func=AF.Copy),
              reads=[bpP[j]], writes=[bSC])
        P.dve(lambda e, SC=SC, ps=ps: e.tensor_tensor(out=SC[:, 5, :], in0=ps[:, 4:8], in1=SC[:, 0, :], op=ALU.subtract),
              reads=[bpP[j], bSC], writes=[bSC])
        P.act(lambda e, SC=SC, ps=ps: e.activation(out=SC[:, 6:8, :], in_=ps[:, 8:16].rearrange("p (a h) -> p a h", a=2),
                                                  func=AF.Exp), reads=[bpP[j]], writes=[bSC])
        P.dve(lambda e, SC=SC: e.tensor_scalar(out=SC[:, 1, :], in0=SC[:, 0, :], scalar1=-1.0, scalar2=None, op0=ALU.mult),
              reads=[bSC], writes=[bSC])
        P.dve(lambda e, SC=SC, j=j: e.tensor_tensor(out=SC[:, 2, :], in0=SC[:, 0, :], in1=LNB[:, j, :], op=ALU.add),
              reads=[bSC, bBG], writes=[bSC])
        P.act(lambda e, SC=SC: e.activation(out=SC[:, 3, :], in_=SC[:, 0, :], func=AF.Exp), reads=[bSC], writes=[bSC])
        P.act(lambda e, SC=SC: e.activation(out=SC[:, 5, :], in_=SC[:, 5, :], func=AF.Exp), reads=[bSC], writes=[bSC])
        P.dve(lambda e, SC=SC, j=j: e.scalar_tensor_tensor(out=SC[:, 4, :], in0=SC[:, 3, :], scalar=-1.0,
                                                          in1=BETA[:, j, :], op0=ALU.mult, op1=ALU.mult),
              reads=[bSC, bBG], writes=[bSC])
        yield
    for j in range(NP):
        pb = PBUF[j]
        P.pool(lambda e, pb=pb, j=j: e.tensor_copy(out=pb["GB"], in_=bc4(GG[:, j, :])), reads=[bBG], writes=[pb["bGB"]])
        yield
    for j in range(NP):
        pb = PBUF[j]
        for h in range(H):
            P.pe(lambda e, pb=pb, j=j, h=h: e.matmul(pP[j][:, h, :], lhsT=pb["GB"][:, h, :], rhs=mask2,
                                                     start=True, stop=True),
                 reads=[pb["bGB"], bc], writes=[bpP[j]])
        yield
    for j in range(NP):
        pb = PBUF[j]
        SC = pb["SC"]
        P.dve(lambda e, pb=pb, j=j, SC=SC: e.tensor_tensor(out=pb["E1"], in0=pP[j], in1=bc4(SC[:, 0, :]), op=ALU.max),
              reads=[bpP[j], pb["bSC"]], writes=[pb["bE1"]])
        if full:
            P.dve(lambda e, pb=pb, j=j, SC=SC: e.tensor_tensor(out=pb["E2"], in0=pP[j], in1=bc4(SC[:, 0, :]), op=ALU.min),
                  reads=[bpP[j], pb["bSC"]], writes=[pb["bE2"]])
            P.act(lambda e, pb=pb, j=j: e.activation(out=pb["EGR"], in_=pP[j], func=AF.Exp),
                  reads=[bpP[j]], writes=[pb["bEGR"]])
        yield
    for j in range(NP):
        pb = PBUF[j]
        SC = pb["SC"]
        for h in range(H):
            P.act(lambda e, pb=pb, h=h, SC=SC: e.activation(out=pb["E1"][:, h, :], in_=pb["E1"][:, h, :], func=AF.Exp,
                                                            scale=-1.0, bias=SC[:, 2, h:h + 1]),
                  reads=[pb["bE1"], pb["bSC"]], writes=[pb["bE1"]])
        if full:
            for h in range(H):
                P.act(lambda e, pb=pb, h=h, SC=SC: e.activation(out=pb["E2"][:, h, :], in_=pb["E2"][:, h, :], func=AF.Exp,
                                                                bias=SC[:, 1, h:h + 1]),
                      reads=[pb["bE2"], pb["bSC"]], writes=[pb["bE2"]])
        yield
    for j in range(NP):
        pb = PBUF[j]
        P.pool(lambda e, pb=pb: e.tensor_tensor(out=pb["E1"], in0=pb["E1"],
                                                in1=maskL.unsqueeze(1).to_broadcast([128, H, 128]), op=ALU.mult),
               reads=[pb["bE1"], bc], writes=[pb["bE1"]])
        if full:
            P.pool(lambda e, pb=pb: e.tensor_tensor(out=pb["E2"], in0=pb["E2"],
                                                    in1=mask2.unsqueeze(1).to_broadcast([128, H, 128]), op=ALU.mult),
                   reads=[pb["bE2"], bc], writes=[pb["bE2"]])
            c0 = j * 128
            P.dve(lambda e, pb=pb, c0=c0: e.tensor_tensor(out=pb["QTG"], in0=QT[:, :, c0:c0 + 128], in1=pb["EGR"], op=ALU.mult),
                  reads=bQT + [pb["bEGR"]], writes=[pb["bQTG"]])
        yield
    for j in range(NP):
        c0 = j * 128
        for h in range(H):
            P.pe(lambda e, j=j, h=h, c0=c0: e.matmul(pP[j][:, h, :], lhsT=KT[:, h, c0:c0 + 128], rhs=KT[:, h, c0:c0 + 128],
                                                     start=True, stop=True), reads=[bKT[h]], writes=[bpP[j]])
        yield
    for j in range(NP):
        pb = PBUF[j]
        P.dve(lambda e, pb=pb, j=j: e.tensor_tensor(out=pb["Lm"][0], in0=pP[j], in1=pb["E1"], op=ALU.mult),
              reads=[bpP[j], pb["bE1"]], writes=[pb["bLm"][0]])
        yield
    if full:
        for j in range(NP):
            c0 = j * 128
            for h in range(H):
                P.pe(lambda e, j=j, h=h, c0=c0: e.matmul(pP[j][:, h, :], lhsT=KT[:, h, c0:c0 + 128],
                                                         rhs=QT[:, h, c0:c0 + 128], start=True, stop=True),
                     reads=[bKT[h], bQT[h]], writes=[bpP[j]])
            yield
        for j in range(NP):
            pb = PBUF[j]
            P.dve(lambda e, pb=pb, j=j: e.tensor_tensor(out=pb["ATT"], in0=pP[j], in1=pb["E2"], op=ALU.mult),
                  reads=[bpP[j], pb["bE2"]], writes=[pb["bATT"]])
            yield
    for j in range(NP):
        pb = PBUF[j]
        for h in range(H):
            P.pe(lambda e, pb=pb, j=j, h=h: e.transpose(out=pPb[j][:, h, :], in_=pb["Lm"][0][:, h, :], identity=ident),
                 reads=[pb["bLm"][0], bc], writes=[bpP[j]])
        yield
    for j in range(NP):
        pb = PBUF[j]
        evac(pb["Um"][0], pPb[j], [bpP[j]], [pb["bUm"][0]])
        yield
    for j in range(NP):
        pb = PBUF[j]
        P.pool(lambda e, pb=pb: e.tensor_tensor(out=pb["Xm"][0], in0=identb4, in1=pb["Um"][0], op=ALU.subtract),
               reads=[pb["bUm"][0], bc], writes=[pb["bXm"][0]])
        yield
    cur, cx = 0, 0
    for lvl in range(5):
        for j in range(NP):
            pb = PBUF[j]
            for h in range(H):
                P.pe(lambda e, pb=pb, j=j, h=h, cur=cur: e.matmul(pP[j][:, h, :], lhsT=pb["Um"][cur][:, h, :],
                                                                  rhs=pb["Lm"][cur][:, h, :], start=True, stop=True),
                     reads=[pb["bUm"][cur], pb["bLm"][cur]], writes=[bpP[j]])
            yield
        for j in range(NP):
            pb = PBUF[j]
            evac(pb["Lm"][1 - cur], pP[j], [bpP[j]], [pb["bLm"][1 - cur]])
            yield
        if lvl < 4:
            for j in range(NP):
                pb = PBUF[j]
                for h in range(H):
                    P.pe(lambda e, pb=pb, j=j, h=h, cur=cur: e.matmul(pP[j][:, h, :], lhsT=pb["Lm"][cur][:, h, :],
                                                                      rhs=pb["Um"][cur][:, h, :], start=True, stop=True),
                         reads=[pb["bUm"][cur], pb["bLm"][cur]], writes=[bpP[j]])
                yield
            for j in range(NP):
                pb = PBUF[j]
                evac(pb["Um"][1 - cur], pP[j], [bpP[j]], [pb["bUm"][1 - cur]])
                yield
        for j in range(NP):
            pb = PBUF[j]
            for h in range(H):
                P.pe(lambda e, pb=pb, j=j, h=h, cur=cur, cx=cx: e.matmul(pP[j][:, h, :], lhsT=pb["Lm"][1 - cur][:, h, :],
                                                                         rhs=pb["Xm"][cx][:, h, :], start=True, stop=False),
                     reads=[pb["bLm"][1 - cur], pb["bXm"][cx]], writes=[bpP[j]])
                P.pe(lambda e, pb=pb, j=j, h=h, cx=cx: e.matmul(pP[j][:, h, :], lhsT=ident, rhs=pb["Xm"][cx][:, h, :],
                                                                start=False, stop=True),
                     reads=[pb["bXm"][cx], bc], writes=[bpP[j]])
            yield
        for j in range(NP):
            pb = PBUF[j]
            if lvl == 4:
                evac(pb["TT"], pP[j], [bpP[j]], [pb["bTT"]])
            else:
                evac(pb["Xm"][1 - cx], pP[j], [bpP[j]], [pb["bXm"][1 - cx]])
            yield
        cur, cx = 1 - cur, 1 - cx
    for j in range(NP):
        pb = PBUF[j]
        c0 = j * 128
        SC = pb["SC"]
        for h in range(H):
            P.pe(lambda e, h=h, c0=c0, j=j: e.transpose(out=pPb[j][:, h, :], in_=KT[:, h, c0:c0 + 128], identity=ident),
                 reads=[bKT[h], bc], writes=[bpP[j]])
        P.dve(lambda e, pb=pb, SC=SC, j=j: e.tensor_tensor(out=pb["KHT"], in0=pPb[j], in1=bc4(SC[:, 5, :]), op=ALU.mult),
              reads=[bpP[j], pb["bSC"]], writes=[pb["bKHT"]])
        for h in range(H):
            P.pe(lambda e, h=h, c0=c0, j=j: e.transpose(out=pPb[j][:, h, :], in_=VT[:, h, c0:c0 + 128], identity=ident),
                 reads=[bVT[h], bc], writes=[bpP[j]])
        P.act(lambda e, pb=pb, j=j: e.activation(out=pb["VTK"], in_=pPb[j], func=AF.Copy), reads=[bpP[j]], writes=[pb["bVTK"]])
        P.pool(lambda e, pb=pb, c0=c0: e.tensor_copy(out=pb["KTP"], in_=KT[:, :, c0:c0 + 128]), reads=bKT, writes=[pb["bKTP"]])
        P.pool(lambda e, pb=pb, j=j: e.tensor_tensor(out=pb["BV"], in0=pb["VTK"], in1=bc4(BETA[:, j, :]), op=ALU.mult),
               reads=[pb["bVTK"], bBG], writes=[pb["bBV"]])
        yield


def _gdn_rec(self, L, full):
    P = self.P
    H = 4
    g = lambda n: L[n]
    bk, bb = self.banks, self.bbank
    bc = self.bconst
    mask2, ident = self.mask2, self.ident
    maskL, bones, ch01, identb4 = g("maskL"), g("bones"), g("ch01"), g("identb4")
    PBUF, GG, LNB, BETA, bBG = g("PB"), g("GG"), g("LNB"), g("BETA"), g("bBG")
    QT, KT, VT, bQT, bKT, bVT = g("QT"), g("KT"), g("VT"), g("bQT"), g("bKT"), g("bVT")
    R, USB, bR, bUSB = g("R"), g("USB"), g("bR"), g("bUSB")
    S32, SBF, bS32, bSBF, sbf_i = g("S32"), g("SBF"), g("bS32"), g("bSBF"), g("sbf_i")
    O32, bO32 = g("O32"), g("bO32")
    evac, nextpj = g("evac"), g("nextpj")
    NP = NT
    pP = [bk[3 + j][:].rearrange("p (h t) -> p h t", h=H) for j in range(NP)]
    pPb = [bk[3 + j][:].bitcast(BF16)[:, 0:512].rearrange("p (h t) -> p h t", h=H) for j in range(NP)]
    bpP = [bb[3 + j] for j in range(NP)]
    ptr4 = bk[5][:].bitcast(BF16)[:, 0:512].rearrange("p (h t) -> p h t", h=H)
    pKS = bk[5][:].rearrange("p (h t) -> p h t", h=H)
    pU = bk[6][:].rearrange("p (h t) -> p h t", h=H)
    po = bk[7][:].rearrange("p (h t) -> p h t", h=H)

    def bc4(ap):
        return ap.unsqueeze(2).to_broadcast([128, H, 128])

    for j in range(NP):
        pb = PBUF[j]
        c0 = j * 128
        SC = pb["SC"]
        TT, bTT = pb["TT"], pb["bTT"]
        for half in range(2):
            r0 = half * 64
            cs = sbf_i[0]
            for h in range(H):
                P.pe(lambda e, h=h, pb=pb, cs=cs: e.matmul(pKS[:, h, :], lhsT=pb["KTP"][:, h, :], rhs=SBF[cs][:, h, :],
                                                           start=True, stop=True),
                     reads=[pb["bKTP"], bSBF[cs][h]], writes=[bb[5]])
            if full:
                for h in range(H):
                    P.pe(lambda e, pb=pb, h=h, cs=cs, r0=r0, half=half: e.matmul(
                        po[:, h, r0:r0 + 64], lhsT=SBF[cs][:, h, :], rhs=pb["QTG"][:, h, r0:r0 + 64],
                        start=(h == 0 and half == 0), stop=False, skip_group_check=True),
                        reads=[bSBF[cs][h], pb["bQTG"]], writes=[bb[7]])
            for h in range(H):
                P.dve(lambda e, pb=pb, h=h, r0=r0, SC=SC: e.scalar_tensor_tensor(
                    out=R[r0:r0 + 64, h, :], in0=pKS[r0:r0 + 64, h, :], scalar=SC[r0:r0 + 64, 4, h:h + 1],
                    in1=pb["BV"][r0:r0 + 64, h, :], op0=ALU.mult, op1=ALU.add),
                    reads=[bb[5], pb["bSC"], pb["bBV"]], writes=[bR])
            yield
            for h in range(H):
                P.pe(lambda e, h=h, r0=r0, TT=TT: e.matmul(pU[:, h, :], lhsT=TT[r0:r0 + 64, h, :], rhs=R[r0:r0 + 64, h, :],
                                                           start=True, stop=True),
                     reads=[bTT, bR], writes=[bb[6]])
            yield
            P.act(lambda e, r0=r0: e.activation(out=USB[r0:r0 + 64], in_=pU[r0:r0 + 64], func=AF.Copy),
                  reads=[bb[6]], writes=[bUSB])
            yield
            pS, bpS = bk[6][:], bb[6]
            pS4 = pS.rearrange("p (h t) -> p h t", h=H)
            for h in range(H):
                P.pe(lambda e, pb=pb, h=h, r0=r0, pS4=pS4: e.matmul(pS4[:, h, :], lhsT=pb["KHT"][r0:r0 + 64, h, :],
                                                                    rhs=USB[r0:r0 + 64, h, :], start=True, stop=True),
                     reads=[pb["bKHT"], bUSB], writes=[bpS])
            yield
            for h in range(H):
                P.dve(lambda e, h=h, half=half, SC=SC, pS4=pS4: e.scalar_tensor_tensor(
                    out=S32[:, h, :], in0=S32[:, h, :], scalar=SC[:, 6 + half, h:h + 1], in1=pS4[:, h, :],
                    op0=ALU.mult, op1=ALU.add), reads=[bS32[h], pb["bSC"], bpS], writes=[bS32[h]])
            yield
            nx = 1 - cs
            P.act(lambda e, nx=nx: e.activation(out=SBF[nx], in_=S32, func=AF.Copy), reads=bS32, writes=bSBF[nx])
            sbf_i[0] = nx
            yield
        if full:
            for h in range(H):
                P.pe(lambda e, pb=pb, h=h: e.matmul(po[:, h, :], lhsT=USB[:, h, :], rhs=pb["ATT"][:, h, :],
                                                    start=False, stop=True, skip_group_check=True),
                     reads=[bUSB, pb["bATT"]], writes=[bb[7]])
            P.act(lambda e, c0=c0: e.activation(out=O32[:, :, c0:c0 + 128], in_=po, func=AF.Copy),
                  reads=[bb[7]], writes=[bO32])


KB._gdn_prep = _gdn_prep
KB._gdn_rec = _gdn_rec


CAP = 384
NEXP = 32
NSLOT = NEXP * CAP
BIG = 1.0e4


def _spill(self, name, sb_ap, bufs, sb):
    P = self.P
    if name not in self.d:
        self.d[name] = self.nc.dram_tensor(name, [128, 4, self.nfull_tok], BF16, kind="Internal").ap()
        self.bspill = getattr(self, "bspill", {})
        self.bspill[name] = {}
    dst = self.d[name]
    tok0 = (sb - self.npre) * SBT
    b = Buf(name)
    self.bspill[name][sb - self.npre] = b
    P.dma(lambda e: e.dma_start(out=dst[:, :, tok0:tok0 + SBT], in_=sb_ap), reads=bufs, writes=[b])


KB.spill = _spill


def _pass3(self):
    nc, P, A = self.nc, self.P, self.A
    d = self.d
    H = 4
    for nm, shp in (("hg_up", [512, D]), ("gd_up", [512, D]), ("w_out", [D, D]), ("norm_ffn_g", [D]),
                    ("router_w", [D, 36]), ("router_b", [36])):
        self.din(nm, shp)
    ntile = self.nfull_tok // 128
    d["h2_s"] = nc.dram_tensor("h2_s", [self.nfull_tok, D], F32, kind="Internal").ap()
    self.bh2s = [Buf("h2_s%d" % i) for i in range(ntile)]
    self.bxbuf = []
    self.W12 = A.alloc((ntile, 2), F32)
    self.DST = A.alloc((ntile, 2), U32)
    self.bW12 = Buf("W12")
    self.bDST = Buf("DST")
    m0 = A.mark()
    W3 = A.alloc((KC, 2048), BF16)
    bW3 = [Buf() for _ in range(KC)]
    wv = d["w_in"].rearrange("(k p) n -> p k n", p=128)
    for k in range(KC):
        P.dma(lambda e, k=k: e.dma_start(out=W3[:, k, :], in_=wv[:, k, C_PA:C_PA + 2048]), writes=[bW3[k]], q="pool")
    HGUP = A.alloc((H, D), BF16)
    GDUP = A.alloc((H, D), BF16)
    WOUT = A.alloc((KC, D), BF16)
    bUP = Buf()
    bWO = [Buf() for _ in range(KC)]
    P.dma(lambda e: e.dma_start(out=HGUP, in_=d["hg_up"].rearrange("(h p) n -> p h n", p=128)), writes=[bUP], q="pool")
    P.dma(lambda e: e.dma_start(out=GDUP, in_=d["gd_up"].rearrange("(h p) n -> p h n", p=128)), writes=[bUP], q="pool")
    wo = d["w_out"].rearrange("(k p) n -> p k n", p=128)
    for k in range(KC):
        P.dma(lambda e, k=k: e.dma_start(out=WOUT[:, k, :], in_=wo[:, k, :]), writes=[bWO[k]], q="pool")
    WR = A.alloc((KC, 36), F32)
    RB = A.alloc((36,), F32)
    G2 = A.alloc((D,), F32)
    ECAP = A.alloc((NEXP,), F32)
    bpar = Buf("par3")
    P.dma(lambda e: e.dma_start(out=WR, in_=d["router_w"].rearrange("(k p) n -> p k n", p=128)), writes=[bpar])
    P.dma(lambda e: e.dma_start(out=RB, in_=d["router_b"].partition_broadcast(128)), writes=[bpar])
    P.dma(lambda e: e.dma_start(out=G2, in_=d["norm_ffn_g"].partition_broadcast(128)), writes=[bpar])
    self.load_gain("norm_mix_g")
    ecapi = A.alloc((NEXP,), I32)
    P.pool(lambda e: e.iota(ecapi, pattern=[[CAP, NEXP]], base=0, channel_multiplier=0), writes=[bpar])
    P.pool(lambda e: e.tensor_copy(out=ECAP, in_=ecapi), reads=[bpar], writes=[bpar])
    triS = A.alloc((128,), BF16)
    trif = A.alloc((128,), F32)
    bc = self.bconst
    P.pool(lambda e: e.memset(trif, 1.0), writes=[bc])
    P.pool(lambda e: e.affine_select(out=trif, in_=trif, pattern=[[1, 128]], compare_op=ALU.is_gt, fill=0.0,
                                     base=0, channel_multiplier=-1), reads=[bc], writes=[bc])
    P.pool(lambda e: e.tensor_copy(out=triS, in_=trif), reads=[bc], writes=[bc])
    BASE = A.alloc((NEXP,), F32)
    bBASE = Buf()
    P.pool(lambda e: e.tensor_copy(out=BASE, in_=ecapi), reads=[bpar], writes=[bBASE])

    xnTs = [A.alloc((KC, SBT), BF16) for _ in range(2)]
    bxnTs = [Buf(), Buf()]
    self._xfetched = set()
    use_fetch = hasattr(self, "bxns")
    SGs = [A.alloc((16, SBT), BF16) for _ in range(2)]
    bSGs = [[Buf() for _ in range(16)] for _ in range(2)]
    OAss = [A.alloc((H, SBT), BF16) for _ in range(2)]
    OBss = [A.alloc((H, SBT), BF16) for _ in range(2)]
    bOAss, bOBss = [Buf(), Buf()], [Buf(), Buf()]
    T1 = [A.alloc((SBT,), F32) for _ in range(2)]
    T2 = [A.alloc((SBT,), F32) for _ in range(2)]
    bT1 = [Buf(), Buf()]
    bT2 = [Buf(), Buf()]
    MG = A.alloc((KC, SBT), BF16)
    bMG = [Buf() for _ in range(KC)]
    XR = A.alloc((D,), F32)
    bXR = Buf()
    H2 = A.alloc((D,), F32)
    bH2 = Buf()
    JK = A.alloc((D,), BF16)
    SS2 = A.alloc((1,), F32)
    bSS2 = Buf()
    XF = A.alloc((D,), F32)
    XB = A.alloc((D,), BF16)
    bXF, bXB = Buf(), Buf()
    XFT = A.alloc((KC, 128), F32)
    bXFT = Buf()
    LG = A.alloc((36,), F32)
    ME = A.alloc((NEXP,), F32)
    SM = A.alloc((16,), F32)
    M8 = A.alloc((8,), F32)
    SEL1 = A.alloc((NEXP,), F32)
    SEL2 = A.alloc((NEXP,), F32)
    SELB = A.alloc((NEXP,), BF16)
    RK = A.alloc((NEXP,), F32)
    JK2 = A.alloc((NEXP,), F32)
    DF = A.alloc((2,), F32)
    brt = Buf("route")

    bk, bb = self.banks, self.bbank
    ptr = bk[0][:].bitcast(BF16).rearrange("p (k t) -> p k t", k=KC)
    pj = [bk[1][:], bk[2][:]]
    bpj = [bb[1], bb[2]]
    pup = [bk[3][:], bk[4][:]]
    pji = [0]

    def nextpj():
        i = pji[0] % 2
        pji[0] += 1
        return pj[i], bpj[i]

    pyi = [0]

    def nextpy():
        i = 5 + pyi[0] % 3
        pyi[0] += 1
        return bk[i][:], bb[i]

    oa_d, ob_d = d["oa_s"], d["ob_s"]
    def X3(sbi):
        sl = sbi % 2
        SG, bSG, OAs, OBs, bOAs, bOBs = SGs[sl], bSGs[sl], OAss[sl], OBss[sl], bOAss[sl], bOBss[sl]
        sb = self.npre + sbi
        tok0 = sbi * SBT
        xnT, bxnT = xnTs[sb % 2], bxnTs[sb % 2]
        if use_fetch:
            yield from self.xnt_fetch_gen(sb, self.nsb - 1, xnTs, bxnTs)
        else:
            yield from self.stage_a_gen(sb, xnT, bxnT, ptr, bb[0])
        P.dma(lambda e, tok0=tok0: e.dma_start(out=OAs, in_=oa_d[:, :, tok0:tok0 + SBT]),
              reads=[self.bspill["oa_s"][sbi]], writes=[bOAs])
        P.dma(lambda e, tok0=tok0: e.dma_start(out=OBs, in_=ob_d[:, :, tok0:tok0 + SBT]),
              reads=[self.bspill["ob_s"][sbi]], writes=[bOBs])
        for cb in range(16):
            p, bp = nextpj()
            for k in range(KC):
                P.pe(lambda e, k=k, p=p, cb=cb: e.matmul(p[:, 0:SBT], lhsT=W3[:, k, cb * 128:(cb + 1) * 128],
                                                         rhs=xnT[:, k, :], start=(k == 0), stop=(k == KC - 1)),
                     reads=[bW3[k], bxnT], writes=[bp])
            P.act(lambda e, cb=cb, p=p: e.activation(out=SG[:, cb, :], in_=p[:, 0:SBT], func=AF.Sigmoid),
                  reads=[bp], writes=[bSG[cb]])
            yield
    def Y3(sbi):
        sl = sbi % 2
        SG, bSG, OAs, OBs, bOAs, bOBs = SGs[sl], bSGs[sl], OAss[sl], OBss[sl], bOAss[sl], bOBss[sl]
        sb = self.npre + sbi
        tok0 = sbi * SBT
        for cb in range(KC):
            i = cb % 2
            for h in range(H):
                P.pe(lambda e, h=h, cb=cb: e.matmul(pup[0][:, 0:SBT], lhsT=HGUP[:, h, cb * 128:(cb + 1) * 128],
                                                    rhs=OAs[:, h, :], start=(h == 0), stop=(h == H - 1)),
                     reads=[bUP, bOAs], writes=[bb[3]])
            for h in range(H):
                P.pe(lambda e, h=h, cb=cb: e.matmul(pup[1][:, 0:SBT], lhsT=GDUP[:, h, cb * 128:(cb + 1) * 128],
                                                    rhs=OBs[:, h, :], start=(h == 0), stop=(h == H - 1)),
                     reads=[bUP, bOBs], writes=[bb[4]])
            P.dve(lambda e, cb=cb, i=i: e.tensor_tensor(out=T1[i], in0=pup[0][:, 0:SBT], in1=SG[:, cb, :], op=ALU.mult),
                  reads=[bb[3], bSG[cb]], writes=[bT1[i]])
            P.dve(lambda e, cb=cb, i=i: e.tensor_tensor(out=T2[i], in0=pup[1][:, 0:SBT], in1=SG[:, 8 + cb, :], op=ALU.mult),
                  reads=[bb[4], bSG[8 + cb]], writes=[bT2[i]])
            P.pool(lambda e, cb=cb, i=i: e.tensor_tensor(out=MG[:, cb, :], in0=T1[i], in1=T2[i], op=ALU.add),
                   reads=[bT1[i], bT2[i]], writes=[bMG[cb]])
            yield
        for t in range(NT):
            gt = sbi * NT + t
            gtok = self.npre * SBT + gt * 128
            P.dma(lambda e, gtok=gtok: e.dma_start(out=XR, in_=d["xs"][gtok:gtok + 128, :]), writes=[bXR])
            for half in range(2):
                p, bp = nextpy()
                for k in range(KC):
                    P.pe(lambda e, k=k, p=p, t=t, half=half: e.matmul(
                        p, lhsT=MG[:, k, t * 128:(t + 1) * 128], rhs=WOUT[:, k, half * 512:(half + 1) * 512],
                        start=(k == 0), stop=(k == KC - 1)), reads=[bMG[k], bWO[k]], writes=[bp])
                P.dve(lambda e, p=p, half=half: e.tensor_tensor(out=H2[:, half * 512:(half + 1) * 512], in0=p,
                                                               in1=XR[:, half * 512:(half + 1) * 512], op=ALU.add),
                      reads=[bp, bXR], writes=[bH2])
                yield
            P.dma(lambda e, gt=gt: e.dma_start(out=d["h2_s"][gt * 128:(gt + 1) * 128, :], in_=H2),
                  reads=[bH2], writes=[self.bh2s[gt]])
            LL = dict(L0)
            LL['nextpj'] = nextpy
            yield from self._route_tile(LL, gt)
    L0 = dict(locals())

    import os
    WTS3 = [int(v) for v in os.environ.get("IL_W3", "1,1").split(",")]

    def run_il(gens):
        gens = list(gens)
        while gens:
            for g_, w_ in list(gens):
                for _ in range(w_):
                    try:
                        next(g_)
                    except StopIteration:
                        gens.remove((g_, w_))
                        break

    n = self.nfull
    for r in range(-1, n):
        gs = []
        if 0 <= r < n:
            gs.append((Y3(r), WTS3[0]))
        if 0 <= r + 1 < n:
            gs.append((X3(r + 1), WTS3[1]))
        run_il(gs)
    A.reset(m0)
    P.barrier()


KB.pass3 = _pass3


def _route_tile(self, L, gt):
    P = self.P
    g = lambda n: L[n]
    bk, bb = self.banks, self.bbank
    bc = self.bconst
    H2, bH2, JK, SS2, bSS2 = g("H2"), g("bH2"), g("JK"), g("SS2"), g("bSS2")
    XF, XB, bXF, bXB, XFT, bXFT = g("XF"), g("XB"), g("bXF"), g("bXB"), g("XFT"), g("bXFT")
    G2, WR, RB, ECAP, bpar = g("G2"), g("WR"), g("RB"), g("ECAP"), g("bpar")
    LG, ME, SM, M8, SEL1, SEL2, SELB, RK, JK2, DF, brt = (g("LG"), g("ME"), g("SM"), g("M8"), g("SEL1"), g("SEL2"),
                                                           g("SELB"), g("RK"), g("JK2"), g("DF"), g("brt"))
    BASE, bBASE, triS = g("BASE"), g("bBASE"), g("triS")
    nextpj = g("nextpj")
    W12, DST = self.W12, self.DST
    P.act(lambda e: e.activation(out=JK, in_=H2, func=AF.Square, accum_out=SS2), reads=[bH2], writes=[bSS2])
    P.act(lambda e: e.activation(out=SM[:, 0:1], in_=SS2, func=AF.Ln, scale=1.0 / D, bias=EPS), reads=[bSS2], writes=[brt])
    P.act(lambda e: e.activation(out=SM[:, 0:1], in_=SM[:, 0:1], func=AF.Exp, scale=-0.5), reads=[brt], writes=[brt])
    P.dve(lambda e: e.scalar_tensor_tensor(out=XF, in0=H2, scalar=SM[:, 0:1], in1=G2, op0=ALU.mult, op1=ALU.mult),
          reads=[bH2, brt, bpar], writes=[bXF])
    P.pool(lambda e: e.tensor_copy(out=XB, in_=XF), reads=[bXF], writes=[bXB])
    yield
    for half in range(2):
        for kk in range(4):
            k = half * 4 + kk
            P.pe(lambda e, k=k, kk=kk, half=half: e.transpose(out=bk[3 + half][:, kk * 128:(kk + 1) * 128],
                                                              in_=XF[:, k * 128:(k + 1) * 128], identity=self.identf),
                 reads=[bXF, bc], writes=[bb[3 + half]])
    P.act(lambda e: e.activation(out=XFT[:, 0:4, :], in_=bk[3][:].rearrange("p (k t) -> p k t", k=4), func=AF.Copy),
          reads=[bb[3]], writes=[bXFT])
    P.dve(lambda e: e.tensor_copy(out=XFT[:, 4:8, :], in_=bk[4][:].rearrange("p (k t) -> p k t", k=4)),
          reads=[bb[4]], writes=[bXFT])
    yield
    p, bp = nextpj()
    for k in range(KC):
        P.pe(lambda e, k=k, p=p: e.matmul(p[:, 0:36], lhsT=XFT[:, k, :], rhs=WR[:, k, :], start=(k == 0), stop=(k == KC - 1)),
             reads=[bXFT, bpar], writes=[bp])
    P.dve(lambda e, p=p: e.tensor_tensor(out=LG, in0=p[:, 0:36], in1=RB, op=ALU.add), reads=[bp, bpar], writes=[brt])
    yield
    P.dve(lambda e: e.tensor_reduce(out=SM[:, 1:2], in_=LG[:, 0:4], axis=AX.X, op=ALU.max), reads=[brt], writes=[brt])
    P.dve(lambda e: e.tensor_scalar(out=SM[:, 2:3], in0=SM[:, 1:2], scalar1=-1.0, scalar2=None, op0=ALU.mult),
          reads=[brt], writes=[brt])
    P.act(lambda e: e.activation(out=JK2[:, 0:4], in_=LG[:, 0:4], func=AF.Exp, bias=SM[:, 2:3], accum_out=SM[:, 3:4]),
          reads=[brt], writes=[brt])
    P.dve(lambda e: e.reciprocal(out=SM[:, 4:5], in_=SM[:, 3:4]), reads=[brt], writes=[brt])
    yield
    P.dve(lambda e: e.tensor_scalar(out=SM[:, 12:16], in0=LG[:, 0:4], scalar1=SM[:, 1:2], scalar2=-1.0,
                                    op0=ALU.is_equal, op1=ALU.add), reads=[brt], writes=[brt])
    P.dve(lambda e: e.scalar_tensor_tensor(out=ME.rearrange("p (g j) -> p g j", g=4),
                                           in0=SM[:, 12:16].unsqueeze(2).to_broadcast([128, 4, 8]), scalar=BIG,
                                           in1=LG[:, 4:36].rearrange("p (g j) -> p g j", g=4),
                                           op0=ALU.mult, op1=ALU.add), reads=[brt], writes=[brt])
    P.dve(lambda e: e.max(out=M8, in_=ME), reads=[brt], writes=[brt])
    yield
    P.dve(lambda e: e.tensor_tensor(out=SM[:, 5:6], in0=M8[:, 1:2], in1=M8[:, 0:1], op=ALU.subtract), reads=[brt], writes=[brt])
    P.act(lambda e: e.activation(out=SM[:, 6:7], in_=SM[:, 5:6], func=AF.Exp), reads=[brt], writes=[brt])
    P.dve(lambda e: e.tensor_scalar(out=SM[:, 7:8], in0=SM[:, 6:7], scalar1=1.0, scalar2=None, op0=ALU.add),
          reads=[brt], writes=[brt])
    P.dve(lambda e: e.reciprocal(out=SM[:, 8:9], in_=SM[:, 7:8]), reads=[brt], writes=[brt])
    P.dve(lambda e, gt=gt: e.tensor_tensor(out=W12[:, gt, 0:1], in0=SM[:, 8:9], in1=SM[:, 4:5], op=ALU.mult),
          reads=[brt], writes=[self.bW12])
    P.dve(lambda e, gt=gt: e.tensor_tensor(out=W12[:, gt, 1:2], in0=SM[:, 4:5], in1=W12[:, gt, 0:1], op=ALU.subtract),
          reads=[brt, self.bW12], writes=[self.bW12])
    P.dve(lambda e: e.tensor_scalar(out=SEL1, in0=ME, scalar1=M8[:, 0:1], scalar2=None, op0=ALU.is_equal),
          reads=[brt], writes=[brt])
    P.dve(lambda e: e.tensor_scalar(out=SEL2, in0=ME, scalar1=M8[:, 1:2], scalar2=None, op0=ALU.is_equal),
          reads=[brt], writes=[brt])
    P.dve(lambda e: e.tensor_tensor(out=SELB, in0=SEL1, in1=SEL2, op=ALU.add), reads=[brt], writes=[brt])
    yield
    p2, bp2 = nextpj()
    P.pe(lambda e, p2=p2: e.matmul(p2[:, 0:32], lhsT=triS, rhs=SELB, start=True, stop=True), reads=[brt, bc], writes=[bp2])
    P.pe(lambda e, p2=p2: e.matmul(p2[:, 32:64], lhsT=self.ones_bf, rhs=SELB, start=True, stop=True),
         reads=[brt, bc], writes=[bp2])
    P.dve(lambda e, p2=p2: e.tensor_tensor(out=RK, in0=p2[:, 0:32], in1=BASE, op=ALU.add), reads=[bp2, bBASE], writes=[brt])
    P.dve(lambda e, p2=p2: e.tensor_tensor(out=BASE, in0=p2[:, 32:64], in1=BASE, op=ALU.add), reads=[bp2, bBASE], writes=[bBASE])
    P.dve(lambda e: e.scalar_tensor_tensor(out=JK2, in0=SEL1, scalar=1.0, in1=RK, op0=ALU.mult, op1=ALU.mult,
                                           accum_out=DF[:, 0:1]), reads=[brt], writes=[brt])
    P.dve(lambda e: e.scalar_tensor_tensor(out=JK2, in0=SEL2, scalar=1.0, in1=RK, op0=ALU.mult, op1=ALU.mult,
                                           accum_out=DF[:, 1:2]), reads=[brt], writes=[brt])
    P.dve(lambda e, gt=gt: e.tensor_copy(out=DST[:, gt, :], in_=DF), reads=[brt], writes=[self.bDST])
    yield
    xb = self.d["x_buf"]
    for k in range(2):
        bx = Buf("xbuf")
        self.bxbuf.append(bx)
        P.dma(lambda e, gt=gt, k=k: e.indirect_dma_start(
            out=xb, out_offset=bass.IndirectOffsetOnAxis(ap=DST[:, gt, k:k + 1], axis=0), in_=XB, in_offset=None),
            reads=[bXB, self.bDST] + self.bxz, writes=[bx], q="pool")


KB._route_tile = _route_tile


def _pass4(self):
    nc, P, A = self.nc, self.P, self.A
    d = self.d
    self.din("w_gate", [NEXP, D, 512])
    self.din("w_up", [NEXP, D, 512])
    self.din("w_down", [NEXP, 512, D])
    d["y_buf"] = nc.dram_tensor("y_buf", [NSLOT, D], F32, kind="Internal").ap()
    self.bybuf = []
    m0 = A.mark()
    NB = CAP // 128
    WG = [A.alloc((KC, 512), BF16) for _ in range(2)]
    WU = [A.alloc((KC, 512), BF16) for _ in range(2)]
    WD = [A.alloc((4, D), BF16) for _ in range(2)]
    bWG = [Buf(), Buf()]
    bWU = [Buf(), Buf()]
    bWD = [Buf(), Buf()]
    XE = [A.alloc((NB, D), BF16) for _ in range(2)]
    bXE = [Buf(), Buf()]
    XET = [A.alloc((KC, CAP), BF16) for _ in range(2)]
    bXET = [Buf(), Buf()]
    SGT = [A.alloc((CAP,), F32) for _ in range(2)]
    bSGT = [Buf(), Buf()]
    HT = A.alloc((4, CAP), BF16)
    bHT = [Buf() for _ in range(4)]
    YS = [A.alloc((D,), F32) for _ in range(2)]
    bYS = [Buf(), Buf()]
    bk, bb = self.banks, self.bbank
    ptr = bk[0][:].bitcast(BF16).rearrange("p (k t) -> p k t", k=KC)
    ident = self.ident
    bc = self.bconst
    xb, yb = d["x_buf"], d["y_buf"]
    cnt = [0]

    def bank(lo, n):
        i = lo + cnt[0] % n
        cnt[0] += 1
        return bk[i][:], bb[i]

    def load_w(e):
        i = e % 2
        P.dma(lambda eng, e=e, i=i: eng.dma_start(out=WG[i], in_=d["w_gate"][e].rearrange("(k p) n -> p k n", p=128)),
              writes=[bWG[i]], q="pool")
        P.dma(lambda eng, e=e, i=i: eng.dma_start(out=WU[i], in_=d["w_up"][e].rearrange("(k p) n -> p k n", p=128)),
              writes=[bWU[i]], q="pool")
        P.dma(lambda eng, e=e, i=i: eng.dma_start(out=WD[i], in_=d["w_down"][e].rearrange("(k p) n -> p k n", p=128)),
              writes=[bWD[i]], q="pool")

    def load_x(e):
        i = e % 2
        P.dma(lambda eng, e=e, i=i: eng.dma_start(out=XE[i], in_=xb[e * CAP:(e + 1) * CAP, :].rearrange("(b p) n -> p b n", p=128)),
              reads=self.bxbuf, writes=[bXE[i]])

    def transposes(e):
        i = e % 2
        for b in range(NB):
            pt, bpt = (ptr, bb[0]) if b % 2 == 0 else (ptr7, bb[7])
            for k in range(KC):
                P.pe(lambda eng, i=i, b=b, k=k, pt=pt: eng.transpose(out=pt[:, k, :], in_=XE[i][:, b, k * 128:(k + 1) * 128], identity=ident),
                     reads=[bXE[i], bc], writes=[bpt])
            if b % 2 == 0:
                P.act(lambda eng, b=b, i=i, pt=pt: eng.activation(out=XET[i][:, :, b * 128:(b + 1) * 128], in_=pt, func=AF.Copy),
                      reads=[bpt], writes=[bXET[i]])
            else:
                P.dve(lambda eng, b=b, i=i, pt=pt: eng.tensor_copy(out=XET[i][:, :, b * 128:(b + 1) * 128], in_=pt),
                      reads=[bpt], writes=[bXET[i]])

    ptr7 = bk[7][:].bitcast(BF16).rearrange("p (k t) -> p k t", k=KC)
    load_w(0)
    load_x(0)
    transposes(0)
    yi = 0
    for e in range(NEXP):
        i = e % 2
        if e + 1 < NEXP:
            load_w(e + 1)
            load_x(e + 1)
        for fc in range(4):
            pg, bpg = bk[1 + fc % 2][:], bb[1 + fc % 2]
            pu, bpu = bk[3 + fc % 2][:], bb[3 + fc % 2]
            for k in range(KC):
                P.pe(lambda eng, i=i, fc=fc, k=k, pg=pg: eng.matmul(pg[:, 0:CAP], lhsT=WG[i][:, k, fc * 128:(fc + 1) * 128],
                                                                    rhs=XET[i][:, k, :], start=(k == 0), stop=(k == KC - 1)),
                     reads=[bWG[i], bXET[i]], writes=[bpg])
            for k in range(KC):
                P.pe(lambda eng, i=i, fc=fc, k=k, pu=pu: eng.matmul(pu[:, 0:CAP], lhsT=WU[i][:, k, fc * 128:(fc + 1) * 128],
                                                                    rhs=XET[i][:, k, :], start=(k == 0), stop=(k == KC - 1)),
                     reads=[bWU[i], bXET[i]], writes=[bpu])
            j = fc % 2
            P.act(lambda eng, pg=pg, j=j: eng.activation(out=SGT[j], in_=pg[:, 0:CAP], func=AF.Silu), reads=[bpg], writes=[bSGT[j]])
            P.dve(lambda eng, pu=pu, j=j, fc=fc: eng.tensor_tensor(out=HT[:, fc, :], in0=pu[:, 0:CAP], in1=SGT[j], op=ALU.mult),
                  reads=[bpu, bSGT[j]], writes=[bHT[fc]])
        if e + 1 < NEXP:
            transposes(e + 1)
        for b in range(NB):
            ys, bys = YS[yi % 2], bYS[yi % 2]
            yi += 1
            for half in range(2):
                pd, bpd = bk[5 + half][:], bb[5 + half]
                for fc in range(4):
                    P.pe(lambda eng, i=i, b=b, fc=fc, half=half, pd=pd: eng.matmul(
                        pd, lhsT=HT[:, fc, b * 128:(b + 1) * 128], rhs=WD[i][:, fc, half * 512:(half + 1) * 512],
                        start=(fc == 0), stop=(fc == 3)), reads=[bHT[fc], bWD[i]], writes=[bpd])
                if half == 0:
                    P.act(lambda eng, pd=pd, ys=ys: eng.activation(out=ys[:, 0:512], in_=pd, func=AF.Copy), reads=[bpd], writes=[bys])
                else:
                    P.dve(lambda eng, pd=pd, ys=ys: eng.tensor_copy(out=ys[:, 512:1024], in_=pd), reads=[bpd], writes=[bys])
            r0 = e * CAP + b * 128
            by = Buf("ybuf")
            self.bybuf.append(by)
            P.dma(lambda eng, r0=r0, ys=ys: eng.dma_start(out=yb[r0:r0 + 128, :], in_=ys), reads=[bys], writes=[by])
    A.reset(m0)
    P.barrier()


def _pass5(self):
    nc, P, A = self.nc, self.P, self.A
    d = self.d
    self.din("final_norm_g", [D])
    out = self.dout("out", [self.nfull_tok, D], F32)
    m0 = A.mark()
    ntile = self.nfull_tok // 128
    FG = A.alloc((D,), F32)
    bFG = Buf()
    P.dma(lambda e: e.dma_start(out=FG, in_=d["final_norm_g"].partition_broadcast(128)), writes=[bFG])
    NB5 = 4
    Y1 = [A.alloc((D,), F32) for _ in range(NB5)]
    Y2 = [A.alloc((D,), F32) for _ in range(NB5)]
    HH = [A.alloc((D,), F32) for _ in range(NB5)]
    OT = [A.alloc((D,), F32) for _ in range(NB5)]
    bY1, bY2, bHH, bOT = ([Buf() for _ in range(NB5)], [Buf() for _ in range(NB5)], [Buf() for _ in range(NB5)],
                          [Buf() for _ in range(NB5)])
    JK = A.alloc((D,), BF16)
    SS = A.alloc((ntile,), F32)
    bSS = Buf()
    yb = d["y_buf"]
    for gt in range(ntile):
        i = gt % NB5
        P.dma(lambda e, gt=gt, i=i: e.indirect_dma_start(
            out=Y1[i], out_offset=None, in_=yb, in_offset=bass.IndirectOffsetOnAxis(ap=self.DST[:, gt, 0:1], axis=0)),
            reads=self.bybuf + [self.bDST], writes=[bY1[i]], q="pool")
        P.dma(lambda e, gt=gt, i=i: e.indirect_dma_start(
            out=Y2[i], out_offset=None, in_=yb, in_offset=bass.IndirectOffsetOnAxis(ap=self.DST[:, gt, 1:2], axis=0)),
            reads=self.bybuf + [self.bDST], writes=[bY2[i]], q="pool")
        P.dma(lambda e, gt=gt, i=i: e.dma_start(out=HH[i], in_=d["h2_s"][gt * 128:(gt + 1) * 128, :]),
              reads=[self.bh2s[gt]], writes=[bHH[i]])
        P.dve(lambda e, gt=gt, i=i: e.scalar_tensor_tensor(out=HH[i], in0=Y1[i], scalar=self.W12[:, gt, 0:1], in1=HH[i],
                                                          op0=ALU.mult, op1=ALU.add),
              reads=[bY1[i], bHH[i], self.bW12], writes=[bHH[i]])
        P.dve(lambda e, gt=gt, i=i: e.scalar_tensor_tensor(out=HH[i], in0=Y2[i], scalar=self.W12[:, gt, 1:2], in1=HH[i],
                                                          op0=ALU.mult, op1=ALU.add),
              reads=[bY2[i], bHH[i], self.bW12], writes=[bHH[i]])
        P.act(lambda e, gt=gt, i=i: e.activation(out=JK, in_=HH[i], func=AF.Square, accum_out=SS[:, gt:gt + 1]),
              reads=[bHH[i]], writes=[bSS])
        P.act(lambda e, gt=gt: e.activation(out=SS[:, gt:gt + 1], in_=SS[:, gt:gt + 1], func=AF.Ln, scale=1.0 / D, bias=EPS),
              reads=[bSS], writes=[bSS])
        P.act(lambda e, gt=gt: e.activation(out=SS[:, gt:gt + 1], in_=SS[:, gt:gt + 1], func=AF.Exp, scale=-0.5),
              reads=[bSS], writes=[bSS])
        P.dve(lambda e, gt=gt, i=i: e.scalar_tensor_tensor(out=OT[i], in0=HH[i], scalar=SS[:, gt:gt + 1], in1=FG,
                                                          op0=ALU.mult, op1=ALU.mult),
              reads=[bHH[i], bSS, bFG], writes=[bOT[i]])
        P.dma(lambda e, gt=gt, i=i: e.dma_start(out=out[gt * 128:(gt + 1) * 128, :], in_=OT[i]), reads=[bOT[i]], out=True)
    A.reset(m0)


KB.pass4 = _pass4
KB.pass5 = _pass5


NPRE_SB = 17
NFULL_SB = 16
_NC_CACHE = {}


def _build_full():
    if "nc" not in _NC_CACHE:
        kb = KB(NPRE_SB, NFULL_SB)
        kb.setup()
        kb.pass1()
        kb.pass2()
        kb.pass3()
        kb.pass4()
        kb.pass5()
        _NC_CACHE["nc"] = kb.finish()
    return _NC_CACHE["nc"]


def kernel(x, meta_tokens, hg_lb_logits, norm_mix_g, w_in, gd_conv_w, gd_A_log, gd_dt_bias, hg_norm_g, gd_norm_g,
           hg_up, gd_up, w_out, norm_ffn_g, router_group_w, router_group_b, router_expert_w, router_expert_b,
           w_gate, w_up, w_down, final_norm_g):
    f32 = np.float32
    c = lambda a: np.ascontiguousarray(np.asarray(a, dtype=f32))
    x = c(x)
    meta = c(meta_tokens)
    B, S, _ = x.shape
    half = S // 2
    npre_tok = NPRE_SB * SBT
    ntok = (NPRE_SB + NFULL_SB) * SBT
    nmeta = meta.shape[0]
    shared = {
        "w_in": c(w_in[0]),
        "norm_mix_g": c(norm_mix_g[0]),
        "hg_lb": c(np.asarray(hg_lb_logits, f32).reshape(2, 4, 128).transpose(2, 0, 1)),
        "hg_norm_g": c(hg_norm_g[0]),
        "conv_wT": c(np.asarray(gd_conv_w[0], f32).reshape(4, 12, 128).transpose(2, 0, 1)),
        "gd_A_log": c(gd_A_log[0]),
        "gd_dt_bias": c(gd_dt_bias[0]),
        "gd_norm_g": c(gd_norm_g[0]),
        "hg_up": c(hg_up[0]),
        "gd_up": c(gd_up[0]),
        "w_out": c(w_out[0]),
        "norm_ffn_g": c(norm_ffn_g[0]),
        "router_w": c(np.concatenate([np.asarray(router_group_w[0], f32), np.asarray(router_expert_w[0], f32)], axis=1)),
        "router_b": c(np.concatenate([np.asarray(router_group_b[0], f32), np.asarray(router_expert_b[0], f32)])),
        "w_gate": c(w_gate[0]),
        "w_up": c(w_up[0]),
        "w_down": c(w_down[0]),
        "final_norm_g": c(final_norm_g),
    }
    in_maps = []
    for core in range(2 * B):
        b, hf = core // 2, core % 2
        xs = np.zeros((ntok, D), f32)
        if hf == 0:
            xs[npre_tok - nmeta:npre_tok] = meta
            xs[npre_tok:] = x[b, 0:half]
        else:
            xs[npre_tok - half - nmeta:npre_tok - half] = meta
            xs[npre_tok - half:] = x[b]
        m = dict(shared)
        m["xs"] = xs
        in_maps.append(m)
    nc = _build_full()
    res = run_bass_kernel_spmd(nc, in_maps, core_ids=list(range(2 * B)))
    out = np.empty((B, S, D), f32)
    for core in range(2 * B):
        b, hf = core // 2, core % 2
        out[b, hf * half:(hf + 1) * half] = np.asarray(res.results[core]["out"], dtype=f32)
    return out
```

```python
import numpy as np
import concourse.bass as bass
import concourse.mybir as mybir
from concourse.bass_utils import run_bass_kernel_spmd

F32 = mybir.dt.float32
BF16 = mybir.dt.bfloat16
I32 = mybir.dt.int32
U32 = mybir.dt.uint32
AF = mybir.ActivationFunctionType
ALU = mybir.AluOpType
AX = mybir.AxisListType


class Buf:
    __slots__ = ("name", "lw", "rd", "psum")

    def __init__(self, name="", psum=False):
        self.name = name
        self.lw = None
        self.rd = {}
        self.psum = psum


class Prog:
    ENGS = ("pe", "act", "dve", "pool", "sp")

    def __init__(self, kdma=6):
        self.ops = {e: [] for e in self.ENGS}
        self.waited = {e: {} for e in self.ENGS}
        self.ndma = {e: 0 for e in self.ENGS}
        self.K = kdma
        self.out_toks = []
        self.pending = {e: [] for e in self.ENGS}

    def barrier(self):
        toks = []
        for e in self.ENGS:
            for i in range(len(self.ops[e]) - 1, -1, -1):
                op = self.ops[e][i]
                if (not op["dma"]) and op["fn"] is not None:
                    toks.append((e, i))
                    break
            n = self.ndma[e]
            for slot in range(min(self.K, n)):
                last = ((n - 1 - slot) // self.K) * self.K + slot
                toks.append((("dma", e, slot), 16 * (last // self.K + 1)))
        for e in self.ENGS:
            self.pending[e] = list(toks)

    cap = None
    COST = {"pe": 0.13, "act": 0.45, "dve": 0.42, "pool": 1.2, "sp": 0.05}
    DMA_LAT = 2.5
    LCOST = {}

    def _emit(self, eng, fn, reads, writes, dma=False, extra=()):
        if self.cap is not None:
            self.cap.append((eng, fn, tuple(reads), tuple(writes), dma, tuple(extra)))
            return None
        return self._emit_real(eng, fn, reads, writes, dma, extra)

    def merge_streams(self, gens):
        lists = []
        for g in gens:
            self.cap = []
            for _ in g:
                pass
            lists.append(self.cap)
            self.cap = None
        ptr = [0] * len(lists)
        free = getattr(self, "_sim_free", None)
        if free is None:
            free = self._sim_free = {e: 0.0 for e in self.ENGS}
            self._sim_buf = {}
        bt = self._sim_buf
        rr = 0
        n = len(lists)
        while True:
            best, bi = None, -1
            for k in range(n):
                i = (rr + k) % n
                if ptr[i] >= len(lists[i]):
                    continue
                eng, fn, reads, writes, dma, extra = lists[i][ptr[i]]
                t = free[eng]
                for b in reads:
                    v = bt.get(id(b))
                    if v is not None and v[0] > t:
                        t = v[0]
                for b in writes:
                    v = bt.get(id(b))
                    if v is not None:
                        if v[0] > t:
                            t = v[0]
                        if v[1] > t:
                            t = v[1]
                if best is None or t < best - 1e-9:
                    best, bi = t, i
            if bi < 0:
                break
            rr = (bi + 1) % n
            eng, fn, reads, writes, dma, extra = lists[bi][ptr[bi]]
            ptr[bi] += 1
            ln = fn.__code__.co_firstlineno if fn is not None else -1
            c = self.LCOST.get((eng, ln), self.COST[eng])
            if dma:
                occ = 0.6 if eng == "pool" else 0.05
                fin = best + occ + self.DMA_LAT
                free[eng] = best + occ
            else:
                fin = best + c
                free[eng] = fin
            for b in writes:
                bt[id(b)] = [fin, fin]
            for b in reads:
                v = bt.get(id(b))
                if v is None:
                    bt[id(b)] = [0.0, fin]
                elif v[1] < fin:
                    v[1] = fin
                if b.psum and bt[id(b)][0] < fin:
                    bt[id(b)][0] = fin
            tok = self._emit_real(eng, fn, reads, writes, dma, extra)
            if dma and getattr(fn, "_is_out", False):
                self.out_toks.append(tok)

    def _emit_real(self, eng, fn, reads, writes, dma=False, extra=()):
        deps = {}

        def need(tok):
            if tok is None:
                return
            k, v = tok
            if eng == "pe" and k == "pe":
                return
            if deps.get(k, -1) < v:
                deps[k] = v
        def need_x(tok):
            if tok is not None and tok[0] != eng:
                need(tok)
        for b in reads:
            if b.psum:
                need_x(b.lw)
            else:
                need(b.lw)
        for b in writes:
            if b.psum:
                need_x(b.lw)
            else:
                need(b.lw)
                for k, v in b.rd.items():
                    need((k, v))
        for t in extra:
            need(t)
        if self.pending[eng]:
            for t in self.pending[eng]:
                need(t)
            self.pending[eng] = []
        if dma:
            i = self.ndma[eng]
            self.ndma[eng] += 1
            slot = i % self.K
            val = 16 * (i // self.K + 1)
            key = ("dma", eng, slot)
            if val > 16:
                need((key, val - 16))
            tok = (key, val)
        else:
            tok = (eng, len(self.ops[eng]))
        waits = []
        w = self.waited[eng]
        for k, v in deps.items():
            if w.get(k, -1) < v:
                w[k] = v
                waits.append((k, v))
        self.ops[eng].append(dict(waits=waits, fn=fn, tok=tok, dma=dma))
        for b in reads:
            if b.psum:
                b.lw = tok
                continue
            k, v = tok
            if b.rd.get(k, -1) < v:
                b.rd[k] = v
        for b in writes:
            b.lw = tok
            b.rd = {}
        return tok

    pe_dummy = None
    _pe_cnt = 0

    def pe(self, fn, reads=(), writes=()):
        t = self._emit("pe", fn, reads, writes)
        if self.pe_dummy is not None:
            dfn, every, buf = self.pe_dummy
            self._pe_cnt += 1
            if self._pe_cnt % every == 0:
                self._emit("pe", dfn, (), (buf,))
        return t

    def act(self, fn, reads=(), writes=()):
        return self._emit("act", fn, reads, writes)

    def dve(self, fn, reads=(), writes=()):
        return self._emit("dve", fn, reads, writes)

    def pool(self, fn, reads=(), writes=()):
        return self._emit("pool", fn, reads, writes)

    def dma(self, fn, reads=(), writes=(), q="sp", out=False):
        if out and self.cap is not None:
            try:
                fn._is_out = True
            except AttributeError:
                pass
        t = self._emit(q, fn, reads, writes, dma=True)
        if out and t is not None:
            self.out_toks.append(t)
        return t

    def finalize(self, nc):
        self._emit("sp", None, (), (), extra=self.out_toks)
        targets = {e: set() for e in self.ENGS}
        for e in self.ENGS:
            for op in self.ops[e]:
                for k, v in op["waits"]:
                    if isinstance(k, str):
                        targets[k].add(v)
        rank = {e: {} for e in self.ENGS}
        for e in self.ENGS:
            r = 0
            for i, op in enumerate(self.ops[e]):
                if (not op["dma"]) and i in targets[e]:
                    assert op["fn"] is not None
                    r += 1
                    rank[e][i] = r
        import contextlib
        with contextlib.ExitStack() as st:
            csem = {e: st.enter_context(nc.semaphore("c_" + e)) for e in self.ENGS}
            dsem = {}
            for e in self.ENGS:
                if self.ndma[e] > 0:
                    for s in range(min(self.K, self.ndma[e])):
                        dsem[("dma", e, s)] = st.enter_context(nc.semaphore("d_%s_%d" % (e, s)))
            block = st.enter_context(nc.Block())

            def run(e):
                def body(engine):
                    for i, op in enumerate(self.ops[e]):
                        for k, v in op["waits"]:
                            if isinstance(k, str):
                                engine.wait_ge(csem[k], rank[k][v])
                            else:
                                engine.wait_ge(dsem[k], v)
                        if op["fn"] is None:
                            continue
                        ins = op["fn"](engine)
                        if op["dma"]:
                            ins.then_inc(dsem[op["tok"][0]], 16)
                        elif i in rank[e]:
                            ins.then_inc(csem[e], 1)
                return body
            block.tensor(run("pe"))
            block.scalar(run("act"))
            block.vector(run("dve"))
            block.gpsimd(run("pool"))
            block.sync(run("sp"))


D = 1024
KC = 8
SBT = 256
NT = SBT // 128
NCH = SBT // 64
EPS = 1e-6
C_HQ, C_HF, C_HI, C_HG = 0, 512, 1024, 1536
C_GQ, C_GK, C_GV, C_GZ = 2048, 2560, 3072, 3584
C_GB, C_GA, C_PA, C_PB = 4096, 4100, 4104, 5128
DPROJ = 6152


class Arena:
    def __init__(self, ap, words):
        self.ap = ap
        self.words = words
        self.off = 0
        self.peak = 0

    def mark(self):
        return self.off

    def reset(self, m):
        self.off = m

    def alloc(self, free_shape, dt):
        n = 1
        for s in free_shape:
            n *= s
        esz = 4 if dt in (F32, I32, U32) else 2
        words = (n * esz + 3) // 4
        words = (words + 7) // 8 * 8
        assert self.off + words <= self.words, ("arena overflow", self.off, words, self.words)
        v = self.ap[:, self.off:self.off + words]
        self.off += words
        self.peak = max(self.peak, self.off)
        if esz == 2:
            v = v.bitcast(dt)
        elif dt != F32:
            v = v.bitcast(dt)
        v = v[:, 0:n]
        if len(free_shape) > 1:
            names = ["a%d" % i for i in range(len(free_shape))]
            pat = "p (%s) -> p %s" % (" ".join(names), " ".join(names))
            v = v.rearrange(pat, **{nm: s for nm, s in zip(names, free_shape)})
        return v


import os as _os_ls
LS = int(_os_ls.environ.get("LISTSCHED", "1"))


class KB:
    def __init__(self, npre, nfull, debug=()):
        import contextlib
        self.npre, self.nfull = npre, nfull
        self.nsb = npre + nfull
        self.ntok = self.nsb * SBT
        self.nfull_tok = nfull * SBT
        self.debug = set(debug)
        self.nc = bass.Bass("TRN2", target_bir_lowering=False)
        self.P = Prog()
        self.d = {}
        self.st = contextlib.ExitStack()

    def din(self, name, shape, dt=F32):
        self.d[name] = self.nc.dram_tensor(name, list(shape), dt, kind="ExternalInput").ap()
        return self.d[name]

    def dout(self, name, shape, dt=F32):
        self.d[name] = self.nc.dram_tensor(name, list(shape), dt, kind="ExternalOutput").ap()
        return self.d[name]

    def setup(self):
        nc, P, st = self.nc, self.P, self.st
        self.din("xs", [self.ntok, D])
        self.din("w_in", [D, DPROJ])
        self.din("norm_mix_g", [D])
        self.din("hg_lb", [128, 2, 4])
        self.din("hg_norm_g", [128])
        AW = 51200
        arena_t = st.enter_context(nc.sbuf_tensor("arena", [128, AW], F32))
        self.A = Arena(arena_t[:], AW)
        self.banks = [st.enter_context(nc.psum_tensor("pb%d" % i, [128, 512], F32)) for i in range(8)]
        self.bbank = [Buf("pb%d" % i, psum=True) for i in range(8)]
        A = self.A
        self.identf = A.alloc((128,), F32)
        self.ident = A.alloc((128,), BF16)
        self.ones_bf = A.alloc((128,), BF16)
        self.ones_f = A.alloc((512,), F32)
        self.mask2 = A.alloc((128,), F32)
        self.bconst = Buf("const")
        bc = self.bconst
        P.pool(lambda e: e.memset(self.identf, 0.0), writes=[bc])
        P.pool(lambda e: e.affine_select(out=self.identf, in_=self.identf, pattern=[[-1, 128]],
                                         compare_op=ALU.not_equal, fill=1.0, base=0, channel_multiplier=1),
               reads=[bc], writes=[bc])
        P.pool(lambda e: e.tensor_copy(out=self.ident, in_=self.identf), reads=[bc], writes=[bc])
        P.pool(lambda e: e.memset(self.ones_bf, 1.0), writes=[bc])
        P.pool(lambda e: e.memset(self.ones_f, 1.0), writes=[bc])
        P.pool(lambda e: e.memset(self.mask2, 1.0), writes=[bc])
        P.pool(lambda e: e.affine_select(out=self.mask2, in_=self.mask2, pattern=[[1, 128]],
                                         compare_op=ALU.is_ge, fill=0.0, base=0, channel_multiplier=-1),
               reads=[bc], writes=[bc])
        P.pool(lambda e: e.memset(self.mask2[0:64, 64:128], 0.0), reads=[bc], writes=[bc])
        self.xt = [A.alloc((D,), F32) for _ in range(2)]
        self.bxt = [Buf("xt%d" % i) for i in range(2)]
        self.junk = A.alloc((D,), BF16)
        self.bjunk = Buf("junk")
        self.ss = A.alloc((NT,), F32)
        self.rstd = A.alloc((NT,), F32)
        self.bss = Buf("ss")
        self.brstd = Buf("rstd")
        self.xsb = [A.alloc((D,), BF16) for _ in range(2)]
        self.bxsb = [Buf("xsb%d" % i) for i in range(2)]
        self.gbc = A.alloc((D,), F32)
        self.bgbc = Buf("gbc")
        self.nxt = 0
        self.d["x_buf"] = nc.dram_tensor("x_buf", [NSLOT, D], BF16, kind="Internal").ap()
        zt = A.alloc((D,), BF16)
        self.bxzero = Buf("xzero")
        P.pool(lambda e: e.memset(zt, 0.0), writes=[bc])
        xbv = self.d["x_buf"].rearrange("(b p) n -> p b n", p=128)
        nblk = NSLOT // 128
        step = 12
        self.bxz = []
        for b0 in range(0, nblk, step):
            bz = Buf("xz")
            self.bxz.append(bz)
            P.dma(lambda e, b0=b0: e.dma_start(out=xbv[:, b0:b0 + step, :],
                                               in_=zt.unsqueeze(1).to_broadcast([128, step, D])),
                  reads=[bc], writes=[bz])

    def stage_a(self, *a, **k):
        for _ in self.stage_a_gen(*a, **k):
            pass

    def stage_a_gen(self, sb, xnT, bxnT, ptr, bptr, gname="norm_mix_g", src="xs", tok_base=0, keep=None):
        nc, P = self.nc, self.P
        xs_d = self.d[src]
        tiles = []
        for t in range(NT):
            i = self.nxt % 2
            self.nxt += 1
            tok0 = tok_base + sb * SBT + t * 128
            xt, bxt = self.xt[i], self.bxt[i]
            P.dma(lambda e, xt=xt, tok0=tok0: e.dma_start(out=xt, in_=xs_d[tok0:tok0 + 128, :]), writes=[bxt])
            P.act(lambda e, xt=xt, t=t: e.activation(out=self.junk, in_=xt, func=AF.Square,
                                                     accum_out=self.ss[:, t:t + 1]),
                  reads=[bxt], writes=[self.bss])
            tiles.append((xt, bxt, i))
            if t % 2 == 1:
                t0 = t - 1
                P.act(lambda e, t0=t0: e.activation(out=self.rstd[:, t0:t0 + 2], in_=self.ss[:, t0:t0 + 2],
                                                    func=AF.Ln, scale=1.0 / D, bias=EPS),
                      reads=[self.bss], writes=[self.brstd])
                P.act(lambda e, t0=t0: e.activation(out=self.rstd[:, t0:t0 + 2], in_=self.rstd[:, t0:t0 + 2],
                                                    func=AF.Exp, scale=-0.5),
                      reads=[self.brstd], writes=[self.brstd])
                for tt in (t0, t):
                    xt2, bxt2, i2 = tiles[tt]
                    xsb, bxsb = self.xsb[i2], self.bxsb[i2]
                    P.dve(lambda e, xt2=xt2, xsb=xsb, tt=tt: e.scalar_tensor_tensor(
                        out=xsb, in0=xt2, scalar=self.rstd[:, tt:tt + 1], in1=self.gbc,
                        op0=ALU.mult, op1=ALU.mult),
                        reads=[bxt2, self.brstd, self.bgbc], writes=[bxsb])
                    for k in range(KC):
                        P.pe(lambda e, k=k, xsb=xsb: e.transpose(out=ptr[:, k, :], in_=xsb[:, k * 128:(k + 1) * 128],
                                                                 identity=self.ident),
                             reads=[bxsb, self.bconst], writes=[bptr])
                    P.dve(lambda e, tt=tt: e.tensor_copy(out=xnT[:, :, tt * 128:(tt + 1) * 128], in_=ptr),
                          reads=[bptr], writes=[bxnT])
                    yield


    def xnt_fetch_gen(self, sb, last_sb, xnTs, bxnTs):
        P = self.P
        src = self.d["xnT_s"]
        if sb not in self._xfetched:
            self._xfetched.add(sb)
            P.dma(lambda e, sb=sb: e.dma_start(out=xnTs[sb % 2], in_=src[:, :, sb * SBT:(sb + 1) * SBT]),
                  reads=[self.bxns[sb]], writes=[bxnTs[sb % 2]])
        nx = sb + 1
        if nx <= last_sb and nx not in self._xfetched:
            self._xfetched.add(nx)
            P.dma(lambda e, nx=nx: e.dma_start(out=xnTs[nx % 2], in_=src[:, :, nx * SBT:(nx + 1) * SBT]),
                  reads=[self.bxns[nx]], writes=[bxnTs[nx % 2]])
        yield

    def load_gain(self, gname):
        P = self.P
        g = self.d[gname]
        P.dma(lambda e: e.dma_start(out=self.gbc, in_=g.partition_broadcast(128)), writes=[self.bgbc])

    def pass1(self):
        nc, P, A = self.nc, self.P, self.A
        d = self.d
        H = 4
        self.W2 = A.alloc((KC, 2056), BF16)
        self.bW2 = [Buf("W2_%d" % k) for k in range(KC)]
        m0 = A.mark()
        self.OA = A.alloc((H, SBT), BF16)
        self.bOA = [Buf("OA%d" % h) for h in range(H)]
        W1 = A.alloc((KC, 2048), BF16)
        bW1 = [Buf("W1_%d" % k) for k in range(KC)]
        wv = d["w_in"].rearrange("(k p) n -> p k n", p=128)
        for k in range(KC):
            P.dma(lambda e, k=k: e.dma_start(out=W1[:, k, :], in_=wv[:, k, 0:2048]), writes=[bW1[k]], q="pool")
        for k in range(KC):
            P.dma(lambda e, k=k: e.dma_start(out=self.W2[:, k, :], in_=wv[:, k, 2048:2048 + 2056]), writes=[self.bW2[k]], q="pool")
        self.load_gain("norm_mix_g")
        lraw = A.alloc((2, H), F32)
        lb = A.alloc((H,), F32)
        oml = A.alloc((H,), F32)
        hgn = A.alloc((1,), F32)
        blb = Buf("lb")
        P.dma(lambda e: e.dma_start(out=lraw, in_=d["hg_lb"]), writes=[blb])
        P.dma(lambda e: e.dma_start(out=hgn, in_=d["hg_norm_g"].rearrange("(p o) -> p o", o=1)), writes=[blb])
        P.dve(lambda e: e.tensor_tensor(out=lb, in0=lraw[:, 0, :], in1=lraw[:, 1, :], op=ALU.subtract),
              reads=[blb], writes=[blb])
        P.act(lambda e: e.activation(out=oml, in_=lb, func=AF.Sigmoid, scale=-1.0), reads=[blb], writes=[blb])
        P.act(lambda e: e.activation(out=lb, in_=lb, func=AF.Sigmoid), reads=[blb], writes=[blb])

        xnT = A.alloc((KC, SBT), BF16)
        bxnT = Buf("xnT")
        Fb = A.alloc((H, SBT), F32)
        CS = A.alloc((H, SBT), F32)
        Kb = A.alloc((H, SBT), BF16)
        EB = A.alloc((H, SBT), BF16)
        ENB = A.alloc((H, SBT), BF16)
        EBEs = [A.alloc((H, NCH), F32) for _ in range(2)]
        QTs = [A.alloc((H, SBT), BF16) for _ in range(2)]
        KTs = [A.alloc((H, SBT), BF16) for _ in range(2)]
        KH = A.alloc((H, SBT), BF16)
        KHTs = [A.alloc((NT, 512), BF16) for _ in range(2)]
        Vs = [A.alloc((NT, 512), BF16) for _ in range(2)]
        Gs = [A.alloc((H, SBT), BF16) for _ in range(2)]
        O32 = A.alloc((H, SBT), F32)
        OSQ = A.alloc((H, SBT), BF16)
        LNV = A.alloc((SBT,), F32)
        ATS = A.alloc((H, 128), BF16)
        S32 = A.alloc((H, 128), F32)
        SBF = [A.alloc((H, 128), BF16) for _ in range(2)]
        bF = [Buf() for _ in range(H)]
        bCS = [Buf() for _ in range(H)]
        bK = Buf()
        bEB = [Buf() for _ in range(H)]
        bENB = [Buf() for _ in range(H)]
        bEBEs = [Buf(), Buf()]
        bQTs = [[Buf() for _ in range(H)] for _ in range(2)]
        bKTs = [Buf(), Buf()]
        bKH = Buf()
        bKHTs = [[Buf() for _ in range(NT)] for _ in range(2)]
        bVs = [[Buf() for _ in range(NT)] for _ in range(2)]
        bGs = [[Buf() for _ in range(H)] for _ in range(2)]
        bO32 = Buf()
        bOSQ = [Buf() for _ in range(H)]
        bLNV = Buf()
        bATS = Buf()
        bS32 = [Buf() for _ in range(H)]
        bSBF = [[Buf() for _ in range(H)] for _ in range(2)]
        sbf_i = [0] * H

        bk = self.banks
        bb = self.bbank
        ptr = bk[0][:].bitcast(BF16).rearrange("p (k t) -> p k t", k=KC)
        pkt = bk[1][:].bitcast(BF16)[:, 0:512]
        pj = [bk[2][:], bk[3][:]]
        bpj = [bb[2], bb[3]]
        pat = bk[4][:].rearrange("p (h t) -> p h t", h=H)
        po = bk[5][:].rearrange("p (h t) -> p h t", h=H)
        pS = [bk[6 + (h % 2)][:, 0:128] for h in range(H)]
        bpS = [bb[6 + (h % 2)] for h in range(H)]
        pji = [0]

        def nextpj():
            i = pji[0] % 2
            pji[0] += 1
            return pj[i], bpj[i]

        for h in range(H):
            P.pool(lambda e, h=h: e.memset(S32[:, h, :], 0.0), writes=[bS32[h]])
            P.pool(lambda e, h=h: e.memset(SBF[0][:, h, :], 0.0), writes=[bSBF[0][h]])

        def proj_fm(col0, h):
            p, bp = nextpj()
            for k in range(KC):
                P.pe(lambda e, k=k, p=p: e.matmul(p[:, 0:SBT], lhsT=W1[:, k, col0 + h * 128:col0 + (h + 1) * 128],
                                                  rhs=xnT[:, k, :], start=(k == 0), stop=(k == KC - 1)),
                     reads=[bW1[k], bxnT], writes=[bp])
            return p, bp

        def X1(sb):
            sl = sb % 2
            QT, KT, KHT, V, G, EBE = QTs[sl], KTs[sl], KHTs[sl], Vs[sl], Gs[sl], EBEs[sl]
            bQT, bKT, bKHT, bV, bG, bEBE = bQTs[sl], bKTs[sl], bKHTs[sl], bVs[sl], bGs[sl], bEBEs[sl]
            full = sb >= self.npre
            yield from self.stage_a_gen(sb, xnT, bxnT, ptr, bb[0])
            if "xnT_s" not in self.d:
                self.d["xnT_s"] = self.nc.dram_tensor("xnT_s", [128, KC, self.ntok], BF16, kind="Internal").ap()
                self.bxns = {}
            bx_ = Buf("xns")
            self.bxns[sb] = bx_
            P.dma(lambda e, sb=sb: e.dma_start(out=self.d["xnT_s"][:, :, sb * SBT:(sb + 1) * SBT], in_=xnT),
                  reads=[bxnT], writes=[bx_])
            for h in range(H):
                p, bp = proj_fm(C_HF, h)
                P.act(lambda e, h=h, p=p: e.activation(out=Fb[:, h, :], in_=p[:, 0:SBT], func=AF.Sigmoid),
                      reads=[bp], writes=[bF[h]])
                yield
            if full:
                for h in range(H):
                    p, bp = proj_fm(C_HQ, h)
                    P.act(lambda e, h=h, p=p: e.activation(out=QT[:, h, :], in_=p[:, 0:SBT], func=AF.Silu),
                          reads=[bp], writes=[bQT[h]])
                    yield
                for h in range(H):
                    p, bp = proj_fm(C_HG, h)
                    P.act(lambda e, h=h, p=p: e.activation(out=G[:, h, :], in_=p[:, 0:SBT], func=AF.Silu),
                          reads=[bp], writes=[bG[h]])
                    yield
            for h in range(H):
                P.dve(lambda e, h=h: e.tensor_scalar(out=Fb[:, h, :], in0=Fb[:, h, :], scalar1=oml[:, h:h + 1],
                                                     scalar2=lb[:, h:h + 1], op0=ALU.mult, op1=ALU.add),
                      reads=[bF[h], blb], writes=[bF[h]])
            P.dve(lambda e: e.tensor_scalar(out=Kb, in0=Fb, scalar1=-1.0, scalar2=1.0, op0=ALU.mult, op1=ALU.add),
                  reads=bF, writes=[bK])
            for h in range(H):
                P.act(lambda e, h=h: e.activation(out=Fb[:, h, :], in_=Fb[:, h, :], func=AF.Ln),
                      reads=[bF[h], bK], writes=[bF[h]])
            for h in range(H):
                P.dve(lambda e, h=h: e.tensor_tensor_scan(out=CS[:, h, :], data0=self.ones_f[:, 0:SBT],
                                                          data1=Fb[:, h, :], initial=0.0,
                                                          op0=ALU.mult, op1=ALU.add),
                      reads=[bF[h], self.bconst], writes=[bCS[h]])
                yield
            if not full:
                for h in range(H):
                    P.act(lambda e, h=h: e.activation(out=ENB[:, h, :], in_=CS[:, h, :], func=AF.Exp, scale=-1.0,
                                                      bias=CS[:, h, SBT - 1:SBT]),
                          reads=[bCS[h]], writes=[bENB[h]])
                    yield
                P.act(lambda e: e.activation(out=EBE[:, :, 0], in_=CS[:, :, SBT - 1], func=AF.Exp), reads=bCS, writes=[bEBE])
                P.dve(lambda e: e.tensor_tensor(out=KH, in0=Kb, in1=ENB, op=ALU.mult), reads=[bK] + bENB, writes=[bKH])
            else:
                Fb4 = Fb.rearrange("p h (c t) -> p h c t", c=NCH)
                CS4 = CS.rearrange("p h (c t) -> p h c t", c=NCH)
                P.dve(lambda e: e.tensor_tensor(out=Fb4[:, :, 1:NCH, :], in0=CS4[:, :, 1:NCH, :],
                                                in1=CS4[:, :, 0:NCH - 1, 63:64].to_broadcast([128, H, NCH - 1, 64]),
                                                op=ALU.subtract),
                      reads=bCS + bF, writes=bF)
                P.dve(lambda e: e.tensor_copy(out=Fb4[:, :, 0, :], in_=CS4[:, :, 0, :]), reads=bCS + bF, writes=bF)
                for h in range(H):
                    P.act(lambda e, h=h: e.activation(out=ENB[:, h, :], in_=Fb[:, h, :], func=AF.Exp, scale=-1.0),
                          reads=[bF[h]], writes=[bENB[h]])
                    yield
                P.act(lambda e: e.activation(out=EBE, in_=Fb4[:, :, :, 63], func=AF.Exp), reads=bF, writes=[bEBE])
                if full:
                    for h in range(H):
                        P.act(lambda e, h=h: e.activation(out=EB[:, h, :], in_=Fb[:, h, :], func=AF.Exp),
                              reads=[bF[h]], writes=[bEB[h]])
                P.dve(lambda e: e.tensor_tensor(out=KT, in0=Kb, in1=ENB, op=ALU.mult), reads=[bK] + bENB, writes=[bKT])
                KT4 = KT.rearrange("p h (c t) -> p h c t", c=NCH)
                KH4 = KH.rearrange("p h (c t) -> p h c t", c=NCH)
                P.dve(lambda e: e.tensor_tensor(out=KH4, in0=KT4,
                                                in1=EBE.unsqueeze(3).to_broadcast([128, H, NCH, 64]), op=ALU.mult),
                      reads=[bKT, bEBE], writes=[bKH])
                if full:
                    P.dve(lambda e: e.tensor_tensor(out=QT, in0=QT, in1=EB, op=ALU.mult), reads=bQT + bEB, writes=bQT)
            for t in range(NT):
                p, bp = nextpj()
                for k in range(KC):
                    P.pe(lambda e, k=k, p=p, t=t: e.matmul(p, lhsT=xnT[:, k, t * 128:(t + 1) * 128],
                                                           rhs=W1[:, k, C_HI:C_HI + 512],
                                                           start=(k == 0), stop=(k == KC - 1)),
                         reads=[bW1[k], bxnT], writes=[bp])
                P.act(lambda e, p=p, t=t: e.activation(out=V[:, t, :], in_=p, func=AF.Copy), reads=[bp], writes=[bV[t]])
                for h in range(H):
                    P.pe(lambda e, h=h, t=t: e.transpose(out=pkt[:, h * 128:(h + 1) * 128],
                                                         in_=KH[:, h, t * 128:(t + 1) * 128], identity=self.ident),
                         reads=[bKH, self.bconst], writes=[bb[1]])
                P.dve(lambda e, t=t: e.tensor_copy(out=KHT[:, t, :], in_=pkt), reads=[bb[1]], writes=[bKHT[t]])
                yield
        def Y1(sb):
            full = sb >= self.npre
            sl = sb % 2
            QT, KT, KHT, V, G, EBE = QTs[sl], KTs[sl], KHTs[sl], Vs[sl], Gs[sl], EBEs[sl]
            bQT, bKT, bKHT, bV, bG, bEBE = bQTs[sl], bKTs[sl], bKHTs[sl], bVs[sl], bGs[sl], bEBEs[sl]
            if not full:
                for h in range(H):
                    for t in range(NT):
                        P.pe(lambda e, h=h, t=t: e.matmul(pS[h], lhsT=KHT[:, t, h * 128:(h + 1) * 128],
                                                          rhs=V[:, t, h * 128:(h + 1) * 128],
                                                          start=(t == 0), stop=(t == NT - 1)),
                             reads=[bKHT[t], bV[t]], writes=[bpS[h]])
                    P.dve(lambda e, h=h: e.scalar_tensor_tensor(
                        out=S32[:, h, :], in0=S32[:, h, :], scalar=EBE[:, h, 0:1], in1=pS[h],
                        op0=ALU.mult, op1=ALU.add),
                        reads=[bS32[h], bEBE, bpS[h]], writes=[bS32[h]])
                    cur = sbf_i[h]
                    nxt = 1 - cur
                    P.act(lambda e, h=h, nxt=nxt: e.activation(out=SBF[nxt][:, h, :], in_=S32[:, h, :], func=AF.Copy),
                          reads=[bS32[h]], writes=[bSBF[nxt][h]])
                    sbf_i[h] = nxt
                    yield
                return
            for j in range(NT):
                c0 = j * 128
                if full:
                    for h in range(H):
                        P.pe(lambda e, h=h, c0=c0: e.matmul(pat[:, h, :], lhsT=KT[:, h, c0:c0 + 128],
                                                            rhs=QT[:, h, c0:c0 + 128], start=True, stop=True),
                             reads=[bKT, bQT[h]], writes=[bb[4]])
                    P.dve(lambda e: e.tensor_tensor(out=ATS, in0=pat,
                                                    in1=self.mask2.unsqueeze(1).to_broadcast([128, H, 128]),
                                                    op=ALU.mult),
                          reads=[bb[4], self.bconst], writes=[bATS])
                    yield
                for half in range(2):
                    ch = 2 * j + half
                    r0 = half * 64
                    for h in range(H):
                        cur = sbf_i[h]
                        if full:
                            P.pe(lambda e, h=h, cur=cur, c0=c0, r0=r0: e.matmul(
                                po[:, h, r0:r0 + 64], lhsT=SBF[cur][:, h, :], rhs=QT[:, h, c0 + r0:c0 + r0 + 64],
                                start=(h == 0 and r0 == 0), stop=False, skip_group_check=True),
                                reads=[bSBF[cur][h], bQT[h]], writes=[bb[5]])
                        P.pe(lambda e, h=h, j=j, r0=r0: e.matmul(
                            pS[h], lhsT=KHT[r0:r0 + 64, j, h * 128:(h + 1) * 128],
                            rhs=V[r0:r0 + 64, j, h * 128:(h + 1) * 128], start=True, stop=True),
                            reads=[bKHT[j], bV[j]], writes=[bpS[h]])
                        P.dve(lambda e, h=h, ch=ch: e.scalar_tensor_tensor(
                            out=S32[:, h, :], in0=S32[:, h, :], scalar=EBE[:, h, ch:ch + 1], in1=pS[h],
                            op0=ALU.mult, op1=ALU.add),
                            reads=[bS32[h], bEBE, bpS[h]], writes=[bS32[h]])
                        nxt = 1 - cur
                        P.act(lambda e, h=h, nxt=nxt: e.activation(out=SBF[nxt][:, h, :], in_=S32[:, h, :], func=AF.Copy),
                              reads=[bS32[h]], writes=[bSBF[nxt][h]])
                        sbf_i[h] = nxt
                        yield
                if full:
                    for h in range(H):
                        P.pe(lambda e, h=h, j=j: e.matmul(po[:, h, :], lhsT=V[:, j, h * 128:(h + 1) * 128],
                                                          rhs=ATS[:, h, :], start=False, stop=True,
                                                          skip_group_check=True),
                             reads=[bV[j], bATS], writes=[bb[5]])
                    P.act(lambda e, c0=c0: e.activation(out=O32[:, :, c0:c0 + 128], in_=po, func=AF.Copy),
                          reads=[bb[5]], writes=[bO32])
                    yield
            if full:
                tok0 = (sb - self.npre) * SBT
                for h in range(H):
                    P.act(lambda e, h=h: e.activation(out=OSQ[:, h, :], in_=O32[:, h, :], func=AF.Square),
                          reads=[bO32], writes=[bOSQ[h]])
                for h in range(H):
                    pss, bpss = bk[4][:], bb[4]
                    P.pe(lambda e, h=h, pss=pss: e.matmul(pss[:, 0:SBT], lhsT=self.ones_bf, rhs=OSQ[:, h, :], start=True, stop=True),
                         reads=[bOSQ[h], self.bconst], writes=[bpss])
                    P.act(lambda e, pss=pss: e.activation(out=LNV, in_=pss[:, 0:SBT], func=AF.Ln, scale=1.0 / 128, bias=EPS),
                          reads=[bpss], writes=[bLNV])
                    P.act(lambda e: e.activation(out=LNV, in_=LNV, func=AF.Exp, scale=-0.5),
                          reads=[bLNV], writes=[bLNV])
                    P.dve(lambda e, h=h: e.tensor_tensor(out=O32[:, h, :], in0=O32[:, h, :], in1=LNV, op=ALU.mult),
                          reads=[bO32, bLNV], writes=[bO32])
                    P.dve(lambda e, h=h: e.scalar_tensor_tensor(
                        out=self.OA[:, h, :], in0=O32[:, h, :], scalar=hgn[:, 0:1], in1=G[:, h, :],
                        op0=ALU.mult, op1=ALU.mult),
                        reads=[bO32, blb, bG[h]], writes=[self.bOA[h]])
                    yield
                self.spill("oa_s", self.OA, self.bOA, sb)
                if "oa" in self.debug:
                    if "dbg_oa" not in self.d:
                        self.dout("dbg_oa", [128, H, self.nfull_tok], BF16)
                    P.dma(lambda e, tok0=tok0: e.dma_start(out=self.d["dbg_oa"][:, :, tok0:tok0 + SBT], in_=self.OA),
                          reads=self.bOA, out=True)
        import os
        WTS1 = [int(v) for v in os.environ.get("IL_W1", "1,1").split(",")]

        def run_il(gens):
            gens = list(gens)
            while gens:
                for g_, w_ in list(gens):
                    for _ in range(w_):
                        try:
                            next(g_)
                        except StopIteration:
                            gens.remove((g_, w_))
                            break

        n = self.nsb
        for r in range(-1, n):
            gs = []
            if 0 <= r < n:
                gs.append((Y1(r), WTS1[0]))
            if 0 <= r + 1 < n:
                gs.append((X1(r + 1), WTS1[1]))
            if LS:
                P.merge_streams([g_ for g_, _w in gs])
            else:
                run_il(gs)
        A.reset(m0)
        P.barrier()

    def finish(self):
        self.P.finalize(self.nc)
        self.st.close()
        return self.nc


def _pass2(self):
    nc, P, A = self.nc, self.P, self.A
    d = self.d
    H = 4
    self.din("conv_wT", [128, 4, 12])
    self.din("gd_A_log", [4])
    self.din("gd_dt_bias", [4])
    self.din("gd_norm_g", [128])
    m0 = A.mark()
    self.OB = A.alloc((H, SBT), BF16)
    self.bOB = [Buf("OB%d" % h) for h in range(H)]
    NW = 2056
    if hasattr(self, "W2"):
        W2, bW2 = self.W2, self.bW2
    else:
        W2 = A.alloc((KC, NW), BF16)
        bW2 = [Buf("W2_%d" % k) for k in range(KC)]
        wv = d["w_in"].rearrange("(k p) n -> p k n", p=128)
        for k in range(KC):
            P.dma(lambda e, k=k: e.dma_start(out=W2[:, k, :], in_=wv[:, k, 2048:2048 + NW]), writes=[bW2[k]], q="pool")
    self.load_gain("norm_mix_g")
    cw = A.alloc((4, 12), F32)
    negA = A.alloc((H,), F32)
    dtb = A.alloc((H,), F32)
    gdn = A.alloc((1,), F32)
    bpar = Buf("par2")
    P.dma(lambda e: e.dma_start(out=cw, in_=d["conv_wT"]), writes=[bpar])
    P.dma(lambda e: e.dma_start(out=negA, in_=d["gd_A_log"].partition_broadcast(128)), writes=[bpar])
    P.dma(lambda e: e.dma_start(out=dtb, in_=d["gd_dt_bias"].partition_broadcast(128)), writes=[bpar])
    P.dma(lambda e: e.dma_start(out=gdn, in_=d["gd_norm_g"].rearrange("(p o) -> p o", o=1)), writes=[bpar])
    P.act(lambda e: e.activation(out=negA, in_=negA, func=AF.Exp), reads=[bpar], writes=[bpar])
    P.dve(lambda e: e.tensor_scalar(out=negA, in0=negA, scalar1=-1.0, scalar2=None, op0=ALU.mult),
          reads=[bpar], writes=[bpar])
    maskL = A.alloc((128,), F32)
    ch01 = A.alloc((2, 128), F32)
    bc = self.bconst
    P.pool(lambda e: e.memset(maskL, 1.0), writes=[bc])
    P.pool(lambda e: e.affine_select(out=maskL, in_=maskL, pattern=[[-1, 128]], compare_op=ALU.is_gt,
                                     fill=0.0, base=0, channel_multiplier=1), reads=[bc], writes=[bc])
    P.pool(lambda e: e.memset(maskL[64:128, 0:64], 0.0), reads=[bc], writes=[bc])
    bones = A.alloc((128,), F32)
    P.pool(lambda e: e.memset(bones, 0.0), writes=[bc])
    P.pool(lambda e: e.memset(bones[0:64, 0:64], 1.0), reads=[bc], writes=[bc])
    P.pool(lambda e: e.memset(bones[64:128, 64:128], 1.0), reads=[bc], writes=[bc])
    P.pool(lambda e: e.memset(ch01, 0.0), writes=[bc])
    P.pool(lambda e: e.memset(ch01[0:64, 0, :], 1.0), reads=[bc], writes=[bc])
    P.pool(lambda e: e.memset(ch01[64:128, 1, :], 1.0), reads=[bc], writes=[bc])

    xnTs = [A.alloc((KC, SBT), BF16) for _ in range(2)]
    bxnTs = [Buf("xnT0"), Buf("xnT1")]
    self._xfetched = set()
    use_fetch = hasattr(self, "bxns")
    XC = A.alloc((12, SBT + 3), BF16)
    DG = A.alloc((48, 128), BF16)
    bDG = Buf("DG")
    for _j in range(4):
        for _cb in range(12):
            P.dve(lambda e, _j=_j, _cb=_cb: e.tensor_scalar(out=DG[:, _j * 12 + _cb, :], in0=self.identf,
                                                           scalar1=cw[:, _j, _cb:_cb + 1], scalar2=None, op0=ALU.mult),
                  reads=[bpar, self.bconst], writes=[bDG])
    bXC = [Buf() for _ in range(12)]
    CV = [A.alloc((SBT,), F32) for _ in range(2)]
    bCV = [Buf(), Buf()]
    QK32 = A.alloc((8, SBT), F32)
    bQK32 = [Buf() for _ in range(8)]
    NQ = 4
    SQs = [A.alloc((SBT,), BF16) for _ in range(NQ)]
    bSQs = [Buf() for _ in range(NQ)]
    RSs = [A.alloc((SBT,), F32) for _ in range(NQ)]
    bRSs = [Buf() for _ in range(NQ)]
    sqi = [0]
    NS = 3
    QTs = [A.alloc((H, SBT), BF16) for _ in range(NS)]
    KTs = [A.alloc((H, SBT), BF16) for _ in range(NS)]
    VTs = [A.alloc((H, SBT), BF16) for _ in range(NS)]
    GZs = [A.alloc((H, SBT), BF16) for _ in range(NS)]
    bQTs = [[Buf() for _ in range(H)] for _ in range(NS)]
    bKTs = [[Buf() for _ in range(H)] for _ in range(NS)]
    bVTs = [[Buf() for _ in range(H)] for _ in range(NS)]
    bGZs = [[Buf() for _ in range(H)] for _ in range(NS)]
    BGraws = [A.alloc((NT, 8), F32) for _ in range(NS)]
    LNBs = [A.alloc((NT, H), F32) for _ in range(NS)]
    BETAs = [A.alloc((NT, H), F32) for _ in range(NS)]
    GGs = [A.alloc((NT, H), F32) for _ in range(NS)]
    bBGs = [Buf() for _ in range(NS)]
    PBUF = []
    for _j in range(NT):
        pb = dict(
            GB=A.alloc((H, 128), F32), bGB=Buf(),
            E1=A.alloc((H, 128), F32), bE1=Buf(),
            E2=A.alloc((H, 128), F32), bE2=Buf(),
            EGR=A.alloc((H, 128), BF16), bEGR=Buf(),
            Lm=[A.alloc((H, 128), BF16) for _ in range(2)], bLm=[Buf(), Buf()],
            Um=[A.alloc((H, 128), BF16) for _ in range(2)], bUm=[Buf(), Buf()],
            Xm=[A.alloc((H, 128), BF16) for _ in range(2)], bXm=[Buf(), Buf()],
            VTK=A.alloc((H, 128), BF16), bVTK=Buf(),
        )
        PBUF.append(pb)
    PCAR = []
    for _s in range(2):
        row = []
        for _j in range(NT):
            row.append(dict(
                SC=A.alloc((8, H), F32), bSC=Buf(),
                QTG=A.alloc((H, 128), BF16), bQTG=Buf(),
                TT=A.alloc((H, 128), BF16), bTT=Buf(),
                ATT=A.alloc((H, 128), BF16), bATT=Buf(),
                KHT=A.alloc((H, 128), BF16), bKHT=Buf(),
                KTP=A.alloc((H, 128), BF16), bKTP=Buf(),
                BV=A.alloc((H, 128), F32), bBV=Buf(),
            ))
        PCAR.append(row)
    R = A.alloc((H, 128), BF16)
    USB = A.alloc((H, 128), BF16)
    bR = Buf()
    bUSB = Buf()
    S32 = A.alloc((H, 128), F32)
    SBF = [A.alloc((H, 128), BF16) for _ in range(2)]
    bS32 = [Buf() for _ in range(H)]
    bSBF = [[Buf() for _ in range(H)] for _ in range(2)]
    sbf_i = [0]
    O32 = A.alloc((H, SBT), F32)
    bO32 = Buf()
    OSQ = A.alloc((H, SBT), BF16)
    bOSQ = [Buf() for _ in range(H)]
    LNV = A.alloc((SBT,), F32)
    bLNV = Buf()
    identb4 = A.alloc((H, 128), BF16)
    P.pool(lambda e: e.tensor_copy(out=identb4, in_=self.ident.unsqueeze(1).to_broadcast([128, H, 128])),
           reads=[bc], writes=[bc])

    bk, bb = self.banks, self.bbank
    ptr = bk[0][:].bitcast(BF16).rearrange("p (k t) -> p k t", k=KC)
    ptr4 = bk[0][:].bitcast(BF16)[:, 0:512].rearrange("p (h t) -> p h t", h=H)
    import os as _os
    DUM = int(_os.environ.get("PE_DUMMY", "0"))
    DUMN = int(_os.environ.get("PE_DUMMY_N", "128"))
    if DUM > 0:
        pj = [bk[1][:], bk[2][:]]
        bpj = [bb[1], bb[2]]
        dones = A.alloc((512,), BF16)
        P.pool(lambda e: e.memset(dones, 1.0), writes=[self.bconst])
        dbuf = Buf("dummy", psum=True)
        P.pe_dummy = (lambda e: e.matmul(bk[0][:, 0:DUMN], lhsT=self.ones_bf, rhs=dones[:, 0:DUMN], start=True, stop=True),
                      DUM, dbuf)
    else:
        pj = [bk[1][:], bk[2][:], bk[0][:]]
        bpj = [bb[1], bb[2], bb[0]]
    pA = bk[3][:].rearrange("p (h t) -> p h t", h=H)
    pB = bk[4][:].rearrange("p (h t) -> p h t", h=H)
    pBb = bk[4][:].bitcast(BF16)[:, 0:512].rearrange("p (h t) -> p h t", h=H)
    pC = bk[5][:].rearrange("p (h t) -> p h t", h=H)
    pR = bk[6][:].rearrange("p (h t) -> p h t", h=H)
    po = bk[7][:].rearrange("p (h t) -> p h t", h=H)
    pji = [0]

    def nextpj():
        i = pji[0] % len(pj)
        pji[0] += 1
        return pj[i], bpj[i]

    for h in range(H):
        P.pool(lambda e, h=h: e.memset(S32[:, h, :], 0.0), writes=[bS32[h]])
        P.pool(lambda e, h=h: e.memset(SBF[0][:, h, :], 0.0), writes=[bSBF[0][h]])
    for cb in range(12):
        P.pool(lambda e, cb=cb: e.memset(XC[:, cb, :], 0.0), writes=[bXC[cb]])

    def proj_fm(col0, xnT, bxnT):
        p, bp = nextpj()
        for k in range(KC):
            P.pe(lambda e, k=k, p=p: e.matmul(p[:, 0:SBT], lhsT=W2[:, k, col0:col0 + 128],
                                              rhs=xnT[:, k, :], start=(k == 0), stop=(k == KC - 1)),
                 reads=[bW2[k], bxnT], writes=[bp])
        return p, bp

    evi = [0]

    def evac(out, in_, reads, writes):
        i = evi[0]
        evi[0] += 1
        if i % 3 != 2:
            P.act(lambda e: e.activation(out=out, in_=in_, func=AF.Copy), reads=reads, writes=writes)
        else:
            P.dve(lambda e: e.tensor_copy(out=out, in_=in_), reads=reads, writes=writes)

    def X(sb):
        sl = sb % 3
        QT, KT, VT, GZ = QTs[sl], KTs[sl], VTs[sl], GZs[sl]
        bQT, bKT, bVT, bGZ = bQTs[sl], bKTs[sl], bVTs[sl], bGZs[sl]
        BGraw, LNB, BETA, GG, bBG = BGraws[sl], LNBs[sl], BETAs[sl], GGs[sl], bBGs[sl]
        full = sb >= self.npre
        xnT, bxnT = xnTs[sb % 2], bxnTs[sb % 2]
        if use_fetch:
            yield from self.xnt_fetch_gen(sb, self.nsb - 1, xnTs, bxnTs)
        else:
            yield from self.stage_a_gen(sb, xnT, bxnT, ptr, bb[0])
        cbs = list(range(12)) if full else list(range(4, 12))
        cbs_proj = list(range(12)) if sb >= self.npre - 1 else list(range(4, 12))
        for cb in cbs_proj:
            if sb > 0:
                P.pool(lambda e, cb=cb: e.tensor_copy(out=XC[:, cb, 0:3], in_=XC[:, cb, SBT:SBT + 3]),
                       reads=[bXC[cb]], writes=[bXC[cb]])
            p, bp = proj_fm(cb * 128, xnT, bxnT)
            evac(XC[:, cb, 3:SBT + 3], p[:, 0:SBT], [bp], [bXC[cb]])
            yield
        pbg, bpbg = nextpj()
        for t in range(NT):
            for k in range(KC):
                P.pe(lambda e, k=k, t=t, pbg=pbg: e.matmul(pbg[:, t * 8:(t + 1) * 8], lhsT=xnT[:, k, t * 128:(t + 1) * 128],
                                                  rhs=W2[:, k, 2048:2056], start=(k == 0), stop=(k == KC - 1)),
                     reads=[bW2[k], bxnT], writes=[bpbg])
        P.act(lambda e, pbg=pbg: e.activation(out=BGraw, in_=pbg[:, 0:NT * 8].rearrange("p (t c) -> p t c", t=NT), func=AF.Copy),
              reads=[bpbg], writes=[bBG])
        P.act(lambda e: e.activation(out=LNB, in_=BGraw[:, :, 0:4], func=AF.Exp, scale=-1.0), reads=[bBG], writes=[bBG])
        P.act(lambda e: e.activation(out=LNB, in_=LNB, func=AF.Ln, bias=1.0), reads=[bBG], writes=[bBG])
        P.dve(lambda e: e.tensor_scalar(out=LNB, in0=LNB, scalar1=-1.0, scalar2=None, op0=ALU.mult),
              reads=[bBG], writes=[bBG])
        P.act(lambda e: e.activation(out=BETA, in_=LNB, func=AF.Exp), reads=[bBG], writes=[bBG])
        P.dve(lambda e: e.tensor_tensor(out=GG, in0=BGraw[:, :, 4:8], in1=dtb.unsqueeze(1).to_broadcast([128, NT, H]),
                                        op=ALU.add), reads=[bBG, bpar], writes=[bBG])
        P.act(lambda e: e.activation(out=GG, in_=GG, func=AF.Exp), reads=[bBG], writes=[bBG])
        P.act(lambda e: e.activation(out=GG, in_=GG, func=AF.Ln, bias=1.0), reads=[bBG], writes=[bBG])
        P.dve(lambda e: e.tensor_tensor(out=GG, in0=GG, in1=negA.unsqueeze(1).to_broadcast([128, NT, H]),
                                        op=ALU.mult), reads=[bBG, bpar], writes=[bBG])
        yield
        if full:
            for h in range(H):
                p, bp = proj_fm(1536 + h * 128, xnT, bxnT)
                P.act(lambda e, h=h, p=p: e.activation(out=GZ[:, h, :], in_=p[:, 0:SBT], func=AF.Silu),
                      reads=[bp], writes=[bGZ[h]])
                yield
        for n, cb in enumerate(cbs):
            cv, bcv = nextpj()
            for j in range(4):
                P.pe(lambda e, cb=cb, cv=cv, j=j: e.matmul(cv[:, 0:SBT], lhsT=DG[:, j * 12 + cb, :], rhs=XC[:, cb, j:SBT + j],
                                                           start=(j == 0), stop=(j == 3)),
                     reads=[bXC[cb], bDG], writes=[bcv])
            if cb < 8:
                P.act(lambda e, cb=cb, cv=cv: e.activation(out=QK32[:, cb, :], in_=cv[:, 0:SBT], func=AF.Silu),
                      reads=[bcv], writes=[bQK32[cb]])
            else:
                P.act(lambda e, cb=cb, cv=cv: e.activation(out=VT[:, cb - 8, :], in_=cv[:, 0:SBT], func=AF.Silu),
                      reads=[bcv], writes=[bVT[cb - 8]])
            yield
        for cb in cbs:
            if cb >= 8:
                continue
            SQ, bSQ, RS, bRS = SQs[sqi[0] % NQ], bSQs[sqi[0] % NQ], RSs[sqi[0] % NQ], bRSs[sqi[0] % NQ]
            sqi[0] += 1
            P.pool(lambda e, cb=cb, SQ=SQ: e.tensor_tensor(out=SQ, in0=QK32[:, cb, :], in1=QK32[:, cb, :], op=ALU.mult),
                   reads=[bQK32[cb]], writes=[bSQ])
            p, bp = nextpj()
            P.pe(lambda e, p=p, SQ=SQ: e.matmul(p[:, 0:SBT], lhsT=self.ones_bf, rhs=SQ, start=True, stop=True),
                 reads=[bSQ, bc], writes=[bp])
            P.act(lambda e, p=p, RS=RS: e.activation(out=RS, in_=p[:, 0:SBT], func=AF.Ln, bias=EPS), reads=[bp], writes=[bRS])
            qbias = -0.5 * float(np.log(128.0)) if cb < 4 else 0.0
            P.act(lambda e, qbias=qbias, RS=RS: e.activation(out=RS, in_=RS, func=AF.Exp, scale=-0.5, bias=qbias),
                  reads=[bRS], writes=[bRS])
            dst, bdst = (QT[:, cb, :], bQT[cb]) if cb < 4 else (KT[:, cb - 4, :], bKT[cb - 4])
            P.pool(lambda e, cb=cb, dst=dst, RS=RS: e.tensor_tensor(out=dst, in0=QK32[:, cb, :], in1=RS, op=ALU.mult),
                   reads=[bQK32[cb], bRS], writes=[bdst])
            yield
    def Yp(sb):
        sl = sb % 3
        full = sb >= self.npre
        QT, KT, VT, GZ = QTs[sl], KTs[sl], VTs[sl], GZs[sl]
        bQT, bKT, bVT, bGZ = bQTs[sl], bKTs[sl], bVTs[sl], bGZs[sl]
        BGraw, LNB, BETA, GG, bBG = BGraws[sl], LNBs[sl], BETAs[sl], GGs[sl], bBGs[sl]
        LL = dict(L0)
        LL['PB'] = [dict(PBUF[j], **PCAR[sb % 2][j]) for j in range(NT)]
        LL.update(QT=QT, KT=KT, VT=VT, GZ=GZ, bQT=bQT, bKT=bKT, bVT=bVT, bGZ=bGZ, LNB=LNB, BETA=BETA, GG=GG, bBG=bBG)
        yield from self._gdn_prep(LL, full)
    def Yr(sb):
        sl = sb % 3
        full = sb >= self.npre
        QT, KT, VT, GZ = QTs[sl], KTs[sl], VTs[sl], GZs[sl]
        bQT, bKT, bVT, bGZ = bQTs[sl], bKTs[sl], bVTs[sl], bGZs[sl]
        BGraw, LNB, BETA, GG, bBG = BGraws[sl], LNBs[sl], BETAs[sl], GGs[sl], bBGs[sl]
        LL = dict(L0)
        LL['PB'] = [dict(PBUF[j], **PCAR[sb % 2][j]) for j in range(NT)]
        LL.update(QT=QT, KT=KT, VT=VT, GZ=GZ, bQT=bQT, bKT=bKT, bVT=bVT, bGZ=bGZ, LNB=LNB, BETA=BETA, GG=GG, bBG=bBG)
        yield from self._gdn_rec(LL, full)
        if full:
            tok0 = (sb - self.npre) * SBT
            for h in range(H):
                P.act(lambda e, h=h: e.activation(out=OSQ[:, h, :], in_=O32[:, h, :], func=AF.Square),
                      reads=[bO32], writes=[bOSQ[h]])
            for h in range(H):
                pss, bpss = bk[5][:], bb[5]
                P.pe(lambda e, h=h, pss=pss: e.matmul(pss[:, 0:SBT], lhsT=self.ones_bf, rhs=OSQ[:, h, :], start=True, stop=True),
                     reads=[bOSQ[h], bc], writes=[bpss])
                P.act(lambda e, pss=pss: e.activation(out=LNV, in_=pss[:, 0:SBT], func=AF.Ln, scale=1.0 / 128, bias=EPS),
                      reads=[bpss], writes=[bLNV])
                P.act(lambda e: e.activation(out=LNV, in_=LNV, func=AF.Exp, scale=-0.5), reads=[bLNV], writes=[bLNV])
                P.dve(lambda e, h=h: e.tensor_tensor(out=O32[:, h, :], in0=O32[:, h, :], in1=LNV, op=ALU.mult),
                      reads=[bO32, bLNV], writes=[bO32])
                P.dve(lambda e, h=h: e.scalar_tensor_tensor(
                    out=self.OB[:, h, :], in0=O32[:, h, :], scalar=gdn[:, 0:1], in1=GZ[:, h, :],
                    op0=ALU.mult, op1=ALU.mult), reads=[bO32, bpar, bGZ[h]], writes=[self.bOB[h]])
                yield
            self.spill("ob_s", self.OB, self.bOB, sb)
            if "ob" in self.debug:
                if "dbg_ob" not in self.d:
                    self.dout("dbg_ob", [128, H, self.nfull_tok], BF16)
                P.dma(lambda e, tok0=tok0: e.dma_start(out=self.d["dbg_ob"][:, :, tok0:tok0 + SBT], in_=self.OB),
                      reads=self.bOB, out=True)
    L0 = dict(locals())

    import os
    WTS = [int(v) for v in os.environ.get("IL_W", "1,2,2").split(",")]

    def run_il(gens):
        gens = list(gens)
        while gens:
            for g_, w_ in list(gens):
                for _ in range(w_):
                    try:
                        next(g_)
                    except StopIteration:
                        gens.remove((g_, w_))
                        break

    n = self.nsb
    for r in range(-2, n):
        gs = []
        if 0 <= r < n:
            gs.append((Yr(r), WTS[0]))
        if 0 <= r + 1 < n:
            gs.append((Yp(r + 1), WTS[1]))
        if 0 <= r + 2 < n:
            gs.append((X(r + 2), WTS[2]))
        if LS:
            P.merge_streams([g_ for g_, _w in gs])
        else:
            run_il(gs)
    P.pe_dummy = None
    A.reset(m0)
    P.barrier()


KB.pass2 = _pass2


def _gdn_prep(self, L, full):
    P = self.P
    H = 4
    g = lambda n: L[n]
    bk, bb = self.banks, self.bbank
    bc = self.bconst
    mask2, ident = self.mask2, self.ident
    maskL, bones, ch01, identb4 = g("maskL"), g("bones"), g("ch01"), g("identb4")
    PBUF, GG, LNB, BETA, bBG = g("PB"), g("GG"), g("LNB"), g("BETA"), g("bBG")
    QT, KT, VT, bQT, bKT, bVT = g("QT"), g("KT"), g("VT"), g("bQT"), g("bKT"), g("bVT")
    R, USB, bR, bUSB = g("R"), g("USB"), g("bR"), g("bUSB")
    S32, SBF, bS32, bSBF, sbf_i = g("S32"), g("SBF"), g("bS32"), g("bSBF"), g("sbf_i")
    O32, bO32 = g("O32"), g("bO32")
    evac, nextpj = g("evac"), g("nextpj")
    NP = NT
    pP = [bk[3 + j][:].rearrange("p (h t) -> p h t", h=H) for j in range(NP)]
    pPb = [bk[3 + j][:].bitcast(BF16)[:, 0:512].rearrange("p (h t) -> p h t", h=H) for j in range(NP)]
    bpP = [bb[3 + j] for j in range(NP)]
    ptr4 = bk[5][:].bitcast(BF16)[:, 0:512].rearrange("p (h t) -> p h t", h=H)
    pKS = bk[5][:].rearrange("p (h t) -> p h t", h=H)
    pU = bk[6][:].rearrange("p (h t) -> p h t", h=H)
    po = bk[7][:].rearrange("p (h t) -> p h t", h=H)

    def bc4(ap):
        return ap.unsqueeze(2).to_broadcast([128, H, 128])

    for j in range(NP):
        pb = PBUF[j]
        SC, bSC = pb["SC"], pb["bSC"]
        ps = bk[3 + j][:, 0:16]
        gj = GG[:, j, :]
        for n, lhs in enumerate((mask2, bones, ch01[:, 0, :], ch01[:, 1, :])):
            P.pe(lambda e, n=n, lhs=lhs, ps=ps, gj=gj: e.matmul(ps[:, 4 * n:4 * n + 4], lhsT=lhs, rhs=gj,
                                                                 start=True, stop=True),
                 reads=[bBG, bc], writes=[bpP[j]])
        P.act(lambda e, SC=SC, ps=ps: e.activation(out=SC[:, 0, :], in_=ps[:, 0:4], func=AF.Copy),
              reads=[bpP[j]], writes=[bSC])
        P.dve(lambda e, SC=SC, ps=ps: e.tensor_tensor(out=SC[:, 5, :], in0=ps[:, 4:8], in1=SC[:, 0, :], op=ALU.subtract),
              reads=[bpP[j], bSC], writes=[bSC])
        P.act(lambda e, SC=SC, ps=ps: e.activation(out=SC[:, 6:8, :], in_=ps[:, 8:16].rearrange("p (a h) -> p a h", a=2),
                                                  func=AF.Exp), reads=[bpP[j]], writes=[bSC])
        P.dve(lambda e, SC=SC: e.tensor_scalar(out=SC[:, 1, :], in0=SC[:, 0, :], scalar1=-1.0, scalar2=None, op0=ALU.mult),
              reads=[bSC], writes=[bSC])
        P.dve(lambda e, SC=SC, j=j: e.tensor_tensor(out=SC[:, 2, :], in0=SC[:, 0, :], in1=LNB[:, j, :], op=ALU.add),
              reads=[bSC, bBG], writes=[bSC])
        P.act(lambda e, SC=SC: e.activation(out=SC[:, 3, :], in_=SC[:, 0, :], func=AF.Exp), reads=[bSC], writes=[bSC])
        P.act(lambda e, SC=SC: e.activation(out=SC[:, 5, :], in_=SC[:, 5, :], func=AF.Exp), reads=[bSC], writes=[bSC])
        P.dve(lambda e, SC=SC, j=j: e.scalar_tensor_tensor(out=SC[:, 4, :], in0=SC[:, 3, :], scalar=-1.0,
                                                          in1=BETA[:, j, :], op0=ALU.mult, op1=ALU.mult),
              reads=[bSC, bBG], writes=[bSC])
        yield
    for j in range(NP):
        pb = PBUF[j]
        P.pool(lambda e, pb=pb, j=j: e.tensor_copy(out=pb["GB"], in_=bc4(GG[:, j, :])), reads=[bBG], writes=[pb["bGB"]])
        yield
    for j in range(NP):
        pb = PBUF[j]
        for h in range(H):
            P.pe(lambda e, pb=pb, j=j, h=h: e.matmul(pP[j][:, h, :], lhsT=pb["GB"][:, h, :], rhs=mask2,
                                                     start=True, stop=True),
                 reads=[pb["bGB"], bc], writes=[bpP[j]])
        yield
    for j in range(NP):
        pb = PBUF[j]
        SC = pb["SC"]
        P.dve(lambda e, pb=pb, j=j, SC=SC: e.tensor_tensor(out=pb["E1"], in0=pP[j], in1=bc4(SC[:, 0, :]), op=ALU.max),
              reads=[bpP[j], pb["bSC"]], writes=[pb["bE1"]])
        if full:
            P.dve(lambda e, pb=pb, j=j, SC=SC: e.tensor_tensor(out=pb["E2"], in0=pP[j], in1=bc4(SC[:, 0, :]), op=ALU.min),
                  reads=[bpP[j], pb["bSC"]], writes=[pb["bE2"]])
            P.act(lambda e, pb=pb, j=j: e.activation(out=pb["EGR"], in_=pP[j], func=AF.Exp),
                  reads=[bpP[j]], writes=[pb["bEGR"]])
        yield
    for j in range(NP):
        pb = PBUF[j]
        SC = pb["SC"]
        for h in range(H):
            P.act(lambda e, pb=pb, h=h, SC=SC: e.activation(out=pb["E1"][:, h, :], in_=pb["E1"][:, h, :], func=AF.Exp,
                                                            scale=-1.0, bias=SC[:, 2, h:h + 1]),
                  reads=[pb["bE1"], pb["bSC"]], writes=[pb["bE1"]])
        if full:
            for h in range(H):
                P.act(lambda e, pb=pb, h=h, SC=SC: e.activation(out=pb["E2"][:, h, :], in_=pb["E2"][:, h, :], func=AF.Exp,
                                                                bias=SC[:, 1, h:h + 1]),
                      reads=[pb["bE2"], pb["bSC"]], writes=[pb["bE2"]])
        yield
    for j in range(NP):
        pb = PBUF[j]
        P.pool(lambda e, pb=pb: e.tensor_tensor(out=pb["E1"], in0=pb["E1"],
                                                in1=maskL.unsqueeze(1).to_broadcast([128, H, 128]), op=ALU.mult),
               reads=[pb["bE1"], bc], writes=[pb["bE1"]])
        if full:
            P.pool(lambda e, pb=pb: e.tensor_tensor(out=pb["E2"], in0=pb["E2"],
                                                    in1=mask2.unsqueeze(1).to_broadcast([128, H, 128]), op=ALU.mult),
                   reads=[pb["bE2"], bc], writes=[pb["bE2"]])
            c0 = j * 128
            P.dve(lambda e, pb=pb, c0=c0: e.tensor_tensor(out=pb["QTG"], in0=QT[:, :, c0:c0 + 128], in1=pb["EGR"], op=ALU.mult),
                  reads=bQT + [pb["bEGR"]], writes=[pb["bQTG"]])
        yield
    for j in range(NP):
        c0 = j * 128
        for h in range(H):
            P.pe(lambda e, j=j, h=h, c0=c0: e.matmul(pP[j][:, h, :], lhsT=KT[:, h, c0:c0 + 128], rhs=KT[:, h, c0:c0 + 128],
                                                     start=True, stop=True), reads=[bKT[h]], writes=[bpP[j]])
        yield
    for j in range(NP):
        pb = PBUF[j]
        P.dve(lambda e, pb=pb, j=j: e.tensor_tensor(out=pb["Lm"][0], in0=pP[j], in1=pb["E1"], op=ALU.mult),
              reads=[bpP[j], pb["bE1"]], writes=[pb["bLm"][0]])
        yield
    if full:
        for j in range(NP):
            c0 = j * 128
            for h in range(H):
                P.pe(lambda e, j=j, h=h, c0=c0: e.matmul(pP[j][:, h, :], lhsT=KT[:, h, c0:c0 + 128],
                                                         rhs=QT[:, h, c0:c0 + 128], start=True, stop=True),
                     reads=[bKT[h], bQT[h]], writes=[bpP[j]])
            yield
        for j in range(NP):
            pb = PBUF[j]
            P.dve(lambda e, pb=pb, j=j: e.tensor_tensor(out=pb["ATT"], in0=pP[j], in1=pb["E2"], op=ALU.mult),
                  reads=[bpP[j], pb["bE2"]], writes=[pb["bATT"]])
            yield
    for j in range(NP):
        pb = PBUF[j]
        for h in range(H):
            P.pe(lambda e, pb=pb, j=j, h=h: e.transpose(out=pPb[j][:, h, :], in_=pb["Lm"][0][:, h, :], identity=ident),
                 reads=[pb["bLm"][0], bc], writes=[bpP[j]])
        yield
    for j in range(NP):
        pb = PBUF[j]
        evac(pb["Um"][0], pPb[j], [bpP[j]], [pb["bUm"][0]])
        yield
    for j in range(NP):
        pb = PBUF[j]
        P.pool(lambda e, pb=pb: e.tensor_tensor(out=pb["Xm"][0], in0=identb4, in1=pb["Um"][0], op=ALU.subtract),
               reads=[pb["bUm"][0], bc], writes=[pb["bXm"][0]])
        yield
    cur, cx = 0, 0
    for lvl in range(5):
        for j in range(NP):
            pb = PBUF[j]
            for h in range(H):
                P.pe(lambda e, pb=pb, j=j, h=h, cur=cur: e.matmul(pP[j][:, h, :], lhsT=pb["Um"][cur][:, h, :],
                                                                  rhs=pb["Lm"][cur][:, h, :], start=True, stop=True),
                     reads=[pb["bUm"][cur], pb["bLm"][cur]], writes=[bpP[j]])
            yield
        for j in range(NP):
            pb = PBUF[j]
            evac(pb["Lm"][1 - cur], pP[j], [bpP[j]], [pb["bLm"][1 - cur]])
            yield
        if lvl < 4:
            for j in range(NP):
                pb = PBUF[j]
                for h in range(H):
                    P.pe(lambda e, pb=pb, j=j, h=h, cur=cur: e.matmul(pP[j][:, h, :], lhsT=pb["Lm"][cur][:, h, :],
                                                                      rhs=pb["Um"][cur][:, h, :], start=True, stop=True),
                         reads=[pb["bUm"][cur], pb["bLm"][cur]], writes=[bpP[j]])
                yield
            for j in range(NP):
                pb = PBUF[j]
                evac(pb["Um"][1 - cur], pP[j], [bpP[j]], [pb["bUm"][1 - cur]])
                yield
        for j in range(NP):
            pb = PBUF[j]
            for h in range(H):
                P.pe(lambda e, pb=pb, j=j, h=h, cur=cur, cx=cx: e.matmul(pP[j][:, h, :], lhsT=pb["Lm"][1 - cur][:, h, :],
                                                                         rhs=pb["Xm"][cx][:, h, :], start=True, stop=True),
                     reads=[pb["bLm"][1 - cur], pb["bXm"][cx]], writes=[bpP[j]])
            yield
        for j in range(NP):
            pb = PBUF[j]
            xo, bxo = (pb["TT"], pb["bTT"]) if lvl == 4 else (pb["Xm"][1 - cx], pb["bXm"][1 - cx])
            P.dve(lambda e, pb=pb, j=j, cx=cx, xo=xo: e.tensor_tensor(out=xo, in0=pP[j], in1=pb["Xm"][cx], op=ALU.add),
                  reads=[bpP[j], pb["bXm"][cx]], writes=[bxo])
            yield
        cur, cx = 1 - cur, 1 - cx
    for j in range(NP):
        pb = PBUF[j]
        c0 = j * 128
        SC = pb["SC"]
        for h in range(H):
            P.pe(lambda e, h=h, c0=c0, j=j: e.transpose(out=pPb[j][:, h, :], in_=KT[:, h, c0:c0 + 128], identity=ident),
                 reads=[bKT[h], bc], writes=[bpP[j]])
        P.dve(lambda e, pb=pb, SC=SC, j=j: e.tensor_tensor(out=pb["KHT"], in0=pPb[j], in1=bc4(SC[:, 5, :]), op=ALU.mult),
              reads=[bpP[j], pb["bSC"]], writes=[pb["bKHT"]])
        for h in range(H):
            P.pe(lambda e, h=h, c0=c0, j=j: e.transpose(out=pPb[j][:, h, :], in_=VT[:, h, c0:c0 + 128], identity=ident),
                 reads=[bVT[h], bc], writes=[bpP[j]])
        P.act(lambda e, pb=pb, j=j: e.activation(out=pb["VTK"], in_=pPb[j], func=AF.Copy), reads=[bpP[j]], writes=[pb["bVTK"]])
        P.pool(lambda e, pb=pb, c0=c0: e.tensor_copy(out=pb["KTP"], in_=KT[:, :, c0:c0 + 128]), reads=bKT, writes=[pb["bKTP"]])
        P.pool(lambda e, pb=pb, j=j: e.tensor_tensor(out=pb["BV"], in0=pb["VTK"], in1=bc4(BETA[:, j, :]), op=ALU.mult),
               reads=[pb["bVTK"], bBG], writes=[pb["bBV"]])
        yield


def _gdn_rec(self, L, full):
    P = self.P
    H = 4
    g = lambda n: L[n]
    bk, bb = self.banks, self.bbank
    bc = self.bconst
    mask2, ident = self.mask2, self.ident
    maskL, bones, ch01, identb4 = g("maskL"), g("bones"), g("ch01"), g("identb4")
    PBUF, GG, LNB, BETA, bBG = g("PB"), g("GG"), g("LNB"), g("BETA"), g("bBG")
    QT, KT, VT, bQT, bKT, bVT = g("QT"), g("KT"), g("VT"), g("bQT"), g("bKT"), g("bVT")
    R, USB, bR, bUSB = g("R"), g("USB"), g("bR"), g("bUSB")
    S32, SBF, bS32, bSBF, sbf_i = g("S32"), g("SBF"), g("bS32"), g("bSBF"), g("sbf_i")
    O32, bO32 = g("O32"), g("bO32")
    evac, nextpj = g("evac"), g("nextpj")
    NP = NT
    pP = [bk[3 + j][:].rearrange("p (h t) -> p h t", h=H) for j in range(NP)]
    pPb = [bk[3 + j][:].bitcast(BF16)[:, 0:512].rearrange("p (h t) -> p h t", h=H) for j in range(NP)]
    bpP = [bb[3 + j] for j in range(NP)]
    ptr4 = bk[5][:].bitcast(BF16)[:, 0:512].rearrange("p (h t) -> p h t", h=H)
    pKS = bk[5][:].rearrange("p (h t) -> p h t", h=H)
    pU = bk[6][:].rearrange("p (h t) -> p h t", h=H)
    po = bk[7][:].rearrange("p (h t) -> p h t", h=H)

    def bc4(ap):
        return ap.unsqueeze(2).to_broadcast([128, H, 128])

    for j in range(NP):
        pb = PBUF[j]
        c0 = j * 128
        SC = pb["SC"]
        TT, bTT = pb["TT"], pb["bTT"]
        for half in range(2):
            r0 = half * 64
            cs = sbf_i[0]
            for h in range(H):
                P.pe(lambda e, h=h, pb=pb, cs=cs: e.matmul(pKS[:, h, :], lhsT=pb["KTP"][:, h, :], rhs=SBF[cs][:, h, :],
                                                           start=True, stop=True),
                     reads=[pb["bKTP"], bSBF[cs][h]], writes=[bb[5]])
            if full:
                for h in range(H):
                    P.pe(lambda e, pb=pb, h=h, cs=cs, r0=r0, half=half: e.matmul(
                        po[:, h, r0:r0 + 64], lhsT=SBF[cs][:, h, :], rhs=pb["QTG"][:, h, r0:r0 + 64],
                        start=(h == 0 and half == 0), stop=False, skip_group_check=True),
                        reads=[bSBF[cs][h], pb["bQTG"]], writes=[bb[7]])
            for h in range(H):
                P.dve(lambda e, pb=pb, h=h, r0=r0, SC=SC: e.scalar_tensor_tensor(
                    out=R[r0:r0 + 64, h, :], in0=pKS[r0:r0 + 64, h, :], scalar=SC[r0:r0 + 64, 4, h:h + 1],
                    in1=pb["BV"][r0:r0 + 64, h, :], op0=ALU.mult, op1=ALU.add),
                    reads=[bb[5], pb["bSC"], pb["bBV"]], writes=[bR])
            yield
            for h in range(H):
                P.pe(lambda e, h=h, r0=r0, TT=TT: e.matmul(pU[:, h, :], lhsT=TT[r0:r0 + 64, h, :], rhs=R[r0:r0 + 64, h, :],
                                                           start=True, stop=True),
                     reads=[bTT, bR], writes=[bb[6]])
            yield
            P.act(lambda e, r0=r0: e.activation(out=USB[r0:r0 + 64], in_=pU[r0:r0 + 64], func=AF.Copy),
                  reads=[bb[6]], writes=[bUSB])
            yield
            pS, bpS = bk[6][:], bb[6]
            pS4 = pS.rearrange("p (h t) -> p h t", h=H)
            for h in range(H):
                P.pe(lambda e, pb=pb, h=h, r0=r0, pS4=pS4: e.matmul(pS4[:, h, :], lhsT=pb["KHT"][r0:r0 + 64, h, :],
                                                                    rhs=USB[r0:r0 + 64, h, :], start=True, stop=True),
                     reads=[pb["bKHT"], bUSB], writes=[bpS])
            yield
            for h in range(H):
                P.dve(lambda e, h=h, half=half, SC=SC, pS4=pS4: e.scalar_tensor_tensor(
                    out=S32[:, h, :], in0=S32[:, h, :], scalar=SC[:, 6 + half, h:h + 1], in1=pS4[:, h, :],
                    op0=ALU.mult, op1=ALU.add), reads=[bS32[h], pb["bSC"], bpS], writes=[bS32[h]])
            yield
            nx = 1 - cs
            P.act(lambda e, nx=nx: e.activation(out=SBF[nx], in_=S32, func=AF.Copy), reads=bS32, writes=bSBF[nx])
            sbf_i[0] = nx
            yield
        if full:
            for h in range(H):
                P.pe(lambda e, pb=pb, h=h: e.matmul(po[:, h, :], lhsT=USB[:, h, :], rhs=pb["ATT"][:, h, :],
                                                    start=False, stop=True, skip_group_check=True),
                     reads=[bUSB, pb["bATT"]], writes=[bb[7]])
            P.act(lambda e, c0=c0: e.activation(out=O32[:, :, c0:c0 + 128], in_=po, func=AF.Copy),
                  reads=[bb[7]], writes=[bO32])


KB._gdn_prep = _gdn_prep
KB._gdn_rec = _gdn_rec


CAP = 384
NEXP = 32
NSLOT = NEXP * CAP
BIG = 1.0e4


def _spill(self, name, sb_ap, bufs, sb):
    P = self.P
    if name not in self.d:
        self.d[name] = self.nc.dram_tensor(name, [128, 4, self.nfull_tok], BF16, kind="Internal").ap()
        self.bspill = getattr(self, "bspill", {})
        self.bspill[name] = {}
    dst = self.d[name]
    tok0 = (sb - self.npre) * SBT
    b = Buf(name)
    self.bspill[name][sb - self.npre] = b
    P.dma(lambda e: e.dma_start(out=dst[:, :, tok0:tok0 + SBT], in_=sb_ap), reads=bufs, writes=[b])


KB.spill = _spill


def _pass3(self):
    nc, P, A = self.nc, self.P, self.A
    d = self.d
    H = 4
    for nm, shp in (("hg_up", [512, D]), ("gd_up", [512, D]), ("w_out", [D, D]), ("norm_ffn_g", [D]),
                    ("router_w", [D, 36]), ("router_b", [36])):
        self.din(nm, shp)
    ntile = self.nfull_tok // 128
    d["h2_s"] = nc.dram_tensor("h2_s", [self.nfull_tok, D], F32, kind="Internal").ap()
    self.bh2s = [Buf("h2_s%d" % i) for i in range(ntile)]
    self.bxbuf = []
    self.W12 = A.alloc((ntile, 2), F32)
    self.DST = A.alloc((ntile, 2), U32)
    self.bW12 = Buf("W12")
    self.bDST = Buf("DST")
    m0 = A.mark()
    W3 = A.alloc((KC, 2048), BF16)
    bW3 = [Buf() for _ in range(KC)]
    wv = d["w_in"].rearrange("(k p) n -> p k n", p=128)
    for k in range(KC):
        P.dma(lambda e, k=k: e.dma_start(out=W3[:, k, :], in_=wv[:, k, C_PA:C_PA + 2048]), writes=[bW3[k]], q="pool")
    HGUP = A.alloc((H, D), BF16)
    GDUP = A.alloc((H, D), BF16)
    WOUT = A.alloc((KC, D), BF16)
    bUP = Buf()
    bWO = [Buf() for _ in range(KC)]
    P.dma(lambda e: e.dma_start(out=HGUP, in_=d["hg_up"].rearrange("(h p) n -> p h n", p=128)), writes=[bUP], q="pool")
    P.dma(lambda e: e.dma_start(out=GDUP, in_=d["gd_up"].rearrange("(h p) n -> p h n", p=128)), writes=[bUP], q="pool")
    wo = d["w_out"].rearrange("(k p) n -> p k n", p=128)
    for k in range(KC):
        P.dma(lambda e, k=k: e.dma_start(out=WOUT[:, k, :], in_=wo[:, k, :]), writes=[bWO[k]], q="pool")
    WR = A.alloc((KC, 36), F32)
    RB = A.alloc((36,), F32)
    G2 = A.alloc((D,), F32)
    ECAP = A.alloc((NEXP,), F32)
    bpar = Buf("par3")
    P.dma(lambda e: e.dma_start(out=WR, in_=d["router_w"].rearrange("(k p) n -> p k n", p=128)), writes=[bpar])
    P.dma(lambda e: e.dma_start(out=RB, in_=d["router_b"].partition_broadcast(128)), writes=[bpar])
    P.dma(lambda e: e.dma_start(out=G2, in_=d["norm_ffn_g"].partition_broadcast(128)), writes=[bpar])
    self.load_gain("norm_mix_g")
    ecapi = A.alloc((NEXP,), I32)
    P.pool(lambda e: e.iota(ecapi, pattern=[[CAP, NEXP]], base=0, channel_multiplier=0), writes=[bpar])
    P.pool(lambda e: e.tensor_copy(out=ECAP, in_=ecapi), reads=[bpar], writes=[bpar])
    triS = A.alloc((128,), BF16)
    trif = A.alloc((128,), F32)
    bc = self.bconst
    P.pool(lambda e: e.memset(trif, 1.0), writes=[bc])
    P.pool(lambda e: e.affine_select(out=trif, in_=trif, pattern=[[1, 128]], compare_op=ALU.is_gt, fill=0.0,
                                     base=0, channel_multiplier=-1), reads=[bc], writes=[bc])
    P.pool(lambda e: e.tensor_copy(out=triS, in_=trif), reads=[bc], writes=[bc])
    BASE = A.alloc((NEXP,), F32)
    bBASE = Buf()
    P.pool(lambda e: e.tensor_copy(out=BASE, in_=ecapi), reads=[bpar], writes=[bBASE])

    xnTs = [A.alloc((KC, SBT), BF16) for _ in range(2)]
    bxnTs = [Buf(), Buf()]
    self._xfetched = set()
    use_fetch = hasattr(self, "bxns")
    SGs = [A.alloc((16, SBT), BF16) for _ in range(2)]
    bSGs = [[Buf() for _ in range(16)] for _ in range(2)]
    OAss = [A.alloc((H, SBT), BF16) for _ in range(2)]
    OBss = [A.alloc((H, SBT), BF16) for _ in range(2)]
    bOAss, bOBss = [Buf(), Buf()], [Buf(), Buf()]
    T1 = [A.alloc((SBT,), F32) for _ in range(2)]
    T2 = [A.alloc((SBT,), F32) for _ in range(2)]
    bT1 = [Buf(), Buf()]
    bT2 = [Buf(), Buf()]
    MG = A.alloc((KC, SBT), BF16)
    bMG = [Buf() for _ in range(KC)]
    XR = A.alloc((D,), F32)
    bXR = Buf()
    H2 = A.alloc((D,), F32)
    bH2 = Buf()
    JK = A.alloc((D,), BF16)
    SS2 = A.alloc((1,), F32)
    bSS2 = Buf()
    XF = A.alloc((D,), F32)
    XB = A.alloc((D,), BF16)
    bXF, bXB = Buf(), Buf()
    XFT = A.alloc((KC, 128), F32)
    bXFT = Buf()
    LG = A.alloc((36,), F32)
    ME = A.alloc((NEXP,), F32)
    SM = A.alloc((16,), F32)
    M8 = A.alloc((8,), F32)
    SEL1 = A.alloc((NEXP,), F32)
    SEL2 = A.alloc((NEXP,), F32)
    SELB = A.alloc((NEXP,), BF16)
    RK = A.alloc((NEXP,), F32)
    JK2 = A.alloc((NEXP,), F32)
    DF = A.alloc((2,), F32)
    brt = Buf("route")

    bk, bb = self.banks, self.bbank
    ptr = bk[0][:].bitcast(BF16).rearrange("p (k t) -> p k t", k=KC)
    pj = [bk[1][:], bk[2][:]]
    bpj = [bb[1], bb[2]]
    pup = [bk[3][:], bk[4][:]]
    pji = [0]

    def nextpj():
        i = pji[0] % 2
        pji[0] += 1
        return pj[i], bpj[i]

    pyi = [0]

    def nextpy():
        i = 5 + pyi[0] % 3
        pyi[0] += 1
        return bk[i][:], bb[i]

    oa_d, ob_d = d["oa_s"], d["ob_s"]
    def X3(sbi):
        sl = sbi % 2
        SG, bSG, OAs, OBs, bOAs, bOBs = SGs[sl], bSGs[sl], OAss[sl], OBss[sl], bOAss[sl], bOBss[sl]
        sb = self.npre + sbi
        tok0 = sbi * SBT
        xnT, bxnT = xnTs[sb % 2], bxnTs[sb % 2]
        if use_fetch:
            yield from self.xnt_fetch_gen(sb, self.nsb - 1, xnTs, bxnTs)
        else:
            yield from self.stage_a_gen(sb, xnT, bxnT, ptr, bb[0])
        P.dma(lambda e, tok0=tok0: e.dma_start(out=OAs, in_=oa_d[:, :, tok0:tok0 + SBT]),
              reads=[self.bspill["oa_s"][sbi]], writes=[bOAs])
        P.dma(lambda e, tok0=tok0: e.dma_start(out=OBs, in_=ob_d[:, :, tok0:tok0 + SBT]),
              reads=[self.bspill["ob_s"][sbi]], writes=[bOBs])
        for cb in range(16):
            p, bp = nextpj()
            for k in range(KC):
                P.pe(lambda e, k=k, p=p, cb=cb: e.matmul(p[:, 0:SBT], lhsT=W3[:, k, cb * 128:(cb + 1) * 128],
                                                         rhs=xnT[:, k, :], start=(k == 0), stop=(k == KC - 1)),
                     reads=[bW3[k], bxnT], writes=[bp])
            P.act(lambda e, cb=cb, p=p: e.activation(out=SG[:, cb, :], in_=p[:, 0:SBT], func=AF.Sigmoid),
                  reads=[bp], writes=[bSG[cb]])
            yield
    def Y3(sbi):
        sl = sbi % 2
        SG, bSG, OAs, OBs, bOAs, bOBs = SGs[sl], bSGs[sl], OAss[sl], OBss[sl], bOAss[sl], bOBss[sl]
        sb = self.npre + sbi
        tok0 = sbi * SBT
        for cb in range(KC):
            i = cb % 2
            for h in range(H):
                P.pe(lambda e, h=h, cb=cb: e.matmul(pup[0][:, 0:SBT], lhsT=HGUP[:, h, cb * 128:(cb + 1) * 128],
                                                    rhs=OAs[:, h, :], start=(h == 0), stop=(h == H - 1)),
                     reads=[bUP, bOAs], writes=[bb[3]])
            for h in range(H):
                P.pe(lambda e, h=h, cb=cb: e.matmul(pup[1][:, 0:SBT], lhsT=GDUP[:, h, cb * 128:(cb + 1) * 128],
                                                    rhs=OBs[:, h, :], start=(h == 0), stop=(h == H - 1)),
                     reads=[bUP, bOBs], writes=[bb[4]])
            P.dve(lambda e, cb=cb, i=i: e.tensor_tensor(out=T1[i], in0=pup[0][:, 0:SBT], in1=SG[:, cb, :], op=ALU.mult),
                  reads=[bb[3], bSG[cb]], writes=[bT1[i]])
            P.dve(lambda e, cb=cb, i=i: e.tensor_tensor(out=T2[i], in0=pup[1][:, 0:SBT], in1=SG[:, 8 + cb, :], op=ALU.mult),
                  reads=[bb[4], bSG[8 + cb]], writes=[bT2[i]])
            P.pool(lambda e, cb=cb, i=i: e.tensor_tensor(out=MG[:, cb, :], in0=T1[i], in1=T2[i], op=ALU.add),
                   reads=[bT1[i], bT2[i]], writes=[bMG[cb]])
            yield
        for t in range(NT):
            gt = sbi * NT + t
            gtok = self.npre * SBT + gt * 128
            P.dma(lambda e, gtok=gtok: e.dma_start(out=XR, in_=d["xs"][gtok:gtok + 128, :]), writes=[bXR])
            for half in range(2):
                p, bp = nextpy()
                for k in range(KC):
                    P.pe(lambda e, k=k, p=p, t=t, half=half: e.matmul(
                        p, lhsT=MG[:, k, t * 128:(t + 1) * 128], rhs=WOUT[:, k, half * 512:(half + 1) * 512],
                        start=(k == 0), stop=(k == KC - 1)), reads=[bMG[k], bWO[k]], writes=[bp])
                P.dve(lambda e, p=p, half=half: e.tensor_tensor(out=H2[:, half * 512:(half + 1) * 512], in0=p,
                                                               in1=XR[:, half * 512:(half + 1) * 512], op=ALU.add),
                      reads=[bp, bXR], writes=[bH2])
                yield
            P.dma(lambda e, gt=gt: e.dma_start(out=d["h2_s"][gt * 128:(gt + 1) * 128, :], in_=H2),
                  reads=[bH2], writes=[self.bh2s[gt]])
            LL = dict(L0)
            LL['nextpj'] = nextpy
            yield from self._route_tile(LL, gt)
    L0 = dict(locals())

    import os
    WTS3 = [int(v) for v in os.environ.get("IL_W3", "1,1").split(",")]

    def run_il(gens):
        gens = list(gens)
        while gens:
            for g_, w_ in list(gens):
                for _ in range(w_):
                    try:
                        next(g_)
                    except StopIteration:
                        gens.remove((g_, w_))
                        break

    n = self.nfull
    for r in range(-1, n):
        gs = []
        if 0 <= r < n:
            gs.append((Y3(r), WTS3[0]))
        if 0 <= r + 1 < n:
            gs.append((X3(r + 1), WTS3[1]))
        if LS:
            P.merge_streams([g_ for g_, _w in gs])
        else:
            run_il(gs)
    A.reset(m0)
    P.barrier()


KB.pass3 = _pass3


def _route_tile(self, L, gt):
    P = self.P
    g = lambda n: L[n]
    bk, bb = self.banks, self.bbank
    bc = self.bconst
    H2, bH2, JK, SS2, bSS2 = g("H2"), g("bH2"), g("JK"), g("SS2"), g("bSS2")
    XF, XB, bXF, bXB, XFT, bXFT = g("XF"), g("XB"), g("bXF"), g("bXB"), g("XFT"), g("bXFT")
    G2, WR, RB, ECAP, bpar = g("G2"), g("WR"), g("RB"), g("ECAP"), g("bpar")
    LG, ME, SM, M8, SEL1, SEL2, SELB, RK, JK2, DF, brt = (g("LG"), g("ME"), g("SM"), g("M8"), g("SEL1"), g("SEL2"),
                                                           g("SELB"), g("RK"), g("JK2"), g("DF"), g("brt"))
    BASE, bBASE, triS = g("BASE"), g("bBASE"), g("triS")
    nextpj = g("nextpj")
    W12, DST = self.W12, self.DST
    P.act(lambda e: e.activation(out=JK, in_=H2, func=AF.Square, accum_out=SS2), reads=[bH2], writes=[bSS2])
    P.act(lambda e: e.activation(out=SM[:, 0:1], in_=SS2, func=AF.Ln, scale=1.0 / D, bias=EPS), reads=[bSS2], writes=[brt])
    P.act(lambda e: e.activation(out=SM[:, 0:1], in_=SM[:, 0:1], func=AF.Exp, scale=-0.5), reads=[brt], writes=[brt])
    P.dve(lambda e: e.scalar_tensor_tensor(out=XF, in0=H2, scalar=SM[:, 0:1], in1=G2, op0=ALU.mult, op1=ALU.mult),
          reads=[bH2, brt, bpar], writes=[bXF])
    P.pool(lambda e: e.tensor_copy(out=XB, in_=XF), reads=[bXF], writes=[bXB])
    yield
    for half in range(2):
        for kk in range(4):
            k = half * 4 + kk
            P.pe(lambda e, k=k, kk=kk, half=half: e.transpose(out=bk[3 + half][:, kk * 128:(kk + 1) * 128],
                                                              in_=XF[:, k * 128:(k + 1) * 128], identity=self.identf),
                 reads=[bXF, bc], writes=[bb[3 + half]])
    P.act(lambda e: e.activation(out=XFT[:, 0:4, :], in_=bk[3][:].rearrange("p (k t) -> p k t", k=4), func=AF.Copy),
          reads=[bb[3]], writes=[bXFT])
    P.dve(lambda e: e.tensor_copy(out=XFT[:, 4:8, :], in_=bk[4][:].rearrange("p (k t) -> p k t", k=4)),
          reads=[bb[4]], writes=[bXFT])
    yield
    p, bp = nextpj()
    for k in range(KC):
        P.pe(lambda e, k=k, p=p: e.matmul(p[:, 0:36], lhsT=XFT[:, k, :], rhs=WR[:, k, :], start=(k == 0), stop=(k == KC - 1)),
             reads=[bXFT, bpar], writes=[bp])
    P.dve(lambda e, p=p: e.tensor_tensor(out=LG, in0=p[:, 0:36], in1=RB, op=ALU.add), reads=[bp, bpar], writes=[brt])
    yield
    P.dve(lambda e: e.tensor_reduce(out=SM[:, 1:2], in_=LG[:, 0:4], axis=AX.X, op=ALU.max), reads=[brt], writes=[brt])
    P.dve(lambda e: e.tensor_scalar(out=SM[:, 2:3], in0=SM[:, 1:2], scalar1=-1.0, scalar2=None, op0=ALU.mult),
          reads=[brt], writes=[brt])
    P.act(lambda e: e.activation(out=JK2[:, 0:4], in_=LG[:, 0:4], func=AF.Exp, bias=SM[:, 2:3], accum_out=SM[:, 3:4]),
          reads=[brt], writes=[brt])
    P.dve(lambda e: e.reciprocal(out=SM[:, 4:5], in_=SM[:, 3:4]), reads=[brt], writes=[brt])
    yield
    P.dve(lambda e: e.tensor_scalar(out=SM[:, 12:16], in0=LG[:, 0:4], scalar1=SM[:, 1:2], scalar2=-1.0,
                                    op0=ALU.is_equal, op1=ALU.add), reads=[brt], writes=[brt])
    P.dve(lambda e: e.scalar_tensor_tensor(out=ME.rearrange("p (g j) -> p g j", g=4),
                                           in0=SM[:, 12:16].unsqueeze(2).to_broadcast([128, 4, 8]), scalar=BIG,
                                           in1=LG[:, 4:36].rearrange("p (g j) -> p g j", g=4),
                                           op0=ALU.mult, op1=ALU.add), reads=[brt], writes=[brt])
    P.dve(lambda e: e.max(out=M8, in_=ME), reads=[brt], writes=[brt])
    yield
    P.dve(lambda e: e.tensor_tensor(out=SM[:, 5:6], in0=M8[:, 1:2], in1=M8[:, 0:1], op=ALU.subtract), reads=[brt], writes=[brt])
    P.act(lambda e: e.activation(out=SM[:, 6:7], in_=SM[:, 5:6], func=AF.Exp), reads=[brt], writes=[brt])
    P.dve(lambda e: e.tensor_scalar(out=SM[:, 7:8], in0=SM[:, 6:7], scalar1=1.0, scalar2=None, op0=ALU.add),
          reads=[brt], writes=[brt])
    P.dve(lambda e: e.reciprocal(out=SM[:, 8:9], in_=SM[:, 7:8]), reads=[brt], writes=[brt])
    P.dve(lambda e, gt=gt: e.tensor_tensor(out=W12[:, gt, 0:1], in0=SM[:, 8:9], in1=SM[:, 4:5], op=ALU.mult),
          reads=[brt], writes=[self.bW12])
    P.dve(lambda e, gt=gt: e.tensor_tensor(out=W12[:, gt, 1:2], in0=SM[:, 4:5], in1=W12[:, gt, 0:1], op=ALU.subtract),
          reads=[brt, self.bW12], writes=[self.bW12])
    P.dve(lambda e: e.tensor_scalar(out=SEL1, in0=ME, scalar1=M8[:, 0:1], scalar2=None, op0=ALU.is_equal),
          reads=[brt], writes=[brt])
    P.dve(lambda e: e.tensor_scalar(out=SEL2, in0=ME, scalar1=M8[:, 1:2], scalar2=None, op0=ALU.is_equal),
          reads=[brt], writes=[brt])
    P.dve(lambda e: e.tensor_tensor(out=SELB, in0=SEL1, in1=SEL2, op=ALU.add), reads=[brt], writes=[brt])
    yield
    p2, bp2 = nextpj()
    P.pe(lambda e, p2=p2: e.matmul(p2[:, 0:32], lhsT=triS, rhs=SELB, start=True, stop=True), reads=[brt, bc], writes=[bp2])
    P.pe(lambda e, p2=p2: e.matmul(p2[:, 32:64], lhsT=self.ones_bf, rhs=SELB, start=True, stop=True),
         reads=[brt, bc], writes=[bp2])
    P.dve(lambda e, p2=p2: e.tensor_tensor(out=RK, in0=p2[:, 0:32], in1=BASE, op=ALU.add), reads=[bp2, bBASE], writes=[brt])
    P.dve(lambda e, p2=p2: e.tensor_tensor(out=BASE, in0=p2[:, 32:64], in1=BASE, op=ALU.add), reads=[bp2, bBASE], writes=[bBASE])
    P.dve(lambda e: e.scalar_tensor_tensor(out=JK2, in0=SEL1, scalar=1.0, in1=RK, op0=ALU.mult, op1=ALU.mult,
                                           accum_out=DF[:, 0:1]), reads=[brt], writes=[brt])
    P.dve(lambda e: e.scalar_tensor_tensor(out=JK2, in0=SEL2, scalar=1.0, in1=RK, op0=ALU.mult, op1=ALU.mult,
                                           accum_out=DF[:, 1:2]), reads=[brt], writes=[brt])
    P.dve(lambda e, gt=gt: e.tensor_copy(out=DST[:, gt, :], in_=DF), reads=[brt], writes=[self.bDST])
    yield
    xb = self.d["x_buf"]
    for k in range(2):
        bx = Buf("xbuf")
        self.bxbuf.append(bx)
        P.dma(lambda e, gt=gt, k=k: e.indirect_dma_start(
            out=xb, out_offset=bass.IndirectOffsetOnAxis(ap=DST[:, gt, k:k + 1], axis=0), in_=XB, in_offset=None),
            reads=[bXB, self.bDST] + self.bxz, writes=[bx], q="pool")


KB._route_tile = _route_tile


def _pass4(self):
    nc, P, A = self.nc, self.P, self.A
    d = self.d
    self.din("w_gate", [NEXP, D, 512])
    self.din("w_up", [NEXP, D, 512])
    self.din("w_down", [NEXP, 512, D])
    d["y_buf"] = nc.dram_tensor("y_buf", [NSLOT, D], F32, kind="Internal").ap()
    self.bybuf = []
    m0 = A.mark()
    NB = CAP // 128
    WG = [A.alloc((KC, 512), BF16) for _ in range(2)]
    WU = [A.alloc((KC, 512), BF16) for _ in range(2)]
    WD = [A.alloc((4, D), BF16) for _ in range(2)]
    bWG = [Buf(), Buf()]
    bWU = [Buf(), Buf()]
    bWD = [Buf(), Buf()]
    XE = [A.alloc((NB, D), BF16) for _ in range(2)]
    bXE = [Buf(), Buf()]
    XET = [A.alloc((KC, CAP), BF16) for _ in range(2)]
    bXET = [Buf(), Buf()]
    SGT = [A.alloc((CAP,), F32) for _ in range(2)]
    bSGT = [Buf(), Buf()]
    HT = A.alloc((4, CAP), BF16)
    bHT = [Buf() for _ in range(4)]
    YS = [A.alloc((D,), F32) for _ in range(2)]
    bYS = [Buf(), Buf()]
    bk, bb = self.banks, self.bbank
    ptr = bk[0][:].bitcast(BF16).rearrange("p (k t) -> p k t", k=KC)
    ident = self.ident
    bc = self.bconst
    xb, yb = d["x_buf"], d["y_buf"]
    cnt = [0]

    def bank(lo, n):
        i = lo + cnt[0] % n
        cnt[0] += 1
        return bk[i][:], bb[i]

    def load_w(e):
        i = e % 2
        P.dma(lambda eng, e=e, i=i: eng.dma_start(out=WG[i], in_=d["w_gate"][e].rearrange("(k p) n -> p k n", p=128)),
              writes=[bWG[i]], q="pool")
        P.dma(lambda eng, e=e, i=i: eng.dma_start(out=WU[i], in_=d["w_up"][e].rearrange("(k p) n -> p k n", p=128)),
              writes=[bWU[i]], q="pool")
        P.dma(lambda eng, e=e, i=i: eng.dma_start(out=WD[i], in_=d["w_down"][e].rearrange("(k p) n -> p k n", p=128)),
              writes=[bWD[i]], q="pool")

    def load_x(e):
        i = e % 2
        P.dma(lambda eng, e=e, i=i: eng.dma_start(out=XE[i], in_=xb[e * CAP:(e + 1) * CAP, :].rearrange("(b p) n -> p b n", p=128)),
              reads=self.bxbuf, writes=[bXE[i]])

    def transposes(e):
        i = e % 2
        for b in range(NB):
            pt, bpt = (ptr, bb[0]) if b % 2 == 0 else (ptr7, bb[7])
            for k in range(KC):
                P.pe(lambda eng, i=i, b=b, k=k, pt=pt: eng.transpose(out=pt[:, k, :], in_=XE[i][:, b, k * 128:(k + 1) * 128], identity=ident),
                     reads=[bXE[i], bc], writes=[bpt])
            if b % 2 == 0:
                P.act(lambda eng, b=b, i=i, pt=pt: eng.activation(out=XET[i][:, :, b * 128:(b + 1) * 128], in_=pt, func=AF.Copy),
                      reads=[bpt], writes=[bXET[i]])
            else:
                P.dve(lambda eng, b=b, i=i, pt=pt: eng.tensor_copy(out=XET[i][:, :, b * 128:(b + 1) * 128], in_=pt),
                      reads=[bpt], writes=[bXET[i]])

    ptr7 = bk[7][:].bitcast(BF16).rearrange("p (k t) -> p k t", k=KC)
    load_w(0)
    load_x(0)
    transposes(0)
    yi = 0
    for e in range(NEXP):
        i = e % 2
        if e + 1 < NEXP:
            load_w(e + 1)
            load_x(e + 1)
        for fc in range(4):
            pg, bpg = bk[1 + fc % 2][:], bb[1 + fc % 2]
            pu, bpu = bk[3 + fc % 2][:], bb[3 + fc % 2]
            for k in range(KC):
                P.pe(lambda eng, i=i, fc=fc, k=k, pg=pg: eng.matmul(pg[:, 0:CAP], lhsT=WG[i][:, k, fc * 128:(fc + 1) * 128],
                                                                    rhs=XET[i][:, k, :], start=(k == 0), stop=(k == KC - 1)),
                     reads=[bWG[i], bXET[i]], writes=[bpg])
            for k in range(KC):
                P.pe(lambda eng, i=i, fc=fc, k=k, pu=pu: eng.matmul(pu[:, 0:CAP], lhsT=WU[i][:, k, fc * 128:(fc + 1) * 128],
                                                                    rhs=XET[i][:, k, :], start=(k == 0), stop=(k == KC - 1)),
                     reads=[bWU[i], bXET[i]], writes=[bpu])
            j = fc % 2
            P.act(lambda eng, pg=pg, j=j: eng.activation(out=SGT[j], in_=pg[:, 0:CAP], func=AF.Silu), reads=[bpg], writes=[bSGT[j]])
            P.dve(lambda eng, pu=pu, j=j, fc=fc: eng.tensor_tensor(out=HT[:, fc, :], in0=pu[:, 0:CAP], in1=SGT[j], op=ALU.mult),
                  reads=[bpu, bSGT[j]], writes=[bHT[fc]])
        if e + 1 < NEXP:
            transposes(e + 1)
        for b in range(NB):
            ys, bys = YS[yi % 2], bYS[yi % 2]
            yi += 1
            for half in range(2):
                pd, bpd = bk[5 + half][:], bb[5 + half]
                for fc in range(4):
                    P.pe(lambda eng, i=i, b=b, fc=fc, half=half, pd=pd: eng.matmul(
                        pd, lhsT=HT[:, fc, b * 128:(b + 1) * 128], rhs=WD[i][:, fc, half * 512:(half + 1) * 512],
                        start=(fc == 0), stop=(fc == 3)), reads=[bHT[fc], bWD[i]], writes=[bpd])
                if half == 0:
                    P.act(lambda eng, pd=pd, ys=ys: eng.activation(out=ys[:, 0:512], in_=pd, func=AF.Copy), reads=[bpd], writes=[bys])
                else:
                    P.dve(lambda eng, pd=pd, ys=ys: eng.tensor_copy(out=ys[:, 512:1024], in_=pd), reads=[bpd], writes=[bys])
            r0 = e * CAP + b * 128
            by = Buf("ybuf")
            self.bybuf.append(by)
            P.dma(lambda eng, r0=r0, ys=ys: eng.dma_start(out=yb[r0:r0 + 128, :], in_=ys), reads=[bys], writes=[by])
    A.reset(m0)
    P.barrier()


def _pass5(self):
    nc, P, A = self.nc, self.P, self.A
    d = self.d
    self.din("final_norm_g", [D])
    out = self.dout("out", [self.nfull_tok, D], F32)
    m0 = A.mark()
    ntile = self.nfull_tok // 128
    FG = A.alloc((D,), F32)
    bFG = Buf()
    P.dma(lambda e: e.dma_start(out=FG, in_=d["final_norm_g"].partition_broadcast(128)), writes=[bFG])
    NB5 = 4
    Y1 = [A.alloc((D,), F32) for _ in range(NB5)]
    Y2 = [A.alloc((D,), F32) for _ in range(NB5)]
    HH = [A.alloc((D,), F32) for _ in range(NB5)]
    OT = [A.alloc((D,), F32) for _ in range(NB5)]
    bY1, bY2, bHH, bOT = ([Buf() for _ in range(NB5)], [Buf() for _ in range(NB5)], [Buf() for _ in range(NB5)],
                          [Buf() for _ in range(NB5)])
    JK = A.alloc((D,), BF16)
    SS = A.alloc((ntile,), F32)
    bSS = Buf()
    yb = d["y_buf"]
    for gt in range(ntile):
        i = gt % NB5
        P.dma(lambda e, gt=gt, i=i: e.indirect_dma_start(
            out=Y1[i], out_offset=None, in_=yb, in_offset=bass.IndirectOffsetOnAxis(ap=self.DST[:, gt, 0:1], axis=0)),
            reads=self.bybuf + [self.bDST], writes=[bY1[i]], q="pool")
        P.dma(lambda e, gt=gt, i=i: e.indirect_dma_start(
            out=Y2[i], out_offset=None, in_=yb, in_offset=bass.IndirectOffsetOnAxis(ap=self.DST[:, gt, 1:2], axis=0)),
            reads=self.bybuf + [self.bDST], writes=[bY2[i]], q="pool")
        P.dma(lambda e, gt=gt, i=i: e.dma_start(out=HH[i], in_=d["h2_s"][gt * 128:(gt + 1) * 128, :]),
              reads=[self.bh2s[gt]], writes=[bHH[i]])
        P.dve(lambda e, gt=gt, i=i: e.scalar_tensor_tensor(out=HH[i], in0=Y1[i], scalar=self.W12[:, gt, 0:1], in1=HH[i],
                                                          op0=ALU.mult, op1=ALU.add),
              reads=[bY1[i], bHH[i], self.bW12], writes=[bHH[i]])
        P.dve(lambda e, gt=gt, i=i: e.scalar_tensor_tensor(out=HH[i], in0=Y2[i], scalar=self.W12[:, gt, 1:2], in1=HH[i],
                                                          op0=ALU.mult, op1=ALU.add),
              reads=[bY2[i], bHH[i], self.bW12], writes=[bHH[i]])
        P.act(lambda e, gt=gt, i=i: e.activation(out=JK, in_=HH[i], func=AF.Square, accum_out=SS[:, gt:gt + 1]),
              reads=[bHH[i]], writes=[bSS])
        P.act(lambda e, gt=gt: e.activation(out=SS[:, gt:gt + 1], in_=SS[:, gt:gt + 1], func=AF.Ln, scale=1.0 / D, bias=EPS),
              reads=[bSS], writes=[bSS])
        P.act(lambda e, gt=gt: e.activation(out=SS[:, gt:gt + 1], in_=SS[:, gt:gt + 1], func=AF.Exp, scale=-0.5),
              reads=[bSS], writes=[bSS])
        P.dve(lambda e, gt=gt, i=i: e.scalar_tensor_tensor(out=OT[i], in0=HH[i], scalar=SS[:, gt:gt + 1], in1=FG,
                                                          op0=ALU.mult, op1=ALU.mult),
              reads=[bHH[i], bSS, bFG], writes=[bOT[i]])
        P.dma(lambda e, gt=gt, i=i: e.dma_start(out=out[gt * 128:(gt + 1) * 128, :], in_=OT[i]), reads=[bOT[i]], out=True)
    A.reset(m0)


KB.pass4 = _pass4
KB.pass5 = _pass5


NPRE_SB = 17
NFULL_SB = 16
_NC_CACHE = {}


def _build_full():
    if "nc" not in _NC_CACHE:
        kb = KB(NPRE_SB, NFULL_SB)
        kb.setup()
        kb.pass1()
        kb.pass2()
        kb.pass3()
        kb.pass4()
        kb.pass5()
        _NC_CACHE["nc"] = kb.finish()
    return _NC_CACHE["nc"]


def kernel(x, meta_tokens, hg_lb_logits, norm_mix_g, w_in, gd_conv_w, gd_A_log, gd_dt_bias, hg_norm_g, gd_norm_g,
           hg_up, gd_up, w_out, norm_ffn_g, router_group_w, router_group_b, router_expert_w, router_expert_b,
           w_gate, w_up, w_down, final_norm_g):
    f32 = np.float32
    c = lambda a: np.ascontiguousarray(np.asarray(a, dtype=f32))
    x = c(x)
    meta = c(meta_tokens)
    B, S, _ = x.shape
    half = S // 2
    npre_tok = NPRE_SB * SBT
    ntok = (NPRE_SB + NFULL_SB) * SBT
    nmeta = meta.shape[0]
    shared = {
        "w_in": c(w_in[0]),
        "norm_mix_g": c(norm_mix_g[0]),
        "hg_lb": c(np.asarray(hg_lb_logits, f32).reshape(2, 4, 128).transpose(2, 0, 1)),
        "hg_norm_g": c(hg_norm_g[0]),
        "conv_wT": c(np.asarray(gd_conv_w[0], f32).reshape(4, 12, 128).transpose(2, 0, 1)),
        "gd_A_log": c(gd_A_log[0]),
        "gd_dt_bias": c(gd_dt_bias[0]),
        "gd_norm_g": c(gd_norm_g[0]),
        "hg_up": c(hg_up[0]),
        "gd_up": c(gd_up[0]),
        "w_out": c(w_out[0]),
        "norm_ffn_g": c(norm_ffn_g[0]),
        "router_w": c(np.concatenate([np.asarray(router_group_w[0], f32), np.asarray(router_expert_w[0], f32)], axis=1)),
        "router_b": c(np.concatenate([np.asarray(router_group_b[0], f32), np.asarray(router_expert_b[0], f32)])),
        "w_gate": c(w_gate[0]),
        "w_up": c(w_up[0]),
        "w_down": c(w_down[0]),
        "final_norm_g": c(final_norm_g),
    }
    in_maps = []
    for core in range(2 * B):
        b, hf = core // 2, core % 2
        xs = np.zeros((ntok, D), f32)
        if hf == 0:
            xs[npre_tok - nmeta:npre_tok] = meta
            xs[npre_tok:] = x[b, 0:half]
        else:
            xs[npre_tok - half - nmeta:npre_tok - half] = meta
            xs[npre_tok - half:] = x[b]
        m = dict(shared)
        m["xs"] = xs
        in_maps.append(m)
    nc = _build_full()
    res = run_bass_kernel_spmd(nc, in_maps, core_ids=list(range(2 * B)))
    out = np.empty((B, S, D), f32)
    for core in range(2 * B):
        b, hf = core // 2, core % 2
        out[b, hf * half:(hf + 1) * half] = np.asarray(res.results[core]["out"], dtype=f32)
    return out

LCOST_TABLE = {('dve', 346): 0.041, ('act', 346): 0.03, ('pool', 346): 0.051, ('pool', 260): 0.019, ('pool', 382): 0.168, ('pool', 506): 0.642, ('pool', 262): 0.019, ('pool', 508): 0.624, ('pool', 582): 0.172, ('pool', 583): 0.154, ('dve', 262): 0.024, ('act', 260): 0.019, ('act', 520): 0.744, ('act', 262): 0.019, ('act', 439): 0.553, ('act', 446): 0.419, ('act', 449): 0.148, ('dve', 260): 0.024, ('dve', 455): 1.239, ('pe', 460): 0.13, ('dve', 463): 0.692, ('pe', 589): 0.149, ('act', 610): 0.553, ('dve', 625): 0.384, ('pe', 680): 0.325, ('dve', 629): 0.693, ('act', 632): 0.523, ('dve', 635): 0.612, ('act', 643): 0.507, ('act', 647): 0.1, ('act', 684): 0.644, ('dve', 648): 0.693, ('pe', 686): 0.101, ('dve', 689): 0.424, ('pe', 699): 0.108, ('dve', 703): 0.324, ('act', 709): 0.329, ('act', 616): 0.542, ('act', 621): 0.371, ('dve', 653): 0.958, ('dve', 657): 0.248, ('act', 659): 0.319, ('act', 662): 0.118, ('act', 665): 0.306, ('dve', 668): 0.646, ('dve', 671): 1.226, ('dve', 675): 0.601, ('pe', 719): 0.072, ('pe', 733): 0.101, ('pe', 738): 0.197, ('dve', 722): 0.647, ('dve', 742): 0.325, ('act', 747): 0.359, ('pe', 753): 0.095, ('act', 757): 0.577, ('act', 764): 0.333, ('pe', 768): 0.346, ('act', 770): 0.355, ('act', 772): 0.375, ('dve', 774): 0.4, ('dve', 776): 0.475, ('dve', 882): 0.286, ('pe', 996): 0.148, ('pool', 988): 0.156, ('pool', 989): 0.154, ('pool', 991): 0.264, ('act', 1008): 0.553, ('dve', 1010): 0.562, ('pe', 1037): 0.023, ('pe', 1065): 0.136, ('act', 1040): 0.219, ('act', 1043): 0.208, ('act', 1044): 0.208, ('dve', 1045): 0.126, ('act', 1047): 0.153, ('dve', 1048): 0.143, ('act', 1050): 0.181, ('act', 1051): 0.18, ('act', 1069): 0.411, ('dve', 1052): 0.121, ('pool', 1081): 0.586, ('act', 1072): 0.364, ('pe', 1084): 0.315, ('act', 1086): 0.509, ('act', 1088): 0.416, ('pool', 1091): 0.723, ('pe', 1209): 0.162, ('act', 1212): 0.243, ('pool', 1028): 0.145, ('dve', 1214): 0.104, ('act', 1216): 0.15, ('dve', 1218): 0.677, ('pool', 1231): 1.828, ('dve', 1220): 0.844, ('act', 1222): 0.177, ('act', 1223): 0.191, ('dve', 1224): 0.139, ('pe', 1236): 0.161, ('dve', 1243): 0.645, ('act', 1255): 0.367, ('pe', 1281): 0.098, ('pool', 1266): 1.268, ('dve', 1286): 0.645, ('pe', 1305): 0.109, ('pool', 1314): 1.146, ('pe', 1323): 0.111, ('pe', 1335): 0.117, ('pe', 1346): 0.116, ('dve', 1354): 0.662, ('pe', 1364): 0.109, ('pool', 1372): 1.832, ('dve', 1366): 0.68, ('pe', 1369): 0.14, ('act', 1371): 0.687, ('pool', 1373): 1.024, ('pe', 1414): 0.126, ('dve', 1424): 0.347, ('pe', 1430): 0.147, ('act', 1434): 0.669, ('pe', 1440): 0.154, ('dve', 1445): 0.279, ('act', 1450): 0.668, ('act', 1058): 0.531, ('dve', 1246): 0.597, ('act', 1248): 0.514, ('act', 1260): 0.361, ('pool', 1270): 1.267, ('dve', 1274): 0.658, ('pe', 1293): 0.165, ('dve', 1299): 0.645, ('pe', 1419): 0.064, ('pe', 1455): 0.128, ('act', 1458): 0.632, ('act', 1118): 0.333, ('pe', 1122): 0.312, ('act', 1124): 0.364, ('act', 1126): 0.402, ('dve', 1127): 0.413, ('dve', 1129): 0.473, ('pool', 1514): 0.633, ('pe', 1625): 0.127, ('pool', 1524): 0.639, ('act', 1628): 0.43, ('pe', 1640): 0.147, ('pe', 1644): 0.125, ('dve', 1647): 0.423, ('dve', 1649): 0.394, ('pool', 1651): 0.727, ('pe', 1662): 0.295, ('dve', 1665): 0.691, ('act', 1722): 0.574, ('act', 1723): 0.418, ('act', 1724): 0.203, ('dve', 1725): 1.284, ('pe', 1733): 0.217, ('pool', 1727): 3.58, ('act', 1736): 0.688, ('dve', 1738): 0.691, ('pe', 1743): 0.199, ('dve', 1745): 0.196, ('dve', 1748): 0.157, ('dve', 1749): 0.154, ('act', 1751): 0.316, ('dve', 1753): 0.164, ('dve', 1755): 0.229, ('dve', 1757): 0.192, ('dve', 1762): 0.194, ('dve', 1764): 0.158, ('act', 1765): 0.203, ('dve', 1766): 0.154, ('dve', 1768): 0.163, ('dve', 1769): 0.159, ('dve', 1771): 0.158, ('dve', 1773): 0.241, ('dve', 1775): 0.24, ('dve', 1777): 0.193, ('pe', 1781): 0.185, ('pe', 1782): 0.028, ('dve', 1784): 0.192, ('dve', 1785): 0.192, ('dve', 1786): 0.053, ('dve', 1788): 0.1, ('dve', 1790): 0.169, ('pool', 1796): 1.151, ('pool', 1845): 1.06, ('pool', 1847): 1.055, ('pool', 1849): 0.9, ('pe', 1862): 0.104, ('act', 1865): 1.115, ('dve', 1868): 0.692, ('pe', 1886): 0.165, ('act', 1894): 0.467, ('pe', 1890): 0.163, ('dve', 1895): 0.536, ('pe', 1906): 0.278, ('act', 1910): 0.678, ('dve', 1912): 0.691, ('pool', 1945): 1.118, ('pool', 1948): 1.353, ('dve', 1953): 1.285, ('pool', 246): 3.293, ('dve', 1956): 1.284, ('act', 1959): 0.574, ('act', 1961): 0.419, ('act', 1963): 0.203, ('dve', 1965): 1.283, ('act', 246): 0.115, ('dve', 246): 0.654}
Prog.LCOST = LCOST_TABLE
```

```python
import numpy as np
import concourse.bass as bass
import concourse.mybir as mybir
from concourse.bass_utils import run_bass_kernel_spmd

F32 = mybir.dt.float32
BF16 = mybir.dt.bfloat16
I32 = mybir.dt.int32
U32 = mybir.dt.uint32
AF = mybir.ActivationFunctionType
ALU = mybir.AluOpType
AX = mybir.AxisListType


class Buf:
    __slots__ = ("name", "lw", "rd", "psum")

    def __init__(self, name="", psum=False):
        self.name = name
        self.lw = None
        self.rd = {}
        self.psum = psum


class Prog:
    ENGS = ("pe", "act", "dve", "pool", "sp")

    def __init__(self, kdma=6):
        self.ops = {e: [] for e in self.ENGS}
        self.waited = {e: {} for e in self.ENGS}
        self.ndma = {e: 0 for e in self.ENGS}
        self.K = kdma
        self.out_toks = []
        self.pending = {e: [] for e in self.ENGS}

    def barrier(self):
        toks = []
        for e in self.ENGS:
            for i in range(len(self.ops[e]) - 1, -1, -1):
                op = self.ops[e][i]
                if (not op["dma"]) and op["fn"] is not None:
                    toks.append((e, i))
                    break
            n = self.ndma[e]
            for slot in range(min(self.K, n)):
                last = ((n - 1 - slot) // self.K) * self.K + slot
                toks.append((("dma", e, slot), 16 * (last // self.K + 1)))
        for e in self.ENGS:
            self.pending[e] = list(toks)

    cap = None
    COST = {"pe": 0.13, "act": 0.45, "dve": 0.42, "pool": 1.2, "sp": 0.05}
    DMA_LAT = 2.5
    LCOST = {}

    def _emit(self, eng, fn, reads, writes, dma=False, extra=()):
        if self.cap is not None:
            self.cap.append((eng, fn, tuple(reads), tuple(writes), dma, tuple(extra)))
            return None
        return self._emit_real(eng, fn, reads, writes, dma, extra)

    _act_tab = ""

    @staticmethod
    def _act_class(fn):
        nm = fn.__code__.co_names
        if "Silu" in nm or "Sigmoid" in nm:
            return "sig"
        if "Exp" in nm or "Ln" in nm:
            return "exp"
        return ""

    def _capture(self, g):
        self.cap = []
        for _ in g:
            pass
        ops = self.cap
        self.cap = None
        return ops

    def merge_streams(self, gens):
        segs = [[self._capture(g)] for g in gens]
        self.merge_pipeline(segs, [lambda s_, done: True] * len(segs))

    def merge_pipeline(self, segs, rules):
        n = len(segs)
        sp = [0] * n
        op = [0] * n
        done = [0] * n
        free = getattr(self, "_sim_free", None)
        if free is None:
            free = self._sim_free = {e: 0.0 for e in self.ENGS}
            self._sim_buf = {}
        bt = self._sim_buf
        rr = 0
        while True:
            best, bi = None, -1
            for k in range(n):
                i = (rr + k) % n
                while sp[i] < len(segs[i]) and op[i] >= len(segs[i][sp[i]]):
                    sp[i] += 1
                    op[i] = 0
                    done[i] = sp[i]
                if sp[i] >= len(segs[i]):
                    continue
                if op[i] == 0 and not rules[i](sp[i], done):
                    continue
                eng, fn, reads, writes, dma, extra = segs[i][sp[i]][op[i]]
                t = free[eng]
                for b in reads:
                    v = bt.get(id(b))
                    if v is not None and v[0] > t:
                        t = v[0]
                for b in writes:
                    v = bt.get(id(b))
                    if v is not None:
                        if v[0] > t:
                            t = v[0]
                        if v[1] > t:
                            t = v[1]
                if eng == "act" and fn is not None:
                    tc = self._act_class(fn)
                    if tc and tc != self._act_tab:
                        t += 0.4
                if best is None or t < best - 1e-9:
                    best, bi = t, i
            if bi < 0:
                if all(sp[i] >= len(segs[i]) for i in range(n)):
                    break
                progressed = False
                for i in range(n):
                    if sp[i] < len(segs[i]) and op[i] == 0 and rules[i](sp[i], done):
                        progressed = True
                assert progressed, ("pipeline gating deadlock", sp, done)
                continue
            rr = (bi + 1) % n
            eng, fn, reads, writes, dma, extra = segs[bi][sp[bi]][op[bi]]
            op[bi] += 1
            ln = fn.__code__.co_firstlineno if fn is not None else -1
            c = self.LCOST.get((eng, ln), self.COST[eng])
            if eng == "act" and fn is not None and not dma:
                tc = self._act_class(fn)
                if tc:
                    self._act_tab = tc
            if dma:
                occ = 0.6 if eng == "pool" else 0.05
                fin = best + occ + self.DMA_LAT
                free[eng] = best + occ
            else:
                fin = best + c
                free[eng] = fin
            for b in writes:
                bt[id(b)] = [fin, fin]
            for b in reads:
                v = bt.get(id(b))
                if v is None:
                    bt[id(b)] = [0.0, fin]
                elif v[1] < fin:
                    v[1] = fin
                if b.psum and bt[id(b)][0] < fin:
                    bt[id(b)][0] = fin
            tok = self._emit_real(eng, fn, reads, writes, dma, extra)
            if dma and getattr(fn, "_is_out", False):
                self.out_toks.append(tok)

    def _emit_real(self, eng, fn, reads, writes, dma=False, extra=()):
        deps = {}

        def need(tok):
            if tok is None:
                return
            k, v = tok
            if eng == "pe" and k == "pe":
                return
            if deps.get(k, -1) < v:
                deps[k] = v
        def need_x(tok):
            if tok is not None and tok[0] != eng:
                need(tok)
        for b in reads:
            if b.psum:
                need_x(b.lw)
            else:
                need(b.lw)
        for b in writes:
            if b.psum:
                need_x(b.lw)
            else:
                need(b.lw)
                for k, v in b.rd.items():
                    need((k, v))
        for t in extra:
            need(t)
        if self.pending[eng]:
            for t in self.pending[eng]:
                need(t)
            self.pending[eng] = []
        if dma:
            i = self.ndma[eng]
            self.ndma[eng] += 1
            slot = i % self.K
            val = 16 * (i // self.K + 1)
            key = ("dma", eng, slot)
            if val > 16:
                need((key, val - 16))
            tok = (key, val)
        else:
            tok = (eng, len(self.ops[eng]))
        waits = []
        w = self.waited[eng]
        for k, v in deps.items():
            if w.get(k, -1) < v:
                w[k] = v
                waits.append((k, v))
        self.ops[eng].append(dict(waits=waits, fn=fn, tok=tok, dma=dma))
        for b in reads:
            if b.psum:
                b.lw = tok
                continue
            k, v = tok
            if b.rd.get(k, -1) < v:
                b.rd[k] = v
        for b in writes:
            b.lw = tok
            b.rd = {}
        return tok

    pe_dummy = None
    _pe_cnt = 0

    def pe(self, fn, reads=(), writes=()):
        t = self._emit("pe", fn, reads, writes)
        if self.pe_dummy is not None:
            dfn, every, buf = self.pe_dummy
            self._pe_cnt += 1
            if self._pe_cnt % every == 0:
                self._emit("pe", dfn, (), (buf,))
        return t

    def act(self, fn, reads=(), writes=()):
        return self._emit("act", fn, reads, writes)

    def dve(self, fn, reads=(), writes=()):
        return self._emit("dve", fn, reads, writes)

    def pool(self, fn, reads=(), writes=()):
        return self._emit("pool", fn, reads, writes)

    def dma(self, fn, reads=(), writes=(), q="sp", out=False):
        if out and self.cap is not None:
            try:
                fn._is_out = True
            except AttributeError:
                pass
        t = self._emit(q, fn, reads, writes, dma=True)
        if out and t is not None:
            self.out_toks.append(t)
        return t

    def finalize(self, nc):
        self._emit("sp", None, (), (), extra=self.out_toks)
        targets = {e: set() for e in self.ENGS}
        for e in self.ENGS:
            for op in self.ops[e]:
                for k, v in op["waits"]:
                    if isinstance(k, str):
                        targets[k].add(v)
        rank = {e: {} for e in self.ENGS}
        for e in self.ENGS:
            r = 0
            for i, op in enumerate(self.ops[e]):
                if (not op["dma"]) and i in targets[e]:
                    assert op["fn"] is not None
                    r += 1
                    rank[e][i] = r
        import contextlib
        with contextlib.ExitStack() as st:
            csem = {e: st.enter_context(nc.semaphore("c_" + e)) for e in self.ENGS}
            dsem = {}
            for e in self.ENGS:
                if self.ndma[e] > 0:
                    for s in range(min(self.K, self.ndma[e])):
                        dsem[("dma", e, s)] = st.enter_context(nc.semaphore("d_%s_%d" % (e, s)))
            block = st.enter_context(nc.Block())

            def run(e):
                def body(engine):
                    for i, op in enumerate(self.ops[e]):
                        for k, v in op["waits"]:
                            if isinstance(k, str):
                                engine.wait_ge(csem[k], rank[k][v])
                            else:
                                engine.wait_ge(dsem[k], v)
                        if op["fn"] is None:
                            continue
                        ins = op["fn"](engine)
                        if op["dma"]:
                            ins.then_inc(dsem[op["tok"][0]], 16)
                        elif i in rank[e]:
                            ins.then_inc(csem[e], 1)
                return body
            block.tensor(run("pe"))
            block.scalar(run("act"))
            block.vector(run("dve"))
            block.gpsimd(run("pool"))
            block.sync(run("sp"))


D = 1024
KC = 8
SBT = 256
NT = SBT // 128
NCH = SBT // 64
EPS = 1e-6
C_HQ, C_HF, C_HI, C_HG = 0, 512, 1024, 1536
C_GQ, C_GK, C_GV, C_GZ = 2048, 2560, 3072, 3584
C_GB, C_GA, C_PA, C_PB = 4096, 4100, 4104, 5128
DPROJ = 6152


class Arena:
    def __init__(self, ap, words):
        self.ap = ap
        self.words = words
        self.off = 0
        self.peak = 0

    def mark(self):
        return self.off

    def reset(self, m):
        self.off = m

    def alloc(self, free_shape, dt):
        n = 1
        for s in free_shape:
            n *= s
        esz = 4 if dt in (F32, I32, U32) else 2
        words = (n * esz + 3) // 4
        words = (words + 7) // 8 * 8
        assert self.off + words <= self.words, ("arena overflow", self.off, words, self.words)
        v = self.ap[:, self.off:self.off + words]
        self.off += words
        self.peak = max(self.peak, self.off)
        if esz == 2:
            v = v.bitcast(dt)
        elif dt != F32:
            v = v.bitcast(dt)
        v = v[:, 0:n]
        if len(free_shape) > 1:
            names = ["a%d" % i for i in range(len(free_shape))]
            pat = "p (%s) -> p %s" % (" ".join(names), " ".join(names))
            v = v.rearrange(pat, **{nm: s for nm, s in zip(names, free_shape)})
        return v


import os as _os_ls
LS = int(_os_ls.environ.get("LISTSCHED", "2"))


class KB:
    def __init__(self, npre, nfull, debug=()):
        import contextlib
        self.npre, self.nfull = npre, nfull
        self.nsb = npre + nfull
        self.ntok = self.nsb * SBT
        self.nfull_tok = nfull * SBT
        self.debug = set(debug)
        self.nc = bass.Bass("TRN2", target_bir_lowering=False)
        self.P = Prog()
        self.d = {}
        self.st = contextlib.ExitStack()

    def din(self, name, shape, dt=F32):
        self.d[name] = self.nc.dram_tensor(name, list(shape), dt, kind="ExternalInput").ap()
        return self.d[name]

    def dout(self, name, shape, dt=F32):
        self.d[name] = self.nc.dram_tensor(name, list(shape), dt, kind="ExternalOutput").ap()
        return self.d[name]

    def setup(self):
        nc, P, st = self.nc, self.P, self.st
        self.din("xs", [self.ntok, D])
        self.din("w_in", [D, DPROJ])
        self.din("norm_mix_g", [D])
        self.din("hg_lb", [128, 2, 4])
        self.din("hg_norm_g", [128])
        AW = 51200
        arena_t = st.enter_context(nc.sbuf_tensor("arena", [128, AW], F32))
        self.A = Arena(arena_t[:], AW)
        self.banks = [st.enter_context(nc.psum_tensor("pb%d" % i, [128, 512], F32)) for i in range(8)]
        self.bbank = [Buf("pb%d" % i, psum=True) for i in range(8)]
        A = self.A
        self.identf = A.alloc((128,), F32)
        self.ident = A.alloc((128,), BF16)
        self.ones_bf = A.alloc((128,), BF16)
        self.ones_f = A.alloc((512,), F32)
        self.mask2 = A.alloc((128,), F32)
        self.bconst = Buf("const")
        bc = self.bconst
        P.pool(lambda e: e.memset(self.identf, 0.0), writes=[bc])
        P.pool(lambda e: e.affine_select(out=self.identf, in_=self.identf, pattern=[[-1, 128]],
                                         compare_op=ALU.not_equal, fill=1.0, base=0, channel_multiplier=1),
               reads=[bc], writes=[bc])
        P.pool(lambda e: e.tensor_copy(out=self.ident, in_=self.identf), reads=[bc], writes=[bc])
        P.pool(lambda e: e.memset(self.ones_bf, 1.0), writes=[bc])
        P.pool(lambda e: e.memset(self.ones_f, 1.0), writes=[bc])
        P.pool(lambda e: e.memset(self.mask2, 1.0), writes=[bc])
        P.pool(lambda e: e.affine_select(out=self.mask2, in_=self.mask2, pattern=[[1, 128]],
                                         compare_op=ALU.is_ge, fill=0.0, base=0, channel_multiplier=-1),
               reads=[bc], writes=[bc])
        P.pool(lambda e: e.memset(self.mask2[0:64, 64:128], 0.0), reads=[bc], writes=[bc])
        self.xt = [A.alloc((D,), F32) for _ in range(2)]
        self.bxt = [Buf("xt%d" % i) for i in range(2)]
        self.junk = A.alloc((D,), BF16)
        self.bjunk = Buf("junk")
        self.ss = A.alloc((NT,), F32)
        self.rstd = A.alloc((NT,), F32)
        self.bss = Buf("ss")
        self.brstd = Buf("rstd")
        self.xsb = [A.alloc((D,), BF16) for _ in range(2)]
        self.bxsb = [Buf("xsb%d" % i) for i in range(2)]
        self.gbc = A.alloc((D,), F32)
        self.bgbc = Buf("gbc")
        self.nxt = 0
        self.d["x_buf"] = nc.dram_tensor("x_buf", [NSLOT, D], BF16, kind="Internal").ap()
        zt = A.alloc((D,), BF16)
        self.bxzero = Buf("xzero")
        P.pool(lambda e: e.memset(zt, 0.0), writes=[bc])
        xbv = self.d["x_buf"].rearrange("(b p) n -> p b n", p=128)
        nblk = NSLOT // 128
        step = 12
        self.bxz = []
        for b0 in range(0, nblk, step):
            bz = Buf("xz")
            self.bxz.append(bz)
            P.dma(lambda e, b0=b0: e.dma_start(out=xbv[:, b0:b0 + step, :],
                                               in_=zt.unsqueeze(1).to_broadcast([128, step, D])),
                  reads=[bc], writes=[bz])

    def stage_a(self, *a, **k):
        for _ in self.stage_a_gen(*a, **k):
            pass

    def stage_a_gen(self, sb, xnT, bxnT, ptr, bptr, gname="norm_mix_g", src="xs", tok_base=0, keep=None):
        nc, P = self.nc, self.P
        xs_d = self.d[src]
        tiles = []
        for t in range(NT):
            i = self.nxt % 2
            self.nxt += 1
            tok0 = tok_base + sb * SBT + t * 128
            xt, bxt = self.xt[i], self.bxt[i]
            P.dma(lambda e, xt=xt, tok0=tok0: e.dma_start(out=xt, in_=xs_d[tok0:tok0 + 128, :]), writes=[bxt])
            P.act(lambda e, xt=xt, t=t: e.activation(out=self.junk, in_=xt, func=AF.Square,
                                                     accum_out=self.ss[:, t:t + 1]),
                  reads=[bxt], writes=[self.bss])
            tiles.append((xt, bxt, i))
            if t % 2 == 1:
                t0 = t - 1
                P.act(lambda e, t0=t0: e.activation(out=self.rstd[:, t0:t0 + 2], in_=self.ss[:, t0:t0 + 2],
                                                    func=AF.Ln, scale=1.0 / D, bias=EPS),
                      reads=[self.bss], writes=[self.brstd])
                P.act(lambda e, t0=t0: e.activation(out=self.rstd[:, t0:t0 + 2], in_=self.rstd[:, t0:t0 + 2],
                                                    func=AF.Exp, scale=-0.5),
                      reads=[self.brstd], writes=[self.brstd])
                for tt in (t0, t):
                    xt2, bxt2, i2 = tiles[tt]
                    xsb, bxsb = self.xsb[i2], self.bxsb[i2]
                    P.dve(lambda e, xt2=xt2, xsb=xsb, tt=tt: e.scalar_tensor_tensor(
                        out=xsb, in0=xt2, scalar=self.rstd[:, tt:tt + 1], in1=self.gbc,
                        op0=ALU.mult, op1=ALU.mult),
                        reads=[bxt2, self.brstd, self.bgbc], writes=[bxsb])
                    for k in range(KC):
                        P.pe(lambda e, k=k, xsb=xsb: e.transpose(out=ptr[:, k, :], in_=xsb[:, k * 128:(k + 1) * 128],
                                                                 identity=self.ident),
                             reads=[bxsb, self.bconst], writes=[bptr])
                    P.dve(lambda e, tt=tt: e.tensor_copy(out=xnT[:, :, tt * 128:(tt + 1) * 128], in_=ptr),
                          reads=[bptr], writes=[bxnT])
                    yield


    def xnt_fetch_gen(self, sb, last_sb, xnTs, bxnTs):
        P = self.P
        src = self.d["xnT_s"]
        if sb not in self._xfetched:
            self._xfetched.add(sb)
            P.dma(lambda e, sb=sb: e.dma_start(out=xnTs[sb % 2], in_=src[:, :, sb * SBT:(sb + 1) * SBT]),
                  reads=[self.bxns[sb]], writes=[bxnTs[sb % 2]])
        nx = sb + 1
        if nx <= last_sb and nx not in self._xfetched:
            self._xfetched.add(nx)
            P.dma(lambda e, nx=nx: e.dma_start(out=xnTs[nx % 2], in_=src[:, :, nx * SBT:(nx + 1) * SBT]),
                  reads=[self.bxns[nx]], writes=[bxnTs[nx % 2]])
        yield

    def load_gain(self, gname):
        P = self.P
        g = self.d[gname]
        P.dma(lambda e: e.dma_start(out=self.gbc, in_=g.partition_broadcast(128)), writes=[self.bgbc])

    def pass1(self):
        nc, P, A = self.nc, self.P, self.A
        d = self.d
        H = 4
        self.W2 = A.alloc((KC, 2056), BF16)
        self.bW2 = [Buf("W2_%d" % k) for k in range(KC)]
        m0 = A.mark()
        self.OA = A.alloc((H, SBT), BF16)
        self.bOA = [Buf("OA%d" % h) for h in range(H)]
        W1 = A.alloc((KC, 2048), BF16)
        bW1 = [Buf("W1_%d" % k) for k in range(KC)]
        wv = d["w_in"].rearrange("(k p) n -> p k n", p=128)
        for k in range(KC):
            P.dma(lambda e, k=k: e.dma_start(out=W1[:, k, :], in_=wv[:, k, 0:2048]), writes=[bW1[k]], q="pool")
        for k in range(KC):
            P.dma(lambda e, k=k: e.dma_start(out=self.W2[:, k, :], in_=wv[:, k, 2048:2048 + 2056]), writes=[self.bW2[k]], q="pool")
        self.load_gain("norm_mix_g")
        lraw = A.alloc((2, H), F32)
        lb = A.alloc((H,), F32)
        oml = A.alloc((H,), F32)
        hgn = A.alloc((1,), F32)
        blb = Buf("lb")
        P.dma(lambda e: e.dma_start(out=lraw, in_=d["hg_lb"]), writes=[blb])
        P.dma(lambda e: e.dma_start(out=hgn, in_=d["hg_norm_g"].rearrange("(p o) -> p o", o=1)), writes=[blb])
        P.dve(lambda e: e.tensor_tensor(out=lb, in0=lraw[:, 0, :], in1=lraw[:, 1, :], op=ALU.subtract),
              reads=[blb], writes=[blb])
        P.act(lambda e: e.activation(out=oml, in_=lb, func=AF.Sigmoid, scale=-1.0), reads=[blb], writes=[blb])
        P.act(lambda e: e.activation(out=lb, in_=lb, func=AF.Sigmoid), reads=[blb], writes=[blb])

        xnT = A.alloc((KC, SBT), BF16)
        bxnT = Buf("xnT")
        Fb = A.alloc((H, SBT), F32)
        CS = A.alloc((H, SBT), F32)
        Kb = A.alloc((H, SBT), BF16)
        EB = A.alloc((H, SBT), BF16)
        ENB = A.alloc((H, SBT), BF16)
        EBEs = [A.alloc((H, NCH), F32) for _ in range(2)]
        QTs = [A.alloc((H, SBT), BF16) for _ in range(2)]
        KTs = [A.alloc((H, SBT), BF16) for _ in range(2)]
        KH = A.alloc((H, SBT), BF16)
        KHTs = [A.alloc((NT, 512), BF16) for _ in range(2)]
        Vs = [A.alloc((NT, 512), BF16) for _ in range(2)]
        Gs = [A.alloc((H, SBT), BF16) for _ in range(2)]
        O32 = A.alloc((H, SBT), F32)
        OSQ = A.alloc((H, SBT), BF16)
        LNV = A.alloc((SBT,), F32)
        ATS = A.alloc((H, 128), BF16)
        S32 = A.alloc((H, 128), F32)
        SBF = [A.alloc((H, 128), BF16) for _ in range(2)]
        bF = [Buf() for _ in range(H)]
        bCS = [Buf() for _ in range(H)]
        bK = Buf()
        bEB = [Buf() for _ in range(H)]
        bENB = [Buf() for _ in range(H)]
        bEBEs = [Buf(), Buf()]
        bQTs = [[Buf() for _ in range(H)] for _ in range(2)]
        bKTs = [Buf(), Buf()]
        bKH = Buf()
        bKHTs = [[Buf() for _ in range(NT)] for _ in range(2)]
        bVs = [[Buf() for _ in range(NT)] for _ in range(2)]
        bGs = [[Buf() for _ in range(H)] for _ in range(2)]
        bO32 = Buf()
        bOSQ = [Buf() for _ in range(H)]
        bLNV = Buf()
        bATS = Buf()
        bS32 = [Buf() for _ in range(H)]
        bSBF = [[Buf() for _ in range(H)] for _ in range(2)]
        sbf_i = [0] * H

        bk = self.banks
        bb = self.bbank
        ptr = bk[0][:].bitcast(BF16).rearrange("p (k t) -> p k t", k=KC)
        pkt = bk[1][:].bitcast(BF16)[:, 0:512]
        pj = [bk[2][:], bk[3][:]]
        bpj = [bb[2], bb[3]]
        pat = bk[4][:].rearrange("p (h t) -> p h t", h=H)
        po = bk[5][:].rearrange("p (h t) -> p h t", h=H)
        pS = [bk[6 + (h % 2)][:, 0:128] for h in range(H)]
        bpS = [bb[6 + (h % 2)] for h in range(H)]
        pji = [0]

        def nextpj():
            i = pji[0] % 2
            pji[0] += 1
            return pj[i], bpj[i]

        for h in range(H):
            P.pool(lambda e, h=h: e.memset(S32[:, h, :], 0.0), writes=[bS32[h]])
            P.pool(lambda e, h=h: e.memset(SBF[0][:, h, :], 0.0), writes=[bSBF[0][h]])

        def proj_fm(col0, h):
            p, bp = nextpj()
            for k in range(KC):
                P.pe(lambda e, k=k, p=p: e.matmul(p[:, 0:SBT], lhsT=W1[:, k, col0 + h * 128:col0 + (h + 1) * 128],
                                                  rhs=xnT[:, k, :], start=(k == 0), stop=(k == KC - 1)),
                     reads=[bW1[k], bxnT], writes=[bp])
            return p, bp

        def X1(sb):
            sl = sb % 2
            QT, KT, KHT, V, G, EBE = QTs[sl], KTs[sl], KHTs[sl], Vs[sl], Gs[sl], EBEs[sl]
            bQT, bKT, bKHT, bV, bG, bEBE = bQTs[sl], bKTs[sl], bKHTs[sl], bVs[sl], bGs[sl], bEBEs[sl]
            full = sb >= self.npre
            yield from self.stage_a_gen(sb, xnT, bxnT, ptr, bb[0])
            if "xnT_s" not in self.d:
                self.d["xnT_s"] = self.nc.dram_tensor("xnT_s", [128, KC, self.ntok], BF16, kind="Internal").ap()
                self.bxns = {}
            bx_ = Buf("xns")
            self.bxns[sb] = bx_
            P.dma(lambda e, sb=sb: e.dma_start(out=self.d["xnT_s"][:, :, sb * SBT:(sb + 1) * SBT], in_=xnT),
                  reads=[bxnT], writes=[bx_])
            for h in range(H):
                p, bp = proj_fm(C_HF, h)
                P.act(lambda e, h=h, p=p: e.activation(out=Fb[:, h, :], in_=p[:, 0:SBT], func=AF.Sigmoid),
                      reads=[bp], writes=[bF[h]])
                yield
            if full:
                for h in range(H):
                    p, bp = proj_fm(C_HQ, h)
                    P.act(lambda e, h=h, p=p: e.activation(out=QT[:, h, :], in_=p[:, 0:SBT], func=AF.Silu),
                          reads=[bp], writes=[bQT[h]])
                    yield
                for h in range(H):
                    p, bp = proj_fm(C_HG, h)
                    P.act(lambda e, h=h, p=p: e.activation(out=G[:, h, :], in_=p[:, 0:SBT], func=AF.Silu),
                          reads=[bp], writes=[bG[h]])
                    yield
            for h in range(H):
                P.dve(lambda e, h=h: e.tensor_scalar(out=Fb[:, h, :], in0=Fb[:, h, :], scalar1=oml[:, h:h + 1],
                                                     scalar2=lb[:, h:h + 1], op0=ALU.mult, op1=ALU.add),
                      reads=[bF[h], blb], writes=[bF[h]])
            P.dve(lambda e: e.tensor_scalar(out=Kb, in0=Fb, scalar1=-1.0, scalar2=1.0, op0=ALU.mult, op1=ALU.add),
                  reads=bF, writes=[bK])
            for h in range(H):
                P.act(lambda e, h=h: e.activation(out=Fb[:, h, :], in_=Fb[:, h, :], func=AF.Ln),
                      reads=[bF[h], bK], writes=[bF[h]])
            for h in range(H):
                P.dve(lambda e, h=h: e.tensor_tensor_scan(out=CS[:, h, :], data0=self.ones_f[:, 0:SBT],
                                                          data1=Fb[:, h, :], initial=0.0,
                                                          op0=ALU.mult, op1=ALU.add),
                      reads=[bF[h], self.bconst], writes=[bCS[h]])
                yield
            if not full:
                for h in range(H):
                    P.act(lambda e, h=h: e.activation(out=ENB[:, h, :], in_=CS[:, h, :], func=AF.Exp, scale=-1.0,
                                                      bias=CS[:, h, SBT - 1:SBT]),
                          reads=[bCS[h]], writes=[bENB[h]])
                    yield
                P.act(lambda e: e.activation(out=EBE[:, :, 0], in_=CS[:, :, SBT - 1], func=AF.Exp), reads=bCS, writes=[bEBE])
                P.dve(lambda e: e.tensor_tensor(out=KH, in0=Kb, in1=ENB, op=ALU.mult), reads=[bK] + bENB, writes=[bKH])
            else:
                Fb4 = Fb.rearrange("p h (c t) -> p h c t", c=NCH)
                CS4 = CS.rearrange("p h (c t) -> p h c t", c=NCH)
                P.dve(lambda e: e.tensor_tensor(out=Fb4[:, :, 1:NCH, :], in0=CS4[:, :, 1:NCH, :],
                                                in1=CS4[:, :, 0:NCH - 1, 63:64].to_broadcast([128, H, NCH - 1, 64]),
                                                op=ALU.subtract),
                      reads=bCS + bF, writes=bF)
                P.dve(lambda e: e.tensor_copy(out=Fb4[:, :, 0, :], in_=CS4[:, :, 0, :]), reads=bCS + bF, writes=bF)
                for h in range(H):
                    P.act(lambda e, h=h: e.activation(out=ENB[:, h, :], in_=Fb[:, h, :], func=AF.Exp, scale=-1.0),
                          reads=[bF[h]], writes=[bENB[h]])
                    yield
                P.act(lambda e: e.activation(out=EBE, in_=Fb4[:, :, :, 63], func=AF.Exp), reads=bF, writes=[bEBE])
                if full:
                    for h in range(H):
                        P.act(lambda e, h=h: e.activation(out=EB[:, h, :], in_=Fb[:, h, :], func=AF.Exp),
                              reads=[bF[h]], writes=[bEB[h]])
                P.dve(lambda e: e.tensor_tensor(out=KT, in0=Kb, in1=ENB, op=ALU.mult), reads=[bK] + bENB, writes=[bKT])
                KT4 = KT.rearrange("p h (c t) -> p h c t", c=NCH)
                KH4 = KH.rearrange("p h (c t) -> p h c t", c=NCH)
                P.dve(lambda e: e.tensor_tensor(out=KH4, in0=KT4,
                                                in1=EBE.unsqueeze(3).to_broadcast([128, H, NCH, 64]), op=ALU.mult),
                      reads=[bKT, bEBE], writes=[bKH])
                if full:
                    P.dve(lambda e: e.tensor_tensor(out=QT, in0=QT, in1=EB, op=ALU.mult), reads=bQT + bEB, writes=bQT)
            for t in range(NT):
                p, bp = nextpj()
                for k in range(KC):
                    P.pe(lambda e, k=k, p=p, t=t: e.matmul(p, lhsT=xnT[:, k, t * 128:(t + 1) * 128],
                                                           rhs=W1[:, k, C_HI:C_HI + 512],
                                                           start=(k == 0), stop=(k == KC - 1)),
                         reads=[bW1[k], bxnT], writes=[bp])
                P.act(lambda e, p=p, t=t: e.activation(out=V[:, t, :], in_=p, func=AF.Copy), reads=[bp], writes=[bV[t]])
                for h in range(H):
                    P.pe(lambda e, h=h, t=t: e.transpose(out=pkt[:, h * 128:(h + 1) * 128],
                                                         in_=KH[:, h, t * 128:(t + 1) * 128], identity=self.ident),
                         reads=[bKH, self.bconst], writes=[bb[1]])
                P.dve(lambda e, t=t: e.tensor_copy(out=KHT[:, t, :], in_=pkt), reads=[bb[1]], writes=[bKHT[t]])
                yield
        def Y1(sb):
            full = sb >= self.npre
            sl = sb % 2
            QT, KT, KHT, V, G, EBE = QTs[sl], KTs[sl], KHTs[sl], Vs[sl], Gs[sl], EBEs[sl]
            bQT, bKT, bKHT, bV, bG, bEBE = bQTs[sl], bKTs[sl], bKHTs[sl], bVs[sl], bGs[sl], bEBEs[sl]
            if not full:
                for h in range(H):
                    for t in range(NT):
                        P.pe(lambda e, h=h, t=t: e.matmul(pS[h], lhsT=KHT[:, t, h * 128:(h + 1) * 128],
                                                          rhs=V[:, t, h * 128:(h + 1) * 128],
                                                          start=(t == 0), stop=(t == NT - 1)),
                             reads=[bKHT[t], bV[t]], writes=[bpS[h]])
                    P.dve(lambda e, h=h: e.scalar_tensor_tensor(
                        out=S32[:, h, :], in0=S32[:, h, :], scalar=EBE[:, h, 0:1], in1=pS[h],
                        op0=ALU.mult, op1=ALU.add),
                        reads=[bS32[h], bEBE, bpS[h]], writes=[bS32[h]])
                    cur = sbf_i[h]
                    nxt = 1 - cur
                    P.act(lambda e, h=h, nxt=nxt: e.activation(out=SBF[nxt][:, h, :], in_=S32[:, h, :], func=AF.Copy),
                          reads=[bS32[h]], writes=[bSBF[nxt][h]])
                    sbf_i[h] = nxt
                    yield
                return
            for j in range(NT):
                c0 = j * 128
                if full:
                    for h in range(H):
                        P.pe(lambda e, h=h, c0=c0: e.matmul(pat[:, h, :], lhsT=KT[:, h, c0:c0 + 128],
                                                            rhs=QT[:, h, c0:c0 + 128], start=True, stop=True),
                             reads=[bKT, bQT[h]], writes=[bb[4]])
                    P.dve(lambda e: e.tensor_tensor(out=ATS, in0=pat,
                                                    in1=self.mask2.unsqueeze(1).to_broadcast([128, H, 128]),
                                                    op=ALU.mult),
                          reads=[bb[4], self.bconst], writes=[bATS])
                    yield
                for half in range(2):
                    ch = 2 * j + half
                    r0 = half * 64
                    for h in range(H):
                        cur = sbf_i[h]
                        if full:
                            P.pe(lambda e, h=h, cur=cur, c0=c0, r0=r0: e.matmul(
                                po[:, h, r0:r0 + 64], lhsT=SBF[cur][:, h, :], rhs=QT[:, h, c0 + r0:c0 + r0 + 64],
                                start=(h == 0 and r0 == 0), stop=False, skip_group_check=True),
                                reads=[bSBF[cur][h], bQT[h]], writes=[bb[5]])
                        P.pe(lambda e, h=h, j=j, r0=r0: e.matmul(
                            pS[h], lhsT=KHT[r0:r0 + 64, j, h * 128:(h + 1) * 128],
                            rhs=V[r0:r0 + 64, j, h * 128:(h + 1) * 128], start=True, stop=True),
                            reads=[bKHT[j], bV[j]], writes=[bpS[h]])
                        P.dve(lambda e, h=h, ch=ch: e.scalar_tensor_tensor(
                            out=S32[:, h, :], in0=S32[:, h, :], scalar=EBE[:, h, ch:ch + 1], in1=pS[h],
                            op0=ALU.mult, op1=ALU.add),
                            reads=[bS32[h], bEBE, bpS[h]], writes=[bS32[h]])
                        nxt = 1 - cur
                        P.act(lambda e, h=h, nxt=nxt: e.activation(out=SBF[nxt][:, h, :], in_=S32[:, h, :], func=AF.Copy),
                              reads=[bS32[h]], writes=[bSBF[nxt][h]])
                        sbf_i[h] = nxt
                        yield
                if full:
                    for h in range(H):
                        P.pe(lambda e, h=h, j=j: e.matmul(po[:, h, :], lhsT=V[:, j, h * 128:(h + 1) * 128],
                                                          rhs=ATS[:, h, :], start=False, stop=True,
                                                          skip_group_check=True),
                             reads=[bV[j], bATS], writes=[bb[5]])
                    P.act(lambda e, c0=c0: e.activation(out=O32[:, :, c0:c0 + 128], in_=po, func=AF.Copy),
                          reads=[bb[5]], writes=[bO32])
                    yield
            if full:
                tok0 = (sb - self.npre) * SBT
                for h in range(H):
                    P.act(lambda e, h=h: e.activation(out=OSQ[:, h, :], in_=O32[:, h, :], func=AF.Square),
                          reads=[bO32], writes=[bOSQ[h]])
                for h in range(H):
                    pss, bpss = bk[4][:], bb[4]
                    P.pe(lambda e, h=h, pss=pss: e.matmul(pss[:, 0:SBT], lhsT=self.ones_bf, rhs=OSQ[:, h, :], start=True, stop=True),
                         reads=[bOSQ[h], self.bconst], writes=[bpss])
                    P.act(lambda e, pss=pss: e.activation(out=LNV, in_=pss[:, 0:SBT], func=AF.Ln, scale=1.0 / 128, bias=EPS),
                          reads=[bpss], writes=[bLNV])
                    P.act(lambda e: e.activation(out=LNV, in_=LNV, func=AF.Exp, scale=-0.5),
                          reads=[bLNV], writes=[bLNV])
                    P.dve(lambda e, h=h: e.tensor_tensor(out=O32[:, h, :], in0=O32[:, h, :], in1=LNV, op=ALU.mult),
                          reads=[bO32, bLNV], writes=[bO32])
                    P.dve(lambda e, h=h: e.scalar_tensor_tensor(
                        out=self.OA[:, h, :], in0=O32[:, h, :], scalar=hgn[:, 0:1], in1=G[:, h, :],
                        op0=ALU.mult, op1=ALU.mult),
                        reads=[bO32, blb, bG[h]], writes=[self.bOA[h]])
                    yield
                self.spill("oa_s", self.OA, self.bOA, sb)
                if "oa" in self.debug:
                    if "dbg_oa" not in self.d:
                        self.dout("dbg_oa", [128, H, self.nfull_tok], BF16)
                    P.dma(lambda e, tok0=tok0: e.dma_start(out=self.d["dbg_oa"][:, :, tok0:tok0 + SBT], in_=self.OA),
                          reads=self.bOA, out=True)
        import os
        WTS1 = [int(v) for v in os.environ.get("IL_W1", "1,1").split(",")]

        def run_il(gens):
            gens = list(gens)
            while gens:
                for g_, w_ in list(gens):
                    for _ in range(w_):
                        try:
                            next(g_)
                        except StopIteration:
                            gens.remove((g_, w_))
                            break

        n = self.nsb
        if LS >= 2:
            sx = [P._capture(X1(i)) for i in range(n)]
            sy = [P._capture(Y1(i)) for i in range(n)]
            P.merge_pipeline([sx, sy], [lambda s_, dn: dn[1] >= s_ - 1, lambda s_, dn: dn[0] >= s_ + 1])
        else:
            for r in range(-1, n):
                gs = []
                if 0 <= r < n:
                    gs.append((Y1(r), WTS1[0]))
                if 0 <= r + 1 < n:
                    gs.append((X1(r + 1), WTS1[1]))
                if LS:
                    P.merge_streams([g_ for g_, _w in gs])
                else:
                    run_il(gs)
        A.reset(m0)
        P.barrier()

    def finish(self):
        self.P.finalize(self.nc)
        self.st.close()
        return self.nc


def _pass2(self):
    nc, P, A = self.nc, self.P, self.A
    d = self.d
    H = 4
    self.din("conv_wT", [128, 4, 12])
    self.din("gd_A_log", [4])
    self.din("gd_dt_bias", [4])
    self.din("gd_norm_g", [128])
    m0 = A.mark()
    self.OB = A.alloc((H, SBT), BF16)
    self.bOB = [Buf("OB%d" % h) for h in range(H)]
    NW = 2056
    if hasattr(self, "W2"):
        W2, bW2 = self.W2, self.bW2
    else:
        W2 = A.alloc((KC, NW), BF16)
        bW2 = [Buf("W2_%d" % k) for k in range(KC)]
        wv = d["w_in"].rearrange("(k p) n -> p k n", p=128)
        for k in range(KC):
            P.dma(lambda e, k=k: e.dma_start(out=W2[:, k, :], in_=wv[:, k, 2048:2048 + NW]), writes=[bW2[k]], q="pool")
    self.load_gain("norm_mix_g")
    cw = A.alloc((4, 12), F32)
    negA = A.alloc((H,), F32)
    dtb = A.alloc((H,), F32)
    gdn = A.alloc((1,), F32)
    bpar = Buf("par2")
    P.dma(lambda e: e.dma_start(out=cw, in_=d["conv_wT"]), writes=[bpar])
    P.dma(lambda e: e.dma_start(out=negA, in_=d["gd_A_log"].partition_broadcast(128)), writes=[bpar])
    P.dma(lambda e: e.dma_start(out=dtb, in_=d["gd_dt_bias"].partition_broadcast(128)), writes=[bpar])
    P.dma(lambda e: e.dma_start(out=gdn, in_=d["gd_norm_g"].rearrange("(p o) -> p o", o=1)), writes=[bpar])
    P.act(lambda e: e.activation(out=negA, in_=negA, func=AF.Exp), reads=[bpar], writes=[bpar])
    P.dve(lambda e: e.tensor_scalar(out=negA, in0=negA, scalar1=-1.0, scalar2=None, op0=ALU.mult),
          reads=[bpar], writes=[bpar])
    maskL = A.alloc((128,), F32)
    ch01 = A.alloc((2, 128), F32)
    bc = self.bconst
    P.pool(lambda e: e.memset(maskL, 1.0), writes=[bc])
    P.pool(lambda e: e.affine_select(out=maskL, in_=maskL, pattern=[[-1, 128]], compare_op=ALU.is_gt,
                                     fill=0.0, base=0, channel_multiplier=1), reads=[bc], writes=[bc])
    P.pool(lambda e: e.memset(maskL[64:128, 0:64], 0.0), reads=[bc], writes=[bc])
    bones = A.alloc((128,), F32)
    P.pool(lambda e: e.memset(bones, 0.0), writes=[bc])
    P.pool(lambda e: e.memset(bones[0:64, 0:64], 1.0), reads=[bc], writes=[bc])
    P.pool(lambda e: e.memset(bones[64:128, 64:128], 1.0), reads=[bc], writes=[bc])
    P.pool(lambda e: e.memset(ch01, 0.0), writes=[bc])
    P.pool(lambda e: e.memset(ch01[0:64, 0, :], 1.0), reads=[bc], writes=[bc])
    P.pool(lambda e: e.memset(ch01[64:128, 1, :], 1.0), reads=[bc], writes=[bc])

    xnTs = [A.alloc((KC, SBT), BF16) for _ in range(2)]
    bxnTs = [Buf("xnT0"), Buf("xnT1")]
    self._xfetched = set()
    use_fetch = hasattr(self, "bxns")
    XC = A.alloc((12, SBT + 3), BF16)
    DG = A.alloc((48, 128), BF16)
    bDG = Buf("DG")
    for _j in range(4):
        for _cb in range(12):
            P.dve(lambda e, _j=_j, _cb=_cb: e.tensor_scalar(out=DG[:, _j * 12 + _cb, :], in0=self.identf,
                                                           scalar1=cw[:, _j, _cb:_cb + 1], scalar2=None, op0=ALU.mult),
                  reads=[bpar, self.bconst], writes=[bDG])
    bXC = [Buf() for _ in range(12)]
    CV = [A.alloc((SBT,), F32) for _ in range(2)]
    bCV = [Buf(), Buf()]
    QK32 = A.alloc((8, SBT), F32)
    bQK32 = [Buf() for _ in range(8)]
    NQ = 4
    SQs = [A.alloc((SBT,), BF16) for _ in range(NQ)]
    bSQs = [Buf() for _ in range(NQ)]
    RSs = [A.alloc((SBT,), F32) for _ in range(NQ)]
    bRSs = [Buf() for _ in range(NQ)]
    sqi = [0]
    NS = 3
    QTs = [A.alloc((H, SBT), BF16) for _ in range(NS)]
    KTs = [A.alloc((H, SBT), BF16) for _ in range(NS)]
    VTs = [A.alloc((H, SBT), BF16) for _ in range(NS)]
    GZs = [A.alloc((H, SBT), BF16) for _ in range(NS)]
    bQTs = [[Buf() for _ in range(H)] for _ in range(NS)]
    bKTs = [[Buf() for _ in range(H)] for _ in range(NS)]
    bVTs = [[Buf() for _ in range(H)] for _ in range(NS)]
    bGZs = [[Buf() for _ in range(H)] for _ in range(NS)]
    BGraws = [A.alloc((NT, 8), F32) for _ in range(NS)]
    LNBs = [A.alloc((NT, H), F32) for _ in range(NS)]
    BETAs = [A.alloc((NT, H), F32) for _ in range(NS)]
    GGs = [A.alloc((NT, H), F32) for _ in range(NS)]
    bBGs = [Buf() for _ in range(NS)]
    PBUF = []
    for _j in range(NT):
        pb = dict(
            GB=A.alloc((H, 128), F32), bGB=Buf(),
            E1=A.alloc((H, 128), F32), bE1=Buf(),
            E2=A.alloc((H, 128), F32), bE2=Buf(),
            EGR=A.alloc((H, 128), BF16), bEGR=Buf(),
            Lm=[A.alloc((H, 128), BF16) for _ in range(2)], bLm=[Buf(), Buf()],
            Um=[A.alloc((H, 128), BF16) for _ in range(2)], bUm=[Buf(), Buf()],
            Xm=[A.alloc((H, 128), BF16) for _ in range(2)], bXm=[Buf(), Buf()],
            VTK=A.alloc((H, 128), BF16), bVTK=Buf(),
        )
        PBUF.append(pb)
    PCAR = []
    for _s in range(2):
        row = []
        for _j in range(NT):
            row.append(dict(
                SC=A.alloc((8, H), F32), bSC=Buf(),
                QTG=A.alloc((H, 128), BF16), bQTG=Buf(),
                TT=A.alloc((H, 128), BF16), bTT=Buf(),
                ATT=A.alloc((H, 128), BF16), bATT=Buf(),
                KHT=A.alloc((H, 128), BF16), bKHT=Buf(),
                KTP=A.alloc((H, 128), BF16), bKTP=Buf(),
                BV=A.alloc((H, 128), F32), bBV=Buf(),
            ))
        PCAR.append(row)
    R = A.alloc((H, 128), BF16)
    USB = A.alloc((H, 128), BF16)
    bR = Buf()
    bUSB = Buf()
    S32 = A.alloc((H, 128), F32)
    SBF = [A.alloc((H, 128), BF16) for _ in range(2)]
    bS32 = [Buf() for _ in range(H)]
    bSBF = [[Buf() for _ in range(H)] for _ in range(2)]
    sbf_i = [0]
    O32 = A.alloc((H, SBT), F32)
    bO32 = Buf()
    OSQ = A.alloc((H, SBT), BF16)
    bOSQ = [Buf() for _ in range(H)]
    LNV = A.alloc((SBT,), F32)
    bLNV = Buf()
    identb4 = A.alloc((H, 128), BF16)
    P.pool(lambda e: e.tensor_copy(out=identb4, in_=self.ident.unsqueeze(1).to_broadcast([128, H, 128])),
           reads=[bc], writes=[bc])

    bk, bb = self.banks, self.bbank
    ptr = bk[0][:].bitcast(BF16).rearrange("p (k t) -> p k t", k=KC)
    ptr4 = bk[0][:].bitcast(BF16)[:, 0:512].rearrange("p (h t) -> p h t", h=H)
    import os as _os
    DUM = int(_os.environ.get("PE_DUMMY", "0"))
    DUMN = int(_os.environ.get("PE_DUMMY_N", "128"))
    if DUM > 0:
        pj = [bk[1][:], bk[2][:]]
        bpj = [bb[1], bb[2]]
        dones = A.alloc((512,), BF16)
        P.pool(lambda e: e.memset(dones, 1.0), writes=[self.bconst])
        dbuf = Buf("dummy", psum=True)
        P.pe_dummy = (lambda e: e.matmul(bk[0][:, 0:DUMN], lhsT=self.ones_bf, rhs=dones[:, 0:DUMN], start=True, stop=True),
                      DUM, dbuf)
    else:
        pj = [bk[1][:], bk[2][:], bk[0][:]]
        bpj = [bb[1], bb[2], bb[0]]
    pA = bk[3][:].rearrange("p (h t) -> p h t", h=H)
    pB = bk[4][:].rearrange("p (h t) -> p h t", h=H)
    pBb = bk[4][:].bitcast(BF16)[:, 0:512].rearrange("p (h t) -> p h t", h=H)
    pC = bk[5][:].rearrange("p (h t) -> p h t", h=H)
    pR = bk[6][:].rearrange("p (h t) -> p h t", h=H)
    po = bk[7][:].rearrange("p (h t) -> p h t", h=H)
    pji = [0]

    def nextpj():
        i = pji[0] % len(pj)
        pji[0] += 1
        return pj[i], bpj[i]

    for h in range(H):
        P.pool(lambda e, h=h: e.memset(S32[:, h, :], 0.0), writes=[bS32[h]])
        P.pool(lambda e, h=h: e.memset(SBF[0][:, h, :], 0.0), writes=[bSBF[0][h]])
    for cb in range(12):
        P.pool(lambda e, cb=cb: e.memset(XC[:, cb, :], 0.0), writes=[bXC[cb]])

    def proj_fm(col0, xnT, bxnT):
        p, bp = nextpj()
        for k in range(KC):
            P.pe(lambda e, k=k, p=p: e.matmul(p[:, 0:SBT], lhsT=W2[:, k, col0:col0 + 128],
                                              rhs=xnT[:, k, :], start=(k == 0), stop=(k == KC - 1)),
                 reads=[bW2[k], bxnT], writes=[bp])
        return p, bp

    evi = [0]

    def evac(out, in_, reads, writes):
        i = evi[0]
        evi[0] += 1
        if i % 3 != 2:
            P.act(lambda e: e.activation(out=out, in_=in_, func=AF.Copy), reads=reads, writes=writes)
        else:
            P.dve(lambda e: e.tensor_copy(out=out, in_=in_), reads=reads, writes=writes)

    def X(sb):
        sl = sb % 3
        QT, KT, VT, GZ = QTs[sl], KTs[sl], VTs[sl], GZs[sl]
        bQT, bKT, bVT, bGZ = bQTs[sl], bKTs[sl], bVTs[sl], bGZs[sl]
        BGraw, LNB, BETA, GG, bBG = BGraws[sl], LNBs[sl], BETAs[sl], GGs[sl], bBGs[sl]
        full = sb >= self.npre
        xnT, bxnT = xnTs[sb % 2], bxnTs[sb % 2]
        if use_fetch:
            yield from self.xnt_fetch_gen(sb, self.nsb - 1, xnTs, bxnTs)
        else:
            yield from self.stage_a_gen(sb, xnT, bxnT, ptr, bb[0])
        cbs = list(range(12)) if full else list(range(4, 12))
        cbs_proj = list(range(12)) if sb >= self.npre - 1 else list(range(4, 12))
        for cb in cbs_proj:
            if sb > 0:
                P.pool(lambda e, cb=cb: e.tensor_copy(out=XC[:, cb, 0:3], in_=XC[:, cb, SBT:SBT + 3]),
                       reads=[bXC[cb]], writes=[bXC[cb]])
            p, bp = proj_fm(cb * 128, xnT, bxnT)
            evac(XC[:, cb, 3:SBT + 3], p[:, 0:SBT], [bp], [bXC[cb]])
            yield
        pbg, bpbg = nextpj()
        for t in range(NT):
            for k in range(KC):
                P.pe(lambda e, k=k, t=t, pbg=pbg: e.matmul(pbg[:, t * 8:(t + 1) * 8], lhsT=xnT[:, k, t * 128:(t + 1) * 128],
                                                  rhs=W2[:, k, 2048:2056], start=(k == 0), stop=(k == KC - 1)),
                     reads=[bW2[k], bxnT], writes=[bpbg])
        P.act(lambda e, pbg=pbg: e.activation(out=BGraw, in_=pbg[:, 0:NT * 8].rearrange("p (t c) -> p t c", t=NT), func=AF.Copy),
              reads=[bpbg], writes=[bBG])
        P.act(lambda e: e.activation(out=LNB, in_=BGraw[:, :, 0:4], func=AF.Exp, scale=-1.0), reads=[bBG], writes=[bBG])
        P.act(lambda e: e.activation(out=LNB, in_=LNB, func=AF.Ln, bias=1.0), reads=[bBG], writes=[bBG])
        P.dve(lambda e: e.tensor_scalar(out=LNB, in0=LNB, scalar1=-1.0, scalar2=None, op0=ALU.mult),
              reads=[bBG], writes=[bBG])
        P.act(lambda e: e.activation(out=BETA, in_=LNB, func=AF.Exp), reads=[bBG], writes=[bBG])
        P.dve(lambda e: e.tensor_tensor(out=GG, in0=BGraw[:, :, 4:8], in1=dtb.unsqueeze(1).to_broadcast([128, NT, H]),
                                        op=ALU.add), reads=[bBG, bpar], writes=[bBG])
        P.act(lambda e: e.activation(out=GG, in_=GG, func=AF.Exp), reads=[bBG], writes=[bBG])
        P.act(lambda e: e.activation(out=GG, in_=GG, func=AF.Ln, bias=1.0), reads=[bBG], writes=[bBG])
        P.dve(lambda e: e.tensor_tensor(out=GG, in0=GG, in1=negA.unsqueeze(1).to_broadcast([128, NT, H]),
                                        op=ALU.mult), reads=[bBG, bpar], writes=[bBG])
        yield
        if full:
            for h in range(H):
                p, bp = proj_fm(1536 + h * 128, xnT, bxnT)
                P.act(lambda e, h=h, p=p: e.activation(out=GZ[:, h, :], in_=p[:, 0:SBT], func=AF.Silu),
                      reads=[bp], writes=[bGZ[h]])
                yield
        for n, cb in enumerate(cbs):
            cv, bcv = nextpj()
            for j in range(4):
                P.pe(lambda e, cb=cb, cv=cv, j=j: e.matmul(cv[:, 0:SBT], lhsT=DG[:, j * 12 + cb, :], rhs=XC[:, cb, j:SBT + j],
                                                           start=(j == 0), stop=(j == 3)),
                     reads=[bXC[cb], bDG], writes=[bcv])
            if cb < 8:
                P.act(lambda e, cb=cb, cv=cv: e.activation(out=QK32[:, cb, :], in_=cv[:, 0:SBT], func=AF.Silu),
                      reads=[bcv], writes=[bQK32[cb]])
            else:
                P.act(lambda e, cb=cb, cv=cv: e.activation(out=VT[:, cb - 8, :], in_=cv[:, 0:SBT], func=AF.Silu),
                      reads=[bcv], writes=[bVT[cb - 8]])
            yield
        for cb in cbs:
            if cb >= 8:
                continue
            SQ, bSQ, RS, bRS = SQs[sqi[0] % NQ], bSQs[sqi[0] % NQ], RSs[sqi[0] % NQ], bRSs[sqi[0] % NQ]
            sqi[0] += 1
            P.pool(lambda e, cb=cb, SQ=SQ: e.tensor_tensor(out=SQ, in0=QK32[:, cb, :], in1=QK32[:, cb, :], op=ALU.mult),
                   reads=[bQK32[cb]], writes=[bSQ])
            p, bp = nextpj()
            P.pe(lambda e, p=p, SQ=SQ: e.matmul(p[:, 0:SBT], lhsT=self.ones_bf, rhs=SQ, start=True, stop=True),
                 reads=[bSQ, bc], writes=[bp])
            P.act(lambda e, p=p, RS=RS: e.activation(out=RS, in_=p[:, 0:SBT], func=AF.Ln, bias=EPS), reads=[bp], writes=[bRS])
            qbias = -0.5 * float(np.log(128.0)) if cb < 4 else 0.0
            P.act(lambda e, qbias=qbias, RS=RS: e.activation(out=RS, in_=RS, func=AF.Exp, scale=-0.5, bias=qbias),
                  reads=[bRS], writes=[bRS])
            dst, bdst = (QT[:, cb, :], bQT[cb]) if cb < 4 else (KT[:, cb - 4, :], bKT[cb - 4])
            P.pool(lambda e, cb=cb, dst=dst, RS=RS: e.tensor_tensor(out=dst, in0=QK32[:, cb, :], in1=RS, op=ALU.mult),
                   reads=[bQK32[cb], bRS], writes=[bdst])
            yield
    def Yp(sb):
        sl = sb % 3
        full = sb >= self.npre
        QT, KT, VT, GZ = QTs[sl], KTs[sl], VTs[sl], GZs[sl]
        bQT, bKT, bVT, bGZ = bQTs[sl], bKTs[sl], bVTs[sl], bGZs[sl]
        BGraw, LNB, BETA, GG, bBG = BGraws[sl], LNBs[sl], BETAs[sl], GGs[sl], bBGs[sl]
        LL = dict(L0)
        LL['PB'] = [dict(PBUF[j], **PCAR[sb % 2][j]) for j in range(NT)]
        LL.update(QT=QT, KT=KT, VT=VT, GZ=GZ, bQT=bQT, bKT=bKT, bVT=bVT, bGZ=bGZ, LNB=LNB, BETA=BETA, GG=GG, bBG=bBG)
        yield from self._gdn_prep(LL, full)
    def Yr(sb):
        sl = sb % 3
        full = sb >= self.npre
        QT, KT, VT, GZ = QTs[sl], KTs[sl], VTs[sl], GZs[sl]
        bQT, bKT, bVT, bGZ = bQTs[sl], bKTs[sl], bVTs[sl], bGZs[sl]
        BGraw, LNB, BETA, GG, bBG = BGraws[sl], LNBs[sl], BETAs[sl], GGs[sl], bBGs[sl]
        LL = dict(L0)
        LL['PB'] = [dict(PBUF[j], **PCAR[sb % 2][j]) for j in range(NT)]
        LL.update(QT=QT, KT=KT, VT=VT, GZ=GZ, bQT=bQT, bKT=bKT, bVT=bVT, bGZ=bGZ, LNB=LNB, BETA=BETA, GG=GG, bBG=bBG)
        yield from self._gdn_rec(LL, full)
        if full:
            tok0 = (sb - self.npre) * SBT
            for h in range(H):
                P.act(lambda e, h=h: e.activation(out=OSQ[:, h, :], in_=O32[:, h, :], func=AF.Square),
                      reads=[bO32], writes=[bOSQ[h]])
            for h in range(H):
                pss, bpss = bk[5][:], bb[5]
                P.pe(lambda e, h=h, pss=pss: e.matmul(pss[:, 0:SBT], lhsT=self.ones_bf, rhs=OSQ[:, h, :], start=True, stop=True),
                     reads=[bOSQ[h], bc], writes=[bpss])
                P.act(lambda e, pss=pss: e.activation(out=LNV, in_=pss[:, 0:SBT], func=AF.Ln, scale=1.0 / 128, bias=EPS),
                      reads=[bpss], writes=[bLNV])
                P.act(lambda e: e.activation(out=LNV, in_=LNV, func=AF.Exp, scale=-0.5), reads=[bLNV], writes=[bLNV])
                P.dve(lambda e, h=h: e.tensor_tensor(out=O32[:, h, :], in0=O32[:, h, :], in1=LNV, op=ALU.mult),
                      reads=[bO32, bLNV], writes=[bO32])
                P.dve(lambda e, h=h: e.scalar_tensor_tensor(
                    out=self.OB[:, h, :], in0=O32[:, h, :], scalar=gdn[:, 0:1], in1=GZ[:, h, :],
                    op0=ALU.mult, op1=ALU.mult), reads=[bO32, bpar, bGZ[h]], writes=[self.bOB[h]])
                yield
            self.spill("ob_s", self.OB, self.bOB, sb)
            if "ob" in self.debug:
                if "dbg_ob" not in self.d:
                    self.dout("dbg_ob", [128, H, self.nfull_tok], BF16)
                P.dma(lambda e, tok0=tok0: e.dma_start(out=self.d["dbg_ob"][:, :, tok0:tok0 + SBT], in_=self.OB),
                      reads=self.bOB, out=True)
    L0 = dict(locals())

    import os
    WTS = [int(v) for v in os.environ.get("IL_W", "1,2,2").split(",")]

    def run_il(gens):
        gens = list(gens)
        while gens:
            for g_, w_ in list(gens):
                for _ in range(w_):
                    try:
                        next(g_)
                    except StopIteration:
                        gens.remove((g_, w_))
                        break

    n = self.nsb
    if LS >= 2:
        sx = [P._capture(X(i)) for i in range(n)]
        sp_ = [P._capture(Yp(i)) for i in range(n)]
        sr = [P._capture(Yr(i)) for i in range(n)]
        P.merge_pipeline([sx, sp_, sr],
                         [lambda s_, dn: dn[2] >= s_ - 2,
                          lambda s_, dn: dn[0] >= s_ + 1 and dn[2] >= s_ - 1,
                          lambda s_, dn: dn[1] >= s_ + 1])
    else:
        for r in range(-2, n):
            gs = []
            if 0 <= r < n:
                gs.append((Yr(r), WTS[0]))
            if 0 <= r + 1 < n:
                gs.append((Yp(r + 1), WTS[1]))
            if 0 <= r + 2 < n:
                gs.append((X(r + 2), WTS[2]))
            if LS:
                P.merge_streams([g_ for g_, _w in gs])
            else:
                run_il(gs)
    P.pe_dummy = None
    A.reset(m0)
    P.barrier()


KB.pass2 = _pass2


def _gdn_prep(self, L, full):
    P = self.P
    H = 4
    g = lambda n: L[n]
    bk, bb = self.banks, self.bbank
    bc = self.bconst
    mask2, ident = self.mask2, self.ident
    maskL, bones, ch01, identb4 = g("maskL"), g("bones"), g("ch01"), g("identb4")
    PBUF, GG, LNB, BETA, bBG = g("PB"), g("GG"), g("LNB"), g("BETA"), g("bBG")
    QT, KT, VT, bQT, bKT, bVT = g("QT"), g("KT"), g("VT"), g("bQT"), g("bKT"), g("bVT")
    R, USB, bR, bUSB = g("R"), g("USB"), g("bR"), g("bUSB")
    S32, SBF, bS32, bSBF, sbf_i = g("S32"), g("SBF"), g("bS32"), g("bSBF"), g("sbf_i")
    O32, bO32 = g("O32"), g("bO32")
    evac, nextpj = g("evac"), g("nextpj")
    NP = NT
    pP = [bk[3 + j][:].rearrange("p (h t) -> p h t", h=H) for j in range(NP)]
    pPb = [bk[3 + j][:].bitcast(BF16)[:, 0:512].rearrange("p (h t) -> p h t", h=H) for j in range(NP)]
    bpP = [bb[3 + j] for j in range(NP)]
    ptr4 = bk[5][:].bitcast(BF16)[:, 0:512].rearrange("p (h t) -> p h t", h=H)
    pKS = bk[5][:].rearrange("p (h t) -> p h t", h=H)
    pU = bk[6][:].rearrange("p (h t) -> p h t", h=H)
    po = bk[7][:].rearrange("p (h t) -> p h t", h=H)

    def bc4(ap):
        return ap.unsqueeze(2).to_broadcast([128, H, 128])

    for j in range(NP):
        pb = PBUF[j]
        SC, bSC = pb["SC"], pb["bSC"]
        ps = bk[3 + j][:, 0:16]
        gj = GG[:, j, :]
        for n, lhs in enumerate((mask2, bones, ch01[:, 0, :], ch01[:, 1, :])):
            P.pe(lambda e, n=n, lhs=lhs, ps=ps, gj=gj: e.matmul(ps[:, 4 * n:4 * n + 4], lhsT=lhs, rhs=gj,
                                                                 start=True, stop=True),
                 reads=[bBG, bc], writes=[bpP[j]])
        P.act(lambda e, SC=SC, ps=ps: e.activation(out=SC[:, 0, :], in_=ps[:, 0:4], func=AF.Copy),
              reads=[bpP[j]], writes=[bSC])
        P.dve(lambda e, SC=SC, ps=ps: e.tensor_tensor(out=SC[:, 5, :], in0=ps[:, 4:8], in1=SC[:, 0, :], op=ALU.subtract),
              reads=[bpP[j], bSC], writes=[bSC])
        P.act(lambda e, SC=SC, ps=ps: e.activation(out=SC[:, 6:8, :], in_=ps[:, 8:16].rearrange("p (a h) -> p a h", a=2),
                                                  func=AF.Exp), reads=[bpP[j]], writes=[bSC])
        P.dve(lambda e, SC=SC: e.tensor_scalar(out=SC[:, 1, :], in0=SC[:, 0, :], scalar1=-1.0, scalar2=None, op0=ALU.mult),
              reads=[bSC], writes=[bSC])
        P.dve(lambda e, SC=SC, j=j: e.tensor_tensor(out=SC[:, 2, :], in0=SC[:, 0, :], in1=LNB[:, j, :], op=ALU.add),
              reads=[bSC, bBG], writes=[bSC])
        P.act(lambda e, SC=SC: e.activation(out=SC[:, 3, :], in_=SC[:, 0, :], func=AF.Exp), reads=[bSC], writes=[bSC])
        P.act(lambda e, SC=SC: e.activation(out=SC[:, 5, :], in_=SC[:, 5, :], func=AF.Exp), reads=[bSC], writes=[bSC])
        P.dve(lambda e, SC=SC, j=j: e.scalar_tensor_tensor(out=SC[:, 4, :], in0=SC[:, 3, :], scalar=-1.0,
                                                          in1=BETA[:, j, :], op0=ALU.mult, op1=ALU.mult),
              reads=[bSC, bBG], writes=[bSC])
        yield
    for j in range(NP):
        pb = PBUF[j]
        P.pool(lambda e, pb=pb, j=j: e.tensor_copy(out=pb["GB"], in_=bc4(GG[:, j, :])), reads=[bBG], writes=[pb["bGB"]])
        yield
    for j in range(NP):
        pb = PBUF[j]
        for h in range(H):
            P.pe(lambda e, pb=pb, j=j, h=h: e.matmul(pP[j][:, h, :], lhsT=pb["GB"][:, h, :], rhs=mask2,
                                                     start=True, stop=True),
                 reads=[pb["bGB"], bc], writes=[bpP[j]])
        yield
    for j in range(NP):
        pb = PBUF[j]
        SC = pb["SC"]
        P.dve(lambda e, pb=pb, j=j, SC=SC: e.tensor_tensor(out=pb["E1"], in0=pP[j], in1=bc4(SC[:, 0, :]), op=ALU.max),
              reads=[bpP[j], pb["bSC"]], writes=[pb["bE1"]])
        if full:
            P.dve(lambda e, pb=pb, j=j, SC=SC: e.tensor_tensor(out=pb["E2"], in0=pP[j], in1=bc4(SC[:, 0, :]), op=ALU.min),
                  reads=[bpP[j], pb["bSC"]], writes=[pb["bE2"]])
            P.act(lambda e, pb=pb, j=j: e.activation(out=pb["EGR"], in_=pP[j], func=AF.Exp),
                  reads=[bpP[j]], writes=[pb["bEGR"]])
        yield
    for j in range(NP):
        pb = PBUF[j]
        SC = pb["SC"]
        for h in range(H):
            P.act(lambda e, pb=pb, h=h, SC=SC: e.activation(out=pb["E1"][:, h, :], in_=pb["E1"][:, h, :], func=AF.Exp,
                                                            scale=-1.0, bias=SC[:, 2, h:h + 1]),
                  reads=[pb["bE1"], pb["bSC"]], writes=[pb["bE1"]])
        if full:
            for h in range(H):
                P.act(lambda e, pb=pb, h=h, SC=SC: e.activation(out=pb["E2"][:, h, :], in_=pb["E2"][:, h, :], func=AF.Exp,
                                                                bias=SC[:, 1, h:h + 1]),
                      reads=[pb["bE2"], pb["bSC"]], writes=[pb["bE2"]])
        yield
    for j in range(NP):
        pb = PBUF[j]
        P.pool(lambda e, pb=pb: e.tensor_tensor(out=pb["E1"], in0=pb["E1"],
                                                in1=maskL.unsqueeze(1).to_broadcast([128, H, 128]), op=ALU.mult),
               reads=[pb["bE1"], bc], writes=[pb["bE1"]])
        if full:
            P.pool(lambda e, pb=pb: e.tensor_tensor(out=pb["E2"], in0=pb["E2"],
                                                    in1=mask2.unsqueeze(1).to_broadcast([128, H, 128]), op=ALU.mult),
                   reads=[pb["bE2"], bc], writes=[pb["bE2"]])
            c0 = j * 128
            P.dve(lambda e, pb=pb, c0=c0: e.tensor_tensor(out=pb["QTG"], in0=QT[:, :, c0:c0 + 128], in1=pb["EGR"], op=ALU.mult),
                  reads=bQT + [pb["bEGR"]], writes=[pb["bQTG"]])
        yield
    for j in range(NP):
        c0 = j * 128
        for h in range(H):
            P.pe(lambda e, j=j, h=h, c0=c0: e.matmul(pP[j][:, h, :], lhsT=KT[:, h, c0:c0 + 128], rhs=KT[:, h, c0:c0 + 128],
                                                     start=True, stop=True), reads=[bKT[h]], writes=[bpP[j]])
        yield
    for j in range(NP):
        pb = PBUF[j]
        P.dve(lambda e, pb=pb, j=j: e.tensor_tensor(out=pb["Lm"][0], in0=pP[j], in1=pb["E1"], op=ALU.mult),
              reads=[bpP[j], pb["bE1"]], writes=[pb["bLm"][0]])
        yield
    if full:
        for j in range(NP):
            c0 = j * 128
            for h in range(H):
                P.pe(lambda e, j=j, h=h, c0=c0: e.matmul(pP[j][:, h, :], lhsT=KT[:, h, c0:c0 + 128],
                                                         rhs=QT[:, h, c0:c0 + 128], start=True, stop=True),
                     reads=[bKT[h], bQT[h]], writes=[bpP[j]])
            yield
        for j in range(NP):
            pb = PBUF[j]
            P.dve(lambda e, pb=pb, j=j: e.tensor_tensor(out=pb["ATT"], in0=pP[j], in1=pb["E2"], op=ALU.mult),
                  reads=[bpP[j], pb["bE2"]], writes=[pb["bATT"]])
            yield
    for j in range(NP):
        pb = PBUF[j]
        for h in range(H):
            P.pe(lambda e, pb=pb, j=j, h=h: e.transpose(out=pPb[j][:, h, :], in_=pb["Lm"][0][:, h, :], identity=ident),
                 reads=[pb["bLm"][0], bc], writes=[bpP[j]])
        yield
    for j in range(NP):
        pb = PBUF[j]
        evac(pb["Um"][0], pPb[j], [bpP[j]], [pb["bUm"][0]])
        yield
    for j in range(NP):
        pb = PBUF[j]
        P.pool(lambda e, pb=pb: e.tensor_tensor(out=pb["Xm"][0], in0=identb4, in1=pb["Um"][0], op=ALU.subtract),
               reads=[pb["bUm"][0], bc], writes=[pb["bXm"][0]])
        yield
    cur, cx = 0, 0
    for lvl in range(5):
        for j in range(NP):
            pb = PBUF[j]
            for h in range(H):
                P.pe(lambda e, pb=pb, j=j, h=h, cur=cur: e.matmul(pP[j][:, h, :], lhsT=pb["Um"][cur][:, h, :],
                                                                  rhs=pb["Lm"][cur][:, h, :], start=True, stop=True),
                     reads=[pb["bUm"][cur], pb["bLm"][cur]], writes=[bpP[j]])
            yield
        for j in range(NP):
            pb = PBUF[j]
            evac(pb["Lm"][1 - cur], pP[j], [bpP[j]], [pb["bLm"][1 - cur]])
            yield
        if lvl < 4:
            for j in range(NP):
                pb = PBUF[j]
                for h in range(H):
                    P.pe(lambda e, pb=pb, j=j, h=h, cur=cur: e.matmul(pP[j][:, h, :], lhsT=pb["Lm"][cur][:, h, :],
                                                                      rhs=pb["Um"][cur][:, h, :], start=True, stop=True),
                         reads=[pb["bUm"][cur], pb["bLm"][cur]], writes=[bpP[j]])
                yield
            for j in range(NP):
                pb = PBUF[j]
                evac(pb["Um"][1 - cur], pP[j], [bpP[j]], [pb["bUm"][1 - cur]])
                yield
        for j in range(NP):
            pb = PBUF[j]
            for h in range(H):
                P.pe(lambda e, pb=pb, j=j, h=h, cur=cur, cx=cx: e.matmul(pP[j][:, h, :], lhsT=pb["Lm"][1 - cur][:, h, :],
                                                                         rhs=pb["Xm"][cx][:, h, :], start=True, stop=True),
                     reads=[pb["bLm"][1 - cur], pb["bXm"][cx]], writes=[bpP[j]])
            yield
        for j in range(NP):
            pb = PBUF[j]
            xo, bxo = (pb["TT"], pb["bTT"]) if lvl == 4 else (pb["Xm"][1 - cx], pb["bXm"][1 - cx])
            P.dve(lambda e, pb=pb, j=j, cx=cx, xo=xo: e.tensor_tensor(out=xo, in0=pP[j], in1=pb["Xm"][cx], op=ALU.add),
                  reads=[bpP[j], pb["bXm"][cx]], writes=[bxo])
            yield
        cur, cx = 1 - cur, 1 - cx
    for j in range(NP):
        pb = PBUF[j]
        c0 = j * 128
        SC = pb["SC"]
        for h in range(H):
            P.pe(lambda e, h=h, c0=c0, j=j: e.transpose(out=pPb[j][:, h, :], in_=KT[:, h, c0:c0 + 128], identity=ident),
                 reads=[bKT[h], bc], writes=[bpP[j]])
        P.dve(lambda e, pb=pb, SC=SC, j=j: e.tensor_tensor(out=pb["KHT"], in0=pPb[j], in1=bc4(SC[:, 5, :]), op=ALU.mult),
              reads=[bpP[j], pb["bSC"]], writes=[pb["bKHT"]])
        for h in range(H):
            P.pe(lambda e, h=h, c0=c0, j=j: e.transpose(out=pPb[j][:, h, :], in_=VT[:, h, c0:c0 + 128], identity=ident),
                 reads=[bVT[h], bc], writes=[bpP[j]])
        P.act(lambda e, pb=pb, j=j: e.activation(out=pb["VTK"], in_=pPb[j], func=AF.Copy), reads=[bpP[j]], writes=[pb["bVTK"]])
        P.pool(lambda e, pb=pb, c0=c0: e.tensor_copy(out=pb["KTP"], in_=KT[:, :, c0:c0 + 128]), reads=bKT, writes=[pb["bKTP"]])
        P.pool(lambda e, pb=pb, j=j: e.tensor_tensor(out=pb["BV"], in0=pb["VTK"], in1=bc4(BETA[:, j, :]), op=ALU.mult),
               reads=[pb["bVTK"], bBG], writes=[pb["bBV"]])
        yield


def _gdn_rec(self, L, full):
    P = self.P
    H = 4
    g = lambda n: L[n]
    bk, bb = self.banks, self.bbank
    bc = self.bconst
    mask2, ident = self.mask2, self.ident
    maskL, bones, ch01, identb4 = g("maskL"), g("bones"), g("ch01"), g("identb4")
    PBUF, GG, LNB, BETA, bBG = g("PB"), g("GG"), g("LNB"), g("BETA"), g("bBG")
    QT, KT, VT, bQT, bKT, bVT = g("QT"), g("KT"), g("VT"), g("bQT"), g("bKT"), g("bVT")
    R, USB, bR, bUSB = g("R"), g("USB"), g("bR"), g("bUSB")
    S32, SBF, bS32, bSBF, sbf_i = g("S32"), g("SBF"), g("bS32"), g("bSBF"), g("sbf_i")
    O32, bO32 = g("O32"), g("bO32")
    evac, nextpj = g("evac"), g("nextpj")
    NP = NT
    pP = [bk[3 + j][:].rearrange("p (h t) -> p h t", h=H) for j in range(NP)]
    pPb = [bk[3 + j][:].bitcast(BF16)[:, 0:512].rearrange("p (h t) -> p h t", h=H) for j in range(NP)]
    bpP = [bb[3 + j] for j in range(NP)]
    ptr4 = bk[5][:].bitcast(BF16)[:, 0:512].rearrange("p (h t) -> p h t", h=H)
    pKS = bk[5][:].rearrange("p (h t) -> p h t", h=H)
    pU = bk[6][:].rearrange("p (h t) -> p h t", h=H)
    po = bk[7][:].rearrange("p (h t) -> p h t", h=H)

    def bc4(ap):
        return ap.unsqueeze(2).to_broadcast([128, H, 128])

    for j in range(NP):
        pb = PBUF[j]
        c0 = j * 128
        SC = pb["SC"]
        TT, bTT = pb["TT"], pb["bTT"]
        for half in range(2):
            r0 = half * 64
            cs = sbf_i[0]
            for h in range(H):
                P.pe(lambda e, h=h, pb=pb, cs=cs: e.matmul(pKS[:, h, :], lhsT=pb["KTP"][:, h, :], rhs=SBF[cs][:, h, :],
                                                           start=True, stop=True),
                     reads=[pb["bKTP"], bSBF[cs][h]], writes=[bb[5]])
            if full:
                for h in range(H):
                    P.pe(lambda e, pb=pb, h=h, cs=cs, r0=r0, half=half: e.matmul(
                        po[:, h, r0:r0 + 64], lhsT=SBF[cs][:, h, :], rhs=pb["QTG"][:, h, r0:r0 + 64],
                        start=(h == 0 and half == 0), stop=False, skip_group_check=True),
                        reads=[bSBF[cs][h], pb["bQTG"]], writes=[bb[7]])
            for h in range(H):
                P.dve(lambda e, pb=pb, h=h, r0=r0, SC=SC: e.scalar_tensor_tensor(
                    out=R[r0:r0 + 64, h, :], in0=pKS[r0:r0 + 64, h, :], scalar=SC[r0:r0 + 64, 4, h:h + 1],
                    in1=pb["BV"][r0:r0 + 64, h, :], op0=ALU.mult, op1=ALU.add),
                    reads=[bb[5], pb["bSC"], pb["bBV"]], writes=[bR])
            yield
            for h in range(H):
                P.pe(lambda e, h=h, r0=r0, TT=TT: e.matmul(pU[:, h, :], lhsT=TT[r0:r0 + 64, h, :], rhs=R[r0:r0 + 64, h, :],
                                                           start=True, stop=True),
                     reads=[bTT, bR], writes=[bb[6]])
            yield
            P.act(lambda e, r0=r0: e.activation(out=USB[r0:r0 + 64], in_=pU[r0:r0 + 64], func=AF.Copy),
                  reads=[bb[6]], writes=[bUSB])
            yield
            pS, bpS = bk[6][:], bb[6]
            pS4 = pS.rearrange("p (h t) -> p h t", h=H)
            for h in range(H):
                P.pe(lambda e, pb=pb, h=h, r0=r0, pS4=pS4: e.matmul(pS4[:, h, :], lhsT=pb["KHT"][r0:r0 + 64, h, :],
                                                                    rhs=USB[r0:r0 + 64, h, :], start=True, stop=True),
                     reads=[pb["bKHT"], bUSB], writes=[bpS])
            yield
            for h in range(H):
                P.dve(lambda e, h=h, half=half, SC=SC, pS4=pS4: e.scalar_tensor_tensor(
                    out=S32[:, h, :], in0=S32[:, h, :], scalar=SC[:, 6 + half, h:h + 1], in1=pS4[:, h, :],
                    op0=ALU.mult, op1=ALU.add), reads=[bS32[h], pb["bSC"], bpS], writes=[bS32[h]])
            yield
            nx = 1 - cs
            P.act(lambda e, nx=nx: e.activation(out=SBF[nx], in_=S32, func=AF.Copy), reads=bS32, writes=bSBF[nx])
            sbf_i[0] = nx
            yield
        if full:
            for h in range(H):
                P.pe(lambda e, pb=pb, h=h: e.matmul(po[:, h, :], lhsT=USB[:, h, :], rhs=pb["ATT"][:, h, :],
                                                    start=False, stop=True, skip_group_check=True),
                     reads=[bUSB, pb["bATT"]], writes=[bb[7]])
            P.act(lambda e, c0=c0: e.activation(out=O32[:, :, c0:c0 + 128], in_=po, func=AF.Copy),
                  reads=[bb[7]], writes=[bO32])


KB._gdn_prep = _gdn_prep
KB._gdn_rec = _gdn_rec


CAP = 384
NEXP = 32
NSLOT = NEXP * CAP
BIG = 1.0e4


def _spill(self, name, sb_ap, bufs, sb):
    P = self.P
    if name not in self.d:
        self.d[name] = self.nc.dram_tensor(name, [128, 4, self.nfull_tok], BF16, kind="Internal").ap()
        self.bspill = getattr(self, "bspill", {})
        self.bspill[name] = {}
    dst = self.d[name]
    tok0 = (sb - self.npre) * SBT
    b = Buf(name)
    self.bspill[name][sb - self.npre] = b
    P.dma(lambda e: e.dma_start(out=dst[:, :, tok0:tok0 + SBT], in_=sb_ap), reads=bufs, writes=[b])


KB.spill = _spill


def _pass3(self):
    nc, P, A = self.nc, self.P, self.A
    d = self.d
    H = 4
    for nm, shp in (("hg_up", [512, D]), ("gd_up", [512, D]), ("w_out", [D, D]), ("norm_ffn_g", [D]),
                    ("router_w", [D, 36]), ("router_b", [36])):
        self.din(nm, shp)
    ntile = self.nfull_tok // 128
    d["h2_s"] = nc.dram_tensor("h2_s", [self.nfull_tok, D], F32, kind="Internal").ap()
    self.bh2s = [Buf("h2_s%d" % i) for i in range(ntile)]
    self.bxbuf = []
    self.W12 = A.alloc((ntile, 2), F32)
    self.DST = A.alloc((ntile, 2), U32)
    self.bW12 = Buf("W12")
    self.bDST = Buf("DST")
    m0 = A.mark()
    W3 = A.alloc((KC, 2048), BF16)
    bW3 = [Buf() for _ in range(KC)]
    wv = d["w_in"].rearrange("(k p) n -> p k n", p=128)
    for k in range(KC):
        P.dma(lambda e, k=k: e.dma_start(out=W3[:, k, :], in_=wv[:, k, C_PA:C_PA + 2048]), writes=[bW3[k]], q="pool")
    HGUP = A.alloc((H, D), BF16)
    GDUP = A.alloc((H, D), BF16)
    WOUT = A.alloc((KC, D), BF16)
    bUP = Buf()
    bWO = [Buf() for _ in range(KC)]
    P.dma(lambda e: e.dma_start(out=HGUP, in_=d["hg_up"].rearrange("(h p) n -> p h n", p=128)), writes=[bUP], q="pool")
    P.dma(lambda e: e.dma_start(out=GDUP, in_=d["gd_up"].rearrange("(h p) n -> p h n", p=128)), writes=[bUP], q="pool")
    wo = d["w_out"].rearrange("(k p) n -> p k n", p=128)
    for k in range(KC):
        P.dma(lambda e, k=k: e.dma_start(out=WOUT[:, k, :], in_=wo[:, k, :]), writes=[bWO[k]], q="pool")
    WR = A.alloc((KC, 36), F32)
    RB = A.alloc((36,), F32)
    G2 = A.alloc((D,), F32)
    ECAP = A.alloc((NEXP,), F32)
    bpar = Buf("par3")
    P.dma(lambda e: e.dma_start(out=WR, in_=d["router_w"].rearrange("(k p) n -> p k n", p=128)), writes=[bpar])
    P.dma(lambda e: e.dma_start(out=RB, in_=d["router_b"].partition_broadcast(128)), writes=[bpar])
    P.dma(lambda e: e.dma_start(out=G2, in_=d["norm_ffn_g"].partition_broadcast(128)), writes=[bpar])
    self.load_gain("norm_mix_g")
    ecapi = A.alloc((NEXP,), I32)
    P.pool(lambda e: e.iota(ecapi, pattern=[[CAP, NEXP]], base=0, channel_multiplier=0), writes=[bpar])
    P.pool(lambda e: e.tensor_copy(out=ECAP, in_=ecapi), reads=[bpar], writes=[bpar])
    triS = A.alloc((128,), BF16)
    trif = A.alloc((128,), F32)
    bc = self.bconst
    P.pool(lambda e: e.memset(trif, 1.0), writes=[bc])
    P.pool(lambda e: e.affine_select(out=trif, in_=trif, pattern=[[1, 128]], compare_op=ALU.is_gt, fill=0.0,
                                     base=0, channel_multiplier=-1), reads=[bc], writes=[bc])
    P.pool(lambda e: e.tensor_copy(out=triS, in_=trif), reads=[bc], writes=[bc])
    BASE = A.alloc((NEXP,), F32)
    bBASE = Buf()
    P.pool(lambda e: e.tensor_copy(out=BASE, in_=ecapi), reads=[bpar], writes=[bBASE])

    xnTs = [A.alloc((KC, SBT), BF16) for _ in range(2)]
    bxnTs = [Buf(), Buf()]
    self._xfetched = set()
    use_fetch = hasattr(self, "bxns")
    SGs = [A.alloc((16, SBT), BF16) for _ in range(2)]
    bSGs = [[Buf() for _ in range(16)] for _ in range(2)]
    OAss = [A.alloc((H, SBT), BF16) for _ in range(2)]
    OBss = [A.alloc((H, SBT), BF16) for _ in range(2)]
    bOAss, bOBss = [Buf(), Buf()], [Buf(), Buf()]
    T1 = [A.alloc((SBT,), F32) for _ in range(2)]
    T2 = [A.alloc((SBT,), F32) for _ in range(2)]
    bT1 = [Buf(), Buf()]
    bT2 = [Buf(), Buf()]
    MG = A.alloc((KC, SBT), BF16)
    bMG = [Buf() for _ in range(KC)]
    XR = A.alloc((D,), F32)
    bXR = Buf()
    H2 = A.alloc((D,), F32)
    bH2 = Buf()
    JK = A.alloc((D,), BF16)
    SS2 = A.alloc((1,), F32)
    bSS2 = Buf()
    XF = A.alloc((D,), F32)
    XB = A.alloc((D,), BF16)
    bXF, bXB = Buf(), Buf()
    XFT = A.alloc((KC, 128), F32)
    bXFT = Buf()
    LG = A.alloc((36,), F32)
    ME = A.alloc((NEXP,), F32)
    SM = A.alloc((16,), F32)
    M8 = A.alloc((8,), F32)
    SEL1 = A.alloc((NEXP,), F32)
    SEL2 = A.alloc((NEXP,), F32)
    SELB = A.alloc((NEXP,), BF16)
    RK = A.alloc((NEXP,), F32)
    JK2 = A.alloc((NEXP,), F32)
    DF = A.alloc((2,), F32)
    brt = Buf("route")

    bk, bb = self.banks, self.bbank
    ptr = bk[0][:].bitcast(BF16).rearrange("p (k t) -> p k t", k=KC)
    pj = [bk[1][:], bk[2][:]]
    bpj = [bb[1], bb[2]]
    pup = [bk[3][:], bk[4][:]]
    pji = [0]

    def nextpj():
        i = pji[0] % 2
        pji[0] += 1
        return pj[i], bpj[i]

    pyi = [0]

    def nextpy():
        i = 5 + pyi[0] % 3
        pyi[0] += 1
        return bk[i][:], bb[i]

    oa_d, ob_d = d["oa_s"], d["ob_s"]
    def X3(sbi):
        sl = sbi % 2
        SG, bSG, OAs, OBs, bOAs, bOBs = SGs[sl], bSGs[sl], OAss[sl], OBss[sl], bOAss[sl], bOBss[sl]
        sb = self.npre + sbi
        tok0 = sbi * SBT
        xnT, bxnT = xnTs[sb % 2], bxnTs[sb % 2]
        if use_fetch:
            yield from self.xnt_fetch_gen(sb, self.nsb - 1, xnTs, bxnTs)
        else:
            yield from self.stage_a_gen(sb, xnT, bxnT, ptr, bb[0])
        P.dma(lambda e, tok0=tok0: e.dma_start(out=OAs, in_=oa_d[:, :, tok0:tok0 + SBT]),
              reads=[self.bspill["oa_s"][sbi]], writes=[bOAs])
        P.dma(lambda e, tok0=tok0: e.dma_start(out=OBs, in_=ob_d[:, :, tok0:tok0 + SBT]),
              reads=[self.bspill["ob_s"][sbi]], writes=[bOBs])
        for cb in range(16):
            p, bp = nextpj()
            for k in range(KC):
                P.pe(lambda e, k=k, p=p, cb=cb: e.matmul(p[:, 0:SBT], lhsT=W3[:, k, cb * 128:(cb + 1) * 128],
                                                         rhs=xnT[:, k, :], start=(k == 0), stop=(k == KC - 1)),
                     reads=[bW3[k], bxnT], writes=[bp])
            P.act(lambda e, cb=cb, p=p: e.activation(out=SG[:, cb, :], in_=p[:, 0:SBT], func=AF.Sigmoid),
                  reads=[bp], writes=[bSG[cb]])
            yield
    def Y3(sbi):
        sl = sbi % 2
        SG, bSG, OAs, OBs, bOAs, bOBs = SGs[sl], bSGs[sl], OAss[sl], OBss[sl], bOAss[sl], bOBss[sl]
        sb = self.npre + sbi
        tok0 = sbi * SBT
        for cb in range(KC):
            i = cb % 2
            for h in range(H):
                P.pe(lambda e, h=h, cb=cb: e.matmul(pup[0][:, 0:SBT], lhsT=HGUP[:, h, cb * 128:(cb + 1) * 128],
                                                    rhs=OAs[:, h, :], start=(h == 0), stop=(h == H - 1)),
                     reads=[bUP, bOAs], writes=[bb[3]])
            for h in range(H):
                P.pe(lambda e, h=h, cb=cb: e.matmul(pup[1][:, 0:SBT], lhsT=GDUP[:, h, cb * 128:(cb + 1) * 128],
                                                    rhs=OBs[:, h, :], start=(h == 0), stop=(h == H - 1)),
                     reads=[bUP, bOBs], writes=[bb[4]])
            P.dve(lambda e, cb=cb, i=i: e.tensor_tensor(out=T1[i], in0=pup[0][:, 0:SBT], in1=SG[:, cb, :], op=ALU.mult),
                  reads=[bb[3], bSG[cb]], writes=[bT1[i]])
            P.dve(lambda e, cb=cb, i=i: e.tensor_tensor(out=T2[i], in0=pup[1][:, 0:SBT], in1=SG[:, 8 + cb, :], op=ALU.mult),
                  reads=[bb[4], bSG[8 + cb]], writes=[bT2[i]])
            P.pool(lambda e, cb=cb, i=i: e.tensor_tensor(out=MG[:, cb, :], in0=T1[i], in1=T2[i], op=ALU.add),
                   reads=[bT1[i], bT2[i]], writes=[bMG[cb]])
            yield
        for t in range(NT):
            gt = sbi * NT + t
            gtok = self.npre * SBT + gt * 128
            P.dma(lambda e, gtok=gtok: e.dma_start(out=XR, in_=d["xs"][gtok:gtok + 128, :]), writes=[bXR])
            for half in range(2):
                p, bp = nextpy()
                for k in range(KC):
                    P.pe(lambda e, k=k, p=p, t=t, half=half: e.matmul(
                        p, lhsT=MG[:, k, t * 128:(t + 1) * 128], rhs=WOUT[:, k, half * 512:(half + 1) * 512],
                        start=(k == 0), stop=(k == KC - 1)), reads=[bMG[k], bWO[k]], writes=[bp])
                P.dve(lambda e, p=p, half=half: e.tensor_tensor(out=H2[:, half * 512:(half + 1) * 512], in0=p,
                                                               in1=XR[:, half * 512:(half + 1) * 512], op=ALU.add),
                      reads=[bp, bXR], writes=[bH2])
                yield
            P.dma(lambda e, gt=gt: e.dma_start(out=d["h2_s"][gt * 128:(gt + 1) * 128, :], in_=H2),
                  reads=[bH2], writes=[self.bh2s[gt]])
            LL = dict(L0)
            LL['nextpj'] = nextpy
            yield from self._route_tile(LL, gt)
    L0 = dict(locals())

    import os
    WTS3 = [int(v) for v in os.environ.get("IL_W3", "1,1").split(",")]

    def run_il(gens):
        gens = list(gens)
        while gens:
            for g_, w_ in list(gens):
                for _ in range(w_):
                    try:
                        next(g_)
                    except StopIteration:
                        gens.remove((g_, w_))
                        break

    n = self.nfull
    if LS >= 2:
        sx = [P._capture(X3(i)) for i in range(n)]
        sy = [P._capture(Y3(i)) for i in range(n)]
        P.merge_pipeline([sx, sy], [lambda s_, dn: dn[1] >= s_ - 1, lambda s_, dn: dn[0] >= s_ + 1])
    else:
        for r in range(-1, n):
            gs = []
            if 0 <= r < n:
                gs.append((Y3(r), WTS3[0]))
            if 0 <= r + 1 < n:
                gs.append((X3(r + 1), WTS3[1]))
            if LS:
                P.merge_streams([g_ for g_, _w in gs])
            else:
                run_il(gs)
    A.reset(m0)
    P.barrier()


KB.pass3 = _pass3


def _route_tile(self, L, gt):
    P = self.P
    g = lambda n: L[n]
    bk, bb = self.banks, self.bbank
    bc = self.bconst
    H2, bH2, JK, SS2, bSS2 = g("H2"), g("bH2"), g("JK"), g("SS2"), g("bSS2")
    XF, XB, bXF, bXB, XFT, bXFT = g("XF"), g("XB"), g("bXF"), g("bXB"), g("XFT"), g("bXFT")
    G2, WR, RB, ECAP, bpar = g("G2"), g("WR"), g("RB"), g("ECAP"), g("bpar")
    LG, ME, SM, M8, SEL1, SEL2, SELB, RK, JK2, DF, brt = (g("LG"), g("ME"), g("SM"), g("M8"), g("SEL1"), g("SEL2"),
                                                           g("SELB"), g("RK"), g("JK2"), g("DF"), g("brt"))
    BASE, bBASE, triS = g("BASE"), g("bBASE"), g("triS")
    nextpj = g("nextpj")
    W12, DST = self.W12, self.DST
    P.act(lambda e: e.activation(out=JK, in_=H2, func=AF.Square, accum_out=SS2), reads=[bH2], writes=[bSS2])
    P.act(lambda e: e.activation(out=SM[:, 0:1], in_=SS2, func=AF.Ln, scale=1.0 / D, bias=EPS), reads=[bSS2], writes=[brt])
    P.act(lambda e: e.activation(out=SM[:, 0:1], in_=SM[:, 0:1], func=AF.Exp, scale=-0.5), reads=[brt], writes=[brt])
    P.dve(lambda e: e.scalar_tensor_tensor(out=XF, in0=H2, scalar=SM[:, 0:1], in1=G2, op0=ALU.mult, op1=ALU.mult),
          reads=[bH2, brt, bpar], writes=[bXF])
    P.pool(lambda e: e.tensor_copy(out=XB, in_=XF), reads=[bXF], writes=[bXB])
    yield
    for half in range(2):
        for kk in range(4):
            k = half * 4 + kk
            P.pe(lambda e, k=k, kk=kk, half=half: e.transpose(out=bk[3 + half][:, kk * 128:(kk + 1) * 128],
                                                              in_=XF[:, k * 128:(k + 1) * 128], identity=self.identf),
                 reads=[bXF, bc], writes=[bb[3 + half]])
    P.act(lambda e: e.activation(out=XFT[:, 0:4, :], in_=bk[3][:].rearrange("p (k t) -> p k t", k=4), func=AF.Copy),
          reads=[bb[3]], writes=[bXFT])
    P.dve(lambda e: e.tensor_copy(out=XFT[:, 4:8, :], in_=bk[4][:].rearrange("p (k t) -> p k t", k=4)),
          reads=[bb[4]], writes=[bXFT])
    yield
    p, bp = nextpj()
    for k in range(KC):
        P.pe(lambda e, k=k, p=p: e.matmul(p[:, 0:36], lhsT=XFT[:, k, :], rhs=WR[:, k, :], start=(k == 0), stop=(k == KC - 1)),
             reads=[bXFT, bpar], writes=[bp])
    P.dve(lambda e, p=p: e.tensor_tensor(out=LG, in0=p[:, 0:36], in1=RB, op=ALU.add), reads=[bp, bpar], writes=[brt])
    yield
    P.dve(lambda e: e.tensor_reduce(out=SM[:, 1:2], in_=LG[:, 0:4], axis=AX.X, op=ALU.max), reads=[brt], writes=[brt])
    P.dve(lambda e: e.tensor_scalar(out=SM[:, 2:3], in0=SM[:, 1:2], scalar1=-1.0, scalar2=None, op0=ALU.mult),
          reads=[brt], writes=[brt])
    P.act(lambda e: e.activation(out=JK2[:, 0:4], in_=LG[:, 0:4], func=AF.Exp, bias=SM[:, 2:3], accum_out=SM[:, 3:4]),
          reads=[brt], writes=[brt])
    P.dve(lambda e: e.reciprocal(out=SM[:, 4:5], in_=SM[:, 3:4]), reads=[brt], writes=[brt])
    yield
    P.dve(lambda e: e.tensor_scalar(out=SM[:, 12:16], in0=LG[:, 0:4], scalar1=SM[:, 1:2], scalar2=-1.0,
                                    op0=ALU.is_equal, op1=ALU.add), reads=[brt], writes=[brt])
    P.dve(lambda e: e.scalar_tensor_tensor(out=ME.rearrange("p (g j) -> p g j", g=4),
                                           in0=SM[:, 12:16].unsqueeze(2).to_broadcast([128, 4, 8]), scalar=BIG,
                                           in1=LG[:, 4:36].rearrange("p (g j) -> p g j", g=4),
                                           op0=ALU.mult, op1=ALU.add), reads=[brt], writes=[brt])
    P.dve(lambda e: e.max(out=M8, in_=ME), reads=[brt], writes=[brt])
    yield
    P.dve(lambda e: e.tensor_tensor(out=SM[:, 5:6], in0=M8[:, 1:2], in1=M8[:, 0:1], op=ALU.subtract), reads=[brt], writes=[brt])
    P.act(lambda e: e.activation(out=SM[:, 6:7], in_=SM[:, 5:6], func=AF.Exp), reads=[brt], writes=[brt])
    P.dve(lambda e: e.tensor_scalar(out=SM[:, 7:8], in0=SM[:, 6:7], scalar1=1.0, scalar2=None, op0=ALU.add),
          reads=[brt], writes=[brt])
    P.dve(lambda e: e.reciprocal(out=SM[:, 8:9], in_=SM[:, 7:8]), reads=[brt], writes=[brt])
    P.dve(lambda e, gt=gt: e.tensor_tensor(out=W12[:, gt, 0:1], in0=SM[:, 8:9], in1=SM[:, 4:5], op=ALU.mult),
          reads=[brt], writes=[self.bW12])
    P.dve(lambda e, gt=gt: e.tensor_tensor(out=W12[:, gt, 1:2], in0=SM[:, 4:5], in1=W12[:, gt, 0:1], op=ALU.subtract),
          reads=[brt, self.bW12], writes=[self.bW12])
    P.dve(lambda e: e.tensor_scalar(out=SEL1, in0=ME, scalar1=M8[:, 0:1], scalar2=None, op0=ALU.is_equal),
          reads=[brt], writes=[brt])
    P.dve(lambda e: e.tensor_scalar(out=SEL2, in0=ME, scalar1=M8[:, 1:2], scalar2=None, op0=ALU.is_equal),
          reads=[brt], writes=[brt])
    P.dve(lambda e: e.tensor_tensor(out=SELB, in0=SEL1, in1=SEL2, op=ALU.add), reads=[brt], writes=[brt])
    yield
    p2, bp2 = nextpj()
    P.pe(lambda e, p2=p2: e.matmul(p2[:, 0:32], lhsT=triS, rhs=SELB, start=True, stop=True), reads=[brt, bc], writes=[bp2])
    P.pe(lambda e, p2=p2: e.matmul(p2[:, 32:64], lhsT=self.ones_bf, rhs=SELB, start=True, stop=True),
         reads=[brt, bc], writes=[bp2])
    P.dve(lambda e, p2=p2: e.tensor_tensor(out=RK, in0=p2[:, 0:32], in1=BASE, op=ALU.add), reads=[bp2, bBASE], writes=[brt])
    P.dve(lambda e, p2=p2: e.tensor_tensor(out=BASE, in0=p2[:, 32:64], in1=BASE, op=ALU.add), reads=[bp2, bBASE], writes=[bBASE])
    P.dve(lambda e: e.scalar_tensor_tensor(out=JK2, in0=SEL1, scalar=1.0, in1=RK, op0=ALU.mult, op1=ALU.mult,
                                           accum_out=DF[:, 0:1]), reads=[brt], writes=[brt])
    P.dve(lambda e: e.scalar_tensor_tensor(out=JK2, in0=SEL2, scalar=1.0, in1=RK, op0=ALU.mult, op1=ALU.mult,
                                           accum_out=DF[:, 1:2]), reads=[brt], writes=[brt])
    P.dve(lambda e, gt=gt: e.tensor_copy(out=DST[:, gt, :], in_=DF), reads=[brt], writes=[self.bDST])
    yield
    xb = self.d["x_buf"]
    for k in range(2):
        bx = Buf("xbuf")
        self.bxbuf.append(bx)
        P.dma(lambda e, gt=gt, k=k: e.indirect_dma_start(
            out=xb, out_offset=bass.IndirectOffsetOnAxis(ap=DST[:, gt, k:k + 1], axis=0), in_=XB, in_offset=None),
            reads=[bXB, self.bDST] + self.bxz, writes=[bx], q="pool")


KB._route_tile = _route_tile


def _pass4(self):
    nc, P, A = self.nc, self.P, self.A
    d = self.d
    self.din("w_gate", [NEXP, D, 512])
    self.din("w_up", [NEXP, D, 512])
    self.din("w_down", [NEXP, 512, D])
    d["y_buf"] = nc.dram_tensor("y_buf", [NSLOT, D], F32, kind="Internal").ap()
    self.bybuf = []
    m0 = A.mark()
    NB = CAP // 128
    WG = [A.alloc((KC, 512), BF16) for _ in range(2)]
    WU = [A.alloc((KC, 512), BF16) for _ in range(2)]
    WD = [A.alloc((4, D), BF16) for _ in range(2)]
    bWG = [Buf(), Buf()]
    bWU = [Buf(), Buf()]
    bWD = [Buf(), Buf()]
    XE = [A.alloc((NB, D), BF16) for _ in range(2)]
    bXE = [Buf(), Buf()]
    XET = [A.alloc((KC, CAP), BF16) for _ in range(2)]
    bXET = [Buf(), Buf()]
    SGT = [A.alloc((CAP,), F32) for _ in range(2)]
    bSGT = [Buf(), Buf()]
    HT = A.alloc((4, CAP), BF16)
    bHT = [Buf() for _ in range(4)]
    YS = [A.alloc((D,), F32) for _ in range(2)]
    bYS = [Buf(), Buf()]
    bk, bb = self.banks, self.bbank
    ptr = bk[0][:].bitcast(BF16).rearrange("p (k t) -> p k t", k=KC)
    ident = self.ident
    bc = self.bconst
    xb, yb = d["x_buf"], d["y_buf"]
    cnt = [0]

    def bank(lo, n):
        i = lo + cnt[0] % n
        cnt[0] += 1
        return bk[i][:], bb[i]

    def load_w(e):
        i = e % 2
        P.dma(lambda eng, e=e, i=i: eng.dma_start(out=WG[i], in_=d["w_gate"][e].rearrange("(k p) n -> p k n", p=128)),
              writes=[bWG[i]], q="pool")
        P.dma(lambda eng, e=e, i=i: eng.dma_start(out=WU[i], in_=d["w_up"][e].rearrange("(k p) n -> p k n", p=128)),
              writes=[bWU[i]], q="pool")
        P.dma(lambda eng, e=e, i=i: eng.dma_start(out=WD[i], in_=d["w_down"][e].rearrange("(k p) n -> p k n", p=128)),
              writes=[bWD[i]], q="pool")

    def load_x(e):
        i = e % 2
        P.dma(lambda eng, e=e, i=i: eng.dma_start(out=XE[i], in_=xb[e * CAP:(e + 1) * CAP, :].rearrange("(b p) n -> p b n", p=128)),
              reads=self.bxbuf, writes=[bXE[i]])

    def transposes(e):
        i = e % 2
        for b in range(NB):
            pt, bpt = (ptr, bb[0]) if b % 2 == 0 else (ptr7, bb[7])
            for k in range(KC):
                P.pe(lambda eng, i=i, b=b, k=k, pt=pt: eng.transpose(out=pt[:, k, :], in_=XE[i][:, b, k * 128:(k + 1) * 128], identity=ident),
                     reads=[bXE[i], bc], writes=[bpt])
            if b % 2 == 0:
                P.act(lambda eng, b=b, i=i, pt=pt: eng.activation(out=XET[i][:, :, b * 128:(b + 1) * 128], in_=pt, func=AF.Copy),
                      reads=[bpt], writes=[bXET[i]])
            else:
                P.dve(lambda eng, b=b, i=i, pt=pt: eng.tensor_copy(out=XET[i][:, :, b * 128:(b + 1) * 128], in_=pt),
                      reads=[bpt], writes=[bXET[i]])

    ptr7 = bk[7][:].bitcast(BF16).rearrange("p (k t) -> p k t", k=KC)
    load_w(0)
    load_x(0)
    transposes(0)
    yi = 0
    for e in range(NEXP):
        i = e % 2
        if e + 1 < NEXP:
            load_w(e + 1)
            load_x(e + 1)
        for fc in range(4):
            pg, bpg = bk[1 + fc % 2][:], bb[1 + fc % 2]
            pu, bpu = bk[3 + fc % 2][:], bb[3 + fc % 2]
            for k in range(KC):
                P.pe(lambda eng, i=i, fc=fc, k=k, pg=pg: eng.matmul(pg[:, 0:CAP], lhsT=WG[i][:, k, fc * 128:(fc + 1) * 128],
                                                                    rhs=XET[i][:, k, :], start=(k == 0), stop=(k == KC - 1)),
                     reads=[bWG[i], bXET[i]], writes=[bpg])
            for k in range(KC):
                P.pe(lambda eng, i=i, fc=fc, k=k, pu=pu: eng.matmul(pu[:, 0:CAP], lhsT=WU[i][:, k, fc * 128:(fc + 1) * 128],
                                                                    rhs=XET[i][:, k, :], start=(k == 0), stop=(k == KC - 1)),
                     reads=[bWU[i], bXET[i]], writes=[bpu])
            j = fc % 2
            P.act(lambda eng, pg=pg, j=j: eng.activation(out=SGT[j], in_=pg[:, 0:CAP], func=AF.Silu), reads=[bpg], writes=[bSGT[j]])
            P.dve(lambda eng, pu=pu, j=j, fc=fc: eng.tensor_tensor(out=HT[:, fc, :], in0=pu[:, 0:CAP], in1=SGT[j], op=ALU.mult),
                  reads=[bpu, bSGT[j]], writes=[bHT[fc]])
        if e + 1 < NEXP:
            transposes(e + 1)
        for b in range(NB):
            ys, bys = YS[yi % 2], bYS[yi % 2]
            yi += 1
            for half in range(2):
                pd, bpd = bk[5 + half][:], bb[5 + half]
                for fc in range(4):
                    P.pe(lambda eng, i=i, b=b, fc=fc, half=half, pd=pd: eng.matmul(
                        pd, lhsT=HT[:, fc, b * 128:(b + 1) * 128], rhs=WD[i][:, fc, half * 512:(half + 1) * 512],
                        start=(fc == 0), stop=(fc == 3)), reads=[bHT[fc], bWD[i]], writes=[bpd])
                if half == 0:
                    P.act(lambda eng, pd=pd, ys=ys: eng.activation(out=ys[:, 0:512], in_=pd, func=AF.Copy), reads=[bpd], writes=[bys])
                else:
                    P.dve(lambda eng, pd=pd, ys=ys: eng.tensor_copy(out=ys[:, 512:1024], in_=pd), reads=[bpd], writes=[bys])
            r0 = e * CAP + b * 128
            by = Buf("ybuf")
            self.bybuf.append(by)
            P.dma(lambda eng, r0=r0, ys=ys: eng.dma_start(out=yb[r0:r0 + 128, :], in_=ys), reads=[bys], writes=[by])
    A.reset(m0)
    P.barrier()


def _pass5(self):
    nc, P, A = self.nc, self.P, self.A
    d = self.d
    self.din("final_norm_g", [D])
    out = self.dout("out", [self.nfull_tok, D], F32)
    m0 = A.mark()
    ntile = self.nfull_tok // 128
    FG = A.alloc((D,), F32)
    bFG = Buf()
    P.dma(lambda e: e.dma_start(out=FG, in_=d["final_norm_g"].partition_broadcast(128)), writes=[bFG])
    NB5 = 4
    Y1 = [A.alloc((D,), F32) for _ in range(NB5)]
    Y2 = [A.alloc((D,), F32) for _ in range(NB5)]
    HH = [A.alloc((D,), F32) for _ in range(NB5)]
    OT = [A.alloc((D,), F32) for _ in range(NB5)]
    bY1, bY2, bHH, bOT = ([Buf() for _ in range(NB5)], [Buf() for _ in range(NB5)], [Buf() for _ in range(NB5)],
                          [Buf() for _ in range(NB5)])
    JK = A.alloc((D,), BF16)
    SS = A.alloc((ntile,), F32)
    bSS = Buf()
    yb = d["y_buf"]
    for gt in range(ntile):
        i = gt % NB5
        P.dma(lambda e, gt=gt, i=i: e.indirect_dma_start(
            out=Y1[i], out_offset=None, in_=yb, in_offset=bass.IndirectOffsetOnAxis(ap=self.DST[:, gt, 0:1], axis=0)),
            reads=self.bybuf + [self.bDST], writes=[bY1[i]], q="pool")
        P.dma(lambda e, gt=gt, i=i: e.indirect_dma_start(
            out=Y2[i], out_offset=None, in_=yb, in_offset=bass.IndirectOffsetOnAxis(ap=self.DST[:, gt, 1:2], axis=0)),
            reads=self.bybuf + [self.bDST], writes=[bY2[i]], q="pool")
        P.dma(lambda e, gt=gt, i=i: e.dma_start(out=HH[i], in_=d["h2_s"][gt * 128:(gt + 1) * 128, :]),
              reads=[self.bh2s[gt]], writes=[bHH[i]])
        P.dve(lambda e, gt=gt, i=i: e.scalar_tensor_tensor(out=HH[i], in0=Y1[i], scalar=self.W12[:, gt, 0:1], in1=HH[i],
                                                          op0=ALU.mult, op1=ALU.add),
              reads=[bY1[i], bHH[i], self.bW12], writes=[bHH[i]])
        P.dve(lambda e, gt=gt, i=i: e.scalar_tensor_tensor(out=HH[i], in0=Y2[i], scalar=self.W12[:, gt, 1:2], in1=HH[i],
                                                          op0=ALU.mult, op1=ALU.add),
              reads=[bY2[i], bHH[i], self.bW12], writes=[bHH[i]])
        P.act(lambda e, gt=gt, i=i: e.activation(out=JK, in_=HH[i], func=AF.Square, accum_out=SS[:, gt:gt + 1]),
              reads=[bHH[i]], writes=[bSS])
        P.act(lambda e, gt=gt: e.activation(out=SS[:, gt:gt + 1], in_=SS[:, gt:gt + 1], func=AF.Ln, scale=1.0 / D, bias=EPS),
              reads=[bSS], writes=[bSS])
        P.act(lambda e, gt=gt: e.activation(out=SS[:, gt:gt + 1], in_=SS[:, gt:gt + 1], func=AF.Exp, scale=-0.5),
              reads=[bSS], writes=[bSS])
        P.dve(lambda e, gt=gt, i=i: e.scalar_tensor_tensor(out=OT[i], in0=HH[i], scalar=SS[:, gt:gt + 1], in1=FG,
                                                          op0=ALU.mult, op1=ALU.mult),
              reads=[bHH[i], bSS, bFG], writes=[bOT[i]])
        P.dma(lambda e, gt=gt, i=i: e.dma_start(out=out[gt * 128:(gt + 1) * 128, :], in_=OT[i]), reads=[bOT[i]], out=True)
    A.reset(m0)


KB.pass4 = _pass4
KB.pass5 = _pass5


NPRE_SB = 17
NFULL_SB = 16
_NC_CACHE = {}


def _build_full():
    if "nc" not in _NC_CACHE:
        kb = KB(NPRE_SB, NFULL_SB)
        kb.setup()
        kb.pass1()
        kb.pass2()
        kb.pass3()
        kb.pass4()
        kb.pass5()
        _NC_CACHE["nc"] = kb.finish()
    return _NC_CACHE["nc"]


def kernel(x, meta_tokens, hg_lb_logits, norm_mix_g, w_in, gd_conv_w, gd_A_log, gd_dt_bias, hg_norm_g, gd_norm_g,
           hg_up, gd_up, w_out, norm_ffn_g, router_group_w, router_group_b, router_expert_w, router_expert_b,
           w_gate, w_up, w_down, final_norm_g):
    f32 = np.float32
    c = lambda a: np.ascontiguousarray(np.asarray(a, dtype=f32))
    x = c(x)
    meta = c(meta_tokens)
    B, S, _ = x.shape
    half = S // 2
    npre_tok = NPRE_SB * SBT
    ntok = (NPRE_SB + NFULL_SB) * SBT
    nmeta = meta.shape[0]
    shared = {
        "w_in": c(w_in[0]),
        "norm_mix_g": c(norm_mix_g[0]),
        "hg_lb": c(np.asarray(hg_lb_logits, f32).reshape(2, 4, 128).transpose(2, 0, 1)),
        "hg_norm_g": c(hg_norm_g[0]),
        "conv_wT": c(np.asarray(gd_conv_w[0], f32).reshape(4, 12, 128).transpose(2, 0, 1)),
        "gd_A_log": c(gd_A_log[0]),
        "gd_dt_bias": c(gd_dt_bias[0]),
        "gd_norm_g": c(gd_norm_g[0]),
        "hg_up": c(hg_up[0]),
        "gd_up": c(gd_up[0]),
        "w_out": c(w_out[0]),
        "norm_ffn_g": c(norm_ffn_g[0]),
        "router_w": c(np.concatenate([np.asarray(router_group_w[0], f32), np.asarray(router_expert_w[0], f32)], axis=1)),
        "router_b": c(np.concatenate([np.asarray(router_group_b[0], f32), np.asarray(router_expert_b[0], f32)])),
        "w_gate": c(w_gate[0]),
        "w_up": c(w_up[0]),
        "w_down": c(w_down[0]),
        "final_norm_g": c(final_norm_g),
    }
    in_maps = []
    for core in range(2 * B):
        b, hf = core // 2, core % 2
        xs = np.zeros((ntok, D), f32)
        if hf == 0:
            xs[npre_tok - nmeta:npre_tok] = meta
            xs[npre_tok:] = x[b, 0:half]
        else:
            xs[npre_tok - half - nmeta:npre_tok - half] = meta
            xs[npre_tok - half:] = x[b]
        m = dict(shared)
        m["xs"] = xs
        in_maps.append(m)
    nc = _build_full()
    res = run_bass_kernel_spmd(nc, in_maps, core_ids=list(range(2 * B)))
    out = np.empty((B, S, D), f32)
    for core in range(2 * B):
        b, hf = core // 2, core % 2
        out[b, hf * half:(hf + 1) * half] = np.asarray(res.results[core]["out"], dtype=f32)
    return out

LCOST_TABLE = {('dve', 389): 0.041, ('act', 389): 0.029, ('pool', 389): 0.051, ('pool', 303): 0.019, ('pool', 425): 0.166, ('pool', 549): 0.643, ('pool', 305): 0.019, ('pool', 551): 0.626, ('pool', 625): 0.173, ('pool', 626): 0.154, ('dve', 305): 0.024, ('act', 303): 0.019, ('act', 563): 0.745, ('act', 305): 0.019, ('act', 482): 0.55, ('act', 489): 0.419, ('act', 492): 0.148, ('dve', 303): 0.024, ('dve', 498): 1.239, ('pe', 503): 0.13, ('dve', 506): 0.691, ('pe', 632): 0.159, ('act', 653): 0.563, ('dve', 668): 0.39, ('pe', 723): 0.354, ('dve', 672): 0.694, ('act', 675): 0.477, ('dve', 678): 0.604, ('act', 686): 0.507, ('act', 690): 0.1, ('act', 727): 0.629, ('dve', 691): 0.693, ('pe', 729): 0.111, ('dve', 732): 0.425, ('pe', 742): 0.112, ('dve', 746): 0.325, ('act', 752): 0.329, ('act', 659): 0.541, ('act', 664): 0.355, ('dve', 696): 0.913, ('dve', 700): 0.249, ('act', 702): 0.334, ('act', 705): 0.12, ('act', 708): 0.306, ('dve', 711): 0.647, ('dve', 714): 1.226, ('dve', 718): 0.601, ('pe', 762): 0.094, ('pe', 776): 0.114, ('pe', 781): 0.204, ('dve', 765): 0.668, ('dve', 785): 0.325, ('act', 790): 0.362, ('pe', 796): 0.108, ('act', 800): 0.604, ('act', 807): 0.333, ('pe', 811): 0.326, ('act', 813): 0.458, ('act', 815): 0.374, ('dve', 817): 0.412, ('dve', 819): 0.473, ('dve', 930): 0.286, ('pe', 1044): 0.148, ('pool', 1036): 0.157, ('pool', 1037): 0.153, ('pool', 1039): 0.264, ('act', 1056): 0.56, ('dve', 1058): 0.566, ('pe', 1085): 0.02, ('pe', 1113): 0.134, ('act', 1088): 0.162, ('act', 1091): 0.181, ('act', 1092): 0.181, ('dve', 1093): 0.148, ('act', 1095): 0.181, ('dve', 1096): 0.236, ('act', 1098): 0.209, ('act', 1099): 0.208, ('act', 1117): 0.423, ('dve', 1100): 0.227, ('pool', 1129): 0.587, ('act', 1120): 0.356, ('pe', 1132): 0.286, ('act', 1134): 0.504, ('act', 1136): 0.407, ('pool', 1139): 0.721, ('pe', 1266): 0.171, ('act', 1269): 0.25, ('pool', 1076): 0.145, ('dve', 1271): 0.161, ('act', 1273): 0.15, ('pool', 1288): 1.826, ('dve', 1275): 0.568, ('dve', 1277): 0.494, ('act', 1279): 0.175, ('act', 1280): 0.177, ('dve', 1281): 0.165, ('pe', 1293): 0.139, ('dve', 1300): 0.668, ('act', 1312): 0.352, ('pe', 1338): 0.07, ('pool', 1323): 1.283, ('dve', 1343): 0.646, ('pe', 1362): 0.111, ('pe', 1380): 0.111, ('pool', 1371): 1.14, ('pe', 1392): 0.109, ('pe', 1403): 0.114, ('dve', 1411): 0.685, ('pool', 1429): 1.83, ('pe', 1421): 0.102, ('dve', 1423): 0.668, ('pe', 1426): 0.133, ('act', 1428): 0.673, ('pool', 1430): 1.027, ('pe', 1471): 0.128, ('dve', 1481): 0.348, ('pe', 1487): 0.141, ('act', 1491): 0.682, ('pe', 1497): 0.144, ('dve', 1502): 0.272, ('act', 1507): 0.693, ('act', 1106): 0.539, ('dve', 1303): 0.598, ('act', 1305): 0.569, ('dve', 1331): 0.332, ('act', 1317): 0.336, ('pool', 1327): 1.266, ('pe', 1350): 0.146, ('dve', 1356): 0.665, ('pe', 1476): 0.065, ('pe', 1512): 0.108, ('act', 1515): 0.631, ('act', 1166): 0.319, ('pe', 1170): 0.365, ('act', 1172): 0.354, ('act', 1174): 0.36, ('dve', 1175): 0.57, ('dve', 1177): 0.7, ('pool', 1571): 0.636, ('pe', 1682): 0.126, ('pool', 1581): 0.638, ('act', 1685): 0.48, ('pe', 1697): 0.145, ('pe', 1701): 0.122, ('dve', 1704): 0.423, ('dve', 1706): 0.394, ('pool', 1708): 0.726, ('pe', 1719): 0.276, ('dve', 1722): 0.692, ('act', 1784): 0.575, ('act', 1785): 0.563, ('act', 1786): 0.203, ('dve', 1787): 1.285, ('pe', 1795): 0.229, ('pool', 1789): 3.579, ('act', 1798): 0.687, ('dve', 1800): 0.692, ('pe', 1805): 0.211, ('dve', 1807): 0.196, ('dve', 1810): 0.157, ('dve', 1811): 0.154, ('act', 1813): 0.316, ('dve', 1815): 0.164, ('dve', 1817): 0.229, ('dve', 1819): 0.193, ('dve', 1824): 0.195, ('dve', 1826): 0.16, ('act', 1827): 0.419, ('dve', 1828): 0.154, ('dve', 1830): 0.164, ('dve', 1831): 0.159, ('dve', 1833): 0.16, ('dve', 1835): 0.241, ('dve', 1837): 0.241, ('dve', 1839): 0.192, ('pe', 1843): 0.184, ('pe', 1844): 0.029, ('dve', 1846): 0.192, ('dve', 1847): 0.193, ('dve', 1848): 0.052, ('dve', 1850): 0.1, ('dve', 1852): 0.169, ('pool', 1858): 1.133, ('pool', 1907): 1.059, ('pool', 1909): 1.055, ('pool', 1911): 0.901, ('pe', 1924): 0.105, ('act', 1927): 1.114, ('dve', 1930): 0.692, ('pe', 1948): 0.165, ('act', 1956): 0.471, ('pe', 1952): 0.164, ('dve', 1957): 0.539, ('pe', 1968): 0.28, ('act', 1972): 0.683, ('dve', 1974): 0.692, ('pool', 2007): 1.112, ('pool', 2010): 1.224, ('dve', 2015): 1.284, ('dve', 2018): 1.284, ('pool', 289): 3.244, ('act', 2021): 0.574, ('act', 2023): 0.419, ('act', 2025): 0.203, ('dve', 2027): 1.284, ('act', 289): 0.115, ('dve', 289): 0.654}
Prog.LCOST = LCOST_TABLE
```

```python
import numpy as np
import concourse.bass as bass
import concourse.mybir as mybir
from concourse.bass_utils import run_bass_kernel_spmd

F32 = mybir.dt.float32
BF16 = mybir.dt.bfloat16
I32 = mybir.dt.int32
U32 = mybir.dt.uint32
AF = mybir.ActivationFunctionType
ALU = mybir.AluOpType
AX = mybir.AxisListType


class Buf:
    __slots__ = ("name", "lw", "rd", "psum")

    def __init__(self, name="", psum=False):
        self.name = name
        self.lw = None
        self.rd = {}
        self.psum = psum


class Prog:
    ENGS = ("pe", "act", "dve", "pool", "sp")

    def __init__(self, kdma=6):
        self.ops = {e: [] for e in self.ENGS}
        self.waited = {e: {} for e in self.ENGS}
        self.ndma = {e: 0 for e in self.ENGS}
        self.K = kdma
        self.out_toks = []
        self.pending = {e: [] for e in self.ENGS}

    def barrier(self):
        toks = []
        for e in self.ENGS:
            for i in range(len(self.ops[e]) - 1, -1, -1):
                op = self.ops[e][i]
                if (not op["dma"]) and op["fn"] is not None:
                    toks.append((e, i))
                    break
            n = self.ndma[e]
            for slot in range(min(self.K, n)):
                last = ((n - 1 - slot) // self.K) * self.K + slot
                toks.append((("dma", e, slot), 16 * (last // self.K + 1)))
        for e in self.ENGS:
            self.pending[e] = list(toks)

    cap = None
    COST = {"pe": 0.13, "act": 0.45, "dve": 0.42, "pool": 1.2, "sp": 0.05}
    DMA_LAT = 2.5
    LCOST = {}

    def _emit(self, eng, fn, reads, writes, dma=False, extra=()):
        if self.cap is not None:
            self.cap.append((eng, fn, tuple(reads), tuple(writes), dma, tuple(extra)))
            return None
        return self._emit_real(eng, fn, reads, writes, dma, extra)

    _act_tab = ""

    @staticmethod
    def _act_class(fn):
        nm = fn.__code__.co_names
        if "Silu" in nm or "Sigmoid" in nm:
            return "sig"
        if "Exp" in nm or "Ln" in nm:
            return "exp"
        return ""

    def _capture(self, g):
        self.cap = []
        for _ in g:
            pass
        ops = self.cap
        self.cap = None
        return ops

    def merge_streams(self, gens):
        segs = [[self._capture(g)] for g in gens]
        self.merge_pipeline(segs, [lambda s_, done: True] * len(segs))

    def merge_pipeline(self, segs, rules):
        n = len(segs)
        sp = [0] * n
        op = [0] * n
        done = [0] * n
        free = getattr(self, "_sim_free", None)
        if free is None:
            free = self._sim_free = {e: 0.0 for e in self.ENGS}
            self._sim_buf = {}
        bt = self._sim_buf
        rr = 0
        while True:
            best, bi = None, -1
            for k in range(n):
                i = (rr + k) % n
                while sp[i] < len(segs[i]) and op[i] >= len(segs[i][sp[i]]):
                    sp[i] += 1
                    op[i] = 0
                    done[i] = sp[i]
                if sp[i] >= len(segs[i]):
                    continue
                if op[i] == 0 and not rules[i](sp[i], done):
                    continue
                eng, fn, reads, writes, dma, extra = segs[i][sp[i]][op[i]]
                t = free[eng]
                for b in reads:
                    v = bt.get(id(b))
                    if v is not None and v[0] > t:
                        t = v[0]
                for b in writes:
                    v = bt.get(id(b))
                    if v is not None:
                        if v[0] > t:
                            t = v[0]
                        if v[1] > t:
                            t = v[1]
                if eng == "act" and fn is not None:
                    tc = self._act_class(fn)
                    if tc and tc != self._act_tab:
                        t += 0.4
                if best is None or t < best - 1e-9:
                    best, bi = t, i
            if bi < 0:
                if all(sp[i] >= len(segs[i]) for i in range(n)):
                    break
                progressed = False
                for i in range(n):
                    if sp[i] < len(segs[i]) and op[i] == 0 and rules[i](sp[i], done):
                        progressed = True
                assert progressed, ("pipeline gating deadlock", sp, done)
                continue
            rr = (bi + 1) % n
            eng, fn, reads, writes, dma, extra = segs[bi][sp[bi]][op[bi]]
            op[bi] += 1
            ln = fn.__code__.co_firstlineno if fn is not None else -1
            c = self.LCOST.get((eng, ln), self.COST[eng])
            if eng == "act" and fn is not None and not dma:
                tc = self._act_class(fn)
                if tc:
                    self._act_tab = tc
            if dma:
                occ = 0.6 if eng == "pool" else 0.05
                fin = best + occ + self.DMA_LAT
                free[eng] = best + occ
            else:
                fin = best + c
                free[eng] = fin
            for b in writes:
                bt[id(b)] = [fin, fin]
            for b in reads:
                v = bt.get(id(b))
                if v is None:
                    bt[id(b)] = [0.0, fin]
                elif v[1] < fin:
                    v[1] = fin
                if b.psum and bt[id(b)][0] < fin:
                    bt[id(b)][0] = fin
            tok = self._emit_real(eng, fn, reads, writes, dma, extra)
            if dma and getattr(fn, "_is_out", False):
                self.out_toks.append(tok)

    def _emit_real(self, eng, fn, reads, writes, dma=False, extra=()):
        deps = {}

        def need(tok):
            if tok is None:
                return
            k, v = tok
            if eng == "pe" and k == "pe":
                return
            if deps.get(k, -1) < v:
                deps[k] = v
        def need_x(tok):
            if tok is not None and tok[0] != eng:
                need(tok)
        for b in reads:
            if b.psum:
                need_x(b.lw)
            else:
                need(b.lw)
        for b in writes:
            if b.psum:
                need_x(b.lw)
            else:
                need(b.lw)
                for k, v in b.rd.items():
                    need((k, v))
        for t in extra:
            need(t)
        if self.pending[eng]:
            for t in self.pending[eng]:
                need(t)
            self.pending[eng] = []
        if dma:
            i = self.ndma[eng]
            self.ndma[eng] += 1
            slot = i % self.K
            val = 16 * (i // self.K + 1)
            key = ("dma", eng, slot)
            if val > 16:
                need((key, val - 16))
            tok = (key, val)
        else:
            tok = (eng, len(self.ops[eng]))
        waits = []
        w = self.waited[eng]
        for k, v in deps.items():
            if w.get(k, -1) < v:
                w[k] = v
                waits.append((k, v))
        self.ops[eng].append(dict(waits=waits, fn=fn, tok=tok, dma=dma))
        for b in reads:
            if b.psum:
                b.lw = tok
                continue
            k, v = tok
            if b.rd.get(k, -1) < v:
                b.rd[k] = v
        for b in writes:
            b.lw = tok
            b.rd = {}
        return tok

    pe_dummy = None
    _pe_cnt = 0

    def pe(self, fn, reads=(), writes=()):
        t = self._emit("pe", fn, reads, writes)
        if self.pe_dummy is not None:
            dfn, every, buf = self.pe_dummy
            self._pe_cnt += 1
            if self._pe_cnt % every == 0:
                self._emit("pe", dfn, (), (buf,))
        return t

    def act(self, fn, reads=(), writes=()):
        return self._emit("act", fn, reads, writes)

    def dve(self, fn, reads=(), writes=()):
        return self._emit("dve", fn, reads, writes)

    def pool(self, fn, reads=(), writes=()):
        return self._emit("pool", fn, reads, writes)

    def dma(self, fn, reads=(), writes=(), q="sp", out=False):
        if out and self.cap is not None:
            try:
                fn._is_out = True
            except AttributeError:
                pass
        t = self._emit(q, fn, reads, writes, dma=True)
        if out and t is not None:
            self.out_toks.append(t)
        return t

    def finalize(self, nc):
        self._emit("sp", None, (), (), extra=self.out_toks)
        targets = {e: set() for e in self.ENGS}
        for e in self.ENGS:
            for op in self.ops[e]:
                for k, v in op["waits"]:
                    if isinstance(k, str):
                        targets[k].add(v)
        rank = {e: {} for e in self.ENGS}
        for e in self.ENGS:
            r = 0
            for i, op in enumerate(self.ops[e]):
                if (not op["dma"]) and i in targets[e]:
                    assert op["fn"] is not None
                    r += 1
                    rank[e][i] = r
        import contextlib
        with contextlib.ExitStack() as st:
            csem = {e: st.enter_context(nc.semaphore("c_" + e)) for e in self.ENGS}
            dsem = {}
            for e in self.ENGS:
                if self.ndma[e] > 0:
                    for s in range(min(self.K, self.ndma[e])):
                        dsem[("dma", e, s)] = st.enter_context(nc.semaphore("d_%s_%d" % (e, s)))
            block = st.enter_context(nc.Block())

            def run(e):
                def body(engine):
                    for i, op in enumerate(self.ops[e]):
                        for k, v in op["waits"]:
                            if isinstance(k, str):
                                engine.wait_ge(csem[k], rank[k][v])
                            else:
                                engine.wait_ge(dsem[k], v)
                        if op["fn"] is None:
                            continue
                        ins = op["fn"](engine)
                        if op["dma"]:
                            ins.then_inc(dsem[op["tok"][0]], 16)
                        elif i in rank[e]:
                            ins.then_inc(csem[e], 1)
                return body
            block.tensor(run("pe"))
            block.scalar(run("act"))
            block.vector(run("dve"))
            block.gpsimd(run("pool"))
            block.sync(run("sp"))


D = 1024
KC = 8
SBT = 256
NT = SBT // 128
NCH = SBT // 64
EPS = 1e-6
C_HQ, C_HF, C_HI, C_HG = 0, 512, 1024, 1536
C_GQ, C_GK, C_GV, C_GZ = 2048, 2560, 3072, 3584
C_GB, C_GA, C_PA, C_PB = 4096, 4100, 4104, 5128
DPROJ = 6152


class Arena:
    def __init__(self, ap, words):
        self.ap = ap
        self.words = words
        self.off = 0
        self.peak = 0

    def mark(self):
        return self.off

    def reset(self, m):
        self.off = m

    def alloc(self, free_shape, dt):
        n = 1
        for s in free_shape:
            n *= s
        esz = 4 if dt in (F32, I32, U32) else 2
        words = (n * esz + 3) // 4
        words = (words + 7) // 8 * 8
        assert self.off + words <= self.words, ("arena overflow", self.off, words, self.words)
        v = self.ap[:, self.off:self.off + words]
        self.off += words
        self.peak = max(self.peak, self.off)
        if esz == 2:
            v = v.bitcast(dt)
        elif dt != F32:
            v = v.bitcast(dt)
        v = v[:, 0:n]
        if len(free_shape) > 1:
            names = ["a%d" % i for i in range(len(free_shape))]
            pat = "p (%s) -> p %s" % (" ".join(names), " ".join(names))
            v = v.rearrange(pat, **{nm: s for nm, s in zip(names, free_shape)})
        return v


import os as _os_ls
LS = int(_os_ls.environ.get("LISTSCHED", "2"))


class KB:
    def __init__(self, npre, nfull, debug=()):
        import contextlib
        self.npre, self.nfull = npre, nfull
        self.nsb = npre + nfull
        self.ntok = self.nsb * SBT
        self.nfull_tok = nfull * SBT
        self.debug = set(debug)
        self.nc = bass.Bass("TRN2", target_bir_lowering=False)
        self.P = Prog()
        self.d = {}
        self.st = contextlib.ExitStack()

    def din(self, name, shape, dt=F32):
        self.d[name] = self.nc.dram_tensor(name, list(shape), dt, kind="ExternalInput").ap()
        return self.d[name]

    def dout(self, name, shape, dt=F32):
        self.d[name] = self.nc.dram_tensor(name, list(shape), dt, kind="ExternalOutput").ap()
        return self.d[name]

    def setup(self):
        nc, P, st = self.nc, self.P, self.st
        self.din("xs", [self.ntok, D])
        self.din("w_in", [D, DPROJ])
        self.din("norm_mix_g", [D])
        self.din("hg_lb", [128, 2, 4])
        self.din("hg_norm_g", [128])
        AW = 51200
        arena_t = st.enter_context(nc.sbuf_tensor("arena", [128, AW], F32))
        self.A = Arena(arena_t[:], AW)
        self.banks = [st.enter_context(nc.psum_tensor("pb%d" % i, [128, 512], F32)) for i in range(8)]
        self.bbank = [Buf("pb%d" % i, psum=True) for i in range(8)]
        A = self.A
        self.identf = A.alloc((128,), F32)
        self.ident = A.alloc((128,), BF16)
        self.ones_bf = A.alloc((128,), BF16)
        self.ones_f = A.alloc((512,), F32)
        self.mask2 = A.alloc((128,), F32)
        self.bconst = Buf("const")
        bc = self.bconst
        P.pool(lambda e: e.memset(self.identf, 0.0), writes=[bc])
        P.pool(lambda e: e.affine_select(out=self.identf, in_=self.identf, pattern=[[-1, 128]],
                                         compare_op=ALU.not_equal, fill=1.0, base=0, channel_multiplier=1),
               reads=[bc], writes=[bc])
        P.pool(lambda e: e.tensor_copy(out=self.ident, in_=self.identf), reads=[bc], writes=[bc])
        P.pool(lambda e: e.memset(self.ones_bf, 1.0), writes=[bc])
        P.pool(lambda e: e.memset(self.ones_f, 1.0), writes=[bc])
        P.pool(lambda e: e.memset(self.mask2, 1.0), writes=[bc])
        P.pool(lambda e: e.affine_select(out=self.mask2, in_=self.mask2, pattern=[[1, 128]],
                                         compare_op=ALU.is_ge, fill=0.0, base=0, channel_multiplier=-1),
               reads=[bc], writes=[bc])
        P.pool(lambda e: e.memset(self.mask2[0:64, 64:128], 0.0), reads=[bc], writes=[bc])
        self.xt = [A.alloc((D,), F32) for _ in range(2)]
        self.bxt = [Buf("xt%d" % i) for i in range(2)]
        self.junk = A.alloc((D,), BF16)
        self.bjunk = Buf("junk")
        self.ss = A.alloc((NT,), F32)
        self.rstd = A.alloc((NT,), F32)
        self.bss = Buf("ss")
        self.brstd = Buf("rstd")
        self.xsb = [A.alloc((D,), BF16) for _ in range(2)]
        self.bxsb = [Buf("xsb%d" % i) for i in range(2)]
        self.gbc = A.alloc((D,), F32)
        self.bgbc = Buf("gbc")
        self.nxt = 0
        self.d["x_buf"] = nc.dram_tensor("x_buf", [NSLOT, D], BF16, kind="Internal").ap()
        zt = A.alloc((D,), BF16)
        self.bxzero = Buf("xzero")
        P.pool(lambda e: e.memset(zt, 0.0), writes=[bc])
        xbv = self.d["x_buf"].rearrange("(b p) n -> p b n", p=128)
        nblk = NSLOT // 128
        step = 12
        self.bxz = []
        for b0 in range(0, nblk, step):
            bz = Buf("xz")
            self.bxz.append(bz)
            P.dma(lambda e, b0=b0: e.dma_start(out=xbv[:, b0:b0 + step, :],
                                               in_=zt.unsqueeze(1).to_broadcast([128, step, D])),
                  reads=[bc], writes=[bz])

    def stage_a(self, *a, **k):
        for _ in self.stage_a_gen(*a, **k):
            pass

    def stage_a_gen(self, sb, xnT, bxnT, ptr, bptr, gname="norm_mix_g", src="xs", tok_base=0, keep=None):
        nc, P = self.nc, self.P
        xs_d = self.d[src]
        tiles = []
        for t in range(NT):
            i = self.nxt % 2
            self.nxt += 1
            tok0 = tok_base + sb * SBT + t * 128
            xt, bxt = self.xt[i], self.bxt[i]
            P.dma(lambda e, xt=xt, tok0=tok0: e.dma_start(out=xt, in_=xs_d[tok0:tok0 + 128, :]), writes=[bxt])
            P.act(lambda e, xt=xt, t=t: e.activation(out=self.junk, in_=xt, func=AF.Square,
                                                     accum_out=self.ss[:, t:t + 1]),
                  reads=[bxt], writes=[self.bss])
            tiles.append((xt, bxt, i))
            if t % 2 == 1:
                t0 = t - 1
                P.act(lambda e, t0=t0: e.activation(out=self.rstd[:, t0:t0 + 2], in_=self.ss[:, t0:t0 + 2],
                                                    func=AF.Ln, scale=1.0 / D, bias=EPS),
                      reads=[self.bss], writes=[self.brstd])
                P.act(lambda e, t0=t0: e.activation(out=self.rstd[:, t0:t0 + 2], in_=self.rstd[:, t0:t0 + 2],
                                                    func=AF.Exp, scale=-0.5),
                      reads=[self.brstd], writes=[self.brstd])
                for tt in (t0, t):
                    xt2, bxt2, i2 = tiles[tt]
                    xsb, bxsb = self.xsb[i2], self.bxsb[i2]
                    P.dve(lambda e, xt2=xt2, xsb=xsb, tt=tt: e.scalar_tensor_tensor(
                        out=xsb, in0=xt2, scalar=self.rstd[:, tt:tt + 1], in1=self.gbc,
                        op0=ALU.mult, op1=ALU.mult),
                        reads=[bxt2, self.brstd, self.bgbc], writes=[bxsb])
                    for k in range(KC):
                        P.pe(lambda e, k=k, xsb=xsb: e.transpose(out=ptr[:, k, :], in_=xsb[:, k * 128:(k + 1) * 128],
                                                                 identity=self.ident),
                             reads=[bxsb, self.bconst], writes=[bptr])
                    P.dve(lambda e, tt=tt: e.tensor_copy(out=xnT[:, :, tt * 128:(tt + 1) * 128], in_=ptr),
                          reads=[bptr], writes=[bxnT])
                    yield


    def xnt_fetch_gen(self, sb, last_sb, xnTs, bxnTs):
        P = self.P
        src = self.d["xnT_s"]
        if sb not in self._xfetched:
            self._xfetched.add(sb)
            P.dma(lambda e, sb=sb: e.dma_start(out=xnTs[sb % 2], in_=src[:, :, sb * SBT:(sb + 1) * SBT]),
                  reads=[self.bxns[sb]], writes=[bxnTs[sb % 2]])
        nx = sb + 1
        if nx <= last_sb and nx not in self._xfetched:
            self._xfetched.add(nx)
            P.dma(lambda e, nx=nx: e.dma_start(out=xnTs[nx % 2], in_=src[:, :, nx * SBT:(nx + 1) * SBT]),
                  reads=[self.bxns[nx]], writes=[bxnTs[nx % 2]])
        yield

    def load_gain(self, gname):
        P = self.P
        g = self.d[gname]
        P.dma(lambda e: e.dma_start(out=self.gbc, in_=g.partition_broadcast(128)), writes=[self.bgbc])

    def pass1(self):
        nc, P, A = self.nc, self.P, self.A
        d = self.d
        H = 4
        self.W2 = A.alloc((KC, 2056), BF16)
        self.bW2 = [Buf("W2_%d" % k) for k in range(KC)]
        m0 = A.mark()
        self.OA = A.alloc((H, SBT), BF16)
        self.bOA = [Buf("OA%d" % h) for h in range(H)]
        W1 = A.alloc((KC, 2048), BF16)
        bW1 = [Buf("W1_%d" % k) for k in range(KC)]
        wv = d["w_in"].rearrange("(k p) n -> p k n", p=128)
        for k in range(KC):
            P.dma(lambda e, k=k: e.dma_start(out=W1[:, k, :], in_=wv[:, k, 0:2048]), writes=[bW1[k]], q="pool")
        for k in range(KC):
            P.dma(lambda e, k=k: e.dma_start(out=self.W2[:, k, :], in_=wv[:, k, 2048:2048 + 2056]), writes=[self.bW2[k]], q="pool")
        self.load_gain("norm_mix_g")
        lraw = A.alloc((2, H), F32)
        lb = A.alloc((H,), F32)
        oml = A.alloc((H,), F32)
        hgn = A.alloc((1,), F32)
        blb = Buf("lb")
        P.dma(lambda e: e.dma_start(out=lraw, in_=d["hg_lb"]), writes=[blb])
        P.dma(lambda e: e.dma_start(out=hgn, in_=d["hg_norm_g"].rearrange("(p o) -> p o", o=1)), writes=[blb])
        P.dve(lambda e: e.tensor_tensor(out=lb, in0=lraw[:, 0, :], in1=lraw[:, 1, :], op=ALU.subtract),
              reads=[blb], writes=[blb])
        P.act(lambda e: e.activation(out=oml, in_=lb, func=AF.Sigmoid, scale=-1.0), reads=[blb], writes=[blb])
        P.act(lambda e: e.activation(out=lb, in_=lb, func=AF.Sigmoid), reads=[blb], writes=[blb])

        xnT = A.alloc((KC, SBT), BF16)
        bxnT = Buf("xnT")
        Fbs = [A.alloc((H, SBT), F32) for _ in range(2)]
        CS = A.alloc((H, SBT), F32)
        Kb = A.alloc((H, SBT), BF16)
        EB = A.alloc((H, SBT), BF16)
        ENB = A.alloc((H, SBT), BF16)
        EBEs = [A.alloc((H, NCH), F32) for _ in range(2)]
        QTs = [A.alloc((H, SBT), BF16) for _ in range(2)]
        KTs = [A.alloc((H, SBT), BF16) for _ in range(2)]
        KH = A.alloc((H, SBT), BF16)
        KHTs = [A.alloc((NT, 512), BF16) for _ in range(2)]
        Vs = [A.alloc((NT, 512), BF16) for _ in range(2)]
        Gs = [A.alloc((H, SBT), BF16) for _ in range(2)]
        O32 = A.alloc((H, SBT), F32)
        OSQ = A.alloc((H, SBT), BF16)
        LNV = A.alloc((SBT,), F32)
        ATS = A.alloc((H, 128), BF16)
        S32 = A.alloc((H, 128), F32)
        SBF = [A.alloc((H, 128), BF16) for _ in range(2)]
        bFs = [[Buf() for _ in range(H)] for _ in range(2)]
        bCS = [Buf() for _ in range(H)]
        bK = Buf()
        bEB = [Buf() for _ in range(H)]
        bENB = [Buf() for _ in range(H)]
        bEBEs = [Buf(), Buf()]
        bQTs = [[Buf() for _ in range(H)] for _ in range(2)]
        bKTs = [Buf(), Buf()]
        bKH = Buf()
        bKHTs = [[Buf() for _ in range(NT)] for _ in range(2)]
        bVs = [[Buf() for _ in range(NT)] for _ in range(2)]
        bGs = [[Buf() for _ in range(H)] for _ in range(2)]
        bO32 = Buf()
        bOSQ = [Buf() for _ in range(H)]
        bLNV = Buf()
        bATS = Buf()
        bS32 = [Buf() for _ in range(H)]
        bSBF = [[Buf() for _ in range(H)] for _ in range(2)]
        sbf_i = [0] * H

        bk = self.banks
        bb = self.bbank
        ptr = bk[0][:].bitcast(BF16).rearrange("p (k t) -> p k t", k=KC)
        pkt = bk[1][:].bitcast(BF16)[:, 0:512]
        pj = [bk[2][:], bk[3][:]]
        bpj = [bb[2], bb[3]]
        pat = bk[4][:].rearrange("p (h t) -> p h t", h=H)
        po = bk[5][:].rearrange("p (h t) -> p h t", h=H)
        pS = [bk[6 + (h % 2)][:, 0:128] for h in range(H)]
        bpS = [bb[6 + (h % 2)] for h in range(H)]
        pji = [0]

        def nextpj():
            i = pji[0] % 2
            pji[0] += 1
            return pj[i], bpj[i]

        for h in range(H):
            P.pool(lambda e, h=h: e.memset(S32[:, h, :], 0.0), writes=[bS32[h]])
            P.pool(lambda e, h=h: e.memset(SBF[0][:, h, :], 0.0), writes=[bSBF[0][h]])

        def proj_fm(col0, h):
            p, bp = nextpj()
            for k in range(KC):
                P.pe(lambda e, k=k, p=p: e.matmul(p[:, 0:SBT], lhsT=W1[:, k, col0 + h * 128:col0 + (h + 1) * 128],
                                                  rhs=xnT[:, k, :], start=(k == 0), stop=(k == KC - 1)),
                     reads=[bW1[k], bxnT], writes=[bp])
            return p, bp

        def X1a(sb):
            sl = sb % 2
            QT, KT, KHT, V, G, EBE = QTs[sl], KTs[sl], KHTs[sl], Vs[sl], Gs[sl], EBEs[sl]
            bQT, bKT, bKHT, bV, bG, bEBE = bQTs[sl], bKTs[sl], bKHTs[sl], bVs[sl], bGs[sl], bEBEs[sl]
            full = sb >= self.npre
            Fb, bF = Fbs[sb % 2], bFs[sb % 2]
            yield from self.stage_a_gen(sb, xnT, bxnT, ptr, bb[0])
            if "xnT_s" not in self.d:
                self.d["xnT_s"] = self.nc.dram_tensor("xnT_s", [128, KC, self.ntok], BF16, kind="Internal").ap()
                self.bxns = {}
            bx_ = Buf("xns")
            self.bxns[sb] = bx_
            P.dma(lambda e, sb=sb: e.dma_start(out=self.d["xnT_s"][:, :, sb * SBT:(sb + 1) * SBT], in_=xnT),
                  reads=[bxnT], writes=[bx_])
            for h in range(H):
                p, bp = proj_fm(C_HF, h)
                P.act(lambda e, h=h, p=p: e.activation(out=Fb[:, h, :], in_=p[:, 0:SBT], func=AF.Sigmoid),
                      reads=[bp], writes=[bF[h]])
                yield
            if full:
                for h in range(H):
                    p, bp = proj_fm(C_HQ, h)
                    P.act(lambda e, h=h, p=p: e.activation(out=QT[:, h, :], in_=p[:, 0:SBT], func=AF.Silu),
                          reads=[bp], writes=[bQT[h]])
                    yield
                for h in range(H):
                    p, bp = proj_fm(C_HG, h)
                    P.act(lambda e, h=h, p=p: e.activation(out=G[:, h, :], in_=p[:, 0:SBT], func=AF.Silu),
                          reads=[bp], writes=[bG[h]])
                    yield
            for t in range(NT):
                p, bp = nextpj()
                for k in range(KC):
                    P.pe(lambda e, k=k, p=p, t=t: e.matmul(p, lhsT=xnT[:, k, t * 128:(t + 1) * 128],
                                                           rhs=W1[:, k, C_HI:C_HI + 512],
                                                           start=(k == 0), stop=(k == KC - 1)),
                         reads=[bW1[k], bxnT], writes=[bp])
                P.act(lambda e, p=p, t=t: e.activation(out=V[:, t, :], in_=p, func=AF.Copy), reads=[bp], writes=[bV[t]])
                yield
        def X1b(sb):
            sl = sb % 2
            QT, KT, KHT, V, G, EBE = QTs[sl], KTs[sl], KHTs[sl], Vs[sl], Gs[sl], EBEs[sl]
            bQT, bKT, bKHT, bV, bG, bEBE = bQTs[sl], bKTs[sl], bKHTs[sl], bVs[sl], bGs[sl], bEBEs[sl]
            full = sb >= self.npre
            Fb, bF = Fbs[sb % 2], bFs[sb % 2]
            for h in range(H):
                P.dve(lambda e, h=h: e.tensor_scalar(out=Fb[:, h, :], in0=Fb[:, h, :], scalar1=oml[:, h:h + 1],
                                                     scalar2=lb[:, h:h + 1], op0=ALU.mult, op1=ALU.add),
                      reads=[bF[h], blb], writes=[bF[h]])
            P.dve(lambda e: e.tensor_scalar(out=Kb, in0=Fb, scalar1=-1.0, scalar2=1.0, op0=ALU.mult, op1=ALU.add),
                  reads=bF, writes=[bK])
            for h in range(H):
                P.act(lambda e, h=h: e.activation(out=Fb[:, h, :], in_=Fb[:, h, :], func=AF.Ln),
                      reads=[bF[h], bK], writes=[bF[h]])
            for h in range(H):
                P.dve(lambda e, h=h: e.tensor_tensor_scan(out=CS[:, h, :], data0=self.ones_f[:, 0:SBT],
                                                          data1=Fb[:, h, :], initial=0.0,
                                                          op0=ALU.mult, op1=ALU.add),
                      reads=[bF[h], self.bconst], writes=[bCS[h]])
                yield
            if not full:
                for h in range(H):
                    P.act(lambda e, h=h: e.activation(out=ENB[:, h, :], in_=CS[:, h, :], func=AF.Exp, scale=-1.0,
                                                      bias=CS[:, h, SBT - 1:SBT]),
                          reads=[bCS[h]], writes=[bENB[h]])
                    yield
                P.act(lambda e: e.activation(out=EBE[:, :, 0], in_=CS[:, :, SBT - 1], func=AF.Exp), reads=bCS, writes=[bEBE])
                P.dve(lambda e: e.tensor_tensor(out=KH, in0=Kb, in1=ENB, op=ALU.mult), reads=[bK] + bENB, writes=[bKH])
            else:
                Fb4 = Fb.rearrange("p h (c t) -> p h c t", c=NCH)
                CS4 = CS.rearrange("p h (c t) -> p h c t", c=NCH)
                P.dve(lambda e: e.tensor_tensor(out=Fb4[:, :, 1:NCH, :], in0=CS4[:, :, 1:NCH, :],
                                                in1=CS4[:, :, 0:NCH - 1, 63:64].to_broadcast([128, H, NCH - 1, 64]),
                                                op=ALU.subtract),
                      reads=bCS + bF, writes=bF)
                P.dve(lambda e: e.tensor_copy(out=Fb4[:, :, 0, :], in_=CS4[:, :, 0, :]), reads=bCS + bF, writes=bF)
                for h in range(H):
                    P.act(lambda e, h=h: e.activation(out=ENB[:, h, :], in_=Fb[:, h, :], func=AF.Exp, scale=-1.0),
                          reads=[bF[h]], writes=[bENB[h]])
                    yield
                P.act(lambda e: e.activation(out=EBE, in_=Fb4[:, :, :, 63], func=AF.Exp), reads=bF, writes=[bEBE])
                if full:
                    for h in range(H):
                        P.act(lambda e, h=h: e.activation(out=EB[:, h, :], in_=Fb[:, h, :], func=AF.Exp),
                              reads=[bF[h]], writes=[bEB[h]])
                P.dve(lambda e: e.tensor_tensor(out=KT, in0=Kb, in1=ENB, op=ALU.mult), reads=[bK] + bENB, writes=[bKT])
                KT4 = KT.rearrange("p h (c t) -> p h c t", c=NCH)
                KH4 = KH.rearrange("p h (c t) -> p h c t", c=NCH)
                P.dve(lambda e: e.tensor_tensor(out=KH4, in0=KT4,
                                                in1=EBE.unsqueeze(3).to_broadcast([128, H, NCH, 64]), op=ALU.mult),
                      reads=[bKT, bEBE], writes=[bKH])
                if full:
                    P.dve(lambda e: e.tensor_tensor(out=QT, in0=QT, in1=EB, op=ALU.mult), reads=bQT + bEB, writes=bQT)
            for t in range(NT):
                for h in range(H):
                    P.pe(lambda e, h=h, t=t: e.transpose(out=pkt[:, h * 128:(h + 1) * 128],
                                                         in_=KH[:, h, t * 128:(t + 1) * 128], identity=self.ident),
                         reads=[bKH, self.bconst], writes=[bb[1]])
                P.dve(lambda e, t=t: e.tensor_copy(out=KHT[:, t, :], in_=pkt), reads=[bb[1]], writes=[bKHT[t]])
                yield
        def X1(sb):
            yield from X1a(sb)
            yield from X1b(sb)

        def Y1(sb):
            full = sb >= self.npre
            sl = sb % 2
            QT, KT, KHT, V, G, EBE = QTs[sl], KTs[sl], KHTs[sl], Vs[sl], Gs[sl], EBEs[sl]
            bQT, bKT, bKHT, bV, bG, bEBE = bQTs[sl], bKTs[sl], bKHTs[sl], bVs[sl], bGs[sl], bEBEs[sl]
            if not full:
                for h in range(H):
                    for t in range(NT):
                        P.pe(lambda e, h=h, t=t: e.matmul(pS[h], lhsT=KHT[:, t, h * 128:(h + 1) * 128],
                                                          rhs=V[:, t, h * 128:(h + 1) * 128],
                                                          start=(t == 0), stop=(t == NT - 1)),
                             reads=[bKHT[t], bV[t]], writes=[bpS[h]])
                    P.dve(lambda e, h=h: e.scalar_tensor_tensor(
                        out=S32[:, h, :], in0=S32[:, h, :], scalar=EBE[:, h, 0:1], in1=pS[h],
                        op0=ALU.mult, op1=ALU.add),
                        reads=[bS32[h], bEBE, bpS[h]], writes=[bS32[h]])
                    cur = sbf_i[h]
                    nxt = 1 - cur
                    P.act(lambda e, h=h, nxt=nxt: e.activation(out=SBF[nxt][:, h, :], in_=S32[:, h, :], func=AF.Copy),
                          reads=[bS32[h]], writes=[bSBF[nxt][h]])
                    sbf_i[h] = nxt
                    yield
                return
            for j in range(NT):
                c0 = j * 128
                if full:
                    for h in range(H):
                        P.pe(lambda e, h=h, c0=c0: e.matmul(pat[:, h, :], lhsT=KT[:, h, c0:c0 + 128],
                                                            rhs=QT[:, h, c0:c0 + 128], start=True, stop=True),
                             reads=[bKT, bQT[h]], writes=[bb[4]])
                    P.dve(lambda e: e.tensor_tensor(out=ATS, in0=pat,
                                                    in1=self.mask2.unsqueeze(1).to_broadcast([128, H, 128]),
                                                    op=ALU.mult),
                          reads=[bb[4], self.bconst], writes=[bATS])
                    yield
                for half in range(2):
                    ch = 2 * j + half
                    r0 = half * 64
                    for h in range(H):
                        cur = sbf_i[h]
                        if full:
                            P.pe(lambda e, h=h, cur=cur, c0=c0, r0=r0: e.matmul(
                                po[:, h, r0:r0 + 64], lhsT=SBF[cur][:, h, :], rhs=QT[:, h, c0 + r0:c0 + r0 + 64],
                                start=(h == 0 and r0 == 0), stop=False, skip_group_check=True),
                                reads=[bSBF[cur][h], bQT[h]], writes=[bb[5]])
                        P.pe(lambda e, h=h, j=j, r0=r0: e.matmul(
                            pS[h], lhsT=KHT[r0:r0 + 64, j, h * 128:(h + 1) * 128],
                            rhs=V[r0:r0 + 64, j, h * 128:(h + 1) * 128], start=True, stop=True),
                            reads=[bKHT[j], bV[j]], writes=[bpS[h]])
                        P.dve(lambda e, h=h, ch=ch: e.scalar_tensor_tensor(
                            out=S32[:, h, :], in0=S32[:, h, :], scalar=EBE[:, h, ch:ch + 1], in1=pS[h],
                            op0=ALU.mult, op1=ALU.add),
                            reads=[bS32[h], bEBE, bpS[h]], writes=[bS32[h]])
                        nxt = 1 - cur
                        P.act(lambda e, h=h, nxt=nxt: e.activation(out=SBF[nxt][:, h, :], in_=S32[:, h, :], func=AF.Copy),
                              reads=[bS32[h]], writes=[bSBF[nxt][h]])
                        sbf_i[h] = nxt
                        yield
                if full:
                    for h in range(H):
                        P.pe(lambda e, h=h, j=j: e.matmul(po[:, h, :], lhsT=V[:, j, h * 128:(h + 1) * 128],
                                                          rhs=ATS[:, h, :], start=False, stop=True,
                                                          skip_group_check=True),
                             reads=[bV[j], bATS], writes=[bb[5]])
                    P.act(lambda e, c0=c0: e.activation(out=O32[:, :, c0:c0 + 128], in_=po, func=AF.Copy),
                          reads=[bb[5]], writes=[bO32])
                    yield
            if full:
                tok0 = (sb - self.npre) * SBT
                for h in range(H):
                    P.act(lambda e, h=h: e.activation(out=OSQ[:, h, :], in_=O32[:, h, :], func=AF.Square),
                          reads=[bO32], writes=[bOSQ[h]])
                for h in range(H):
                    pss, bpss = bk[4][:], bb[4]
                    P.pe(lambda e, h=h, pss=pss: e.matmul(pss[:, 0:SBT], lhsT=self.ones_bf, rhs=OSQ[:, h, :], start=True, stop=True),
                         reads=[bOSQ[h], self.bconst], writes=[bpss])
                    P.act(lambda e, pss=pss: e.activation(out=LNV, in_=pss[:, 0:SBT], func=AF.Ln, scale=1.0 / 128, bias=EPS),
                          reads=[bpss], writes=[bLNV])
                    P.act(lambda e: e.activation(out=LNV, in_=LNV, func=AF.Exp, scale=-0.5),
                          reads=[bLNV], writes=[bLNV])
                    P.dve(lambda e, h=h: e.tensor_tensor(out=O32[:, h, :], in0=O32[:, h, :], in1=LNV, op=ALU.mult),
                          reads=[bO32, bLNV], writes=[bO32])
                    P.dve(lambda e, h=h: e.scalar_tensor_tensor(
                        out=self.OA[:, h, :], in0=O32[:, h, :], scalar=hgn[:, 0:1], in1=G[:, h, :],
                        op0=ALU.mult, op1=ALU.mult),
                        reads=[bO32, blb, bG[h]], writes=[self.bOA[h]])
                    yield
                self.spill("oa_s", self.OA, self.bOA, sb)
                if "oa" in self.debug:
                    if "dbg_oa" not in self.d:
                        self.dout("dbg_oa", [128, H, self.nfull_tok], BF16)
                    P.dma(lambda e, tok0=tok0: e.dma_start(out=self.d["dbg_oa"][:, :, tok0:tok0 + SBT], in_=self.OA),
                          reads=self.bOA, out=True)
        import os
        WTS1 = [int(v) for v in os.environ.get("IL_W1", "1,1").split(",")]

        def run_il(gens):
            gens = list(gens)
            while gens:
                for g_, w_ in list(gens):
                    for _ in range(w_):
                        try:
                            next(g_)
                        except StopIteration:
                            gens.remove((g_, w_))
                            break

        n = self.nsb
        if LS >= 2:
            sa = [P._capture(X1a(i)) for i in range(n)]
            sb_ = [P._capture(X1b(i)) for i in range(n)]
            sy = [P._capture(Y1(i)) for i in range(n)]
            P.merge_pipeline([sa, sb_, sy],
                             [lambda s_, dn: dn[2] >= s_ - 1,
                              lambda s_, dn: dn[0] >= s_ + 1 and dn[2] >= s_ - 1,
                              lambda s_, dn: dn[1] >= s_ + 1])
        else:
            for r in range(-1, n):
                gs = []
                if 0 <= r < n:
                    gs.append((Y1(r), WTS1[0]))
                if 0 <= r + 1 < n:
                    gs.append((X1(r + 1), WTS1[1]))
                if LS:
                    P.merge_streams([g_ for g_, _w in gs])
                else:
                    run_il(gs)
        A.reset(m0)
        P.barrier()

    def finish(self):
        self.P.finalize(self.nc)
        self.st.close()
        return self.nc


def _pass2(self):
    nc, P, A = self.nc, self.P, self.A
    d = self.d
    H = 4
    self.din("conv_wT", [128, 4, 12])
    self.din("gd_A_log", [4])
    self.din("gd_dt_bias", [4])
    self.din("gd_norm_g", [128])
    m0 = A.mark()
    self.OB = A.alloc((H, SBT), BF16)
    self.bOB = [Buf("OB%d" % h) for h in range(H)]
    NW = 2056
    if hasattr(self, "W2"):
        W2, bW2 = self.W2, self.bW2
    else:
        W2 = A.alloc((KC, NW), BF16)
        bW2 = [Buf("W2_%d" % k) for k in range(KC)]
        wv = d["w_in"].rearrange("(k p) n -> p k n", p=128)
        for k in range(KC):
            P.dma(lambda e, k=k: e.dma_start(out=W2[:, k, :], in_=wv[:, k, 2048:2048 + NW]), writes=[bW2[k]], q="pool")
    self.load_gain("norm_mix_g")
    cw = A.alloc((4, 12), F32)
    negA = A.alloc((H,), F32)
    dtb = A.alloc((H,), F32)
    gdn = A.alloc((1,), F32)
    bpar = Buf("par2")
    P.dma(lambda e: e.dma_start(out=cw, in_=d["conv_wT"]), writes=[bpar])
    P.dma(lambda e: e.dma_start(out=negA, in_=d["gd_A_log"].partition_broadcast(128)), writes=[bpar])
    P.dma(lambda e: e.dma_start(out=dtb, in_=d["gd_dt_bias"].partition_broadcast(128)), writes=[bpar])
    P.dma(lambda e: e.dma_start(out=gdn, in_=d["gd_norm_g"].rearrange("(p o) -> p o", o=1)), writes=[bpar])
    P.act(lambda e: e.activation(out=negA, in_=negA, func=AF.Exp), reads=[bpar], writes=[bpar])
    P.dve(lambda e: e.tensor_scalar(out=negA, in0=negA, scalar1=-1.0, scalar2=None, op0=ALU.mult),
          reads=[bpar], writes=[bpar])
    maskL = A.alloc((128,), F32)
    ch01 = A.alloc((2, 128), F32)
    bc = self.bconst
    P.pool(lambda e: e.memset(maskL, 1.0), writes=[bc])
    P.pool(lambda e: e.affine_select(out=maskL, in_=maskL, pattern=[[-1, 128]], compare_op=ALU.is_gt,
                                     fill=0.0, base=0, channel_multiplier=1), reads=[bc], writes=[bc])
    P.pool(lambda e: e.memset(maskL[64:128, 0:64], 0.0), reads=[bc], writes=[bc])
    bones = A.alloc((128,), F32)
    P.pool(lambda e: e.memset(bones, 0.0), writes=[bc])
    P.pool(lambda e: e.memset(bones[0:64, 0:64], 1.0), reads=[bc], writes=[bc])
    P.pool(lambda e: e.memset(bones[64:128, 64:128], 1.0), reads=[bc], writes=[bc])
    P.pool(lambda e: e.memset(ch01, 0.0), writes=[bc])
    P.pool(lambda e: e.memset(ch01[0:64, 0, :], 1.0), reads=[bc], writes=[bc])
    P.pool(lambda e: e.memset(ch01[64:128, 1, :], 1.0), reads=[bc], writes=[bc])

    xnTs = [A.alloc((KC, SBT), BF16) for _ in range(2)]
    bxnTs = [Buf("xnT0"), Buf("xnT1")]
    self._xfetched = set()
    use_fetch = hasattr(self, "bxns")
    XC = A.alloc((12, SBT + 3), BF16)
    DG = A.alloc((48, 128), BF16)
    bDG = Buf("DG")
    for _j in range(4):
        for _cb in range(12):
            P.dve(lambda e, _j=_j, _cb=_cb: e.tensor_scalar(out=DG[:, _j * 12 + _cb, :], in0=self.identf,
                                                           scalar1=cw[:, _j, _cb:_cb + 1], scalar2=None, op0=ALU.mult),
                  reads=[bpar, self.bconst], writes=[bDG])
    bXC = [Buf() for _ in range(12)]
    CV = [A.alloc((SBT,), F32) for _ in range(2)]
    bCV = [Buf(), Buf()]
    QK32 = A.alloc((8, SBT), F32)
    bQK32 = [Buf() for _ in range(8)]
    NQ = 4
    SQs = [A.alloc((SBT,), BF16) for _ in range(NQ)]
    bSQs = [Buf() for _ in range(NQ)]
    RSs = [A.alloc((SBT,), F32) for _ in range(NQ)]
    bRSs = [Buf() for _ in range(NQ)]
    sqi = [0]
    NS = 3
    QTs = [A.alloc((H, SBT), BF16) for _ in range(NS)]
    KTs = [A.alloc((H, SBT), BF16) for _ in range(NS)]
    VTs = [A.alloc((H, SBT), BF16) for _ in range(NS)]
    GZs = [A.alloc((H, SBT), BF16) for _ in range(NS)]
    bQTs = [[Buf() for _ in range(H)] for _ in range(NS)]
    bKTs = [[Buf() for _ in range(H)] for _ in range(NS)]
    bVTs = [[Buf() for _ in range(H)] for _ in range(NS)]
    bGZs = [[Buf() for _ in range(H)] for _ in range(NS)]
    BGraws = [A.alloc((NT, 8), F32) for _ in range(NS)]
    LNBs = [A.alloc((NT, H), F32) for _ in range(NS)]
    BETAs = [A.alloc((NT, H), F32) for _ in range(NS)]
    GGs = [A.alloc((NT, H), F32) for _ in range(NS)]
    bBGs = [Buf() for _ in range(NS)]
    PBUF = []
    for _j in range(NT):
        pb = dict(
            GB=A.alloc((H, 128), F32), bGB=Buf(),
            E1=A.alloc((H, 128), F32), bE1=Buf(),
            E2=A.alloc((H, 128), F32), bE2=Buf(),
            EGR=A.alloc((H, 128), BF16), bEGR=Buf(),
            Lm=[A.alloc((H, 128), BF16) for _ in range(2)], bLm=[Buf(), Buf()],
            Um=[A.alloc((H, 128), BF16) for _ in range(2)], bUm=[Buf(), Buf()],
            Xm=[A.alloc((H, 128), BF16) for _ in range(2)], bXm=[Buf(), Buf()],
            VTK=A.alloc((H, 128), BF16), bVTK=Buf(),
        )
        PBUF.append(pb)
    PCAR = []
    for _s in range(2):
        row = []
        for _j in range(NT):
            row.append(dict(
                SC=A.alloc((8, H), F32), bSC=Buf(),
                QTG=A.alloc((H, 128), BF16), bQTG=Buf(),
                TT=A.alloc((H, 128), BF16), bTT=Buf(),
                ATT=A.alloc((H, 128), BF16), bATT=Buf(),
                KHT=A.alloc((H, 128), BF16), bKHT=Buf(),
                KTP=A.alloc((H, 128), BF16), bKTP=Buf(),
                BV=A.alloc((H, 128), F32), bBV=Buf(),
            ))
        PCAR.append(row)
    R = A.alloc((H, 128), BF16)
    USB = A.alloc((H, 128), BF16)
    bR = Buf()
    bUSB = Buf()
    S32 = A.alloc((H, 128), F32)
    SBF = [A.alloc((H, 128), BF16) for _ in range(2)]
    bS32 = [Buf() for _ in range(H)]
    bSBF = [[Buf() for _ in range(H)] for _ in range(2)]
    sbf_i = [0]
    O32 = A.alloc((H, SBT), F32)
    bO32 = Buf()
    OSQ = A.alloc((H, SBT), BF16)
    bOSQ = [Buf() for _ in range(H)]
    LNV = A.alloc((SBT,), F32)
    bLNV = Buf()
    identb4 = A.alloc((H, 128), BF16)
    P.pool(lambda e: e.tensor_copy(out=identb4, in_=self.ident.unsqueeze(1).to_broadcast([128, H, 128])),
           reads=[bc], writes=[bc])

    bk, bb = self.banks, self.bbank
    ptr = bk[0][:].bitcast(BF16).rearrange("p (k t) -> p k t", k=KC)
    ptr4 = bk[0][:].bitcast(BF16)[:, 0:512].rearrange("p (h t) -> p h t", h=H)
    import os as _os
    DUM = int(_os.environ.get("PE_DUMMY", "0"))
    DUMN = int(_os.environ.get("PE_DUMMY_N", "128"))
    if DUM > 0:
        pj = [bk[1][:], bk[2][:]]
        bpj = [bb[1], bb[2]]
        dones = A.alloc((512,), BF16)
        P.pool(lambda e: e.memset(dones, 1.0), writes=[self.bconst])
        dbuf = Buf("dummy", psum=True)
        P.pe_dummy = (lambda e: e.matmul(bk[0][:, 0:DUMN], lhsT=self.ones_bf, rhs=dones[:, 0:DUMN], start=True, stop=True),
                      DUM, dbuf)
    else:
        pj = [bk[1][:], bk[2][:], bk[0][:]]
        bpj = [bb[1], bb[2], bb[0]]
    pA = bk[3][:].rearrange("p (h t) -> p h t", h=H)
    pB = bk[4][:].rearrange("p (h t) -> p h t", h=H)
    pBb = bk[4][:].bitcast(BF16)[:, 0:512].rearrange("p (h t) -> p h t", h=H)
    pC = bk[5][:].rearrange("p (h t) -> p h t", h=H)
    pR = bk[6][:].rearrange("p (h t) -> p h t", h=H)
    po = bk[7][:].rearrange("p (h t) -> p h t", h=H)
    pji = [0]

    def nextpj():
        i = pji[0] % len(pj)
        pji[0] += 1
        return pj[i], bpj[i]

    for h in range(H):
        P.pool(lambda e, h=h: e.memset(S32[:, h, :], 0.0), writes=[bS32[h]])
        P.pool(lambda e, h=h: e.memset(SBF[0][:, h, :], 0.0), writes=[bSBF[0][h]])
    for cb in range(12):
        P.pool(lambda e, cb=cb: e.memset(XC[:, cb, :], 0.0), writes=[bXC[cb]])

    def proj_fm(col0, xnT, bxnT):
        p, bp = nextpj()
        for k in range(KC):
            P.pe(lambda e, k=k, p=p: e.matmul(p[:, 0:SBT], lhsT=W2[:, k, col0:col0 + 128],
                                              rhs=xnT[:, k, :], start=(k == 0), stop=(k == KC - 1)),
                 reads=[bW2[k], bxnT], writes=[bp])
        return p, bp

    evi = [0]

    def evac(out, in_, reads, writes):
        i = evi[0]
        evi[0] += 1
        if i % 3 != 2:
            P.act(lambda e: e.activation(out=out, in_=in_, func=AF.Copy), reads=reads, writes=writes)
        else:
            P.dve(lambda e: e.tensor_copy(out=out, in_=in_), reads=reads, writes=writes)

    def X(sb):
        sl = sb % 3
        QT, KT, VT, GZ = QTs[sl], KTs[sl], VTs[sl], GZs[sl]
        bQT, bKT, bVT, bGZ = bQTs[sl], bKTs[sl], bVTs[sl], bGZs[sl]
        BGraw, LNB, BETA, GG, bBG = BGraws[sl], LNBs[sl], BETAs[sl], GGs[sl], bBGs[sl]
        full = sb >= self.npre
        xnT, bxnT = xnTs[sb % 2], bxnTs[sb % 2]
        if use_fetch:
            yield from self.xnt_fetch_gen(sb, self.nsb - 1, xnTs, bxnTs)
        else:
            yield from self.stage_a_gen(sb, xnT, bxnT, ptr, bb[0])
        cbs = list(range(12)) if full else list(range(4, 12))
        cbs_proj = list(range(12)) if sb >= self.npre - 1 else list(range(4, 12))
        for cb in cbs_proj:
            if sb > 0:
                P.pool(lambda e, cb=cb: e.tensor_copy(out=XC[:, cb, 0:3], in_=XC[:, cb, SBT:SBT + 3]),
                       reads=[bXC[cb]], writes=[bXC[cb]])
            p, bp = proj_fm(cb * 128, xnT, bxnT)
            evac(XC[:, cb, 3:SBT + 3], p[:, 0:SBT], [bp], [bXC[cb]])
            yield
        pbg, bpbg = nextpj()
        for t in range(NT):
            for k in range(KC):
                P.pe(lambda e, k=k, t=t, pbg=pbg: e.matmul(pbg[:, t * 8:(t + 1) * 8], lhsT=xnT[:, k, t * 128:(t + 1) * 128],
                                                  rhs=W2[:, k, 2048:2056], start=(k == 0), stop=(k == KC - 1)),
                     reads=[bW2[k], bxnT], writes=[bpbg])
        P.act(lambda e, pbg=pbg: e.activation(out=BGraw, in_=pbg[:, 0:NT * 8].rearrange("p (t c) -> p t c", t=NT), func=AF.Copy),
              reads=[bpbg], writes=[bBG])
        P.act(lambda e: e.activation(out=LNB, in_=BGraw[:, :, 0:4], func=AF.Exp, scale=-1.0), reads=[bBG], writes=[bBG])
        P.act(lambda e: e.activation(out=LNB, in_=LNB, func=AF.Ln, bias=1.0), reads=[bBG], writes=[bBG])
        P.dve(lambda e: e.tensor_scalar(out=LNB, in0=LNB, scalar1=-1.0, scalar2=None, op0=ALU.mult),
              reads=[bBG], writes=[bBG])
        P.act(lambda e: e.activation(out=BETA, in_=LNB, func=AF.Exp), reads=[bBG], writes=[bBG])
        P.dve(lambda e: e.tensor_tensor(out=GG, in0=BGraw[:, :, 4:8], in1=dtb.unsqueeze(1).to_broadcast([128, NT, H]),
                                        op=ALU.add), reads=[bBG, bpar], writes=[bBG])
        P.act(lambda e: e.activation(out=GG, in_=GG, func=AF.Exp), reads=[bBG], writes=[bBG])
        P.act(lambda e: e.activation(out=GG, in_=GG, func=AF.Ln, bias=1.0), reads=[bBG], writes=[bBG])
        P.dve(lambda e: e.tensor_tensor(out=GG, in0=GG, in1=negA.unsqueeze(1).to_broadcast([128, NT, H]),
                                        op=ALU.mult), reads=[bBG, bpar], writes=[bBG])
        yield
        if full:
            for h in range(H):
                p, bp = proj_fm(1536 + h * 128, xnT, bxnT)
                P.act(lambda e, h=h, p=p: e.activation(out=GZ[:, h, :], in_=p[:, 0:SBT], func=AF.Silu),
                      reads=[bp], writes=[bGZ[h]])
                yield
        for n, cb in enumerate(cbs):
            cv, bcv = nextpj()
            for j in range(4):
                P.pe(lambda e, cb=cb, cv=cv, j=j: e.matmul(cv[:, 0:SBT], lhsT=DG[:, j * 12 + cb, :], rhs=XC[:, cb, j:SBT + j],
                                                           start=(j == 0), stop=(j == 3)),
                     reads=[bXC[cb], bDG], writes=[bcv])
            if cb < 8:
                P.act(lambda e, cb=cb, cv=cv: e.activation(out=QK32[:, cb, :], in_=cv[:, 0:SBT], func=AF.Silu),
                      reads=[bcv], writes=[bQK32[cb]])
            else:
                P.act(lambda e, cb=cb, cv=cv: e.activation(out=VT[:, cb - 8, :], in_=cv[:, 0:SBT], func=AF.Silu),
                      reads=[bcv], writes=[bVT[cb - 8]])
            yield
        for cb in cbs:
            if cb >= 8:
                continue
            SQ, bSQ, RS, bRS = SQs[sqi[0] % NQ], bSQs[sqi[0] % NQ], RSs[sqi[0] % NQ], bRSs[sqi[0] % NQ]
            sqi[0] += 1
            P.pool(lambda e, cb=cb, SQ=SQ: e.tensor_tensor(out=SQ, in0=QK32[:, cb, :], in1=QK32[:, cb, :], op=ALU.mult),
                   reads=[bQK32[cb]], writes=[bSQ])
            p, bp = nextpj()
            P.pe(lambda e, p=p, SQ=SQ: e.matmul(p[:, 0:SBT], lhsT=self.ones_bf, rhs=SQ, start=True, stop=True),
                 reads=[bSQ, bc], writes=[bp])
            P.act(lambda e, p=p, RS=RS: e.activation(out=RS, in_=p[:, 0:SBT], func=AF.Ln, bias=EPS), reads=[bp], writes=[bRS])
            qbias = -0.5 * float(np.log(128.0)) if cb < 4 else 0.0
            P.act(lambda e, qbias=qbias, RS=RS: e.activation(out=RS, in_=RS, func=AF.Exp, scale=-0.5, bias=qbias),
                  reads=[bRS], writes=[bRS])
            dst, bdst = (QT[:, cb, :], bQT[cb]) if cb < 4 else (KT[:, cb - 4, :], bKT[cb - 4])
            P.pool(lambda e, cb=cb, dst=dst, RS=RS: e.tensor_tensor(out=dst, in0=QK32[:, cb, :], in1=RS, op=ALU.mult),
                   reads=[bQK32[cb], bRS], writes=[bdst])
            yield
    def Yp(sb):
        sl = sb % 3
        full = sb >= self.npre
        QT, KT, VT, GZ = QTs[sl], KTs[sl], VTs[sl], GZs[sl]
        bQT, bKT, bVT, bGZ = bQTs[sl], bKTs[sl], bVTs[sl], bGZs[sl]
        BGraw, LNB, BETA, GG, bBG = BGraws[sl], LNBs[sl], BETAs[sl], GGs[sl], bBGs[sl]
        LL = dict(L0)
        LL['PB'] = [dict(PBUF[j], **PCAR[sb % 2][j]) for j in range(NT)]
        LL.update(QT=QT, KT=KT, VT=VT, GZ=GZ, bQT=bQT, bKT=bKT, bVT=bVT, bGZ=bGZ, LNB=LNB, BETA=BETA, GG=GG, bBG=bBG)
        yield from self._gdn_prep(LL, full)
    def Yr(sb):
        sl = sb % 3
        full = sb >= self.npre
        QT, KT, VT, GZ = QTs[sl], KTs[sl], VTs[sl], GZs[sl]
        bQT, bKT, bVT, bGZ = bQTs[sl], bKTs[sl], bVTs[sl], bGZs[sl]
        BGraw, LNB, BETA, GG, bBG = BGraws[sl], LNBs[sl], BETAs[sl], GGs[sl], bBGs[sl]
        LL = dict(L0)
        LL['PB'] = [dict(PBUF[j], **PCAR[sb % 2][j]) for j in range(NT)]
        LL.update(QT=QT, KT=KT, VT=VT, GZ=GZ, bQT=bQT, bKT=bKT, bVT=bVT, bGZ=bGZ, LNB=LNB, BETA=BETA, GG=GG, bBG=bBG)
        yield from self._gdn_rec(LL, full)
        if full:
            tok0 = (sb - self.npre) * SBT
            for h in range(H):
                P.act(lambda e, h=h: e.activation(out=OSQ[:, h, :], in_=O32[:, h, :], func=AF.Square),
                      reads=[bO32], writes=[bOSQ[h]])
            for h in range(H):
                pss, bpss = bk[5][:], bb[5]
                P.pe(lambda e, h=h, pss=pss: e.matmul(pss[:, 0:SBT], lhsT=self.ones_bf, rhs=OSQ[:, h, :], start=True, stop=True),
                     reads=[bOSQ[h], bc], writes=[bpss])
                P.act(lambda e, pss=pss: e.activation(out=LNV, in_=pss[:, 0:SBT], func=AF.Ln, scale=1.0 / 128, bias=EPS),
                      reads=[bpss], writes=[bLNV])
                P.act(lambda e: e.activation(out=LNV, in_=LNV, func=AF.Exp, scale=-0.5), reads=[bLNV], writes=[bLNV])
                P.dve(lambda e, h=h: e.tensor_tensor(out=O32[:, h, :], in0=O32[:, h, :], in1=LNV, op=ALU.mult),
                      reads=[bO32, bLNV], writes=[bO32])
                P.dve(lambda e, h=h: e.scalar_tensor_tensor(
                    out=self.OB[:, h, :], in0=O32[:, h, :], scalar=gdn[:, 0:1], in1=GZ[:, h, :],
                    op0=ALU.mult, op1=ALU.mult), reads=[bO32, bpar, bGZ[h]], writes=[self.bOB[h]])
                yield
            self.spill("ob_s", self.OB, self.bOB, sb)
            if "ob" in self.debug:
                if "dbg_ob" not in self.d:
                    self.dout("dbg_ob", [128, H, self.nfull_tok], BF16)
                P.dma(lambda e, tok0=tok0: e.dma_start(out=self.d["dbg_ob"][:, :, tok0:tok0 + SBT], in_=self.OB),
                      reads=self.bOB, out=True)
    L0 = dict(locals())

    import os
    WTS = [int(v) for v in os.environ.get("IL_W", "1,2,2").split(",")]

    def run_il(gens):
        gens = list(gens)
        while gens:
            for g_, w_ in list(gens):
                for _ in range(w_):
                    try:
                        next(g_)
                    except StopIteration:
                        gens.remove((g_, w_))
                        break

    n = self.nsb
    if LS >= 2:
        sx = [P._capture(X(i)) for i in range(n)]
        sp_ = [P._capture(Yp(i)) for i in range(n)]
        sr = [P._capture(Yr(i)) for i in range(n)]
        P.merge_pipeline([sx, sp_, sr],
                         [lambda s_, dn: dn[2] >= s_ - 2,
                          lambda s_, dn: dn[0] >= s_ + 1 and dn[2] >= s_ - 1,
                          lambda s_, dn: dn[1] >= s_ + 1])
    else:
        for r in range(-2, n):
            gs = []
            if 0 <= r < n:
                gs.append((Yr(r), WTS[0]))
            if 0 <= r + 1 < n:
                gs.append((Yp(r + 1), WTS[1]))
            if 0 <= r + 2 < n:
                gs.append((X(r + 2), WTS[2]))
            if LS:
                P.merge_streams([g_ for g_, _w in gs])
            else:
                run_il(gs)
    P.pe_dummy = None
    A.reset(m0)
    P.barrier()


KB.pass2 = _pass2


def _gdn_prep(self, L, full):
    P = self.P
    H = 4
    g = lambda n: L[n]
    bk, bb = self.banks, self.bbank
    bc = self.bconst
    mask2, ident = self.mask2, self.ident
    maskL, bones, ch01, identb4 = g("maskL"), g("bones"), g("ch01"), g("identb4")
    PBUF, GG, LNB, BETA, bBG = g("PB"), g("GG"), g("LNB"), g("BETA"), g("bBG")
    QT, KT, VT, bQT, bKT, bVT = g("QT"), g("KT"), g("VT"), g("bQT"), g("bKT"), g("bVT")
    R, USB, bR, bUSB = g("R"), g("USB"), g("bR"), g("bUSB")
    S32, SBF, bS32, bSBF, sbf_i = g("S32"), g("SBF"), g("bS32"), g("bSBF"), g("sbf_i")
    O32, bO32 = g("O32"), g("bO32")
    evac, nextpj = g("evac"), g("nextpj")
    NP = NT
    pP = [bk[3 + j][:].rearrange("p (h t) -> p h t", h=H) for j in range(NP)]
    pPb = [bk[3 + j][:].bitcast(BF16)[:, 0:512].rearrange("p (h t) -> p h t", h=H) for j in range(NP)]
    bpP = [bb[3 + j] for j in range(NP)]
    ptr4 = bk[5][:].bitcast(BF16)[:, 0:512].rearrange("p (h t) -> p h t", h=H)
    pKS = bk[5][:].rearrange("p (h t) -> p h t", h=H)
    pU = bk[6][:].rearrange("p (h t) -> p h t", h=H)
    po = bk[7][:].rearrange("p (h t) -> p h t", h=H)

    def bc4(ap):
        return ap.unsqueeze(2).to_broadcast([128, H, 128])

    for j in range(NP):
        pb = PBUF[j]
        SC, bSC = pb["SC"], pb["bSC"]
        ps = bk[3 + j][:, 0:16]
        gj = GG[:, j, :]
        for n, lhs in enumerate((mask2, bones, ch01[:, 0, :], ch01[:, 1, :])):
            P.pe(lambda e, n=n, lhs=lhs, ps=ps, gj=gj: e.matmul(ps[:, 4 * n:4 * n + 4], lhsT=lhs, rhs=gj,
                                                                 start=True, stop=True),
                 reads=[bBG, bc], writes=[bpP[j]])
        P.act(lambda e, SC=SC, ps=ps: e.activation(out=SC[:, 0, :], in_=ps[:, 0:4], func=AF.Copy),
              reads=[bpP[j]], writes=[bSC])
        P.dve(lambda e, SC=SC, ps=ps: e.tensor_tensor(out=SC[:, 5, :], in0=ps[:, 4:8], in1=SC[:, 0, :], op=ALU.subtract),
              reads=[bpP[j], bSC], writes=[bSC])
        P.act(lambda e, SC=SC, ps=ps: e.activation(out=SC[:, 6:8, :], in_=ps[:, 8:16].rearrange("p (a h) -> p a h", a=2),
                                                  func=AF.Exp), reads=[bpP[j]], writes=[bSC])
        P.dve(lambda e, SC=SC: e.tensor_scalar(out=SC[:, 1, :], in0=SC[:, 0, :], scalar1=-1.0, scalar2=None, op0=ALU.mult),
              reads=[bSC], writes=[bSC])
        P.dve(lambda e, SC=SC, j=j: e.tensor_tensor(out=SC[:, 2, :], in0=SC[:, 0, :], in1=LNB[:, j, :], op=ALU.add),
              reads=[bSC, bBG], writes=[bSC])
        P.act(lambda e, SC=SC: e.activation(out=SC[:, 3, :], in_=SC[:, 0, :], func=AF.Exp), reads=[bSC], writes=[bSC])
        P.act(lambda e, SC=SC: e.activation(out=SC[:, 5, :], in_=SC[:, 5, :], func=AF.Exp), reads=[bSC], writes=[bSC])
        P.dve(lambda e, SC=SC, j=j: e.scalar_tensor_tensor(out=SC[:, 4, :], in0=SC[:, 3, :], scalar=-1.0,
                                                          in1=BETA[:, j, :], op0=ALU.mult, op1=ALU.mult),
              reads=[bSC, bBG], writes=[bSC])
        yield
    for j in range(NP):
        pb = PBUF[j]
        P.pool(lambda e, pb=pb, j=j: e.tensor_copy(out=pb["GB"], in_=bc4(GG[:, j, :])), reads=[bBG], writes=[pb["bGB"]])
        yield
    for j in range(NP):
        pb = PBUF[j]
        for h in range(H):
            P.pe(lambda e, pb=pb, j=j, h=h: e.matmul(pP[j][:, h, :], lhsT=pb["GB"][:, h, :], rhs=mask2,
                                                     start=True, stop=True),
                 reads=[pb["bGB"], bc], writes=[bpP[j]])
        yield
    for j in range(NP):
        pb = PBUF[j]
        SC = pb["SC"]
        P.dve(lambda e, pb=pb, j=j, SC=SC: e.tensor_tensor(out=pb["E1"], in0=pP[j], in1=bc4(SC[:, 0, :]), op=ALU.max),
              reads=[bpP[j], pb["bSC"]], writes=[pb["bE1"]])
        if full:
            P.dve(lambda e, pb=pb, j=j, SC=SC: e.tensor_tensor(out=pb["E2"], in0=pP[j], in1=bc4(SC[:, 0, :]), op=ALU.min),
                  reads=[bpP[j], pb["bSC"]], writes=[pb["bE2"]])
            P.act(lambda e, pb=pb, j=j: e.activation(out=pb["EGR"], in_=pP[j], func=AF.Exp),
                  reads=[bpP[j]], writes=[pb["bEGR"]])
        yield
    for j in range(NP):
        pb = PBUF[j]
        SC = pb["SC"]
        for h in range(H):
            P.act(lambda e, pb=pb, h=h, SC=SC: e.activation(out=pb["E1"][:, h, :], in_=pb["E1"][:, h, :], func=AF.Exp,
                                                            scale=-1.0, bias=SC[:, 2, h:h + 1]),
                  reads=[pb["bE1"], pb["bSC"]], writes=[pb["bE1"]])
        if full:
            for h in range(H):
                P.act(lambda e, pb=pb, h=h, SC=SC: e.activation(out=pb["E2"][:, h, :], in_=pb["E2"][:, h, :], func=AF.Exp,
                                                                bias=SC[:, 1, h:h + 1]),
                      reads=[pb["bE2"], pb["bSC"]], writes=[pb["bE2"]])
        yield
    for j in range(NP):
        pb = PBUF[j]
        P.pool(lambda e, pb=pb: e.tensor_tensor(out=pb["E1"], in0=pb["E1"],
                                                in1=maskL.unsqueeze(1).to_broadcast([128, H, 128]), op=ALU.mult),
               reads=[pb["bE1"], bc], writes=[pb["bE1"]])
        if full:
            P.pool(lambda e, pb=pb: e.tensor_tensor(out=pb["E2"], in0=pb["E2"],
                                                    in1=mask2.unsqueeze(1).to_broadcast([128, H, 128]), op=ALU.mult),
                   reads=[pb["bE2"], bc], writes=[pb["bE2"]])
            c0 = j * 128
            P.dve(lambda e, pb=pb, c0=c0: e.tensor_tensor(out=pb["QTG"], in0=QT[:, :, c0:c0 + 128], in1=pb["EGR"], op=ALU.mult),
                  reads=bQT + [pb["bEGR"]], writes=[pb["bQTG"]])
        yield
    for j in range(NP):
        c0 = j * 128
        for h in range(H):
            P.pe(lambda e, j=j, h=h, c0=c0: e.matmul(pP[j][:, h, :], lhsT=KT[:, h, c0:c0 + 128], rhs=KT[:, h, c0:c0 + 128],
                                                     start=True, stop=True), reads=[bKT[h]], writes=[bpP[j]])
        yield
    for j in range(NP):
        pb = PBUF[j]
        P.dve(lambda e, pb=pb, j=j: e.tensor_tensor(out=pb["Lm"][0], in0=pP[j], in1=pb["E1"], op=ALU.mult),
              reads=[bpP[j], pb["bE1"]], writes=[pb["bLm"][0]])
        yield
    if full:
        for j in range(NP):
            c0 = j * 128
            for h in range(H):
                P.pe(lambda e, j=j, h=h, c0=c0: e.matmul(pP[j][:, h, :], lhsT=KT[:, h, c0:c0 + 128],
                                                         rhs=QT[:, h, c0:c0 + 128], start=True, stop=True),
                     reads=[bKT[h], bQT[h]], writes=[bpP[j]])
            yield
        for j in range(NP):
            pb = PBUF[j]
            P.dve(lambda e, pb=pb, j=j: e.tensor_tensor(out=pb["ATT"], in0=pP[j], in1=pb["E2"], op=ALU.mult),
                  reads=[bpP[j], pb["bE2"]], writes=[pb["bATT"]])
            yield
    for j in range(NP):
        pb = PBUF[j]
        for h in range(H):
            P.pe(lambda e, pb=pb, j=j, h=h: e.transpose(out=pPb[j][:, h, :], in_=pb["Lm"][0][:, h, :], identity=ident),
                 reads=[pb["bLm"][0], bc], writes=[bpP[j]])
        yield
    for j in range(NP):
        pb = PBUF[j]
        evac(pb["Um"][0], pPb[j], [bpP[j]], [pb["bUm"][0]])
        yield
    for j in range(NP):
        pb = PBUF[j]
        P.pool(lambda e, pb=pb: e.tensor_tensor(out=pb["Xm"][0], in0=identb4, in1=pb["Um"][0], op=ALU.subtract),
               reads=[pb["bUm"][0], bc], writes=[pb["bXm"][0]])
        yield
    cur, cx = 0, 0
    for lvl in range(5):
        for j in range(NP):
            pb = PBUF[j]
            for h in range(H):
                P.pe(lambda e, pb=pb, j=j, h=h, cur=cur: e.matmul(pP[j][:, h, :], lhsT=pb["Um"][cur][:, h, :],
                                                                  rhs=pb["Lm"][cur][:, h, :], start=True, stop=True),
                     reads=[pb["bUm"][cur], pb["bLm"][cur]], writes=[bpP[j]])
            yield
        for j in range(NP):
            pb = PBUF[j]
            evac(pb["Lm"][1 - cur], pP[j], [bpP[j]], [pb["bLm"][1 - cur]])
            yield
        if lvl < 4:
            for j in range(NP):
                pb = PBUF[j]
                for h in range(H):
                    P.pe(lambda e, pb=pb, j=j, h=h, cur=cur: e.matmul(pP[j][:, h, :], lhsT=pb["Lm"][cur][:, h, :],
                                                                      rhs=pb["Um"][cur][:, h, :], start=True, stop=True),
                         reads=[pb["bUm"][cur], pb["bLm"][cur]], writes=[bpP[j]])
                yield
            for j in range(NP):
                pb = PBUF[j]
                evac(pb["Um"][1 - cur], pP[j], [bpP[j]], [pb["bUm"][1 - cur]])
                yield
        for j in range(NP):
            pb = PBUF[j]
            for h in range(H):
                P.pe(lambda e, pb=pb, j=j, h=h, cur=cur, cx=cx: e.matmul(pP[j][:, h, :], lhsT=pb["Lm"][1 - cur][:, h, :],
                                                                         rhs=pb["Xm"][cx][:, h, :], start=True, stop=True),
                     reads=[pb["bLm"][1 - cur], pb["bXm"][cx]], writes=[bpP[j]])
            yield
        for j in range(NP):
            pb = PBUF[j]
            xo, bxo = (pb["TT"], pb["bTT"]) if lvl == 4 else (pb["Xm"][1 - cx], pb["bXm"][1 - cx])
            P.dve(lambda e, pb=pb, j=j, cx=cx, xo=xo: e.tensor_tensor(out=xo, in0=pP[j], in1=pb["Xm"][cx], op=ALU.add),
                  reads=[bpP[j], pb["bXm"][cx]], writes=[bxo])
            yield
        cur, cx = 1 - cur, 1 - cx
    for j in range(NP):
        pb = PBUF[j]
        c0 = j * 128
        SC = pb["SC"]
        for h in range(H):
            P.pe(lambda e, h=h, c0=c0, j=j: e.transpose(out=pPb[j][:, h, :], in_=KT[:, h, c0:c0 + 128], identity=ident),
                 reads=[bKT[h], bc], writes=[bpP[j]])
        P.dve(lambda e, pb=pb, SC=SC, j=j: e.tensor_tensor(out=pb["KHT"], in0=pPb[j], in1=bc4(SC[:, 5, :]), op=ALU.mult),
              reads=[bpP[j], pb["bSC"]], writes=[pb["bKHT"]])
        for h in range(H):
            P.pe(lambda e, h=h, c0=c0, j=j: e.transpose(out=pPb[j][:, h, :], in_=VT[:, h, c0:c0 + 128], identity=ident),
                 reads=[bVT[h], bc], writes=[bpP[j]])
        P.act(lambda e, pb=pb, j=j: e.activation(out=pb["VTK"], in_=pPb[j], func=AF.Copy), reads=[bpP[j]], writes=[pb["bVTK"]])
        P.pool(lambda e, pb=pb, c0=c0: e.tensor_copy(out=pb["KTP"], in_=KT[:, :, c0:c0 + 128]), reads=bKT, writes=[pb["bKTP"]])
        P.pool(lambda e, pb=pb, j=j: e.tensor_tensor(out=pb["BV"], in0=pb["VTK"], in1=bc4(BETA[:, j, :]), op=ALU.mult),
               reads=[pb["bVTK"], bBG], writes=[pb["bBV"]])
        yield


def _gdn_rec(self, L, full):
    P = self.P
    H = 4
    g = lambda n: L[n]
    bk, bb = self.banks, self.bbank
    bc = self.bconst
    mask2, ident = self.mask2, self.ident
    maskL, bones, ch01, identb4 = g("maskL"), g("bones"), g("ch01"), g("identb4")
    PBUF, GG, LNB, BETA, bBG = g("PB"), g("GG"), g("LNB"), g("BETA"), g("bBG")
    QT, KT, VT, bQT, bKT, bVT = g("QT"), g("KT"), g("VT"), g("bQT"), g("bKT"), g("bVT")
    R, USB, bR, bUSB = g("R"), g("USB"), g("bR"), g("bUSB")
    S32, SBF, bS32, bSBF, sbf_i = g("S32"), g("SBF"), g("bS32"), g("bSBF"), g("sbf_i")
    O32, bO32 = g("O32"), g("bO32")
    evac, nextpj = g("evac"), g("nextpj")
    NP = NT
    pP = [bk[3 + j][:].rearrange("p (h t) -> p h t", h=H) for j in range(NP)]
    pPb = [bk[3 + j][:].bitcast(BF16)[:, 0:512].rearrange("p (h t) -> p h t", h=H) for j in range(NP)]
    bpP = [bb[3 + j] for j in range(NP)]
    ptr4 = bk[5][:].bitcast(BF16)[:, 0:512].rearrange("p (h t) -> p h t", h=H)
    pKS = bk[5][:].rearrange("p (h t) -> p h t", h=H)
    pU = bk[6][:].rearrange("p (h t) -> p h t", h=H)
    po = bk[7][:].rearrange("p (h t) -> p h t", h=H)

    def bc4(ap):
        return ap.unsqueeze(2).to_broadcast([128, H, 128])

    for j in range(NP):
        pb = PBUF[j]
        c0 = j * 128
        SC = pb["SC"]
        TT, bTT = pb["TT"], pb["bTT"]
        for half in range(2):
            r0 = half * 64
            cs = sbf_i[0]
            for h in range(H):
                P.pe(lambda e, h=h, pb=pb, cs=cs: e.matmul(pKS[:, h, :], lhsT=pb["KTP"][:, h, :], rhs=SBF[cs][:, h, :],
                                                           start=True, stop=True),
                     reads=[pb["bKTP"], bSBF[cs][h]], writes=[bb[5]])
            if full:
                for h in range(H):
                    P.pe(lambda e, pb=pb, h=h, cs=cs, r0=r0, half=half: e.matmul(
                        po[:, h, r0:r0 + 64], lhsT=SBF[cs][:, h, :], rhs=pb["QTG"][:, h, r0:r0 + 64],
                        start=(h == 0 and half == 0), stop=False, skip_group_check=True),
                        reads=[bSBF[cs][h], pb["bQTG"]], writes=[bb[7]])
            for h in range(H):
                P.dve(lambda e, pb=pb, h=h, r0=r0, SC=SC: e.scalar_tensor_tensor(
                    out=R[r0:r0 + 64, h, :], in0=pKS[r0:r0 + 64, h, :], scalar=SC[r0:r0 + 64, 4, h:h + 1],
                    in1=pb["BV"][r0:r0 + 64, h, :], op0=ALU.mult, op1=ALU.add),
                    reads=[bb[5], pb["bSC"], pb["bBV"]], writes=[bR])
            yield
            for h in range(H):
                P.pe(lambda e, h=h, r0=r0, TT=TT: e.matmul(pU[:, h, :], lhsT=TT[r0:r0 + 64, h, :], rhs=R[r0:r0 + 64, h, :],
                                                           start=True, stop=True),
                     reads=[bTT, bR], writes=[bb[6]])
            yield
            P.act(lambda e, r0=r0: e.activation(out=USB[r0:r0 + 64], in_=pU[r0:r0 + 64], func=AF.Copy),
                  reads=[bb[6]], writes=[bUSB])
            yield
            pS, bpS = bk[6][:], bb[6]
            pS4 = pS.rearrange("p (h t) -> p h t", h=H)
            for h in range(H):
                P.pe(lambda e, pb=pb, h=h, r0=r0, pS4=pS4: e.matmul(pS4[:, h, :], lhsT=pb["KHT"][r0:r0 + 64, h, :],
                                                                    rhs=USB[r0:r0 + 64, h, :], start=True, stop=True),
                     reads=[pb["bKHT"], bUSB], writes=[bpS])
            yield
            for h in range(H):
                P.dve(lambda e, h=h, half=half, SC=SC, pS4=pS4: e.scalar_tensor_tensor(
                    out=S32[:, h, :], in0=S32[:, h, :], scalar=SC[:, 6 + half, h:h + 1], in1=pS4[:, h, :],
                    op0=ALU.mult, op1=ALU.add), reads=[bS32[h], pb["bSC"], bpS], writes=[bS32[h]])
            yield
            nx = 1 - cs
            P.act(lambda e, nx=nx: e.activation(out=SBF[nx], in_=S32, func=AF.Copy), reads=bS32, writes=bSBF[nx])
            sbf_i[0] = nx
            yield
        if full:
            for h in range(H):
                P.pe(lambda e, pb=pb, h=h: e.matmul(po[:, h, :], lhsT=USB[:, h, :], rhs=pb["ATT"][:, h, :],
                                                    start=False, stop=True, skip_group_check=True),
                     reads=[bUSB, pb["bATT"]], writes=[bb[7]])
            P.act(lambda e, c0=c0: e.activation(out=O32[:, :, c0:c0 + 128], in_=po, func=AF.Copy),
                  reads=[bb[7]], writes=[bO32])


KB._gdn_prep = _gdn_prep
KB._gdn_rec = _gdn_rec


CAP = 384
NEXP = 32
NSLOT = NEXP * CAP
BIG = 1.0e4


def _spill(self, name, sb_ap, bufs, sb):
    P = self.P
    if name not in self.d:
        self.d[name] = self.nc.dram_tensor(name, [128, 4, self.nfull_tok], BF16, kind="Internal").ap()
        self.bspill = getattr(self, "bspill", {})
        self.bspill[name] = {}
    dst = self.d[name]
    tok0 = (sb - self.npre) * SBT
    b = Buf(name)
    self.bspill[name][sb - self.npre] = b
    P.dma(lambda e: e.dma_start(out=dst[:, :, tok0:tok0 + SBT], in_=sb_ap), reads=bufs, writes=[b])


KB.spill = _spill


def _pass3(self):
    nc, P, A = self.nc, self.P, self.A
    d = self.d
    H = 4
    for nm, shp in (("hg_up", [512, D]), ("gd_up", [512, D]), ("w_out", [D, D]), ("norm_ffn_g", [D]),
                    ("router_w", [D, 36]), ("router_b", [36])):
        self.din(nm, shp)
    ntile = self.nfull_tok // 128
    d["h2_s"] = nc.dram_tensor("h2_s", [self.nfull_tok, D], F32, kind="Internal").ap()
    self.bh2s = [Buf("h2_s%d" % i) for i in range(ntile)]
    self.bxbuf = []
    self.W12 = A.alloc((ntile, 2), F32)
    self.DST = A.alloc((ntile, 2), U32)
    self.bW12 = Buf("W12")
    self.bDST = Buf("DST")
    m0 = A.mark()
    W3 = A.alloc((KC, 2048), BF16)
    bW3 = [Buf() for _ in range(KC)]
    wv = d["w_in"].rearrange("(k p) n -> p k n", p=128)
    for k in range(KC):
        P.dma(lambda e, k=k: e.dma_start(out=W3[:, k, :], in_=wv[:, k, C_PA:C_PA + 2048]), writes=[bW3[k]], q="pool")
    HGUP = A.alloc((H, D), BF16)
    GDUP = A.alloc((H, D), BF16)
    WOUT = A.alloc((KC, D), BF16)
    bUP = Buf()
    bWO = [Buf() for _ in range(KC)]
    P.dma(lambda e: e.dma_start(out=HGUP, in_=d["hg_up"].rearrange("(h p) n -> p h n", p=128)), writes=[bUP], q="pool")
    P.dma(lambda e: e.dma_start(out=GDUP, in_=d["gd_up"].rearrange("(h p) n -> p h n", p=128)), writes=[bUP], q="pool")
    wo = d["w_out"].rearrange("(k p) n -> p k n", p=128)
    for k in range(KC):
        P.dma(lambda e, k=k: e.dma_start(out=WOUT[:, k, :], in_=wo[:, k, :]), writes=[bWO[k]], q="pool")
    WR = A.alloc((KC, 36), F32)
    RB = A.alloc((36,), F32)
    G2 = A.alloc((D,), F32)
    ECAP = A.alloc((NEXP,), F32)
    bpar = Buf("par3")
    P.dma(lambda e: e.dma_start(out=WR, in_=d["router_w"].rearrange("(k p) n -> p k n", p=128)), writes=[bpar])
    P.dma(lambda e: e.dma_start(out=RB, in_=d["router_b"].partition_broadcast(128)), writes=[bpar])
    P.dma(lambda e: e.dma_start(out=G2, in_=d["norm_ffn_g"].partition_broadcast(128)), writes=[bpar])
    self.load_gain("norm_mix_g")
    ecapi = A.alloc((NEXP,), I32)
    P.pool(lambda e: e.iota(ecapi, pattern=[[CAP, NEXP]], base=0, channel_multiplier=0), writes=[bpar])
    P.pool(lambda e: e.tensor_copy(out=ECAP, in_=ecapi), reads=[bpar], writes=[bpar])
    triS = A.alloc((128,), BF16)
    trif = A.alloc((128,), F32)
    bc = self.bconst
    P.pool(lambda e: e.memset(trif, 1.0), writes=[bc])
    P.pool(lambda e: e.affine_select(out=trif, in_=trif, pattern=[[1, 128]], compare_op=ALU.is_gt, fill=0.0,
                                     base=0, channel_multiplier=-1), reads=[bc], writes=[bc])
    P.pool(lambda e: e.tensor_copy(out=triS, in_=trif), reads=[bc], writes=[bc])
    BASE = A.alloc((NEXP,), F32)
    bBASE = Buf()
    P.pool(lambda e: e.tensor_copy(out=BASE, in_=ecapi), reads=[bpar], writes=[bBASE])

    xnTs = [A.alloc((KC, SBT), BF16) for _ in range(2)]
    bxnTs = [Buf(), Buf()]
    self._xfetched = set()
    use_fetch = hasattr(self, "bxns")
    SGs = [A.alloc((16, SBT), BF16) for _ in range(2)]
    bSGs = [[Buf() for _ in range(16)] for _ in range(2)]
    OAss = [A.alloc((H, SBT), BF16) for _ in range(2)]
    OBss = [A.alloc((H, SBT), BF16) for _ in range(2)]
    bOAss, bOBss = [Buf(), Buf()], [Buf(), Buf()]
    T1 = [A.alloc((SBT,), F32) for _ in range(2)]
    T2 = [A.alloc((SBT,), F32) for _ in range(2)]
    bT1 = [Buf(), Buf()]
    bT2 = [Buf(), Buf()]
    MG = A.alloc((KC, SBT), BF16)
    bMG = [Buf() for _ in range(KC)]
    XR = A.alloc((D,), F32)
    bXR = Buf()
    H2 = A.alloc((D,), F32)
    bH2 = Buf()
    JK = A.alloc((D,), BF16)
    SS2 = A.alloc((1,), F32)
    bSS2 = Buf()
    XF = A.alloc((D,), F32)
    XB = A.alloc((D,), BF16)
    bXF, bXB = Buf(), Buf()
    XFT = A.alloc((KC, 128), F32)
    bXFT = Buf()
    LG = A.alloc((36,), F32)
    ME = A.alloc((NEXP,), F32)
    SM = A.alloc((16,), F32)
    M8 = A.alloc((8,), F32)
    SEL1 = A.alloc((NEXP,), F32)
    SEL2 = A.alloc((NEXP,), F32)
    SELB = A.alloc((NEXP,), BF16)
    RK = A.alloc((NEXP,), F32)
    JK2 = A.alloc((NEXP,), F32)
    DF = A.alloc((2,), F32)
    brt = Buf("route")

    bk, bb = self.banks, self.bbank
    ptr = bk[0][:].bitcast(BF16).rearrange("p (k t) -> p k t", k=KC)
    pj = [bk[1][:], bk[2][:]]
    bpj = [bb[1], bb[2]]
    pup = [bk[3][:], bk[4][:]]
    pji = [0]

    def nextpj():
        i = pji[0] % 2
        pji[0] += 1
        return pj[i], bpj[i]

    pyi = [0]

    def nextpy():
        i = 5 + pyi[0] % 3
        pyi[0] += 1
        return bk[i][:], bb[i]

    oa_d, ob_d = d["oa_s"], d["ob_s"]
    def X3(sbi):
        sl = sbi % 2
        SG, bSG, OAs, OBs, bOAs, bOBs = SGs[sl], bSGs[sl], OAss[sl], OBss[sl], bOAss[sl], bOBss[sl]
        sb = self.npre + sbi
        tok0 = sbi * SBT
        xnT, bxnT = xnTs[sb % 2], bxnTs[sb % 2]
        if use_fetch:
            yield from self.xnt_fetch_gen(sb, self.nsb - 1, xnTs, bxnTs)
        else:
            yield from self.stage_a_gen(sb, xnT, bxnT, ptr, bb[0])
        P.dma(lambda e, tok0=tok0: e.dma_start(out=OAs, in_=oa_d[:, :, tok0:tok0 + SBT]),
              reads=[self.bspill["oa_s"][sbi]], writes=[bOAs])
        P.dma(lambda e, tok0=tok0: e.dma_start(out=OBs, in_=ob_d[:, :, tok0:tok0 + SBT]),
              reads=[self.bspill["ob_s"][sbi]], writes=[bOBs])
        for cb in range(16):
            p, bp = nextpj()
            for k in range(KC):
                P.pe(lambda e, k=k, p=p, cb=cb: e.matmul(p[:, 0:SBT], lhsT=W3[:, k, cb * 128:(cb + 1) * 128],
                                                         rhs=xnT[:, k, :], start=(k == 0), stop=(k == KC - 1)),
                     reads=[bW3[k], bxnT], writes=[bp])
            P.act(lambda e, cb=cb, p=p: e.activation(out=SG[:, cb, :], in_=p[:, 0:SBT], func=AF.Sigmoid),
                  reads=[bp], writes=[bSG[cb]])
            yield
    def Y3(sbi):
        sl = sbi % 2
        SG, bSG, OAs, OBs, bOAs, bOBs = SGs[sl], bSGs[sl], OAss[sl], OBss[sl], bOAss[sl], bOBss[sl]
        sb = self.npre + sbi
        tok0 = sbi * SBT
        for cb in range(KC):
            i = cb % 2
            for h in range(H):
                P.pe(lambda e, h=h, cb=cb: e.matmul(pup[0][:, 0:SBT], lhsT=HGUP[:, h, cb * 128:(cb + 1) * 128],
                                                    rhs=OAs[:, h, :], start=(h == 0), stop=(h == H - 1)),
                     reads=[bUP, bOAs], writes=[bb[3]])
            for h in range(H):
                P.pe(lambda e, h=h, cb=cb: e.matmul(pup[1][:, 0:SBT], lhsT=GDUP[:, h, cb * 128:(cb + 1) * 128],
                                                    rhs=OBs[:, h, :], start=(h == 0), stop=(h == H - 1)),
                     reads=[bUP, bOBs], writes=[bb[4]])
            P.dve(lambda e, cb=cb, i=i: e.tensor_tensor(out=T1[i], in0=pup[0][:, 0:SBT], in1=SG[:, cb, :], op=ALU.mult),
                  reads=[bb[3], bSG[cb]], writes=[bT1[i]])
            P.dve(lambda e, cb=cb, i=i: e.tensor_tensor(out=T2[i], in0=pup[1][:, 0:SBT], in1=SG[:, 8 + cb, :], op=ALU.mult),
                  reads=[bb[4], bSG[8 + cb]], writes=[bT2[i]])
            P.pool(lambda e, cb=cb, i=i: e.tensor_tensor(out=MG[:, cb, :], in0=T1[i], in1=T2[i], op=ALU.add),
                   reads=[bT1[i], bT2[i]], writes=[bMG[cb]])
            yield
        for t in range(NT):
            gt = sbi * NT + t
            gtok = self.npre * SBT + gt * 128
            P.dma(lambda e, gtok=gtok: e.dma_start(out=XR, in_=d["xs"][gtok:gtok + 128, :]), writes=[bXR])
            for half in range(2):
                p, bp = nextpy()
                for k in range(KC):
                    P.pe(lambda e, k=k, p=p, t=t, half=half: e.matmul(
                        p, lhsT=MG[:, k, t * 128:(t + 1) * 128], rhs=WOUT[:, k, half * 512:(half + 1) * 512],
                        start=(k == 0), stop=(k == KC - 1)), reads=[bMG[k], bWO[k]], writes=[bp])
                P.dve(lambda e, p=p, half=half: e.tensor_tensor(out=H2[:, half * 512:(half + 1) * 512], in0=p,
                                                               in1=XR[:, half * 512:(half + 1) * 512], op=ALU.add),
                      reads=[bp, bXR], writes=[bH2])
                yield
            P.dma(lambda e, gt=gt: e.dma_start(out=d["h2_s"][gt * 128:(gt + 1) * 128, :], in_=H2),
                  reads=[bH2], writes=[self.bh2s[gt]])
            LL = dict(L0)
            LL['nextpj'] = nextpy
            yield from self._route_tile(LL, gt)
    L0 = dict(locals())

    import os
    WTS3 = [int(v) for v in os.environ.get("IL_W3", "1,1").split(",")]

    def run_il(gens):
        gens = list(gens)
        while gens:
            for g_, w_ in list(gens):
                for _ in range(w_):
                    try:
                        next(g_)
                    except StopIteration:
                        gens.remove((g_, w_))
                        break

    n = self.nfull
    if LS >= 2:
        sx = [P._capture(X3(i)) for i in range(n)]
        sy = [P._capture(Y3(i)) for i in range(n)]
        P.merge_pipeline([sx, sy], [lambda s_, dn: dn[1] >= s_ - 1, lambda s_, dn: dn[0] >= s_ + 1])
    else:
        for r in range(-1, n):
            gs = []
            if 0 <= r < n:
                gs.append((Y3(r), WTS3[0]))
            if 0 <= r + 1 < n:
                gs.append((X3(r + 1), WTS3[1]))
            if LS:
                P.merge_streams([g_ for g_, _w in gs])
            else:
                run_il(gs)
    A.reset(m0)
    P.barrier()


KB.pass3 = _pass3


def _route_tile(self, L, gt):
    P = self.P
    g = lambda n: L[n]
    bk, bb = self.banks, self.bbank
    bc = self.bconst
    H2, bH2, JK, SS2, bSS2 = g("H2"), g("bH2"), g("JK"), g("SS2"), g("bSS2")
    XF, XB, bXF, bXB, XFT, bXFT = g("XF"), g("XB"), g("bXF"), g("bXB"), g("XFT"), g("bXFT")
    G2, WR, RB, ECAP, bpar = g("G2"), g("WR"), g("RB"), g("ECAP"), g("bpar")
    LG, ME, SM, M8, SEL1, SEL2, SELB, RK, JK2, DF, brt = (g("LG"), g("ME"), g("SM"), g("M8"), g("SEL1"), g("SEL2"),
                                                           g("SELB"), g("RK"), g("JK2"), g("DF"), g("brt"))
    BASE, bBASE, triS = g("BASE"), g("bBASE"), g("triS")
    nextpj = g("nextpj")
    W12, DST = self.W12, self.DST
    P.act(lambda e: e.activation(out=JK, in_=H2, func=AF.Square, accum_out=SS2), reads=[bH2], writes=[bSS2])
    P.act(lambda e: e.activation(out=SM[:, 0:1], in_=SS2, func=AF.Ln, scale=1.0 / D, bias=EPS), reads=[bSS2], writes=[brt])
    P.act(lambda e: e.activation(out=SM[:, 0:1], in_=SM[:, 0:1], func=AF.Exp, scale=-0.5), reads=[brt], writes=[brt])
    P.dve(lambda e: e.scalar_tensor_tensor(out=XF, in0=H2, scalar=SM[:, 0:1], in1=G2, op0=ALU.mult, op1=ALU.mult),
          reads=[bH2, brt, bpar], writes=[bXF])
    P.pool(lambda e: e.tensor_copy(out=XB, in_=XF), reads=[bXF], writes=[bXB])
    yield
    for half in range(2):
        for kk in range(4):
            k = half * 4 + kk
            P.pe(lambda e, k=k, kk=kk, half=half: e.transpose(out=bk[3 + half][:, kk * 128:(kk + 1) * 128],
                                                              in_=XF[:, k * 128:(k + 1) * 128], identity=self.identf),
                 reads=[bXF, bc], writes=[bb[3 + half]])
    P.act(lambda e: e.activation(out=XFT[:, 0:4, :], in_=bk[3][:].rearrange("p (k t) -> p k t", k=4), func=AF.Copy),
          reads=[bb[3]], writes=[bXFT])
    P.dve(lambda e: e.tensor_copy(out=XFT[:, 4:8, :], in_=bk[4][:].rearrange("p (k t) -> p k t", k=4)),
          reads=[bb[4]], writes=[bXFT])
    yield
    p, bp = nextpj()
    for k in range(KC):
        P.pe(lambda e, k=k, p=p: e.matmul(p[:, 0:36], lhsT=XFT[:, k, :], rhs=WR[:, k, :], start=(k == 0), stop=(k == KC - 1)),
             reads=[bXFT, bpar], writes=[bp])
    P.dve(lambda e, p=p: e.tensor_tensor(out=LG, in0=p[:, 0:36], in1=RB, op=ALU.add), reads=[bp, bpar], writes=[brt])
    yield
    P.dve(lambda e: e.tensor_reduce(out=SM[:, 1:2], in_=LG[:, 0:4], axis=AX.X, op=ALU.max), reads=[brt], writes=[brt])
    P.dve(lambda e: e.tensor_scalar(out=SM[:, 2:3], in0=SM[:, 1:2], scalar1=-1.0, scalar2=None, op0=ALU.mult),
          reads=[brt], writes=[brt])
    P.act(lambda e: e.activation(out=JK2[:, 0:4], in_=LG[:, 0:4], func=AF.Exp, bias=SM[:, 2:3], accum_out=SM[:, 3:4]),
          reads=[brt], writes=[brt])
    P.dve(lambda e: e.reciprocal(out=SM[:, 4:5], in_=SM[:, 3:4]), reads=[brt], writes=[brt])
    yield
    P.dve(lambda e: e.tensor_scalar(out=SM[:, 12:16], in0=LG[:, 0:4], scalar1=SM[:, 1:2], scalar2=-1.0,
                                    op0=ALU.is_equal, op1=ALU.add), reads=[brt], writes=[brt])
    P.dve(lambda e: e.scalar_tensor_tensor(out=ME.rearrange("p (g j) -> p g j", g=4),
                                           in0=SM[:, 12:16].unsqueeze(2).to_broadcast([128, 4, 8]), scalar=BIG,
                                           in1=LG[:, 4:36].rearrange("p (g j) -> p g j", g=4),
                                           op0=ALU.mult, op1=ALU.add), reads=[brt], writes=[brt])
    P.dve(lambda e: e.max(out=M8, in_=ME), reads=[brt], writes=[brt])
    yield
    P.dve(lambda e: e.tensor_tensor(out=SM[:, 5:6], in0=M8[:, 1:2], in1=M8[:, 0:1], op=ALU.subtract), reads=[brt], writes=[brt])
    P.act(lambda e: e.activation(out=SM[:, 6:7], in_=SM[:, 5:6], func=AF.Exp), reads=[brt], writes=[brt])
    P.dve(lambda e: e.tensor_scalar(out=SM[:, 7:8], in0=SM[:, 6:7], scalar1=1.0, scalar2=None, op0=ALU.add),
          reads=[brt], writes=[brt])
    P.dve(lambda e: e.reciprocal(out=SM[:, 8:9], in_=SM[:, 7:8]), reads=[brt], writes=[brt])
    P.dve(lambda e, gt=gt: e.tensor_tensor(out=W12[:, gt, 0:1], in0=SM[:, 8:9], in1=SM[:, 4:5], op=ALU.mult),
          reads=[brt], writes=[self.bW12])
    P.dve(lambda e, gt=gt: e.tensor_tensor(out=W12[:, gt, 1:2], in0=SM[:, 4:5], in1=W12[:, gt, 0:1], op=ALU.subtract),
          reads=[brt, self.bW12], writes=[self.bW12])
    P.dve(lambda e: e.tensor_scalar(out=SEL1, in0=ME, scalar1=M8[:, 0:1], scalar2=None, op0=ALU.is_equal),
          reads=[brt], writes=[brt])
    P.dve(lambda e: e.tensor_scalar(out=SEL2, in0=ME, scalar1=M8[:, 1:2], scalar2=None, op0=ALU.is_equal),
          reads=[brt], writes=[brt])
    P.dve(lambda e: e.tensor_tensor(out=SELB, in0=SEL1, in1=SEL2, op=ALU.add), reads=[brt], writes=[brt])
    yield
    p2, bp2 = nextpj()
    P.pe(lambda e, p2=p2: e.matmul(p2[:, 0:32], lhsT=triS, rhs=SELB, start=True, stop=True), reads=[brt, bc], writes=[bp2])
    P.pe(lambda e, p2=p2: e.matmul(p2[:, 32:64], lhsT=self.ones_bf, rhs=SELB, start=True, stop=True),
         reads=[brt, bc], writes=[bp2])
    P.dve(lambda e, p2=p2: e.tensor_tensor(out=RK, in0=p2[:, 0:32], in1=BASE, op=ALU.add), reads=[bp2, bBASE], writes=[brt])
    P.dve(lambda e, p2=p2: e.tensor_tensor(out=BASE, in0=p2[:, 32:64], in1=BASE, op=ALU.add), reads=[bp2, bBASE], writes=[bBASE])
    P.dve(lambda e: e.scalar_tensor_tensor(out=JK2, in0=SEL1, scalar=1.0, in1=RK, op0=ALU.mult, op1=ALU.mult,
                                           accum_out=DF[:, 0:1]), reads=[brt], writes=[brt])
    P.dve(lambda e: e.scalar_tensor_tensor(out=JK2, in0=SEL2, scalar=1.0, in1=RK, op0=ALU.mult, op1=ALU.mult,
                                           accum_out=DF[:, 1:2]), reads=[brt], writes=[brt])
    P.dve(lambda e, gt=gt: e.tensor_copy(out=DST[:, gt, :], in_=DF), reads=[brt], writes=[self.bDST])
    yield
    xb = self.d["x_buf"]
    for k in range(2):
        bx = Buf("xbuf")
        self.bxbuf.append(bx)
        P.dma(lambda e, gt=gt, k=k: e.indirect_dma_start(
            out=xb, out_offset=bass.IndirectOffsetOnAxis(ap=DST[:, gt, k:k + 1], axis=0), in_=XB, in_offset=None),
            reads=[bXB, self.bDST] + self.bxz, writes=[bx], q="pool")


KB._route_tile = _route_tile


def _pass4(self):
    nc, P, A = self.nc, self.P, self.A
    d = self.d
    self.din("w_gate", [NEXP, D, 512])
    self.din("w_up", [NEXP, D, 512])
    self.din("w_down", [NEXP, 512, D])
    d["y_buf"] = nc.dram_tensor("y_buf", [NSLOT, D], F32, kind="Internal").ap()
    self.bybuf = []
    m0 = A.mark()
    NB = CAP // 128
    WG = [A.alloc((KC, 512), BF16) for _ in range(2)]
    WU = [A.alloc((KC, 512), BF16) for _ in range(2)]
    WD = [A.alloc((4, D), BF16) for _ in range(2)]
    bWG = [Buf(), Buf()]
    bWU = [Buf(), Buf()]
    bWD = [Buf(), Buf()]
    XE = [A.alloc((NB, D), BF16) for _ in range(2)]
    bXE = [Buf(), Buf()]
    XET = [A.alloc((KC, CAP), BF16) for _ in range(2)]
    bXET = [Buf(), Buf()]
    SGT = [A.alloc((CAP,), F32) for _ in range(2)]
    bSGT = [Buf(), Buf()]
    HT = A.alloc((4, CAP), BF16)
    bHT = [Buf() for _ in range(4)]
    YS = [A.alloc((D,), F32) for _ in range(2)]
    bYS = [Buf(), Buf()]
    bk, bb = self.banks, self.bbank
    ptr = bk[0][:].bitcast(BF16).rearrange("p (k t) -> p k t", k=KC)
    ident = self.ident
    bc = self.bconst
    xb, yb = d["x_buf"], d["y_buf"]
    cnt = [0]

    def bank(lo, n):
        i = lo + cnt[0] % n
        cnt[0] += 1
        return bk[i][:], bb[i]

    def load_w(e):
        i = e % 2
        P.dma(lambda eng, e=e, i=i: eng.dma_start(out=WG[i], in_=d["w_gate"][e].rearrange("(k p) n -> p k n", p=128)),
              writes=[bWG[i]], q="pool")
        P.dma(lambda eng, e=e, i=i: eng.dma_start(out=WU[i], in_=d["w_up"][e].rearrange("(k p) n -> p k n", p=128)),
              writes=[bWU[i]], q="pool")
        P.dma(lambda eng, e=e, i=i: eng.dma_start(out=WD[i], in_=d["w_down"][e].rearrange("(k p) n -> p k n", p=128)),
              writes=[bWD[i]], q="pool")

    def load_x(e):
        i = e % 2
        P.dma(lambda eng, e=e, i=i: eng.dma_start(out=XE[i], in_=xb[e * CAP:(e + 1) * CAP, :].rearrange("(b p) n -> p b n", p=128)),
              reads=self.bxbuf, writes=[bXE[i]])

    def transposes(e):
        i = e % 2
        for b in range(NB):
            pt, bpt = (ptr, bb[0]) if b % 2 == 0 else (ptr7, bb[7])
            for k in range(KC):
                P.pe(lambda eng, i=i, b=b, k=k, pt=pt: eng.transpose(out=pt[:, k, :], in_=XE[i][:, b, k * 128:(k + 1) * 128], identity=ident),
                     reads=[bXE[i], bc], writes=[bpt])
            if b % 2 == 0:
                P.act(lambda eng, b=b, i=i, pt=pt: eng.activation(out=XET[i][:, :, b * 128:(b + 1) * 128], in_=pt, func=AF.Copy),
                      reads=[bpt], writes=[bXET[i]])
            else:
                P.dve(lambda eng, b=b, i=i, pt=pt: eng.tensor_copy(out=XET[i][:, :, b * 128:(b + 1) * 128], in_=pt),
                      reads=[bpt], writes=[bXET[i]])

    ptr7 = bk[7][:].bitcast(BF16).rearrange("p (k t) -> p k t", k=KC)
    load_w(0)
    load_x(0)
    transposes(0)
    yi = 0
    for e in range(NEXP):
        i = e % 2
        if e + 1 < NEXP:
            load_w(e + 1)
            load_x(e + 1)
        for fc in range(4):
            pg, bpg = bk[1 + fc % 2][:], bb[1 + fc % 2]
            pu, bpu = bk[3 + fc % 2][:], bb[3 + fc % 2]
            for k in range(KC):
                P.pe(lambda eng, i=i, fc=fc, k=k, pg=pg: eng.matmul(pg[:, 0:CAP], lhsT=WG[i][:, k, fc * 128:(fc + 1) * 128],
                                                                    rhs=XET[i][:, k, :], start=(k == 0), stop=(k == KC - 1)),
                     reads=[bWG[i], bXET[i]], writes=[bpg])
            for k in range(KC):
                P.pe(lambda eng, i=i, fc=fc, k=k, pu=pu: eng.matmul(pu[:, 0:CAP], lhsT=WU[i][:, k, fc * 128:(fc + 1) * 128],
                                                                    rhs=XET[i][:, k, :], start=(k == 0), stop=(k == KC - 1)),
                     reads=[bWU[i], bXET[i]], writes=[bpu])
            j = fc % 2
            P.act(lambda eng, pg=pg, j=j: eng.activation(out=SGT[j], in_=pg[:, 0:CAP], func=AF.Silu), reads=[bpg], writes=[bSGT[j]])
            P.dve(lambda eng, pu=pu, j=j, fc=fc: eng.tensor_tensor(out=HT[:, fc, :], in0=pu[:, 0:CAP], in1=SGT[j], op=ALU.mult),
                  reads=[bpu, bSGT[j]], writes=[bHT[fc]])
        if e + 1 < NEXP:
            transposes(e + 1)
        for b in range(NB):
            ys, bys = YS[yi % 2], bYS[yi % 2]
            yi += 1
            for half in range(2):
                pd, bpd = bk[5 + half][:], bb[5 + half]
                for fc in range(4):
                    P.pe(lambda eng, i=i, b=b, fc=fc, half=half, pd=pd: eng.matmul(
                        pd, lhsT=HT[:, fc, b * 128:(b + 1) * 128], rhs=WD[i][:, fc, half * 512:(half + 1) * 512],
                        start=(fc == 0), stop=(fc == 3)), reads=[bHT[fc], bWD[i]], writes=[bpd])
                if half == 0:
                    P.act(lambda eng, pd=pd, ys=ys: eng.activation(out=ys[:, 0:512], in_=pd, func=AF.Copy), reads=[bpd], writes=[bys])
                else:
                    P.dve(lambda eng, pd=pd, ys=ys: eng.tensor_copy(out=ys[:, 512:1024], in_=pd), reads=[bpd], writes=[bys])
            r0 = e * CAP + b * 128
            by = Buf("ybuf")
            self.bybuf.append(by)
            P.dma(lambda eng, r0=r0, ys=ys: eng.dma_start(out=yb[r0:r0 + 128, :], in_=ys), reads=[bys], writes=[by])
    A.reset(m0)
    P.barrier()


def _pass5(self):
    nc, P, A = self.nc, self.P, self.A
    d = self.d
    self.din("final_norm_g", [D])
    out = self.dout("out", [self.nfull_tok, D], F32)
    m0 = A.mark()
    ntile = self.nfull_tok // 128
    FG = A.alloc((D,), F32)
    bFG = Buf()
    P.dma(lambda e: e.dma_start(out=FG, in_=d["final_norm_g"].partition_broadcast(128)), writes=[bFG])
    NB5 = 4
    Y1 = [A.alloc((D,), F32) for _ in range(NB5)]
    Y2 = [A.alloc((D,), F32) for _ in range(NB5)]
    HH = [A.alloc((D,), F32) for _ in range(NB5)]
    OT = [A.alloc((D,), F32) for _ in range(NB5)]
    bY1, bY2, bHH, bOT = ([Buf() for _ in range(NB5)], [Buf() for _ in range(NB5)], [Buf() for _ in range(NB5)],
                          [Buf() for _ in range(NB5)])
    JK = A.alloc((D,), BF16)
    SS = A.alloc((ntile,), F32)
    bSS = Buf()
    yb = d["y_buf"]
    for gt in range(ntile):
        i = gt % NB5
        P.dma(lambda e, gt=gt, i=i: e.indirect_dma_start(
            out=Y1[i], out_offset=None, in_=yb, in_offset=bass.IndirectOffsetOnAxis(ap=self.DST[:, gt, 0:1], axis=0)),
            reads=self.bybuf + [self.bDST], writes=[bY1[i]], q="pool")
        P.dma(lambda e, gt=gt, i=i: e.indirect_dma_start(
            out=Y2[i], out_offset=None, in_=yb, in_offset=bass.IndirectOffsetOnAxis(ap=self.DST[:, gt, 1:2], axis=0)),
            reads=self.bybuf + [self.bDST], writes=[bY2[i]], q="pool")
        P.dma(lambda e, gt=gt, i=i: e.dma_start(out=HH[i], in_=d["h2_s"][gt * 128:(gt + 1) * 128, :]),
              reads=[self.bh2s[gt]], writes=[bHH[i]])
        P.dve(lambda e, gt=gt, i=i: e.scalar_tensor_tensor(out=HH[i], in0=Y1[i], scalar=self.W12[:, gt, 0:1], in1=HH[i],
                                                          op0=ALU.mult, op1=ALU.add),
              reads=[bY1[i], bHH[i], self.bW12], writes=[bHH[i]])
        P.dve(lambda e, gt=gt, i=i: e.scalar_tensor_tensor(out=HH[i], in0=Y2[i], scalar=self.W12[:, gt, 1:2], in1=HH[i],
                                                          op0=ALU.mult, op1=ALU.add),
              reads=[bY2[i], bHH[i], self.bW12], writes=[bHH[i]])
        P.act(lambda e, gt=gt, i=i: e.activation(out=JK, in_=HH[i], func=AF.Square, accum_out=SS[:, gt:gt + 1]),
              reads=[bHH[i]], writes=[bSS])
        P.act(lambda e, gt=gt: e.activation(out=SS[:, gt:gt + 1], in_=SS[:, gt:gt + 1], func=AF.Ln, scale=1.0 / D, bias=EPS),
              reads=[bSS], writes=[bSS])
        P.act(lambda e, gt=gt: e.activation(out=SS[:, gt:gt + 1], in_=SS[:, gt:gt + 1], func=AF.Exp, scale=-0.5),
              reads=[bSS], writes=[bSS])
        P.dve(lambda e, gt=gt, i=i: e.scalar_tensor_tensor(out=OT[i], in0=HH[i], scalar=SS[:, gt:gt + 1], in1=FG,
                                                          op0=ALU.mult, op1=ALU.mult),
              reads=[bHH[i], bSS, bFG], writes=[bOT[i]])
        P.dma(lambda e, gt=gt, i=i: e.dma_start(out=out[gt * 128:(gt + 1) * 128, :], in_=OT[i]), reads=[bOT[i]], out=True)
    A.reset(m0)


KB.pass4 = _pass4
KB.pass5 = _pass5


NPRE_SB = 17
NFULL_SB = 16
_NC_CACHE = {}


def _build_full():
    if "nc" not in _NC_CACHE:
        kb = KB(NPRE_SB, NFULL_SB)
        kb.setup()
        kb.pass1()
        kb.pass2()
        kb.pass3()
        kb.pass4()
        kb.pass5()
        _NC_CACHE["nc"] = kb.finish()
    return _NC_CACHE["nc"]


def kernel(x, meta_tokens, hg_lb_logits, norm_mix_g, w_in, gd_conv_w, gd_A_log, gd_dt_bias, hg_norm_g, gd_norm_g,
           hg_up, gd_up, w_out, norm_ffn_g, router_group_w, router_group_b, router_expert_w, router_expert_b,
           w_gate, w_up, w_down, final_norm_g):
    f32 = np.float32
    c = lambda a: np.ascontiguousarray(np.asarray(a, dtype=f32))
    x = c(x)
    meta = c(meta_tokens)
    B, S, _ = x.shape
    half = S // 2
    npre_tok = NPRE_SB * SBT
    ntok = (NPRE_SB + NFULL_SB) * SBT
    nmeta = meta.shape[0]
    shared = {
        "w_in": c(w_in[0]),
        "norm_mix_g": c(norm_mix_g[0]),
        "hg_lb": c(np.asarray(hg_lb_logits, f32).reshape(2, 4, 128).transpose(2, 0, 1)),
        "hg_norm_g": c(hg_norm_g[0]),
        "conv_wT": c(np.asarray(gd_conv_w[0], f32).reshape(4, 12, 128).transpose(2, 0, 1)),
        "gd_A_log": c(gd_A_log[0]),
        "gd_dt_bias": c(gd_dt_bias[0]),
        "gd_norm_g": c(gd_norm_g[0]),
        "hg_up": c(hg_up[0]),
        "gd_up": c(gd_up[0]),
        "w_out": c(w_out[0]),
        "norm_ffn_g": c(norm_ffn_g[0]),
        "router_w": c(np.concatenate([np.asarray(router_group_w[0], f32), np.asarray(router_expert_w[0], f32)], axis=1)),
        "router_b": c(np.concatenate([np.asarray(router_group_b[0], f32), np.asarray(router_expert_b[0], f32)])),
        "w_gate": c(w_gate[0]),
        "w_up": c(w_up[0]),
        "w_down": c(w_down[0]),
        "final_norm_g": c(final_norm_g),
    }
    in_maps = []
    for core in range(2 * B):
        b, hf = core // 2, core % 2
        xs = np.zeros((ntok, D), f32)
        if hf == 0:
            xs[npre_tok - nmeta:npre_tok] = meta
            xs[npre_tok:] = x[b, 0:half]
        else:
            xs[npre_tok - half - nmeta:npre_tok - half] = meta
            xs[npre_tok - half:] = x[b]
        m = dict(shared)
        m["xs"] = xs
        in_maps.append(m)
    nc = _build_full()
    res = run_bass_kernel_spmd(nc, in_maps, core_ids=list(range(2 * B)))
    out = np.empty((B, S, D), f32)
    for core in range(2 * B):
        b, hf = core // 2, core % 2
        out[b, hf * half:(hf + 1) * half] = np.asarray(res.results[core]["out"], dtype=f32)
    return out

LCOST_TABLE = {('dve', 389): 0.041, ('act', 389): 0.03, ('pool', 389): 0.051, ('pool', 303): 0.019, ('pool', 425): 0.168, ('pool', 549): 0.65, ('pool', 305): 0.019, ('pool', 551): 0.63, ('pool', 625): 0.174, ('pool', 626): 0.155, ('dve', 305): 0.024, ('act', 303): 0.019, ('act', 563): 0.745, ('act', 305): 0.019, ('act', 482): 0.553, ('act', 489): 0.354, ('act', 492): 0.122, ('dve', 303): 0.024, ('dve', 498): 1.238, ('pe', 503): 0.126, ('dve', 506): 0.627, ('pe', 632): 0.143, ('act', 654): 0.553, ('dve', 685): 0.379, ('pe', 672): 0.23, ('dve', 689): 0.694, ('act', 676): 0.687, ('act', 692): 0.523, ('dve', 695): 0.588, ('act', 703): 0.508, ('act', 707): 0.1, ('dve', 708): 0.603, ('pe', 739): 0.135, ('dve', 742): 0.425, ('pe', 756): 0.146, ('dve', 760): 0.348, ('act', 766): 0.402, ('act', 660): 0.53, ('act', 665): 0.342, ('dve', 713): 0.867, ('dve', 717): 0.203, ('act', 719): 0.319, ('act', 722): 0.12, ('act', 725): 0.306, ('dve', 728): 0.599, ('dve', 731): 1.226, ('dve', 735): 0.601, ('pe', 776): 0.081, ('pe', 790): 0.101, ('dve', 779): 0.692, ('pe', 795): 0.197, ('dve', 799): 0.339, ('act', 804): 0.368, ('pe', 810): 0.102, ('act', 814): 0.604, ('act', 821): 0.334, ('pe', 825): 0.358, ('act', 827): 0.356, ('act', 829): 0.388, ('dve', 831): 0.401, ('dve', 833): 0.463, ('dve', 948): 0.286, ('pe', 1062): 0.15, ('pool', 1054): 0.157, ('pool', 1055): 0.154, ('pool', 1057): 0.265, ('act', 1074): 0.557, ('dve', 1076): 0.565, ('pe', 1103): 0.022, ('pe', 1131): 0.134, ('act', 1106): 0.163, ('act', 1109): 0.181, ('act', 1110): 0.181, ('dve', 1111): 0.148, ('act', 1113): 0.18, ('dve', 1114): 0.237, ('act', 1116): 0.208, ('act', 1117): 0.209, ('act', 1135): 0.42, ('dve', 1118): 0.204, ('pool', 1147): 0.588, ('act', 1138): 0.357, ('pe', 1150): 0.299, ('act', 1152): 0.504, ('act', 1154): 0.407, ('pool', 1157): 0.724, ('pe', 1284): 0.174, ('act', 1287): 0.251, ('pool', 1094): 0.145, ('dve', 1289): 0.161, ('act', 1291): 0.15, ('pool', 1306): 1.827, ('dve', 1293): 0.762, ('dve', 1295): 0.495, ('act', 1297): 0.174, ('act', 1298): 0.191, ('dve', 1299): 0.165, ('pe', 1311): 0.143, ('dve', 1318): 0.692, ('act', 1330): 0.356, ('pe', 1356): 0.07, ('pool', 1341): 1.285, ('dve', 1361): 0.635, ('pe', 1380): 0.124, ('pe', 1398): 0.111, ('pool', 1389): 1.143, ('pe', 1410): 0.106, ('pe', 1421): 0.116, ('dve', 1429): 0.68, ('pool', 1447): 1.831, ('pe', 1439): 0.11, ('dve', 1441): 0.685, ('pe', 1444): 0.134, ('act', 1446): 0.646, ('pool', 1448): 1.02, ('pe', 1489): 0.128, ('dve', 1499): 0.348, ('pe', 1505): 0.143, ('act', 1509): 0.683, ('pe', 1515): 0.14, ('dve', 1520): 0.275, ('act', 1525): 0.691, ('act', 1124): 0.542, ('dve', 1321): 0.597, ('act', 1323): 0.597, ('dve', 1349): 0.332, ('act', 1335): 0.335, ('pool', 1345): 1.268, ('pe', 1368): 0.146, ('dve', 1374): 0.666, ('pe', 1494): 0.063, ('pe', 1530): 0.121, ('act', 1533): 0.632, ('act', 1184): 0.319, ('pe', 1188): 0.358, ('act', 1190): 0.355, ('act', 1192): 0.361, ('dve', 1193): 0.57, ('dve', 1195): 0.7, ('pool', 1589): 0.642, ('pe', 1700): 0.127, ('pool', 1599): 0.638, ('act', 1703): 0.427, ('pe', 1715): 0.147, ('pe', 1719): 0.124, ('dve', 1722): 0.423, ('dve', 1724): 0.394, ('pool', 1726): 0.728, ('pe', 1737): 0.29, ('dve', 1740): 0.692, ('act', 1802): 0.574, ('act', 1803): 0.419, ('act', 1804): 0.203, ('dve', 1805): 1.285, ('pe', 1813): 0.224, ('pool', 1807): 3.579, ('act', 1816): 0.687, ('dve', 1818): 0.692, ('pe', 1823): 0.225, ('dve', 1825): 0.196, ('dve', 1828): 0.157, ('dve', 1829): 0.154, ('act', 1831): 0.316, ('dve', 1833): 0.164, ('dve', 1835): 0.229, ('dve', 1837): 0.192, ('dve', 1842): 0.194, ('dve', 1844): 0.16, ('act', 1845): 0.203, ('dve', 1846): 0.154, ('dve', 1848): 0.163, ('dve', 1849): 0.16, ('dve', 1851): 0.16, ('dve', 1853): 0.241, ('dve', 1855): 0.242, ('dve', 1857): 0.193, ('pe', 1861): 0.185, ('pe', 1862): 0.028, ('dve', 1864): 0.192, ('dve', 1865): 0.192, ('dve', 1866): 0.053, ('dve', 1868): 0.1, ('dve', 1870): 0.169, ('pool', 1876): 1.145, ('pool', 1925): 1.063, ('pool', 1927): 1.058, ('pool', 1929): 0.903, ('pe', 1942): 0.108, ('act', 1945): 1.115, ('dve', 1948): 0.692, ('pe', 1966): 0.165, ('act', 1974): 0.473, ('pe', 1970): 0.164, ('dve', 1975): 0.542, ('pe', 1986): 0.276, ('act', 1990): 0.682, ('dve', 1992): 0.692, ('pool', 2025): 1.115, ('pool', 2028): 1.228, ('dve', 2033): 1.284, ('pool', 289): 3.113, ('dve', 2036): 1.284, ('act', 2039): 0.574, ('act', 2041): 0.419, ('act', 2043): 0.203, ('dve', 2045): 1.284, ('act', 289): 0.115, ('dve', 289): 0.655}
Prog.LCOST = LCOST_TABLE
```

```python
import numpy as np
import concourse.bass as bass
import concourse.mybir as mybir
from concourse.bass_utils import run_bass_kernel_spmd

F32 = mybir.dt.float32
BF16 = mybir.dt.bfloat16
I32 = mybir.dt.int32
U32 = mybir.dt.uint32
AF = mybir.ActivationFunctionType
ALU = mybir.AluOpType
AX = mybir.AxisListType


class Buf:
    __slots__ = ("name", "lw", "rd", "psum")

    def __init__(self, name="", psum=False):
        self.name = name
        self.lw = None
        self.rd = {}
        self.psum = psum


class Prog:
    ENGS = ("pe", "act", "dve", "pool", "sp")

    def __init__(self, kdma=6):
        self.ops = {e: [] for e in self.ENGS}
        self.waited = {e: {} for e in self.ENGS}
        self.ndma = {e: 0 for e in self.ENGS}
        self.K = kdma
        self.out_toks = []
        self.pending = {e: [] for e in self.ENGS}

    def barrier(self):
        toks = []
        for e in self.ENGS:
            for i in range(len(self.ops[e]) - 1, -1, -1):
                op = self.ops[e][i]
                if (not op["dma"]) and op["fn"] is not None:
                    toks.append((e, i))
                    break
            n = self.ndma[e]
            for slot in range(min(self.K, n)):
                last = ((n - 1 - slot) // self.K) * self.K + slot
                toks.append((("dma", e, slot), 16 * (last // self.K + 1)))
        for e in self.ENGS:
            self.pending[e] = list(toks)

    cap = None
    COST = {"pe": 0.13, "act": 0.45, "dve": 0.42, "pool": 1.2, "sp": 0.05}
    DMA_LAT = 2.5
    LCOST = {}

    def _emit(self, eng, fn, reads, writes, dma=False, extra=()):
        if self.cap is not None:
            self.cap.append((eng, fn, tuple(reads), tuple(writes), dma, tuple(extra)))
            return None
        return self._emit_real(eng, fn, reads, writes, dma, extra)

    _act_tab = ""

    @staticmethod
    def _act_class(fn):
        nm = fn.__code__.co_names
        if "Silu" in nm or "Sigmoid" in nm:
            return "sig"
        if "Exp" in nm or "Ln" in nm:
            return "exp"
        return ""

    def _capture(self, g):
        self.cap = []
        for _ in g:
            pass
        ops = self.cap
        self.cap = None
        return ops

    def merge_streams(self, gens):
        segs = [[self._capture(g)] for g in gens]
        self.merge_pipeline(segs, [lambda s_, done: True] * len(segs))

    def merge_pipeline(self, segs, rules):
        n = len(segs)
        sp = [0] * n
        op = [0] * n
        done = [0] * n
        free = getattr(self, "_sim_free", None)
        if free is None:
            free = self._sim_free = {e: 0.0 for e in self.ENGS}
            self._sim_buf = {}
        bt = self._sim_buf
        rr = 0
        while True:
            best, bi = None, -1
            for k in range(n):
                i = (rr + k) % n
                while sp[i] < len(segs[i]) and op[i] >= len(segs[i][sp[i]]):
                    sp[i] += 1
                    op[i] = 0
                    done[i] = sp[i]
                if sp[i] >= len(segs[i]):
                    continue
                if op[i] == 0 and not rules[i](sp[i], done):
                    continue
                eng, fn, reads, writes, dma, extra = segs[i][sp[i]][op[i]]
                t = free[eng]
                for b in reads:
                    v = bt.get(id(b))
                    if v is not None and v[0] > t:
                        t = v[0]
                for b in writes:
                    v = bt.get(id(b))
                    if v is not None:
                        if v[0] > t:
                            t = v[0]
                        if v[1] > t:
                            t = v[1]
                if eng == "act" and fn is not None:
                    tc = self._act_class(fn)
                    if tc and tc != self._act_tab:
                        t += 0.4
                if best is None or t < best - 1e-9:
                    best, bi = t, i
            if bi < 0:
                if all(sp[i] >= len(segs[i]) for i in range(n)):
                    break
                progressed = False
                for i in range(n):
                    if sp[i] < len(segs[i]) and op[i] == 0 and rules[i](sp[i], done):
                        progressed = True
                assert progressed, ("pipeline gating deadlock", sp, done)
                continue
            rr = (bi + 1) % n
            eng, fn, reads, writes, dma, extra = segs[bi][sp[bi]][op[bi]]
            op[bi] += 1
            ln = fn.__code__.co_firstlineno if fn is not None else -1
            c = self.LCOST.get((eng, ln), self.COST[eng])
            if eng == "act" and fn is not None and not dma:
                tc = self._act_class(fn)
                if tc:
                    self._act_tab = tc
            if dma:
                occ = 0.6 if eng == "pool" else 0.05
                fin = best + occ + self.DMA_LAT
                free[eng] = best + occ
            else:
                fin = best + c
                free[eng] = fin
            for b in writes:
                bt[id(b)] = [fin, fin]
            for b in reads:
                v = bt.get(id(b))
                if v is None:
                    bt[id(b)] = [0.0, fin]
                elif v[1] < fin:
                    v[1] = fin
                if b.psum and bt[id(b)][0] < fin:
                    bt[id(b)][0] = fin
            tok = self._emit_real(eng, fn, reads, writes, dma, extra)
            if dma and getattr(fn, "_is_out", False):
                self.out_toks.append(tok)

    def _emit_real(self, eng, fn, reads, writes, dma=False, extra=()):
        deps = {}

        def need(tok):
            if tok is None:
                return
            k, v = tok
            if eng == "pe" and k == "pe":
                return
            if deps.get(k, -1) < v:
                deps[k] = v
        def need_x(tok):
            if tok is not None and tok[0] != eng:
                need(tok)
        for b in reads:
            if b.psum:
                need_x(b.lw)
            else:
                need(b.lw)
        for b in writes:
            if b.psum:
                need_x(b.lw)
            else:
                need(b.lw)
                for k, v in b.rd.items():
                    need((k, v))
        for t in extra:
            need(t)
        if self.pending[eng]:
            for t in self.pending[eng]:
                need(t)
            self.pending[eng] = []
        if dma:
            i = self.ndma[eng]
            self.ndma[eng] += 1
            slot = i % self.K
            val = 16 * (i // self.K + 1)
            key = ("dma", eng, slot)
            if val > 16:
                need((key, val - 16))
            tok = (key, val)
        else:
            tok = (eng, len(self.ops[eng]))
        waits = []
        w = self.waited[eng]
        for k, v in deps.items():
            if w.get(k, -1) < v:
                w[k] = v
                waits.append((k, v))
        self.ops[eng].append(dict(waits=waits, fn=fn, tok=tok, dma=dma))
        for b in reads:
            if b.psum:
                b.lw = tok
                continue
            k, v = tok
            if b.rd.get(k, -1) < v:
                b.rd[k] = v
        for b in writes:
            b.lw = tok
            b.rd = {}
        return tok

    pe_dummy = None
    _pe_cnt = 0

    def pe(self, fn, reads=(), writes=()):
        t = self._emit("pe", fn, reads, writes)
        if self.pe_dummy is not None:
            dfn, every, buf = self.pe_dummy
            self._pe_cnt += 1
            if self._pe_cnt % every == 0:
                self._emit("pe", dfn, (), (buf,))
        return t

    def act(self, fn, reads=(), writes=()):
        return self._emit("act", fn, reads, writes)

    def dve(self, fn, reads=(), writes=()):
        return self._emit("dve", fn, reads, writes)

    def pool(self, fn, reads=(), writes=()):
        return self._emit("pool", fn, reads, writes)

    def dma(self, fn, reads=(), writes=(), q="sp", out=False):
        if out and self.cap is not None:
            try:
                fn._is_out = True
            except AttributeError:
                pass
        t = self._emit(q, fn, reads, writes, dma=True)
        if out and t is not None:
            self.out_toks.append(t)
        return t

    def finalize(self, nc):
        self._emit("sp", None, (), (), extra=self.out_toks)
        targets = {e: set() for e in self.ENGS}
        for e in self.ENGS:
            for op in self.ops[e]:
                for k, v in op["waits"]:
                    if isinstance(k, str):
                        targets[k].add(v)
        rank = {e: {} for e in self.ENGS}
        for e in self.ENGS:
            r = 0
            for i, op in enumerate(self.ops[e]):
                if (not op["dma"]) and i in targets[e]:
                    assert op["fn"] is not None
                    r += 1
                    rank[e][i] = r
        import contextlib
        with contextlib.ExitStack() as st:
            csem = {e: st.enter_context(nc.semaphore("c_" + e)) for e in self.ENGS}
            dsem = {}
            for e in self.ENGS:
                if self.ndma[e] > 0:
                    for s in range(min(self.K, self.ndma[e])):
                        dsem[("dma", e, s)] = st.enter_context(nc.semaphore("d_%s_%d" % (e, s)))
            block = st.enter_context(nc.Block())

            def run(e):
                def body(engine):
                    for i, op in enumerate(self.ops[e]):
                        for k, v in op["waits"]:
                            if isinstance(k, str):
                                engine.wait_ge(csem[k], rank[k][v])
                            else:
                                engine.wait_ge(dsem[k], v)
                        if op["fn"] is None:
                            continue
                        ins = op["fn"](engine)
                        if op["dma"]:
                            ins.then_inc(dsem[op["tok"][0]], 16)
                        elif i in rank[e]:
                            ins.then_inc(csem[e], 1)
                return body
            block.tensor(run("pe"))
            block.scalar(run("act"))
            block.vector(run("dve"))
            block.gpsimd(run("pool"))
            block.sync(run("sp"))


D = 1024
KC = 8
SBT = 256
NT = SBT // 128
NCH = SBT // 64
EPS = 1e-6
C_HQ, C_HF, C_HI, C_HG = 0, 512, 1024, 1536
C_GQ, C_GK, C_GV, C_GZ = 2048, 2560, 3072, 3584
C_GB, C_GA, C_PA, C_PB = 4096, 4100, 4104, 5128
DPROJ = 6152


class Arena:
    def __init__(self, ap, words):
        self.ap = ap
        self.words = words
        self.off = 0
        self.peak = 0

    def mark(self):
        return self.off

    def reset(self, m):
        self.off = m

    def alloc(self, free_shape, dt):
        n = 1
        for s in free_shape:
            n *= s
        esz = 4 if dt in (F32, I32, U32) else 2
        words = (n * esz + 3) // 4
        words = (words + 7) // 8 * 8
        assert self.off + words <= self.words, ("arena overflow", self.off, words, self.words)
        v = self.ap[:, self.off:self.off + words]
        self.off += words
        self.peak = max(self.peak, self.off)
        if esz == 2:
            v = v.bitcast(dt)
        elif dt != F32:
            v = v.bitcast(dt)
        v = v[:, 0:n]
        if len(free_shape) > 1:
            names = ["a%d" % i for i in range(len(free_shape))]
            pat = "p (%s) -> p %s" % (" ".join(names), " ".join(names))
            v = v.rearrange(pat, **{nm: s for nm, s in zip(names, free_shape)})
        return v


import os as _os_ls
LS = int(_os_ls.environ.get("LISTSCHED", "2"))


class KB:
    def __init__(self, npre, nfull, debug=()):
        import contextlib
        self.npre, self.nfull = npre, nfull
        self.nsb = npre + nfull
        self.ntok = self.nsb * SBT
        self.nfull_tok = nfull * SBT
        self.debug = set(debug)
        self.nc = bass.Bass("TRN2", target_bir_lowering=False)
        self.P = Prog()
        self.d = {}
        self.st = contextlib.ExitStack()

    def din(self, name, shape, dt=F32):
        self.d[name] = self.nc.dram_tensor(name, list(shape), dt, kind="ExternalInput").ap()
        return self.d[name]

    def dout(self, name, shape, dt=F32):
        self.d[name] = self.nc.dram_tensor(name, list(shape), dt, kind="ExternalOutput").ap()
        return self.d[name]

    def setup(self):
        nc, P, st = self.nc, self.P, self.st
        self.din("xs", [self.ntok, D])
        self.din("w_in", [D, DPROJ])
        self.din("norm_mix_g", [D])
        self.din("hg_lb", [128, 2, 4])
        self.din("hg_norm_g", [128])
        AW = 51200
        arena_t = st.enter_context(nc.sbuf_tensor("arena", [128, AW], F32))
        self.A = Arena(arena_t[:], AW)
        self.banks = [st.enter_context(nc.psum_tensor("pb%d" % i, [128, 512], F32)) for i in range(8)]
        self.bbank = [Buf("pb%d" % i, psum=True) for i in range(8)]
        A = self.A
        self.identf = A.alloc((128,), F32)
        self.ident = A.alloc((128,), BF16)
        self.ones_bf = A.alloc((128,), BF16)
        self.ones_f = A.alloc((512,), F32)
        self.mask2 = A.alloc((128,), F32)
        self.bconst = Buf("const")
        bc = self.bconst
        P.pool(lambda e: e.memset(self.identf, 0.0), writes=[bc])
        P.pool(lambda e: e.affine_select(out=self.identf, in_=self.identf, pattern=[[-1, 128]],
                                         compare_op=ALU.not_equal, fill=1.0, base=0, channel_multiplier=1),
               reads=[bc], writes=[bc])
        P.pool(lambda e: e.tensor_copy(out=self.ident, in_=self.identf), reads=[bc], writes=[bc])
        P.pool(lambda e: e.memset(self.ones_bf, 1.0), writes=[bc])
        P.pool(lambda e: e.memset(self.ones_f, 1.0), writes=[bc])
        P.pool(lambda e: e.memset(self.mask2, 1.0), writes=[bc])
        P.pool(lambda e: e.affine_select(out=self.mask2, in_=self.mask2, pattern=[[1, 128]],
                                         compare_op=ALU.is_ge, fill=0.0, base=0, channel_multiplier=-1),
               reads=[bc], writes=[bc])
        P.pool(lambda e: e.memset(self.mask2[0:64, 64:128], 0.0), reads=[bc], writes=[bc])
        self.xt = [A.alloc((D,), F32) for _ in range(2)]
        self.bxt = [Buf("xt%d" % i) for i in range(2)]
        self.junk = A.alloc((D,), BF16)
        self.bjunk = Buf("junk")
        self.ss = A.alloc((NT,), F32)
        self.rstd = A.alloc((NT,), F32)
        self.bss = Buf("ss")
        self.brstd = Buf("rstd")
        self.xsb = [A.alloc((D,), BF16) for _ in range(2)]
        self.bxsb = [Buf("xsb%d" % i) for i in range(2)]
        self.gbc = A.alloc((D,), F32)
        self.bgbc = Buf("gbc")
        self.nxt = 0
        self.d["x_buf"] = nc.dram_tensor("x_buf", [NSLOT, D], BF16, kind="Internal").ap()
        zt = A.alloc((D,), BF16)
        self.bxzero = Buf("xzero")
        P.pool(lambda e: e.memset(zt, 0.0), writes=[bc])
        xbv = self.d["x_buf"].rearrange("(b p) n -> p b n", p=128)
        nblk = NSLOT // 128
        step = 12
        self.bxz = []
        for b0 in range(0, nblk, step):
            bz = Buf("xz")
            self.bxz.append(bz)
            P.dma(lambda e, b0=b0: e.dma_start(out=xbv[:, b0:b0 + step, :],
                                               in_=zt.unsqueeze(1).to_broadcast([128, step, D])),
                  reads=[bc], writes=[bz])

    def stage_a(self, *a, **k):
        for _ in self.stage_a_gen(*a, **k):
            pass

    def stage_a_gen(self, sb, xnT, bxnT, ptr, bptr, gname="norm_mix_g", src="xs", tok_base=0, keep=None):
        nc, P = self.nc, self.P
        xs_d = self.d[src]
        tiles = []
        for t in range(NT):
            i = self.nxt % 2
            self.nxt += 1
            tok0 = tok_base + sb * SBT + t * 128
            xt, bxt = self.xt[i], self.bxt[i]
            P.dma(lambda e, xt=xt, tok0=tok0: e.dma_start(out=xt, in_=xs_d[tok0:tok0 + 128, :]), writes=[bxt])
            P.act(lambda e, xt=xt, t=t: e.activation(out=self.junk, in_=xt, func=AF.Square,
                                                     accum_out=self.ss[:, t:t + 1]),
                  reads=[bxt], writes=[self.bss])
            tiles.append((xt, bxt, i))
            if t % 2 == 1:
                t0 = t - 1
                P.act(lambda e, t0=t0: e.activation(out=self.rstd[:, t0:t0 + 2], in_=self.ss[:, t0:t0 + 2],
                                                    func=AF.Ln, scale=1.0 / D, bias=EPS),
                      reads=[self.bss], writes=[self.brstd])
                P.act(lambda e, t0=t0: e.activation(out=self.rstd[:, t0:t0 + 2], in_=self.rstd[:, t0:t0 + 2],
                                                    func=AF.Exp, scale=-0.5),
                      reads=[self.brstd], writes=[self.brstd])
                for tt in (t0, t):
                    xt2, bxt2, i2 = tiles[tt]
                    xsb, bxsb = self.xsb[i2], self.bxsb[i2]
                    P.dve(lambda e, xt2=xt2, xsb=xsb, tt=tt: e.scalar_tensor_tensor(
                        out=xsb, in0=xt2, scalar=self.rstd[:, tt:tt + 1], in1=self.gbc,
                        op0=ALU.mult, op1=ALU.mult),
                        reads=[bxt2, self.brstd, self.bgbc], writes=[bxsb])
                    for k in range(KC):
                        P.pe(lambda e, k=k, xsb=xsb: e.transpose(out=ptr[:, k, :], in_=xsb[:, k * 128:(k + 1) * 128],
                                                                 identity=self.ident),
                             reads=[bxsb, self.bconst], writes=[bptr])
                    P.dve(lambda e, tt=tt: e.tensor_copy(out=xnT[:, :, tt * 128:(tt + 1) * 128], in_=ptr),
                          reads=[bptr], writes=[bxnT])
                    yield


    def xnt_fetch_gen(self, sb, last_sb, xnTs, bxnTs):
        P = self.P
        src = self.d["xnT_s"]
        if sb not in self._xfetched:
            self._xfetched.add(sb)
            P.dma(lambda e, sb=sb: e.dma_start(out=xnTs[sb % 2], in_=src[:, :, sb * SBT:(sb + 1) * SBT]),
                  reads=[self.bxns[sb]], writes=[bxnTs[sb % 2]])
        nx = sb + 1
        if nx <= last_sb and nx not in self._xfetched:
            self._xfetched.add(nx)
            P.dma(lambda e, nx=nx: e.dma_start(out=xnTs[nx % 2], in_=src[:, :, nx * SBT:(nx + 1) * SBT]),
                  reads=[self.bxns[nx]], writes=[bxnTs[nx % 2]])
        yield

    def load_gain(self, gname):
        P = self.P
        g = self.d[gname]
        P.dma(lambda e: e.dma_start(out=self.gbc, in_=g.partition_broadcast(128)), writes=[self.bgbc])

    def pass1(self):
        nc, P, A = self.nc, self.P, self.A
        d = self.d
        H = 4
        self.W2 = A.alloc((KC, 2056), BF16)
        self.bW2 = [Buf("W2_%d" % k) for k in range(KC)]
        m0 = A.mark()
        self.OA = A.alloc((H, SBT), BF16)
        self.bOA = [Buf("OA%d" % h) for h in range(H)]
        W1 = A.alloc((KC, 2048), BF16)
        bW1 = [Buf("W1_%d" % k) for k in range(KC)]
        wv = d["w_in"].rearrange("(k p) n -> p k n", p=128)
        for k in range(KC):
            P.dma(lambda e, k=k: e.dma_start(out=W1[:, k, :], in_=wv[:, k, 0:2048]), writes=[bW1[k]], q="pool")
        for k in range(KC):
            P.dma(lambda e, k=k: e.dma_start(out=self.W2[:, k, :], in_=wv[:, k, 2048:2048 + 2056]), writes=[self.bW2[k]], q="pool")
        self.load_gain("norm_mix_g")
        lraw = A.alloc((2, H), F32)
        lb = A.alloc((H,), F32)
        oml = A.alloc((H,), F32)
        hgn = A.alloc((1,), F32)
        blb = Buf("lb")
        P.dma(lambda e: e.dma_start(out=lraw, in_=d["hg_lb"]), writes=[blb])
        P.dma(lambda e: e.dma_start(out=hgn, in_=d["hg_norm_g"].rearrange("(p o) -> p o", o=1)), writes=[blb])
        P.dve(lambda e: e.tensor_tensor(out=lb, in0=lraw[:, 0, :], in1=lraw[:, 1, :], op=ALU.subtract),
              reads=[blb], writes=[blb])
        P.act(lambda e: e.activation(out=oml, in_=lb, func=AF.Sigmoid, scale=-1.0), reads=[blb], writes=[blb])
        P.act(lambda e: e.activation(out=lb, in_=lb, func=AF.Sigmoid), reads=[blb], writes=[blb])

        xnT = A.alloc((KC, SBT), BF16)
        bxnT = Buf("xnT")
        Fbs = [A.alloc((H, SBT), F32) for _ in range(2)]
        CS = A.alloc((H, SBT), F32)
        Kb = A.alloc((H, SBT), BF16)
        EB = A.alloc((H, SBT), BF16)
        ENB = A.alloc((H, SBT), BF16)
        EBEs = [A.alloc((H, NCH), F32) for _ in range(2)]
        QTs = [A.alloc((H, SBT), BF16) for _ in range(2)]
        KTs = [A.alloc((H, SBT), BF16) for _ in range(2)]
        KH = A.alloc((H, SBT), BF16)
        KHTs = [A.alloc((NT, 512), BF16) for _ in range(2)]
        Vs = [A.alloc((NT, 512), BF16) for _ in range(2)]
        Gs = [A.alloc((H, SBT), BF16) for _ in range(2)]
        O32 = A.alloc((H, SBT), F32)
        OSQ = A.alloc((H, SBT), BF16)
        LNV = A.alloc((SBT,), F32)
        ATS = A.alloc((H, 128), BF16)
        S32 = A.alloc((H, 128), F32)
        SBF = [A.alloc((H, 128), BF16) for _ in range(2)]
        bFs = [[Buf() for _ in range(H)] for _ in range(2)]
        bCS = [Buf() for _ in range(H)]
        bK = Buf()
        bEB = [Buf() for _ in range(H)]
        bENB = [Buf() for _ in range(H)]
        bEBEs = [Buf(), Buf()]
        bQTs = [[Buf() for _ in range(H)] for _ in range(2)]
        bKTs = [Buf(), Buf()]
        bKH = Buf()
        bKHTs = [[Buf() for _ in range(NT)] for _ in range(2)]
        bVs = [[Buf() for _ in range(NT)] for _ in range(2)]
        bGs = [[Buf() for _ in range(H)] for _ in range(2)]
        bO32 = Buf()
        bOSQ = [Buf() for _ in range(H)]
        bLNV = Buf()
        bATS = Buf()
        bS32 = [Buf() for _ in range(H)]
        bSBF = [[Buf() for _ in range(H)] for _ in range(2)]
        sbf_i = [0] * H

        bk = self.banks
        bb = self.bbank
        ptr = bk[0][:].bitcast(BF16).rearrange("p (k t) -> p k t", k=KC)
        pkt = bk[1][:].bitcast(BF16)[:, 0:512]
        pj = [bk[2][:], bk[3][:]]
        bpj = [bb[2], bb[3]]
        pat = bk[4][:].rearrange("p (h t) -> p h t", h=H)
        po = bk[5][:].rearrange("p (h t) -> p h t", h=H)
        pS = [bk[6 + (h % 2)][:, 0:128] for h in range(H)]
        bpS = [bb[6 + (h % 2)] for h in range(H)]
        pji = [0]

        def nextpj():
            i = pji[0] % 2
            pji[0] += 1
            return pj[i], bpj[i]

        for h in range(H):
            P.pool(lambda e, h=h: e.memset(S32[:, h, :], 0.0), writes=[bS32[h]])
            P.pool(lambda e, h=h: e.memset(SBF[0][:, h, :], 0.0), writes=[bSBF[0][h]])

        def proj_fm(col0, h):
            p, bp = nextpj()
            for k in range(KC):
                P.pe(lambda e, k=k, p=p: e.matmul(p[:, 0:SBT], lhsT=W1[:, k, col0 + h * 128:col0 + (h + 1) * 128],
                                                  rhs=xnT[:, k, :], start=(k == 0), stop=(k == KC - 1)),
                     reads=[bW1[k], bxnT], writes=[bp])
            return p, bp

        def X1a(sb):
            sl = sb % 2
            QT, KT, KHT, V, G, EBE = QTs[sl], KTs[sl], KHTs[sl], Vs[sl], Gs[sl], EBEs[sl]
            bQT, bKT, bKHT, bV, bG, bEBE = bQTs[sl], bKTs[sl], bKHTs[sl], bVs[sl], bGs[sl], bEBEs[sl]
            full = sb >= self.npre
            Fb, bF = Fbs[sb % 2], bFs[sb % 2]
            yield from self.stage_a_gen(sb, xnT, bxnT, ptr, bb[0])
            if "xnT_s" not in self.d:
                self.d["xnT_s"] = self.nc.dram_tensor("xnT_s", [128, KC, self.ntok], BF16, kind="Internal").ap()
                self.bxns = {}
            bx_ = Buf("xns")
            self.bxns[sb] = bx_
            P.dma(lambda e, sb=sb: e.dma_start(out=self.d["xnT_s"][:, :, sb * SBT:(sb + 1) * SBT], in_=xnT),
                  reads=[bxnT], writes=[bx_])
            for h in range(H):
                p, bp = proj_fm(C_HF, h)
                P.act(lambda e, h=h, p=p: e.activation(out=Fb[:, h, :], in_=p[:, 0:SBT], func=AF.Sigmoid),
                      reads=[bp], writes=[bF[h]])
                yield
            if full:
                for h in range(H):
                    p, bp = proj_fm(C_HQ, h)
                    P.act(lambda e, h=h, p=p: e.activation(out=QT[:, h, :], in_=p[:, 0:SBT], func=AF.Silu),
                          reads=[bp], writes=[bQT[h]])
                    yield
                for h in range(H):
                    p, bp = proj_fm(C_HG, h)
                    P.act(lambda e, h=h, p=p: e.activation(out=G[:, h, :], in_=p[:, 0:SBT], func=AF.Silu),
                          reads=[bp], writes=[bG[h]])
                    yield
            for t in range(NT):
                p, bp = nextpj()
                for k in range(KC):
                    P.pe(lambda e, k=k, p=p, t=t: e.matmul(p, lhsT=xnT[:, k, t * 128:(t + 1) * 128],
                                                           rhs=W1[:, k, C_HI:C_HI + 512],
                                                           start=(k == 0), stop=(k == KC - 1)),
                         reads=[bW1[k], bxnT], writes=[bp])
                P.act(lambda e, p=p, t=t: e.activation(out=V[:, t, :], in_=p, func=AF.Copy), reads=[bp], writes=[bV[t]])
                yield
        def X1b(sb):
            sl = sb % 2
            QT, KT, KHT, V, G, EBE = QTs[sl], KTs[sl], KHTs[sl], Vs[sl], Gs[sl], EBEs[sl]
            bQT, bKT, bKHT, bV, bG, bEBE = bQTs[sl], bKTs[sl], bKHTs[sl], bVs[sl], bGs[sl], bEBEs[sl]
            full = sb >= self.npre
            Fb, bF = Fbs[sb % 2], bFs[sb % 2]
            for h in range(H):
                P.dve(lambda e, h=h: e.tensor_scalar(out=Fb[:, h, :], in0=Fb[:, h, :], scalar1=oml[:, h:h + 1],
                                                     scalar2=lb[:, h:h + 1], op0=ALU.mult, op1=ALU.add),
                      reads=[bF[h], blb], writes=[bF[h]])
            P.dve(lambda e: e.tensor_scalar(out=Kb, in0=Fb, scalar1=-1.0, scalar2=1.0, op0=ALU.mult, op1=ALU.add),
                  reads=bF, writes=[bK])
            for h in range(H):
                P.act(lambda e, h=h: e.activation(out=Fb[:, h, :], in_=Fb[:, h, :], func=AF.Ln),
                      reads=[bF[h], bK], writes=[bF[h]])
            for h in range(H):
                P.dve(lambda e, h=h: e.tensor_tensor_scan(out=CS[:, h, :], data0=self.ones_f[:, 0:SBT],
                                                          data1=Fb[:, h, :], initial=0.0,
                                                          op0=ALU.mult, op1=ALU.add),
                      reads=[bF[h], self.bconst], writes=[bCS[h]])
                yield
            if not full:
                for h in range(H):
                    P.act(lambda e, h=h: e.activation(out=ENB[:, h, :], in_=CS[:, h, :], func=AF.Exp, scale=-1.0,
                                                      bias=CS[:, h, SBT - 1:SBT]),
                          reads=[bCS[h]], writes=[bENB[h]])
                    yield
                P.act(lambda e: e.activation(out=EBE[:, :, 0], in_=CS[:, :, SBT - 1], func=AF.Exp), reads=bCS, writes=[bEBE])
                P.dve(lambda e: e.tensor_tensor(out=KH, in0=Kb, in1=ENB, op=ALU.mult), reads=[bK] + bENB, writes=[bKH])
            else:
                Fb4 = Fb.rearrange("p h (c t) -> p h c t", c=NCH)
                CS4 = CS.rearrange("p h (c t) -> p h c t", c=NCH)
                P.dve(lambda e: e.tensor_tensor(out=Fb4[:, :, 1:NCH, :], in0=CS4[:, :, 1:NCH, :],
                                                in1=CS4[:, :, 0:NCH - 1, 63:64].to_broadcast([128, H, NCH - 1, 64]),
                                                op=ALU.subtract),
                      reads=bCS + bF, writes=bF)
                P.dve(lambda e: e.tensor_copy(out=Fb4[:, :, 0, :], in_=CS4[:, :, 0, :]), reads=bCS + bF, writes=bF)
                for h in range(H):
                    P.act(lambda e, h=h: e.activation(out=ENB[:, h, :], in_=Fb[:, h, :], func=AF.Exp, scale=-1.0),
                          reads=[bF[h]], writes=[bENB[h]])
                    yield
                P.act(lambda e: e.activation(out=EBE, in_=Fb4[:, :, :, 63], func=AF.Exp), reads=bF, writes=[bEBE])
                if full:
                    for h in range(H):
                        P.act(lambda e, h=h: e.activation(out=EB[:, h, :], in_=Fb[:, h, :], func=AF.Exp),
                              reads=[bF[h]], writes=[bEB[h]])
                P.dve(lambda e: e.tensor_tensor(out=KT, in0=Kb, in1=ENB, op=ALU.mult), reads=[bK] + bENB, writes=[bKT])
                KT4 = KT.rearrange("p h (c t) -> p h c t", c=NCH)
                KH4 = KH.rearrange("p h (c t) -> p h c t", c=NCH)
                P.dve(lambda e: e.tensor_tensor(out=KH4, in0=KT4,
                                                in1=EBE.unsqueeze(3).to_broadcast([128, H, NCH, 64]), op=ALU.mult),
                      reads=[bKT, bEBE], writes=[bKH])
                if full:
                    P.dve(lambda e: e.tensor_tensor(out=QT, in0=QT, in1=EB, op=ALU.mult), reads=bQT + bEB, writes=bQT)
            for t in range(NT):
                for h in range(H):
                    P.pe(lambda e, h=h, t=t: e.transpose(out=pkt[:, h * 128:(h + 1) * 128],
                                                         in_=KH[:, h, t * 128:(t + 1) * 128], identity=self.ident),
                         reads=[bKH, self.bconst], writes=[bb[1]])
                P.dve(lambda e, t=t: e.tensor_copy(out=KHT[:, t, :], in_=pkt), reads=[bb[1]], writes=[bKHT[t]])
                yield
        def X1(sb):
            yield from X1a(sb)
            yield from X1b(sb)

        def Y1(sb):
            full = sb >= self.npre
            sl = sb % 2
            QT, KT, KHT, V, G, EBE = QTs[sl], KTs[sl], KHTs[sl], Vs[sl], Gs[sl], EBEs[sl]
            bQT, bKT, bKHT, bV, bG, bEBE = bQTs[sl], bKTs[sl], bKHTs[sl], bVs[sl], bGs[sl], bEBEs[sl]
            if not full:
                for h in range(H):
                    for t in range(NT):
                        P.pe(lambda e, h=h, t=t: e.matmul(pS[h], lhsT=KHT[:, t, h * 128:(h + 1) * 128],
                                                          rhs=V[:, t, h * 128:(h + 1) * 128],
                                                          start=(t == 0), stop=(t == NT - 1)),
                             reads=[bKHT[t], bV[t]], writes=[bpS[h]])
                    P.dve(lambda e, h=h: e.scalar_tensor_tensor(
                        out=S32[:, h, :], in0=S32[:, h, :], scalar=EBE[:, h, 0:1], in1=pS[h],
                        op0=ALU.mult, op1=ALU.add),
                        reads=[bS32[h], bEBE, bpS[h]], writes=[bS32[h]])
                    cur = sbf_i[h]
                    nxt = 1 - cur
                    P.act(lambda e, h=h, nxt=nxt: e.activation(out=SBF[nxt][:, h, :], in_=S32[:, h, :], func=AF.Copy),
                          reads=[bS32[h]], writes=[bSBF[nxt][h]])
                    sbf_i[h] = nxt
                    yield
                return
            for j in range(NT):
                c0 = j * 128
                if full:
                    for h in range(H):
                        P.pe(lambda e, h=h, c0=c0: e.matmul(pat[:, h, :], lhsT=KT[:, h, c0:c0 + 128],
                                                            rhs=QT[:, h, c0:c0 + 128], start=True, stop=True),
                             reads=[bKT, bQT[h]], writes=[bb[4]])
                    P.dve(lambda e: e.tensor_tensor(out=ATS, in0=pat,
                                                    in1=self.mask2.unsqueeze(1).to_broadcast([128, H, 128]),
                                                    op=ALU.mult),
                          reads=[bb[4], self.bconst], writes=[bATS])
                    yield
                for half in range(2):
                    ch = 2 * j + half
                    r0 = half * 64
                    for h in range(H):
                        cur = sbf_i[h]
                        if full:
                            P.pe(lambda e, h=h, cur=cur, c0=c0, r0=r0: e.matmul(
                                po[:, h, r0:r0 + 64], lhsT=SBF[cur][:, h, :], rhs=QT[:, h, c0 + r0:c0 + r0 + 64],
                                start=(h == 0 and r0 == 0), stop=False, skip_group_check=True),
                                reads=[bSBF[cur][h], bQT[h]], writes=[bb[5]])
                        P.pe(lambda e, h=h, j=j, r0=r0: e.matmul(
                            pS[h], lhsT=KHT[r0:r0 + 64, j, h * 128:(h + 1) * 128],
                            rhs=V[r0:r0 + 64, j, h * 128:(h + 1) * 128], start=True, stop=True),
                            reads=[bKHT[j], bV[j]], writes=[bpS[h]])
                        P.dve(lambda e, h=h, ch=ch: e.scalar_tensor_tensor(
                            out=S32[:, h, :], in0=S32[:, h, :], scalar=EBE[:, h, ch:ch + 1], in1=pS[h],
                            op0=ALU.mult, op1=ALU.add),
                            reads=[bS32[h], bEBE, bpS[h]], writes=[bS32[h]])
                        nxt = 1 - cur
                        P.act(lambda e, h=h, nxt=nxt: e.activation(out=SBF[nxt][:, h, :], in_=S32[:, h, :], func=AF.Copy),
                              reads=[bS32[h]], writes=[bSBF[nxt][h]])
                        sbf_i[h] = nxt
                        yield
                if full:
                    for h in range(H):
                        P.pe(lambda e, h=h, j=j: e.matmul(po[:, h, :], lhsT=V[:, j, h * 128:(h + 1) * 128],
                                                          rhs=ATS[:, h, :], start=False, stop=True,
                                                          skip_group_check=True),
                             reads=[bV[j], bATS], writes=[bb[5]])
                    P.act(lambda e, c0=c0: e.activation(out=O32[:, :, c0:c0 + 128], in_=po, func=AF.Copy),
                          reads=[bb[5]], writes=[bO32])
                    yield
            if full:
                tok0 = (sb - self.npre) * SBT
                for h in range(H):
                    P.act(lambda e, h=h: e.activation(out=OSQ[:, h, :], in_=O32[:, h, :], func=AF.Square),
                          reads=[bO32], writes=[bOSQ[h]])
                for h in range(H):
                    pss, bpss = bk[4][:], bb[4]
                    P.pe(lambda e, h=h, pss=pss: e.matmul(pss[:, 0:SBT], lhsT=self.ones_bf, rhs=OSQ[:, h, :], start=True, stop=True),
                         reads=[bOSQ[h], self.bconst], writes=[bpss])
                    P.act(lambda e, pss=pss: e.activation(out=LNV, in_=pss[:, 0:SBT], func=AF.Ln, scale=1.0 / 128, bias=EPS),
                          reads=[bpss], writes=[bLNV])
                    P.act(lambda e: e.activation(out=LNV, in_=LNV, func=AF.Exp, scale=-0.5),
                          reads=[bLNV], writes=[bLNV])
                    P.dve(lambda e, h=h: e.tensor_tensor(out=O32[:, h, :], in0=O32[:, h, :], in1=LNV, op=ALU.mult),
                          reads=[bO32, bLNV], writes=[bO32])
                    P.dve(lambda e, h=h: e.scalar_tensor_tensor(
                        out=self.OA[:, h, :], in0=O32[:, h, :], scalar=hgn[:, 0:1], in1=G[:, h, :],
                        op0=ALU.mult, op1=ALU.mult),
                        reads=[bO32, blb, bG[h]], writes=[self.bOA[h]])
                    yield
                self.spill("oa_s", self.OA, self.bOA, sb)
                if "oa" in self.debug:
                    if "dbg_oa" not in self.d:
                        self.dout("dbg_oa", [128, H, self.nfull_tok], BF16)
                    P.dma(lambda e, tok0=tok0: e.dma_start(out=self.d["dbg_oa"][:, :, tok0:tok0 + SBT], in_=self.OA),
                          reads=self.bOA, out=True)
        import os
        WTS1 = [int(v) for v in os.environ.get("IL_W1", "1,1").split(",")]

        def run_il(gens):
            gens = list(gens)
            while gens:
                for g_, w_ in list(gens):
                    for _ in range(w_):
                        try:
                            next(g_)
                        except StopIteration:
                            gens.remove((g_, w_))
                            break

        n = self.nsb
        if LS >= 2:
            sa = [P._capture(X1a(i)) for i in range(n)]
            sb_ = [P._capture(X1b(i)) for i in range(n)]
            sy = [P._capture(Y1(i)) for i in range(n)]
            P.merge_pipeline([sa, sb_, sy],
                             [lambda s_, dn: dn[2] >= s_ - 1,
                              lambda s_, dn: dn[0] >= s_ + 1 and dn[2] >= s_ - 1,
                              lambda s_, dn: dn[1] >= s_ + 1])
        else:
            for r in range(-1, n):
                gs = []
                if 0 <= r < n:
                    gs.append((Y1(r), WTS1[0]))
                if 0 <= r + 1 < n:
                    gs.append((X1(r + 1), WTS1[1]))
                if LS:
                    P.merge_streams([g_ for g_, _w in gs])
                else:
                    run_il(gs)
        A.reset(m0)
        P.barrier()

    def finish(self):
        self.P.finalize(self.nc)
        self.st.close()
        return self.nc


def _pass2(self):
    nc, P, A = self.nc, self.P, self.A
    d = self.d
    H = 4
    self.din("conv_wT", [128, 4, 12])
    self.din("gd_A_log", [4])
    self.din("gd_dt_bias", [4])
    self.din("gd_norm_g", [128])
    m0 = A.mark()
    self.OB = A.alloc((H, SBT), BF16)
    self.bOB = [Buf("OB%d" % h) for h in range(H)]
    NW = 2056
    if hasattr(self, "W2"):
        W2, bW2 = self.W2, self.bW2
    else:
        W2 = A.alloc((KC, NW), BF16)
        bW2 = [Buf("W2_%d" % k) for k in range(KC)]
        wv = d["w_in"].rearrange("(k p) n -> p k n", p=128)
        for k in range(KC):
            P.dma(lambda e, k=k: e.dma_start(out=W2[:, k, :], in_=wv[:, k, 2048:2048 + NW]), writes=[bW2[k]], q="pool")
    self.load_gain("norm_mix_g")
    cw = A.alloc((4, 12), F32)
    negA = A.alloc((H,), F32)
    dtb = A.alloc((H,), F32)
    gdn = A.alloc((1,), F32)
    bpar = Buf("par2")
    P.dma(lambda e: e.dma_start(out=cw, in_=d["conv_wT"]), writes=[bpar])
    P.dma(lambda e: e.dma_start(out=negA, in_=d["gd_A_log"].partition_broadcast(128)), writes=[bpar])
    P.dma(lambda e: e.dma_start(out=dtb, in_=d["gd_dt_bias"].partition_broadcast(128)), writes=[bpar])
    P.dma(lambda e: e.dma_start(out=gdn, in_=d["gd_norm_g"].rearrange("(p o) -> p o", o=1)), writes=[bpar])
    P.act(lambda e: e.activation(out=negA, in_=negA, func=AF.Exp), reads=[bpar], writes=[bpar])
    P.dve(lambda e: e.tensor_scalar(out=negA, in0=negA, scalar1=-1.0, scalar2=None, op0=ALU.mult),
          reads=[bpar], writes=[bpar])
    maskL = A.alloc((128,), F32)
    ch01 = A.alloc((2, 128), F32)
    bc = self.bconst
    P.pool(lambda e: e.memset(maskL, 1.0), writes=[bc])
    P.pool(lambda e: e.affine_select(out=maskL, in_=maskL, pattern=[[-1, 128]], compare_op=ALU.is_gt,
                                     fill=0.0, base=0, channel_multiplier=1), reads=[bc], writes=[bc])
    P.pool(lambda e: e.memset(maskL[64:128, 0:64], 0.0), reads=[bc], writes=[bc])
    bones = A.alloc((128,), F32)
    P.pool(lambda e: e.memset(bones, 0.0), writes=[bc])
    P.pool(lambda e: e.memset(bones[0:64, 0:64], 1.0), reads=[bc], writes=[bc])
    P.pool(lambda e: e.memset(bones[64:128, 64:128], 1.0), reads=[bc], writes=[bc])
    P.pool(lambda e: e.memset(ch01, 0.0), writes=[bc])
    P.pool(lambda e: e.memset(ch01[0:64, 0, :], 1.0), reads=[bc], writes=[bc])
    P.pool(lambda e: e.memset(ch01[64:128, 1, :], 1.0), reads=[bc], writes=[bc])

    xnTs = [A.alloc((KC, SBT), BF16) for _ in range(2)]
    bxnTs = [Buf("xnT0"), Buf("xnT1")]
    self._xfetched = set()
    use_fetch = hasattr(self, "bxns")
    XC = A.alloc((12, SBT + 3), BF16)
    DG = A.alloc((48, 128), BF16)
    bDG = Buf("DG")
    for _j in range(4):
        for _cb in range(12):
            P.dve(lambda e, _j=_j, _cb=_cb: e.tensor_scalar(out=DG[:, _j * 12 + _cb, :], in0=self.identf,
                                                           scalar1=cw[:, _j, _cb:_cb + 1], scalar2=None, op0=ALU.mult),
                  reads=[bpar, self.bconst], writes=[bDG])
    bXC = [Buf() for _ in range(12)]
    CV = [A.alloc((SBT,), F32) for _ in range(2)]
    bCV = [Buf(), Buf()]
    QK32 = A.alloc((8, SBT), F32)
    bQK32 = [Buf() for _ in range(8)]
    NQ = 4
    SQs = [A.alloc((SBT,), BF16) for _ in range(NQ)]
    bSQs = [Buf() for _ in range(NQ)]
    RSs = [A.alloc((SBT,), F32) for _ in range(NQ)]
    bRSs = [Buf() for _ in range(NQ)]
    sqi = [0]
    NS = 3
    QTs = [A.alloc((H, SBT), BF16) for _ in range(NS)]
    KTs = [A.alloc((H, SBT), BF16) for _ in range(NS)]
    VTs = [A.alloc((H, SBT), BF16) for _ in range(NS)]
    GZs = [A.alloc((H, SBT), BF16) for _ in range(NS)]
    bQTs = [[Buf() for _ in range(H)] for _ in range(NS)]
    bKTs = [[Buf() for _ in range(H)] for _ in range(NS)]
    bVTs = [[Buf() for _ in range(H)] for _ in range(NS)]
    bGZs = [[Buf() for _ in range(H)] for _ in range(NS)]
    BGraws = [A.alloc((NT, 8), F32) for _ in range(NS)]
    LNBs = [A.alloc((NT, H), F32) for _ in range(NS)]
    BETAs = [A.alloc((NT, H), F32) for _ in range(NS)]
    GGs = [A.alloc((NT, H), F32) for _ in range(NS)]
    bBGs = [Buf() for _ in range(NS)]
    PBUF = []
    for _j in range(NT):
        pb = dict(
            GB=A.alloc((H, 128), F32), bGB=Buf(),
            E1=A.alloc((H, 128), F32), bE1=Buf(),
            E2=A.alloc((H, 128), F32), bE2=Buf(),
            EGR=A.alloc((H, 128), BF16), bEGR=Buf(),
            Lm=[A.alloc((H, 128), BF16) for _ in range(2)], bLm=[Buf(), Buf()],
            Um=[A.alloc((H, 128), BF16) for _ in range(2)], bUm=[Buf(), Buf()],
            Xm=[A.alloc((H, 128), BF16) for _ in range(2)], bXm=[Buf(), Buf()],
            VTK=A.alloc((H, 128), BF16), bVTK=Buf(),
        )
        PBUF.append(pb)
    PCAR = []
    for _s in range(2):
        row = []
        for _j in range(NT):
            row.append(dict(
                SC=A.alloc((8, H), F32), bSC=Buf(),
                QTG=A.alloc((H, 128), BF16), bQTG=Buf(),
                TT=A.alloc((H, 128), BF16), bTT=Buf(),
                ATT=A.alloc((H, 128), BF16), bATT=Buf(),
                KHT=A.alloc((H, 128), BF16), bKHT=Buf(),
                KTP=A.alloc((H, 128), BF16), bKTP=Buf(),
                BV=A.alloc((H, 128), F32), bBV=Buf(),
            ))
        PCAR.append(row)
    R = A.alloc((H, 128), BF16)
    USB = A.alloc((H, 128), BF16)
    bR = Buf()
    bUSB = Buf()
    S32 = A.alloc((H, 128), F32)
    SBF = [A.alloc((H, 128), BF16) for _ in range(2)]
    bS32 = [Buf() for _ in range(H)]
    bSBF = [[Buf() for _ in range(H)] for _ in range(2)]
    sbf_i = [0]
    O32 = A.alloc((H, SBT), F32)
    bO32 = Buf()
    OSQ = A.alloc((H, SBT), BF16)
    bOSQ = [Buf() for _ in range(H)]
    LNV = A.alloc((SBT,), F32)
    bLNV = Buf()
    identb4 = A.alloc((H, 128), BF16)
    P.pool(lambda e: e.tensor_copy(out=identb4, in_=self.ident.unsqueeze(1).to_broadcast([128, H, 128])),
           reads=[bc], writes=[bc])

    bk, bb = self.banks, self.bbank
    ptr = bk[0][:].bitcast(BF16).rearrange("p (k t) -> p k t", k=KC)
    ptr4 = bk[0][:].bitcast(BF16)[:, 0:512].rearrange("p (h t) -> p h t", h=H)
    import os as _os
    DUM = int(_os.environ.get("PE_DUMMY", "0"))
    DUMN = int(_os.environ.get("PE_DUMMY_N", "128"))
    if DUM > 0:
        pj = [bk[1][:], bk[2][:]]
        bpj = [bb[1], bb[2]]
        dones = A.alloc((512,), BF16)
        P.pool(lambda e: e.memset(dones, 1.0), writes=[self.bconst])
        dbuf = Buf("dummy", psum=True)
        P.pe_dummy = (lambda e: e.matmul(bk[0][:, 0:DUMN], lhsT=self.ones_bf, rhs=dones[:, 0:DUMN], start=True, stop=True),
                      DUM, dbuf)
    else:
        pj = [bk[1][:], bk[2][:], bk[0][:]]
        bpj = [bb[1], bb[2], bb[0]]
    pA = bk[3][:].rearrange("p (h t) -> p h t", h=H)
    pB = bk[4][:].rearrange("p (h t) -> p h t", h=H)
    pBb = bk[4][:].bitcast(BF16)[:, 0:512].rearrange("p (h t) -> p h t", h=H)
    pC = bk[5][:].rearrange("p (h t) -> p h t", h=H)
    pR = bk[6][:].rearrange("p (h t) -> p h t", h=H)
    po = bk[7][:].rearrange("p (h t) -> p h t", h=H)
    pji = [0]

    def nextpj():
        i = pji[0] % len(pj)
        pji[0] += 1
        return pj[i], bpj[i]

    for h in range(H):
        P.pool(lambda e, h=h: e.memset(S32[:, h, :], 0.0), writes=[bS32[h]])
        P.pool(lambda e, h=h: e.memset(SBF[0][:, h, :], 0.0), writes=[bSBF[0][h]])
    for cb in range(12):
        P.pool(lambda e, cb=cb: e.memset(XC[:, cb, :], 0.0), writes=[bXC[cb]])

    def proj_fm(col0, xnT, bxnT):
        p, bp = nextpj()
        for k in range(KC):
            P.pe(lambda e, k=k, p=p: e.matmul(p[:, 0:SBT], lhsT=W2[:, k, col0:col0 + 128],
                                              rhs=xnT[:, k, :], start=(k == 0), stop=(k == KC - 1)),
                 reads=[bW2[k], bxnT], writes=[bp])
        return p, bp

    evi = [0]

    def evac(out, in_, reads, writes):
        i = evi[0]
        evi[0] += 1
        if i % 3 != 2:
            P.act(lambda e: e.activation(out=out, in_=in_, func=AF.Copy), reads=reads, writes=writes)
        else:
            P.dve(lambda e: e.tensor_copy(out=out, in_=in_), reads=reads, writes=writes)

    def X(sb):
        sl = sb % 3
        QT, KT, VT, GZ = QTs[sl], KTs[sl], VTs[sl], GZs[sl]
        bQT, bKT, bVT, bGZ = bQTs[sl], bKTs[sl], bVTs[sl], bGZs[sl]
        BGraw, LNB, BETA, GG, bBG = BGraws[sl], LNBs[sl], BETAs[sl], GGs[sl], bBGs[sl]
        full = sb >= self.npre
        xnT, bxnT = xnTs[sb % 2], bxnTs[sb % 2]
        if use_fetch:
            yield from self.xnt_fetch_gen(sb, self.nsb - 1, xnTs, bxnTs)
        else:
            yield from self.stage_a_gen(sb, xnT, bxnT, ptr, bb[0])
        cbs = list(range(12)) if full else list(range(4, 12))
        cbs_proj = list(range(12)) if sb >= self.npre - 1 else list(range(4, 12))
        for cb in cbs_proj:
            if sb > 0:
                P.pool(lambda e, cb=cb: e.tensor_copy(out=XC[:, cb, 0:3], in_=XC[:, cb, SBT:SBT + 3]),
                       reads=[bXC[cb]], writes=[bXC[cb]])
            p, bp = proj_fm(cb * 128, xnT, bxnT)
            evac(XC[:, cb, 3:SBT + 3], p[:, 0:SBT], [bp], [bXC[cb]])
            yield
        pbg, bpbg = nextpj()
        for t in range(NT):
            for k in range(KC):
                P.pe(lambda e, k=k, t=t, pbg=pbg: e.matmul(pbg[:, t * 8:(t + 1) * 8], lhsT=xnT[:, k, t * 128:(t + 1) * 128],
                                                  rhs=W2[:, k, 2048:2056], start=(k == 0), stop=(k == KC - 1)),
                     reads=[bW2[k], bxnT], writes=[bpbg])
        P.act(lambda e, pbg=pbg: e.activation(out=BGraw, in_=pbg[:, 0:NT * 8].rearrange("p (t c) -> p t c", t=NT), func=AF.Copy),
              reads=[bpbg], writes=[bBG])
        P.act(lambda e: e.activation(out=LNB, in_=BGraw[:, :, 0:4], func=AF.Exp, scale=-1.0), reads=[bBG], writes=[bBG])
        P.act(lambda e: e.activation(out=LNB, in_=LNB, func=AF.Ln, bias=1.0), reads=[bBG], writes=[bBG])
        P.dve(lambda e: e.tensor_scalar(out=LNB, in0=LNB, scalar1=-1.0, scalar2=None, op0=ALU.mult),
              reads=[bBG], writes=[bBG])
        P.act(lambda e: e.activation(out=BETA, in_=LNB, func=AF.Exp), reads=[bBG], writes=[bBG])
        P.dve(lambda e: e.tensor_tensor(out=GG, in0=BGraw[:, :, 4:8], in1=dtb.unsqueeze(1).to_broadcast([128, NT, H]),
                                        op=ALU.add), reads=[bBG, bpar], writes=[bBG])
        P.act(lambda e: e.activation(out=GG, in_=GG, func=AF.Exp), reads=[bBG], writes=[bBG])
        P.act(lambda e: e.activation(out=GG, in_=GG, func=AF.Ln, bias=1.0), reads=[bBG], writes=[bBG])
        P.dve(lambda e: e.tensor_tensor(out=GG, in0=GG, in1=negA.unsqueeze(1).to_broadcast([128, NT, H]),
                                        op=ALU.mult), reads=[bBG, bpar], writes=[bBG])
        yield
        if full:
            for h in range(H):
                p, bp = proj_fm(1536 + h * 128, xnT, bxnT)
                P.act(lambda e, h=h, p=p: e.activation(out=GZ[:, h, :], in_=p[:, 0:SBT], func=AF.Silu),
                      reads=[bp], writes=[bGZ[h]])
                yield
        for n, cb in enumerate(cbs):
            cv, bcv = nextpj()
            for j in range(4):
                P.pe(lambda e, cb=cb, cv=cv, j=j: e.matmul(cv[:, 0:SBT], lhsT=DG[:, j * 12 + cb, :], rhs=XC[:, cb, j:SBT + j],
                                                           start=(j == 0), stop=(j == 3)),
                     reads=[bXC[cb], bDG], writes=[bcv])
            if cb < 8:
                P.act(lambda e, cb=cb, cv=cv: e.activation(out=QK32[:, cb, :], in_=cv[:, 0:SBT], func=AF.Silu),
                      reads=[bcv], writes=[bQK32[cb]])
            else:
                P.act(lambda e, cb=cb, cv=cv: e.activation(out=VT[:, cb - 8, :], in_=cv[:, 0:SBT], func=AF.Silu),
                      reads=[bcv], writes=[bVT[cb - 8]])
            yield
        for cb in cbs:
            if cb >= 8:
                continue
            SQ, bSQ, RS, bRS = SQs[sqi[0] % NQ], bSQs[sqi[0] % NQ], RSs[sqi[0] % NQ], bRSs[sqi[0] % NQ]
            sqi[0] += 1
            P.pool(lambda e, cb=cb, SQ=SQ: e.tensor_tensor(out=SQ, in0=QK32[:, cb, :], in1=QK32[:, cb, :], op=ALU.mult),
                   reads=[bQK32[cb]], writes=[bSQ])
            p, bp = nextpj()
            P.pe(lambda e, p=p, SQ=SQ: e.matmul(p[:, 0:SBT], lhsT=self.ones_bf, rhs=SQ, start=True, stop=True),
                 reads=[bSQ, bc], writes=[bp])
            P.act(lambda e, p=p, RS=RS: e.activation(out=RS, in_=p[:, 0:SBT], func=AF.Ln, bias=EPS), reads=[bp], writes=[bRS])
            qbias = -0.5 * float(np.log(128.0)) if cb < 4 else 0.0
            P.act(lambda e, qbias=qbias, RS=RS: e.activation(out=RS, in_=RS, func=AF.Exp, scale=-0.5, bias=qbias),
                  reads=[bRS], writes=[bRS])
            dst, bdst = (QT[:, cb, :], bQT[cb]) if cb < 4 else (KT[:, cb - 4, :], bKT[cb - 4])
            P.pool(lambda e, cb=cb, dst=dst, RS=RS: e.tensor_tensor(out=dst, in0=QK32[:, cb, :], in1=RS, op=ALU.mult),
                   reads=[bQK32[cb], bRS], writes=[bdst])
            yield
    def Yp(sb):
        sl = sb % 3
        full = sb >= self.npre
        QT, KT, VT, GZ = QTs[sl], KTs[sl], VTs[sl], GZs[sl]
        bQT, bKT, bVT, bGZ = bQTs[sl], bKTs[sl], bVTs[sl], bGZs[sl]
        BGraw, LNB, BETA, GG, bBG = BGraws[sl], LNBs[sl], BETAs[sl], GGs[sl], bBGs[sl]
        LL = dict(L0)
        LL['PB'] = [dict(PBUF[j], **PCAR[sb % 2][j]) for j in range(NT)]
        LL.update(QT=QT, KT=KT, VT=VT, GZ=GZ, bQT=bQT, bKT=bKT, bVT=bVT, bGZ=bGZ, LNB=LNB, BETA=BETA, GG=GG, bBG=bBG)
        yield from self._gdn_prep(LL, full)
    def Yr(sb):
        sl = sb % 3
        full = sb >= self.npre
        QT, KT, VT, GZ = QTs[sl], KTs[sl], VTs[sl], GZs[sl]
        bQT, bKT, bVT, bGZ = bQTs[sl], bKTs[sl], bVTs[sl], bGZs[sl]
        BGraw, LNB, BETA, GG, bBG = BGraws[sl], LNBs[sl], BETAs[sl], GGs[sl], bBGs[sl]
        LL = dict(L0)
        LL['PB'] = [dict(PBUF[j], **PCAR[sb % 2][j]) for j in range(NT)]
        LL.update(QT=QT, KT=KT, VT=VT, GZ=GZ, bQT=bQT, bKT=bKT, bVT=bVT, bGZ=bGZ, LNB=LNB, BETA=BETA, GG=GG, bBG=bBG)
        yield from self._gdn_rec(LL, full)
        if full:
            tok0 = (sb - self.npre) * SBT
            for h in range(H):
                P.act(lambda e, h=h: e.activation(out=OSQ[:, h, :], in_=O32[:, h, :], func=AF.Square),
                      reads=[bO32], writes=[bOSQ[h]])
            for h in range(H):
                pss, bpss = bk[5][:], bb[5]
                P.pe(lambda e, h=h, pss=pss: e.matmul(pss[:, 0:SBT], lhsT=self.ones_bf, rhs=OSQ[:, h, :], start=True, stop=True),
                     reads=[bOSQ[h], bc], writes=[bpss])
                P.act(lambda e, pss=pss: e.activation(out=LNV, in_=pss[:, 0:SBT], func=AF.Ln, scale=1.0 / 128, bias=EPS),
                      reads=[bpss], writes=[bLNV])
                P.act(lambda e: e.activation(out=LNV, in_=LNV, func=AF.Exp, scale=-0.5), reads=[bLNV], writes=[bLNV])
                P.dve(lambda e, h=h: e.tensor_tensor(out=O32[:, h, :], in0=O32[:, h, :], in1=LNV, op=ALU.mult),
                      reads=[bO32, bLNV], writes=[bO32])
                P.dve(lambda e, h=h: e.scalar_tensor_tensor(
                    out=self.OB[:, h, :], in0=O32[:, h, :], scalar=gdn[:, 0:1], in1=GZ[:, h, :],
                    op0=ALU.mult, op1=ALU.mult), reads=[bO32, bpar, bGZ[h]], writes=[self.bOB[h]])
                yield
            self.spill("ob_s", self.OB, self.bOB, sb)
            if "ob" in self.debug:
                if "dbg_ob" not in self.d:
                    self.dout("dbg_ob", [128, H, self.nfull_tok], BF16)
                P.dma(lambda e, tok0=tok0: e.dma_start(out=self.d["dbg_ob"][:, :, tok0:tok0 + SBT], in_=self.OB),
                      reads=self.bOB, out=True)
    L0 = dict(locals())

    import os
    WTS = [int(v) for v in os.environ.get("IL_W", "1,2,2").split(",")]

    def run_il(gens):
        gens = list(gens)
        while gens:
            for g_, w_ in list(gens):
                for _ in range(w_):
                    try:
                        next(g_)
                    except StopIteration:
                        gens.remove((g_, w_))
                        break

    n = self.nsb
    if LS >= 2:
        sx = [P._capture(X(i)) for i in range(n)]
        sp_ = [P._capture(Yp(i)) for i in range(n)]
        sr = [P._capture(Yr(i)) for i in range(n)]
        P.merge_pipeline([sx, sp_, sr],
                         [lambda s_, dn: dn[2] >= s_ - 2,
                          lambda s_, dn: dn[0] >= s_ + 1 and dn[2] >= s_ - 1,
                          lambda s_, dn: dn[1] >= s_ + 1])
    else:
        for r in range(-2, n):
            gs = []
            if 0 <= r < n:
                gs.append((Yr(r), WTS[0]))
            if 0 <= r + 1 < n:
                gs.append((Yp(r + 1), WTS[1]))
            if 0 <= r + 2 < n:
                gs.append((X(r + 2), WTS[2]))
            if LS:
                P.merge_streams([g_ for g_, _w in gs])
            else:
                run_il(gs)
    P.pe_dummy = None
    A.reset(m0)
    P.barrier()


KB.pass2 = _pass2


def _gdn_prep(self, L, full):
    P = self.P
    H = 4
    g = lambda n: L[n]
    bk, bb = self.banks, self.bbank
    bc = self.bconst
    mask2, ident = self.mask2, self.ident
    maskL, bones, ch01, identb4 = g("maskL"), g("bones"), g("ch01"), g("identb4")
    PBUF, GG, LNB, BETA, bBG = g("PB"), g("GG"), g("LNB"), g("BETA"), g("bBG")
    QT, KT, VT, bQT, bKT, bVT = g("QT"), g("KT"), g("VT"), g("bQT"), g("bKT"), g("bVT")
    R, USB, bR, bUSB = g("R"), g("USB"), g("bR"), g("bUSB")
    S32, SBF, bS32, bSBF, sbf_i = g("S32"), g("SBF"), g("bS32"), g("bSBF"), g("sbf_i")
    O32, bO32 = g("O32"), g("bO32")
    evac, nextpj = g("evac"), g("nextpj")
    NP = NT
    pP = [bk[3 + j][:].rearrange("p (h t) -> p h t", h=H) for j in range(NP)]
    pPb = [bk[3 + j][:].bitcast(BF16)[:, 0:512].rearrange("p (h t) -> p h t", h=H) for j in range(NP)]
    bpP = [bb[3 + j] for j in range(NP)]
    ptr4 = bk[5][:].bitcast(BF16)[:, 0:512].rearrange("p (h t) -> p h t", h=H)
    pKS = bk[5][:].rearrange("p (h t) -> p h t", h=H)
    pU = bk[6][:].rearrange("p (h t) -> p h t", h=H)
    po = bk[7][:].rearrange("p (h t) -> p h t", h=H)

    def bc4(ap):
        return ap.unsqueeze(2).to_broadcast([128, H, 128])

    for j in range(NP):
        pb = PBUF[j]
        SC, bSC = pb["SC"], pb["bSC"]
        ps = bk[3 + j][:, 0:16]
        gj = GG[:, j, :]
        for n, lhs in enumerate((mask2, bones, ch01[:, 0, :], ch01[:, 1, :])):
            P.pe(lambda e, n=n, lhs=lhs, ps=ps, gj=gj: e.matmul(ps[:, 4 * n:4 * n + 4], lhsT=lhs, rhs=gj,
                                                                 start=True, stop=True),
                 reads=[bBG, bc], writes=[bpP[j]])
        P.act(lambda e, SC=SC, ps=ps: e.activation(out=SC[:, 0, :], in_=ps[:, 0:4], func=AF.Copy),
              reads=[bpP[j]], writes=[bSC])
        P.dve(lambda e, SC=SC, ps=ps: e.tensor_tensor(out=SC[:, 5, :], in0=ps[:, 4:8], in1=SC[:, 0, :], op=ALU.subtract),
              reads=[bpP[j], bSC], writes=[bSC])
        P.act(lambda e, SC=SC, ps=ps: e.activation(out=SC[:, 6:8, :], in_=ps[:, 8:16].rearrange("p (a h) -> p a h", a=2),
                                                  func=AF.Exp), reads=[bpP[j]], writes=[bSC])
        P.dve(lambda e, SC=SC: e.tensor_scalar(out=SC[:, 1, :], in0=SC[:, 0, :], scalar1=-1.0, scalar2=None, op0=ALU.mult),
              reads=[bSC], writes=[bSC])
        P.dve(lambda e, SC=SC, j=j: e.tensor_tensor(out=SC[:, 2, :], in0=SC[:, 0, :], in1=LNB[:, j, :], op=ALU.add),
              reads=[bSC, bBG], writes=[bSC])
        P.act(lambda e, SC=SC: e.activation(out=SC[:, 3, :], in_=SC[:, 0, :], func=AF.Exp), reads=[bSC], writes=[bSC])
        P.act(lambda e, SC=SC: e.activation(out=SC[:, 5, :], in_=SC[:, 5, :], func=AF.Exp), reads=[bSC], writes=[bSC])
        P.dve(lambda e, SC=SC, j=j: e.scalar_tensor_tensor(out=SC[:, 4, :], in0=SC[:, 3, :], scalar=-1.0,
                                                          in1=BETA[:, j, :], op0=ALU.mult, op1=ALU.mult),
              reads=[bSC, bBG], writes=[bSC])
        yield
    for j in range(NP):
        pb = PBUF[j]
        P.pool(lambda e, pb=pb, j=j: e.tensor_copy(out=pb["GB"], in_=bc4(GG[:, j, :])), reads=[bBG], writes=[pb["bGB"]])
        yield
    for j in range(NP):
        pb = PBUF[j]
        for h in range(H):
            P.pe(lambda e, pb=pb, j=j, h=h: e.matmul(pP[j][:, h, :], lhsT=pb["GB"][:, h, :], rhs=mask2,
                                                     start=True, stop=True),
                 reads=[pb["bGB"], bc], writes=[bpP[j]])
        yield
    for j in range(NP):
        pb = PBUF[j]
        SC = pb["SC"]
        P.dve(lambda e, pb=pb, j=j, SC=SC: e.tensor_tensor(out=pb["E1"], in0=pP[j], in1=bc4(SC[:, 0, :]), op=ALU.max),
              reads=[bpP[j], pb["bSC"]], writes=[pb["bE1"]])
        if full:
            P.dve(lambda e, pb=pb, j=j, SC=SC: e.tensor_tensor(out=pb["E2"], in0=pP[j], in1=bc4(SC[:, 0, :]), op=ALU.min),
                  reads=[bpP[j], pb["bSC"]], writes=[pb["bE2"]])
            P.act(lambda e, pb=pb, j=j: e.activation(out=pb["EGR"], in_=pP[j], func=AF.Exp),
                  reads=[bpP[j]], writes=[pb["bEGR"]])
        yield
    for j in range(NP):
        pb = PBUF[j]
        SC = pb["SC"]
        for h in range(H):
            P.act(lambda e, pb=pb, h=h, SC=SC: e.activation(out=pb["E1"][:, h, :], in_=pb["E1"][:, h, :], func=AF.Exp,
                                                            scale=-1.0, bias=SC[:, 2, h:h + 1]),
                  reads=[pb["bE1"], pb["bSC"]], writes=[pb["bE1"]])
        if full:
            for h in range(H):
                P.act(lambda e, pb=pb, h=h, SC=SC: e.activation(out=pb["E2"][:, h, :], in_=pb["E2"][:, h, :], func=AF.Exp,
                                                                bias=SC[:, 1, h:h + 1]),
                      reads=[pb["bE2"], pb["bSC"]], writes=[pb["bE2"]])
        yield
    for j in range(NP):
        pb = PBUF[j]
        P.pool(lambda e, pb=pb: e.tensor_tensor(out=pb["E1"], in0=pb["E1"],
                                                in1=maskL.unsqueeze(1).to_broadcast([128, H, 128]), op=ALU.mult),
               reads=[pb["bE1"], bc], writes=[pb["bE1"]])
        if full:
            P.pool(lambda e, pb=pb: e.tensor_tensor(out=pb["E2"], in0=pb["E2"],
                                                    in1=mask2.unsqueeze(1).to_broadcast([128, H, 128]), op=ALU.mult),
                   reads=[pb["bE2"], bc], writes=[pb["bE2"]])
            c0 = j * 128
            P.dve(lambda e, pb=pb, c0=c0: e.tensor_tensor(out=pb["QTG"], in0=QT[:, :, c0:c0 + 128], in1=pb["EGR"], op=ALU.mult),
                  reads=bQT + [pb["bEGR"]], writes=[pb["bQTG"]])
        yield
    for j in range(NP):
        c0 = j * 128
        for h in range(H):
            P.pe(lambda e, j=j, h=h, c0=c0: e.matmul(pP[j][:, h, :], lhsT=KT[:, h, c0:c0 + 128], rhs=KT[:, h, c0:c0 + 128],
                                                     start=True, stop=True), reads=[bKT[h]], writes=[bpP[j]])
        yield
    for j in range(NP):
        pb = PBUF[j]
        P.dve(lambda e, pb=pb, j=j: e.tensor_tensor(out=pb["Lm"][0], in0=pP[j], in1=pb["E1"], op=ALU.mult),
              reads=[bpP[j], pb["bE1"]], writes=[pb["bLm"][0]])
        yield
    if full:
        for j in range(NP):
            c0 = j * 128
            for h in range(H):
                P.pe(lambda e, j=j, h=h, c0=c0: e.matmul(pP[j][:, h, :], lhsT=KT[:, h, c0:c0 + 128],
                                                         rhs=QT[:, h, c0:c0 + 128], start=True, stop=True),
                     reads=[bKT[h], bQT[h]], writes=[bpP[j]])
            yield
        for j in range(NP):
            pb = PBUF[j]
            P.dve(lambda e, pb=pb, j=j: e.tensor_tensor(out=pb["ATT"], in0=pP[j], in1=pb["E2"], op=ALU.mult),
                  reads=[bpP[j], pb["bE2"]], writes=[pb["bATT"]])
            yield
    for j in range(NP):
        pb = PBUF[j]
        for h in range(H):
            P.pe(lambda e, pb=pb, j=j, h=h: e.transpose(out=pPb[j][:, h, :], in_=pb["Lm"][0][:, h, :], identity=ident),
                 reads=[pb["bLm"][0], bc], writes=[bpP[j]])
        yield
    for j in range(NP):
        pb = PBUF[j]
        evac(pb["Um"][0], pPb[j], [bpP[j]], [pb["bUm"][0]])
        yield
    for j in range(NP):
        pb = PBUF[j]
        P.pool(lambda e, pb=pb: e.tensor_tensor(out=pb["Xm"][0], in0=identb4, in1=pb["Um"][0], op=ALU.subtract),
               reads=[pb["bUm"][0], bc], writes=[pb["bXm"][0]])
        yield
    cur, cx = 0, 0
    for lvl in range(5):
        for j in range(NP):
            pb = PBUF[j]
            for h in range(H):
                P.pe(lambda e, pb=pb, j=j, h=h, cur=cur: e.matmul(pP[j][:, h, :], lhsT=pb["Um"][cur][:, h, :],
                                                                  rhs=pb["Lm"][cur][:, h, :], start=True, stop=True),
                     reads=[pb["bUm"][cur], pb["bLm"][cur]], writes=[bpP[j]])
            yield
        for j in range(NP):
            pb = PBUF[j]
            evac(pb["Lm"][1 - cur], pP[j], [bpP[j]], [pb["bLm"][1 - cur]])
            yield
        if lvl < 4:
            for j in range(NP):
                pb = PBUF[j]
                for h in range(H):
                    P.pe(lambda e, pb=pb, j=j, h=h, cur=cur: e.matmul(pP[j][:, h, :], lhsT=pb["Lm"][cur][:, h, :],
                                                                      rhs=pb["Um"][cur][:, h, :], start=True, stop=True),
                         reads=[pb["bUm"][cur], pb["bLm"][cur]], writes=[bpP[j]])
                yield
            for j in range(NP):
                pb = PBUF[j]
                evac(pb["Um"][1 - cur], pP[j], [bpP[j]], [pb["bUm"][1 - cur]])
                yield
        for j in range(NP):
            pb = PBUF[j]
            for h in range(H):
                P.pe(lambda e, pb=pb, j=j, h=h, cur=cur, cx=cx: e.matmul(pP[j][:, h, :], lhsT=pb["Lm"][1 - cur][:, h, :],
                                                                         rhs=pb["Xm"][cx][:, h, :], start=True, stop=True),
                     reads=[pb["bLm"][1 - cur], pb["bXm"][cx]], writes=[bpP[j]])
            yield
        for j in range(NP):
            pb = PBUF[j]
            xo, bxo = (pb["TT"], pb["bTT"]) if lvl == 4 else (pb["Xm"][1 - cx], pb["bXm"][1 - cx])
            P.dve(lambda e, pb=pb, j=j, cx=cx, xo=xo: e.tensor_tensor(out=xo, in0=pP[j], in1=pb["Xm"][cx], op=ALU.add),
                  reads=[bpP[j], pb["bXm"][cx]], writes=[bxo])
            yield
        cur, cx = 1 - cur, 1 - cx
    for j in range(NP):
        pb = PBUF[j]
        c0 = j * 128
        SC = pb["SC"]
        for h in range(H):
            P.pe(lambda e, h=h, c0=c0, j=j: e.transpose(out=pPb[j][:, h, :], in_=KT[:, h, c0:c0 + 128], identity=ident),
                 reads=[bKT[h], bc], writes=[bpP[j]])
        P.dve(lambda e, pb=pb, SC=SC, j=j: e.tensor_tensor(out=pb["KHT"], in0=pPb[j], in1=bc4(SC[:, 5, :]), op=ALU.mult),
              reads=[bpP[j], pb["bSC"]], writes=[pb["bKHT"]])
        for h in range(H):
            P.pe(lambda e, h=h, c0=c0, j=j: e.transpose(out=pPb[j][:, h, :], in_=VT[:, h, c0:c0 + 128], identity=ident),
                 reads=[bVT[h], bc], writes=[bpP[j]])
        P.act(lambda e, pb=pb, j=j: e.activation(out=pb["VTK"], in_=pPb[j], func=AF.Copy), reads=[bpP[j]], writes=[pb["bVTK"]])
        P.pool(lambda e, pb=pb, c0=c0: e.tensor_copy(out=pb["KTP"], in_=KT[:, :, c0:c0 + 128]), reads=bKT, writes=[pb["bKTP"]])
        P.pool(lambda e, pb=pb, j=j: e.tensor_tensor(out=pb["BV"], in0=pb["VTK"], in1=bc4(BETA[:, j, :]), op=ALU.mult),
               reads=[pb["bVTK"], bBG], writes=[pb["bBV"]])
        yield


def _gdn_rec(self, L, full):
    P = self.P
    H = 4
    g = lambda n: L[n]
    bk, bb = self.banks, self.bbank
    bc = self.bconst
    mask2, ident = self.mask2, self.ident
    maskL, bones, ch01, identb4 = g("maskL"), g("bones"), g("ch01"), g("identb4")
    PBUF, GG, LNB, BETA, bBG = g("PB"), g("GG"), g("LNB"), g("BETA"), g("bBG")
    QT, KT, VT, bQT, bKT, bVT = g("QT"), g("KT"), g("VT"), g("bQT"), g("bKT"), g("bVT")
    R, USB, bR, bUSB = g("R"), g("USB"), g("bR"), g("bUSB")
    S32, SBF, bS32, bSBF, sbf_i = g("S32"), g("SBF"), g("bS32"), g("bSBF"), g("sbf_i")
    O32, bO32 = g("O32"), g("bO32")
    evac, nextpj = g("evac"), g("nextpj")
    NP = NT
    pP = [bk[3 + j][:].rearrange("p (h t) -> p h t", h=H) for j in range(NP)]
    pPb = [bk[3 + j][:].bitcast(BF16)[:, 0:512].rearrange("p (h t) -> p h t", h=H) for j in range(NP)]
    bpP = [bb[3 + j] for j in range(NP)]
    ptr4 = bk[5][:].bitcast(BF16)[:, 0:512].rearrange("p (h t) -> p h t", h=H)
    pKS = bk[5][:].rearrange("p (h t) -> p h t", h=H)
    pU = bk[6][:].rearrange("p (h t) -> p h t", h=H)
    po = bk[7][:].rearrange("p (h t) -> p h t", h=H)

    def bc4(ap):
        return ap.unsqueeze(2).to_broadcast([128, H, 128])

    for j in range(NP):
        pb = PBUF[j]
        c0 = j * 128
        SC = pb["SC"]
        TT, bTT = pb["TT"], pb["bTT"]
        for half in range(2):
            r0 = half * 64
            cs = sbf_i[0]
            for h in range(H):
                P.pe(lambda e, h=h, pb=pb, cs=cs: e.matmul(pKS[:, h, :], lhsT=pb["KTP"][:, h, :], rhs=SBF[cs][:, h, :],
                                                           start=True, stop=True),
                     reads=[pb["bKTP"], bSBF[cs][h]], writes=[bb[5]])
            if full:
                for h in range(H):
                    P.pe(lambda e, pb=pb, h=h, cs=cs, r0=r0, half=half: e.matmul(
                        po[:, h, r0:r0 + 64], lhsT=SBF[cs][:, h, :], rhs=pb["QTG"][:, h, r0:r0 + 64],
                        start=(h == 0 and half == 0), stop=False, skip_group_check=True),
                        reads=[bSBF[cs][h], pb["bQTG"]], writes=[bb[7]])
            for h in range(H):
                P.dve(lambda e, pb=pb, h=h, r0=r0, SC=SC: e.scalar_tensor_tensor(
                    out=R[r0:r0 + 64, h, :], in0=pKS[r0:r0 + 64, h, :], scalar=SC[r0:r0 + 64, 4, h:h + 1],
                    in1=pb["BV"][r0:r0 + 64, h, :], op0=ALU.mult, op1=ALU.add),
                    reads=[bb[5], pb["bSC"], pb["bBV"]], writes=[bR])
            yield
            for h in range(H):
                P.pe(lambda e, h=h, r0=r0, TT=TT: e.matmul(pU[:, h, :], lhsT=TT[r0:r0 + 64, h, :], rhs=R[r0:r0 + 64, h, :],
                                                           start=True, stop=True),
                     reads=[bTT, bR], writes=[bb[6]])
            yield
            P.act(lambda e, r0=r0: e.activation(out=USB[r0:r0 + 64], in_=pU[r0:r0 + 64], func=AF.Copy),
                  reads=[bb[6]], writes=[bUSB])
            yield
            pS, bpS = bk[6][:], bb[6]
            pS4 = pS.rearrange("p (h t) -> p h t", h=H)
            for h in range(H):
                P.pe(lambda e, pb=pb, h=h, r0=r0, pS4=pS4: e.matmul(pS4[:, h, :], lhsT=pb["KHT"][r0:r0 + 64, h, :],
                                                                    rhs=USB[r0:r0 + 64, h, :], start=True, stop=True),
                     reads=[pb["bKHT"], bUSB], writes=[bpS])
            yield
            for h in range(H):
                P.dve(lambda e, h=h, half=half, SC=SC, pS4=pS4: e.scalar_tensor_tensor(
                    out=S32[:, h, :], in0=S32[:, h, :], scalar=SC[:, 6 + half, h:h + 1], in1=pS4[:, h, :],
                    op0=ALU.mult, op1=ALU.add), reads=[bS32[h], pb["bSC"], bpS], writes=[bS32[h]])
            yield
            nx = 1 - cs
            P.act(lambda e, nx=nx: e.activation(out=SBF[nx], in_=S32, func=AF.Copy), reads=bS32, writes=bSBF[nx])
            sbf_i[0] = nx
            yield
        if full:
            for h in range(H):
                P.pe(lambda e, pb=pb, h=h: e.matmul(po[:, h, :], lhsT=USB[:, h, :], rhs=pb["ATT"][:, h, :],
                                                    start=False, stop=True, skip_group_check=True),
                     reads=[bUSB, pb["bATT"]], writes=[bb[7]])
            P.act(lambda e, c0=c0: e.activation(out=O32[:, :, c0:c0 + 128], in_=po, func=AF.Copy),
                  reads=[bb[7]], writes=[bO32])


KB._gdn_prep = _gdn_prep
KB._gdn_rec = _gdn_rec


CAP = 384
NEXP = 32
NSLOT = NEXP * CAP
BIG = 1.0e4


def _spill(self, name, sb_ap, bufs, sb):
    P = self.P
    if name not in self.d:
        self.d[name] = self.nc.dram_tensor(name, [128, 4, self.nfull_tok], BF16, kind="Internal").ap()
        self.bspill = getattr(self, "bspill", {})
        self.bspill[name] = {}
    dst = self.d[name]
    tok0 = (sb - self.npre) * SBT
    b = Buf(name)
    self.bspill[name][sb - self.npre] = b
    P.dma(lambda e: e.dma_start(out=dst[:, :, tok0:tok0 + SBT], in_=sb_ap), reads=bufs, writes=[b])


KB.spill = _spill


def _pass3(self):
    nc, P, A = self.nc, self.P, self.A
    d = self.d
    H = 4
    for nm, shp in (("hg_up", [512, D]), ("gd_up", [512, D]), ("w_out", [D, D]), ("norm_ffn_g", [D]),
                    ("router_w", [D, 36]), ("router_b", [36])):
        self.din(nm, shp)
    ntile = self.nfull_tok // 128
    d["h2_s"] = nc.dram_tensor("h2_s", [self.nfull_tok, D], F32, kind="Internal").ap()
    self.bh2s = [Buf("h2_s%d" % i) for i in range(ntile)]
    self.bxbuf = []
    self.W12 = A.alloc((ntile, 2), F32)
    self.DST = A.alloc((ntile, 2), U32)
    self.bW12 = Buf("W12")
    self.bDST = Buf("DST")
    m0 = A.mark()
    W3 = A.alloc((KC, 2048), BF16)
    bW3 = [Buf() for _ in range(KC)]
    wv = d["w_in"].rearrange("(k p) n -> p k n", p=128)
    for k in range(KC):
        P.dma(lambda e, k=k: e.dma_start(out=W3[:, k, :], in_=wv[:, k, C_PA:C_PA + 2048]), writes=[bW3[k]], q="pool")
    HGUP = A.alloc((H, D), BF16)
    GDUP = A.alloc((H, D), BF16)
    WOUT = A.alloc((KC, D), BF16)
    bUP = Buf()
    bWO = [Buf() for _ in range(KC)]
    P.dma(lambda e: e.dma_start(out=HGUP, in_=d["hg_up"].rearrange("(h p) n -> p h n", p=128)), writes=[bUP], q="pool")
    P.dma(lambda e: e.dma_start(out=GDUP, in_=d["gd_up"].rearrange("(h p) n -> p h n", p=128)), writes=[bUP], q="pool")
    wo = d["w_out"].rearrange("(k p) n -> p k n", p=128)
    for k in range(KC):
        P.dma(lambda e, k=k: e.dma_start(out=WOUT[:, k, :], in_=wo[:, k, :]), writes=[bWO[k]], q="pool")
    WR = A.alloc((KC, 36), F32)
    RB = A.alloc((36,), F32)
    G2 = A.alloc((D,), F32)
    ECAP = A.alloc((NEXP,), F32)
    bpar = Buf("par3")
    P.dma(lambda e: e.dma_start(out=WR, in_=d["router_w"].rearrange("(k p) n -> p k n", p=128)), writes=[bpar])
    P.dma(lambda e: e.dma_start(out=RB, in_=d["router_b"].partition_broadcast(128)), writes=[bpar])
    P.dma(lambda e: e.dma_start(out=G2, in_=d["norm_ffn_g"].partition_broadcast(128)), writes=[bpar])
    self.load_gain("norm_mix_g")
    ecapi = A.alloc((NEXP,), I32)
    P.pool(lambda e: e.iota(ecapi, pattern=[[CAP, NEXP]], base=0, channel_multiplier=0), writes=[bpar])
    P.pool(lambda e: e.tensor_copy(out=ECAP, in_=ecapi), reads=[bpar], writes=[bpar])
    triS = A.alloc((128,), BF16)
    trif = A.alloc((128,), F32)
    bc = self.bconst
    P.pool(lambda e: e.memset(trif, 1.0), writes=[bc])
    P.pool(lambda e: e.affine_select(out=trif, in_=trif, pattern=[[1, 128]], compare_op=ALU.is_gt, fill=0.0,
                                     base=0, channel_multiplier=-1), reads=[bc], writes=[bc])
    P.pool(lambda e: e.tensor_copy(out=triS, in_=trif), reads=[bc], writes=[bc])
    BASE = A.alloc((NEXP,), F32)
    bBASE = Buf()
    P.pool(lambda e: e.tensor_copy(out=BASE, in_=ecapi), reads=[bpar], writes=[bBASE])

    xnTs = [A.alloc((KC, SBT), BF16) for _ in range(2)]
    bxnTs = [Buf(), Buf()]
    self._xfetched = set()
    use_fetch = hasattr(self, "bxns")
    SGs = [A.alloc((16, SBT), BF16) for _ in range(2)]
    bSGs = [[Buf() for _ in range(16)] for _ in range(2)]
    OAss = [A.alloc((H, SBT), BF16) for _ in range(2)]
    OBss = [A.alloc((H, SBT), BF16) for _ in range(2)]
    bOAss, bOBss = [Buf(), Buf()], [Buf(), Buf()]
    T1 = [A.alloc((SBT,), F32) for _ in range(2)]
    T2 = [A.alloc((SBT,), F32) for _ in range(2)]
    bT1 = [Buf(), Buf()]
    bT2 = [Buf(), Buf()]
    MGs = [A.alloc((KC, SBT), BF16) for _ in range(2)]
    bMGs = [[Buf() for _ in range(KC)] for _ in range(2)]
    XR = A.alloc((D,), F32)
    bXR = Buf()
    H2 = A.alloc((D,), F32)
    bH2 = Buf()
    JK = A.alloc((D,), BF16)
    SS2 = A.alloc((1,), F32)
    bSS2 = Buf()
    XF = A.alloc((D,), F32)
    XB = A.alloc((D,), BF16)
    bXF, bXB = Buf(), Buf()
    XFT = A.alloc((KC, 128), F32)
    bXFT = Buf()
    LG = A.alloc((36,), F32)
    ME = A.alloc((NEXP,), F32)
    SM = A.alloc((16,), F32)
    M8 = A.alloc((8,), F32)
    SEL1 = A.alloc((NEXP,), F32)
    SEL2 = A.alloc((NEXP,), F32)
    SELB = A.alloc((NEXP,), BF16)
    RK = A.alloc((NEXP,), F32)
    JK2 = A.alloc((NEXP,), F32)
    DF = A.alloc((2,), F32)
    brt = Buf("route")

    bk, bb = self.banks, self.bbank
    ptr = bk[0][:].bitcast(BF16).rearrange("p (k t) -> p k t", k=KC)
    pj = [bk[1][:], bk[2][:]]
    bpj = [bb[1], bb[2]]
    pup = [bk[3][:], bk[4][:]]
    pji = [0]

    def nextpj():
        i = pji[0] % 2
        pji[0] += 1
        return pj[i], bpj[i]

    pyi = [0]

    def nextpy():
        i = 5 + pyi[0] % 2
        pyi[0] += 1
        return bk[i][:], bb[i]

    oa_d, ob_d = d["oa_s"], d["ob_s"]
    def X3(sbi):
        sl = sbi % 2
        SG, bSG, OAs, OBs, bOAs, bOBs = SGs[sl], bSGs[sl], OAss[sl], OBss[sl], bOAss[sl], bOBss[sl]
        sb = self.npre + sbi
        tok0 = sbi * SBT
        xnT, bxnT = xnTs[sb % 2], bxnTs[sb % 2]
        if use_fetch:
            yield from self.xnt_fetch_gen(sb, self.nsb - 1, xnTs, bxnTs)
        else:
            yield from self.stage_a_gen(sb, xnT, bxnT, ptr, bb[0])
        P.dma(lambda e, tok0=tok0: e.dma_start(out=OAs, in_=oa_d[:, :, tok0:tok0 + SBT]),
              reads=[self.bspill["oa_s"][sbi]], writes=[bOAs])
        P.dma(lambda e, tok0=tok0: e.dma_start(out=OBs, in_=ob_d[:, :, tok0:tok0 + SBT]),
              reads=[self.bspill["ob_s"][sbi]], writes=[bOBs])
        for cb in range(16):
            p, bp = nextpj()
            for k in range(KC):
                P.pe(lambda e, k=k, p=p, cb=cb: e.matmul(p[:, 0:SBT], lhsT=W3[:, k, cb * 128:(cb + 1) * 128],
                                                         rhs=xnT[:, k, :], start=(k == 0), stop=(k == KC - 1)),
                     reads=[bW3[k], bxnT], writes=[bp])
            P.act(lambda e, cb=cb, p=p: e.activation(out=SG[:, cb, :], in_=p[:, 0:SBT], func=AF.Sigmoid),
                  reads=[bp], writes=[bSG[cb]])
            yield
    def Y3a(sbi):
        sl = sbi % 2
        SG, bSG, OAs, OBs, bOAs, bOBs = SGs[sl], bSGs[sl], OAss[sl], OBss[sl], bOAss[sl], bOBss[sl]
        sb = self.npre + sbi
        tok0 = sbi * SBT
        MG, bMG = MGs[sbi % 2], bMGs[sbi % 2]
        for cb in range(KC):
            i = cb % 2
            for h in range(H):
                P.pe(lambda e, h=h, cb=cb: e.matmul(pup[0][:, 0:SBT], lhsT=HGUP[:, h, cb * 128:(cb + 1) * 128],
                                                    rhs=OAs[:, h, :], start=(h == 0), stop=(h == H - 1)),
                     reads=[bUP, bOAs], writes=[bb[3]])
            for h in range(H):
                P.pe(lambda e, h=h, cb=cb: e.matmul(pup[1][:, 0:SBT], lhsT=GDUP[:, h, cb * 128:(cb + 1) * 128],
                                                    rhs=OBs[:, h, :], start=(h == 0), stop=(h == H - 1)),
                     reads=[bUP, bOBs], writes=[bb[4]])
            P.dve(lambda e, cb=cb, i=i: e.tensor_tensor(out=T1[i], in0=pup[0][:, 0:SBT], in1=SG[:, cb, :], op=ALU.mult),
                  reads=[bb[3], bSG[cb]], writes=[bT1[i]])
            P.dve(lambda e, cb=cb, i=i: e.tensor_tensor(out=T2[i], in0=pup[1][:, 0:SBT], in1=SG[:, 8 + cb, :], op=ALU.mult),
                  reads=[bb[4], bSG[8 + cb]], writes=[bT2[i]])
            P.pool(lambda e, cb=cb, i=i: e.tensor_tensor(out=MG[:, cb, :], in0=T1[i], in1=T2[i], op=ALU.add),
                   reads=[bT1[i], bT2[i]], writes=[bMG[cb]])
            yield
    def Y3b(sbi):
        sl = sbi % 2
        SG, bSG, OAs, OBs, bOAs, bOBs = SGs[sl], bSGs[sl], OAss[sl], OBss[sl], bOAss[sl], bOBss[sl]
        sb = self.npre + sbi
        tok0 = sbi * SBT
        MG, bMG = MGs[sbi % 2], bMGs[sbi % 2]
        for t in range(NT):
            gt = sbi * NT + t
            gtok = self.npre * SBT + gt * 128
            P.dma(lambda e, gtok=gtok: e.dma_start(out=XR, in_=d["xs"][gtok:gtok + 128, :]), writes=[bXR])
            for half in range(2):
                p, bp = nextpy()
                for k in range(KC):
                    P.pe(lambda e, k=k, p=p, t=t, half=half: e.matmul(
                        p, lhsT=MG[:, k, t * 128:(t + 1) * 128], rhs=WOUT[:, k, half * 512:(half + 1) * 512],
                        start=(k == 0), stop=(k == KC - 1)), reads=[bMG[k], bWO[k]], writes=[bp])
                P.dve(lambda e, p=p, half=half: e.tensor_tensor(out=H2[:, half * 512:(half + 1) * 512], in0=p,
                                                               in1=XR[:, half * 512:(half + 1) * 512], op=ALU.add),
                      reads=[bp, bXR], writes=[bH2])
                yield
            P.dma(lambda e, gt=gt: e.dma_start(out=d["h2_s"][gt * 128:(gt + 1) * 128, :], in_=H2),
                  reads=[bH2], writes=[self.bh2s[gt]])
            LL = dict(L0)
            LL['nextpj'] = nextpy
            yield from self._route_tile(LL, gt)
    def Y3(sbi):
        yield from Y3a(sbi)
        yield from Y3b(sbi)

    L0 = dict(locals())

    import os
    WTS3 = [int(v) for v in os.environ.get("IL_W3", "1,1").split(",")]

    def run_il(gens):
        gens = list(gens)
        while gens:
            for g_, w_ in list(gens):
                for _ in range(w_):
                    try:
                        next(g_)
                    except StopIteration:
                        gens.remove((g_, w_))
                        break

    n = self.nfull
    if LS >= 2:
        sx = [P._capture(X3(i)) for i in range(n)]
        sa = [P._capture(Y3a(i)) for i in range(n)]
        sb_ = [P._capture(Y3b(i)) for i in range(n)]
        P.merge_pipeline([sx, sa, sb_],
                         [lambda s_, dn: dn[1] >= s_ - 1,
                          lambda s_, dn: dn[0] >= s_ + 1 and dn[2] >= s_ - 1,
                          lambda s_, dn: dn[1] >= s_ + 1])
    else:
        for r in range(-1, n):
            gs = []
            if 0 <= r < n:
                gs.append((Y3(r), WTS3[0]))
            if 0 <= r + 1 < n:
                gs.append((X3(r + 1), WTS3[1]))
            if LS:
                P.merge_streams([g_ for g_, _w in gs])
            else:
                run_il(gs)
    A.reset(m0)
    P.barrier()


KB.pass3 = _pass3


def _route_tile(self, L, gt):
    P = self.P
    g = lambda n: L[n]
    bk, bb = self.banks, self.bbank
    bc = self.bconst
    H2, bH2, JK, SS2, bSS2 = g("H2"), g("bH2"), g("JK"), g("SS2"), g("bSS2")
    XF, XB, bXF, bXB, XFT, bXFT = g("XF"), g("XB"), g("bXF"), g("bXB"), g("XFT"), g("bXFT")
    G2, WR, RB, ECAP, bpar = g("G2"), g("WR"), g("RB"), g("ECAP"), g("bpar")
    LG, ME, SM, M8, SEL1, SEL2, SELB, RK, JK2, DF, brt = (g("LG"), g("ME"), g("SM"), g("M8"), g("SEL1"), g("SEL2"),
                                                           g("SELB"), g("RK"), g("JK2"), g("DF"), g("brt"))
    BASE, bBASE, triS = g("BASE"), g("bBASE"), g("triS")
    nextpj = g("nextpj")
    W12, DST = self.W12, self.DST
    P.act(lambda e: e.activation(out=JK, in_=H2, func=AF.Square, accum_out=SS2), reads=[bH2], writes=[bSS2])
    P.act(lambda e: e.activation(out=SM[:, 0:1], in_=SS2, func=AF.Ln, scale=1.0 / D, bias=EPS), reads=[bSS2], writes=[brt])
    P.act(lambda e: e.activation(out=SM[:, 0:1], in_=SM[:, 0:1], func=AF.Exp, scale=-0.5), reads=[brt], writes=[brt])
    P.dve(lambda e: e.scalar_tensor_tensor(out=XF, in0=H2, scalar=SM[:, 0:1], in1=G2, op0=ALU.mult, op1=ALU.mult),
          reads=[bH2, brt, bpar], writes=[bXF])
    P.pool(lambda e: e.tensor_copy(out=XB, in_=XF), reads=[bXF], writes=[bXB])
    yield
    for half in range(2):
        for kk in range(4):
            k = half * 4 + kk
            P.pe(lambda e, k=k, kk=kk, half=half: e.transpose(out=bk[7 * half][:, kk * 128:(kk + 1) * 128],
                                                              in_=XF[:, k * 128:(k + 1) * 128], identity=self.identf),
                 reads=[bXF, bc], writes=[bb[7 * half]])
    P.act(lambda e: e.activation(out=XFT[:, 0:4, :], in_=bk[0][:].rearrange("p (k t) -> p k t", k=4), func=AF.Copy),
          reads=[bb[0]], writes=[bXFT])
    P.dve(lambda e: e.tensor_copy(out=XFT[:, 4:8, :], in_=bk[7][:].rearrange("p (k t) -> p k t", k=4)),
          reads=[bb[7]], writes=[bXFT])
    yield
    p, bp = nextpj()
    for k in range(KC):
        P.pe(lambda e, k=k, p=p: e.matmul(p[:, 0:36], lhsT=XFT[:, k, :], rhs=WR[:, k, :], start=(k == 0), stop=(k == KC - 1)),
             reads=[bXFT, bpar], writes=[bp])
    P.dve(lambda e, p=p: e.tensor_tensor(out=LG, in0=p[:, 0:36], in1=RB, op=ALU.add), reads=[bp, bpar], writes=[brt])
    yield
    P.dve(lambda e: e.tensor_reduce(out=SM[:, 1:2], in_=LG[:, 0:4], axis=AX.X, op=ALU.max), reads=[brt], writes=[brt])
    P.dve(lambda e: e.tensor_scalar(out=SM[:, 2:3], in0=SM[:, 1:2], scalar1=-1.0, scalar2=None, op0=ALU.mult),
          reads=[brt], writes=[brt])
    P.act(lambda e: e.activation(out=JK2[:, 0:4], in_=LG[:, 0:4], func=AF.Exp, bias=SM[:, 2:3], accum_out=SM[:, 3:4]),
          reads=[brt], writes=[brt])
    P.dve(lambda e: e.reciprocal(out=SM[:, 4:5], in_=SM[:, 3:4]), reads=[brt], writes=[brt])
    yield
    P.dve(lambda e: e.tensor_scalar(out=SM[:, 12:16], in0=LG[:, 0:4], scalar1=SM[:, 1:2], scalar2=-1.0,
                                    op0=ALU.is_equal, op1=ALU.add), reads=[brt], writes=[brt])
    P.dve(lambda e: e.scalar_tensor_tensor(out=ME.rearrange("p (g j) -> p g j", g=4),
                                           in0=SM[:, 12:16].unsqueeze(2).to_broadcast([128, 4, 8]), scalar=BIG,
                                           in1=LG[:, 4:36].rearrange("p (g j) -> p g j", g=4),
                                           op0=ALU.mult, op1=ALU.add), reads=[brt], writes=[brt])
    P.dve(lambda e: e.max(out=M8, in_=ME), reads=[brt], writes=[brt])
    yield
    P.dve(lambda e: e.tensor_tensor(out=SM[:, 5:6], in0=M8[:, 1:2], in1=M8[:, 0:1], op=ALU.subtract), reads=[brt], writes=[brt])
    P.act(lambda e: e.activation(out=SM[:, 6:7], in_=SM[:, 5:6], func=AF.Exp), reads=[brt], writes=[brt])
    P.dve(lambda e: e.tensor_scalar(out=SM[:, 7:8], in0=SM[:, 6:7], scalar1=1.0, scalar2=None, op0=ALU.add),
          reads=[brt], writes=[brt])
    P.dve(lambda e: e.reciprocal(out=SM[:, 8:9], in_=SM[:, 7:8]), reads=[brt], writes=[brt])
    P.dve(lambda e, gt=gt: e.tensor_tensor(out=W12[:, gt, 0:1], in0=SM[:, 8:9], in1=SM[:, 4:5], op=ALU.mult),
          reads=[brt], writes=[self.bW12])
    P.dve(lambda e, gt=gt: e.tensor_tensor(out=W12[:, gt, 1:2], in0=SM[:, 4:5], in1=W12[:, gt, 0:1], op=ALU.subtract),
          reads=[brt, self.bW12], writes=[self.bW12])
    P.dve(lambda e: e.tensor_scalar(out=SEL1, in0=ME, scalar1=M8[:, 0:1], scalar2=None, op0=ALU.is_equal),
          reads=[brt], writes=[brt])
    P.dve(lambda e: e.tensor_scalar(out=SEL2, in0=ME, scalar1=M8[:, 1:2], scalar2=None, op0=ALU.is_equal),
          reads=[brt], writes=[brt])
    P.dve(lambda e: e.tensor_tensor(out=SELB, in0=SEL1, in1=SEL2, op=ALU.add), reads=[brt], writes=[brt])
    yield
    p2, bp2 = nextpj()
    P.pe(lambda e, p2=p2: e.matmul(p2[:, 0:32], lhsT=triS, rhs=SELB, start=True, stop=True), reads=[brt, bc], writes=[bp2])
    P.pe(lambda e, p2=p2: e.matmul(p2[:, 32:64], lhsT=self.ones_bf, rhs=SELB, start=True, stop=True),
         reads=[brt, bc], writes=[bp2])
    P.dve(lambda e, p2=p2: e.tensor_tensor(out=RK, in0=p2[:, 0:32], in1=BASE, op=ALU.add), reads=[bp2, bBASE], writes=[brt])
    P.dve(lambda e, p2=p2: e.tensor_tensor(out=BASE, in0=p2[:, 32:64], in1=BASE, op=ALU.add), reads=[bp2, bBASE], writes=[bBASE])
    P.dve(lambda e: e.scalar_tensor_tensor(out=JK2, in0=SEL1, scalar=1.0, in1=RK, op0=ALU.mult, op1=ALU.mult,
                                           accum_out=DF[:, 0:1]), reads=[brt], writes=[brt])
    P.dve(lambda e: e.scalar_tensor_tensor(out=JK2, in0=SEL2, scalar=1.0, in1=RK, op0=ALU.mult, op1=ALU.mult,
                                           accum_out=DF[:, 1:2]), reads=[brt], writes=[brt])
    P.dve(lambda e, gt=gt: e.tensor_copy(out=DST[:, gt, :], in_=DF), reads=[brt], writes=[self.bDST])
    yield
    xb = self.d["x_buf"]
    for k in range(2):
        bx = Buf("xbuf")
        self.bxbuf.append(bx)
        P.dma(lambda e, gt=gt, k=k: e.indirect_dma_start(
            out=xb, out_offset=bass.IndirectOffsetOnAxis(ap=DST[:, gt, k:k + 1], axis=0), in_=XB, in_offset=None),
            reads=[bXB, self.bDST] + self.bxz, writes=[bx], q="pool")


KB._route_tile = _route_tile


def _pass4(self):
    nc, P, A = self.nc, self.P, self.A
    d = self.d
    self.din("w_gate", [NEXP, D, 512])
    self.din("w_up", [NEXP, D, 512])
    self.din("w_down", [NEXP, 512, D])
    d["y_buf"] = nc.dram_tensor("y_buf", [NSLOT, D], BF16, kind="Internal").ap()
    self.bybuf = []
    m0 = A.mark()
    NB = CAP // 128
    WG = [A.alloc((KC, 512), BF16) for _ in range(2)]
    WU = [A.alloc((KC, 512), BF16) for _ in range(2)]
    WD = [A.alloc((4, D), BF16) for _ in range(2)]
    bWG = [Buf(), Buf()]
    bWU = [Buf(), Buf()]
    bWD = [Buf(), Buf()]
    XE = [A.alloc((NB, D), BF16) for _ in range(2)]
    bXE = [Buf(), Buf()]
    XET = [A.alloc((KC, CAP), BF16) for _ in range(2)]
    bXET = [Buf(), Buf()]
    SGT = [A.alloc((CAP,), F32) for _ in range(2)]
    bSGT = [Buf(), Buf()]
    HT = A.alloc((4, CAP), BF16)
    bHT = [Buf() for _ in range(4)]
    YS = [A.alloc((D,), BF16) for _ in range(2)]
    bYS = [Buf(), Buf()]
    bk, bb = self.banks, self.bbank
    ptr = bk[0][:].bitcast(BF16).rearrange("p (k t) -> p k t", k=KC)
    ident = self.ident
    bc = self.bconst
    xb, yb = d["x_buf"], d["y_buf"]
    cnt = [0]

    def bank(lo, n):
        i = lo + cnt[0] % n
        cnt[0] += 1
        return bk[i][:], bb[i]

    def load_w(e):
        i = e % 2
        P.dma(lambda eng, e=e, i=i: eng.dma_start(out=WG[i], in_=d["w_gate"][e].rearrange("(k p) n -> p k n", p=128)),
              writes=[bWG[i]], q="pool")
        P.dma(lambda eng, e=e, i=i: eng.dma_start(out=WU[i], in_=d["w_up"][e].rearrange("(k p) n -> p k n", p=128)),
              writes=[bWU[i]], q="pool")
        P.dma(lambda eng, e=e, i=i: eng.dma_start(out=WD[i], in_=d["w_down"][e].rearrange("(k p) n -> p k n", p=128)),
              writes=[bWD[i]], q="pool")

    def load_x(e):
        i = e % 2
        P.dma(lambda eng, e=e, i=i: eng.dma_start(out=XE[i], in_=xb[e * CAP:(e + 1) * CAP, :].rearrange("(b p) n -> p b n", p=128)),
              reads=self.bxbuf, writes=[bXE[i]])

    def transposes(e):
        i = e % 2
        for b in range(NB):
            pt, bpt = (ptr, bb[0]) if b % 2 == 0 else (ptr7, bb[7])
            for k in range(KC):
                P.pe(lambda eng, i=i, b=b, k=k, pt=pt: eng.transpose(out=pt[:, k, :], in_=XE[i][:, b, k * 128:(k + 1) * 128], identity=ident),
                     reads=[bXE[i], bc], writes=[bpt])
            if b % 2 == 0:
                P.act(lambda eng, b=b, i=i, pt=pt: eng.activation(out=XET[i][:, :, b * 128:(b + 1) * 128], in_=pt, func=AF.Copy),
                      reads=[bpt], writes=[bXET[i]])
            else:
                P.dve(lambda eng, b=b, i=i, pt=pt: eng.tensor_copy(out=XET[i][:, :, b * 128:(b + 1) * 128], in_=pt),
                      reads=[bpt], writes=[bXET[i]])

    ptr7 = bk[7][:].bitcast(BF16).rearrange("p (k t) -> p k t", k=KC)
    load_w(0)
    load_x(0)
    transposes(0)
    yi = 0
    for e in range(NEXP):
        i = e % 2
        if e + 1 < NEXP:
            load_w(e + 1)
            load_x(e + 1)
        for fc in range(4):
            pg, bpg = bk[1 + fc % 2][:], bb[1 + fc % 2]
            pu, bpu = bk[3 + fc % 2][:], bb[3 + fc % 2]
            for k in range(KC):
                P.pe(lambda eng, i=i, fc=fc, k=k, pg=pg: eng.matmul(pg[:, 0:CAP], lhsT=WG[i][:, k, fc * 128:(fc + 1) * 128],
                                                                    rhs=XET[i][:, k, :], start=(k == 0), stop=(k == KC - 1)),
                     reads=[bWG[i], bXET[i]], writes=[bpg])
            for k in range(KC):
                P.pe(lambda eng, i=i, fc=fc, k=k, pu=pu: eng.matmul(pu[:, 0:CAP], lhsT=WU[i][:, k, fc * 128:(fc + 1) * 128],
                                                                    rhs=XET[i][:, k, :], start=(k == 0), stop=(k == KC - 1)),
                     reads=[bWU[i], bXET[i]], writes=[bpu])
            j = fc % 2
            P.act(lambda eng, pg=pg, j=j: eng.activation(out=SGT[j], in_=pg[:, 0:CAP], func=AF.Silu), reads=[bpg], writes=[bSGT[j]])
            P.dve(lambda eng, pu=pu, j=j, fc=fc: eng.tensor_tensor(out=HT[:, fc, :], in0=pu[:, 0:CAP], in1=SGT[j], op=ALU.mult),
                  reads=[bpu, bSGT[j]], writes=[bHT[fc]])
        if e + 1 < NEXP:
            transposes(e + 1)
        for b in range(NB):
            ys, bys = YS[yi % 2], bYS[yi % 2]
            yi += 1
            for half in range(2):
                pd, bpd = bk[5 + half][:], bb[5 + half]
                for fc in range(4):
                    P.pe(lambda eng, i=i, b=b, fc=fc, half=half, pd=pd: eng.matmul(
                        pd, lhsT=HT[:, fc, b * 128:(b + 1) * 128], rhs=WD[i][:, fc, half * 512:(half + 1) * 512],
                        start=(fc == 0), stop=(fc == 3)), reads=[bHT[fc], bWD[i]], writes=[bpd])
                if half == 0:
                    P.act(lambda eng, pd=pd, ys=ys: eng.activation(out=ys[:, 0:512], in_=pd, func=AF.Copy), reads=[bpd], writes=[bys])
                else:
                    P.dve(lambda eng, pd=pd, ys=ys: eng.tensor_copy(out=ys[:, 512:1024], in_=pd), reads=[bpd], writes=[bys])
            r0 = e * CAP + b * 128
            by = Buf("ybuf")
            self.bybuf.append(by)
            P.dma(lambda eng, r0=r0, ys=ys: eng.dma_start(out=yb[r0:r0 + 128, :], in_=ys), reads=[bys], writes=[by])
    A.reset(m0)
    P.barrier()


def _pass5(self):
    nc, P, A = self.nc, self.P, self.A
    d = self.d
    self.din("final_norm_g", [D])
    out = self.dout("out", [self.nfull_tok, D], F32)
    m0 = A.mark()
    ntile = self.nfull_tok // 128
    FG = A.alloc((D,), F32)
    bFG = Buf()
    P.dma(lambda e: e.dma_start(out=FG, in_=d["final_norm_g"].partition_broadcast(128)), writes=[bFG])
    NB5 = 4
    Y1 = [A.alloc((D,), BF16) for _ in range(NB5)]
    Y2 = [A.alloc((D,), BF16) for _ in range(NB5)]
    HH = [A.alloc((D,), F32) for _ in range(NB5)]
    OT = [A.alloc((D,), F32) for _ in range(NB5)]
    bY1, bY2, bHH, bOT = ([Buf() for _ in range(NB5)], [Buf() for _ in range(NB5)], [Buf() for _ in range(NB5)],
                          [Buf() for _ in range(NB5)])
    JK = A.alloc((D,), BF16)
    SS = A.alloc((ntile,), F32)
    bSS = Buf()
    yb = d["y_buf"]
    for gt in range(ntile):
        i = gt % NB5
        P.dma(lambda e, gt=gt, i=i: e.indirect_dma_start(
            out=Y1[i], out_offset=None, in_=yb, in_offset=bass.IndirectOffsetOnAxis(ap=self.DST[:, gt, 0:1], axis=0)),
            reads=self.bybuf + [self.bDST], writes=[bY1[i]], q="pool")
        P.dma(lambda e, gt=gt, i=i: e.indirect_dma_start(
            out=Y2[i], out_offset=None, in_=yb, in_offset=bass.IndirectOffsetOnAxis(ap=self.DST[:, gt, 1:2], axis=0)),
            reads=self.bybuf + [self.bDST], writes=[bY2[i]], q="pool")
        P.dma(lambda e, gt=gt, i=i: e.dma_start(out=HH[i], in_=d["h2_s"][gt * 128:(gt + 1) * 128, :]),
              reads=[self.bh2s[gt]], writes=[bHH[i]])
        P.dve(lambda e, gt=gt, i=i: e.scalar_tensor_tensor(out=HH[i], in0=Y1[i], scalar=self.W12[:, gt, 0:1], in1=HH[i],
                                                          op0=ALU.mult, op1=ALU.add),
              reads=[bY1[i], bHH[i], self.bW12], writes=[bHH[i]])
        P.dve(lambda e, gt=gt, i=i: e.scalar_tensor_tensor(out=HH[i], in0=Y2[i], scalar=self.W12[:, gt, 1:2], in1=HH[i],
                                                          op0=ALU.mult, op1=ALU.add),
              reads=[bY2[i], bHH[i], self.bW12], writes=[bHH[i]])
        P.act(lambda e, gt=gt, i=i: e.activation(out=JK, in_=HH[i], func=AF.Square, accum_out=SS[:, gt:gt + 1]),
              reads=[bHH[i]], writes=[bSS])
        P.act(lambda e, gt=gt: e.activation(out=SS[:, gt:gt + 1], in_=SS[:, gt:gt + 1], func=AF.Ln, scale=1.0 / D, bias=EPS),
              reads=[bSS], writes=[bSS])
        P.act(lambda e, gt=gt: e.activation(out=SS[:, gt:gt + 1], in_=SS[:, gt:gt + 1], func=AF.Exp, scale=-0.5),
              reads=[bSS], writes=[bSS])
        P.dve(lambda e, gt=gt, i=i: e.scalar_tensor_tensor(out=OT[i], in0=HH[i], scalar=SS[:, gt:gt + 1], in1=FG,
                                                          op0=ALU.mult, op1=ALU.mult),
              reads=[bHH[i], bSS, bFG], writes=[bOT[i]])
        P.dma(lambda e, gt=gt, i=i: e.dma_start(out=out[gt * 128:(gt + 1) * 128, :], in_=OT[i]), reads=[bOT[i]], out=True)
    A.reset(m0)


KB.pass4 = _pass4
KB.pass5 = _pass5


NPRE_SB = 17
NFULL_SB = 16
_NC_CACHE = {}


def _build_full():
    if "nc" not in _NC_CACHE:
        kb = KB(NPRE_SB, NFULL_SB)
        kb.setup()
        kb.pass1()
        kb.pass2()
        kb.pass3()
        kb.pass4()
        kb.pass5()
        _NC_CACHE["nc"] = kb.finish()
    return _NC_CACHE["nc"]


def kernel(x, meta_tokens, hg_lb_logits, norm_mix_g, w_in, gd_conv_w, gd_A_log, gd_dt_bias, hg_norm_g, gd_norm_g,
           hg_up, gd_up, w_out, norm_ffn_g, router_group_w, router_group_b, router_expert_w, router_expert_b,
           w_gate, w_up, w_down, final_norm_g):
    f32 = np.float32
    c = lambda a: np.ascontiguousarray(np.asarray(a, dtype=f32))
    x = c(x)
    meta = c(meta_tokens)
    B, S, _ = x.shape
    half = S // 2
    npre_tok = NPRE_SB * SBT
    ntok = (NPRE_SB + NFULL_SB) * SBT
    nmeta = meta.shape[0]
    shared = {
        "w_in": c(w_in[0]),
        "norm_mix_g": c(norm_mix_g[0]),
        "hg_lb": c(np.asarray(hg_lb_logits, f32).reshape(2, 4, 128).transpose(2, 0, 1)),
        "hg_norm_g": c(hg_norm_g[0]),
        "conv_wT": c(np.asarray(gd_conv_w[0], f32).reshape(4, 12, 128).transpose(2, 0, 1)),
        "gd_A_log": c(gd_A_log[0]),
        "gd_dt_bias": c(gd_dt_bias[0]),
        "gd_norm_g": c(gd_norm_g[0]),
        "hg_up": c(hg_up[0]),
        "gd_up": c(gd_up[0]),
        "w_out": c(w_out[0]),
        "norm_ffn_g": c(norm_ffn_g[0]),
        "router_w": c(np.concatenate([np.asarray(router_group_w[0], f32), np.asarray(router_expert_w[0], f32)], axis=1)),
        "router_b": c(np.concatenate([np.asarray(router_group_b[0], f32), np.asarray(router_expert_b[0], f32)])),
        "w_gate": c(w_gate[0]),
        "w_up": c(w_up[0]),
        "w_down": c(w_down[0]),
        "final_norm_g": c(final_norm_g),
    }
    in_maps = []
    for core in range(2 * B):
        b, hf = core // 2, core % 2
        xs = np.zeros((ntok, D), f32)
        if hf == 0:
            xs[npre_tok - nmeta:npre_tok] = meta
            xs[npre_tok:] = x[b, 0:half]
        else:
            xs[npre_tok - half - nmeta:npre_tok - half] = meta
            xs[npre_tok - half:] = x[b]
        m = dict(shared)
        m["xs"] = xs
        in_maps.append(m)
    nc = _build_full()
    res = run_bass_kernel_spmd(nc, in_maps, core_ids=list(range(2 * B)))
    out = np.empty((B, S, D), f32)
    for core in range(2 * B):
        b, hf = core // 2, core % 2
        out[b, hf * half:(hf + 1) * half] = np.asarray(res.results[core]["out"], dtype=f32)
    return out

LCOST_TABLE = {('dve', 389): 0.041, ('act', 389): 0.031, ('pool', 389): 0.051, ('pool', 303): 0.019, ('pool', 425): 0.166, ('pool', 549): 0.643, ('pool', 305): 0.019, ('pool', 551): 0.622, ('pool', 625): 0.172, ('pool', 626): 0.155, ('dve', 305): 0.024, ('act', 303): 0.019, ('act', 563): 0.745, ('act', 305): 0.019, ('act', 482): 0.553, ('act', 489): 0.353, ('act', 492): 0.121, ('dve', 303): 0.024, ('dve', 498): 1.232, ('pe', 503): 0.127, ('dve', 506): 0.651, ('pe', 632): 0.142, ('act', 654): 0.562, ('dve', 685): 0.342, ('pe', 672): 0.232, ('dve', 689): 0.671, ('act', 676): 0.673, ('act', 692): 0.523, ('dve', 695): 0.593, ('act', 703): 0.508, ('act', 707): 0.101, ('dve', 708): 0.603, ('pe', 739): 0.119, ('dve', 742): 0.401, ('pe', 756): 0.1, ('dve', 760): 0.348, ('act', 766): 0.328, ('act', 660): 0.541, ('act', 665): 0.341, ('dve', 713): 0.869, ('dve', 717): 0.249, ('act', 719): 0.333, ('act', 722): 0.12, ('act', 725): 0.306, ('dve', 728): 0.694, ('dve', 731): 1.226, ('dve', 735): 0.601, ('pe', 776): 0.081, ('pe', 790): 0.108, ('dve', 779): 0.693, ('pe', 795): 0.199, ('dve', 799): 0.331, ('act', 804): 0.371, ('pe', 810): 0.102, ('act', 814): 0.602, ('act', 821): 0.333, ('pe', 825): 0.359, ('act', 827): 0.355, ('act', 829): 0.402, ('dve', 831): 0.398, ('dve', 833): 0.46, ('dve', 948): 0.286, ('pe', 1062): 0.156, ('pool', 1054): 0.156, ('pool', 1055): 0.154, ('pool', 1057): 0.264, ('act', 1074): 0.556, ('dve', 1076): 0.565, ('pe', 1103): 0.029, ('pe', 1131): 0.136, ('act', 1106): 0.218, ('act', 1109): 0.181, ('act', 1110): 0.181, ('dve', 1111): 0.339, ('act', 1113): 0.152, ('dve', 1114): 0.143, ('act', 1116): 0.181, ('act', 1117): 0.209, ('act', 1135): 0.415, ('dve', 1118): 0.214, ('pool', 1147): 0.584, ('act', 1138): 0.358, ('pe', 1150): 0.325, ('act', 1152): 0.503, ('act', 1154): 0.407, ('pool', 1157): 0.723, ('pe', 1284): 0.171, ('act', 1287): 0.249, ('pool', 1094): 0.145, ('dve', 1289): 0.163, ('act', 1291): 0.135, ('pool', 1306): 1.828, ('dve', 1293): 0.797, ('dve', 1295): 0.495, ('act', 1297): 0.177, ('act', 1298): 0.205, ('dve', 1299): 0.152, ('pe', 1311): 0.142, ('dve', 1318): 0.692, ('act', 1330): 0.351, ('pool', 1341): 1.268, ('pe', 1356): 0.073, ('dve', 1361): 0.645, ('pe', 1380): 0.109, ('pool', 1389): 1.147, ('pe', 1398): 0.111, ('pe', 1410): 0.111, ('pe', 1421): 0.116, ('dve', 1429): 0.672, ('pe', 1439): 0.106, ('dve', 1441): 0.674, ('pe', 1444): 0.134, ('pool', 1447): 1.829, ('act', 1446): 0.673, ('pool', 1448): 1.02, ('pe', 1489): 0.124, ('dve', 1499): 0.347, ('pe', 1505): 0.152, ('act', 1509): 0.654, ('pe', 1515): 0.154, ('dve', 1520): 0.278, ('act', 1525): 0.679, ('act', 1124): 0.519, ('dve', 1321): 0.597, ('act', 1323): 0.545, ('act', 1335): 0.36, ('pool', 1345): 1.274, ('dve', 1349): 0.501, ('pe', 1368): 0.131, ('dve', 1374): 0.622, ('pe', 1494): 0.06, ('pe', 1530): 0.121, ('act', 1533): 0.687, ('act', 1184): 0.333, ('pe', 1188): 0.312, ('act', 1190): 0.355, ('act', 1192): 0.416, ('dve', 1193): 0.591, ('dve', 1195): 0.461, ('pool', 1589): 0.635, ('pe', 1700): 0.117, ('pool', 1599): 0.638, ('act', 1703): 0.426, ('pe', 1716): 0.143, ('pe', 1720): 0.118, ('dve', 1723): 0.408, ('dve', 1725): 0.395, ('pool', 1727): 0.728, ('pe', 1744): 0.303, ('dve', 1747): 0.692, ('act', 1817): 0.575, ('act', 1818): 0.419, ('act', 1819): 0.203, ('dve', 1820): 1.284, ('pe', 1828): 0.242, ('pool', 1822): 3.58, ('act', 1831): 0.687, ('dve', 1833): 0.693, ('pe', 1838): 0.195, ('dve', 1840): 0.196, ('dve', 1843): 0.157, ('dve', 1844): 0.154, ('act', 1846): 0.316, ('dve', 1848): 0.164, ('dve', 1850): 0.229, ('dve', 1852): 0.192, ('dve', 1857): 0.171, ('dve', 1859): 0.136, ('act', 1860): 0.202, ('dve', 1861): 0.13, ('dve', 1863): 0.164, ('dve', 1864): 0.226, ('dve', 1866): 0.137, ('dve', 1868): 0.218, ('dve', 1870): 0.344, ('dve', 1872): 0.192, ('pe', 1876): 0.183, ('pe', 1877): 0.027, ('dve', 1879): 0.168, ('dve', 1880): 0.192, ('dve', 1881): 0.105, ('dve', 1883): 0.089, ('dve', 1885): 0.146, ('pool', 1891): 1.135, ('pool', 1940): 1.059, ('pool', 1942): 1.055, ('pool', 1944): 0.9, ('pe', 1957): 0.086, ('act', 1960): 1.113, ('dve', 1963): 0.692, ('pe', 1981): 0.165, ('act', 1989): 0.459, ('pe', 1985): 0.163, ('dve', 1990): 0.53, ('pe', 2001): 0.266, ('act', 2005): 0.673, ('dve', 2007): 0.692, ('pool', 2040): 1.114, ('pool', 2043): 1.106, ('dve', 2048): 1.285, ('pool', 289): 2.89, ('dve', 2051): 1.285, ('act', 2054): 0.574, ('act', 2056): 0.419, ('act', 2058): 0.203, ('dve', 2060): 1.284, ('act', 289): 0.115, ('dve', 289): 0.658}
Prog.LCOST = LCOST_TABLE
```

```python
import numpy as np
import concourse.bass as bass
import concourse.mybir as mybir
from concourse.bass_utils import run_bass_kernel_spmd

F32 = mybir.dt.float32
BF16 = mybir.dt.bfloat16
I32 = mybir.dt.int32
U32 = mybir.dt.uint32
AF = mybir.ActivationFunctionType
ALU = mybir.AluOpType
AX = mybir.AxisListType


class Buf:
    __slots__ = ("name", "lw", "rd", "psum")

    def __init__(self, name="", psum=False):
        self.name = name
        self.lw = None
        self.rd = {}
        self.psum = psum


class Prog:
    ENGS = ("pe", "act", "dve", "pool", "sp")

    def __init__(self, kdma=6):
        self.ops = {e: [] for e in self.ENGS}
        self.waited = {e: {} for e in self.ENGS}
        self.ndma = {e: 0 for e in self.ENGS}
        self.K = kdma
        self.out_toks = []
        self.pending = {e: [] for e in self.ENGS}

    def barrier(self):
        toks = []
        for e in self.ENGS:
            for i in range(len(self.ops[e]) - 1, -1, -1):
                op = self.ops[e][i]
                if (not op["dma"]) and op["fn"] is not None:
                    toks.append((e, i))
                    break
            n = self.ndma[e]
            for slot in range(min(self.K, n)):
                last = ((n - 1 - slot) // self.K) * self.K + slot
                toks.append((("dma", e, slot), 16 * (last // self.K + 1)))
        for e in self.ENGS:
            self.pending[e] = list(toks)

    cap = None
    COST = {"pe": 0.13, "act": 0.45, "dve": 0.42, "pool": 1.2, "sp": 0.05}
    DMA_LAT = 2.5
    LCOST = {}

    def _emit(self, eng, fn, reads, writes, dma=False, extra=()):
        if self.cap is not None:
            self.cap.append((eng, fn, tuple(reads), tuple(writes), dma, tuple(extra)))
            return None
        return self._emit_real(eng, fn, reads, writes, dma, extra)

    _act_tab = ""

    @staticmethod
    def _act_class(fn):
        nm = fn.__code__.co_names
        if "Silu" in nm or "Sigmoid" in nm:
            return "sig"
        if "Exp" in nm or "Ln" in nm:
            return "exp"
        return ""

    def _capture(self, g):
        self.cap = []
        for _ in g:
            pass
        ops = self.cap
        self.cap = None
        return ops

    def merge_streams(self, gens):
        segs = [[self._capture(g)] for g in gens]
        self.merge_pipeline(segs, [lambda s_, done: True] * len(segs))

    def merge_pipeline(self, segs, rules, bias=None):
        n = len(segs)
        sp = [0] * n
        op = [0] * n
        done = [0] * n
        free = getattr(self, "_sim_free", None)
        if free is None:
            free = self._sim_free = {e: 0.0 for e in self.ENGS}
            self._sim_buf = {}
        bt = self._sim_buf
        rr = 0
        while True:
            best, bestc, bi = None, None, -1
            for k in range(n):
                i = (rr + k) % n
                while sp[i] < len(segs[i]) and op[i] >= len(segs[i][sp[i]]):
                    sp[i] += 1
                    op[i] = 0
                    done[i] = sp[i]
                if sp[i] >= len(segs[i]):
                    continue
                if op[i] == 0 and not rules[i](sp[i], done):
                    continue
                eng, fn, reads, writes, dma, extra = segs[i][sp[i]][op[i]]
                t = free[eng]
                for b in reads:
                    v = bt.get(id(b))
                    if v is not None and v[0] > t:
                        t = v[0]
                for b in writes:
                    v = bt.get(id(b))
                    if v is not None:
                        if v[0] > t:
                            t = v[0]
                        if v[1] > t:
                            t = v[1]
                if eng == "act" and fn is not None:
                    tc = self._act_class(fn)
                    if tc and tc != self._act_tab:
                        t += 0.4
                if bias is not None:
                    t_cmp = t - bias[i]
                else:
                    t_cmp = t
                if best is None or t_cmp < bestc - 1e-9:
                    best, bestc, bi = t, t_cmp, i
            if bi < 0:
                if all(sp[i] >= len(segs[i]) for i in range(n)):
                    break
                progressed = False
                for i in range(n):
                    if sp[i] < len(segs[i]) and op[i] == 0 and rules[i](sp[i], done):
                        progressed = True
                assert progressed, ("pipeline gating deadlock", sp, done)
                continue
            rr = (bi + 1) % n
            eng, fn, reads, writes, dma, extra = segs[bi][sp[bi]][op[bi]]
            op[bi] += 1
            ln = fn.__code__.co_firstlineno if fn is not None else -1
            c = self.LCOST.get((eng, ln), self.COST[eng])
            if eng == "act" and fn is not None and not dma:
                tc = self._act_class(fn)
                if tc:
                    self._act_tab = tc
            if dma:
                occ = 0.6 if eng == "pool" else 0.05
                fin = best + occ + self.DMA_LAT
                free[eng] = best + occ
            else:
                fin = best + c
                free[eng] = fin
            for b in writes:
                bt[id(b)] = [fin, fin]
            for b in reads:
                v = bt.get(id(b))
                if v is None:
                    bt[id(b)] = [0.0, fin]
                elif v[1] < fin:
                    v[1] = fin
                if b.psum and bt[id(b)][0] < fin:
                    bt[id(b)][0] = fin
            tok = self._emit_real(eng, fn, reads, writes, dma, extra)
            if dma and getattr(fn, "_is_out", False):
                self.out_toks.append(tok)

    def _emit_real(self, eng, fn, reads, writes, dma=False, extra=()):
        deps = {}

        def need(tok):
            if tok is None:
                return
            k, v = tok
            if eng == "pe" and k == "pe":
                return
            if deps.get(k, -1) < v:
                deps[k] = v
        def need_x(tok):
            if tok is not None and tok[0] != eng:
                need(tok)
        for b in reads:
            if b.psum:
                need_x(b.lw)
            else:
                need(b.lw)
        for b in writes:
            if b.psum:
                need_x(b.lw)
            else:
                need(b.lw)
                for k, v in b.rd.items():
                    need((k, v))
        for t in extra:
            need(t)
        if self.pending[eng]:
            for t in self.pending[eng]:
                need(t)
            self.pending[eng] = []
        if dma:
            i = self.ndma[eng]
            self.ndma[eng] += 1
            slot = i % self.K
            val = 16 * (i // self.K + 1)
            key = ("dma", eng, slot)
            if val > 16:
                need((key, val - 16))
            tok = (key, val)
        else:
            tok = (eng, len(self.ops[eng]))
        waits = []
        w = self.waited[eng]
        for k, v in deps.items():
            if w.get(k, -1) < v:
                w[k] = v
                waits.append((k, v))
        self.ops[eng].append(dict(waits=waits, fn=fn, tok=tok, dma=dma))
        for b in reads:
            if b.psum:
                b.lw = tok
                continue
            k, v = tok
            if b.rd.get(k, -1) < v:
                b.rd[k] = v
        for b in writes:
            b.lw = tok
            b.rd = {}
        return tok

    pe_dummy = None
    _pe_cnt = 0

    def pe(self, fn, reads=(), writes=()):
        t = self._emit("pe", fn, reads, writes)
        if self.pe_dummy is not None:
            dfn, every, buf = self.pe_dummy
            self._pe_cnt += 1
            if self._pe_cnt % every == 0:
                self._emit("pe", dfn, (), (buf,))
        return t

    def act(self, fn, reads=(), writes=()):
        return self._emit("act", fn, reads, writes)

    def dve(self, fn, reads=(), writes=()):
        return self._emit("dve", fn, reads, writes)

    def pool(self, fn, reads=(), writes=()):
        return self._emit("pool", fn, reads, writes)

    def dma(self, fn, reads=(), writes=(), q="sp", out=False):
        if out and self.cap is not None:
            try:
                fn._is_out = True
            except AttributeError:
                pass
        t = self._emit(q, fn, reads, writes, dma=True)
        if out and t is not None:
            self.out_toks.append(t)
        return t

    def finalize(self, nc):
        self._emit("sp", None, (), (), extra=self.out_toks)
        targets = {e: set() for e in self.ENGS}
        for e in self.ENGS:
            for op in self.ops[e]:
                for k, v in op["waits"]:
                    if isinstance(k, str):
                        targets[k].add(v)
        rank = {e: {} for e in self.ENGS}
        for e in self.ENGS:
            r = 0
            for i, op in enumerate(self.ops[e]):
                if (not op["dma"]) and i in targets[e]:
                    assert op["fn"] is not None
                    r += 1
                    rank[e][i] = r
        import contextlib
        with contextlib.ExitStack() as st:
            csem = {e: st.enter_context(nc.semaphore("c_" + e)) for e in self.ENGS}
            dsem = {}
            for e in self.ENGS:
                if self.ndma[e] > 0:
                    for s in range(min(self.K, self.ndma[e])):
                        dsem[("dma", e, s)] = st.enter_context(nc.semaphore("d_%s_%d" % (e, s)))
            block = st.enter_context(nc.Block())

            def run(e):
                def body(engine):
                    for i, op in enumerate(self.ops[e]):
                        for k, v in op["waits"]:
                            if isinstance(k, str):
                                engine.wait_ge(csem[k], rank[k][v])
                            else:
                                engine.wait_ge(dsem[k], v)
                        if op["fn"] is None:
                            continue
                        ins = op["fn"](engine)
                        if op["dma"]:
                            ins.then_inc(dsem[op["tok"][0]], 16)
                        elif i in rank[e]:
                            ins.then_inc(csem[e], 1)
                return body
            block.tensor(run("pe"))
            block.scalar(run("act"))
            block.vector(run("dve"))
            block.gpsimd(run("pool"))
            block.sync(run("sp"))


D = 1024
KC = 8
SBT = 256
NT = SBT // 128
NCH = SBT // 64
EPS = 1e-6
C_HQ, C_HF, C_HI, C_HG = 0, 512, 1024, 1536
C_GQ, C_GK, C_GV, C_GZ = 2048, 2560, 3072, 3584
C_GB, C_GA, C_PA, C_PB = 4096, 4100, 4104, 5128
DPROJ = 6152


class Arena:
    def __init__(self, ap, words):
        self.ap = ap
        self.words = words
        self.off = 0
        self.peak = 0

    def mark(self):
        return self.off

    def reset(self, m):
        self.off = m

    def alloc(self, free_shape, dt):
        n = 1
        for s in free_shape:
            n *= s
        esz = 4 if dt in (F32, I32, U32) else 2
        words = (n * esz + 3) // 4
        words = (words + 7) // 8 * 8
        assert self.off + words <= self.words, ("arena overflow", self.off, words, self.words)
        v = self.ap[:, self.off:self.off + words]
        self.off += words
        self.peak = max(self.peak, self.off)
        if esz == 2:
            v = v.bitcast(dt)
        elif dt != F32:
            v = v.bitcast(dt)
        v = v[:, 0:n]
        if len(free_shape) > 1:
            names = ["a%d" % i for i in range(len(free_shape))]
            pat = "p (%s) -> p %s" % (" ".join(names), " ".join(names))
            v = v.rearrange(pat, **{nm: s for nm, s in zip(names, free_shape)})
        return v


import os as _os_ls
LS = int(_os_ls.environ.get("LISTSCHED", "2"))


class KB:
    def __init__(self, npre, nfull, debug=()):
        import contextlib
        self.npre, self.nfull = npre, nfull
        self.nsb = npre + nfull
        self.ntok = self.nsb * SBT
        self.nfull_tok = nfull * SBT
        self.debug = set(debug)
        self.nc = bass.Bass("TRN2", target_bir_lowering=False)
        self.P = Prog()
        self.d = {}
        self.st = contextlib.ExitStack()

    def din(self, name, shape, dt=F32):
        self.d[name] = self.nc.dram_tensor(name, list(shape), dt, kind="ExternalInput").ap()
        return self.d[name]

    def dout(self, name, shape, dt=F32):
        self.d[name] = self.nc.dram_tensor(name, list(shape), dt, kind="ExternalOutput").ap()
        return self.d[name]

    def setup(self):
        nc, P, st = self.nc, self.P, self.st
        self.din("xs", [self.ntok, D])
        self.din("w_in", [D, DPROJ])
        self.din("norm_mix_g", [D])
        self.din("hg_lb", [128, 2, 4])
        self.din("hg_norm_g", [128])
        AW = 51200
        arena_t = st.enter_context(nc.sbuf_tensor("arena", [128, AW], F32))
        self.A = Arena(arena_t[:], AW)
        self.banks = [st.enter_context(nc.psum_tensor("pb%d" % i, [128, 512], F32)) for i in range(8)]
        self.bbank = [Buf("pb%d" % i, psum=True) for i in range(8)]
        A = self.A
        self.identf = A.alloc((128,), F32)
        self.ident = A.alloc((128,), BF16)
        self.ones_bf = A.alloc((128,), BF16)
        self.ones_f = A.alloc((512,), F32)
        self.mask2 = A.alloc((128,), F32)
        self.bconst = Buf("const")
        bc = self.bconst
        P.pool(lambda e: e.memset(self.identf, 0.0), writes=[bc])
        P.pool(lambda e: e.affine_select(out=self.identf, in_=self.identf, pattern=[[-1, 128]],
                                         compare_op=ALU.not_equal, fill=1.0, base=0, channel_multiplier=1),
               reads=[bc], writes=[bc])
        P.pool(lambda e: e.tensor_copy(out=self.ident, in_=self.identf), reads=[bc], writes=[bc])
        P.pool(lambda e: e.memset(self.ones_bf, 1.0), writes=[bc])
        P.pool(lambda e: e.memset(self.ones_f, 1.0), writes=[bc])
        P.pool(lambda e: e.memset(self.mask2, 1.0), writes=[bc])
        P.pool(lambda e: e.affine_select(out=self.mask2, in_=self.mask2, pattern=[[1, 128]],
                                         compare_op=ALU.is_ge, fill=0.0, base=0, channel_multiplier=-1),
               reads=[bc], writes=[bc])
        P.pool(lambda e: e.memset(self.mask2[0:64, 64:128], 0.0), reads=[bc], writes=[bc])
        self.xt = [A.alloc((D,), F32) for _ in range(2)]
        self.bxt = [Buf("xt%d" % i) for i in range(2)]
        self.junk = A.alloc((D,), BF16)
        self.bjunk = Buf("junk")
        self.ss = A.alloc((NT,), F32)
        self.rstd = A.alloc((NT,), F32)
        self.bss = Buf("ss")
        self.brstd = Buf("rstd")
        self.xsb = [A.alloc((D,), BF16) for _ in range(2)]
        self.bxsb = [Buf("xsb%d" % i) for i in range(2)]
        self.gbc = A.alloc((D,), F32)
        self.bgbc = Buf("gbc")
        self.nxt = 0
        self.d["x_buf"] = nc.dram_tensor("x_buf", [NSLOT, D], BF16, kind="Internal").ap()
        zt = A.alloc((D,), BF16)
        self.bxzero = Buf("xzero")
        P.pool(lambda e: e.memset(zt, 0.0), writes=[bc])
        xbv = self.d["x_buf"].rearrange("(b p) n -> p b n", p=128)
        nblk = NSLOT // 128
        step = 12
        self.bxz = []
        for b0 in range(0, nblk, step):
            bz = Buf("xz")
            self.bxz.append(bz)
            P.dma(lambda e, b0=b0: e.dma_start(out=xbv[:, b0:b0 + step, :],
                                               in_=zt.unsqueeze(1).to_broadcast([128, step, D])),
                  reads=[bc], writes=[bz])

    def stage_a(self, *a, **k):
        for _ in self.stage_a_gen(*a, **k):
            pass

    def stage_a_gen(self, sb, xnT, bxnT, ptr, bptr, gname="norm_mix_g", src="xs", tok_base=0, keep=None):
        nc, P = self.nc, self.P
        xs_d = self.d[src]
        tiles = []
        for t in range(NT):
            i = self.nxt % 2
            self.nxt += 1
            tok0 = tok_base + sb * SBT + t * 128
            xt, bxt = self.xt[i], self.bxt[i]
            P.dma(lambda e, xt=xt, tok0=tok0: e.dma_start(out=xt, in_=xs_d[tok0:tok0 + 128, :]), writes=[bxt])
            P.act(lambda e, xt=xt, t=t: e.activation(out=self.junk, in_=xt, func=AF.Square,
                                                     accum_out=self.ss[:, t:t + 1]),
                  reads=[bxt], writes=[self.bss])
            tiles.append((xt, bxt, i))
            if t % 2 == 1:
                t0 = t - 1
                P.act(lambda e, t0=t0: e.activation(out=self.rstd[:, t0:t0 + 2], in_=self.ss[:, t0:t0 + 2],
                                                    func=AF.Ln, scale=1.0 / D, bias=EPS),
                      reads=[self.bss], writes=[self.brstd])
                P.act(lambda e, t0=t0: e.activation(out=self.rstd[:, t0:t0 + 2], in_=self.rstd[:, t0:t0 + 2],
                                                    func=AF.Exp, scale=-0.5),
                      reads=[self.brstd], writes=[self.brstd])
                for tt in (t0, t):
                    xt2, bxt2, i2 = tiles[tt]
                    xsb, bxsb = self.xsb[i2], self.bxsb[i2]
                    P.dve(lambda e, xt2=xt2, xsb=xsb, tt=tt: e.scalar_tensor_tensor(
                        out=xsb, in0=xt2, scalar=self.rstd[:, tt:tt + 1], in1=self.gbc,
                        op0=ALU.mult, op1=ALU.mult),
                        reads=[bxt2, self.brstd, self.bgbc], writes=[bxsb])
                    for k in range(KC):
                        P.pe(lambda e, k=k, xsb=xsb: e.transpose(out=ptr[:, k, :], in_=xsb[:, k * 128:(k + 1) * 128],
                                                                 identity=self.ident),
                             reads=[bxsb, self.bconst], writes=[bptr])
                    P.dve(lambda e, tt=tt: e.tensor_copy(out=xnT[:, :, tt * 128:(tt + 1) * 128], in_=ptr),
                          reads=[bptr], writes=[bxnT])
                    yield


    def xnt_fetch_gen(self, sb, last_sb, xnTs, bxnTs):
        P = self.P
        src = self.d["xnT_s"]
        if sb not in self._xfetched:
            self._xfetched.add(sb)
            P.dma(lambda e, sb=sb: e.dma_start(out=xnTs[sb % 2], in_=src[:, :, sb * SBT:(sb + 1) * SBT]),
                  reads=[self.bxns[sb]], writes=[bxnTs[sb % 2]])
        nx = sb + 1
        if nx <= last_sb and nx not in self._xfetched:
            self._xfetched.add(nx)
            P.dma(lambda e, nx=nx: e.dma_start(out=xnTs[nx % 2], in_=src[:, :, nx * SBT:(nx + 1) * SBT]),
                  reads=[self.bxns[nx]], writes=[bxnTs[nx % 2]])
        yield

    def load_gain(self, gname):
        P = self.P
        g = self.d[gname]
        P.dma(lambda e: e.dma_start(out=self.gbc, in_=g.partition_broadcast(128)), writes=[self.bgbc])

    def pass1(self):
        nc, P, A = self.nc, self.P, self.A
        d = self.d
        H = 4
        self.W2 = A.alloc((KC, 2056), BF16)
        self.bW2 = [Buf("W2_%d" % k) for k in range(KC)]
        m0 = A.mark()
        self.OA = A.alloc((H, SBT), BF16)
        self.bOA = [Buf("OA%d" % h) for h in range(H)]
        W1 = A.alloc((KC, 2048), BF16)
        bW1 = [Buf("W1_%d" % k) for k in range(KC)]
        wv = d["w_in"].rearrange("(k p) n -> p k n", p=128)
        for k in range(KC):
            P.dma(lambda e, k=k: e.dma_start(out=W1[:, k, :], in_=wv[:, k, 0:2048]), writes=[bW1[k]], q="pool")
        for k in range(KC):
            P.dma(lambda e, k=k: e.dma_start(out=self.W2[:, k, :], in_=wv[:, k, 2048:2048 + 2056]), writes=[self.bW2[k]], q="pool")
        self.load_gain("norm_mix_g")
        lraw = A.alloc((2, H), F32)
        lb = A.alloc((H,), F32)
        oml = A.alloc((H,), F32)
        hgn = A.alloc((1,), F32)
        blb = Buf("lb")
        P.dma(lambda e: e.dma_start(out=lraw, in_=d["hg_lb"]), writes=[blb])
        P.dma(lambda e: e.dma_start(out=hgn, in_=d["hg_norm_g"].rearrange("(p o) -> p o", o=1)), writes=[blb])
        P.dve(lambda e: e.tensor_tensor(out=lb, in0=lraw[:, 0, :], in1=lraw[:, 1, :], op=ALU.subtract),
              reads=[blb], writes=[blb])
        P.act(lambda e: e.activation(out=oml, in_=lb, func=AF.Sigmoid, scale=-1.0), reads=[blb], writes=[blb])
        P.act(lambda e: e.activation(out=lb, in_=lb, func=AF.Sigmoid), reads=[blb], writes=[blb])

        xnT = A.alloc((KC, SBT), BF16)
        bxnT = Buf("xnT")
        Fbs = [A.alloc((H, SBT), F32) for _ in range(2)]
        CS = A.alloc((H, SBT), F32)
        Kb = A.alloc((H, SBT), BF16)
        EB = A.alloc((H, SBT), BF16)
        ENB = A.alloc((H, SBT), BF16)
        EBEs = [A.alloc((H, NCH), F32) for _ in range(2)]
        QTs = [A.alloc((H, SBT), BF16) for _ in range(2)]
        KTs = [A.alloc((H, SBT), BF16) for _ in range(2)]
        KH = A.alloc((H, SBT), BF16)
        KHTs = [A.alloc((NT, 512), BF16) for _ in range(2)]
        Vs = [A.alloc((NT, 512), BF16) for _ in range(2)]
        Gs = [A.alloc((H, SBT), BF16) for _ in range(2)]
        O32 = A.alloc((H, SBT), F32)
        OSQ = A.alloc((H, SBT), BF16)
        LNV = A.alloc((SBT,), F32)
        ATS = A.alloc((H, 128), BF16)
        S32 = A.alloc((H, 128), F32)
        SBF = [A.alloc((H, 128), BF16) for _ in range(2)]
        bFs = [[Buf() for _ in range(H)] for _ in range(2)]
        bCS = [Buf() for _ in range(H)]
        bK = Buf()
        bEB = [Buf() for _ in range(H)]
        bENB = [Buf() for _ in range(H)]
        bEBEs = [Buf(), Buf()]
        bQTs = [[Buf() for _ in range(H)] for _ in range(2)]
        bKTs = [Buf(), Buf()]
        bKH = Buf()
        bKHTs = [[Buf() for _ in range(NT)] for _ in range(2)]
        bVs = [[Buf() for _ in range(NT)] for _ in range(2)]
        bGs = [[Buf() for _ in range(H)] for _ in range(2)]
        bO32 = Buf()
        bOSQ = [Buf() for _ in range(H)]
        bLNV = Buf()
        bATS = Buf()
        bS32 = [Buf() for _ in range(H)]
        bSBF = [[Buf() for _ in range(H)] for _ in range(2)]
        sbf_i = [0] * H

        bk = self.banks
        bb = self.bbank
        ptr = bk[0][:].bitcast(BF16).rearrange("p (k t) -> p k t", k=KC)
        pkt = bk[1][:].bitcast(BF16)[:, 0:512]
        pj = [bk[2][:], bk[3][:]]
        bpj = [bb[2], bb[3]]
        pat = bk[4][:].rearrange("p (h t) -> p h t", h=H)
        po = bk[5][:].rearrange("p (h t) -> p h t", h=H)
        pS = [bk[6 + (h % 2)][:, 0:128] for h in range(H)]
        bpS = [bb[6 + (h % 2)] for h in range(H)]
        pji = [0]

        def nextpj():
            i = pji[0] % 2
            pji[0] += 1
            return pj[i], bpj[i]

        for h in range(H):
            P.pool(lambda e, h=h: e.memset(S32[:, h, :], 0.0), writes=[bS32[h]])
            P.pool(lambda e, h=h: e.memset(SBF[0][:, h, :], 0.0), writes=[bSBF[0][h]])

        def proj_fm(col0, h):
            p, bp = nextpj()
            for k in range(KC):
                P.pe(lambda e, k=k, p=p: e.matmul(p[:, 0:SBT], lhsT=W1[:, k, col0 + h * 128:col0 + (h + 1) * 128],
                                                  rhs=xnT[:, k, :], start=(k == 0), stop=(k == KC - 1)),
                     reads=[bW1[k], bxnT], writes=[bp])
            return p, bp

        def X1a(sb):
            sl = sb % 2
            QT, KT, KHT, V, G, EBE = QTs[sl], KTs[sl], KHTs[sl], Vs[sl], Gs[sl], EBEs[sl]
            bQT, bKT, bKHT, bV, bG, bEBE = bQTs[sl], bKTs[sl], bKHTs[sl], bVs[sl], bGs[sl], bEBEs[sl]
            full = sb >= self.npre
            Fb, bF = Fbs[sb % 2], bFs[sb % 2]
            yield from self.stage_a_gen(sb, xnT, bxnT, ptr, bb[0])
            if "xnT_s" not in self.d:
                self.d["xnT_s"] = self.nc.dram_tensor("xnT_s", [128, KC, self.ntok], BF16, kind="Internal").ap()
                self.bxns = {}
            bx_ = Buf("xns")
            self.bxns[sb] = bx_
            P.dma(lambda e, sb=sb: e.dma_start(out=self.d["xnT_s"][:, :, sb * SBT:(sb + 1) * SBT], in_=xnT),
                  reads=[bxnT], writes=[bx_])
            for h in range(H):
                p, bp = proj_fm(C_HF, h)
                P.act(lambda e, h=h, p=p: e.activation(out=Fb[:, h, :], in_=p[:, 0:SBT], func=AF.Sigmoid),
                      reads=[bp], writes=[bF[h]])
                yield
            if full:
                for h in range(H):
                    p, bp = proj_fm(C_HQ, h)
                    P.act(lambda e, h=h, p=p: e.activation(out=QT[:, h, :], in_=p[:, 0:SBT], func=AF.Silu),
                          reads=[bp], writes=[bQT[h]])
                    yield
                for h in range(H):
                    p, bp = proj_fm(C_HG, h)
                    P.act(lambda e, h=h, p=p: e.activation(out=G[:, h, :], in_=p[:, 0:SBT], func=AF.Silu),
                          reads=[bp], writes=[bG[h]])
                    yield
            for t in range(NT):
                p, bp = nextpj()
                for k in range(KC):
                    P.pe(lambda e, k=k, p=p, t=t: e.matmul(p, lhsT=xnT[:, k, t * 128:(t + 1) * 128],
                                                           rhs=W1[:, k, C_HI:C_HI + 512],
                                                           start=(k == 0), stop=(k == KC - 1)),
                         reads=[bW1[k], bxnT], writes=[bp])
                P.act(lambda e, p=p, t=t: e.activation(out=V[:, t, :], in_=p, func=AF.Copy), reads=[bp], writes=[bV[t]])
                yield
        def X1b(sb):
            sl = sb % 2
            QT, KT, KHT, V, G, EBE = QTs[sl], KTs[sl], KHTs[sl], Vs[sl], Gs[sl], EBEs[sl]
            bQT, bKT, bKHT, bV, bG, bEBE = bQTs[sl], bKTs[sl], bKHTs[sl], bVs[sl], bGs[sl], bEBEs[sl]
            full = sb >= self.npre
            Fb, bF = Fbs[sb % 2], bFs[sb % 2]
            for h in range(H):
                P.dve(lambda e, h=h: e.tensor_scalar(out=Fb[:, h, :], in0=Fb[:, h, :], scalar1=oml[:, h:h + 1],
                                                     scalar2=lb[:, h:h + 1], op0=ALU.mult, op1=ALU.add),
                      reads=[bF[h], blb], writes=[bF[h]])
            P.dve(lambda e: e.tensor_scalar(out=Kb, in0=Fb, scalar1=-1.0, scalar2=1.0, op0=ALU.mult, op1=ALU.add),
                  reads=bF, writes=[bK])
            for h in range(H):
                P.act(lambda e, h=h: e.activation(out=Fb[:, h, :], in_=Fb[:, h, :], func=AF.Ln),
                      reads=[bF[h], bK], writes=[bF[h]])
            for h in range(H):
                P.dve(lambda e, h=h: e.tensor_tensor_scan(out=CS[:, h, :], data0=self.ones_f[:, 0:SBT],
                                                          data1=Fb[:, h, :], initial=0.0,
                                                          op0=ALU.mult, op1=ALU.add),
                      reads=[bF[h], self.bconst], writes=[bCS[h]])
                yield
            if not full:
                for h in range(H):
                    P.act(lambda e, h=h: e.activation(out=ENB[:, h, :], in_=CS[:, h, :], func=AF.Exp, scale=-1.0,
                                                      bias=CS[:, h, SBT - 1:SBT]),
                          reads=[bCS[h]], writes=[bENB[h]])
                    yield
                P.act(lambda e: e.activation(out=EBE[:, :, 0], in_=CS[:, :, SBT - 1], func=AF.Exp), reads=bCS, writes=[bEBE])
                P.dve(lambda e: e.tensor_tensor(out=KH, in0=Kb, in1=ENB, op=ALU.mult), reads=[bK] + bENB, writes=[bKH])
            else:
                Fb4 = Fb.rearrange("p h (c t) -> p h c t", c=NCH)
                CS4 = CS.rearrange("p h (c t) -> p h c t", c=NCH)
                P.dve(lambda e: e.tensor_tensor(out=Fb4[:, :, 1:NCH, :], in0=CS4[:, :, 1:NCH, :],
                                                in1=CS4[:, :, 0:NCH - 1, 63:64].to_broadcast([128, H, NCH - 1, 64]),
                                                op=ALU.subtract),
                      reads=bCS + bF, writes=bF)
                P.dve(lambda e: e.tensor_copy(out=Fb4[:, :, 0, :], in_=CS4[:, :, 0, :]), reads=bCS + bF, writes=bF)
                for h in range(H):
                    P.act(lambda e, h=h: e.activation(out=ENB[:, h, :], in_=Fb[:, h, :], func=AF.Exp, scale=-1.0),
                          reads=[bF[h]], writes=[bENB[h]])
                    yield
                P.act(lambda e: e.activation(out=EBE, in_=Fb4[:, :, :, 63], func=AF.Exp), reads=bF, writes=[bEBE])
                if full:
                    for h in range(H):
                        P.act(lambda e, h=h: e.activation(out=EB[:, h, :], in_=Fb[:, h, :], func=AF.Exp),
                              reads=[bF[h]], writes=[bEB[h]])
                P.dve(lambda e: e.tensor_tensor(out=KT, in0=Kb, in1=ENB, op=ALU.mult), reads=[bK] + bENB, writes=[bKT])
                KT4 = KT.rearrange("p h (c t) -> p h c t", c=NCH)
                KH4 = KH.rearrange("p h (c t) -> p h c t", c=NCH)
                P.dve(lambda e: e.tensor_tensor(out=KH4, in0=KT4,
                                                in1=EBE.unsqueeze(3).to_broadcast([128, H, NCH, 64]), op=ALU.mult),
                      reads=[bKT, bEBE], writes=[bKH])
                if full:
                    P.dve(lambda e: e.tensor_tensor(out=QT, in0=QT, in1=EB, op=ALU.mult), reads=bQT + bEB, writes=bQT)
            for t in range(NT):
                for h in range(H):
                    P.pe(lambda e, h=h, t=t: e.transpose(out=pkt[:, h * 128:(h + 1) * 128],
                                                         in_=KH[:, h, t * 128:(t + 1) * 128], identity=self.ident),
                         reads=[bKH, self.bconst], writes=[bb[1]])
                P.dve(lambda e, t=t: e.tensor_copy(out=KHT[:, t, :], in_=pkt), reads=[bb[1]], writes=[bKHT[t]])
                yield
        def X1(sb):
            yield from X1a(sb)
            yield from X1b(sb)

        def Y1(sb):
            full = sb >= self.npre
            sl = sb % 2
            QT, KT, KHT, V, G, EBE = QTs[sl], KTs[sl], KHTs[sl], Vs[sl], Gs[sl], EBEs[sl]
            bQT, bKT, bKHT, bV, bG, bEBE = bQTs[sl], bKTs[sl], bKHTs[sl], bVs[sl], bGs[sl], bEBEs[sl]
            if not full:
                for h in range(H):
                    for t in range(NT):
                        P.pe(lambda e, h=h, t=t: e.matmul(pS[h], lhsT=KHT[:, t, h * 128:(h + 1) * 128],
                                                          rhs=V[:, t, h * 128:(h + 1) * 128],
                                                          start=(t == 0), stop=(t == NT - 1)),
                             reads=[bKHT[t], bV[t]], writes=[bpS[h]])
                    P.dve(lambda e, h=h: e.scalar_tensor_tensor(
                        out=S32[:, h, :], in0=S32[:, h, :], scalar=EBE[:, h, 0:1], in1=pS[h],
                        op0=ALU.mult, op1=ALU.add),
                        reads=[bS32[h], bEBE, bpS[h]], writes=[bS32[h]])
                    cur = sbf_i[h]
                    nxt = 1 - cur
                    P.act(lambda e, h=h, nxt=nxt: e.activation(out=SBF[nxt][:, h, :], in_=S32[:, h, :], func=AF.Copy),
                          reads=[bS32[h]], writes=[bSBF[nxt][h]])
                    sbf_i[h] = nxt
                    yield
                return
            for j in range(NT):
                c0 = j * 128
                if full:
                    for h in range(H):
                        P.pe(lambda e, h=h, c0=c0: e.matmul(pat[:, h, :], lhsT=KT[:, h, c0:c0 + 128],
                                                            rhs=QT[:, h, c0:c0 + 128], start=True, stop=True),
                             reads=[bKT, bQT[h]], writes=[bb[4]])
                    P.dve(lambda e: e.tensor_tensor(out=ATS, in0=pat,
                                                    in1=self.mask2.unsqueeze(1).to_broadcast([128, H, 128]),
                                                    op=ALU.mult),
                          reads=[bb[4], self.bconst], writes=[bATS])
                    yield
                for half in range(2):
                    ch = 2 * j + half
                    r0 = half * 64
                    for h in range(H):
                        cur = sbf_i[h]
                        if full:
                            P.pe(lambda e, h=h, cur=cur, c0=c0, r0=r0: e.matmul(
                                po[:, h, r0:r0 + 64], lhsT=SBF[cur][:, h, :], rhs=QT[:, h, c0 + r0:c0 + r0 + 64],
                                start=(h == 0 and r0 == 0), stop=False, skip_group_check=True),
                                reads=[bSBF[cur][h], bQT[h]], writes=[bb[5]])
                        P.pe(lambda e, h=h, j=j, r0=r0: e.matmul(
                            pS[h], lhsT=KHT[r0:r0 + 64, j, h * 128:(h + 1) * 128],
                            rhs=V[r0:r0 + 64, j, h * 128:(h + 1) * 128], start=True, stop=True),
                            reads=[bKHT[j], bV[j]], writes=[bpS[h]])
                        P.dve(lambda e, h=h, ch=ch: e.scalar_tensor_tensor(
                            out=S32[:, h, :], in0=S32[:, h, :], scalar=EBE[:, h, ch:ch + 1], in1=pS[h],
                            op0=ALU.mult, op1=ALU.add),
                            reads=[bS32[h], bEBE, bpS[h]], writes=[bS32[h]])
                        nxt = 1 - cur
                        P.act(lambda e, h=h, nxt=nxt: e.activation(out=SBF[nxt][:, h, :], in_=S32[:, h, :], func=AF.Copy),
                              reads=[bS32[h]], writes=[bSBF[nxt][h]])
                        sbf_i[h] = nxt
                        yield
                if full:
                    for h in range(H):
                        P.pe(lambda e, h=h, j=j: e.matmul(po[:, h, :], lhsT=V[:, j, h * 128:(h + 1) * 128],
                                                          rhs=ATS[:, h, :], start=False, stop=True,
                                                          skip_group_check=True),
                             reads=[bV[j], bATS], writes=[bb[5]])
                    P.act(lambda e, c0=c0: e.activation(out=O32[:, :, c0:c0 + 128], in_=po, func=AF.Copy),
                          reads=[bb[5]], writes=[bO32])
                    yield
            if full:
                tok0 = (sb - self.npre) * SBT
                for h in range(H):
                    P.act(lambda e, h=h: e.activation(out=OSQ[:, h, :], in_=O32[:, h, :], func=AF.Square),
                          reads=[bO32], writes=[bOSQ[h]])
                for h in range(H):
                    pss, bpss = bk[4][:], bb[4]
                    P.pe(lambda e, h=h, pss=pss: e.matmul(pss[:, 0:SBT], lhsT=self.ones_bf, rhs=OSQ[:, h, :], start=True, stop=True),
                         reads=[bOSQ[h], self.bconst], writes=[bpss])
                    P.act(lambda e, pss=pss: e.activation(out=LNV, in_=pss[:, 0:SBT], func=AF.Ln, scale=1.0 / 128, bias=EPS),
                          reads=[bpss], writes=[bLNV])
                    P.act(lambda e: e.activation(out=LNV, in_=LNV, func=AF.Exp, scale=-0.5),
                          reads=[bLNV], writes=[bLNV])
                    P.dve(lambda e, h=h: e.tensor_tensor(out=O32[:, h, :], in0=O32[:, h, :], in1=LNV, op=ALU.mult),
                          reads=[bO32, bLNV], writes=[bO32])
                    P.dve(lambda e, h=h: e.scalar_tensor_tensor(
                        out=self.OA[:, h, :], in0=O32[:, h, :], scalar=hgn[:, 0:1], in1=G[:, h, :],
                        op0=ALU.mult, op1=ALU.mult),
                        reads=[bO32, blb, bG[h]], writes=[self.bOA[h]])
                    yield
                self.spill("oa_s", self.OA, self.bOA, sb)
                if "oa" in self.debug:
                    if "dbg_oa" not in self.d:
                        self.dout("dbg_oa", [128, H, self.nfull_tok], BF16)
                    P.dma(lambda e, tok0=tok0: e.dma_start(out=self.d["dbg_oa"][:, :, tok0:tok0 + SBT], in_=self.OA),
                          reads=self.bOA, out=True)
        import os
        WTS1 = [int(v) for v in os.environ.get("IL_W1", "1,1").split(",")]

        def run_il(gens):
            gens = list(gens)
            while gens:
                for g_, w_ in list(gens):
                    for _ in range(w_):
                        try:
                            next(g_)
                        except StopIteration:
                            gens.remove((g_, w_))
                            break

        n = self.nsb
        if LS >= 2:
            sa = [P._capture(X1a(i)) for i in range(n)]
            sb_ = [P._capture(X1b(i)) for i in range(n)]
            sy = [P._capture(Y1(i)) for i in range(n)]
            P.merge_pipeline([sa, sb_, sy],
                             [lambda s_, dn: dn[2] >= s_ - 1,
                              lambda s_, dn: dn[0] >= s_ + 1 and dn[2] >= s_ - 1,
                              lambda s_, dn: dn[1] >= s_ + 1])
        else:
            for r in range(-1, n):
                gs = []
                if 0 <= r < n:
                    gs.append((Y1(r), WTS1[0]))
                if 0 <= r + 1 < n:
                    gs.append((X1(r + 1), WTS1[1]))
                if LS:
                    P.merge_streams([g_ for g_, _w in gs])
                else:
                    run_il(gs)
        A.reset(m0)
        P.barrier()

    def finish(self):
        self.P.finalize(self.nc)
        self.st.close()
        return self.nc


def _pass2(self):
    nc, P, A = self.nc, self.P, self.A
    d = self.d
    H = 4
    self.din("conv_wT", [128, 4, 12])
    self.din("gd_A_log", [4])
    self.din("gd_dt_bias", [4])
    self.din("gd_norm_g", [128])
    m0 = A.mark()
    self.OB = A.alloc((H, SBT), BF16)
    self.bOB = [Buf("OB%d" % h) for h in range(H)]
    NW = 2056
    if hasattr(self, "W2"):
        W2, bW2 = self.W2, self.bW2
    else:
        W2 = A.alloc((KC, NW), BF16)
        bW2 = [Buf("W2_%d" % k) for k in range(KC)]
        wv = d["w_in"].rearrange("(k p) n -> p k n", p=128)
        for k in range(KC):
            P.dma(lambda e, k=k: e.dma_start(out=W2[:, k, :], in_=wv[:, k, 2048:2048 + NW]), writes=[bW2[k]], q="pool")
    self.load_gain("norm_mix_g")
    cw = A.alloc((4, 12), F32)
    negA = A.alloc((H,), F32)
    dtb = A.alloc((H,), F32)
    gdn = A.alloc((1,), F32)
    bpar = Buf("par2")
    P.dma(lambda e: e.dma_start(out=cw, in_=d["conv_wT"]), writes=[bpar])
    P.dma(lambda e: e.dma_start(out=negA, in_=d["gd_A_log"].partition_broadcast(128)), writes=[bpar])
    P.dma(lambda e: e.dma_start(out=dtb, in_=d["gd_dt_bias"].partition_broadcast(128)), writes=[bpar])
    P.dma(lambda e: e.dma_start(out=gdn, in_=d["gd_norm_g"].rearrange("(p o) -> p o", o=1)), writes=[bpar])
    P.act(lambda e: e.activation(out=negA, in_=negA, func=AF.Exp), reads=[bpar], writes=[bpar])
    P.dve(lambda e: e.tensor_scalar(out=negA, in0=negA, scalar1=-1.0, scalar2=None, op0=ALU.mult),
          reads=[bpar], writes=[bpar])
    maskL = A.alloc((128,), F32)
    ch01 = A.alloc((2, 128), F32)
    bc = self.bconst
    P.pool(lambda e: e.memset(maskL, 1.0), writes=[bc])
    P.pool(lambda e: e.affine_select(out=maskL, in_=maskL, pattern=[[-1, 128]], compare_op=ALU.is_gt,
                                     fill=0.0, base=0, channel_multiplier=1), reads=[bc], writes=[bc])
    P.pool(lambda e: e.memset(maskL[64:128, 0:64], 0.0), reads=[bc], writes=[bc])
    bones = A.alloc((128,), F32)
    P.pool(lambda e: e.memset(bones, 0.0), writes=[bc])
    P.pool(lambda e: e.memset(bones[0:64, 0:64], 1.0), reads=[bc], writes=[bc])
    P.pool(lambda e: e.memset(bones[64:128, 64:128], 1.0), reads=[bc], writes=[bc])
    P.pool(lambda e: e.memset(ch01, 0.0), writes=[bc])
    P.pool(lambda e: e.memset(ch01[0:64, 0, :], 1.0), reads=[bc], writes=[bc])
    P.pool(lambda e: e.memset(ch01[64:128, 1, :], 1.0), reads=[bc], writes=[bc])

    xnTs = [A.alloc((KC, SBT), BF16) for _ in range(2)]
    bxnTs = [Buf("xnT0"), Buf("xnT1")]
    self._xfetched = set()
    use_fetch = hasattr(self, "bxns")
    XC = A.alloc((12, SBT + 3), BF16)
    DG = A.alloc((48, 128), BF16)
    bDG = Buf("DG")
    for _j in range(4):
        for _cb in range(12):
            P.dve(lambda e, _j=_j, _cb=_cb: e.tensor_scalar(out=DG[:, _j * 12 + _cb, :], in0=self.identf,
                                                           scalar1=cw[:, _j, _cb:_cb + 1], scalar2=None, op0=ALU.mult),
                  reads=[bpar, self.bconst], writes=[bDG])
    bXC = [Buf() for _ in range(12)]
    CV = [A.alloc((SBT,), F32) for _ in range(2)]
    bCV = [Buf(), Buf()]
    QK32 = A.alloc((8, SBT), F32)
    bQK32 = [Buf() for _ in range(8)]
    NQ = 4
    SQs = [A.alloc((SBT,), BF16) for _ in range(NQ)]
    bSQs = [Buf() for _ in range(NQ)]
    RSs = [A.alloc((SBT,), F32) for _ in range(NQ)]
    bRSs = [Buf() for _ in range(NQ)]
    sqi = [0]
    NS = 3
    QTs = [A.alloc((H, SBT), BF16) for _ in range(NS)]
    KTs = [A.alloc((H, SBT), BF16) for _ in range(NS)]
    VTs = [A.alloc((H, SBT), BF16) for _ in range(NS)]
    GZs = [A.alloc((H, SBT), BF16) for _ in range(NS)]
    bQTs = [[Buf() for _ in range(H)] for _ in range(NS)]
    bKTs = [[Buf() for _ in range(H)] for _ in range(NS)]
    bVTs = [[Buf() for _ in range(H)] for _ in range(NS)]
    bGZs = [[Buf() for _ in range(H)] for _ in range(NS)]
    BGraws = [A.alloc((NT, 8), F32) for _ in range(NS)]
    LNBs = [A.alloc((NT, H), F32) for _ in range(NS)]
    BETAs = [A.alloc((NT, H), F32) for _ in range(NS)]
    GGs = [A.alloc((NT, H), F32) for _ in range(NS)]
    bBGs = [Buf() for _ in range(NS)]
    PBUF = []
    for _j in range(NT):
        pb = dict(
            GB=A.alloc((H, 128), F32), bGB=Buf(),
            E1=A.alloc((H, 128), F32), bE1=Buf(),
            E2=A.alloc((H, 128), F32), bE2=Buf(),
            EGR=A.alloc((H, 128), BF16), bEGR=Buf(),
            Lm=[A.alloc((H, 128), BF16) for _ in range(2)], bLm=[Buf(), Buf()],
            Um=[A.alloc((H, 128), BF16) for _ in range(2)], bUm=[Buf(), Buf()],
            Xm=[A.alloc((H, 128), BF16) for _ in range(2)], bXm=[Buf(), Buf()],
            VTK=A.alloc((H, 128), BF16), bVTK=Buf(),
        )
        PBUF.append(pb)
    PCAR = []
    for _s in range(2):
        row = []
        for _j in range(NT):
            row.append(dict(
                SC=A.alloc((8, H), F32), bSC=Buf(),
                QTG=A.alloc((H, 128), BF16), bQTG=Buf(),
                TT=A.alloc((H, 128), BF16), bTT=Buf(),
                ATT=A.alloc((H, 128), BF16), bATT=Buf(),
                KHT=A.alloc((H, 128), BF16), bKHT=Buf(),
                KTP=A.alloc((H, 128), BF16), bKTP=Buf(),
                BV=A.alloc((H, 128), F32), bBV=Buf(),
            ))
        PCAR.append(row)
    R = A.alloc((H, 128), BF16)
    USB = A.alloc((H, 128), BF16)
    bR = Buf()
    bUSB = Buf()
    S32 = A.alloc((H, 128), F32)
    SBF = [A.alloc((H, 128), BF16) for _ in range(2)]
    bS32 = [Buf() for _ in range(H)]
    bSBF = [[Buf() for _ in range(H)] for _ in range(2)]
    sbf_i = [0]
    O32 = A.alloc((H, SBT), F32)
    bO32 = Buf()
    OSQ = A.alloc((H, SBT), BF16)
    bOSQ = [Buf() for _ in range(H)]
    LNV = A.alloc((SBT,), F32)
    bLNV = Buf()
    identb4 = A.alloc((H, 128), BF16)
    P.pool(lambda e: e.tensor_copy(out=identb4, in_=self.ident.unsqueeze(1).to_broadcast([128, H, 128])),
           reads=[bc], writes=[bc])

    bk, bb = self.banks, self.bbank
    ptr = bk[0][:].bitcast(BF16).rearrange("p (k t) -> p k t", k=KC)
    ptr4 = bk[0][:].bitcast(BF16)[:, 0:512].rearrange("p (h t) -> p h t", h=H)
    import os as _os
    DUM = int(_os.environ.get("PE_DUMMY", "0"))
    DUMN = int(_os.environ.get("PE_DUMMY_N", "128"))
    if DUM > 0:
        pj = [bk[1][:], bk[2][:]]
        bpj = [bb[1], bb[2]]
        dones = A.alloc((512,), BF16)
        P.pool(lambda e: e.memset(dones, 1.0), writes=[self.bconst])
        dbuf = Buf("dummy", psum=True)
        P.pe_dummy = (lambda e: e.matmul(bk[0][:, 0:DUMN], lhsT=self.ones_bf, rhs=dones[:, 0:DUMN], start=True, stop=True),
                      DUM, dbuf)
    else:
        pj = [bk[1][:], bk[2][:], bk[0][:]]
        bpj = [bb[1], bb[2], bb[0]]
    pA = bk[3][:].rearrange("p (h t) -> p h t", h=H)
    pB = bk[4][:].rearrange("p (h t) -> p h t", h=H)
    pBb = bk[4][:].bitcast(BF16)[:, 0:512].rearrange("p (h t) -> p h t", h=H)
    pC = bk[5][:].rearrange("p (h t) -> p h t", h=H)
    pR = bk[6][:].rearrange("p (h t) -> p h t", h=H)
    po = bk[7][:].rearrange("p (h t) -> p h t", h=H)
    pji = [0]

    def nextpj():
        i = pji[0] % len(pj)
        pji[0] += 1
        return pj[i], bpj[i]

    for h in range(H):
        P.pool(lambda e, h=h: e.memset(S32[:, h, :], 0.0), writes=[bS32[h]])
        P.pool(lambda e, h=h: e.memset(SBF[0][:, h, :], 0.0), writes=[bSBF[0][h]])
    for cb in range(12):
        P.pool(lambda e, cb=cb: e.memset(XC[:, cb, :], 0.0), writes=[bXC[cb]])

    def proj_fm(col0, xnT, bxnT):
        p, bp = nextpj()
        for k in range(KC):
            P.pe(lambda e, k=k, p=p: e.matmul(p[:, 0:SBT], lhsT=W2[:, k, col0:col0 + 128],
                                              rhs=xnT[:, k, :], start=(k == 0), stop=(k == KC - 1)),
                 reads=[bW2[k], bxnT], writes=[bp])
        return p, bp

    evi = [0]

    def evac(out, in_, reads, writes):
        i = evi[0]
        evi[0] += 1
        if i % 3 != 2:
            P.act(lambda e: e.activation(out=out, in_=in_, func=AF.Copy), reads=reads, writes=writes)
        else:
            P.dve(lambda e: e.tensor_copy(out=out, in_=in_), reads=reads, writes=writes)

    def X(sb):
        sl = sb % 3
        QT, KT, VT, GZ = QTs[sl], KTs[sl], VTs[sl], GZs[sl]
        bQT, bKT, bVT, bGZ = bQTs[sl], bKTs[sl], bVTs[sl], bGZs[sl]
        BGraw, LNB, BETA, GG, bBG = BGraws[sl], LNBs[sl], BETAs[sl], GGs[sl], bBGs[sl]
        full = sb >= self.npre
        xnT, bxnT = xnTs[sb % 2], bxnTs[sb % 2]
        if use_fetch:
            yield from self.xnt_fetch_gen(sb, self.nsb - 1, xnTs, bxnTs)
        else:
            yield from self.stage_a_gen(sb, xnT, bxnT, ptr, bb[0])
        cbs = list(range(12)) if full else list(range(4, 12))
        cbs_proj = list(range(12)) if sb >= self.npre - 1 else list(range(4, 12))
        for cb in cbs_proj:
            if sb > 0:
                P.pool(lambda e, cb=cb: e.tensor_copy(out=XC[:, cb, 0:3], in_=XC[:, cb, SBT:SBT + 3]),
                       reads=[bXC[cb]], writes=[bXC[cb]])
            p, bp = proj_fm(cb * 128, xnT, bxnT)
            evac(XC[:, cb, 3:SBT + 3], p[:, 0:SBT], [bp], [bXC[cb]])
            yield
        pbg, bpbg = nextpj()
        for t in range(NT):
            for k in range(KC):
                P.pe(lambda e, k=k, t=t, pbg=pbg: e.matmul(pbg[:, t * 8:(t + 1) * 8], lhsT=xnT[:, k, t * 128:(t + 1) * 128],
                                                  rhs=W2[:, k, 2048:2056], start=(k == 0), stop=(k == KC - 1)),
                     reads=[bW2[k], bxnT], writes=[bpbg])
        P.act(lambda e, pbg=pbg: e.activation(out=BGraw, in_=pbg[:, 0:NT * 8].rearrange("p (t c) -> p t c", t=NT), func=AF.Copy),
              reads=[bpbg], writes=[bBG])
        P.act(lambda e: e.activation(out=LNB, in_=BGraw[:, :, 0:4], func=AF.Exp, scale=-1.0), reads=[bBG], writes=[bBG])
        P.act(lambda e: e.activation(out=LNB, in_=LNB, func=AF.Ln, bias=1.0), reads=[bBG], writes=[bBG])
        P.dve(lambda e: e.tensor_scalar(out=LNB, in0=LNB, scalar1=-1.0, scalar2=None, op0=ALU.mult),
              reads=[bBG], writes=[bBG])
        P.act(lambda e: e.activation(out=BETA, in_=LNB, func=AF.Exp), reads=[bBG], writes=[bBG])
        P.dve(lambda e: e.tensor_tensor(out=GG, in0=BGraw[:, :, 4:8], in1=dtb.unsqueeze(1).to_broadcast([128, NT, H]),
                                        op=ALU.add), reads=[bBG, bpar], writes=[bBG])
        P.act(lambda e: e.activation(out=GG, in_=GG, func=AF.Exp), reads=[bBG], writes=[bBG])
        P.act(lambda e: e.activation(out=GG, in_=GG, func=AF.Ln, bias=1.0), reads=[bBG], writes=[bBG])
        P.dve(lambda e: e.tensor_tensor(out=GG, in0=GG, in1=negA.unsqueeze(1).to_broadcast([128, NT, H]),
                                        op=ALU.mult), reads=[bBG, bpar], writes=[bBG])
        yield
        if full:
            for h in range(H):
                p, bp = proj_fm(1536 + h * 128, xnT, bxnT)
                P.act(lambda e, h=h, p=p: e.activation(out=GZ[:, h, :], in_=p[:, 0:SBT], func=AF.Silu),
                      reads=[bp], writes=[bGZ[h]])
                yield
        for n, cb in enumerate(cbs):
            cv, bcv = nextpj()
            for j in range(4):
                P.pe(lambda e, cb=cb, cv=cv, j=j: e.matmul(cv[:, 0:SBT], lhsT=DG[:, j * 12 + cb, :], rhs=XC[:, cb, j:SBT + j],
                                                           start=(j == 0), stop=(j == 3)),
                     reads=[bXC[cb], bDG], writes=[bcv])
            if cb < 8:
                P.act(lambda e, cb=cb, cv=cv: e.activation(out=QK32[:, cb, :], in_=cv[:, 0:SBT], func=AF.Silu),
                      reads=[bcv], writes=[bQK32[cb]])
            else:
                P.act(lambda e, cb=cb, cv=cv: e.activation(out=VT[:, cb - 8, :], in_=cv[:, 0:SBT], func=AF.Silu),
                      reads=[bcv], writes=[bVT[cb - 8]])
            yield
        for cb in cbs:
            if cb >= 8:
                continue
            SQ, bSQ, RS, bRS = SQs[sqi[0] % NQ], bSQs[sqi[0] % NQ], RSs[sqi[0] % NQ], bRSs[sqi[0] % NQ]
            sqi[0] += 1
            P.pool(lambda e, cb=cb, SQ=SQ: e.tensor_tensor(out=SQ, in0=QK32[:, cb, :], in1=QK32[:, cb, :], op=ALU.mult),
                   reads=[bQK32[cb]], writes=[bSQ])
            p, bp = nextpj()
            P.pe(lambda e, p=p, SQ=SQ: e.matmul(p[:, 0:SBT], lhsT=self.ones_bf, rhs=SQ, start=True, stop=True),
                 reads=[bSQ, bc], writes=[bp])
            P.act(lambda e, p=p, RS=RS: e.activation(out=RS, in_=p[:, 0:SBT], func=AF.Ln, bias=EPS), reads=[bp], writes=[bRS])
            qbias = -0.5 * float(np.log(128.0)) if cb < 4 else 0.0
            P.act(lambda e, qbias=qbias, RS=RS: e.activation(out=RS, in_=RS, func=AF.Exp, scale=-0.5, bias=qbias),
                  reads=[bRS], writes=[bRS])
            dst, bdst = (QT[:, cb, :], bQT[cb]) if cb < 4 else (KT[:, cb - 4, :], bKT[cb - 4])
            P.pool(lambda e, cb=cb, dst=dst, RS=RS: e.tensor_tensor(out=dst, in0=QK32[:, cb, :], in1=RS, op=ALU.mult),
                   reads=[bQK32[cb], bRS], writes=[bdst])
            yield
    def Yp(sb):
        sl = sb % 3
        full = sb >= self.npre
        QT, KT, VT, GZ = QTs[sl], KTs[sl], VTs[sl], GZs[sl]
        bQT, bKT, bVT, bGZ = bQTs[sl], bKTs[sl], bVTs[sl], bGZs[sl]
        BGraw, LNB, BETA, GG, bBG = BGraws[sl], LNBs[sl], BETAs[sl], GGs[sl], bBGs[sl]
        LL = dict(L0)
        LL['PB'] = [dict(PBUF[j], **PCAR[sb % 2][j]) for j in range(NT)]
        LL.update(QT=QT, KT=KT, VT=VT, GZ=GZ, bQT=bQT, bKT=bKT, bVT=bVT, bGZ=bGZ, LNB=LNB, BETA=BETA, GG=GG, bBG=bBG)
        yield from self._gdn_prep(LL, full)
    def Yr(sb):
        sl = sb % 3
        full = sb >= self.npre
        QT, KT, VT, GZ = QTs[sl], KTs[sl], VTs[sl], GZs[sl]
        bQT, bKT, bVT, bGZ = bQTs[sl], bKTs[sl], bVTs[sl], bGZs[sl]
        BGraw, LNB, BETA, GG, bBG = BGraws[sl], LNBs[sl], BETAs[sl], GGs[sl], bBGs[sl]
        LL = dict(L0)
        LL['PB'] = [dict(PBUF[j], **PCAR[sb % 2][j]) for j in range(NT)]
        LL.update(QT=QT, KT=KT, VT=VT, GZ=GZ, bQT=bQT, bKT=bKT, bVT=bVT, bGZ=bGZ, LNB=LNB, BETA=BETA, GG=GG, bBG=bBG)
        yield from self._gdn_rec(LL, full)
        if full:
            tok0 = (sb - self.npre) * SBT
            for h in range(H):
                P.act(lambda e, h=h: e.activation(out=OSQ[:, h, :], in_=O32[:, h, :], func=AF.Square),
                      reads=[bO32], writes=[bOSQ[h]])
            for h in range(H):
                pss, bpss = bk[5][:], bb[5]
                P.pe(lambda e, h=h, pss=pss: e.matmul(pss[:, 0:SBT], lhsT=self.ones_bf, rhs=OSQ[:, h, :], start=True, stop=True),
                     reads=[bOSQ[h], bc], writes=[bpss])
                P.act(lambda e, pss=pss: e.activation(out=LNV, in_=pss[:, 0:SBT], func=AF.Ln, scale=1.0 / 128, bias=EPS),
                      reads=[bpss], writes=[bLNV])
                P.act(lambda e: e.activation(out=LNV, in_=LNV, func=AF.Exp, scale=-0.5), reads=[bLNV], writes=[bLNV])
                P.dve(lambda e, h=h: e.tensor_tensor(out=O32[:, h, :], in0=O32[:, h, :], in1=LNV, op=ALU.mult),
                      reads=[bO32, bLNV], writes=[bO32])
                P.dve(lambda e, h=h: e.scalar_tensor_tensor(
                    out=self.OB[:, h, :], in0=O32[:, h, :], scalar=gdn[:, 0:1], in1=GZ[:, h, :],
                    op0=ALU.mult, op1=ALU.mult), reads=[bO32, bpar, bGZ[h]], writes=[self.bOB[h]])
                yield
            self.spill("ob_s", self.OB, self.bOB, sb)
            if "ob" in self.debug:
                if "dbg_ob" not in self.d:
                    self.dout("dbg_ob", [128, H, self.nfull_tok], BF16)
                P.dma(lambda e, tok0=tok0: e.dma_start(out=self.d["dbg_ob"][:, :, tok0:tok0 + SBT], in_=self.OB),
                      reads=self.bOB, out=True)
    L0 = dict(locals())

    import os
    WTS = [int(v) for v in os.environ.get("IL_W", "1,2,2").split(",")]

    def run_il(gens):
        gens = list(gens)
        while gens:
            for g_, w_ in list(gens):
                for _ in range(w_):
                    try:
                        next(g_)
                    except StopIteration:
                        gens.remove((g_, w_))
                        break

    n = self.nsb
    if LS >= 2:
        sx = [P._capture(X(i)) for i in range(n)]
        sp_ = [P._capture(Yp(i)) for i in range(n)]
        sr = [P._capture(Yr(i)) for i in range(n)]
        P.merge_pipeline([sx, sp_, sr],
                         [lambda s_, dn: dn[2] >= s_ - 2,
                          lambda s_, dn: dn[0] >= s_ + 1 and dn[2] >= s_ - 1,
                          lambda s_, dn: dn[1] >= s_ + 1],
                         bias=[0.0, 0.0, float(_os.environ.get("YR_BIAS", "0"))])
    else:
        for r in range(-2, n):
            gs = []
            if 0 <= r < n:
                gs.append((Yr(r), WTS[0]))
            if 0 <= r + 1 < n:
                gs.append((Yp(r + 1), WTS[1]))
            if 0 <= r + 2 < n:
                gs.append((X(r + 2), WTS[2]))
            if LS:
                P.merge_streams([g_ for g_, _w in gs])
            else:
                run_il(gs)
    P.pe_dummy = None
    A.reset(m0)
    P.barrier()


KB.pass2 = _pass2


def _gdn_prep(self, L, full):
    P = self.P
    H = 4
    g = lambda n: L[n]
    bk, bb = self.banks, self.bbank
    bc = self.bconst
    mask2, ident = self.mask2, self.ident
    maskL, bones, ch01, identb4 = g("maskL"), g("bones"), g("ch01"), g("identb4")
    PBUF, GG, LNB, BETA, bBG = g("PB"), g("GG"), g("LNB"), g("BETA"), g("bBG")
    QT, KT, VT, bQT, bKT, bVT = g("QT"), g("KT"), g("VT"), g("bQT"), g("bKT"), g("bVT")
    R, USB, bR, bUSB = g("R"), g("USB"), g("bR"), g("bUSB")
    S32, SBF, bS32, bSBF, sbf_i = g("S32"), g("SBF"), g("bS32"), g("bSBF"), g("sbf_i")
    O32, bO32 = g("O32"), g("bO32")
    evac, nextpj = g("evac"), g("nextpj")
    NP = NT
    pP = [bk[3 + j][:].rearrange("p (h t) -> p h t", h=H) for j in range(NP)]
    pPb = [bk[3 + j][:].bitcast(BF16)[:, 0:512].rearrange("p (h t) -> p h t", h=H) for j in range(NP)]
    bpP = [bb[3 + j] for j in range(NP)]
    ptr4 = bk[5][:].bitcast(BF16)[:, 0:512].rearrange("p (h t) -> p h t", h=H)
    pKS = bk[5][:].rearrange("p (h t) -> p h t", h=H)
    pU = bk[6][:].rearrange("p (h t) -> p h t", h=H)
    po = bk[7][:].rearrange("p (h t) -> p h t", h=H)

    def bc4(ap):
        return ap.unsqueeze(2).to_broadcast([128, H, 128])

    for j in range(NP):
        pb = PBUF[j]
        SC, bSC = pb["SC"], pb["bSC"]
        ps = bk[3 + j][:, 0:16]
        gj = GG[:, j, :]
        for n, lhs in enumerate((mask2, bones, ch01[:, 0, :], ch01[:, 1, :])):
            P.pe(lambda e, n=n, lhs=lhs, ps=ps, gj=gj: e.matmul(ps[:, 4 * n:4 * n + 4], lhsT=lhs, rhs=gj,
                                                                 start=True, stop=True),
                 reads=[bBG, bc], writes=[bpP[j]])
        P.act(lambda e, SC=SC, ps=ps: e.activation(out=SC[:, 0, :], in_=ps[:, 0:4], func=AF.Copy),
              reads=[bpP[j]], writes=[bSC])
        P.dve(lambda e, SC=SC, ps=ps: e.tensor_tensor(out=SC[:, 5, :], in0=ps[:, 4:8], in1=SC[:, 0, :], op=ALU.subtract),
              reads=[bpP[j], bSC], writes=[bSC])
        P.act(lambda e, SC=SC, ps=ps: e.activation(out=SC[:, 6:8, :], in_=ps[:, 8:16].rearrange("p (a h) -> p a h", a=2),
                                                  func=AF.Exp), reads=[bpP[j]], writes=[bSC])
        P.dve(lambda e, SC=SC: e.tensor_scalar(out=SC[:, 1, :], in0=SC[:, 0, :], scalar1=-1.0, scalar2=None, op0=ALU.mult),
              reads=[bSC], writes=[bSC])
        P.dve(lambda e, SC=SC, j=j: e.tensor_tensor(out=SC[:, 2, :], in0=SC[:, 0, :], in1=LNB[:, j, :], op=ALU.add),
              reads=[bSC, bBG], writes=[bSC])
        P.act(lambda e, SC=SC: e.activation(out=SC[:, 3, :], in_=SC[:, 0, :], func=AF.Exp), reads=[bSC], writes=[bSC])
        P.act(lambda e, SC=SC: e.activation(out=SC[:, 5, :], in_=SC[:, 5, :], func=AF.Exp), reads=[bSC], writes=[bSC])
        P.dve(lambda e, SC=SC, j=j: e.scalar_tensor_tensor(out=SC[:, 4, :], in0=SC[:, 3, :], scalar=-1.0,
                                                          in1=BETA[:, j, :], op0=ALU.mult, op1=ALU.mult),
              reads=[bSC, bBG], writes=[bSC])
        yield
    for j in range(NP):
        pb = PBUF[j]
        P.pool(lambda e, pb=pb, j=j: e.tensor_copy(out=pb["GB"], in_=bc4(GG[:, j, :])), reads=[bBG], writes=[pb["bGB"]])
        yield
    for j in range(NP):
        pb = PBUF[j]
        for h in range(H):
            P.pe(lambda e, pb=pb, j=j, h=h: e.matmul(pP[j][:, h, :], lhsT=pb["GB"][:, h, :], rhs=mask2,
                                                     start=True, stop=True),
                 reads=[pb["bGB"], bc], writes=[bpP[j]])
        yield
    for j in range(NP):
        pb = PBUF[j]
        SC = pb["SC"]
        P.dve(lambda e, pb=pb, j=j, SC=SC: e.tensor_tensor(out=pb["E1"], in0=pP[j], in1=bc4(SC[:, 0, :]), op=ALU.max),
              reads=[bpP[j], pb["bSC"]], writes=[pb["bE1"]])
        if full:
            P.dve(lambda e, pb=pb, j=j, SC=SC: e.tensor_tensor(out=pb["E2"], in0=pP[j], in1=bc4(SC[:, 0, :]), op=ALU.min),
                  reads=[bpP[j], pb["bSC"]], writes=[pb["bE2"]])
            P.act(lambda e, pb=pb, j=j: e.activation(out=pb["EGR"], in_=pP[j], func=AF.Exp),
                  reads=[bpP[j]], writes=[pb["bEGR"]])
        yield
    for j in range(NP):
        pb = PBUF[j]
        SC = pb["SC"]
        for h in range(H):
            P.act(lambda e, pb=pb, h=h, SC=SC: e.activation(out=pb["E1"][:, h, :], in_=pb["E1"][:, h, :], func=AF.Exp,
                                                            scale=-1.0, bias=SC[:, 2, h:h + 1]),
                  reads=[pb["bE1"], pb["bSC"]], writes=[pb["bE1"]])
        if full:
            for h in range(H):
                P.act(lambda e, pb=pb, h=h, SC=SC: e.activation(out=pb["E2"][:, h, :], in_=pb["E2"][:, h, :], func=AF.Exp,
                                                                bias=SC[:, 1, h:h + 1]),
                      reads=[pb["bE2"], pb["bSC"]], writes=[pb["bE2"]])
        yield
    for j in range(NP):
        pb = PBUF[j]
        P.pool(lambda e, pb=pb: e.tensor_tensor(out=pb["E1"], in0=pb["E1"],
                                                in1=maskL.unsqueeze(1).to_broadcast([128, H, 128]), op=ALU.mult),
               reads=[pb["bE1"], bc], writes=[pb["bE1"]])
        if full:
            P.pool(lambda e, pb=pb: e.tensor_tensor(out=pb["E2"], in0=pb["E2"],
                                                    in1=mask2.unsqueeze(1).to_broadcast([128, H, 128]), op=ALU.mult),
                   reads=[pb["bE2"], bc], writes=[pb["bE2"]])
            c0 = j * 128
            P.dve(lambda e, pb=pb, c0=c0: e.tensor_tensor(out=pb["QTG"], in0=QT[:, :, c0:c0 + 128], in1=pb["EGR"], op=ALU.mult),
                  reads=bQT + [pb["bEGR"]], writes=[pb["bQTG"]])
        yield
    for j in range(NP):
        c0 = j * 128
        for h in range(H):
            P.pe(lambda e, j=j, h=h, c0=c0: e.matmul(pP[j][:, h, :], lhsT=KT[:, h, c0:c0 + 128], rhs=KT[:, h, c0:c0 + 128],
                                                     start=True, stop=True), reads=[bKT[h]], writes=[bpP[j]])
        yield
    for j in range(NP):
        pb = PBUF[j]
        P.dve(lambda e, pb=pb, j=j: e.tensor_tensor(out=pb["Lm"][0], in0=pP[j], in1=pb["E1"], op=ALU.mult),
              reads=[bpP[j], pb["bE1"]], writes=[pb["bLm"][0]])
        yield
    if full:
        for j in range(NP):
            c0 = j * 128
            for h in range(H):
                P.pe(lambda e, j=j, h=h, c0=c0: e.matmul(pP[j][:, h, :], lhsT=KT[:, h, c0:c0 + 128],
                                                         rhs=QT[:, h, c0:c0 + 128], start=True, stop=True),
                     reads=[bKT[h], bQT[h]], writes=[bpP[j]])
            yield
        for j in range(NP):
            pb = PBUF[j]
            P.dve(lambda e, pb=pb, j=j: e.tensor_tensor(out=pb["ATT"], in0=pP[j], in1=pb["E2"], op=ALU.mult),
                  reads=[bpP[j], pb["bE2"]], writes=[pb["bATT"]])
            yield
    for j in range(NP):
        pb = PBUF[j]
        for h in range(H):
            P.pe(lambda e, pb=pb, j=j, h=h: e.transpose(out=pPb[j][:, h, :], in_=pb["Lm"][0][:, h, :], identity=ident),
                 reads=[pb["bLm"][0], bc], writes=[bpP[j]])
        yield
    for j in range(NP):
        pb = PBUF[j]
        evac(pb["Um"][0], pPb[j], [bpP[j]], [pb["bUm"][0]])
        yield
    for j in range(NP):
        pb = PBUF[j]
        P.pool(lambda e, pb=pb: e.tensor_tensor(out=pb["Xm"][0], in0=identb4, in1=pb["Um"][0], op=ALU.subtract),
               reads=[pb["bUm"][0], bc], writes=[pb["bXm"][0]])
        yield
    cur, cx = 0, 0
    for lvl in range(5):
        for j in range(NP):
            pb = PBUF[j]
            for h in range(H):
                P.pe(lambda e, pb=pb, j=j, h=h, cur=cur: e.matmul(pP[j][:, h, :], lhsT=pb["Um"][cur][:, h, :],
                                                                  rhs=pb["Lm"][cur][:, h, :], start=True, stop=True),
                     reads=[pb["bUm"][cur], pb["bLm"][cur]], writes=[bpP[j]])
            yield
        for j in range(NP):
            pb = PBUF[j]
            evac(pb["Lm"][1 - cur], pP[j], [bpP[j]], [pb["bLm"][1 - cur]])
            yield
        if lvl < 4:
            for j in range(NP):
                pb = PBUF[j]
                for h in range(H):
                    P.pe(lambda e, pb=pb, j=j, h=h, cur=cur: e.matmul(pP[j][:, h, :], lhsT=pb["Lm"][cur][:, h, :],
                                                                      rhs=pb["Um"][cur][:, h, :], start=True, stop=True),
                         reads=[pb["bUm"][cur], pb["bLm"][cur]], writes=[bpP[j]])
                yield
            for j in range(NP):
                pb = PBUF[j]
                evac(pb["Um"][1 - cur], pP[j], [bpP[j]], [pb["bUm"][1 - cur]])
                yield
        for j in range(NP):
            pb = PBUF[j]
            for h in range(H):
                P.pe(lambda e, pb=pb, j=j, h=h, cur=cur, cx=cx: e.matmul(pP[j][:, h, :], lhsT=pb["Lm"][1 - cur][:, h, :],
                                                                         rhs=pb["Xm"][cx][:, h, :], start=True, stop=True),
                     reads=[pb["bLm"][1 - cur], pb["bXm"][cx]], writes=[bpP[j]])
            yield
        for j in range(NP):
            pb = PBUF[j]
            xo, bxo = (pb["TT"], pb["bTT"]) if lvl == 4 else (pb["Xm"][1 - cx], pb["bXm"][1 - cx])
            P.dve(lambda e, pb=pb, j=j, cx=cx, xo=xo: e.tensor_tensor(out=xo, in0=pP[j], in1=pb["Xm"][cx], op=ALU.add),
                  reads=[bpP[j], pb["bXm"][cx]], writes=[bxo])
            yield
        cur, cx = 1 - cur, 1 - cx
    for j in range(NP):
        pb = PBUF[j]
        c0 = j * 128
        SC = pb["SC"]
        for h in range(H):
            P.pe(lambda e, h=h, c0=c0, j=j: e.transpose(out=pPb[j][:, h, :], in_=KT[:, h, c0:c0 + 128], identity=ident),
                 reads=[bKT[h], bc], writes=[bpP[j]])
        P.dve(lambda e, pb=pb, SC=SC, j=j: e.tensor_tensor(out=pb["KHT"], in0=pPb[j], in1=bc4(SC[:, 5, :]), op=ALU.mult),
              reads=[bpP[j], pb["bSC"]], writes=[pb["bKHT"]])
        for h in range(H):
            P.pe(lambda e, h=h, c0=c0, j=j: e.transpose(out=pPb[j][:, h, :], in_=VT[:, h, c0:c0 + 128], identity=ident),
                 reads=[bVT[h], bc], writes=[bpP[j]])
        P.act(lambda e, pb=pb, j=j: e.activation(out=pb["VTK"], in_=pPb[j], func=AF.Copy), reads=[bpP[j]], writes=[pb["bVTK"]])
        P.pool(lambda e, pb=pb, c0=c0: e.tensor_copy(out=pb["KTP"], in_=KT[:, :, c0:c0 + 128]), reads=bKT, writes=[pb["bKTP"]])
        P.pool(lambda e, pb=pb, j=j: e.tensor_tensor(out=pb["BV"], in0=pb["VTK"], in1=bc4(BETA[:, j, :]), op=ALU.mult),
               reads=[pb["bVTK"], bBG], writes=[pb["bBV"]])
        yield


def _gdn_rec(self, L, full):
    P = self.P
    H = 4
    g = lambda n: L[n]
    bk, bb = self.banks, self.bbank
    bc = self.bconst
    mask2, ident = self.mask2, self.ident
    maskL, bones, ch01, identb4 = g("maskL"), g("bones"), g("ch01"), g("identb4")
    PBUF, GG, LNB, BETA, bBG = g("PB"), g("GG"), g("LNB"), g("BETA"), g("bBG")
    QT, KT, VT, bQT, bKT, bVT = g("QT"), g("KT"), g("VT"), g("bQT"), g("bKT"), g("bVT")
    R, USB, bR, bUSB = g("R"), g("USB"), g("bR"), g("bUSB")
    S32, SBF, bS32, bSBF, sbf_i = g("S32"), g("SBF"), g("bS32"), g("bSBF"), g("sbf_i")
    O32, bO32 = g("O32"), g("bO32")
    evac, nextpj = g("evac"), g("nextpj")
    NP = NT
    pP = [bk[3 + j][:].rearrange("p (h t) -> p h t", h=H) for j in range(NP)]
    pPb = [bk[3 + j][:].bitcast(BF16)[:, 0:512].rearrange("p (h t) -> p h t", h=H) for j in range(NP)]
    bpP = [bb[3 + j] for j in range(NP)]
    ptr4 = bk[5][:].bitcast(BF16)[:, 0:512].rearrange("p (h t) -> p h t", h=H)
    pKS = bk[5][:].rearrange("p (h t) -> p h t", h=H)
    pU = bk[6][:].rearrange("p (h t) -> p h t", h=H)
    po = bk[7][:].rearrange("p (h t) -> p h t", h=H)

    def bc4(ap):
        return ap.unsqueeze(2).to_broadcast([128, H, 128])

    for j in range(NP):
        pb = PBUF[j]
        c0 = j * 128
        SC = pb["SC"]
        TT, bTT = pb["TT"], pb["bTT"]
        for half in range(2):
            r0 = half * 64
            cs = sbf_i[0]
            for h in range(H):
                P.pe(lambda e, h=h, pb=pb, cs=cs: e.matmul(pKS[:, h, :], lhsT=pb["KTP"][:, h, :], rhs=SBF[cs][:, h, :],
                                                           start=True, stop=True),
                     reads=[pb["bKTP"], bSBF[cs][h]], writes=[bb[5]])
            if full:
                for h in range(H):
                    P.pe(lambda e, pb=pb, h=h, cs=cs, r0=r0, half=half: e.matmul(
                        po[:, h, r0:r0 + 64], lhsT=SBF[cs][:, h, :], rhs=pb["QTG"][:, h, r0:r0 + 64],
                        start=(h == 0 and half == 0), stop=False, skip_group_check=True),
                        reads=[bSBF[cs][h], pb["bQTG"]], writes=[bb[7]])
            for h in range(H):
                P.dve(lambda e, pb=pb, h=h, r0=r0, SC=SC: e.scalar_tensor_tensor(
                    out=R[r0:r0 + 64, h, :], in0=pKS[r0:r0 + 64, h, :], scalar=SC[r0:r0 + 64, 4, h:h + 1],
                    in1=pb["BV"][r0:r0 + 64, h, :], op0=ALU.mult, op1=ALU.add),
                    reads=[bb[5], pb["bSC"], pb["bBV"]], writes=[bR])
            yield
            for h in range(H):
                P.pe(lambda e, h=h, r0=r0, TT=TT: e.matmul(pU[:, h, :], lhsT=TT[r0:r0 + 64, h, :], rhs=R[r0:r0 + 64, h, :],
                                                           start=True, stop=True),
                     reads=[bTT, bR], writes=[bb[6]])
            yield
            P.act(lambda e, r0=r0: e.activation(out=USB[r0:r0 + 64], in_=pU[r0:r0 + 64], func=AF.Copy),
                  reads=[bb[6]], writes=[bUSB])
            yield
            pS, bpS = bk[6][:], bb[6]
            pS4 = pS.rearrange("p (h t) -> p h t", h=H)
            for h in range(H):
                P.pe(lambda e, pb=pb, h=h, r0=r0, pS4=pS4: e.matmul(pS4[:, h, :], lhsT=pb["KHT"][r0:r0 + 64, h, :],
                                                                    rhs=USB[r0:r0 + 64, h, :], start=True, stop=True),
                     reads=[pb["bKHT"], bUSB], writes=[bpS])
            yield
            for h in range(H):
                P.dve(lambda e, h=h, half=half, SC=SC, pS4=pS4: e.scalar_tensor_tensor(
                    out=S32[:, h, :], in0=S32[:, h, :], scalar=SC[:, 6 + half, h:h + 1], in1=pS4[:, h, :],
                    op0=ALU.mult, op1=ALU.add), reads=[bS32[h], pb["bSC"], bpS], writes=[bS32[h]])
            yield
            nx = 1 - cs
            P.act(lambda e, nx=nx: e.activation(out=SBF[nx], in_=S32, func=AF.Copy), reads=bS32, writes=bSBF[nx])
            sbf_i[0] = nx
            yield
        if full:
            for h in range(H):
                P.pe(lambda e, pb=pb, h=h: e.matmul(po[:, h, :], lhsT=USB[:, h, :], rhs=pb["ATT"][:, h, :],
                                                    start=False, stop=True, skip_group_check=True),
                     reads=[bUSB, pb["bATT"]], writes=[bb[7]])
            P.act(lambda e, c0=c0: e.activation(out=O32[:, :, c0:c0 + 128], in_=po, func=AF.Copy),
                  reads=[bb[7]], writes=[bO32])


KB._gdn_prep = _gdn_prep
KB._gdn_rec = _gdn_rec


CAP = 384
NEXP = 32
NSLOT = NEXP * CAP
BIG = 1.0e4


def _spill(self, name, sb_ap, bufs, sb):
    P = self.P
    if name not in self.d:
        self.d[name] = self.nc.dram_tensor(name, [128, 4, self.nfull_tok], BF16, kind="Internal").ap()
        self.bspill = getattr(self, "bspill", {})
        self.bspill[name] = {}
    dst = self.d[name]
    tok0 = (sb - self.npre) * SBT
    b = Buf(name)
    self.bspill[name][sb - self.npre] = b
    P.dma(lambda e: e.dma_start(out=dst[:, :, tok0:tok0 + SBT], in_=sb_ap), reads=bufs, writes=[b])


KB.spill = _spill


def _pass3(self):
    nc, P, A = self.nc, self.P, self.A
    d = self.d
    H = 4
    for nm, shp in (("hg_up", [512, D]), ("gd_up", [512, D]), ("w_out", [D, D]), ("norm_ffn_g", [D]),
                    ("router_w", [D, 36]), ("router_b", [36])):
        self.din(nm, shp)
    ntile = self.nfull_tok // 128
    d["h2_s"] = nc.dram_tensor("h2_s", [self.nfull_tok, D], F32, kind="Internal").ap()
    self.bh2s = [Buf("h2_s%d" % i) for i in range(ntile)]
    self.bxbuf = []
    self.W12 = A.alloc((ntile, 2), F32)
    self.DST = A.alloc((ntile, 2), U32)
    self.bW12 = Buf("W12")
    self.bDST = Buf("DST")
    m0 = A.mark()
    W3 = A.alloc((KC, 2048), BF16)
    bW3 = [Buf() for _ in range(KC)]
    wv = d["w_in"].rearrange("(k p) n -> p k n", p=128)
    for k in range(KC):
        P.dma(lambda e, k=k: e.dma_start(out=W3[:, k, :], in_=wv[:, k, C_PA:C_PA + 2048]), writes=[bW3[k]], q="pool")
    HGUP = A.alloc((H, D), BF16)
    GDUP = A.alloc((H, D), BF16)
    WOUT = A.alloc((KC, D), BF16)
    bUP = Buf()
    bWO = [Buf() for _ in range(KC)]
    P.dma(lambda e: e.dma_start(out=HGUP, in_=d["hg_up"].rearrange("(h p) n -> p h n", p=128)), writes=[bUP], q="pool")
    P.dma(lambda e: e.dma_start(out=GDUP, in_=d["gd_up"].rearrange("(h p) n -> p h n", p=128)), writes=[bUP], q="pool")
    wo = d["w_out"].rearrange("(k p) n -> p k n", p=128)
    for k in range(KC):
        P.dma(lambda e, k=k: e.dma_start(out=WOUT[:, k, :], in_=wo[:, k, :]), writes=[bWO[k]], q="pool")
    WR = A.alloc((KC, 36), F32)
    RB = A.alloc((36,), F32)
    G2 = A.alloc((D,), F32)
    ECAP = A.alloc((NEXP,), F32)
    bpar = Buf("par3")
    P.dma(lambda e: e.dma_start(out=WR, in_=d["router_w"].rearrange("(k p) n -> p k n", p=128)), writes=[bpar])
    P.dma(lambda e: e.dma_start(out=RB, in_=d["router_b"].partition_broadcast(128)), writes=[bpar])
    P.dma(lambda e: e.dma_start(out=G2, in_=d["norm_ffn_g"].partition_broadcast(128)), writes=[bpar])
    self.load_gain("norm_mix_g")
    ecapi = A.alloc((NEXP,), I32)
    P.pool(lambda e: e.iota(ecapi, pattern=[[CAP, NEXP]], base=0, channel_multiplier=0), writes=[bpar])
    P.pool(lambda e: e.tensor_copy(out=ECAP, in_=ecapi), reads=[bpar], writes=[bpar])
    triS = A.alloc((128,), BF16)
    trif = A.alloc((128,), F32)
    bc = self.bconst
    P.pool(lambda e: e.memset(trif, 1.0), writes=[bc])
    P.pool(lambda e: e.affine_select(out=trif, in_=trif, pattern=[[1, 128]], compare_op=ALU.is_gt, fill=0.0,
                                     base=0, channel_multiplier=-1), reads=[bc], writes=[bc])
    P.pool(lambda e: e.tensor_copy(out=triS, in_=trif), reads=[bc], writes=[bc])
    BASE = A.alloc((NEXP,), F32)
    bBASE = Buf()
    P.pool(lambda e: e.tensor_copy(out=BASE, in_=ecapi), reads=[bpar], writes=[bBASE])

    xnTs = [A.alloc((KC, SBT), BF16) for _ in range(2)]
    bxnTs = [Buf(), Buf()]
    self._xfetched = set()
    use_fetch = hasattr(self, "bxns")
    SGs = [A.alloc((16, SBT), BF16) for _ in range(2)]
    bSGs = [[Buf() for _ in range(16)] for _ in range(2)]
    OAss = [A.alloc((H, SBT), BF16) for _ in range(2)]
    OBss = [A.alloc((H, SBT), BF16) for _ in range(2)]
    bOAss, bOBss = [Buf(), Buf()], [Buf(), Buf()]
    T1 = [A.alloc((SBT,), F32) for _ in range(2)]
    T2 = [A.alloc((SBT,), F32) for _ in range(2)]
    bT1 = [Buf(), Buf()]
    bT2 = [Buf(), Buf()]
    MGs = [A.alloc((KC, SBT), BF16) for _ in range(2)]
    bMGs = [[Buf() for _ in range(KC)] for _ in range(2)]
    XR = A.alloc((D,), F32)
    bXR = Buf()
    H2 = A.alloc((D,), F32)
    bH2 = Buf()
    JK = A.alloc((D,), BF16)
    SS2 = A.alloc((1,), F32)
    bSS2 = Buf()
    XF = A.alloc((D,), F32)
    XB = A.alloc((D,), BF16)
    bXF, bXB = Buf(), Buf()
    XFT = A.alloc((KC, 128), F32)
    bXFT = Buf()
    LG = A.alloc((36,), F32)
    ME = A.alloc((NEXP,), F32)
    SM = A.alloc((16,), F32)
    M8 = A.alloc((8,), F32)
    SEL1 = A.alloc((NEXP,), F32)
    SEL2 = A.alloc((NEXP,), F32)
    SELB = A.alloc((NEXP,), BF16)
    RK = A.alloc((NEXP,), F32)
    JK2 = A.alloc((NEXP,), F32)
    DF = A.alloc((2,), F32)
    brt = Buf("route")

    bk, bb = self.banks, self.bbank
    ptr = bk[0][:].bitcast(BF16).rearrange("p (k t) -> p k t", k=KC)
    pj = [bk[1][:], bk[2][:]]
    bpj = [bb[1], bb[2]]
    pup = [bk[3][:], bk[4][:]]
    pji = [0]

    def nextpj():
        i = pji[0] % 2
        pji[0] += 1
        return pj[i], bpj[i]

    pyi = [0]

    def nextpy():
        i = 5 + pyi[0] % 2
        pyi[0] += 1
        return bk[i][:], bb[i]

    oa_d, ob_d = d["oa_s"], d["ob_s"]
    def X3(sbi):
        sl = sbi % 2
        SG, bSG, OAs, OBs, bOAs, bOBs = SGs[sl], bSGs[sl], OAss[sl], OBss[sl], bOAss[sl], bOBss[sl]
        sb = self.npre + sbi
        tok0 = sbi * SBT
        xnT, bxnT = xnTs[sb % 2], bxnTs[sb % 2]
        if use_fetch:
            yield from self.xnt_fetch_gen(sb, self.nsb - 1, xnTs, bxnTs)
        else:
            yield from self.stage_a_gen(sb, xnT, bxnT, ptr, bb[0])
        P.dma(lambda e, tok0=tok0: e.dma_start(out=OAs, in_=oa_d[:, :, tok0:tok0 + SBT]),
              reads=[self.bspill["oa_s"][sbi]], writes=[bOAs])
        P.dma(lambda e, tok0=tok0: e.dma_start(out=OBs, in_=ob_d[:, :, tok0:tok0 + SBT]),
              reads=[self.bspill["ob_s"][sbi]], writes=[bOBs])
        for cb in range(16):
            p, bp = nextpj()
            for k in range(KC):
                P.pe(lambda e, k=k, p=p, cb=cb: e.matmul(p[:, 0:SBT], lhsT=W3[:, k, cb * 128:(cb + 1) * 128],
                                                         rhs=xnT[:, k, :], start=(k == 0), stop=(k == KC - 1)),
                     reads=[bW3[k], bxnT], writes=[bp])
            P.act(lambda e, cb=cb, p=p: e.activation(out=SG[:, cb, :], in_=p[:, 0:SBT], func=AF.Sigmoid),
                  reads=[bp], writes=[bSG[cb]])
            yield
    def Y3a(sbi):
        sl = sbi % 2
        SG, bSG, OAs, OBs, bOAs, bOBs = SGs[sl], bSGs[sl], OAss[sl], OBss[sl], bOAss[sl], bOBss[sl]
        sb = self.npre + sbi
        tok0 = sbi * SBT
        MG, bMG = MGs[sbi % 2], bMGs[sbi % 2]
        for cb in range(KC):
            i = cb % 2
            for h in range(H):
                P.pe(lambda e, h=h, cb=cb: e.matmul(pup[0][:, 0:SBT], lhsT=HGUP[:, h, cb * 128:(cb + 1) * 128],
                                                    rhs=OAs[:, h, :], start=(h == 0), stop=(h == H - 1)),
                     reads=[bUP, bOAs], writes=[bb[3]])
            for h in range(H):
                P.pe(lambda e, h=h, cb=cb: e.matmul(pup[1][:, 0:SBT], lhsT=GDUP[:, h, cb * 128:(cb + 1) * 128],
                                                    rhs=OBs[:, h, :], start=(h == 0), stop=(h == H - 1)),
                     reads=[bUP, bOBs], writes=[bb[4]])
            P.dve(lambda e, cb=cb, i=i: e.tensor_tensor(out=T1[i], in0=pup[0][:, 0:SBT], in1=SG[:, cb, :], op=ALU.mult),
                  reads=[bb[3], bSG[cb]], writes=[bT1[i]])
            P.dve(lambda e, cb=cb, i=i: e.tensor_tensor(out=T2[i], in0=pup[1][:, 0:SBT], in1=SG[:, 8 + cb, :], op=ALU.mult),
                  reads=[bb[4], bSG[8 + cb]], writes=[bT2[i]])
            P.pool(lambda e, cb=cb, i=i: e.tensor_tensor(out=MG[:, cb, :], in0=T1[i], in1=T2[i], op=ALU.add),
                   reads=[bT1[i], bT2[i]], writes=[bMG[cb]])
            yield
    def Y3b(sbi):
        sl = sbi % 2
        SG, bSG, OAs, OBs, bOAs, bOBs = SGs[sl], bSGs[sl], OAss[sl], OBss[sl], bOAss[sl], bOBss[sl]
        sb = self.npre + sbi
        tok0 = sbi * SBT
        MG, bMG = MGs[sbi % 2], bMGs[sbi % 2]
        for t in range(NT):
            gt = sbi * NT + t
            gtok = self.npre * SBT + gt * 128
            P.dma(lambda e, gtok=gtok: e.dma_start(out=XR, in_=d["xs"][gtok:gtok + 128, :]), writes=[bXR])
            for half in range(2):
                p, bp = nextpy()
                for k in range(KC):
                    P.pe(lambda e, k=k, p=p, t=t, half=half: e.matmul(
                        p, lhsT=MG[:, k, t * 128:(t + 1) * 128], rhs=WOUT[:, k, half * 512:(half + 1) * 512],
                        start=(k == 0), stop=(k == KC - 1)), reads=[bMG[k], bWO[k]], writes=[bp])
                P.dve(lambda e, p=p, half=half: e.tensor_tensor(out=H2[:, half * 512:(half + 1) * 512], in0=p,
                                                               in1=XR[:, half * 512:(half + 1) * 512], op=ALU.add),
                      reads=[bp, bXR], writes=[bH2])
                yield
            P.dma(lambda e, gt=gt: e.dma_start(out=d["h2_s"][gt * 128:(gt + 1) * 128, :], in_=H2),
                  reads=[bH2], writes=[self.bh2s[gt]])
            LL = dict(L0)
            LL['nextpj'] = nextpy
            yield from self._route_tile(LL, gt)
    def Y3(sbi):
        yield from Y3a(sbi)
        yield from Y3b(sbi)

    L0 = dict(locals())

    import os
    WTS3 = [int(v) for v in os.environ.get("IL_W3", "1,1").split(",")]

    def run_il(gens):
        gens = list(gens)
        while gens:
            for g_, w_ in list(gens):
                for _ in range(w_):
                    try:
                        next(g_)
                    except StopIteration:
                        gens.remove((g_, w_))
                        break

    n = self.nfull
    if LS >= 2:
        sx = [P._capture(X3(i)) for i in range(n)]
        sa = [P._capture(Y3a(i)) for i in range(n)]
        sb_ = [P._capture(Y3b(i)) for i in range(n)]
        P.merge_pipeline([sx, sa, sb_],
                         [lambda s_, dn: dn[1] >= s_ - 1,
                          lambda s_, dn: dn[0] >= s_ + 1 and dn[2] >= s_ - 1,
                          lambda s_, dn: dn[1] >= s_ + 1])
    else:
        for r in range(-1, n):
            gs = []
            if 0 <= r < n:
                gs.append((Y3(r), WTS3[0]))
            if 0 <= r + 1 < n:
                gs.append((X3(r + 1), WTS3[1]))
            if LS:
                P.merge_streams([g_ for g_, _w in gs])
            else:
                run_il(gs)
    A.reset(m0)
    P.barrier()


KB.pass3 = _pass3


def _route_tile(self, L, gt):
    P = self.P
    g = lambda n: L[n]
    bk, bb = self.banks, self.bbank
    bc = self.bconst
    H2, bH2, JK, SS2, bSS2 = g("H2"), g("bH2"), g("JK"), g("SS2"), g("bSS2")
    XF, XB, bXF, bXB, XFT, bXFT = g("XF"), g("XB"), g("bXF"), g("bXB"), g("XFT"), g("bXFT")
    G2, WR, RB, ECAP, bpar = g("G2"), g("WR"), g("RB"), g("ECAP"), g("bpar")
    LG, ME, SM, M8, SEL1, SEL2, SELB, RK, JK2, DF, brt = (g("LG"), g("ME"), g("SM"), g("M8"), g("SEL1"), g("SEL2"),
                                                           g("SELB"), g("RK"), g("JK2"), g("DF"), g("brt"))
    BASE, bBASE, triS = g("BASE"), g("bBASE"), g("triS")
    nextpj = g("nextpj")
    W12, DST = self.W12, self.DST
    P.act(lambda e: e.activation(out=JK, in_=H2, func=AF.Square, accum_out=SS2), reads=[bH2], writes=[bSS2])
    P.act(lambda e: e.activation(out=SM[:, 0:1], in_=SS2, func=AF.Ln, scale=1.0 / D, bias=EPS), reads=[bSS2], writes=[brt])
    P.act(lambda e: e.activation(out=SM[:, 0:1], in_=SM[:, 0:1], func=AF.Exp, scale=-0.5), reads=[brt], writes=[brt])
    P.dve(lambda e: e.scalar_tensor_tensor(out=XF, in0=H2, scalar=SM[:, 0:1], in1=G2, op0=ALU.mult, op1=ALU.mult),
          reads=[bH2, brt, bpar], writes=[bXF])
    P.pool(lambda e: e.tensor_copy(out=XB, in_=XF), reads=[bXF], writes=[bXB])
    yield
    for half in range(2):
        for kk in range(4):
            k = half * 4 + kk
            P.pe(lambda e, k=k, kk=kk, half=half: e.transpose(out=bk[7 * half][:, kk * 128:(kk + 1) * 128],
                                                              in_=XF[:, k * 128:(k + 1) * 128], identity=self.identf),
                 reads=[bXF, bc], writes=[bb[7 * half]])
    P.act(lambda e: e.activation(out=XFT[:, 0:4, :], in_=bk[0][:].rearrange("p (k t) -> p k t", k=4), func=AF.Copy),
          reads=[bb[0]], writes=[bXFT])
    P.dve(lambda e: e.tensor_copy(out=XFT[:, 4:8, :], in_=bk[7][:].rearrange("p (k t) -> p k t", k=4)),
          reads=[bb[7]], writes=[bXFT])
    yield
    p, bp = nextpj()
    for k in range(KC):
        P.pe(lambda e, k=k, p=p: e.matmul(p[:, 0:36], lhsT=XFT[:, k, :], rhs=WR[:, k, :], start=(k == 0), stop=(k == KC - 1)),
             reads=[bXFT, bpar], writes=[bp])
    P.dve(lambda e, p=p: e.tensor_tensor(out=LG, in0=p[:, 0:36], in1=RB, op=ALU.add), reads=[bp, bpar], writes=[brt])
    yield
    P.dve(lambda e: e.tensor_reduce(out=SM[:, 1:2], in_=LG[:, 0:4], axis=AX.X, op=ALU.max), reads=[brt], writes=[brt])
    P.dve(lambda e: e.tensor_scalar(out=SM[:, 2:3], in0=SM[:, 1:2], scalar1=-1.0, scalar2=None, op0=ALU.mult),
          reads=[brt], writes=[brt])
    P.act(lambda e: e.activation(out=JK2[:, 0:4], in_=LG[:, 0:4], func=AF.Exp, bias=SM[:, 2:3], accum_out=SM[:, 3:4]),
          reads=[brt], writes=[brt])
    P.dve(lambda e: e.reciprocal(out=SM[:, 4:5], in_=SM[:, 3:4]), reads=[brt], writes=[brt])
    yield
    P.dve(lambda e: e.tensor_scalar(out=SM[:, 12:16], in0=LG[:, 0:4], scalar1=SM[:, 1:2], scalar2=-1.0,
                                    op0=ALU.is_equal, op1=ALU.add), reads=[brt], writes=[brt])
    P.dve(lambda e: e.scalar_tensor_tensor(out=ME.rearrange("p (g j) -> p g j", g=4),
                                           in0=SM[:, 12:16].unsqueeze(2).to_broadcast([128, 4, 8]), scalar=BIG,
                                           in1=LG[:, 4:36].rearrange("p (g j) -> p g j", g=4),
                                           op0=ALU.mult, op1=ALU.add), reads=[brt], writes=[brt])
    P.dve(lambda e: e.max(out=M8, in_=ME), reads=[brt], writes=[brt])
    yield
    P.dve(lambda e: e.tensor_tensor(out=SM[:, 5:6], in0=M8[:, 1:2], in1=M8[:, 0:1], op=ALU.subtract), reads=[brt], writes=[brt])
    P.act(lambda e: e.activation(out=SM[:, 6:7], in_=SM[:, 5:6], func=AF.Exp), reads=[brt], writes=[brt])
    P.dve(lambda e: e.tensor_scalar(out=SM[:, 7:8], in0=SM[:, 6:7], scalar1=1.0, scalar2=None, op0=ALU.add),
          reads=[brt], writes=[brt])
    P.dve(lambda e: e.reciprocal(out=SM[:, 8:9], in_=SM[:, 7:8]), reads=[brt], writes=[brt])
    P.dve(lambda e, gt=gt: e.tensor_tensor(out=W12[:, gt, 0:1], in0=SM[:, 8:9], in1=SM[:, 4:5], op=ALU.mult),
          reads=[brt], writes=[self.bW12])
    P.dve(lambda e, gt=gt: e.tensor_tensor(out=W12[:, gt, 1:2], in0=SM[:, 4:5], in1=W12[:, gt, 0:1], op=ALU.subtract),
          reads=[brt, self.bW12], writes=[self.bW12])
    P.dve(lambda e: e.tensor_scalar(out=SEL1, in0=ME, scalar1=M8[:, 0:1], scalar2=None, op0=ALU.is_equal),
          reads=[brt], writes=[brt])
    P.dve(lambda e: e.tensor_scalar(out=SEL2, in0=ME, scalar1=M8[:, 1:2], scalar2=None, op0=ALU.is_equal),
          reads=[brt], writes=[brt])
    P.dve(lambda e: e.tensor_tensor(out=SELB, in0=SEL1, in1=SEL2, op=ALU.add), reads=[brt], writes=[brt])
    yield
    p2, bp2 = nextpj()
    P.pe(lambda e, p2=p2: e.matmul(p2[:, 0:32], lhsT=triS, rhs=SELB, start=True, stop=True), reads=[brt, bc], writes=[bp2])
    P.pe(lambda e, p2=p2: e.matmul(p2[:, 32:64], lhsT=self.ones_bf, rhs=SELB, start=True, stop=True),
         reads=[brt, bc], writes=[bp2])
    P.dve(lambda e, p2=p2: e.tensor_tensor(out=RK, in0=p2[:, 0:32], in1=BASE, op=ALU.add), reads=[bp2, bBASE], writes=[brt])
    P.dve(lambda e, p2=p2: e.tensor_tensor(out=BASE, in0=p2[:, 32:64], in1=BASE, op=ALU.add), reads=[bp2, bBASE], writes=[bBASE])
    P.dve(lambda e: e.scalar_tensor_tensor(out=JK2, in0=SEL1, scalar=1.0, in1=RK, op0=ALU.mult, op1=ALU.mult,
                                           accum_out=DF[:, 0:1]), reads=[brt], writes=[brt])
    P.dve(lambda e: e.scalar_tensor_tensor(out=JK2, in0=SEL2, scalar=1.0, in1=RK, op0=ALU.mult, op1=ALU.mult,
                                           accum_out=DF[:, 1:2]), reads=[brt], writes=[brt])
    P.dve(lambda e, gt=gt: e.tensor_copy(out=DST[:, gt, :], in_=DF), reads=[brt], writes=[self.bDST])
    yield
    xb = self.d["x_buf"]
    for k in range(2):
        bx = Buf("xbuf")
        self.bxbuf.append(bx)
        P.dma(lambda e, gt=gt, k=k: e.indirect_dma_start(
            out=xb, out_offset=bass.IndirectOffsetOnAxis(ap=DST[:, gt, k:k + 1], axis=0), in_=XB, in_offset=None),
            reads=[bXB, self.bDST] + self.bxz, writes=[bx], q="pool")


KB._route_tile = _route_tile


def _pass4(self):
    nc, P, A = self.nc, self.P, self.A
    d = self.d
    self.din("w_gate", [NEXP, D, 512])
    self.din("w_up", [NEXP, D, 512])
    self.din("w_down", [NEXP, 512, D])
    d["y_buf"] = nc.dram_tensor("y_buf", [NSLOT, D], BF16, kind="Internal").ap()
    self.bybuf = []
    m0 = A.mark()
    NB = CAP // 128
    WG = [A.alloc((KC, 512), BF16) for _ in range(2)]
    WU = [A.alloc((KC, 512), BF16) for _ in range(2)]
    WD = [A.alloc((4, D), BF16) for _ in range(2)]
    bWG = [Buf(), Buf()]
    bWU = [Buf(), Buf()]
    bWD = [Buf(), Buf()]
    XE = [A.alloc((NB, D), BF16) for _ in range(2)]
    bXE = [Buf(), Buf()]
    XET = [A.alloc((KC, CAP), BF16) for _ in range(2)]
    bXET = [Buf(), Buf()]
    SGT = [A.alloc((CAP,), F32) for _ in range(2)]
    bSGT = [Buf(), Buf()]
    HT = A.alloc((4, CAP), BF16)
    bHT = [Buf() for _ in range(4)]
    YS = [A.alloc((D,), BF16) for _ in range(2)]
    bYS = [Buf(), Buf()]
    bk, bb = self.banks, self.bbank
    ptr = bk[0][:].bitcast(BF16).rearrange("p (k t) -> p k t", k=KC)
    ident = self.ident
    bc = self.bconst
    xb, yb = d["x_buf"], d["y_buf"]
    cnt = [0]

    def bank(lo, n):
        i = lo + cnt[0] % n
        cnt[0] += 1
        return bk[i][:], bb[i]

    def load_w(e):
        i = e % 2
        P.dma(lambda eng, e=e, i=i: eng.dma_start(out=WG[i], in_=d["w_gate"][e].rearrange("(k p) n -> p k n", p=128)),
              writes=[bWG[i]], q="pool")
        P.dma(lambda eng, e=e, i=i: eng.dma_start(out=WU[i], in_=d["w_up"][e].rearrange("(k p) n -> p k n", p=128)),
              writes=[bWU[i]], q="pool")
        P.dma(lambda eng, e=e, i=i: eng.dma_start(out=WD[i], in_=d["w_down"][e].rearrange("(k p) n -> p k n", p=128)),
              writes=[bWD[i]], q="pool")

    def load_x(e):
        i = e % 2
        P.dma(lambda eng, e=e, i=i: eng.dma_start(out=XE[i], in_=xb[e * CAP:(e + 1) * CAP, :].rearrange("(b p) n -> p b n", p=128)),
              reads=self.bxbuf, writes=[bXE[i]])

    def transposes(e):
        i = e % 2
        for b in range(NB):
            pt, bpt = (ptr, bb[0]) if b % 2 == 0 else (ptr7, bb[7])
            for k in range(KC):
                P.pe(lambda eng, i=i, b=b, k=k, pt=pt: eng.transpose(out=pt[:, k, :], in_=XE[i][:, b, k * 128:(k + 1) * 128], identity=ident),
                     reads=[bXE[i], bc], writes=[bpt])
            if b % 2 == 0:
                P.act(lambda eng, b=b, i=i, pt=pt: eng.activation(out=XET[i][:, :, b * 128:(b + 1) * 128], in_=pt, func=AF.Copy),
                      reads=[bpt], writes=[bXET[i]])
            else:
                P.dve(lambda eng, b=b, i=i, pt=pt: eng.tensor_copy(out=XET[i][:, :, b * 128:(b + 1) * 128], in_=pt),
                      reads=[bpt], writes=[bXET[i]])

    ptr7 = bk[7][:].bitcast(BF16).rearrange("p (k t) -> p k t", k=KC)
    load_w(0)
    load_x(0)
    transposes(0)
    yi = 0
    for e in range(NEXP):
        i = e % 2
        if e + 1 < NEXP:
            load_w(e + 1)
            load_x(e + 1)
        for fc in range(4):
            pg, bpg = bk[1 + fc % 2][:], bb[1 + fc % 2]
            pu, bpu = bk[3 + fc % 2][:], bb[3 + fc % 2]
            for k in range(KC):
                P.pe(lambda eng, i=i, fc=fc, k=k, pg=pg: eng.matmul(pg[:, 0:CAP], lhsT=WG[i][:, k, fc * 128:(fc + 1) * 128],
                                                                    rhs=XET[i][:, k, :], start=(k == 0), stop=(k == KC - 1)),
                     reads=[bWG[i], bXET[i]], writes=[bpg])
            for k in range(KC):
                P.pe(lambda eng, i=i, fc=fc, k=k, pu=pu: eng.matmul(pu[:, 0:CAP], lhsT=WU[i][:, k, fc * 128:(fc + 1) * 128],
                                                                    rhs=XET[i][:, k, :], start=(k == 0), stop=(k == KC - 1)),
                     reads=[bWU[i], bXET[i]], writes=[bpu])
            j = fc % 2
            P.act(lambda eng, pg=pg, j=j: eng.activation(out=SGT[j], in_=pg[:, 0:CAP], func=AF.Silu), reads=[bpg], writes=[bSGT[j]])
            P.dve(lambda eng, pu=pu, j=j, fc=fc: eng.tensor_tensor(out=HT[:, fc, :], in0=pu[:, 0:CAP], in1=SGT[j], op=ALU.mult),
                  reads=[bpu, bSGT[j]], writes=[bHT[fc]])
        if e + 1 < NEXP:
            transposes(e + 1)
        for b in range(NB):
            ys, bys = YS[yi % 2], bYS[yi % 2]
            yi += 1
            for half in range(2):
                pd, bpd = bk[5 + half][:], bb[5 + half]
                for fc in range(4):
                    P.pe(lambda eng, i=i, b=b, fc=fc, half=half, pd=pd: eng.matmul(
                        pd, lhsT=HT[:, fc, b * 128:(b + 1) * 128], rhs=WD[i][:, fc, half * 512:(half + 1) * 512],
                        start=(fc == 0), stop=(fc == 3)), reads=[bHT[fc], bWD[i]], writes=[bpd])
                if half == 0:
                    P.act(lambda eng, pd=pd, ys=ys: eng.activation(out=ys[:, 0:512], in_=pd, func=AF.Copy), reads=[bpd], writes=[bys])
                else:
                    P.dve(lambda eng, pd=pd, ys=ys: eng.tensor_copy(out=ys[:, 512:1024], in_=pd), reads=[bpd], writes=[bys])
            r0 = e * CAP + b * 128
            by = Buf("ybuf")
            self.bybuf.append(by)
            P.dma(lambda eng, r0=r0, ys=ys: eng.dma_start(out=yb[r0:r0 + 128, :], in_=ys), reads=[bys], writes=[by])
    A.reset(m0)
    P.barrier()


def _pass5(self):
    nc, P, A = self.nc, self.P, self.A
    d = self.d
    self.din("final_norm_g", [D])
    out = self.dout("out", [self.nfull_tok, D], F32)
    m0 = A.mark()
    ntile = self.nfull_tok // 128
    FG = A.alloc((D,), F32)
    bFG = Buf()
    P.dma(lambda e: e.dma_start(out=FG, in_=d["final_norm_g"].partition_broadcast(128)), writes=[bFG])
    NB5 = 4
    Y1 = [A.alloc((D,), BF16) for _ in range(NB5)]
    Y2 = [A.alloc((D,), BF16) for _ in range(NB5)]
    HH = [A.alloc((D,), F32) for _ in range(NB5)]
    OT = [A.alloc((D,), F32) for _ in range(NB5)]
    bY1, bY2, bHH, bOT = ([Buf() for _ in range(NB5)], [Buf() for _ in range(NB5)], [Buf() for _ in range(NB5)],
                          [Buf() for _ in range(NB5)])
    JK = A.alloc((D,), BF16)
    SS = A.alloc((ntile,), F32)
    bSS = Buf()
    yb = d["y_buf"]
    for gt in range(ntile):
        i = gt % NB5
        P.dma(lambda e, gt=gt, i=i: e.indirect_dma_start(
            out=Y1[i], out_offset=None, in_=yb, in_offset=bass.IndirectOffsetOnAxis(ap=self.DST[:, gt, 0:1], axis=0)),
            reads=self.bybuf + [self.bDST], writes=[bY1[i]], q="pool")
        P.dma(lambda e, gt=gt, i=i: e.indirect_dma_start(
            out=Y2[i], out_offset=None, in_=yb, in_offset=bass.IndirectOffsetOnAxis(ap=self.DST[:, gt, 1:2], axis=0)),
            reads=self.bybuf + [self.bDST], writes=[bY2[i]], q="pool")
        P.dma(lambda e, gt=gt, i=i: e.dma_start(out=HH[i], in_=d["h2_s"][gt * 128:(gt + 1) * 128, :]),
              reads=[self.bh2s[gt]], writes=[bHH[i]])
        P.dve(lambda e, gt=gt, i=i: e.scalar_tensor_tensor(out=HH[i], in0=Y1[i], scalar=self.W12[:, gt, 0:1], in1=HH[i],
                                                          op0=ALU.mult, op1=ALU.add),
              reads=[bY1[i], bHH[i], self.bW12], writes=[bHH[i]])
        P.dve(lambda e, gt=gt, i=i: e.scalar_tensor_tensor(out=HH[i], in0=Y2[i], scalar=self.W12[:, gt, 1:2], in1=HH[i],
                                                          op0=ALU.mult, op1=ALU.add),
              reads=[bY2[i], bHH[i], self.bW12], writes=[bHH[i]])
        P.act(lambda e, gt=gt, i=i: e.activation(out=JK, in_=HH[i], func=AF.Square, accum_out=SS[:, gt:gt + 1]),
              reads=[bHH[i]], writes=[bSS])
        P.act(lambda e, gt=gt: e.activation(out=SS[:, gt:gt + 1], in_=SS[:, gt:gt + 1], func=AF.Ln, scale=1.0 / D, bias=EPS),
              reads=[bSS], writes=[bSS])
        P.act(lambda e, gt=gt: e.activation(out=SS[:, gt:gt + 1], in_=SS[:, gt:gt + 1], func=AF.Exp, scale=-0.5),
              reads=[bSS], writes=[bSS])
        P.dve(lambda e, gt=gt, i=i: e.scalar_tensor_tensor(out=OT[i], in0=HH[i], scalar=SS[:, gt:gt + 1], in1=FG,
                                                          op0=ALU.mult, op1=ALU.mult),
              reads=[bHH[i], bSS, bFG], writes=[bOT[i]])
        P.dma(lambda e, gt=gt, i=i: e.dma_start(out=out[gt * 128:(gt + 1) * 128, :], in_=OT[i]), reads=[bOT[i]], out=True)
    A.reset(m0)


KB.pass4 = _pass4
KB.pass5 = _pass5


NPRE_SB = 17
NFULL_SB = 16
_NC_CACHE = {}


def _build_full():
    if "nc" not in _NC_CACHE:
        kb = KB(NPRE_SB, NFULL_SB)
        kb.setup()
        kb.pass1()
        kb.pass2()
        kb.pass3()
        kb.pass4()
        kb.pass5()
        _NC_CACHE["nc"] = kb.finish()
    return _NC_CACHE["nc"]


def kernel(x, meta_tokens, hg_lb_logits, norm_mix_g, w_in, gd_conv_w, gd_A_log, gd_dt_bias, hg_norm_g, gd_norm_g,
           hg_up, gd_up, w_out, norm_ffn_g, router_group_w, router_group_b, router_expert_w, router_expert_b,
           w_gate, w_up, w_down, final_norm_g):
    f32 = np.float32
    c = lambda a: np.ascontiguousarray(np.asarray(a, dtype=f32))
    x = c(x)
    meta = c(meta_tokens)
    B, S, _ = x.shape
    half = S // 2
    npre_tok = NPRE_SB * SBT
    ntok = (NPRE_SB + NFULL_SB) * SBT
    nmeta = meta.shape[0]
    shared = {
        "w_in": c(w_in[0]),
        "norm_mix_g": c(norm_mix_g[0]),
        "hg_lb": c(np.asarray(hg_lb_logits, f32).reshape(2, 4, 128).transpose(2, 0, 1)),
        "hg_norm_g": c(hg_norm_g[0]),
        "conv_wT": c(np.asarray(gd_conv_w[0], f32).reshape(4, 12, 128).transpose(2, 0, 1)),
        "gd_A_log": c(gd_A_log[0]),
        "gd_dt_bias": c(gd_dt_bias[0]),
        "gd_norm_g": c(gd_norm_g[0]),
        "hg_up": c(hg_up[0]),
        "gd_up": c(gd_up[0]),
        "w_out": c(w_out[0]),
        "norm_ffn_g": c(norm_ffn_g[0]),
        "router_w": c(np.concatenate([np.asarray(router_group_w[0], f32), np.asarray(router_expert_w[0], f32)], axis=1)),
        "router_b": c(np.concatenate([np.asarray(router_group_b[0], f32), np.asarray(router_expert_b[0], f32)])),
        "w_gate": c(w_gate[0]),
        "w_up": c(w_up[0]),
        "w_down": c(w_down[0]),
        "final_norm_g": c(final_norm_g),
    }
    in_maps = []
    for core in range(2 * B):
        b, hf = core // 2, core % 2
        xs = np.zeros((ntok, D), f32)
        if hf == 0:
            xs[npre_tok - nmeta:npre_tok] = meta
            xs[npre_tok:] = x[b, 0:half]
        else:
            xs[npre_tok - half - nmeta:npre_tok - half] = meta
            xs[npre_tok - half:] = x[b]
        m = dict(shared)
        m["xs"] = xs
        in_maps.append(m)
    nc = _build_full()
    res = run_bass_kernel_spmd(nc, in_maps, core_ids=list(range(2 * B)))
    out = np.empty((B, S, D), f32)
    for core in range(2 * B):
        b, hf = core // 2, core % 2
        out[b, hf * half:(hf + 1) * half] = np.asarray(res.results[core]["out"], dtype=f32)
    return out

LCOST_TABLE = {('dve', 393): 0.041, ('act', 393): 0.03, ('pool', 393): 0.051, ('pool', 307): 0.019, ('pool', 429): 0.166, ('pool', 553): 0.64, ('pool', 309): 0.019, ('pool', 555): 0.622, ('pool', 629): 0.172, ('pool', 630): 0.154, ('dve', 309): 0.024, ('act', 307): 0.019, ('act', 567): 0.744, ('act', 309): 0.019, ('act', 486): 0.553, ('act', 493): 0.354, ('act', 496): 0.121, ('dve', 307): 0.024, ('dve', 502): 1.239, ('pe', 507): 0.126, ('dve', 510): 0.627, ('pe', 636): 0.147, ('act', 658): 0.56, ('dve', 689): 0.357, ('pe', 676): 0.229, ('dve', 693): 0.694, ('act', 680): 0.687, ('act', 696): 0.523, ('dve', 699): 0.596, ('act', 707): 0.508, ('act', 711): 0.102, ('dve', 712): 0.599, ('pe', 743): 0.135, ('dve', 746): 0.405, ('pe', 760): 0.094, ('dve', 764): 0.348, ('act', 770): 0.351, ('act', 664): 0.541, ('act', 669): 0.383, ('dve', 717): 0.911, ('dve', 721): 0.249, ('act', 723): 0.333, ('act', 726): 0.118, ('act', 729): 0.305, ('dve', 732): 0.6, ('dve', 735): 1.226, ('dve', 739): 0.602, ('pe', 780): 0.081, ('pe', 794): 0.116, ('dve', 783): 0.669, ('pe', 799): 0.206, ('dve', 803): 0.33, ('act', 808): 0.353, ('pe', 814): 0.115, ('act', 818): 0.575, ('act', 825): 0.333, ('pe', 829): 0.351, ('act', 831): 0.354, ('act', 833): 0.389, ('dve', 835): 0.414, ('dve', 837): 0.461, ('dve', 952): 0.286, ('pe', 1066): 0.149, ('pool', 1058): 0.157, ('pool', 1059): 0.154, ('pool', 1061): 0.264, ('act', 1078): 0.563, ('dve', 1080): 0.567, ('pe', 1107): 0.019, ('pe', 1135): 0.136, ('act', 1110): 0.19, ('act', 1113): 0.181, ('act', 1114): 0.181, ('dve', 1115): 0.179, ('act', 1117): 0.125, ('dve', 1118): 0.358, ('act', 1120): 0.181, ('act', 1121): 0.181, ('act', 1139): 0.41, ('dve', 1122): 0.126, ('pool', 1151): 0.587, ('act', 1142): 0.37, ('pe', 1154): 0.298, ('act', 1156): 0.508, ('act', 1158): 0.402, ('pool', 1161): 0.721, ('pe', 1289): 0.17, ('act', 1292): 0.236, ('pool', 1098): 0.145, ('dve', 1294): 0.161, ('act', 1296): 0.162, ('pool', 1311): 1.826, ('dve', 1298): 0.45, ('dve', 1300): 0.53, ('act', 1302): 0.164, ('act', 1303): 0.162, ('dve', 1304): 0.129, ('pe', 1316): 0.139, ('dve', 1323): 0.678, ('act', 1335): 0.368, ('pe', 1361): 0.069, ('pool', 1346): 1.266, ('dve', 1366): 0.646, ('pe', 1385): 0.111, ('pe', 1403): 0.107, ('pool', 1394): 1.141, ('pe', 1415): 0.109, ('pe', 1426): 0.112, ('dve', 1434): 0.679, ('pool', 1452): 1.831, ('pe', 1444): 0.102, ('dve', 1446): 0.68, ('pe', 1449): 0.128, ('act', 1451): 0.673, ('pool', 1453): 1.023, ('pe', 1494): 0.134, ('dve', 1504): 0.348, ('pe', 1510): 0.148, ('act', 1514): 0.683, ('pe', 1520): 0.15, ('dve', 1525): 0.279, ('act', 1530): 0.701, ('act', 1128): 0.547, ('dve', 1326): 0.598, ('act', 1328): 0.597, ('act', 1340): 0.346, ('pool', 1350): 1.332, ('dve', 1354): 0.335, ('pe', 1373): 0.146, ('dve', 1379): 0.689, ('pe', 1499): 0.064, ('pe', 1535): 0.13, ('act', 1538): 0.63, ('act', 1188): 0.334, ('pe', 1192): 0.339, ('act', 1194): 0.341, ('act', 1196): 0.36, ('dve', 1197): 0.532, ('dve', 1199): 0.7, ('pool', 1594): 0.633, ('pe', 1705): 0.126, ('pool', 1604): 0.637, ('act', 1708): 0.426, ('pe', 1721): 0.143, ('pe', 1725): 0.124, ('dve', 1728): 0.413, ('dve', 1730): 0.391, ('pool', 1732): 0.728, ('pe', 1749): 0.284, ('dve', 1752): 0.692, ('act', 1822): 0.574, ('act', 1823): 0.419, ('act', 1824): 0.203, ('dve', 1825): 1.284, ('pe', 1833): 0.231, ('pool', 1827): 3.58, ('act', 1836): 0.687, ('dve', 1838): 0.692, ('pe', 1843): 0.196, ('dve', 1845): 0.196, ('dve', 1848): 0.157, ('dve', 1849): 0.154, ('act', 1851): 0.316, ('dve', 1853): 0.164, ('dve', 1855): 0.229, ('dve', 1857): 0.193, ('dve', 1862): 0.171, ('dve', 1864): 0.137, ('act', 1865): 0.203, ('dve', 1866): 0.13, ('dve', 1868): 0.163, ('dve', 1869): 0.226, ('dve', 1871): 0.16, ('dve', 1873): 0.218, ('dve', 1875): 0.219, ('dve', 1877): 0.309, ('pe', 1881): 0.183, ('pe', 1882): 0.027, ('dve', 1884): 0.169, ('dve', 1885): 0.193, ('dve', 1886): 0.053, ('dve', 1888): 0.094, ('dve', 1890): 0.145, ('pool', 1896): 1.129, ('pool', 1945): 1.059, ('pool', 1947): 1.056, ('pool', 1949): 0.9, ('pe', 1962): 0.083, ('act', 1965): 1.113, ('dve', 1968): 0.692, ('pe', 1986): 0.165, ('act', 1994): 0.46, ('pe', 1990): 0.164, ('dve', 1995): 0.532, ('pe', 2006): 0.267, ('act', 2010): 0.673, ('dve', 2012): 0.692, ('pool', 2045): 1.115, ('pool', 2048): 1.108, ('dve', 2053): 1.285, ('pool', 293): 2.81, ('dve', 2056): 1.284, ('act', 2059): 0.575, ('act', 2061): 0.419, ('act', 2063): 0.203, ('dve', 2065): 1.285, ('act', 293): 0.115, ('dve', 293): 0.655}
Prog.LCOST = LCOST_TABLE
```
